# Optimizing a Trainium2 kernel written in Bass

```python
import numpy as np
import jax
import jax.numpy as jnp
from jax import lax

D_MODEL = 1024
BATCH = 4
SEQ = 4096
DEPTH = 1

NSA_HEADS = 8
NSA_HEAD_DIM = 64
NSA_KV_GROUPS = 2
NSA_HEADS_PER_GROUP = NSA_HEADS // NSA_KV_GROUPS
NSA_KV_DIM = NSA_KV_GROUPS * NSA_HEAD_DIM
CMP_BLOCK = 32
CMP_STRIDE = 16
CMP_HIDDEN = 256
SLC_BLOCK = 64
SLC_TOPK = 16
WINDOW = 512
SLC_Q_BLOCK = 64
WIN_Q_BLOCK = 128
RET_HEADS = 4
RET_DK = 128
RET_DV = 256
RET_V_DIM = RET_HEADS * RET_DV
RET_CHUNK = 128
MOE_GROUPS = 4
EXPERTS_PER_GROUP = 8
N_EXPERTS = MOE_GROUPS * EXPERTS_PER_GROUP
D_FF_EXPERT = 512
INNER_TOP_K = 2
ROPE_THETA = 10000.0
LN_EPS = 1e-5
GN_EPS = 1e-5
NEG_INF = -1e30
DEEPNORM_ALPHA = (2.0 * DEPTH) ** 0.25
DEEPNORM_BETA = (8.0 * DEPTH) ** -0.25

IN_LAYOUT = (
    ("nsa_q", NSA_HEADS * NSA_HEAD_DIM),
    ("cmp_k", NSA_KV_DIM), ("cmp_v", NSA_KV_DIM),
    ("slc_k", NSA_KV_DIM), ("slc_v", NSA_KV_DIM),
    ("win_k", NSA_KV_DIM), ("win_v", NSA_KV_DIM),
    ("nsa_gate", 3 * NSA_HEADS),
    ("ret_q", RET_HEADS * RET_DK), ("ret_k", RET_HEADS * RET_DK),
    ("ret_v", RET_V_DIM), ("ret_gate", RET_V_DIM),
    ("merge_gate", 2 * D_MODEL),
)
IN_DIM = sum(w for _, w in IN_LAYOUT)
VALUE_SLOTS = ("cmp_v", "slc_v", "win_v", "ret_v")

kernel_name = "hybrid_nsa_retention_hmoe_deepnorm"


def split_projection(h):
    out = {}
    off = 0
    for name, width in IN_LAYOUT:
        out[name] = h[..., off:off + width]
        off += width
    return out


def layer_norm(x, g, b):
    x32 = x.astype(jnp.float32)
    mu = jnp.mean(x32, -1, keepdims=True)
    var = jnp.mean(jnp.square(x32 - mu), -1, keepdims=True)
    return ((x32 - mu) * lax.rsqrt(var + LN_EPS) * g + b).astype(x.dtype)


def rope(x, pos):
    d = x.shape[-1]
    half = d // 2
    inv_freq = ROPE_THETA ** (-jnp.arange(half, dtype=jnp.float32) * 2.0 / d)
    ang = pos.astype(jnp.float32)[:, None] * inv_freq[None, :]
    cos = jnp.cos(ang)[:, None, :]
    sin = jnp.sin(ang)[:, None, :]
    x32 = x.astype(jnp.float32)
    x1, x2 = x32[..., :half], x32[..., half:]
    return jnp.concatenate([x1 * cos - x2 * sin, x1 * sin + x2 * cos], -1).astype(x.dtype)


def masked_softmax(s, mask):
    s = jnp.where(mask, s.astype(jnp.float32), NEG_INF)
    return jnp.where(mask, jax.nn.softmax(s, axis=-1), 0.0)


def compress_blocks(t, pos_emb, w1, b1, w2, b2):
    B, T, G, hd = t.shape
    n_cmp = (T - CMP_BLOCK) // CMP_STRIDE + 1
    tok = np.arange(n_cmp)[:, None] * CMP_STRIDE + np.arange(CMP_BLOCK)[None, :]
    blk = t[:, tok] + pos_emb[None, None, :, None, :]
    blk = blk.transpose(0, 1, 3, 2, 4).reshape(B, n_cmp, G, CMP_BLOCK * hd)
    return jax.nn.gelu(blk @ w1 + b1) @ w2 + b2


def nsa_attention(q, k_c, v_c, k_s, v_s, k_w, v_w, gate_logits, cmp_k_params, cmp_v_params):
    B, T = q.shape[:2]
    G, HPG, hd = NSA_KV_GROUPS, NSA_HEADS_PER_GROUP, NSA_HEAD_DIM
    pos = jnp.arange(T)
    qg = (rope(q, pos) * hd ** -0.5).reshape(B, T, G, HPG, hd).transpose(0, 2, 3, 1, 4)
    k_s = rope(k_s, pos)
    k_w = rope(k_w, pos)

    n_cmp = (T - CMP_BLOCK) // CMP_STRIDE + 1
    cmp_start = np.arange(n_cmp) * CMP_STRIDE
    cmp_end = cmp_start + CMP_BLOCK - 1
    k_cmp = rope(compress_blocks(k_c, *cmp_k_params), jnp.asarray(cmp_end))
    v_cmp = compress_blocks(v_c, *cmp_v_params)
    s_cmp = jnp.einsum('bghtd,bngd->bghtn', qg, k_cmp)
    mask_cmp = jnp.asarray(cmp_end)[None, :] <= pos[:, None]
    p_cmp = masked_softmax(s_cmp, mask_cmp)
    o_cmp = jnp.einsum('bghtn,bngd->bghtd', p_cmp.astype(v_cmp.dtype), v_cmp)

    n_sel = T // SLC_BLOCK
    k_top = min(SLC_TOPK, n_sel)
    sel_start = np.arange(n_sel) * SLC_BLOCK
    overlap = np.clip(np.minimum(cmp_start[None, :] + CMP_BLOCK, sel_start[:, None] + SLC_BLOCK)
                      - np.maximum(cmp_start[None, :], sel_start[:, None]), 0, None)
    overlap = jnp.asarray(overlap.astype(np.float32) / CMP_STRIDE)
    importance = jnp.einsum('bghtn,sn->bgts', p_cmp, overlap)
    blk_t = pos // SLC_BLOCK
    j = jnp.arange(n_sel)
    forced = (j[None, :] == 0) | (j[None, :] == blk_t[:, None]) | (j[None, :] == blk_t[:, None] - 1)
    causal = j[None, :] <= blk_t[:, None]
    score = jnp.where(forced, jnp.float32(1e9), jnp.where(causal, importance, jnp.float32(-1e9)))
    _, sel_idx = lax.top_k(score, k_top)

    k_blocks = k_s.reshape(B, n_sel, SLC_BLOCK, G, hd).transpose(0, 3, 1, 2, 4)
    v_blocks = v_s.reshape(B, n_sel, SLC_BLOCK, G, hd).transpose(0, 3, 1, 2, 4)
    nq = T // SLC_Q_BLOCK
    q_b = qg.reshape(B, G, HPG, nq, SLC_Q_BLOCK, hd).transpose(3, 0, 1, 2, 4, 5)
    idx_b = sel_idx.reshape(B, G, nq, SLC_Q_BLOCK, k_top).transpose(2, 0, 1, 3, 4)
    t_b = pos.reshape(nq, SLC_Q_BLOCK)
    bi = jnp.arange(B)[:, None, None, None]
    gi = jnp.arange(G)[None, :, None, None]

    def slc_block(args):
        qb, ib, tb = args
        kg = k_blocks[bi, gi, ib]
        vg = v_blocks[bi, gi, ib]
        s = jnp.einsum('bghqd,bgqskd->bghqsk', qb, kg)
        key_pos = ib[..., None] * SLC_BLOCK + jnp.arange(SLC_BLOCK)
        mask = (key_pos <= tb[:, None, None])[:, :, None]
        p = masked_softmax(s.reshape(B, G, HPG, SLC_Q_BLOCK, -1),
                           mask.reshape(B, G, 1, SLC_Q_BLOCK, -1)).reshape(s.shape)
        return jnp.einsum('bghqsk,bgqskd->bghqd', p.astype(vg.dtype), vg)

    o_slc = lax.map(slc_block, (q_b, idx_b, t_b))
    o_slc = o_slc.transpose(1, 2, 3, 0, 4, 5).reshape(B, G, HPG, T, hd)

    k_wp = jnp.pad(k_w, ((0, 0), (WINDOW, 0), (0, 0), (0, 0)))
    v_wp = jnp.pad(v_w, ((0, 0), (WINDOW, 0), (0, 0), (0, 0)))
    nqw = T // WIN_Q_BLOCK

    def win_block(n):
        start = n * WIN_Q_BLOCK
        qb = lax.dynamic_slice_in_dim(qg, start, WIN_Q_BLOCK, axis=3)
        kb = lax.dynamic_slice_in_dim(k_wp, start, WINDOW + WIN_Q_BLOCK, axis=1)
        vb = lax.dynamic_slice_in_dim(v_wp, start, WINDOW + WIN_Q_BLOCK, axis=1)
        s = jnp.einsum('bghqd,bkgd->bghqk', qb, kb)
        tq = start + jnp.arange(WIN_Q_BLOCK)
        tk = start - WINDOW + jnp.arange(WINDOW + WIN_Q_BLOCK)
        mask = (tk[None, :] <= tq[:, None]) & (tq[:, None] - tk[None, :] < WINDOW) & (tk[None, :] >= 0)
        p = masked_softmax(s, mask)
        return jnp.einsum('bghqk,bkgd->bghqd', p.astype(vb.dtype), vb)

    o_win = lax.map(win_block, jnp.arange(nqw))
    o_win = o_win.transpose(1, 2, 3, 0, 4, 5).reshape(B, G, HPG, T, hd)

    gates = jax.nn.sigmoid(gate_logits.astype(jnp.float32)).reshape(B, T, G, HPG, 3).transpose(0, 2, 3, 1, 4)
    o = gates[..., 0:1] * o_cmp + gates[..., 1:2] * o_slc + gates[..., 2:3] * o_win
    return o.transpose(0, 3, 1, 2, 4).reshape(B, T, NSA_HEADS * hd).astype(q.dtype)


def retention(q, k, v, gn_g, gn_b):
    B, T = q.shape[:2]
    C = RET_CHUNK
    N = T // C
    pos = jnp.arange(T)
    q = rope(q, pos).astype(jnp.float32)
    k = (rope(k, pos).astype(jnp.float32)) * RET_DK ** -0.5
    v = v.astype(jnp.float32)
    qc = q.reshape(B, N, C, RET_HEADS, RET_DK).transpose(1, 0, 3, 2, 4)
    kc = k.reshape(B, N, C, RET_HEADS, RET_DK).transpose(1, 0, 3, 2, 4)
    vc = v.reshape(B, N, C, RET_HEADS, RET_DV).transpose(1, 0, 3, 2, 4)
    gamma = 1.0 - 2.0 ** (-5.0 - jnp.arange(RET_HEADS, dtype=jnp.float32))
    log_g = jnp.log(gamma)
    i = jnp.arange(C, dtype=jnp.float32)
    diff = i[:, None] - i[None, :]
    decay_intra = jnp.where(diff >= 0, jnp.exp(jnp.maximum(diff, 0.0) * log_g[:, None, None]), 0.0)
    xi = jnp.exp((i + 1.0) * log_g[:, None])
    zeta = jnp.exp((C - 1.0 - i) * log_g[:, None])
    chunk_decay = jnp.exp(C * log_g)

    scores = jnp.einsum('nbhcd,nbhmd->nbhcm', qc, kc) * decay_intra
    intra = jnp.einsum('nbhcm,nbhme->nbhce', scores, vc)

    def step(R, xs):
        qn, kn, vn = xs
        cross = jnp.einsum('bhcd,bhde->bhce', qn, R) * xi[None, :, :, None]
        R_new = R * chunk_decay[None, :, None, None] + jnp.einsum('bhmd,bhme->bhde', kn * zeta[None, :, :, None], vn)
        return R_new, cross

    R0 = jnp.zeros((B, RET_HEADS, RET_DK, RET_DV), jnp.float32)
    _, cross = lax.scan(step, R0, (qc, kc, vc))
    o = (intra + cross).transpose(1, 0, 3, 2, 4).reshape(B, T, RET_HEADS, RET_DV)
    mu = jnp.mean(o, -1, keepdims=True)
    var = jnp.mean(jnp.square(o - mu), -1, keepdims=True)
    o = (o - mu) * lax.rsqrt(var + GN_EPS)
    o = o * gn_g.astype(jnp.float32).reshape(RET_HEADS, RET_DV) + gn_b.astype(jnp.float32).reshape(RET_HEADS, RET_DV)
    return o.reshape(B, T, RET_V_DIM)


def hier_moe(xt, rg_w, rg_b, ri_w, ri_b, w_gate, w_up, w_down):
    group_logits = (xt @ rg_w).astype(jnp.float32) + rg_b.astype(jnp.float32)
    p_group = jax.nn.softmax(group_logits, axis=-1)
    g_prob, g_idx = lax.top_k(p_group, 1)
    g_prob, g_idx = g_prob[:, 0], g_idx[:, 0]
    inner_all = jnp.einsum('md,gde->mge', xt, ri_w).astype(jnp.float32) + ri_b.astype(jnp.float32)
    inner = jnp.take_along_axis(inner_all, g_idx[:, None, None], axis=1)[:, 0]
    e_logit, e_idx = lax.top_k(inner, INNER_TOP_K)
    w = jax.nn.softmax(e_logit, axis=-1) * g_prob[:, None]
    expert_id = g_idx[:, None] * EXPERTS_PER_GROUP + e_idx
    combine = jnp.sum(jax.nn.one_hot(expert_id, N_EXPERTS, dtype=jnp.float32) * w[..., None], axis=1)
    out = jnp.zeros(xt.shape, jnp.float32)
    for g in range(MOE_GROUPS):
        sl = slice(g * EXPERTS_PER_GROUP, (g + 1) * EXPERTS_PER_GROUP)
        hg = jax.nn.silu(jnp.einsum('md,edf->mef', xt, w_gate[sl])) * jnp.einsum('md,edf->mef', xt, w_up[sl])
        hg = hg * combine[:, sl, None].astype(hg.dtype)
        out = out + jnp.einsum('mef,efd->md', hg, w_down[sl])
    return out


def setup_inputs(seed: int = 0) -> dict:
    key = jax.random.key(seed)
    ks = jax.random.split(key, 32)
    f32 = jnp.float32
    L, D, hd = DEPTH, D_MODEL, NSA_HEAD_DIM

    def nrm(k, shape, scale):
        return jax.random.normal(k, shape, f32) * scale

    col_scale = np.concatenate([np.full((w,), DEEPNORM_BETA if name in VALUE_SLOTS else 1.0, np.float32)
                                for name, w in IN_LAYOUT])
    cmp_in = CMP_BLOCK * hd
    return {
        "x": nrm(ks[0], (BATCH, SEQ, D), 1.0),
        "w_in": nrm(ks[1], (L, D, IN_DIM), D ** -0.5) * jnp.asarray(col_scale),
        "cmp_pos_k": nrm(ks[2], (L, CMP_BLOCK, hd), 0.1),
        "cmp_k_w1": nrm(ks[3], (L, cmp_in, CMP_HIDDEN), cmp_in ** -0.5),
        "cmp_k_b1": nrm(ks[4], (L, CMP_HIDDEN), 0.01),
        "cmp_k_w2": nrm(ks[5], (L, CMP_HIDDEN, hd), CMP_HIDDEN ** -0.5),
        "cmp_k_b2": nrm(ks[6], (L, hd), 0.01),
        "cmp_pos_v": nrm(ks[7], (L, CMP_BLOCK, hd), 0.1),
        "cmp_v_w1": nrm(ks[8], (L, cmp_in, CMP_HIDDEN), cmp_in ** -0.5),
        "cmp_v_b1": nrm(ks[9], (L, CMP_HIDDEN), 0.01),
        "cmp_v_w2": nrm(ks[10], (L, CMP_HIDDEN, hd), CMP_HIDDEN ** -0.5),
        "cmp_v_b2": nrm(ks[11], (L, hd), 0.01),
        "ret_gn_g": 1.0 + nrm(ks[12], (L, RET_V_DIM), 0.05),
        "ret_gn_b": nrm(ks[13], (L, RET_V_DIM), 0.01),
        "w_up_attn": nrm(ks[14], (L, NSA_HEADS * hd, D), (NSA_HEADS * hd) ** -0.5 * DEEPNORM_BETA),
        "w_up_ret": nrm(ks[15], (L, RET_V_DIM, D), RET_V_DIM ** -0.5 * DEEPNORM_BETA),
        "w_out": nrm(ks[16], (L, D, D), D ** -0.5 * DEEPNORM_BETA),
        "ln1_g": 1.0 + nrm(ks[17], (L, D), 0.05),
        "ln1_b": nrm(ks[18], (L, D), 0.01),
        "router_group_w": nrm(ks[19], (L, D, MOE_GROUPS), D ** -0.5),
        "router_group_b": nrm(ks[20], (L, MOE_GROUPS), 0.01),
        "router_inner_w": nrm(ks[21], (L, MOE_GROUPS, D, EXPERTS_PER_GROUP), D ** -0.5),
        "router_inner_b": nrm(ks[22], (L, MOE_GROUPS, EXPERTS_PER_GROUP), 0.01),
        "expert_w_gate": nrm(ks[23], (L, N_EXPERTS, D, D_FF_EXPERT), D ** -0.5),
        "expert_w_up": nrm(ks[24], (L, N_EXPERTS, D, D_FF_EXPERT), D ** -0.5 * DEEPNORM_BETA),
        "expert_w_down": nrm(ks[25], (L, N_EXPERTS, D_FF_EXPERT, D), D_FF_EXPERT ** -0.5 * DEEPNORM_BETA),
        "ln2_g": 1.0 + nrm(ks[26], (L, D), 0.05),
        "ln2_b": nrm(ks[27], (L, D), 0.01),
    }


def reference(x, w_in, cmp_pos_k, cmp_k_w1, cmp_k_b1, cmp_k_w2, cmp_k_b2,
              cmp_pos_v, cmp_v_w1, cmp_v_b1, cmp_v_w2, cmp_v_b2,
              ret_gn_g, ret_gn_b, w_up_attn, w_up_ret, w_out, ln1_g, ln1_b,
              router_group_w, router_group_b, router_inner_w, router_inner_b,
              expert_w_gate, expert_w_up, expert_w_down, ln2_g, ln2_b):
    B, T, D = x.shape
    G, hd = NSA_KV_GROUPS, NSA_HEAD_DIM
    for l in range(DEPTH):
        p = split_projection(x @ w_in[l])
        o_attn = nsa_attention(
            p["nsa_q"].reshape(B, T, NSA_HEADS, hd),
            p["cmp_k"].reshape(B, T, G, hd), p["cmp_v"].reshape(B, T, G, hd),
            p["slc_k"].reshape(B, T, G, hd), p["slc_v"].reshape(B, T, G, hd),
            p["win_k"].reshape(B, T, G, hd), p["win_v"].reshape(B, T, G, hd),
            p["nsa_gate"],
            (cmp_pos_k[l], cmp_k_w1[l], cmp_k_b1[l], cmp_k_w2[l], cmp_k_b2[l]),
            (cmp_pos_v[l], cmp_v_w1[l], cmp_v_b1[l], cmp_v_w2[l], cmp_v_b2[l]))
        o_ret = retention(p["ret_q"].reshape(B, T, RET_HEADS, RET_DK),
                          p["ret_k"].reshape(B, T, RET_HEADS, RET_DK),
                          p["ret_v"].reshape(B, T, RET_HEADS, RET_DV),
                          ret_gn_g[l], ret_gn_b[l])
        o_ret = (o_ret * jax.nn.silu(p["ret_gate"].astype(jnp.float32))).astype(x.dtype)
        gate = jax.nn.sigmoid(p["merge_gate"].astype(jnp.float32)).reshape(B, T, 2, D)
        merged = gate[:, :, 0] * (o_attn @ w_up_attn[l]) + gate[:, :, 1] * (o_ret @ w_up_ret[l])
        mix = merged.astype(x.dtype) @ w_out[l]
        x = layer_norm(DEEPNORM_ALPHA * x + mix.astype(x.dtype), ln1_g[l], ln1_b[l])
        moe = hier_moe(x.reshape(B * T, D), router_group_w[l], router_group_b[l],
                       router_inner_w[l], router_inner_b[l],
                       expert_w_gate[l], expert_w_up[l], expert_w_down[l]).reshape(B, T, D)
        x = layer_norm(DEEPNORM_ALPHA * x + moe.astype(x.dtype), ln2_g[l], ln2_b[l])
    return x
```

```python
import numpy as np
import ml_dtypes
import concourse.bass as bass
import concourse.mybir as mybir
from concourse.bass_utils import run_bass_kernel_spmd
from contextlib import ExitStack

F32 = mybir.dt.float32
BF16 = mybir.dt.bfloat16
AF = mybir.ActivationFunctionType
ALU = mybir.AluOpType
NPBF = ml_dtypes.bfloat16

T = 4096
D = 1024
TO = 2048
NEGM = -30000.0
LN_EPS = 1e-5
ALPHA = 2.0 ** 0.25
DEBUG = {}


class Op:
    __slots__ = ("eng", "fn", "reads", "writes", "dma", "sem", "deps", "needs_inc", "idx", "id", "extra")

    def __init__(self, eng, fn, reads, writes, dma, sem):
        self.eng = eng
        self.fn = fn
        self.reads = tuple(reads)
        self.writes = tuple(writes)
        self.dma = dma
        self.sem = sem
        self.deps = []
        self.needs_inc = dma
        self.idx = 0
        self.extra = ()


class _Rec:
    def __getattr__(self, name):
        return lambda *a, **k: (name, a, k)


_REC = _Rec()


class Prog:
    ENGS = ("pe", "act", "dve", "pool", "sp")

    def __init__(self, nc, same_eng_sync=True):
        self.nc = nc
        self.ops = []
        self.same_eng_sync = same_eng_sync
        self.last_by_sem = {}
        self.psum_keys = set()

    def add(self, eng, fn, reads=(), writes=(), dma=False, sem=None):
        lim = DEBUG.get("max_ops")
        self.nadd = getattr(self, "nadd", -1) + 1
        if (lim is not None and self.nadd >= lim and sem not in ("dbg", "fin")) or self.nadd in DEBUG.get("skip", ()):
            return Op(eng, None, reads, writes, dma, sem)
        if dma and sem is None:
            sem = "dma_" + str(writes[0])
        if not dma:
            sem = "eng_" + eng
        op = Op(eng, fn(_REC), reads, writes, dma, sem)
        if DEBUG.get("trace_ops"):
            print(len(self.ops), eng, op.fn[0], reads, writes)
        op.id = len(self.ops)
        self.ops.append(op)
        self.last_by_sem[sem] = op
        return op

    def pe(self, fn, reads=(), writes=()):
        return self.add("pe", fn, reads, writes)

    def act(self, fn, reads=(), writes=()):
        return self.add("act", fn, reads, writes)

    def dve(self, fn, reads=(), writes=()):
        return self.add("dve", fn, reads, writes)

    def pool(self, fn, reads=(), writes=()):
        return self.add("pool", fn, reads, writes)

    def dma(self, out, in_, reads=(), writes=(), sem=None, q="sp", **kw):
        return self.add(q, lambda e: e.dma_start(out=out, in_=in_, **kw), reads, writes, dma=True, sem=sem)

    def barrier(self):
        lasts = list(self.last_by_sem.values())
        for eng in self.ENGS:
            op = self.add(eng, lambda e: e.nop())
            op.extra = tuple(lasts)
        self.last_by_sem = {k: v for k, v in self.last_by_sem.items() if k.startswith("eng_")}

    def analyze(self):
        state = {}
        for op in self.ops:
            deps = set(op.extra)
            for k in op.reads:
                st = state.get(k)
                if st:
                    deps.update(st[0])
                    if k in self.psum_keys:
                        deps.update(r for r in st[1] if r.eng != op.eng)
            for k in op.writes:
                st = state.get(k)
                if st is None:
                    st = state[k] = [[], []]
                if st[1]:
                    deps.update(st[1])
                    deps.update(st[0])
                    st[0] = [op]
                    st[1] = []
                else:
                    same_group = op.dma and all(w.dma and w.sem == op.sem for w in st[0])
                    if same_group:
                        st[0].append(op)
                    else:
                        deps.update(st[0])
                        st[0] = [op]
            for k in op.reads:
                st = state.get(k)
                if st is None:
                    st = state[k] = [[], []]
                st[1].append(op)
            deps.discard(op)
            red = {}
            for d in deps:
                if (not d.dma) and (not op.dma) and d.eng == op.eng:
                    if op.eng == "pe" or not self.same_eng_sync:
                        continue
                cur = red.get(d.sem)
                if cur is None or d.id > cur.id:
                    red[d.sem] = d
            op.deps = list(red.values())
            for d in op.deps:
                d.needs_inc = True
        cnt = {}
        for op in self.ops:
            if op.needs_inc:
                cnt[op.sem] = cnt.get(op.sem, 0) + 1
                op.idx = cnt[op.sem]
        self.sem_names = sorted(cnt.keys())
        return cnt

    def emit(self, stack):
        nc = self.nc
        cnt = self.analyze()
        sems = {}
        for name in self.sem_names:
            sems[name] = stack.enter_context(nc.semaphore(name))
        block = stack.enter_context(nc.Block())
        per_eng = {e: [o for o in self.ops if o.eng == e] for e in self.ENGS}

        def run(eng_obj, ops):
            known = {}
            for op in ops:
                for d in op.deps:
                    val = d.idx * (16 if d.dma else 1)
                    if known.get(d.sem, 0) < val:
                        eng_obj.wait_ge(sems[d.sem], val)
                        known[d.sem] = val
                name, a, k = op.fn
                inst = getattr(eng_obj, name)(*a, **k)
                if op.needs_inc:
                    inst.then_inc(sems[op.sem], 16 if op.dma else 1)

        @block.sync
        def _(e):
            run(e, per_eng["sp"])

        @block.tensor
        def _(e):
            run(e, per_eng["pe"])

        @block.scalar
        def _(e):
            run(e, per_eng["act"])

        @block.vector
        def _(e):
            run(e, per_eng["dve"])

        @block.gpsimd
        def _(e):
            run(e, per_eng["pool"])
        return cnt


class Ring:
    def __init__(self, items):
        self.items = items
        self.i = 0

    def next(self):
        it = self.items[self.i % len(self.items)]
        self.i += 1
        return it


def tile_w(w):
    K, N = w.shape
    return np.ascontiguousarray(w.reshape(K // 128, 128, N).transpose(1, 0, 2).reshape(128, -1))


def rope_tabs(pos, d, scale):
    half = d // 2
    inv = 10000.0 ** (-np.arange(half, dtype=np.float64) * 2.0 / d)
    ang = pos.astype(np.float64)[None, :] * inv[:, None]
    cos = np.cos(ang) * scale
    sin = np.sin(ang) * scale
    reps = 128 // half
    return (np.tile(cos, (reps, 1)).astype(np.float32), np.tile(sin, (reps, 1)).astype(np.float32))


def rot_lhsT(d):
    half = d // 2
    Pm = np.zeros((128, 128), np.float32)
    for blk in range(128 // d):
        o = blk * d
        for m in range(half):
            Pm[o + m, o + m + half] = -1.0
            Pm[o + m + half, o + m] = 1.0
    return np.ascontiguousarray(Pm.T)


_CONST_CACHE = {}


def make_consts(c):
    if c in _CONST_CACHE:
        return _CONST_CACHE[c]
    cs = {}
    own_pos = np.concatenate([np.arange(128) + (2 * i + c) * 128 for i in range(16)])
    allpos = np.arange(T)
    cs["cosK"], cs["sinK"] = rope_tabs(allpos, 64, 1.0)
    cs["cosQ"], cs["sinQ"] = rope_tabs(own_pos, 64, 0.125)
    cs["cosRK"], cs["sinRK"] = rope_tabs(allpos, 128, 128.0 ** -0.5)
    cs["cosRQ"], cs["sinRQ"] = rope_tabs(own_pos, 128, 1.0)
    cend = np.arange(256) * 16 + 31
    cs["cosC"], cs["sinC"] = rope_tabs(cend, 64, 1.0)
    cs["pt64"] = rot_lhsT(64).astype(NPBF)
    cs["pt128"] = rot_lhsT(128).astype(NPBF)
    cs["identb"] = np.eye(128, dtype=np.float32).astype(NPBF)
    E = np.zeros((128, 32, 128), np.float32)
    for j in range(32):
        for k in range(128):
            E[2 * j + k // 64, j, k] = 1.0
            E[64 + 2 * j + k // 64, j, k] = 1.0
    cs["eall"] = E.reshape(128, -1).astype(NPBF)
    wm = np.zeros((128, 6, 128), np.float32)
    kk = np.arange(128)[:, None]
    tt = np.arange(128)[None, :]
    for r in range(6):
        dj = (r - 4) - c
        tk = dj * 128 + kk
        ok = (tk <= tt) & (tt - tk < 512)
        wm[:, r, :] = np.where(ok, 0.0, NEGM)
    cs["wmask"] = wm.reshape(128, -1).astype(NPBF)
    cm = np.zeros((128, 2, 16, 128), np.float32)
    for a in range(2):
        for i in range(16):
            G = 2 * i + c
            n = a * 128 + kk
            t = G * 128 + tt
            cm[:, a, i, :] = np.where(16 * n + 31 <= t, 0.0, NEGM)
    cs["cmpmask"] = cm.reshape(128, -1).astype(NPBF)
    cstart = np.arange(255) * 16
    sstart = np.arange(64) * 64
    ov = np.clip(np.minimum(cstart[None, :] + 32, sstart[:, None] + 64) - np.maximum(cstart[None, :], sstart[:, None]), 0, None) / 16.0
    ovT = np.zeros((256, 64), np.float32)
    ovT[:255] = ov.T
    cs["ovT"] = np.ascontiguousarray(ovT.reshape(2, 128, 64).transpose(1, 0, 2).reshape(128, -1)).astype(NPBF)
    tkm = np.zeros((128, 16, 64), np.float32)
    tkb = np.zeros((128, 16, 64), np.float32)
    for i in range(16):
        G = 2 * i + c
        for p in range(128):
            bt = (G * 128 + p) // 64
            for s in range(64):
                if s == 0:
                    tkb[p, i, s] = 1e9
                elif s == bt:
                    tkb[p, i, s] = 2e9
                elif s == bt - 1:
                    tkb[p, i, s] = 3e9
                elif s <= bt:
                    tkm[p, i, s] = 1.0
                else:
                    tkb[p, i, s] = -1e9 - 1e6 * s
    cs["tkm"] = tkm.reshape(128, -1)
    cs["tkb"] = tkb.reshape(128, -1)
    gam = 1.0 - 2.0 ** (-5.0 - np.arange(4, dtype=np.float64))
    lg = np.log(gam)
    m = np.arange(256)[:, None]
    cq = np.arange(128)[None, :]
    qq = 128 * c + cq
    Dc = np.zeros((128, 2, 4, 128), np.float32)
    for h in range(4):
        dd = np.where(qq >= m, np.exp(np.maximum(qq - m, 0) * lg[h]), 0.0)
        Dc[:, :, h, :] = dd.reshape(2, 128, 128).transpose(1, 0, 2)
    cs["Dc"] = Dc.reshape(128, -1)
    xi = np.zeros((128, 4, 128), np.float32)
    for h in range(4):
        xi[:, h, :] = np.exp((qq + 1.0) * lg[h])
    cs["xi"] = xi.reshape(128, -1)
    zt = np.zeros((128, 2, 4), np.float32)
    for h in range(4):
        zt[:, :, h] = np.exp((255.0 - np.arange(256)) * lg[h]).reshape(2, 128).T
    cs["zeta"] = zt.reshape(128, -1)
    cs["_decay256"] = [float(np.exp(256.0 * lg[h])) for h in range(4)]
    _CONST_CACHE[c] = cs
    return cs


CONST_SHAPES = None


def build_program(n_stage=6, debug=()):
    nc = bass.Bass("TRN2", target_bir_lowering=False)
    cs0 = make_consts(0)
    dram = {}

    def din(name, shape, dt=F32):
        dram[name] = nc.dram_tensor(name, list(shape), dt, kind="ExternalInput").ap()
        return dram[name]

    xT = din("xT", [1024, T])
    xTo = din("xTo", [1024, TO])
    xo = din("xo", [TO, 1024])
    w1t = din("w1t", [128, 8 * 1304])
    w4t = din("w4t", [128, 8 * 2048])
    wmgt = din("wmgt", [128, 8 * 2048])
    cw1 = {kv: din("cw1" + kv, [128, 32 * 256]) for kv in "kv"}
    cpos = {kv: din("cpos" + kv, [128, 32]) for kv in "kv"}
    cb1 = {kv: din("cb1" + kv, [128, 2]) for kv in "kv"}
    cw2k = din("cw2k", [128, 2 * 128])
    cw2v = din("cw2v", [128, 2 * 64])
    cb2k = din("cb2k", [128, 1])
    cb2v = din("cb2v", [64])
    gng8 = din("gng8", [128, 8])
    gnb8 = din("gnb8", [128, 8])
    wgtt = din("wgtt", [128, 8 * 1024])
    wat = din("wat", [128, 4 * 1024])
    wrt = din("wrt", [128, 8 * 1024])
    wot = din("wot", [128, 8 * 1024])
    ln1g = din("ln1g", [1024])
    ln1b = din("ln1b", [1024])
    ln2g = din("ln2g", [1024])
    ln2b = din("ln2b", [1024])
    wrout = din("wrout", [128, 8 * 36])
    brout = din("brout", [36])
    wexp = din("wexp", [32, 128, 12288])
    cdr = {}
    for k, v in cs0.items():
        if k.startswith("_"):
            continue
        cdr[k] = din("c_" + k, v.shape, BF16 if v.dtype == NPBF else F32)
    out = nc.dram_tensor("out", [TO, 1024], F32, kind="ExternalOutput").ap()
    dbg_out = {}

    decay256 = cs0["_decay256"]

    with ExitStack() as G:
        P = Prog(nc)

        def sb(stack, name, shape, dt):
            return stack.enter_context(nc.sbuf_tensor(name, list(shape), dt))

        def ps(stack, name, shape, dt=F32):
            P.psum_keys.add(name)
            ncol = 512 if dt == F32 else 1024
            full = stack.enter_context(nc.psum_tensor(name, [128, ncol], dt))
            n = 1
            for d_ in shape[1:]:
                n *= d_
            v = full[0:shape[0], 0:n]
            if len(shape) == 3:
                v = v.rearrange("p (a b) -> p a b", a=shape[1])
            return v

        def dump(name, ap, shape, key):
            if name in debug:
                t = nc.dram_tensor("dbg_" + name, list(shape), ap.dtype, kind="ExternalOutput").ap()
                dbg_out[name] = t
                P.dma(t, ap, reads=[key], writes=["dbg_" + name], sem="dbg")

        identb = sb(G, "identb", [128, 128], BF16)
        P.dma(identb[:], cdr["identb"], writes=["identb"])
        wst = sb(G, "wst", [128, 4096], F32)
        cast_rr = [0]

        def load_cast(dst_ap, src_ap, n, dst_key, shape3=None):
            o = 0
            while o < n:
                m = min(4096, n - o)
                P.dma(wst[:, 0:m], src_ap[:, o:o + m], writes=["wst"])
                d = dst_ap[:, o:o + m]
                if cast_rr[0] % 2 == 0:
                    P.act(lambda e, d=d, m=m: e.copy(out=d, in_=wst[:, 0:m]), ["wst"], [dst_key])
                else:
                    P.dve(lambda e, d=d, m=m: e.tensor_copy(out=d, in_=wst[:, 0:m]), ["wst"], [dst_key])
                cast_rr[0] += 1
                o += m

        x1T = sb(G, "x1T", [128, 8, TO], BF16)
        wst3 = wst[:].rearrange("p (k n) -> p k n", k=8)
        A_ = ExitStack()
        oattnT = sb(A_, "oattnT", [128, 4, TO], BF16)

        with ExitStack() as SN:
            QT = sb(SN, "QT", [128, 4, TO], BF16)
            slckT = sb(SN, "slckT", [128, T], BF16)
            winkT = sb(SN, "winkT", [128, T], BF16)
            slcv1 = sb(SN, "slcv1", [128, 32, 2, 65], BF16)
            winv1 = sb(SN, "winv1", [128, 32, 2, 65], BF16)
            gates = sb(SN, "gates", [128, 16, 24], F32)
            kcmpT = sb(SN, "kcmpT", [128, 256], BF16)
            vcmp1 = sb(SN, "vcmp1", [128, 2, 2, 65], BF16)
            pt64 = sb(SN, "pt64", [128, 128], BF16)
            P.dma(pt64[:], cdr["pt64"], writes=["pt64"])
            P.dve(lambda e: e.memset(slcv1[:].rearrange("p a g d -> p (a g d)"), 1.0), [], ["slcv1"])
            P.dve(lambda e: e.memset(winv1[:].rearrange("p a g d -> p (a g d)"), 1.0), [], ["winv1"])
            P.dve(lambda e: e.memset(kcmpT[:], 0.0), [], ["kcmpT"])
            P.dve(lambda e: e.memset(vcmp1[:].rearrange("p a g d -> p (a g d)"), 0.0), [], ["vcmp1"])
            P.dve(lambda e: e.memset(vcmp1[:, :, :, 64:65], 1.0), [], ["vcmp1"])

            with ExitStack() as S12:
                cmpT = {"k": sb(S12, "cmpkT", [128, T], BF16), "v": sb(S12, "cmpvT", [128, T], BF16)}
                with ExitStack() as S1:
                    Wn = sb(S1, "Wn", [128, 8, 1304], BF16)
                    load_cast(Wn[:].rearrange("p k n -> p (k n)"), w1t, 8 * 1304, "Wn")
                    xb = [sb(S1, "xb%d" % i, [128, 8, 512], BF16) for i in range(2)]
                    tabs = [sb(S1, "tab%d" % i, [128, 2, 512], F32) for i in range(2)]
                    ybf = [sb(S1, "ybf%d" % i, [128, 512], BF16) for i in range(2)]
                    t1 = [sb(S1, "t1_%d" % i, [128, 512], F32) for i in range(2)]
                    t2 = [sb(S1, "t2_%d" % i, [128, 512], F32) for i in range(2)]
                    pj = [ps(S1, "pj%d" % i, [128, 512]) for i in range(3)]
                    prot = [ps(S1, "prot%d" % i, [128, 512]) for i in range(2)]
                    pv = [ps(S1, "pv%d" % i, [128, 256]) for i in range(2)]
                    pjr = Ring(list(range(3)))
                    rr = Ring(list(range(2)))
                    pvr = Ring(list(range(2)))
                    xTv = xT.rearrange("(k p) t -> p k t", p=128)
                    xTov = xTo.rearrange("(k p) t -> p k t", p=128)

                    def load_x(src_view, c0, n, slot):
                        P.dma(wst3[:, :, 0:n], src_view[:, :, c0:c0 + n], writes=["wst"])
                        P.act(lambda e: e.copy(out=xb[slot][:, 0:4, 0:n], in_=wst3[:, 0:4, 0:n]), ["wst"], ["xb%d" % slot])
                        P.dve(lambda e: e.tensor_copy(out=xb[slot][:, 4:8, 0:n], in_=wst3[:, 4:8, 0:n]), ["wst"], ["xb%d" % slot])

                    def proj_fm(col0, slot, n=512):
                        pi = pjr.next()
                        for k in range(8):
                            P.pe(lambda e, k=k, pi=pi: e.matmul(pj[pi][:, 0:n], lhsT=Wn[:, k, col0:col0 + 128], rhs=xb[slot][:, k, 0:n],
                                                                 start=(k == 0), stop=(k == 7)), ["Wn", "xb%d" % slot], ["pj%d" % pi])
                        return pi

                    def rope_fm(pi, tslot, dst_ap, dst_key, ptm, ptkey, n=512, src=None, srckey=None):
                        r = rr.next()
                        srcap = pj[pi][:, 0:n] if src is None else src
                        sk = ("pj%d" % pi) if srckey is None else srckey
                        P.act(lambda e: e.copy(out=ybf[r][:, 0:n], in_=srcap), [sk], ["ybf%d" % r])
                        P.pe(lambda e: e.matmul(prot[r][:, 0:n], lhsT=ptm[:], rhs=ybf[r][:, 0:n], start=True, stop=True),
                             [ptkey, "ybf%d" % r], ["prot%d" % r])
                        P.dve(lambda e: e.tensor_tensor(out=t1[r][:, 0:n], in0=srcap, in1=tabs[tslot][:, 0, 0:n], op=ALU.mult),
                              [sk, "tab%d" % tslot], ["t1_%d" % r])
                        P.dve(lambda e: e.tensor_tensor(out=t2[r][:, 0:n], in0=prot[r][:, 0:n], in1=tabs[tslot][:, 1, 0:n], op=ALU.mult),
                              ["prot%d" % r, "tab%d" % tslot], ["t2_%d" % r])
                        P.pool(lambda e: e.tensor_tensor(out=dst_ap, in0=t1[r][:, 0:n], in1=t2[r][:, 0:n], op=ALU.add),
                               ["t1_%d" % r, "t2_%d" % r], [dst_key])

                    for ch in range(8):
                        slot = ch % 2
                        c0 = ch * 512
                        load_x(xTv, c0, 512, slot)
                        P.dma(tabs[slot][:, 0, :], cdr["cosK"][:, c0:c0 + 512], writes=["tab%d" % slot])
                        P.dma(tabs[slot][:, 1, :], cdr["sinK"][:, c0:c0 + 512], writes=["tab%d" % slot])
                        for col0, kv in ((512, "k"), (640, "v")):
                            pi = proj_fm(col0, slot)
                            P.act(lambda e, pi=pi, kv=kv: e.copy(out=cmpT[kv][:, c0:c0 + 512], in_=pj[pi][:]), ["pj%d" % pi], ["cmp" + kv + "T"])
                        for col0, dst, dk in ((768, slckT, "slckT"), (896, winkT, "winkT")):
                            pi = proj_fm(col0, slot)
                            rope_fm(pi, slot, dst[:, c0:c0 + 512], dk, pt64, "pt64")
                        for tt in range(4):
                            vi = pvr.next()
                            for k in range(8):
                                P.pe(lambda e, k=k, vi=vi, tt=tt: e.matmul(pv[vi][:], lhsT=xb[slot][:, k, tt * 128:(tt + 1) * 128], rhs=Wn[:, k, 1024:1280],
                                                                            start=(k == 0), stop=(k == 7)), ["Wn", "xb%d" % slot], ["pv%d" % vi])
                            tg = ch * 4 + tt
                            P.act(lambda e, vi=vi, tg=tg: e.copy(out=slcv1[:, tg, :, 0:64], in_=pv[vi][:, 0:128].rearrange("p (g d) -> p g d", g=2)),
                                  ["pv%d" % vi], ["slcv1"])
                            P.dve(lambda e, vi=vi, tg=tg: e.tensor_copy(out=winv1[:, tg, :, 0:64], in_=pv[vi][:, 128:256].rearrange("p (g d) -> p g d", g=2)),
                                  ["pv%d" % vi], ["winv1"])
                    for oc in range(4):
                        slot = oc % 2
                        c0 = oc * 512
                        load_x(xTov, c0, 512, slot)
                        P.dma(tabs[slot][:, 0, :], cdr["cosQ"][:, c0:c0 + 512], writes=["tab%d" % slot])
                        P.dma(tabs[slot][:, 1, :], cdr["sinQ"][:, c0:c0 + 512], writes=["tab%d" % slot])
                        for hh in range(4):
                            pi = proj_fm(hh * 128, slot)
                            rope_fm(pi, slot, QT[:, hh, c0:c0 + 512], "QT", pt64, "pt64")
                        for tt in range(4):
                            vi = pvr.next()
                            for k in range(8):
                                P.pe(lambda e, k=k, vi=vi, tt=tt: e.matmul(pv[vi][:, 0:24], lhsT=xb[slot][:, k, tt * 128:(tt + 1) * 128], rhs=Wn[:, k, 1280:1304],
                                                                            start=(k == 0), stop=(k == 7)), ["Wn", "xb%d" % slot], ["pv%d" % vi])
                            tg = oc * 4 + tt
                            P.act(lambda e, vi=vi, tg=tg: e.activation(out=gates[:, tg, :], in_=pv[vi][:, 0:24], func=AF.Sigmoid), ["pv%d" % vi], ["gates"])
                    dump("QT", QT[:].rearrange("p a t -> p (a t)"), [128, 4 * TO], "QT")
                    dump("slckT", slckT[:], [128, T], "slckT")
                    dump("cmpkT", cmpT["k"][:], [128, T], "cmpkT")
                    dump("slcv1", slcv1[:].rearrange("p a g d -> p (a g d)"), [128, 32 * 130], "slcv1")
                    dump("gates", gates[:].rearrange("p a g -> p (a g)"), [128, 16 * 24], "gates")
                P.barrier()
                if n_stage >= 2:
                    with ExitStack() as S2:
                        w1b = sb(S2, "w1b", [128, 32, 256], BF16)
                        posT = sb(S2, "posT", [128, 32], F32)
                        posTb = sb(S2, "posTb", [128, 32], BF16)
                        b1 = sb(S2, "b1", [128, 2], F32)
                        bias1 = sb(S2, "bias1", [128, 2], F32)
                        w2kf = sb(S2, "w2kf", [128, 2, 128], F32)
                        w2k = sb(S2, "w2k", [128, 2, 128], BF16)
                        w2vf = sb(S2, "w2vf", [128, 2, 64], F32)
                        w2v = sb(S2, "w2v", [128, 2, 64], BF16)
                        b2k = sb(S2, "b2k", [128, 1], F32)
                        b2v = sb(S2, "b2v", [128, 64], F32)
                        tabC = sb(S2, "tabC", [128, 2, 256], F32)
                        h1 = sb(S2, "h1", [128, 2, 256], BF16)
                        xg = sb(S2, "xg", [128, 256], F32)
                        ug = sb(S2, "ug", [128, 256], F32)
                        sg_ = sb(S2, "sg_", [128, 256], F32)
                        yk = sb(S2, "yk", [128, 256], F32)
                        ykb = sb(S2, "ykb", [128, 256], BF16)
                        tk1 = sb(S2, "tk1", [128, 256], F32)
                        tk2 = sb(S2, "tk2", [128, 256], F32)
                        ph = [ps(S2, "ph%d" % i, [128, 256]) for i in range(2)]
                        pcv = ps(S2, "pcv", [128, 2])
                        pkc = ps(S2, "pkc", [128, 256])
                        prk = ps(S2, "prk", [128, 256])
                        pvc = ps(S2, "pvc", [128, 64])
                        P.dma(w2kf[:].rearrange("p a n -> p (a n)"), cw2k, writes=["w2kf"])
                        P.dve(lambda e: e.tensor_copy(out=w2k[:], in_=w2kf[:]), ["w2kf"], ["w2k"])
                        P.dma(w2vf[:].rearrange("p a n -> p (a n)"), cw2v, writes=["w2vf"])
                        P.dve(lambda e: e.tensor_copy(out=w2v[:], in_=w2vf[:]), ["w2vf"], ["w2v"])
                        P.dma(b2k[:], cb2k, writes=["b2k"])
                        P.dma(b2v[:], cb2v.partition_broadcast(128), writes=["b2v"])
                        P.dma(tabC[:, 0, :], cdr["cosC"], writes=["tabC"])
                        P.dma(tabC[:, 1, :], cdr["sinC"], writes=["tabC"])
                        for kv in "kv":
                            load_cast(w1b[:].rearrange("p l n -> p (l n)"), cw1[kv], 32 * 256, "w1b")
                            P.dma(posT[:], cpos[kv], writes=["posT"])
                            P.dve(lambda e: e.tensor_copy(out=posTb[:], in_=posT[:]), ["posT"], ["posTb"])
                            P.dma(b1[:], cb1[kv], writes=["b1"])
                            for ht in range(2):
                                for l in range(32):
                                    P.pe(lambda e, ht=ht, l=l: e.matmul(pcv[:, ht:ht + 1], lhsT=w1b[0:64, l, ht * 128:(ht + 1) * 128], rhs=posTb[0:64, l:l + 1],
                                                                         start=(l == 0), stop=(l == 31)), ["w1b", "posTb"], ["pcv"])
                            P.dve(lambda e: e.tensor_tensor(out=bias1[:], in0=pcv[:], in1=b1[:], op=ALU.add), ["pcv", "b1"], ["bias1"])
                            for g in range(2):
                                gp = slice(g * 64, (g + 1) * 64)
                                for ht in range(2):
                                    for l in range(32):
                                        P.pe(lambda e, ht=ht, l=l, gp=gp, kv=kv: e.matmul(ph[ht][:, 0:255], lhsT=w1b[gp, l, ht * 128:(ht + 1) * 128],
                                                                                        rhs=cmpT[kv][gp, l:l + 16 * 254 + 1:16],
                                                                                        start=(l == 0), stop=(l == 31)), ["w1b", "cmp" + kv + "T"], ["ph%d" % ht])
                                    P.act(lambda e, ht=ht: e.activation(out=xg[:, 0:255], in_=ph[ht][:, 0:255], func=AF.Identity, bias=bias1[:, ht:ht + 1], scale=1.0),
                                          ["ph%d" % ht, "bias1"], ["xg"])
                                    P.dve(lambda e: e.tensor_tensor(out=ug[:, 0:255], in0=xg[:, 0:255], in1=xg[:, 0:255], op=ALU.mult), ["xg"], ["ug"])
                                    P.dve(lambda e: e.tensor_scalar(out=ug[:, 0:255], in0=ug[:, 0:255], scalar1=0.044715, scalar2=1.0, op0=ALU.mult, op1=ALU.add), ["ug"], ["ug"])
                                    P.dve(lambda e: e.tensor_tensor(out=ug[:, 0:255], in0=ug[:, 0:255], in1=xg[:, 0:255], op=ALU.mult), ["ug", "xg"], ["ug"])
                                    P.act(lambda e: e.activation(out=sg_[:, 0:255], in_=ug[:, 0:255], func=AF.Sigmoid, scale=1.5957691216057308), ["ug"], ["sg_"])
                                    P.dve(lambda e, ht=ht: e.tensor_tensor(out=h1[:, ht, 0:255], in0=xg[:, 0:255], in1=sg_[:, 0:255], op=ALU.mult), ["xg", "sg_"], ["h1"])
                                if kv == "k":
                                    for ht in range(2):
                                        P.pe(lambda e, ht=ht: e.matmul(pkc[:, 0:255], lhsT=w2k[:, ht, :], rhs=h1[:, ht, 0:255], start=(ht == 0), stop=(ht == 1)),
                                             ["w2k", "h1"], ["pkc"])
                                    P.act(lambda e: e.activation(out=yk[:, 0:255], in_=pkc[:, 0:255], func=AF.Identity, bias=b2k[:, 0:1], scale=1.0), ["pkc", "b2k"], ["yk"])
                                    P.act(lambda e: e.copy(out=ykb[:, 0:255], in_=yk[:, 0:255]), ["yk"], ["ykb"])
                                    P.pe(lambda e: e.matmul(prk[:, 0:255], lhsT=pt64[:], rhs=ykb[:, 0:255], start=True, stop=True), ["pt64", "ykb"], ["prk"])
                                    P.dve(lambda e: e.tensor_tensor(out=tk1[:, 0:255], in0=yk[:, 0:255], in1=tabC[:, 0, 0:255], op=ALU.mult), ["yk", "tabC"], ["tk1"])
                                    P.dve(lambda e: e.tensor_tensor(out=tk2[:, 0:255], in0=prk[:, 0:255], in1=tabC[:, 1, 0:255], op=ALU.mult), ["prk", "tabC"], ["tk2"])
                                    P.dve(lambda e, gp=gp: e.tensor_tensor(out=kcmpT[gp, 0:255], in0=tk1[gp, 0:255], in1=tk2[gp, 0:255], op=ALU.add), ["tk1", "tk2"], ["kcmpT"])
                                else:
                                    for a in range(2):
                                        cntn = 128 if a == 0 else 127
                                        for ht in range(2):
                                            P.pe(lambda e, ht=ht, a=a, cntn=cntn: e.matmul(pvc[0:cntn, :], lhsT=h1[:, ht, a * 128:a * 128 + cntn], rhs=w2v[:, ht, :],
                                                                                            start=(ht == 0), stop=(ht == 1)), ["w2v", "h1"], ["pvc"])
                                        P.dve(lambda e, a=a, cntn=cntn, g=g: e.tensor_tensor(out=vcmp1[0:cntn, a, g, 0:64], in0=pvc[0:cntn, :], in1=b2v[0:cntn, :], op=ALU.add),
                                              ["pvc", "b2v"], ["vcmp1"])
                        dump("kcmpT", kcmpT[:], [128, 256], "kcmpT")
                        dump("vcmp1", vcmp1[:].rearrange("p a g d -> p (a g d)"), [128, 260], "vcmp1")
                    P.barrier()
            P.barrier()
            if n_stage >= 3:
                with ExitStack() as S3:
                    eall = sb(S3, "eall", [128, 32, 128], BF16)
                    wmask = sb(S3, "wmask", [128, 6, 128], BF16)
                    cmpmask = sb(S3, "cmpmask", [128, 2, 16, 128], BF16)
                    ovT = sb(S3, "ovT", [128, 2, 64], BF16)
                    tkm = sb(S3, "tkm", [128, 16, 64], F32)
                    tkb = sb(S3, "tkb", [128, 16, 64], F32)
                    P.dma(eall[:].rearrange("p a k -> p (a k)"), cdr["eall"], writes=["eall"])
                    P.dma(wmask[:].rearrange("p a k -> p (a k)"), cdr["wmask"], writes=["wmask"])
                    P.dma(cmpmask[:].rearrange("p a i k -> p (a i k)"), cdr["cmpmask"], writes=["cmpmask"])
                    P.dma(ovT[:].rearrange("p a k -> p (a k)"), cdr["ovT"], writes=["ovT"])
                    P.dma(tkm[:].rearrange("p a k -> p (a k)"), cdr["tkm"], writes=["tkm"])
                    P.dma(tkb[:].rearrange("p a k -> p (a k)"), cdr["tkb"], writes=["tkb"])
                    eT = [sb(S3, "eT%d" % i, [128, 512], BF16) for i in range(3)]
                    oacc = sb(S3, "oacc", [128, 512], F32)
                    oab = sb(S3, "oab", [128, 512], BF16)
                    rz = sb(S3, "rz", [128, 4], F32)
                    coef = sb(S3, "coef", [128, 4], F32)
                    imp = sb(S3, "imp", [128, 64], F32)
                    score = sb(S3, "score", [128, 64], F32)
                    work = sb(S3, "work", [128, 64], F32)
                    m8 = sb(S3, "m8", [128, 16], F32)
                    nmk = sb(S3, "nmk", [128, 2, 64], BF16)
                    nmT = sb(S3, "nmT", [128, 128], BF16)
                    pST = [ps(S3, "pST%d" % i, [128, 512]) for i in range(2)]
                    pA = ps(S3, "pA", [128, 4, 65])
                    pB = ps(S3, "pB", [128, 4, 64])
                    pS = ps(S3, "pS", [128, 4, 65])
                    pW = ps(S3, "pW", [128, 4, 65])
                    pTr = ps(S3, "pTr", [128, 128], BF16)
                    str_ = Ring([0, 1])
                    etr = Ring([0, 1, 2])

                    def scores(kT_ap, kkey, g, i, masks):
                        gp = slice(g * 64, (g + 1) * 64)
                        si = str_.next()
                        ei = etr.next()
                        nm = len(masks)
                        P.pe(lambda e: e.matmul(pST[si][:].rearrange("p (a t) -> p a t", a=4), lhsT=kT_ap, rhs=QT[gp, :, i * 128:(i + 1) * 128],
                                                start=True, stop=(nm == 0)), [kkey, "QT"], ["pST%d" % si])
                        for mi, (ml, mr, mkeys) in enumerate(masks):
                            P.pe(lambda e, ml=ml, mr=mr, mi=mi: e.matmul(pST[si][:].rearrange("p (a t) -> p a t", a=4), lhsT=ml, rhs=mr,
                                                                           start=False, stop=(mi == nm - 1)), mkeys, ["pST%d" % si])
                        P.act(lambda e: e.activation(out=eT[ei][:], in_=pST[si][:], func=AF.Exp), ["pST%d" % si], ["eT%d" % ei])
                        return ei

                    def bc4(ap):
                        return ap.unsqueeze(1).broadcast_to([ap.shape[0], 4, ap.shape[1]])

                    def finish_branch(pacc, pkey, i, g, br, first):
                        P.dve(lambda e: e.tensor_scalar(out=rz[:], in0=pacc[:, :, 64], scalar1=1e-30, scalar2=None, op0=ALU.max), [pkey], ["rz"])
                        P.dve(lambda e: e.reciprocal(out=rz[:], in_=rz[:]), ["rz"], ["rz"])
                        P.dve(lambda e: e.tensor_tensor(out=coef[:], in0=rz[:], in1=gates[:, i, g * 12 + br:g * 12 + 12:3], op=ALU.mult), ["rz", "gates"], ["coef"])
                        for hh in range(4):
                            o = oacc[:, g * 256 + hh * 64:g * 256 + (hh + 1) * 64]
                            if first:
                                P.dve(lambda e, hh=hh, o=o: e.tensor_scalar(out=o, in0=pacc[:, hh, 0:64], scalar1=coef[:, hh:hh + 1], scalar2=None, op0=ALU.mult),
                                      [pkey, "coef"], ["oacc"])
                            else:
                                P.dve(lambda e, hh=hh, o=o: e.scalar_tensor_tensor(out=o, in0=pacc[:, hh, 0:64], scalar=coef[:, hh:hh + 1], in1=o, op0=ALU.mult, op1=ALU.add),
                                      [pkey, "coef", "oacc"], ["oacc"])

                    for i in range(16):
                        for g in range(2):
                            gp = slice(g * 64, (g + 1) * 64)
                            na = 1 if i < 8 else 2
                            for a in range(na):
                                ei = scores(kcmpT[gp, a * 128:(a + 1) * 128], "kcmpT", g, i,
                                            [(identb[:], bc4(cmpmask[:, a, i, :]), ["identb", "cmpmask"])])
                                for hh in range(4):
                                    P.pe(lambda e, hh=hh, ei=ei, a=a: e.matmul(pA[:, hh, :], lhsT=eT[ei][:, hh * 128:(hh + 1) * 128], rhs=vcmp1[:, a, g, :],
                                                                                start=(a == 0 and hh == 0), stop=(a == na - 1 and hh == 3)), ["eT%d" % ei, "vcmp1"], ["pA"])
                                    P.pe(lambda e, hh=hh, ei=ei, a=a: e.matmul(pB[:, hh, :], lhsT=eT[ei][:, hh * 128:(hh + 1) * 128], rhs=ovT[:, a, :],
                                                                                start=(a == 0 and hh == 0), stop=(a == na - 1 and hh == 3)), ["eT%d" % ei, "ovT"], ["pB"])
                            finish_branch(pA, "pA", i, g, 0, True)
                            P.dve(lambda e: e.tensor_scalar(out=imp[:], in0=pB[:, 0, :], scalar1=rz[:, 0:1], scalar2=None, op0=ALU.mult), ["pB", "rz"], ["imp"])
                            for hh in range(1, 4):
                                P.dve(lambda e, hh=hh: e.scalar_tensor_tensor(out=imp[:], in0=pB[:, hh, :], scalar=rz[:, hh:hh + 1], in1=imp[:], op0=ALU.mult, op1=ALU.add),
                                      ["pB", "rz", "imp"], ["imp"])
                            P.dve(lambda e: e.tensor_tensor(out=score[:], in0=imp[:], in1=tkm[:, i, :], op=ALU.mult), ["imp", "tkm"], ["score"])
                            P.dve(lambda e: e.tensor_tensor(out=score[:], in0=score[:], in1=tkb[:, i, :], op=ALU.add), ["score", "tkb"], ["score"])
                            P.dve(lambda e: e.max(out=m8[:, 0:8], in_=score[:]), ["score"], ["m8"])
                            P.dve(lambda e: e.match_replace(out=work[:], in_to_replace=m8[:, 0:8], in_values=score[:], imm_value=-3.0e38), ["score", "m8"], ["work"])
                            P.dve(lambda e: e.max(out=m8[:, 8:16], in_=work[:]), ["work"], ["m8"])
                            P.dve(lambda e: e.tensor_scalar(out=nmk[:], in0=score[:].unsqueeze(1).broadcast_to([128, 2, 64]), scalar1=m8[:, 15:16], scalar2=NEGM, op0=ALU.is_lt, op1=ALU.mult), ["score", "m8"], ["nmk"])
                            P.pe(lambda e: e.transpose(out=pTr[:], in_=nmk[:].rearrange("p a s -> p (a s)"), identity=identb[:]), ["nmk", "identb"], ["pTr"])
                            P.act(lambda e: e.copy(out=nmT[gp, :], in_=pTr[gp, :]), ["pTr"], ["nmT"])
                            if ("imp%d_%d" % (i, g)) in debug:
                                dump("imp%d_%d" % (i, g), imp[:], [128, 64], "imp")
                                dump("score%d_%d" % (i, g), score[:], [128, 64], "score")
                                dump("m8%d_%d" % (i, g), m8[:], [128, 16], "m8")
                            nj = 2 * i + 2
                            for j in range(nj):
                                masks = [(eall[gp, j, :], bc4(nmT[gp, :]), ["eall", "nmT"])]
                                if j >= 2 * i:
                                    masks.append((identb[:], bc4(wmask[:, 4 + (j - 2 * i), :]), ["identb", "wmask"]))
                                ei = scores(slckT[gp, j * 128:(j + 1) * 128], "slckT", g, i, masks)
                                for hh in range(4):
                                    P.pe(lambda e, hh=hh, ei=ei, j=j: e.matmul(pS[:, hh, :], lhsT=eT[ei][:, hh * 128:(hh + 1) * 128], rhs=slcv1[:, j, g, :],
                                                                                start=(j == 0 and hh == 0), stop=(j == nj - 1 and hh == 3)), ["eT%d" % ei, "slcv1"], ["pS"])
                            finish_branch(pS, "pS", i, g, 1, False)
                            js = [(r, 2 * i - 4 + r) for r in range(6) if 2 * i - 4 + r >= 0]
                            for idx, (r, j) in enumerate(js):
                                ei = scores(winkT[gp, j * 128:(j + 1) * 128], "winkT", g, i,
                                            [(identb[:], bc4(wmask[:, r, :]), ["identb", "wmask"])])
                                for hh in range(4):
                                    P.pe(lambda e, hh=hh, ei=ei, j=j, idx=idx: e.matmul(pW[:, hh, :], lhsT=eT[ei][:, hh * 128:(hh + 1) * 128], rhs=winv1[:, j, g, :],
                                                                                         start=(idx == 0 and hh == 0), stop=(idx == len(js) - 1 and hh == 3)), ["eT%d" % ei, "winv1"], ["pW"])
                            finish_branch(pW, "pW", i, g, 2, False)
                        if ("oacc%d" % i) in debug:
                            dump("oacc%d" % i, oacc[:], [128, 512], "oacc")
                        P.act(lambda e: e.copy(out=oab[:], in_=oacc[:]), ["oacc"], ["oab"])
                        for ct in range(4):
                            P.pe(lambda e, ct=ct: e.transpose(out=pTr[:], in_=oab[:, ct * 128:(ct + 1) * 128], identity=identb[:]), ["oab", "identb"], ["pTr"])
                            P.dve(lambda e, ct=ct, i=i: e.tensor_copy(out=oattnT[:, ct, i * 128:(i + 1) * 128], in_=pTr[:]), ["pTr"], ["oattnT"])
                    dump("oattnT", oattnT[:].rearrange("p a t -> p (a t)"), [128, 4 * TO], "oattnT")
                P.barrier()
        P.barrier()

        B_ = ExitStack()
        oretT = sb(B_, "oretT", [128, 8, TO], BF16)
        if n_stage >= 4:
            with ExitStack() as S4:
                W4 = sb(S4, "W4", [128, 8, 2048], BF16)
                load_cast(W4[:].rearrange("p k n -> p (k n)"), w4t, 8 * 2048, "W4")
                pt128 = sb(S4, "pt128", [128, 128], BF16)
                P.dma(pt128[:], cdr["pt128"], writes=["pt128"])
                Dc = sb(S4, "Dc", [128, 2, 4, 128], F32)
                xi = sb(S4, "xi", [128, 4, 128], F32)
                zeta = sb(S4, "zeta", [128, 2, 4], F32)
                P.dma(Dc[:].rearrange("p a h c -> p (a h c)"), cdr["Dc"], writes=["Dc"])
                P.dma(xi[:].rearrange("p h c -> p (h c)"), cdr["xi"], writes=["xi"])
                P.dma(zeta[:].rearrange("p a h -> p (a h)"), cdr["zeta"], writes=["zeta"])
                xst = wst3
                xb = sb(S4, "xb4", [128, 8, 512], BF16)
                xob = sb(S4, "xob4", [128, 8, 256], BF16)
                tabs = sb(S4, "tab4", [128, 2, 512], F32)
                tabq = sb(S4, "tabq4", [128, 2, 256], F32)
                ybf = sb(S4, "ybf4", [128, 512], BF16)
                t1 = sb(S4, "t1_4", [128, 512], F32)
                t2 = sb(S4, "t2_4", [128, 512], F32)
                kT = sb(S4, "kT4", [128, 4, 512], BF16)
                qT = sb(S4, "qT4", [128, 4, 256], BF16)
                qxT = sb(S4, "qxT4", [128, 4, 256], BF16)
                vtok = sb(S4, "vtok", [128, 4, 1024], BF16)
                kz = sb(S4, "kz", [128, 4, 4, 128], BF16)
                R = sb(S4, "R", [128, 4, 256], F32)
                Rb = sb(S4, "Rb", [128, 4, 256], BF16)
                sc = sb(S4, "sc", [128, 2, 128], BF16)
                st6 = sb(S4, "st6", [128, 6], F32)
                mv = sb(S4, "mv", [128, 2], F32)
                rstd = sb(S4, "rstd", [128, 1], F32)
                oretb = sb(S4, "oretb", [128, 1024], BF16)
                pj = [ps(S4, "pj4_%d" % i, [128, 512]) for i in range(2)]
                prot = ps(S4, "prot4", [128, 512])
                psc = ps(S4, "psc", [128, 2, 128])
                po = ps(S4, "po", [128, 256])
                pR = ps(S4, "pR", [128, 256])
                pTr = ps(S4, "pTr4", [128, 128], BF16)
                pjr = Ring([0, 1])
                P.dve(lambda e: e.memset(R[:], 0.0), [], ["R"])
                P.dve(lambda e: e.memset(Rb[:], 0.0), [], ["Rb"])
                xTv = xT.rearrange("(k p) t -> p k t", p=128)
                xTov = xTo.rearrange("(k p) t -> p k t", p=128)

                def rope4(pi, n, tab, tabkey, dst_ap, dst_key):
                    P.act(lambda e: e.copy(out=ybf[:, 0:n], in_=pj[pi][:, 0:n]), ["pj4_%d" % pi], ["ybf4"])
                    P.pe(lambda e: e.matmul(prot[:, 0:n], lhsT=pt128[:], rhs=ybf[:, 0:n], start=True, stop=True), ["pt128", "ybf4"], ["prot4"])
                    P.dve(lambda e: e.tensor_tensor(out=t1[:, 0:n], in0=pj[pi][:, 0:n], in1=tab[:, 0, 0:n], op=ALU.mult), ["pj4_%d" % pi, tabkey], ["t1_4"])
                    P.dve(lambda e: e.tensor_tensor(out=t2[:, 0:n], in0=prot[:, 0:n], in1=tab[:, 1, 0:n], op=ALU.mult), ["prot4", tabkey], ["t2_4"])
                    P.pool(lambda e: e.tensor_tensor(out=dst_ap, in0=t1[:, 0:n], in1=t2[:, 0:n], op=ALU.add), ["t1_4", "t2_4"], [dst_key])

                for gch in range(8):
                    c0 = gch * 512
                    o0 = gch * 256
                    P.dma(xst[:], xTv[:, :, c0:c0 + 512], writes=["wst"])
                    P.act(lambda e: e.copy(out=xb[:, 0:4, :], in_=xst[:, 0:4, :]), ["wst"], ["xb4"])
                    P.dve(lambda e: e.tensor_copy(out=xb[:, 4:8, :], in_=xst[:, 4:8, :]), ["wst"], ["xb4"])
                    P.dma(xst[:, :, 0:256], xTov[:, :, o0:o0 + 256], writes=["wst"])
                    P.act(lambda e: e.copy(out=xob[:, 0:4, :], in_=xst[:, 0:4, 0:256]), ["wst"], ["xob4"])
                    P.dve(lambda e: e.tensor_copy(out=xob[:, 4:8, :], in_=xst[:, 4:8, 0:256]), ["wst"], ["xob4"])
                    P.dma(tabs[:, 0, :], cdr["cosRK"][:, c0:c0 + 512], writes=["tab4"])
                    P.dma(tabs[:, 1, :], cdr["sinRK"][:, c0:c0 + 512], writes=["tab4"])
                    P.dma(tabq[:, 0, :], cdr["cosRQ"][:, o0:o0 + 256], writes=["tabq4"])
                    P.dma(tabq[:, 1, :], cdr["sinRQ"][:, o0:o0 + 256], writes=["tabq4"])
                    for h in range(4):
                        pi = pjr.next()
                        for k in range(8):
                            P.pe(lambda e, k=k, pi=pi, h=h: e.matmul(pj[pi][:], lhsT=W4[:, k, 512 + h * 128:512 + (h + 1) * 128], rhs=xb[:, k, :],
                                                                      start=(k == 0), stop=(k == 7)), ["W4", "xb4"], ["pj4_%d" % pi])
                        rope4(pi, 512, tabs, "tab4", kT[:, h, :], "kT4")
                    for h in range(4):
                        pi = pjr.next()
                        for k in range(8):
                            P.pe(lambda e, k=k, pi=pi, h=h: e.matmul(pj[pi][:, 0:256], lhsT=W4[:, k, h * 128:(h + 1) * 128], rhs=xob[:, k, :],
                                                                      start=(k == 0), stop=(k == 7)), ["W4", "xob4"], ["pj4_%d" % pi])
                        rope4(pi, 256, tabq, "tabq4", qT[:, h, :], "qT4")
                    for pp in range(2):
                        P.dve(lambda e, pp=pp: e.tensor_tensor(out=qxT[:, :, pp * 128:(pp + 1) * 128], in0=qT[:, :, pp * 128:(pp + 1) * 128], in1=xi[:], op=ALU.mult),
                              ["qT4", "xi"], ["qxT4"])
                    for tt in range(4):
                        for hf in range(2):
                            pi = pjr.next()
                            for k in range(8):
                                P.pe(lambda e, k=k, pi=pi, tt=tt, hf=hf: e.matmul(pj[pi][:], lhsT=xb[:, k, tt * 128:(tt + 1) * 128],
                                                                                   rhs=W4[:, k, 1024 + hf * 512:1024 + (hf + 1) * 512],
                                                                                   start=(k == 0), stop=(k == 7)), ["W4", "xb4"], ["pj4_%d" % pi])
                            P.act(lambda e, pi=pi, tt=tt, hf=hf: e.copy(out=vtok[:, tt, hf * 512:(hf + 1) * 512], in_=pj[pi][:]), ["pj4_%d" % pi], ["vtok"])
                    for tt in range(4):
                        for h in range(4):
                            P.pe(lambda e, tt=tt, h=h: e.transpose(out=pTr[:], in_=kT[:, h, tt * 128:(tt + 1) * 128], identity=identb[:]), ["kT4", "identb"], ["pTr4"])
                            P.dve(lambda e, tt=tt, h=h: e.tensor_scalar(out=kz[:, tt, h, :], in0=pTr[:], scalar1=zeta[:, tt % 2, h:h + 1], scalar2=None, op0=ALU.mult),
                                  ["pTr4", "zeta"], ["kz"])
                    for pp in range(2):
                        i = gch * 2 + pp
                        qs = slice(pp * 128, (pp + 1) * 128)
                        for h in range(4):
                            hs = slice(h * 256, (h + 1) * 256)
                            for mt in range(2):
                                tt = pp * 2 + mt
                                P.pe(lambda e, mt=mt, tt=tt, h=h: e.matmul(psc[:, mt, :], lhsT=kT[:, h, tt * 128:(tt + 1) * 128], rhs=qT[:, h, qs], start=True, stop=True),
                                     ["kT4", "qT4"], ["psc"])
                            P.dve(lambda e, h=h: e.tensor_tensor(out=sc[:], in0=psc[:], in1=Dc[:, :, h, :], op=ALU.mult), ["psc", "Dc"], ["sc"])
                            for mt in range(2):
                                tt = pp * 2 + mt
                                P.pe(lambda e, mt=mt, tt=tt, hs=hs: e.matmul(po[:], lhsT=sc[:, mt, :], rhs=vtok[:, tt, hs], start=(mt == 0), stop=False), ["sc", "vtok"], ["po"])
                            P.pe(lambda e, h=h: e.matmul(po[:], lhsT=qxT[:, h, qs], rhs=Rb[:, h, :], start=False, stop=True), ["qxT4", "Rb"], ["po"])
                            P.dve(lambda e: e.bn_stats(out=st6[:], in_=po[:]), ["po"], ["st6"])
                            P.dve(lambda e: e.bn_aggr(out=mv[:], in_=st6[:]), ["st6"], ["mv"])
                            P.dve(lambda e: e.tensor_scalar(out=rstd[:], in0=mv[:, 1:2], scalar1=LN_EPS, scalar2=None, op0=ALU.add), ["mv"], ["rstd"])
                            P.act(lambda e: e.activation(out=rstd[:], in_=rstd[:], func=AF.Sqrt), ["rstd"], ["rstd"])
                            P.dve(lambda e: e.reciprocal(out=rstd[:], in_=rstd[:]), ["rstd"], ["rstd"])
                            P.dve(lambda e, hs=hs: e.tensor_scalar(out=oretb[:, hs], in0=po[:], scalar1=mv[:, 0:1], scalar2=rstd[:, 0:1], op0=ALU.subtract, op1=ALU.mult),
                                  ["po", "mv", "rstd"], ["oretb"])
                            for mt in range(2):
                                tt = pp * 2 + mt
                                P.pe(lambda e, mt=mt, tt=tt, h=h, hs=hs: e.matmul(pR[:], lhsT=kz[:, tt, h, :], rhs=vtok[:, tt, hs], start=(mt == 0), stop=(mt == 1)),
                                     ["kz", "vtok"], ["pR"])
                            P.dve(lambda e, h=h: e.scalar_tensor_tensor(out=R[:, h, :], in0=R[:, h, :], scalar=decay256[h], in1=pR[:], op0=ALU.mult, op1=ALU.add),
                                  ["R", "pR"], ["R"])
                            P.act(lambda e, h=h: e.copy(out=Rb[:, h, :], in_=R[:, h, :]), ["R"], ["Rb"])
                        for et in range(8):
                            P.pe(lambda e, et=et: e.transpose(out=pTr[:], in_=oretb[:, et * 128:(et + 1) * 128], identity=identb[:]), ["oretb", "identb"], ["pTr4"])
                            P.act(lambda e, et=et, i=i: e.copy(out=oretT[:, et, i * 128:(i + 1) * 128], in_=pTr[:]), ["pTr4"], ["oretT"])
                dump("oretT", oretT[:].rearrange("p a t -> p (a t)"), [128, 8 * TO], "oretT")
            P.barrier()

        if n_stage >= 5:
            with ExitStack() as S5a:
                Wmg = sb(S5a, "Wmg", [128, 8, 2048], BF16)
                Wa = sb(S5a, "Wa", [128, 4, 1024], BF16)
                Wr = sb(S5a, "Wr", [128, 8, 1024], BF16)
                Wgt = sb(S5a, "Wgt", [128, 8, 1024], BF16)
                load_cast(Wmg[:].rearrange("p k n -> p (k n)"), wmgt, 8 * 2048, "Wmg")
                load_cast(Wa[:].rearrange("p k n -> p (k n)"), wat, 4 * 1024, "Wa")
                load_cast(Wr[:].rearrange("p k n -> p (k n)"), wrt, 8 * 1024, "Wr")
                load_cast(Wgt[:].rearrange("p k n -> p (k n)"), wgtt, 8 * 1024, "Wgt")
                gg8 = sb(S5a, "gg8", [128, 8], F32)
                gb8 = sb(S5a, "gb8", [128, 8], F32)
                P.dma(gg8[:], gng8, writes=["gg8"])
                P.dma(gb8[:], gnb8, writes=["gb8"])
                xb = sb(S5a, "xb5", [128, 8, 512], BF16)
                og = sb(S5a, "og", [128, 8, 512], BF16)
                sgt = [sb(S5a, "sgt%d" % i, [128, 512], F32) for i in range(2)]
                yn = [sb(S5a, "yn%d" % i, [128, 512], F32) for i in range(2)]
                ga = sb(S5a, "ga", [128, 512], F32)
                gr = sb(S5a, "gr", [128, 512], F32)
                ma = sb(S5a, "ma", [128, 512], F32)
                pg = [ps(S5a, "pg%d" % i, [128, 512]) for i in range(2)]
                pu = [ps(S5a, "pu%d" % i, [128, 512]) for i in range(2)]
                pgt = [ps(S5a, "pgt%d" % i, [128, 512]) for i in range(2)]
                xTov = xTo.rearrange("(k p) t -> p k t", p=128)
                for oc in range(4):
                    c0 = oc * 512
                    cs_ = slice(c0, c0 + 512)
                    P.dma(wst3[:], xTov[:, :, cs_], writes=["wst"])
                    P.act(lambda e: e.copy(out=xb[:, 0:4, :], in_=wst3[:, 0:4, :]), ["wst"], ["xb5"])
                    P.dve(lambda e: e.tensor_copy(out=xb[:, 4:8, :], in_=wst3[:, 4:8, :]), ["wst"], ["xb5"])
                    for et in range(8):
                        b_ = et % 2
                        for k in range(8):
                            P.pe(lambda e: e.matmul(pgt[b_][:], lhsT=Wgt[:, k, et * 128:(et + 1) * 128], rhs=xb[:, k, :], start=(k == 0), stop=(k == 7)),
                                 ["Wgt", "xb5"], ["pgt%d" % b_])
                        P.act(lambda e: e.activation(out=sgt[b_][:], in_=pgt[b_][:], func=AF.Silu), ["pgt%d" % b_], ["sgt%d" % b_])
                        P.act(lambda e: e.activation(out=yn[b_][:], in_=oretT[:, et, cs_], func=AF.Identity, scale=gg8[:, et:et + 1], bias=gb8[:, et:et + 1]),
                              ["oretT", "gg8", "gb8"], ["yn%d" % b_])
                        P.dve(lambda e: e.tensor_tensor(out=og[:, et, :], in0=yn[b_][:], in1=sgt[b_][:], op=ALU.mult), ["yn%d" % b_, "sgt%d" % b_], ["og"])
                    for ct in range(8):
                        for k in range(8):
                            P.pe(lambda e: e.matmul(pg[0][:], lhsT=Wmg[:, k, ct * 128:(ct + 1) * 128], rhs=xb[:, k, :], start=(k == 0), stop=(k == 7)),
                                 ["Wmg", "xb5"], ["pg0"])
                        for k in range(8):
                            P.pe(lambda e: e.matmul(pg[1][:], lhsT=Wmg[:, k, 1024 + ct * 128:1024 + (ct + 1) * 128], rhs=xb[:, k, :], start=(k == 0), stop=(k == 7)),
                                 ["Wmg", "xb5"], ["pg1"])
                        for k in range(4):
                            P.pe(lambda e: e.matmul(pu[0][:], lhsT=Wa[:, k, ct * 128:(ct + 1) * 128], rhs=oattnT[:, k, cs_], start=(k == 0), stop=(k == 3)),
                                 ["Wa", "oattnT"], ["pu0"])
                        for k in range(8):
                            P.pe(lambda e: e.matmul(pu[1][:], lhsT=Wr[:, k, ct * 128:(ct + 1) * 128], rhs=og[:, k, :], start=(k == 0), stop=(k == 7)),
                                 ["Wr", "og"], ["pu1"])
                        P.act(lambda e: e.activation(out=ga[:], in_=pg[0][:], func=AF.Sigmoid), ["pg0"], ["ga"])
                        P.act(lambda e: e.activation(out=gr[:], in_=pg[1][:], func=AF.Sigmoid), ["pg1"], ["gr"])
                        P.dve(lambda e: e.tensor_tensor(out=ma[:], in0=pu[0][:], in1=ga[:], op=ALU.mult), ["pu0", "ga"], ["ma"])
                        P.dve(lambda e: e.tensor_tensor(out=gr[:], in0=pu[1][:], in1=gr[:], op=ALU.mult), ["pu1", "gr"], ["gr"])
                        P.pool(lambda e: e.tensor_tensor(out=x1T[:, ct, cs_], in0=ma[:], in1=gr[:], op=ALU.add), ["ma", "gr"], ["mx%d" % (oc * 4 + t_) for t_ in range(4)])
                dump("mergedT", x1T[:].rearrange("p a t -> p (a t)"), [128, 8 * TO], "mx0")
            P.barrier()
        B_.close()
        A_.close()
        if n_stage >= 5:
            with ExitStack() as S56:
                acc = sb(S56, "acc", [128, 16, 1024], F32)
                lng = sb(S56, "lng", [128, 1024], F32)
                lnb = sb(S56, "lnb", [128, 1024], F32)
                st12 = sb(S56, "st12", [128, 2, 6], F32)
                mv = sb(S56, "mv5", [128, 2], F32)
                rstd = sb(S56, "rstd5", [128, 1], F32)

                def layer_norm(src_ap, src_key, dst_ap, dst_key, tmp_ap, tmp_key):
                    for hf in range(2):
                        P.dve(lambda e: e.bn_stats(out=st12[:, hf, :], in_=src_ap[:, hf * 512:(hf + 1) * 512]), [src_key], ["st12"])
                    P.dve(lambda e: e.bn_aggr(out=mv[:], in_=st12[:].rearrange("p a s -> p (a s)")), ["st12"], ["mv5"])
                    P.dve(lambda e: e.tensor_scalar(out=rstd[:], in0=mv[:, 1:2], scalar1=LN_EPS, scalar2=None, op0=ALU.add), ["mv5"], ["rstd5"])
                    P.act(lambda e: e.activation(out=rstd[:], in_=rstd[:], func=AF.Sqrt), ["rstd5"], ["rstd5"])
                    P.dve(lambda e: e.reciprocal(out=rstd[:], in_=rstd[:]), ["rstd5"], ["rstd5"])
                    P.dve(lambda e: e.tensor_scalar(out=tmp_ap, in0=src_ap, scalar1=mv[:, 0:1], scalar2=rstd[:, 0:1], op0=ALU.subtract, op1=ALU.mult),
                          [src_key, "mv5", "rstd5"], [tmp_key])
                    P.pool(lambda e: e.tensor_tensor(out=tmp_ap, in0=tmp_ap, in1=lng[:], op=ALU.mult), [tmp_key, "lng"], [tmp_key])
                    P.pool(lambda e: e.tensor_tensor(out=dst_ap, in0=tmp_ap, in1=lnb[:], op=ALU.add), [tmp_key, "lnb"], [dst_key])

                with ExitStack() as S5b:
                    Wo = sb(S5b, "Wo", [128, 8, 1024], BF16)
                    load_cast(Wo[:].rearrange("p k n -> p (k n)"), wot, 8 * 1024, "Wo")
                    P.dma(lng[:], ln1g.partition_broadcast(128), writes=["lng"])
                    P.dma(lnb[:], ln1b.partition_broadcast(128), writes=["lnb"])
                    xres = sb(S5b, "xres", [128, 1024], F32)
                    yt = sb(S5b, "yt", [128, 1024], F32)
                    x1 = sb(S5b, "x1", [128, 1024], F32)
                    x1b = sb(S5b, "x1b", [128, 1024], BF16)
                    pm = [ps(S5b, "pm%d" % i, [128, 512]) for i in range(2)]
                    pTr = ps(S5b, "pTr5", [128, 128], BF16)
                    for i in range(16):
                        ts_ = slice(i * 128, (i + 1) * 128)
                        P.dma(xres[:], xo[ts_, :], writes=["xres"])
                        for hf in range(2):
                            for k in range(8):
                                P.pe(lambda e: e.matmul(pm[hf][:], lhsT=x1T[:, k, ts_], rhs=Wo[:, k, hf * 512:(hf + 1) * 512], start=(k == 0), stop=(k == 7)),
                                     ["mx%d" % i, "Wo"], ["pm%d" % hf])
                            P.dve(lambda e: e.scalar_tensor_tensor(out=yt[:, hf * 512:(hf + 1) * 512], in0=xres[:, hf * 512:(hf + 1) * 512], scalar=ALPHA,
                                                                   in1=pm[hf][:], op0=ALU.mult, op1=ALU.add), ["xres", "pm%d" % hf], ["yt"])
                        layer_norm(yt[:], "yt", x1[:], "x1", yt[:], "yt")
                        if ("x1_%d" % i) in debug:
                            dump("x1_%d" % i, x1[:], [128, 1024], "x1")
                        P.dve(lambda e: e.tensor_scalar(out=acc[:, i, :], in0=x1[:], scalar1=ALPHA, scalar2=None, op0=ALU.mult), ["x1"], ["acc%d" % i])
                        P.act(lambda e: e.copy(out=x1b[:], in_=x1[:]), ["x1"], ["x1b"])
                        for dt_ in range(8):
                            P.pe(lambda e: e.transpose(out=pTr[:], in_=x1b[:, dt_ * 128:(dt_ + 1) * 128], identity=identb[:]), ["x1b", "identb"], ["pTr5"])
                            P.act(lambda e: e.copy(out=x1T[:, dt_, ts_], in_=pTr[:]), ["pTr5"], ["mx%d" % i])
                P.barrier()
                if n_stage >= 6:
                    with ExitStack() as S6:
                        P.dma(lng[:], ln2g.partition_broadcast(128), writes=["lng"])
                        P.dma(lnb[:], ln2b.partition_broadcast(128), writes=["lnb"])
                        comb = sb(S6, "comb", [128, 16, 32], F32)
                        wrf = sb(S6, "wrf", [128, 8, 36], F32)
                        wrb = sb(S6, "wrb", [128, 8, 36], BF16)
                        brb = sb(S6, "brb", [128, 36], F32)
                        P.dma(wrf[:].rearrange("p k n -> p (k n)"), wrout, writes=["wrf"])
                        P.dve(lambda e: e.tensor_copy(out=wrb[:], in_=wrf[:]), ["wrf"], ["wrb"])
                        P.dma(brb[:], brout.partition_broadcast(128), writes=["brb"])
                        lg = sb(S6, "lg", [128, 36], F32)
                        gmx = sb(S6, "gmx", [128, 1], F32)
                        ngmx = sb(S6, "ngmx", [128, 1], F32)
                        gex = sb(S6, "gex", [128, 4], F32)
                        gsum = sb(S6, "gsum", [128, 1], F32)
                        gprob = sb(S6, "gprob", [128, 1], F32)
                        ohg = sb(S6, "ohg", [128, 4], F32)
                        tmp48 = sb(S6, "tmp48", [128, 4, 8], F32)
                        isel = sb(S6, "isel", [128, 8], F32)
                        m8 = sb(S6, "m8r", [128, 8], F32)
                        dlt = sb(S6, "dlt", [128, 1], F32)
                        w2e = sb(S6, "w2e", [128, 1], F32)
                        wsum = sb(S6, "wsum", [128, 1], F32)
                        wt1 = sb(S6, "wt1", [128, 1], F32)
                        wt2 = sb(S6, "wt2", [128, 1], F32)
                        ce = sb(S6, "ce", [128, 8], F32)
                        ce2 = sb(S6, "ce2", [128, 8], F32)
                        plg = ps(S6, "plg", [128, 36])
                        AXX = mybir.AxisListType.X
                        for i in range(16):
                            ts_ = slice(i * 128, (i + 1) * 128)
                            for k in range(8):
                                P.pe(lambda e: e.matmul(plg[:], lhsT=x1T[:, k, ts_], rhs=wrb[:, k, :], start=(k == 0), stop=(k == 7)), ["mx%d" % i, "wrb"], ["plg"])
                            P.dve(lambda e: e.tensor_tensor(out=lg[:], in0=plg[:], in1=brb[:], op=ALU.add), ["plg", "brb"], ["lg"])
                            P.dve(lambda e: e.tensor_reduce(out=gmx[:], in_=lg[:, 0:4], axis=AXX, op=ALU.max), ["lg"], ["gmx"])
                            P.dve(lambda e: e.tensor_scalar(out=ngmx[:], in0=gmx[:], scalar1=-1.0, scalar2=None, op0=ALU.mult), ["gmx"], ["ngmx"])
                            P.act(lambda e: e.activation(out=gex[:], in_=lg[:, 0:4], func=AF.Exp, bias=ngmx[:, 0:1], scale=1.0), ["lg", "ngmx"], ["gex"])
                            P.dve(lambda e: e.tensor_reduce(out=gsum[:], in_=gex[:], axis=AXX, op=ALU.add), ["gex"], ["gsum"])
                            P.dve(lambda e: e.reciprocal(out=gprob[:], in_=gsum[:]), ["gsum"], ["gprob"])
                            P.dve(lambda e: e.tensor_scalar(out=ohg[:], in0=lg[:, 0:4], scalar1=gmx[:, 0:1], scalar2=None, op0=ALU.is_ge), ["lg", "gmx"], ["ohg"])
                            P.dve(lambda e: e.tensor_tensor(out=tmp48[:], in0=lg[:, 4:36].rearrange("p (g e) -> p g e", g=4),
                                                            in1=ohg[:].unsqueeze(2).broadcast_to([128, 4, 8]), op=ALU.mult), ["lg", "ohg"], ["tmp48"])
                            P.dve(lambda e: e.tensor_reduce(out=isel[:], in_=tmp48[:].rearrange("p g e -> p e g"), axis=AXX, op=ALU.add), ["tmp48"], ["isel"])
                            P.dve(lambda e: e.max(out=m8[:], in_=isel[:]), ["isel"], ["m8r"])
                            P.dve(lambda e: e.tensor_tensor(out=dlt[:], in0=m8[:, 1:2], in1=m8[:, 0:1], op=ALU.subtract), ["m8r"], ["dlt"])
                            P.act(lambda e: e.activation(out=w2e[:], in_=dlt[:], func=AF.Exp), ["dlt"], ["w2e"])
                            P.dve(lambda e: e.tensor_scalar(out=wsum[:], in0=w2e[:], scalar1=1.0, scalar2=None, op0=ALU.add), ["w2e"], ["wsum"])
                            P.dve(lambda e: e.reciprocal(out=wsum[:], in_=wsum[:]), ["wsum"], ["wsum"])
                            P.dve(lambda e: e.tensor_tensor(out=wt1[:], in0=wsum[:], in1=gprob[:], op=ALU.mult), ["wsum", "gprob"], ["wt1"])
                            P.dve(lambda e: e.tensor_tensor(out=wt2[:], in0=wt1[:], in1=w2e[:], op=ALU.mult), ["wt1", "w2e"], ["wt2"])
                            P.dve(lambda e: e.tensor_scalar(out=ce[:], in0=isel[:], scalar1=m8[:, 0:1], scalar2=wt1[:, 0:1], op0=ALU.is_equal, op1=ALU.mult), ["isel", "m8r", "wt1"], ["ce"])
                            P.dve(lambda e: e.tensor_scalar(out=ce2[:], in0=isel[:], scalar1=m8[:, 1:2], scalar2=wt2[:, 0:1], op0=ALU.is_equal, op1=ALU.mult), ["isel", "m8r", "wt2"], ["ce2"])
                            P.dve(lambda e: e.tensor_tensor(out=ce[:], in0=ce[:], in1=ce2[:], op=ALU.add), ["ce", "ce2"], ["ce"])
                            P.dve(lambda e: e.tensor_tensor(out=comb[:, i, :].rearrange("p (g e) -> p g e", g=4), in0=ce[:].unsqueeze(1).broadcast_to([128, 4, 8]),
                                                            in1=ohg[:].unsqueeze(2).broadcast_to([128, 4, 8]), op=ALU.mult), ["ce", "ohg"], ["comb"])
                        dump("comb", comb[:].rearrange("p a e -> p (a e)"), [128, 512], "comb")
                        wstE = [wst[:, 0:2048], wst[:, 2048:4096]]
                        wE = [sb(S6, "wE%d" % i, [128, 12288], BF16) for i in range(2)]
                        sgE = [sb(S6, "sgE%d" % i, [128, 512], F32) for i in range(2)]
                        hT = [sb(S6, "hT%d" % i, [128, 4, 512], BF16) for i in range(2)]
                        pG = [ps(S6, "pG%d" % i, [128, 512]) for i in range(2)]
                        pU = [ps(S6, "pU%d" % i, [128, 512]) for i in range(2)]
                        pO = [ps(S6, "pO%d" % i, [128, 512]) for i in range(3)]
                        wsr = Ring([0, 1])
                        gr_ = Ring([0, 1])
                        or_ = Ring([0, 1, 2])
                        crr = [0]
                        n_exp = DEBUG.get("n_exp", 32)
                        for ex in range(n_exp):
                            ws = ex % 2
                            for pc in range(6):
                                si = wsr.next()
                                P.dma(wstE[si], wexp[ex, :, pc * 2048:(pc + 1) * 2048], writes=["wstE%d" % si])
                                d = wE[ws][:, pc * 2048:(pc + 1) * 2048]
                                if crr[0] % 2 == 0:
                                    P.act(lambda e: e.copy(out=d, in_=wstE[si]), ["wstE%d" % si], ["wE%d" % ws])
                                else:
                                    P.pool(lambda e: e.tensor_copy(out=d, in_=wstE[si]), ["wstE%d" % si], ["wE%d" % ws])
                                crr[0] += 1
                            Wg = wE[ws][:, 0:4096].rearrange("p (k n) -> p k n", k=8)
                            Wu = wE[ws][:, 4096:8192].rearrange("p (k n) -> p k n", k=8)
                            Wd = wE[ws][:, 8192:12288].rearrange("p (k n) -> p k n", k=4)
                            wkey = "wE%d" % ws
                            for cth in range(4):
                                cs_ = slice(cth * 512, (cth + 1) * 512)
                                xkeys = ["mx%d" % (cth * 4 + t_) for t_ in range(4)]
                                hs_ = cth % 2
                                for ft in range(4):
                                    gi = gr_.next()
                                    for k in range(8):
                                        P.pe(lambda e: e.matmul(pG[gi][:], lhsT=Wg[:, k, ft * 128:(ft + 1) * 128], rhs=x1T[:, k, cs_], start=(k == 0), stop=(k == 7)),
                                             [wkey] + xkeys, ["pG%d" % gi])
                                    for k in range(8):
                                        P.pe(lambda e: e.matmul(pU[gi][:], lhsT=Wu[:, k, ft * 128:(ft + 1) * 128], rhs=x1T[:, k, cs_], start=(k == 0), stop=(k == 7)),
                                             [wkey] + xkeys, ["pU%d" % gi])
                                    P.act(lambda e: e.activation(out=sgE[gi][:], in_=pG[gi][:], func=AF.Silu), ["pG%d" % gi], ["sgE%d" % gi])
                                    P.dve(lambda e: e.tensor_tensor(out=hT[hs_][:, ft, :], in0=pU[gi][:], in1=sgE[gi][:], op=ALU.mult),
                                          ["pU%d" % gi, "sgE%d" % gi], ["hT%d" % hs_])
                                for tt in range(4):
                                    ti = cth * 4 + tt
                                    for hf in range(2):
                                        oi = or_.next()
                                        for ft in range(4):
                                            P.pe(lambda e: e.matmul(pO[oi][:], lhsT=hT[hs_][:, ft, tt * 128:(tt + 1) * 128],
                                                                    rhs=Wd[:, ft, hf * 512:(hf + 1) * 512], start=(ft == 0), stop=(ft == 3)),
                                                 [wkey, "hT%d" % hs_], ["pO%d" % oi])
                                        a_ = acc[:, ti, hf * 512:(hf + 1) * 512]
                                        P.dve(lambda e: e.scalar_tensor_tensor(out=a_, in0=pO[oi][:], scalar=comb[:, ti, ex:ex + 1], in1=a_, op0=ALU.mult, op1=ALU.add),
                                              ["pO%d" % oi, "comb", "acc%d" % ti], ["acc%d" % ti])
                        yo = [sb(S6, "yo%d" % i, [128, 1024], F32) for i in range(2)]
                        for i in range(16):
                            yi = i % 2
                            layer_norm(acc[:, i, :], "acc%d" % i, yo[yi][:], "yo%d" % yi, yo[yi][:], "yo%d" % yi)
                            P.dma(out[i * 128:(i + 1) * 128, :], yo[yi][:], reads=["yo%d" % yi], writes=["out%d" % yi], sem="outd%d" % yi)
        fin_reads = ["out0", "out1"] + ["dbg_" + n for n in dbg_out]
        P.add("sp", lambda e: e.nop(), reads=fin_reads, sem="fin")
        if n_stage < 6:
            pass
        cnt = P.emit(G)
        nsem = len(cnt)
    return nc, dbg_out, nsem


def prep_shared(inp):
    f = lambda a: np.ascontiguousarray(np.asarray(a, dtype=np.float32))
    w_in = f(inp["w_in"])[0]
    sh = {}
    q = w_in[:, 0:512].reshape(1024, 2, 4, 64).transpose(0, 2, 1, 3).reshape(1024, 512)
    w1 = np.concatenate([q, w_in[:, 512:640], w_in[:, 640:768], w_in[:, 768:896], w_in[:, 1024:1152],
                         w_in[:, 896:1024], w_in[:, 1152:1280], w_in[:, 1280:1304]], axis=1)
    sh["w1t"] = tile_w(w1)
    sh["w4t"] = tile_w(w_in[:, 1304:3352])
    sh["wgtt"] = tile_w(w_in[:, 3352:4376])
    sh["wmgt"] = tile_w(w_in[:, 4376:6424])
    for kv in "kv":
        cw1 = f(inp["cmp_%s_w1" % kv])[0]
        r = cw1.reshape(32, 64, 256).transpose(1, 0, 2).reshape(64, 32 * 256)
        sh["cw1" + kv] = np.ascontiguousarray(np.concatenate([r, r], axis=0))
        pos = f(inp["cmp_pos_" + kv])[0]
        sh["cpos" + kv] = np.ascontiguousarray(np.concatenate([pos.T, pos.T], axis=0))
        sh["cb1" + kv] = np.ascontiguousarray(f(inp["cmp_%s_b1" % kv])[0].reshape(2, 128).T)
    w2k = f(inp["cmp_k_w2"])[0]
    sh["cw2k"] = tile_w(np.concatenate([w2k, w2k], axis=1))
    sh["cw2v"] = tile_w(f(inp["cmp_v_w2"])[0])
    b2k = f(inp["cmp_k_b2"])[0]
    sh["cb2k"] = np.ascontiguousarray(np.concatenate([b2k, b2k])[:, None])
    sh["cb2v"] = f(inp["cmp_v_b2"])[0]
    sh["gng8"] = np.ascontiguousarray(f(inp["ret_gn_g"])[0].reshape(8, 128).T)
    sh["gnb8"] = np.ascontiguousarray(f(inp["ret_gn_b"])[0].reshape(8, 128).T)
    sh["wat"] = tile_w(f(inp["w_up_attn"])[0])
    sh["wrt"] = tile_w(f(inp["w_up_ret"])[0])
    sh["wot"] = tile_w(f(inp["w_out"])[0])
    for n in ("ln1_g", "ln1_b", "ln2_g", "ln2_b"):
        sh[n.replace("_", "")] = f(inp[n])[0]
    rg = f(inp["router_group_w"])[0]
    ri = f(inp["router_inner_w"])[0]
    sh["wrout"] = tile_w(np.concatenate([rg, ri.transpose(1, 0, 2).reshape(1024, 32)], axis=1))
    sh["brout"] = np.ascontiguousarray(np.concatenate([f(inp["router_group_b"])[0], f(inp["router_inner_b"])[0].reshape(32)]))
    wg = f(inp["expert_w_gate"])[0]
    wu = f(inp["expert_w_up"])[0]
    wd = f(inp["expert_w_down"])[0]
    we = np.empty((32, 128, 12288), np.float32)
    we[:, :, 0:4096] = wg.reshape(32, 8, 128, 512).transpose(0, 2, 1, 3).reshape(32, 128, 4096)
    we[:, :, 4096:8192] = wu.reshape(32, 8, 128, 512).transpose(0, 2, 1, 3).reshape(32, 128, 4096)
    we[:, :, 8192:12288] = wd.reshape(32, 4, 128, 1024).transpose(0, 2, 1, 3).reshape(32, 128, 4096)
    sh["wexp"] = we
    return sh


def make_in_maps(inp):
    sh = prep_shared(inp)
    x = np.asarray(inp["x"], dtype=np.float32)
    maps = []
    for core in range(8):
        b, c = core // 2, core % 2
        m = dict(sh)
        xb = x[b]
        own = xb.reshape(16, 2, 128, 1024)[:, c].reshape(TO, 1024)
        m["xT"] = np.ascontiguousarray(xb.T)
        m["xTo"] = np.ascontiguousarray(own.T)
        m["xo"] = np.ascontiguousarray(own)
        for k, v in make_consts(c).items():
            if not k.startswith("_"):
                m["c_" + k] = v
        maps.append(m)
    return maps


_PROG_CACHE = {}


def kernel(**inputs):
    if "prog" not in _PROG_CACHE:
        _PROG_CACHE["prog"] = build_program()
    nc, _, _ = _PROG_CACHE["prog"]
    maps = make_in_maps(inputs)
    res = run_bass_kernel_spmd(nc, maps, core_ids=list(range(8)))
    outp = np.empty((4, 16, 2, 128, 1024), np.float32)
    for core in range(8):
        b, c = core // 2, core % 2
        outp[b, :, c] = res.results[core]["out"].reshape(16, 128, 1024)
    return outp.reshape(4, T, 1024)
```

```python
import numpy as np
import ml_dtypes
import concourse.bass as bass
import concourse.mybir as mybir
from concourse.bass_utils import run_bass_kernel_spmd
from contextlib import ExitStack

F32 = mybir.dt.float32
BF16 = mybir.dt.bfloat16
AF = mybir.ActivationFunctionType
ALU = mybir.AluOpType
NPBF = ml_dtypes.bfloat16

T = 4096
D = 1024
TO = 2048
NEGM = -30000.0
LN_EPS = 1e-5
ALPHA = 2.0 ** 0.25
DEBUG = {}


class Op:
    __slots__ = ("eng", "fn", "reads", "writes", "dma", "sem", "deps", "needs_inc", "idx", "id", "extra")

    def __init__(self, eng, fn, reads, writes, dma, sem):
        self.eng = eng
        self.fn = fn
        self.reads = tuple(reads)
        self.writes = tuple(writes)
        self.dma = dma
        self.sem = sem
        self.deps = []
        self.needs_inc = dma
        self.idx = 0
        self.extra = ()


class _Rec:
    def __getattr__(self, name):
        return lambda *a, **k: (name, a, k)


_REC = _Rec()


class Prog:
    ENGS = ("pe", "act", "dve", "pool", "sp")

    def __init__(self, nc, same_eng_sync=True):
        self.nc = nc
        self.ops = []
        self.same_eng_sync = same_eng_sync
        self.last_by_sem = {}
        self.psum_keys = set()

    def add(self, eng, fn, reads=(), writes=(), dma=False, sem=None):
        lim = DEBUG.get("max_ops")
        self.nadd = getattr(self, "nadd", -1) + 1
        if (lim is not None and self.nadd >= lim and sem not in ("dbg", "fin")) or self.nadd in DEBUG.get("skip", ()):
            return Op(eng, None, reads, writes, dma, sem)
        if dma and sem is None:
            sem = "dma_" + str(writes[0])
        if not dma:
            sem = "eng_" + eng
        op = Op(eng, fn(_REC), reads, writes, dma, sem)
        if DEBUG.get("trace_ops"):
            print(len(self.ops), eng, op.fn[0], reads, writes)
        op.id = len(self.ops)
        self.ops.append(op)
        self.last_by_sem[sem] = op
        return op

    def pe(self, fn, reads=(), writes=()):
        return self.add("pe", fn, reads, writes)

    def act(self, fn, reads=(), writes=()):
        return self.add("act", fn, reads, writes)

    def dve(self, fn, reads=(), writes=()):
        return self.add("dve", fn, reads, writes)

    def pool(self, fn, reads=(), writes=()):
        return self.add("pool", fn, reads, writes)

    def dma(self, out, in_, reads=(), writes=(), sem=None, q="sp", **kw):
        return self.add(q, lambda e: e.dma_start(out=out, in_=in_, **kw), reads, writes, dma=True, sem=sem)

    def barrier(self):
        lasts = list(self.last_by_sem.values())
        for eng in self.ENGS:
            op = self.add(eng, lambda e: e.nop())
            op.extra = tuple(lasts)
        self.last_by_sem = {k: v for k, v in self.last_by_sem.items() if k.startswith("eng_")}

    def analyze(self):
        state = {}
        for op in self.ops:
            deps = set(op.extra)
            for k in op.reads:
                st = state.get(k)
                if st:
                    deps.update(st[0])
                    if k in self.psum_keys:
                        deps.update(r for r in st[1] if r.eng != op.eng)
            for k in op.writes:
                st = state.get(k)
                if st is None:
                    st = state[k] = [[], []]
                if st[1]:
                    deps.update(st[1])
                    deps.update(st[0])
                    st[0] = [op]
                    st[1] = []
                else:
                    same_group = op.dma and all(w.dma and w.sem == op.sem for w in st[0])
                    if same_group:
                        st[0].append(op)
                    else:
                        deps.update(st[0])
                        st[0] = [op]
            for k in op.reads:
                st = state.get(k)
                if st is None:
                    st = state[k] = [[], []]
                st[1].append(op)
            deps.discard(op)
            red = {}
            for d in deps:
                if (not d.dma) and (not op.dma) and d.eng == op.eng:
                    if op.eng == "pe" or not self.same_eng_sync:
                        continue
                cur = red.get(d.sem)
                if cur is None or d.id > cur.id:
                    red[d.sem] = d
            op.deps = list(red.values())
            for d in op.deps:
                d.needs_inc = True
        cnt = {}
        for op in self.ops:
            if op.needs_inc:
                cnt[op.sem] = cnt.get(op.sem, 0) + 1
                op.idx = cnt[op.sem]
        self.sem_names = sorted(cnt.keys())
        return cnt

    def emit(self, stack):
        nc = self.nc
        cnt = self.analyze()
        sems = {}
        for name in self.sem_names:
            sems[name] = stack.enter_context(nc.semaphore(name))
        block = stack.enter_context(nc.Block())
        per_eng = {e: [o for o in self.ops if o.eng == e] for e in self.ENGS}

        def run(eng_obj, ops):
            known = {}
            for op in ops:
                for d in op.deps:
                    val = d.idx * (16 if d.dma else 1)
                    if known.get(d.sem, 0) < val:
                        eng_obj.wait_ge(sems[d.sem], val)
                        known[d.sem] = val
                name, a, k = op.fn
                inst = getattr(eng_obj, name)(*a, **k)
                if op.needs_inc:
                    inst.then_inc(sems[op.sem], 16 if op.dma else 1)

        @block.sync
        def _(e):
            run(e, per_eng["sp"])

        @block.tensor
        def _(e):
            run(e, per_eng["pe"])

        @block.scalar
        def _(e):
            run(e, per_eng["act"])

        @block.vector
        def _(e):
            run(e, per_eng["dve"])

        @block.gpsimd
        def _(e):
            run(e, per_eng["pool"])
        return cnt


class Ring:
    def __init__(self, items):
        self.items = items
        self.i = 0

    def next(self):
        it = self.items[self.i % len(self.items)]
        self.i += 1
        return it


def tile_w(w):
    K, N = w.shape
    return np.ascontiguousarray(w.reshape(K // 128, 128, N).transpose(1, 0, 2).reshape(128, -1))


def rope_tabs(pos, d, scale):
    half = d // 2
    inv = 10000.0 ** (-np.arange(half, dtype=np.float64) * 2.0 / d)
    ang = pos.astype(np.float64)[None, :] * inv[:, None]
    cos = np.cos(ang) * scale
    sin = np.sin(ang) * scale
    reps = 128 // half
    return (np.tile(cos, (reps, 1)).astype(np.float32), np.tile(sin, (reps, 1)).astype(np.float32))


def rot_lhsT(d):
    half = d // 2
    Pm = np.zeros((128, 128), np.float32)
    for blk in range(128 // d):
        o = blk * d
        for m in range(half):
            Pm[o + m, o + m + half] = -1.0
            Pm[o + m + half, o + m] = 1.0
    return np.ascontiguousarray(Pm.T)


_CONST_CACHE = {}


def make_consts(c):
    if c in _CONST_CACHE:
        return _CONST_CACHE[c]
    cs = {}
    own_pos = np.concatenate([np.arange(128) + (2 * i + c) * 128 for i in range(16)])
    allpos = np.arange(T)
    cs["cosK"], cs["sinK"] = rope_tabs(allpos, 64, 1.0)
    cs["cosQ"], cs["sinQ"] = rope_tabs(own_pos, 64, 0.125)
    cs["cosRK"], cs["sinRK"] = rope_tabs(allpos, 128, 128.0 ** -0.5)
    cs["cosRQ"], cs["sinRQ"] = rope_tabs(own_pos, 128, 1.0)
    cend = np.arange(256) * 16 + 31
    cs["cosC"], cs["sinC"] = rope_tabs(cend, 64, 1.0)
    cs["pt64"] = rot_lhsT(64).astype(NPBF)
    cs["pt128"] = rot_lhsT(128).astype(NPBF)
    cs["identb"] = np.eye(128, dtype=np.float32).astype(NPBF)
    E = np.zeros((128, 32, 128), np.float32)
    for j in range(32):
        for k in range(128):
            E[2 * j + k // 64, j, k] = 1.0
            E[64 + 2 * j + k // 64, j, k] = 1.0
    cs["eall"] = E.reshape(128, -1).astype(NPBF)
    wm = np.zeros((128, 6, 128), np.float32)
    kk = np.arange(128)[:, None]
    tt = np.arange(128)[None, :]
    for r in range(6):
        dj = (r - 4) - c
        tk = dj * 128 + kk
        ok = (tk <= tt) & (tt - tk < 512)
        wm[:, r, :] = np.where(ok, 0.0, NEGM)
    cs["wmask"] = wm.reshape(128, -1).astype(NPBF)
    cm = np.zeros((128, 2, 16, 128), np.float32)
    for a in range(2):
        for i in range(16):
            G = 2 * i + c
            n = a * 128 + kk
            t = G * 128 + tt
            cm[:, a, i, :] = np.where(16 * n + 31 <= t, 0.0, NEGM)
    cs["cmpmask"] = cm.reshape(128, -1).astype(NPBF)
    cstart = np.arange(255) * 16
    sstart = np.arange(64) * 64
    ov = np.clip(np.minimum(cstart[None, :] + 32, sstart[:, None] + 64) - np.maximum(cstart[None, :], sstart[:, None]), 0, None) / 16.0
    ovT = np.zeros((256, 64), np.float32)
    ovT[:255] = ov.T
    cs["ovT"] = np.ascontiguousarray(ovT.reshape(2, 128, 64).transpose(1, 0, 2).reshape(128, -1)).astype(NPBF)
    tkm = np.zeros((128, 16, 64), np.float32)
    tkb = np.zeros((128, 16, 64), np.float32)
    for i in range(16):
        G = 2 * i + c
        for p in range(128):
            bt = (G * 128 + p) // 64
            for s in range(64):
                if s == 0:
                    tkb[p, i, s] = 1e9
                elif s == bt:
                    tkb[p, i, s] = 2e9
                elif s == bt - 1:
                    tkb[p, i, s] = 3e9
                elif s <= bt:
                    tkm[p, i, s] = 1.0
                else:
                    tkb[p, i, s] = -1e9 - 1e6 * s
    cs["tkm"] = tkm.reshape(128, -1)
    cs["tkb"] = tkb.reshape(128, -1)
    gam = 1.0 - 2.0 ** (-5.0 - np.arange(4, dtype=np.float64))
    lg = np.log(gam)
    m = np.arange(256)[:, None]
    cq = np.arange(128)[None, :]
    qq = 128 * c + cq
    Dc = np.zeros((128, 2, 4, 128), np.float32)
    for h in range(4):
        dd = np.where(qq >= m, np.exp(np.maximum(qq - m, 0) * lg[h]), 0.0)
        Dc[:, :, h, :] = dd.reshape(2, 128, 128).transpose(1, 0, 2)
    cs["Dc"] = Dc.reshape(128, -1)
    xi = np.zeros((128, 4, 128), np.float32)
    for h in range(4):
        xi[:, h, :] = np.exp((qq + 1.0) * lg[h])
    cs["xi"] = xi.reshape(128, -1)
    zt = np.zeros((128, 2, 4), np.float32)
    for h in range(4):
        zt[:, :, h] = np.exp((255.0 - np.arange(256)) * lg[h]).reshape(2, 128).T
    cs["zeta"] = zt.reshape(128, -1)
    cs["_decay256"] = [float(np.exp(256.0 * lg[h])) for h in range(4)]
    _CONST_CACHE[c] = cs
    return cs


CONST_SHAPES = None


def build_program(n_stage=6, debug=()):
    nc = bass.Bass("TRN2", target_bir_lowering=False)
    cs0 = make_consts(0)
    dram = {}

    def din(name, shape, dt=F32):
        dram[name] = nc.dram_tensor(name, list(shape), dt, kind="ExternalInput").ap()
        return dram[name]

    xT = din("xT", [1024, T])
    xTo = din("xTo", [1024, TO])
    xo = din("xo", [TO, 1024])
    w1t = din("w1t", [128, 8 * 1304])
    w4t = din("w4t", [128, 8 * 2048])
    wmgt = din("wmgt", [128, 8 * 2048])
    cw1 = {kv: din("cw1" + kv, [128, 32 * 256]) for kv in "kv"}
    cpos = {kv: din("cpos" + kv, [128, 32]) for kv in "kv"}
    cb1 = {kv: din("cb1" + kv, [128, 2]) for kv in "kv"}
    cw2k = din("cw2k", [128, 2 * 128])
    cw2v = din("cw2v", [128, 2 * 64])
    cb2k = din("cb2k", [128, 1])
    cb2v = din("cb2v", [64])
    gng8 = din("gng8", [128, 8])
    gnb8 = din("gnb8", [128, 8])
    wgtt = din("wgtt", [128, 8 * 1024])
    wat = din("wat", [128, 4 * 1024])
    wrt = din("wrt", [128, 8 * 1024])
    wot = din("wot", [128, 8 * 1024])
    ln1g = din("ln1g", [1024])
    ln1b = din("ln1b", [1024])
    ln2g = din("ln2g", [1024])
    ln2b = din("ln2b", [1024])
    wrout = din("wrout", [128, 8 * 36])
    brout = din("brout", [36])
    wexp = din("wexp", [32, 128, 12288])
    cdr = {}
    for k, v in cs0.items():
        if k.startswith("_"):
            continue
        cdr[k] = din("c_" + k, v.shape, BF16 if v.dtype == NPBF else F32)
    out = nc.dram_tensor("out", [TO, 1024], F32, kind="ExternalOutput").ap()
    dbg_out = {}

    decay256 = cs0["_decay256"]

    with ExitStack() as G:
        P = Prog(nc)

        def sb(stack, name, shape, dt):
            return stack.enter_context(nc.sbuf_tensor(name, list(shape), dt))

        def ps(stack, name, shape, dt=F32):
            P.psum_keys.add(name)
            ncol = 512 if dt == F32 else 1024
            full = stack.enter_context(nc.psum_tensor(name, [128, ncol], dt))
            n = 1
            for d_ in shape[1:]:
                n *= d_
            v = full[0:shape[0], 0:n]
            if len(shape) == 3:
                v = v.rearrange("p (a b) -> p a b", a=shape[1])
            return v

        def dump(name, ap, shape, key):
            if name in debug:
                t = nc.dram_tensor("dbg_" + name, list(shape), ap.dtype, kind="ExternalOutput").ap()
                dbg_out[name] = t
                P.dma(t, ap, reads=[key], writes=["dbg_" + name], sem="dbg")

        identb = sb(G, "identb", [128, 128], BF16)
        P.dma(identb[:], cdr["identb"], writes=["identb"])
        wst = sb(G, "wst", [128, 4096], F32)
        cast_rr = [0]

        def load_cast(dst_ap, src_ap, n, dst_key, shape3=None):
            o = 0
            while o < n:
                m = min(4096, n - o)
                P.dma(wst[:, 0:m], src_ap[:, o:o + m], writes=["wst"])
                d = dst_ap[:, o:o + m]
                if cast_rr[0] % 2 == 0:
                    P.act(lambda e, d=d, m=m: e.copy(out=d, in_=wst[:, 0:m]), ["wst"], [dst_key])
                else:
                    P.dve(lambda e, d=d, m=m: e.tensor_copy(out=d, in_=wst[:, 0:m]), ["wst"], [dst_key])
                cast_rr[0] += 1
                o += m

        x1T = sb(G, "x1T", [128, 8, TO], BF16)
        wst3 = wst[:].rearrange("p (k n) -> p k n", k=8)
        A_ = ExitStack()
        oattnT = sb(A_, "oattnT", [128, 4, TO], BF16)

        with ExitStack() as SN:
            QT = sb(SN, "QT", [128, 4, TO], BF16)
            slckT = sb(SN, "slckT", [128, T], BF16)
            winkT = sb(SN, "winkT", [128, T], BF16)
            slcv1 = sb(SN, "slcv1", [128, 32, 2, 65], BF16)
            winv1 = sb(SN, "winv1", [128, 32, 2, 65], BF16)
            gates = sb(SN, "gates", [128, 16, 24], F32)
            kcmpT = sb(SN, "kcmpT", [128, 256], BF16)
            vcmp1 = sb(SN, "vcmp1", [128, 2, 2, 65], BF16)
            pt64 = sb(SN, "pt64", [128, 128], BF16)
            P.dma(pt64[:], cdr["pt64"], writes=["pt64"])
            P.dve(lambda e: e.memset(slcv1[:].rearrange("p a g d -> p (a g d)"), 1.0), [], ["slcv1"])
            P.dve(lambda e: e.memset(winv1[:].rearrange("p a g d -> p (a g d)"), 1.0), [], ["winv1"])
            P.dve(lambda e: e.memset(kcmpT[:], 0.0), [], ["kcmpT"])
            P.dve(lambda e: e.memset(vcmp1[:].rearrange("p a g d -> p (a g d)"), 0.0), [], ["vcmp1"])
            P.dve(lambda e: e.memset(vcmp1[:, :, :, 64:65], 1.0), [], ["vcmp1"])

            with ExitStack() as S12:
                cmpT = {"k": sb(S12, "cmpkT", [128, T], BF16), "v": sb(S12, "cmpvT", [128, T], BF16)}
                with ExitStack() as S1:
                    Wn = sb(S1, "Wn", [128, 8, 1304], BF16)
                    load_cast(Wn[:].rearrange("p k n -> p (k n)"), w1t, 8 * 1304, "Wn")
                    xb = [sb(S1, "xb%d" % i, [128, 8, 512], BF16) for i in range(2)]
                    tabs = [sb(S1, "tab%d" % i, [128, 2, 512], F32) for i in range(2)]
                    ybf = [sb(S1, "ybf%d" % i, [128, 512], BF16) for i in range(2)]
                    t1 = [sb(S1, "t1_%d" % i, [128, 512], F32) for i in range(2)]
                    t2 = [sb(S1, "t2_%d" % i, [128, 512], F32) for i in range(2)]
                    pj = [ps(S1, "pj%d" % i, [128, 512]) for i in range(3)]
                    prot = [ps(S1, "prot%d" % i, [128, 512]) for i in range(2)]
                    pv = [ps(S1, "pv%d" % i, [128, 256]) for i in range(2)]
                    pjr = Ring(list(range(3)))
                    rr = Ring(list(range(2)))
                    pvr = Ring(list(range(2)))
                    xTv = xT.rearrange("(k p) t -> p k t", p=128)
                    xTov = xTo.rearrange("(k p) t -> p k t", p=128)

                    def load_x(src_view, c0, n, slot):
                        P.dma(wst3[:, :, 0:n], src_view[:, :, c0:c0 + n], writes=["wst"])
                        P.act(lambda e: e.copy(out=xb[slot][:, 0:4, 0:n], in_=wst3[:, 0:4, 0:n]), ["wst"], ["xb%d" % slot])
                        P.dve(lambda e: e.tensor_copy(out=xb[slot][:, 4:8, 0:n], in_=wst3[:, 4:8, 0:n]), ["wst"], ["xb%d" % slot])

                    def proj_fm(col0, slot, n=512):
                        pi = pjr.next()
                        for k in range(8):
                            P.pe(lambda e, k=k, pi=pi: e.matmul(pj[pi][:, 0:n], lhsT=Wn[:, k, col0:col0 + 128], rhs=xb[slot][:, k, 0:n],
                                                                 start=(k == 0), stop=(k == 7)), ["Wn", "xb%d" % slot], ["pj%d" % pi])
                        return pi

                    def rope_fm(pi, tslot, dst_ap, dst_key, ptm, ptkey, n=512, src=None, srckey=None):
                        r = rr.next()
                        srcap = pj[pi][:, 0:n] if src is None else src
                        sk = ("pj%d" % pi) if srckey is None else srckey
                        P.act(lambda e: e.copy(out=ybf[r][:, 0:n], in_=srcap), [sk], ["ybf%d" % r])
                        P.pe(lambda e: e.matmul(prot[r][:, 0:n], lhsT=ptm[:], rhs=ybf[r][:, 0:n], start=True, stop=True),
                             [ptkey, "ybf%d" % r], ["prot%d" % r])
                        P.dve(lambda e: e.tensor_tensor(out=t1[r][:, 0:n], in0=srcap, in1=tabs[tslot][:, 0, 0:n], op=ALU.mult),
                              [sk, "tab%d" % tslot], ["t1_%d" % r])
                        P.dve(lambda e: e.tensor_tensor(out=t2[r][:, 0:n], in0=prot[r][:, 0:n], in1=tabs[tslot][:, 1, 0:n], op=ALU.mult),
                              ["prot%d" % r, "tab%d" % tslot], ["t2_%d" % r])
                        P.pool(lambda e: e.tensor_tensor(out=dst_ap, in0=t1[r][:, 0:n], in1=t2[r][:, 0:n], op=ALU.add),
                               ["t1_%d" % r, "t2_%d" % r], [dst_key])

                    for ch in range(8):
                        slot = ch % 2
                        c0 = ch * 512
                        load_x(xTv, c0, 512, slot)
                        P.dma(tabs[slot][:, 0, :], cdr["cosK"][:, c0:c0 + 512], writes=["tab%d" % slot])
                        P.dma(tabs[slot][:, 1, :], cdr["sinK"][:, c0:c0 + 512], writes=["tab%d" % slot])
                        for col0, kv in ((512, "k"), (640, "v")):
                            pi = proj_fm(col0, slot)
                            P.act(lambda e, pi=pi, kv=kv: e.copy(out=cmpT[kv][:, c0:c0 + 512], in_=pj[pi][:]), ["pj%d" % pi], ["cmp" + kv + "T"])
                        for col0, dst, dk in ((768, slckT, "slckT"), (896, winkT, "winkT")):
                            pi = proj_fm(col0, slot)
                            rope_fm(pi, slot, dst[:, c0:c0 + 512], dk, pt64, "pt64")
                        for tt in range(4):
                            vi = pvr.next()
                            for k in range(8):
                                P.pe(lambda e, k=k, vi=vi, tt=tt: e.matmul(pv[vi][:], lhsT=xb[slot][:, k, tt * 128:(tt + 1) * 128], rhs=Wn[:, k, 1024:1280],
                                                                            start=(k == 0), stop=(k == 7)), ["Wn", "xb%d" % slot], ["pv%d" % vi])
                            tg = ch * 4 + tt
                            P.act(lambda e, vi=vi, tg=tg: e.copy(out=slcv1[:, tg, :, 0:64], in_=pv[vi][:, 0:128].rearrange("p (g d) -> p g d", g=2)),
                                  ["pv%d" % vi], ["slcv1"])
                            P.dve(lambda e, vi=vi, tg=tg: e.tensor_copy(out=winv1[:, tg, :, 0:64], in_=pv[vi][:, 128:256].rearrange("p (g d) -> p g d", g=2)),
                                  ["pv%d" % vi], ["winv1"])
                    for oc in range(4):
                        slot = oc % 2
                        c0 = oc * 512
                        load_x(xTov, c0, 512, slot)
                        P.dma(tabs[slot][:, 0, :], cdr["cosQ"][:, c0:c0 + 512], writes=["tab%d" % slot])
                        P.dma(tabs[slot][:, 1, :], cdr["sinQ"][:, c0:c0 + 512], writes=["tab%d" % slot])
                        for hh in range(4):
                            pi = proj_fm(hh * 128, slot)
                            rope_fm(pi, slot, QT[:, hh, c0:c0 + 512], "QT", pt64, "pt64")
                        for tt in range(4):
                            vi = pvr.next()
                            for k in range(8):
                                P.pe(lambda e, k=k, vi=vi, tt=tt: e.matmul(pv[vi][:, 0:24], lhsT=xb[slot][:, k, tt * 128:(tt + 1) * 128], rhs=Wn[:, k, 1280:1304],
                                                                            start=(k == 0), stop=(k == 7)), ["Wn", "xb%d" % slot], ["pv%d" % vi])
                            tg = oc * 4 + tt
                            P.act(lambda e, vi=vi, tg=tg: e.activation(out=gates[:, tg, :], in_=pv[vi][:, 0:24], func=AF.Sigmoid), ["pv%d" % vi], ["gates"])
                    dump("QT", QT[:].rearrange("p a t -> p (a t)"), [128, 4 * TO], "QT")
                    dump("slckT", slckT[:], [128, T], "slckT")
                    dump("cmpkT", cmpT["k"][:], [128, T], "cmpkT")
                    dump("slcv1", slcv1[:].rearrange("p a g d -> p (a g d)"), [128, 32 * 130], "slcv1")
                    dump("gates", gates[:].rearrange("p a g -> p (a g)"), [128, 16 * 24], "gates")
                P.barrier()
                if n_stage >= 2:
                    with ExitStack() as S2:
                        w1b = sb(S2, "w1b", [128, 32, 256], BF16)
                        posT = sb(S2, "posT", [128, 32], F32)
                        posTb = sb(S2, "posTb", [128, 32], BF16)
                        b1 = sb(S2, "b1", [128, 2], F32)
                        bias1 = sb(S2, "bias1", [128, 2], F32)
                        w2kf = sb(S2, "w2kf", [128, 2, 128], F32)
                        w2k = sb(S2, "w2k", [128, 2, 128], BF16)
                        w2vf = sb(S2, "w2vf", [128, 2, 64], F32)
                        w2v = sb(S2, "w2v", [128, 2, 64], BF16)
                        b2k = sb(S2, "b2k", [128, 1], F32)
                        b2v = sb(S2, "b2v", [128, 64], F32)
                        tabC = sb(S2, "tabC", [128, 2, 256], F32)
                        h1 = sb(S2, "h1", [128, 2, 256], BF16)
                        xg = sb(S2, "xg", [128, 256], F32)
                        ug = sb(S2, "ug", [128, 256], F32)
                        sg_ = sb(S2, "sg_", [128, 256], F32)
                        yk = sb(S2, "yk", [128, 256], F32)
                        ykb = sb(S2, "ykb", [128, 256], BF16)
                        tk1 = sb(S2, "tk1", [128, 256], F32)
                        tk2 = sb(S2, "tk2", [128, 256], F32)
                        ph = [ps(S2, "ph%d" % i, [128, 256]) for i in range(2)]
                        pcv = ps(S2, "pcv", [128, 2])
                        pkc = ps(S2, "pkc", [128, 256])
                        prk = ps(S2, "prk", [128, 256])
                        pvc = ps(S2, "pvc", [128, 64])
                        P.dma(w2kf[:].rearrange("p a n -> p (a n)"), cw2k, writes=["w2kf"])
                        P.dve(lambda e: e.tensor_copy(out=w2k[:], in_=w2kf[:]), ["w2kf"], ["w2k"])
                        P.dma(w2vf[:].rearrange("p a n -> p (a n)"), cw2v, writes=["w2vf"])
                        P.dve(lambda e: e.tensor_copy(out=w2v[:], in_=w2vf[:]), ["w2vf"], ["w2v"])
                        P.dma(b2k[:], cb2k, writes=["b2k"])
                        P.dma(b2v[:], cb2v.partition_broadcast(128), writes=["b2v"])
                        P.dma(tabC[:, 0, :], cdr["cosC"], writes=["tabC"])
                        P.dma(tabC[:, 1, :], cdr["sinC"], writes=["tabC"])
                        for kv in "kv":
                            load_cast(w1b[:].rearrange("p l n -> p (l n)"), cw1[kv], 32 * 256, "w1b")
                            P.dma(posT[:], cpos[kv], writes=["posT"])
                            P.dve(lambda e: e.tensor_copy(out=posTb[:], in_=posT[:]), ["posT"], ["posTb"])
                            P.dma(b1[:], cb1[kv], writes=["b1"])
                            for ht in range(2):
                                for l in range(32):
                                    P.pe(lambda e, ht=ht, l=l: e.matmul(pcv[:, ht:ht + 1], lhsT=w1b[0:64, l, ht * 128:(ht + 1) * 128], rhs=posTb[0:64, l:l + 1],
                                                                         start=(l == 0), stop=(l == 31)), ["w1b", "posTb"], ["pcv"])
                            P.dve(lambda e: e.tensor_tensor(out=bias1[:], in0=pcv[:], in1=b1[:], op=ALU.add), ["pcv", "b1"], ["bias1"])
                            for g in range(2):
                                gp = slice(g * 64, (g + 1) * 64)
                                for ht in range(2):
                                    for l in range(32):
                                        P.pe(lambda e, ht=ht, l=l, gp=gp, kv=kv: e.matmul(ph[ht][:, 0:255], lhsT=w1b[gp, l, ht * 128:(ht + 1) * 128],
                                                                                        rhs=cmpT[kv][gp, l:l + 16 * 254 + 1:16],
                                                                                        start=(l == 0), stop=(l == 31)), ["w1b", "cmp" + kv + "T"], ["ph%d" % ht])
                                    P.act(lambda e, ht=ht: e.activation(out=xg[:, 0:255], in_=ph[ht][:, 0:255], func=AF.Identity, bias=bias1[:, ht:ht + 1], scale=1.0),
                                          ["ph%d" % ht, "bias1"], ["xg"])
                                    P.dve(lambda e: e.tensor_tensor(out=ug[:, 0:255], in0=xg[:, 0:255], in1=xg[:, 0:255], op=ALU.mult), ["xg"], ["ug"])
                                    P.dve(lambda e: e.tensor_scalar(out=ug[:, 0:255], in0=ug[:, 0:255], scalar1=0.044715, scalar2=1.0, op0=ALU.mult, op1=ALU.add), ["ug"], ["ug"])
                                    P.dve(lambda e: e.tensor_tensor(out=ug[:, 0:255], in0=ug[:, 0:255], in1=xg[:, 0:255], op=ALU.mult), ["ug", "xg"], ["ug"])
                                    P.act(lambda e: e.activation(out=sg_[:, 0:255], in_=ug[:, 0:255], func=AF.Sigmoid, scale=1.5957691216057308), ["ug"], ["sg_"])
                                    P.dve(lambda e, ht=ht: e.tensor_tensor(out=h1[:, ht, 0:255], in0=xg[:, 0:255], in1=sg_[:, 0:255], op=ALU.mult), ["xg", "sg_"], ["h1"])
                                if kv == "k":
                                    for ht in range(2):
                                        P.pe(lambda e, ht=ht: e.matmul(pkc[:, 0:255], lhsT=w2k[:, ht, :], rhs=h1[:, ht, 0:255], start=(ht == 0), stop=(ht == 1)),
                                             ["w2k", "h1"], ["pkc"])
                                    P.act(lambda e: e.activation(out=yk[:, 0:255], in_=pkc[:, 0:255], func=AF.Identity, bias=b2k[:, 0:1], scale=1.0), ["pkc", "b2k"], ["yk"])
                                    P.act(lambda e: e.copy(out=ykb[:, 0:255], in_=yk[:, 0:255]), ["yk"], ["ykb"])
                                    P.pe(lambda e: e.matmul(prk[:, 0:255], lhsT=pt64[:], rhs=ykb[:, 0:255], start=True, stop=True), ["pt64", "ykb"], ["prk"])
                                    P.dve(lambda e: e.tensor_tensor(out=tk1[:, 0:255], in0=yk[:, 0:255], in1=tabC[:, 0, 0:255], op=ALU.mult), ["yk", "tabC"], ["tk1"])
                                    P.dve(lambda e: e.tensor_tensor(out=tk2[:, 0:255], in0=prk[:, 0:255], in1=tabC[:, 1, 0:255], op=ALU.mult), ["prk", "tabC"], ["tk2"])
                                    P.dve(lambda e, gp=gp: e.tensor_tensor(out=kcmpT[gp, 0:255], in0=tk1[gp, 0:255], in1=tk2[gp, 0:255], op=ALU.add), ["tk1", "tk2"], ["kcmpT"])
                                else:
                                    for a in range(2):
                                        cntn = 128 if a == 0 else 127
                                        for ht in range(2):
                                            P.pe(lambda e, ht=ht, a=a, cntn=cntn: e.matmul(pvc[0:cntn, :], lhsT=h1[:, ht, a * 128:a * 128 + cntn], rhs=w2v[:, ht, :],
                                                                                            start=(ht == 0), stop=(ht == 1)), ["w2v", "h1"], ["pvc"])
                                        P.dve(lambda e, a=a, cntn=cntn, g=g: e.tensor_tensor(out=vcmp1[0:cntn, a, g, 0:64], in0=pvc[0:cntn, :], in1=b2v[0:cntn, :], op=ALU.add),
                                              ["pvc", "b2v"], ["vcmp1"])
                        dump("kcmpT", kcmpT[:], [128, 256], "kcmpT")
                        dump("vcmp1", vcmp1[:].rearrange("p a g d -> p (a g d)"), [128, 260], "vcmp1")
                    P.barrier()
            P.barrier()
            if n_stage >= 3:
                with ExitStack() as S3:
                    eall = sb(S3, "eall", [128, 32, 128], BF16)
                    wmask = sb(S3, "wmask", [128, 6, 128], BF16)
                    cmpmask = sb(S3, "cmpmask", [128, 2, 16, 128], BF16)
                    ovT = sb(S3, "ovT", [128, 2, 64], BF16)
                    tkm = sb(S3, "tkm", [128, 16, 64], F32)
                    tkb = sb(S3, "tkb", [128, 16, 64], F32)
                    P.dma(eall[:].rearrange("p a k -> p (a k)"), cdr["eall"], writes=["eall"])
                    P.dma(wmask[:].rearrange("p a k -> p (a k)"), cdr["wmask"], writes=["wmask"])
                    P.dma(cmpmask[:].rearrange("p a i k -> p (a i k)"), cdr["cmpmask"], writes=["cmpmask"])
                    P.dma(ovT[:].rearrange("p a k -> p (a k)"), cdr["ovT"], writes=["ovT"])
                    P.dma(tkm[:].rearrange("p a k -> p (a k)"), cdr["tkm"], writes=["tkm"])
                    P.dma(tkb[:].rearrange("p a k -> p (a k)"), cdr["tkb"], writes=["tkb"])
                    eT = [sb(S3, "eT%d" % i, [128, 512], BF16) for i in range(3)]
                    oacc = sb(S3, "oacc", [128, 512], F32)
                    oab = sb(S3, "oab", [128, 512], BF16)
                    rz = sb(S3, "rz", [128, 4], F32)
                    coef = sb(S3, "coef", [128, 4], F32)
                    imp = sb(S3, "imp", [128, 64], F32)
                    score = sb(S3, "score", [128, 64], F32)
                    work = sb(S3, "work", [128, 64], F32)
                    m8 = sb(S3, "m8", [128, 16], F32)
                    nmk = [sb(S3, "nmk%d" % i_, [128, 2, 64], BF16) for i_ in range(2)]
                    nmT = sb(S3, "nmT", [128, 128], BF16)
                    pST = [ps(S3, "pST%d" % i, [128, 512]) for i in range(2)]
                    pA = ps(S3, "pA", [128, 4, 65])
                    pB = ps(S3, "pB", [128, 4, 64])
                    pS = ps(S3, "pS", [128, 4, 65])
                    pW = ps(S3, "pW", [128, 4, 65])
                    pTr = ps(S3, "pTr", [128, 128], BF16)
                    str_ = Ring([0, 1])
                    etr = Ring([0, 1, 2])

                    def scores(kT_ap, kkey, g, i, masks):
                        gp = slice(g * 64, (g + 1) * 64)
                        si = str_.next()
                        ei = etr.next()
                        nm = len(masks)
                        P.pe(lambda e: e.matmul(pST[si][:].rearrange("p (a t) -> p a t", a=4), lhsT=kT_ap, rhs=QT[gp, :, i * 128:(i + 1) * 128],
                                                start=True, stop=(nm == 0)), [kkey, "QT"], ["pST%d" % si])
                        for mi, (ml, mr, mkeys) in enumerate(masks):
                            P.pe(lambda e, ml=ml, mr=mr, mi=mi: e.matmul(pST[si][:].rearrange("p (a t) -> p a t", a=4), lhsT=ml, rhs=mr,
                                                                           start=False, stop=(mi == nm - 1)), mkeys, ["pST%d" % si])
                        P.act(lambda e: e.activation(out=eT[ei][:], in_=pST[si][:], func=AF.Exp), ["pST%d" % si], ["eT%d" % ei])
                        return ei

                    def bc4(ap):
                        return ap.unsqueeze(1).broadcast_to([ap.shape[0], 4, ap.shape[1]])

                    def finish_branch(pacc, pkey, i, g, br, first):
                        P.dve(lambda e: e.tensor_scalar(out=rz[:], in0=pacc[:, :, 64], scalar1=1e-30, scalar2=None, op0=ALU.max), [pkey], ["rz"])
                        P.dve(lambda e: e.reciprocal(out=rz[:], in_=rz[:]), ["rz"], ["rz"])
                        P.dve(lambda e: e.tensor_tensor(out=coef[:], in0=rz[:], in1=gates[:, i, g * 12 + br:g * 12 + 12:3], op=ALU.mult), ["rz", "gates"], ["coef"])
                        for hh in range(4):
                            o = oacc[:, g * 256 + hh * 64:g * 256 + (hh + 1) * 64]
                            if first:
                                P.dve(lambda e, hh=hh, o=o: e.tensor_scalar(out=o, in0=pacc[:, hh, 0:64], scalar1=coef[:, hh:hh + 1], scalar2=None, op0=ALU.mult),
                                      [pkey, "coef"], ["oacc"])
                            else:
                                P.dve(lambda e, hh=hh, o=o: e.scalar_tensor_tensor(out=o, in0=pacc[:, hh, 0:64], scalar=coef[:, hh:hh + 1], in1=o, op0=ALU.mult, op1=ALU.add),
                                      [pkey, "coef", "oacc"], ["oacc"])

                    tasks = []

                    def mk_cmp(i, g, a, na):
                        gp = slice(g * 64, (g + 1) * 64)

                        def sc():
                            return scores(kcmpT[gp, a * 128:(a + 1) * 128], "kcmpT", g, i,
                                          [(identb[:], bc4(cmpmask[:, a, i, :]), ["identb", "cmpmask"])])

                        def pvf(ei):
                            for hh in range(4):
                                P.pe(lambda e: e.matmul(pA[:, hh, :], lhsT=eT[ei][:, hh * 128:(hh + 1) * 128], rhs=vcmp1[:, a, g, :],
                                                        start=(a == 0 and hh == 0), stop=(a == na - 1 and hh == 3)), ["eT%d" % ei, "vcmp1"], ["pA"])
                                P.pe(lambda e: e.matmul(pB[:, hh, :], lhsT=eT[ei][:, hh * 128:(hh + 1) * 128], rhs=ovT[:, a, :],
                                                        start=(a == 0 and hh == 0), stop=(a == na - 1 and hh == 3)), ["eT%d" % ei, "ovT"], ["pB"])

                        def post():
                            finish_branch(pA, "pA", i, g, 0, True)
                            P.dve(lambda e: e.tensor_scalar(out=imp[:], in0=pB[:, 0, :], scalar1=rz[:, 0:1], scalar2=None, op0=ALU.mult), ["pB", "rz"], ["imp"])
                            for hh in range(1, 4):
                                P.dve(lambda e: e.scalar_tensor_tensor(out=imp[:], in0=pB[:, hh, :], scalar=rz[:, hh:hh + 1], in1=imp[:], op0=ALU.mult, op1=ALU.add),
                                      ["pB", "rz", "imp"], ["imp"])
                            P.dve(lambda e: e.tensor_tensor(out=score[:], in0=imp[:], in1=tkm[:, i, :], op=ALU.mult), ["imp", "tkm"], ["score"])
                            P.dve(lambda e: e.tensor_tensor(out=score[:], in0=score[:], in1=tkb[:, i, :], op=ALU.add), ["score", "tkb"], ["score"])
                            P.dve(lambda e: e.max(out=m8[:, 0:8], in_=score[:]), ["score"], ["m8"])
                            P.dve(lambda e: e.match_replace(out=work[:], in_to_replace=m8[:, 0:8], in_values=score[:], imm_value=-3.0e38), ["score", "m8"], ["work"])
                            P.dve(lambda e: e.max(out=m8[:, 8:16], in_=work[:]), ["work"], ["m8"])
                            P.dve(lambda e: e.tensor_scalar(out=nmk[g][:], in0=score[:].unsqueeze(1).broadcast_to([128, 2, 64]), scalar1=m8[:, 15:16], scalar2=NEGM,
                                                            op0=ALU.is_lt, op1=ALU.mult), ["score", "m8"], ["nmk%d" % g])
                            if ("imp%d_%d" % (i, g)) in debug:
                                dump("imp%d_%d" % (i, g), imp[:], [128, 64], "imp")
                                dump("score%d_%d" % (i, g), score[:], [128, 64], "score")
                                dump("m8%d_%d" % (i, g), m8[:], [128, 16], "m8")
                        return [None, sc, pvf, post if a == na - 1 else None]

                    def mk_win(i, g, idx, r, j, nw):
                        gp = slice(g * 64, (g + 1) * 64)

                        def sc():
                            return scores(winkT[gp, j * 128:(j + 1) * 128], "winkT", g, i,
                                          [(identb[:], bc4(wmask[:, r, :]), ["identb", "wmask"])])

                        def pvf(ei):
                            for hh in range(4):
                                P.pe(lambda e: e.matmul(pW[:, hh, :], lhsT=eT[ei][:, hh * 128:(hh + 1) * 128], rhs=winv1[:, j, g, :],
                                                        start=(idx == 0 and hh == 0), stop=(idx == nw - 1 and hh == 3)), ["eT%d" % ei, "winv1"], ["pW"])

                        def post():
                            finish_branch(pW, "pW", i, g, 2, False)
                        return [None, sc, pvf, post if idx == nw - 1 else None]

                    def tile_end_pe(i):
                        for ct in range(4):
                            P.pe(lambda e: e.transpose(out=pTr[:], in_=oab[:, ct * 128:(ct + 1) * 128], identity=identb[:]), ["oab", "identb"], ["pTr"])
                            P.dve(lambda e: e.tensor_copy(out=oattnT[:, ct, i * 128:(i + 1) * 128], in_=pTr[:]), ["pTr"], ["oattnT"])

                    def mk_slc(i, g, j, nj):
                        gp = slice(g * 64, (g + 1) * 64)

                        def pre():
                            P.pe(lambda e: e.transpose(out=pTr[:], in_=nmk[g][:].rearrange("p a s -> p (a s)"), identity=identb[:]), ["nmk%d" % g, "identb"], ["pTr"])
                            P.act(lambda e: e.copy(out=nmT[gp, :], in_=pTr[gp, :]), ["pTr"], ["nmT%d" % g])
                            if g == 0 and i > 0:
                                tile_end_pe(i - 1)

                        def sc():
                            masks = [(eall[gp, j, :], bc4(nmT[gp, :]), ["eall", "nmT%d" % g])]
                            if j >= 2 * i:
                                masks.append((identb[:], bc4(wmask[:, 4 + (j - 2 * i), :]), ["identb", "wmask"]))
                            return scores(slckT[gp, j * 128:(j + 1) * 128], "slckT", g, i, masks)

                        def pvf(ei):
                            for hh in range(4):
                                P.pe(lambda e: e.matmul(pS[:, hh, :], lhsT=eT[ei][:, hh * 128:(hh + 1) * 128], rhs=slcv1[:, j, g, :],
                                                        start=(j == 0 and hh == 0), stop=(j == nj - 1 and hh == 3)), ["eT%d" % ei, "slcv1"], ["pS"])

                        def post():
                            finish_branch(pS, "pS", i, g, 1, False)
                            if g == 1:
                                if ("oacc%d" % i) in debug:
                                    dump("oacc%d" % i, oacc[:], [128, 512], "oacc")
                                P.pool(lambda e: e.tensor_copy(out=oab[:], in_=oacc[:]), ["oacc"], ["oab"])
                        return [pre if j == 0 else None, sc, pvf, post if j == nj - 1 else None]

                    for i in range(16):
                        for g in range(2):
                            na = 1 if i < 8 else 2
                            for a in range(na):
                                tasks.append(mk_cmp(i, g, a, na))
                            js = [(r, 2 * i - 4 + r) for r in range(6) if 2 * i - 4 + r >= 0]
                            for idx, (r, j) in enumerate(js):
                                tasks.append(mk_win(i, g, idx, r, j, len(js)))
                            nj = 2 * i + 2
                            for j in range(nj):
                                tasks.append(mk_slc(i, g, j, nj))
                    nt = len(tasks)
                    eis = [None] * nt

                    def emit_score(k):
                        if tasks[k][0] is not None:
                            tasks[k][0]()
                        eis[k] = tasks[k][1]()

                    emit_score(0)
                    for k in range(nt):
                        if k + 1 < nt:
                            emit_score(k + 1)
                        tasks[k][2](eis[k])
                        if tasks[k][3] is not None:
                            tasks[k][3]()
                    tile_end_pe(15)
                    dump("oattnT", oattnT[:].rearrange("p a t -> p (a t)"), [128, 4 * TO], "oattnT")
                P.barrier()
        P.barrier()

        B_ = ExitStack()
        oretT = sb(B_, "oretT", [128, 8, TO], BF16)
        if n_stage >= 4:
            with ExitStack() as S4:
                W4 = sb(S4, "W4", [128, 8, 2048], BF16)
                load_cast(W4[:].rearrange("p k n -> p (k n)"), w4t, 8 * 2048, "W4")
                pt128 = sb(S4, "pt128", [128, 128], BF16)
                P.dma(pt128[:], cdr["pt128"], writes=["pt128"])
                Dc = sb(S4, "Dc", [128, 2, 4, 128], F32)
                xi = sb(S4, "xi", [128, 4, 128], F32)
                zeta = sb(S4, "zeta", [128, 2, 4], F32)
                P.dma(Dc[:].rearrange("p a h c -> p (a h c)"), cdr["Dc"], writes=["Dc"])
                P.dma(xi[:].rearrange("p h c -> p (h c)"), cdr["xi"], writes=["xi"])
                P.dma(zeta[:].rearrange("p a h -> p (a h)"), cdr["zeta"], writes=["zeta"])
                xst = wst3
                xb = sb(S4, "xb4", [128, 8, 512], BF16)
                xob = sb(S4, "xob4", [128, 8, 256], BF16)
                tabs = sb(S4, "tab4", [128, 2, 512], F32)
                tabq = sb(S4, "tabq4", [128, 2, 256], F32)
                ybf = sb(S4, "ybf4", [128, 512], BF16)
                t1 = sb(S4, "t1_4", [128, 512], F32)
                t2 = sb(S4, "t2_4", [128, 512], F32)
                kT = sb(S4, "kT4", [128, 4, 512], BF16)
                qT = sb(S4, "qT4", [128, 4, 256], BF16)
                qxT = sb(S4, "qxT4", [128, 4, 256], BF16)
                vtok = sb(S4, "vtok", [128, 4, 1024], BF16)
                kz = sb(S4, "kz", [128, 4, 4, 128], BF16)
                R = sb(S4, "R", [128, 4, 256], F32)
                Rb = sb(S4, "Rb", [128, 4, 256], BF16)
                sc = sb(S4, "sc", [128, 2, 128], BF16)
                st6 = sb(S4, "st6", [128, 6], F32)
                mv = sb(S4, "mv", [128, 2], F32)
                rstd = sb(S4, "rstd", [128, 1], F32)
                oretb = sb(S4, "oretb", [128, 1024], BF16)
                pj = [ps(S4, "pj4_%d" % i, [128, 512]) for i in range(2)]
                prot = ps(S4, "prot4", [128, 512])
                psc = ps(S4, "psc", [128, 2, 128])
                po = ps(S4, "po", [128, 256])
                pR = ps(S4, "pR", [128, 256])
                pTr = ps(S4, "pTr4", [128, 128], BF16)
                pjr = Ring([0, 1])
                P.dve(lambda e: e.memset(R[:], 0.0), [], ["R"])
                P.dve(lambda e: e.memset(Rb[:], 0.0), [], ["Rb"])
                xTv = xT.rearrange("(k p) t -> p k t", p=128)
                xTov = xTo.rearrange("(k p) t -> p k t", p=128)

                def rope4(pi, n, tab, tabkey, dst_ap, dst_key):
                    P.act(lambda e: e.copy(out=ybf[:, 0:n], in_=pj[pi][:, 0:n]), ["pj4_%d" % pi], ["ybf4"])
                    P.pe(lambda e: e.matmul(prot[:, 0:n], lhsT=pt128[:], rhs=ybf[:, 0:n], start=True, stop=True), ["pt128", "ybf4"], ["prot4"])
                    P.dve(lambda e: e.tensor_tensor(out=t1[:, 0:n], in0=pj[pi][:, 0:n], in1=tab[:, 0, 0:n], op=ALU.mult), ["pj4_%d" % pi, tabkey], ["t1_4"])
                    P.dve(lambda e: e.tensor_tensor(out=t2[:, 0:n], in0=prot[:, 0:n], in1=tab[:, 1, 0:n], op=ALU.mult), ["prot4", tabkey], ["t2_4"])
                    P.pool(lambda e: e.tensor_tensor(out=dst_ap, in0=t1[:, 0:n], in1=t2[:, 0:n], op=ALU.add), ["t1_4", "t2_4"], [dst_key])

                for gch in range(8):
                    c0 = gch * 512
                    o0 = gch * 256
                    P.dma(xst[:], xTv[:, :, c0:c0 + 512], writes=["wst"])
                    P.act(lambda e: e.copy(out=xb[:, 0:4, :], in_=xst[:, 0:4, :]), ["wst"], ["xb4"])
                    P.dve(lambda e: e.tensor_copy(out=xb[:, 4:8, :], in_=xst[:, 4:8, :]), ["wst"], ["xb4"])
                    P.dma(xst[:, :, 0:256], xTov[:, :, o0:o0 + 256], writes=["wst"])
                    P.act(lambda e: e.copy(out=xob[:, 0:4, :], in_=xst[:, 0:4, 0:256]), ["wst"], ["xob4"])
                    P.dve(lambda e: e.tensor_copy(out=xob[:, 4:8, :], in_=xst[:, 4:8, 0:256]), ["wst"], ["xob4"])
                    P.dma(tabs[:, 0, :], cdr["cosRK"][:, c0:c0 + 512], writes=["tab4"])
                    P.dma(tabs[:, 1, :], cdr["sinRK"][:, c0:c0 + 512], writes=["tab4"])
                    P.dma(tabq[:, 0, :], cdr["cosRQ"][:, o0:o0 + 256], writes=["tabq4"])
                    P.dma(tabq[:, 1, :], cdr["sinRQ"][:, o0:o0 + 256], writes=["tabq4"])
                    for h in range(4):
                        pi = pjr.next()
                        for k in range(8):
                            P.pe(lambda e, k=k, pi=pi, h=h: e.matmul(pj[pi][:], lhsT=W4[:, k, 512 + h * 128:512 + (h + 1) * 128], rhs=xb[:, k, :],
                                                                      start=(k == 0), stop=(k == 7)), ["W4", "xb4"], ["pj4_%d" % pi])
                        rope4(pi, 512, tabs, "tab4", kT[:, h, :], "kT4")
                    for h in range(4):
                        pi = pjr.next()
                        for k in range(8):
                            P.pe(lambda e, k=k, pi=pi, h=h: e.matmul(pj[pi][:, 0:256], lhsT=W4[:, k, h * 128:(h + 1) * 128], rhs=xob[:, k, :],
                                                                      start=(k == 0), stop=(k == 7)), ["W4", "xob4"], ["pj4_%d" % pi])
                        rope4(pi, 256, tabq, "tabq4", qT[:, h, :], "qT4")
                    for pp in range(2):
                        P.dve(lambda e, pp=pp: e.tensor_tensor(out=qxT[:, :, pp * 128:(pp + 1) * 128], in0=qT[:, :, pp * 128:(pp + 1) * 128], in1=xi[:], op=ALU.mult),
                              ["qT4", "xi"], ["qxT4"])
                    for tt in range(4):
                        for hf in range(2):
                            pi = pjr.next()
                            for k in range(8):
                                P.pe(lambda e, k=k, pi=pi, tt=tt, hf=hf: e.matmul(pj[pi][:], lhsT=xb[:, k, tt * 128:(tt + 1) * 128],
                                                                                   rhs=W4[:, k, 1024 + hf * 512:1024 + (hf + 1) * 512],
                                                                                   start=(k == 0), stop=(k == 7)), ["W4", "xb4"], ["pj4_%d" % pi])
                            P.act(lambda e, pi=pi, tt=tt, hf=hf: e.copy(out=vtok[:, tt, hf * 512:(hf + 1) * 512], in_=pj[pi][:]), ["pj4_%d" % pi], ["vtok"])
                    for tt in range(4):
                        for h in range(4):
                            P.pe(lambda e, tt=tt, h=h: e.transpose(out=pTr[:], in_=kT[:, h, tt * 128:(tt + 1) * 128], identity=identb[:]), ["kT4", "identb"], ["pTr4"])
                            P.dve(lambda e, tt=tt, h=h: e.tensor_scalar(out=kz[:, tt, h, :], in0=pTr[:], scalar1=zeta[:, tt % 2, h:h + 1], scalar2=None, op0=ALU.mult),
                                  ["pTr4", "zeta"], ["kz"])
                    for pp in range(2):
                        i = gch * 2 + pp
                        qs = slice(pp * 128, (pp + 1) * 128)
                        for h in range(4):
                            hs = slice(h * 256, (h + 1) * 256)
                            for mt in range(2):
                                tt = pp * 2 + mt
                                P.pe(lambda e, mt=mt, tt=tt, h=h: e.matmul(psc[:, mt, :], lhsT=kT[:, h, tt * 128:(tt + 1) * 128], rhs=qT[:, h, qs], start=True, stop=True),
                                     ["kT4", "qT4"], ["psc"])
                            P.dve(lambda e, h=h: e.tensor_tensor(out=sc[:], in0=psc[:], in1=Dc[:, :, h, :], op=ALU.mult), ["psc", "Dc"], ["sc"])
                            for mt in range(2):
                                tt = pp * 2 + mt
                                P.pe(lambda e, mt=mt, tt=tt, hs=hs: e.matmul(po[:], lhsT=sc[:, mt, :], rhs=vtok[:, tt, hs], start=(mt == 0), stop=False), ["sc", "vtok"], ["po"])
                            P.pe(lambda e, h=h: e.matmul(po[:], lhsT=qxT[:, h, qs], rhs=Rb[:, h, :], start=False, stop=True), ["qxT4", "Rb"], ["po"])
                            P.dve(lambda e: e.bn_stats(out=st6[:], in_=po[:]), ["po"], ["st6"])
                            P.dve(lambda e: e.bn_aggr(out=mv[:], in_=st6[:]), ["st6"], ["mv"])
                            P.dve(lambda e: e.tensor_scalar(out=rstd[:], in0=mv[:, 1:2], scalar1=LN_EPS, scalar2=None, op0=ALU.add), ["mv"], ["rstd"])
                            P.act(lambda e: e.activation(out=rstd[:], in_=rstd[:], func=AF.Sqrt), ["rstd"], ["rstd"])
                            P.dve(lambda e: e.reciprocal(out=rstd[:], in_=rstd[:]), ["rstd"], ["rstd"])
                            P.dve(lambda e, hs=hs: e.tensor_scalar(out=oretb[:, hs], in0=po[:], scalar1=mv[:, 0:1], scalar2=rstd[:, 0:1], op0=ALU.subtract, op1=ALU.mult),
                                  ["po", "mv", "rstd"], ["oretb"])
                            for mt in range(2):
                                tt = pp * 2 + mt
                                P.pe(lambda e, mt=mt, tt=tt, h=h, hs=hs: e.matmul(pR[:], lhsT=kz[:, tt, h, :], rhs=vtok[:, tt, hs], start=(mt == 0), stop=(mt == 1)),
                                     ["kz", "vtok"], ["pR"])
                            P.dve(lambda e, h=h: e.scalar_tensor_tensor(out=R[:, h, :], in0=R[:, h, :], scalar=decay256[h], in1=pR[:], op0=ALU.mult, op1=ALU.add),
                                  ["R", "pR"], ["R"])
                            P.act(lambda e, h=h: e.copy(out=Rb[:, h, :], in_=R[:, h, :]), ["R"], ["Rb"])
                        for et in range(8):
                            P.pe(lambda e, et=et: e.transpose(out=pTr[:], in_=oretb[:, et * 128:(et + 1) * 128], identity=identb[:]), ["oretb", "identb"], ["pTr4"])
                            P.act(lambda e, et=et, i=i: e.copy(out=oretT[:, et, i * 128:(i + 1) * 128], in_=pTr[:]), ["pTr4"], ["oretT"])
                dump("oretT", oretT[:].rearrange("p a t -> p (a t)"), [128, 8 * TO], "oretT")
            P.barrier()

        if n_stage >= 5:
            with ExitStack() as S5a:
                Wmg = sb(S5a, "Wmg", [128, 8, 2048], BF16)
                Wa = sb(S5a, "Wa", [128, 4, 1024], BF16)
                Wr = sb(S5a, "Wr", [128, 8, 1024], BF16)
                Wgt = sb(S5a, "Wgt", [128, 8, 1024], BF16)
                load_cast(Wmg[:].rearrange("p k n -> p (k n)"), wmgt, 8 * 2048, "Wmg")
                load_cast(Wa[:].rearrange("p k n -> p (k n)"), wat, 4 * 1024, "Wa")
                load_cast(Wr[:].rearrange("p k n -> p (k n)"), wrt, 8 * 1024, "Wr")
                load_cast(Wgt[:].rearrange("p k n -> p (k n)"), wgtt, 8 * 1024, "Wgt")
                gg8 = sb(S5a, "gg8", [128, 8], F32)
                gb8 = sb(S5a, "gb8", [128, 8], F32)
                P.dma(gg8[:], gng8, writes=["gg8"])
                P.dma(gb8[:], gnb8, writes=["gb8"])
                xb = sb(S5a, "xb5", [128, 8, 512], BF16)
                og = sb(S5a, "og", [128, 8, 512], BF16)
                sgt = [sb(S5a, "sgt%d" % i, [128, 512], F32) for i in range(2)]
                yn = [sb(S5a, "yn%d" % i, [128, 512], F32) for i in range(2)]
                ga = sb(S5a, "ga", [128, 512], F32)
                gr = sb(S5a, "gr", [128, 512], F32)
                ma = sb(S5a, "ma", [128, 512], F32)
                pg = [ps(S5a, "pg%d" % i, [128, 512]) for i in range(2)]
                pu = [ps(S5a, "pu%d" % i, [128, 512]) for i in range(2)]
                pgt = [ps(S5a, "pgt%d" % i, [128, 512]) for i in range(2)]
                xTov = xTo.rearrange("(k p) t -> p k t", p=128)
                for oc in range(4):
                    c0 = oc * 512
                    cs_ = slice(c0, c0 + 512)
                    P.dma(wst3[:], xTov[:, :, cs_], writes=["wst"])
                    P.act(lambda e: e.copy(out=xb[:, 0:4, :], in_=wst3[:, 0:4, :]), ["wst"], ["xb5"])
                    P.dve(lambda e: e.tensor_copy(out=xb[:, 4:8, :], in_=wst3[:, 4:8, :]), ["wst"], ["xb5"])
                    for et in range(8):
                        b_ = et % 2
                        for k in range(8):
                            P.pe(lambda e: e.matmul(pgt[b_][:], lhsT=Wgt[:, k, et * 128:(et + 1) * 128], rhs=xb[:, k, :], start=(k == 0), stop=(k == 7)),
                                 ["Wgt", "xb5"], ["pgt%d" % b_])
                        P.act(lambda e: e.activation(out=sgt[b_][:], in_=pgt[b_][:], func=AF.Silu), ["pgt%d" % b_], ["sgt%d" % b_])
                        P.act(lambda e: e.activation(out=yn[b_][:], in_=oretT[:, et, cs_], func=AF.Identity, scale=gg8[:, et:et + 1], bias=gb8[:, et:et + 1]),
                              ["oretT", "gg8", "gb8"], ["yn%d" % b_])
                        P.dve(lambda e: e.tensor_tensor(out=og[:, et, :], in0=yn[b_][:], in1=sgt[b_][:], op=ALU.mult), ["yn%d" % b_, "sgt%d" % b_], ["og"])
                    for ct in range(8):
                        for k in range(8):
                            P.pe(lambda e: e.matmul(pg[0][:], lhsT=Wmg[:, k, ct * 128:(ct + 1) * 128], rhs=xb[:, k, :], start=(k == 0), stop=(k == 7)),
                                 ["Wmg", "xb5"], ["pg0"])
                        for k in range(8):
                            P.pe(lambda e: e.matmul(pg[1][:], lhsT=Wmg[:, k, 1024 + ct * 128:1024 + (ct + 1) * 128], rhs=xb[:, k, :], start=(k == 0), stop=(k == 7)),
                                 ["Wmg", "xb5"], ["pg1"])
                        for k in range(4):
                            P.pe(lambda e: e.matmul(pu[0][:], lhsT=Wa[:, k, ct * 128:(ct + 1) * 128], rhs=oattnT[:, k, cs_], start=(k == 0), stop=(k == 3)),
                                 ["Wa", "oattnT"], ["pu0"])
                        for k in range(8):
                            P.pe(lambda e: e.matmul(pu[1][:], lhsT=Wr[:, k, ct * 128:(ct + 1) * 128], rhs=og[:, k, :], start=(k == 0), stop=(k == 7)),
                                 ["Wr", "og"], ["pu1"])
                        P.act(lambda e: e.activation(out=ga[:], in_=pg[0][:], func=AF.Sigmoid), ["pg0"], ["ga"])
                        P.act(lambda e: e.activation(out=gr[:], in_=pg[1][:], func=AF.Sigmoid), ["pg1"], ["gr"])
                        P.dve(lambda e: e.tensor_tensor(out=ma[:], in0=pu[0][:], in1=ga[:], op=ALU.mult), ["pu0", "ga"], ["ma"])
                        P.dve(lambda e: e.tensor_tensor(out=gr[:], in0=pu[1][:], in1=gr[:], op=ALU.mult), ["pu1", "gr"], ["gr"])
                        P.pool(lambda e: e.tensor_tensor(out=x1T[:, ct, cs_], in0=ma[:], in1=gr[:], op=ALU.add), ["ma", "gr"], ["mx%d" % (oc * 4 + t_) for t_ in range(4)])
                dump("mergedT", x1T[:].rearrange("p a t -> p (a t)"), [128, 8 * TO], "mx0")
            P.barrier()
        B_.close()
        A_.close()
        if n_stage >= 5:
            with ExitStack() as S56:
                acc = sb(S56, "acc", [128, 16, 1024], F32)
                lng = sb(S56, "lng", [128, 1024], F32)
                lnb = sb(S56, "lnb", [128, 1024], F32)
                st12 = sb(S56, "st12", [128, 2, 6], F32)
                mv = sb(S56, "mv5", [128, 2], F32)
                rstd = sb(S56, "rstd5", [128, 1], F32)

                def layer_norm(src_ap, src_key, dst_ap, dst_key, tmp_ap, tmp_key):
                    for hf in range(2):
                        P.dve(lambda e: e.bn_stats(out=st12[:, hf, :], in_=src_ap[:, hf * 512:(hf + 1) * 512]), [src_key], ["st12"])
                    P.dve(lambda e: e.bn_aggr(out=mv[:], in_=st12[:].rearrange("p a s -> p (a s)")), ["st12"], ["mv5"])
                    P.dve(lambda e: e.tensor_scalar(out=rstd[:], in0=mv[:, 1:2], scalar1=LN_EPS, scalar2=None, op0=ALU.add), ["mv5"], ["rstd5"])
                    P.act(lambda e: e.activation(out=rstd[:], in_=rstd[:], func=AF.Sqrt), ["rstd5"], ["rstd5"])
                    P.dve(lambda e: e.reciprocal(out=rstd[:], in_=rstd[:]), ["rstd5"], ["rstd5"])
                    P.dve(lambda e: e.tensor_scalar(out=tmp_ap, in0=src_ap, scalar1=mv[:, 0:1], scalar2=rstd[:, 0:1], op0=ALU.subtract, op1=ALU.mult),
                          [src_key, "mv5", "rstd5"], [tmp_key])
                    P.pool(lambda e: e.tensor_tensor(out=tmp_ap, in0=tmp_ap, in1=lng[:], op=ALU.mult), [tmp_key, "lng"], [tmp_key])
                    P.pool(lambda e: e.tensor_tensor(out=dst_ap, in0=tmp_ap, in1=lnb[:], op=ALU.add), [tmp_key, "lnb"], [dst_key])

                with ExitStack() as S5b:
                    Wo = sb(S5b, "Wo", [128, 8, 1024], BF16)
                    load_cast(Wo[:].rearrange("p k n -> p (k n)"), wot, 8 * 1024, "Wo")
                    P.dma(lng[:], ln1g.partition_broadcast(128), writes=["lng"])
                    P.dma(lnb[:], ln1b.partition_broadcast(128), writes=["lnb"])
                    xres = sb(S5b, "xres", [128, 1024], F32)
                    yt = sb(S5b, "yt", [128, 1024], F32)
                    x1 = sb(S5b, "x1", [128, 1024], F32)
                    x1b = sb(S5b, "x1b", [128, 1024], BF16)
                    pm = [ps(S5b, "pm%d" % i, [128, 512]) for i in range(2)]
                    pTr = ps(S5b, "pTr5", [128, 128], BF16)
                    for i in range(16):
                        ts_ = slice(i * 128, (i + 1) * 128)
                        P.dma(xres[:], xo[ts_, :], writes=["xres"])
                        for hf in range(2):
                            for k in range(8):
                                P.pe(lambda e: e.matmul(pm[hf][:], lhsT=x1T[:, k, ts_], rhs=Wo[:, k, hf * 512:(hf + 1) * 512], start=(k == 0), stop=(k == 7)),
                                     ["mx%d" % i, "Wo"], ["pm%d" % hf])
                            P.dve(lambda e: e.scalar_tensor_tensor(out=yt[:, hf * 512:(hf + 1) * 512], in0=xres[:, hf * 512:(hf + 1) * 512], scalar=ALPHA,
                                                                   in1=pm[hf][:], op0=ALU.mult, op1=ALU.add), ["xres", "pm%d" % hf], ["yt"])
                        layer_norm(yt[:], "yt", x1[:], "x1", yt[:], "yt")
                        if ("x1_%d" % i) in debug:
                            dump("x1_%d" % i, x1[:], [128, 1024], "x1")
                        P.dve(lambda e: e.tensor_scalar(out=acc[:, i, :], in0=x1[:], scalar1=ALPHA, scalar2=None, op0=ALU.mult), ["x1"], ["acc%d" % i])
                        P.act(lambda e: e.copy(out=x1b[:], in_=x1[:]), ["x1"], ["x1b"])
                        for dt_ in range(8):
                            P.pe(lambda e: e.transpose(out=pTr[:], in_=x1b[:, dt_ * 128:(dt_ + 1) * 128], identity=identb[:]), ["x1b", "identb"], ["pTr5"])
                            P.act(lambda e: e.copy(out=x1T[:, dt_, ts_], in_=pTr[:]), ["pTr5"], ["mx%d" % i])
                P.barrier()
                if n_stage >= 6:
                    with ExitStack() as S6:
                        P.dma(lng[:], ln2g.partition_broadcast(128), writes=["lng"])
                        P.dma(lnb[:], ln2b.partition_broadcast(128), writes=["lnb"])
                        comb = sb(S6, "comb", [128, 16, 32], F32)
                        wrf = sb(S6, "wrf", [128, 8, 36], F32)
                        wrb = sb(S6, "wrb", [128, 8, 36], BF16)
                        brb = sb(S6, "brb", [128, 36], F32)
                        P.dma(wrf[:].rearrange("p k n -> p (k n)"), wrout, writes=["wrf"])
                        P.dve(lambda e: e.tensor_copy(out=wrb[:], in_=wrf[:]), ["wrf"], ["wrb"])
                        P.dma(brb[:], brout.partition_broadcast(128), writes=["brb"])
                        lg = sb(S6, "lg", [128, 36], F32)
                        gmx = sb(S6, "gmx", [128, 1], F32)
                        ngmx = sb(S6, "ngmx", [128, 1], F32)
                        gex = sb(S6, "gex", [128, 4], F32)
                        gsum = sb(S6, "gsum", [128, 1], F32)
                        gprob = sb(S6, "gprob", [128, 1], F32)
                        ohg = sb(S6, "ohg", [128, 4], F32)
                        tmp48 = sb(S6, "tmp48", [128, 4, 8], F32)
                        isel = sb(S6, "isel", [128, 8], F32)
                        m8 = sb(S6, "m8r", [128, 8], F32)
                        dlt = sb(S6, "dlt", [128, 1], F32)
                        w2e = sb(S6, "w2e", [128, 1], F32)
                        wsum = sb(S6, "wsum", [128, 1], F32)
                        wt1 = sb(S6, "wt1", [128, 1], F32)
                        wt2 = sb(S6, "wt2", [128, 1], F32)
                        ce = sb(S6, "ce", [128, 8], F32)
                        ce2 = sb(S6, "ce2", [128, 8], F32)
                        plg = ps(S6, "plg", [128, 36])
                        AXX = mybir.AxisListType.X
                        for i in range(16):
                            ts_ = slice(i * 128, (i + 1) * 128)
                            for k in range(8):
                                P.pe(lambda e: e.matmul(plg[:], lhsT=x1T[:, k, ts_], rhs=wrb[:, k, :], start=(k == 0), stop=(k == 7)), ["mx%d" % i, "wrb"], ["plg"])
                            P.dve(lambda e: e.tensor_tensor(out=lg[:], in0=plg[:], in1=brb[:], op=ALU.add), ["plg", "brb"], ["lg"])
                            P.dve(lambda e: e.tensor_reduce(out=gmx[:], in_=lg[:, 0:4], axis=AXX, op=ALU.max), ["lg"], ["gmx"])
                            P.dve(lambda e: e.tensor_scalar(out=ngmx[:], in0=gmx[:], scalar1=-1.0, scalar2=None, op0=ALU.mult), ["gmx"], ["ngmx"])
                            P.act(lambda e: e.activation(out=gex[:], in_=lg[:, 0:4], func=AF.Exp, bias=ngmx[:, 0:1], scale=1.0), ["lg", "ngmx"], ["gex"])
                            P.dve(lambda e: e.tensor_reduce(out=gsum[:], in_=gex[:], axis=AXX, op=ALU.add), ["gex"], ["gsum"])
                            P.dve(lambda e: e.reciprocal(out=gprob[:], in_=gsum[:]), ["gsum"], ["gprob"])
                            P.dve(lambda e: e.tensor_scalar(out=ohg[:], in0=lg[:, 0:4], scalar1=gmx[:, 0:1], scalar2=None, op0=ALU.is_ge), ["lg", "gmx"], ["ohg"])
                            P.dve(lambda e: e.tensor_tensor(out=tmp48[:], in0=lg[:, 4:36].rearrange("p (g e) -> p g e", g=4),
                                                            in1=ohg[:].unsqueeze(2).broadcast_to([128, 4, 8]), op=ALU.mult), ["lg", "ohg"], ["tmp48"])
                            P.dve(lambda e: e.tensor_reduce(out=isel[:], in_=tmp48[:].rearrange("p g e -> p e g"), axis=AXX, op=ALU.add), ["tmp48"], ["isel"])
                            P.dve(lambda e: e.max(out=m8[:], in_=isel[:]), ["isel"], ["m8r"])
                            P.dve(lambda e: e.tensor_tensor(out=dlt[:], in0=m8[:, 1:2], in1=m8[:, 0:1], op=ALU.subtract), ["m8r"], ["dlt"])
                            P.act(lambda e: e.activation(out=w2e[:], in_=dlt[:], func=AF.Exp), ["dlt"], ["w2e"])
                            P.dve(lambda e: e.tensor_scalar(out=wsum[:], in0=w2e[:], scalar1=1.0, scalar2=None, op0=ALU.add), ["w2e"], ["wsum"])
                            P.dve(lambda e: e.reciprocal(out=wsum[:], in_=wsum[:]), ["wsum"], ["wsum"])
                            P.dve(lambda e: e.tensor_tensor(out=wt1[:], in0=wsum[:], in1=gprob[:], op=ALU.mult), ["wsum", "gprob"], ["wt1"])
                            P.dve(lambda e: e.tensor_tensor(out=wt2[:], in0=wt1[:], in1=w2e[:], op=ALU.mult), ["wt1", "w2e"], ["wt2"])
                            P.dve(lambda e: e.tensor_scalar(out=ce[:], in0=isel[:], scalar1=m8[:, 0:1], scalar2=wt1[:, 0:1], op0=ALU.is_equal, op1=ALU.mult), ["isel", "m8r", "wt1"], ["ce"])
                            P.dve(lambda e: e.tensor_scalar(out=ce2[:], in0=isel[:], scalar1=m8[:, 1:2], scalar2=wt2[:, 0:1], op0=ALU.is_equal, op1=ALU.mult), ["isel", "m8r", "wt2"], ["ce2"])
                            P.dve(lambda e: e.tensor_tensor(out=ce[:], in0=ce[:], in1=ce2[:], op=ALU.add), ["ce", "ce2"], ["ce"])
                            P.dve(lambda e: e.tensor_tensor(out=comb[:, i, :].rearrange("p (g e) -> p g e", g=4), in0=ce[:].unsqueeze(1).broadcast_to([128, 4, 8]),
                                                            in1=ohg[:].unsqueeze(2).broadcast_to([128, 4, 8]), op=ALU.mult), ["ce", "ohg"], ["comb"])
                        dump("comb", comb[:].rearrange("p a e -> p (a e)"), [128, 512], "comb")
                        wstE = [wst[:, 0:2048], wst[:, 2048:4096]]
                        wE = [sb(S6, "wE%d" % i, [128, 12288], BF16) for i in range(2)]
                        sgE = [sb(S6, "sgE%d" % i, [128, 512], F32) for i in range(2)]
                        hT = [sb(S6, "hT%d" % i, [128, 4, 512], BF16) for i in range(2)]
                        pG = [ps(S6, "pG%d" % i, [128, 512]) for i in range(2)]
                        pU = [ps(S6, "pU%d" % i, [128, 512]) for i in range(2)]
                        pO = [ps(S6, "pO%d" % i, [128, 512]) for i in range(3)]
                        wsr = Ring([0, 1])
                        gr_ = Ring([0, 1])
                        or_ = Ring([0, 1, 2])
                        crr = [0]
                        n_exp = DEBUG.get("n_exp", 32)

                        def load_expert(ex):
                            ws = ex % 2
                            for pc in range(6):
                                si = wsr.next()
                                P.dma(wstE[si], wexp[ex, :, pc * 2048:(pc + 1) * 2048], writes=["wstE%d" % si])
                                d = wE[ws][:, pc * 2048:(pc + 1) * 2048]
                                P.pool(lambda e: e.tensor_copy(out=d, in_=wstE[si]), ["wstE%d" % si], ["wE%d" % ws])

                        def gu_phase(ex, cth):
                            ws = ex % 2
                            Wg = wE[ws][:, 0:4096].rearrange("p (k n) -> p k n", k=8)
                            Wu = wE[ws][:, 4096:8192].rearrange("p (k n) -> p k n", k=8)
                            wkey = "wE%d" % ws
                            cs_ = slice(cth * 512, (cth + 1) * 512)
                            xkeys = ["mx%d" % (cth * 4 + t_) for t_ in range(4)]
                            hs_ = cth % 2
                            for ft in range(4):
                                gi = gr_.next()
                                for k in range(8):
                                    P.pe(lambda e: e.matmul(pG[gi][:], lhsT=Wg[:, k, ft * 128:(ft + 1) * 128], rhs=x1T[:, k, cs_], start=(k == 0), stop=(k == 7)),
                                         [wkey] + xkeys, ["pG%d" % gi])
                                for k in range(8):
                                    P.pe(lambda e: e.matmul(pU[gi][:], lhsT=Wu[:, k, ft * 128:(ft + 1) * 128], rhs=x1T[:, k, cs_], start=(k == 0), stop=(k == 7)),
                                         [wkey] + xkeys, ["pU%d" % gi])
                                P.act(lambda e: e.activation(out=sgE[gi][:], in_=pG[gi][:], func=AF.Silu), ["pG%d" % gi], ["sgE%d" % gi])
                                P.dve(lambda e: e.tensor_tensor(out=hT[hs_][:, ft, :], in0=pU[gi][:], in1=sgE[gi][:], op=ALU.mult),
                                      ["pU%d" % gi, "sgE%d" % gi], ["hT%d" % hs_])

                        def d_phase(ex, cth):
                            ws = ex % 2
                            Wd = wE[ws][:, 8192:12288].rearrange("p (k n) -> p k n", k=4)
                            wkey = "wE%d" % ws
                            hs_ = cth % 2
                            for tt in range(4):
                                ti = cth * 4 + tt
                                for hf in range(2):
                                    oi = or_.next()
                                    for ft in range(4):
                                        P.pe(lambda e: e.matmul(pO[oi][:], lhsT=hT[hs_][:, ft, tt * 128:(tt + 1) * 128],
                                                                rhs=Wd[:, ft, hf * 512:(hf + 1) * 512], start=(ft == 0), stop=(ft == 3)),
                                             [wkey, "hT%d" % hs_], ["pO%d" % oi])
                                    a_ = acc[:, ti, hf * 512:(hf + 1) * 512]
                                    P.dve(lambda e: e.scalar_tensor_tensor(out=a_, in0=pO[oi][:], scalar=comb[:, ti, ex:ex + 1], in1=a_, op0=ALU.mult, op1=ALU.add),
                                          ["pO%d" % oi, "comb", "acc%d" % ti], ["acc%d" % ti])

                        units = [(ex, cth) for ex in range(n_exp) for cth in range(4)]
                        load_expert(0)
                        if n_exp > 1:
                            load_expert(1)
                        gu_phase(*units[0])
                        for u, (ex, cth) in enumerate(units):
                            if u + 1 < len(units):
                                gu_phase(*units[u + 1])
                            d_phase(ex, cth)
                            if cth == 3 and ex + 2 < n_exp:
                                load_expert(ex + 2)
                        yo = [sb(S6, "yo%d" % i, [128, 1024], F32) for i in range(2)]
                        for i in range(16):
                            yi = i % 2
                            layer_norm(acc[:, i, :], "acc%d" % i, yo[yi][:], "yo%d" % yi, yo[yi][:], "yo%d" % yi)
                            P.dma(out[i * 128:(i + 1) * 128, :], yo[yi][:], reads=["yo%d" % yi], writes=["out%d" % yi], sem="outd%d" % yi)
        fin_reads = ["out0", "out1"] + ["dbg_" + n for n in dbg_out]
        P.add("sp", lambda e: e.nop(), reads=fin_reads, sem="fin")
        if n_stage < 6:
            pass
        cnt = P.emit(G)
        nsem = len(cnt)
    return nc, dbg_out, nsem


def prep_shared(inp):
    f = lambda a: np.ascontiguousarray(np.asarray(a, dtype=np.float32))
    w_in = f(inp["w_in"])[0]
    sh = {}
    q = w_in[:, 0:512].reshape(1024, 2, 4, 64).transpose(0, 2, 1, 3).reshape(1024, 512)
    w1 = np.concatenate([q, w_in[:, 512:640], w_in[:, 640:768], w_in[:, 768:896], w_in[:, 1024:1152],
                         w_in[:, 896:1024], w_in[:, 1152:1280], w_in[:, 1280:1304]], axis=1)
    sh["w1t"] = tile_w(w1)
    sh["w4t"] = tile_w(w_in[:, 1304:3352])
    sh["wgtt"] = tile_w(w_in[:, 3352:4376])
    sh["wmgt"] = tile_w(w_in[:, 4376:6424])
    lit = {"k": (inp["cmp_k_w1"], inp["cmp_k_b1"], inp["cmp_pos_k"]), "v": (inp["cmp_v_w1"], inp["cmp_v_b1"], inp["cmp_pos_v"])}
    for kv in "kv":
        cw1 = f(lit[kv][0])[0]
        r = cw1.reshape(32, 64, 256).transpose(1, 0, 2).reshape(64, 32 * 256)
        sh["cw1" + kv] = np.ascontiguousarray(np.concatenate([r, r], axis=0))
        pos = f(lit[kv][2])[0]
        sh["cpos" + kv] = np.ascontiguousarray(np.concatenate([pos.T, pos.T], axis=0))
        sh["cb1" + kv] = np.ascontiguousarray(f(lit[kv][1])[0].reshape(2, 128).T)
    w2k = f(inp["cmp_k_w2"])[0]
    sh["cw2k"] = tile_w(np.concatenate([w2k, w2k], axis=1))
    sh["cw2v"] = tile_w(f(inp["cmp_v_w2"])[0])
    b2k = f(inp["cmp_k_b2"])[0]
    sh["cb2k"] = np.ascontiguousarray(np.concatenate([b2k, b2k])[:, None])
    sh["cb2v"] = f(inp["cmp_v_b2"])[0]
    sh["gng8"] = np.ascontiguousarray(f(inp["ret_gn_g"])[0].reshape(8, 128).T)
    sh["gnb8"] = np.ascontiguousarray(f(inp["ret_gn_b"])[0].reshape(8, 128).T)
    sh["wat"] = tile_w(f(inp["w_up_attn"])[0])
    sh["wrt"] = tile_w(f(inp["w_up_ret"])[0])
    sh["wot"] = tile_w(f(inp["w_out"])[0])
    for n in ("ln1_g", "ln1_b", "ln2_g", "ln2_b"):
        sh[n.replace("_", "")] = f(inp[n])[0]
    rg = f(inp["router_group_w"])[0]
    ri = f(inp["router_inner_w"])[0]
    sh["wrout"] = tile_w(np.concatenate([rg, ri.transpose(1, 0, 2).reshape(1024, 32)], axis=1))
    sh["brout"] = np.ascontiguousarray(np.concatenate([f(inp["router_group_b"])[0], f(inp["router_inner_b"])[0].reshape(32)]))
    wg = f(inp["expert_w_gate"])[0]
    wu = f(inp["expert_w_up"])[0]
    wd = f(inp["expert_w_down"])[0]
    we = np.empty((32, 128, 12288), np.float32)
    we[:, :, 0:4096] = wg.reshape(32, 8, 128, 512).transpose(0, 2, 1, 3).reshape(32, 128, 4096)
    we[:, :, 4096:8192] = wu.reshape(32, 8, 128, 512).transpose(0, 2, 1, 3).reshape(32, 128, 4096)
    we[:, :, 8192:12288] = wd.reshape(32, 4, 128, 1024).transpose(0, 2, 1, 3).reshape(32, 128, 4096)
    sh["wexp"] = we
    return sh


def make_in_maps(inp):
    sh = prep_shared(inp)
    x = np.asarray(inp["x"], dtype=np.float32)
    maps = []
    for core in range(8):
        b, c = core // 2, core % 2
        m = dict(sh)
        xb = x[b]
        own = xb.reshape(16, 2, 128, 1024)[:, c].reshape(TO, 1024)
        m["xT"] = np.ascontiguousarray(xb.T)
        m["xTo"] = np.ascontiguousarray(own.T)
        m["xo"] = np.ascontiguousarray(own)
        for k, v in make_consts(c).items():
            if not k.startswith("_"):
                m["c_" + k] = v
        maps.append(m)
    return maps


_PROG_CACHE = {}


def kernel(**inputs):
    if "prog" not in _PROG_CACHE:
        _PROG_CACHE["prog"] = build_program()
    nc, _, _ = _PROG_CACHE["prog"]
    maps = make_in_maps(inputs)
    res = run_bass_kernel_spmd(nc, maps, core_ids=list(range(8)))
    outp = np.empty((4, 16, 2, 128, 1024), np.float32)
    for core in range(8):
        b, c = core // 2, core % 2
        outp[b, :, c] = res.results[core]["out"].reshape(16, 128, 1024)
    return outp.reshape(4, T, 1024)
```

```python
import numpy as np
import ml_dtypes
import concourse.bass as bass
import concourse.mybir as mybir
from concourse.bass_utils import run_bass_kernel_spmd
from contextlib import ExitStack

F32 = mybir.dt.float32
BF16 = mybir.dt.bfloat16
AF = mybir.ActivationFunctionType
ALU = mybir.AluOpType
NPBF = ml_dtypes.bfloat16

T = 4096
D = 1024
TO = 2048
NEGM = -30000.0
LN_EPS = 1e-5
ALPHA = 2.0 ** 0.25
DEBUG = {}


class Op:
    __slots__ = ("eng", "fn", "reads", "writes", "dma", "sem", "deps", "needs_inc", "idx", "id", "extra")

    def __init__(self, eng, fn, reads, writes, dma, sem):
        self.eng = eng
        self.fn = fn
        self.reads = tuple(reads)
        self.writes = tuple(writes)
        self.dma = dma
        self.sem = sem
        self.deps = []
        self.needs_inc = dma
        self.idx = 0
        self.extra = ()


class _Rec:
    def __getattr__(self, name):
        return lambda *a, **k: (name, a, k)


_REC = _Rec()


class Prog:
    ENGS = ("pe", "act", "dve", "pool", "sp")

    def __init__(self, nc, same_eng_sync=True):
        self.nc = nc
        self.ops = []
        self.same_eng_sync = same_eng_sync
        self.last_by_sem = {}
        self.psum_keys = set()

    def add(self, eng, fn, reads=(), writes=(), dma=False, sem=None):
        lim = DEBUG.get("max_ops")
        self.nadd = getattr(self, "nadd", -1) + 1
        if (lim is not None and self.nadd >= lim and sem not in ("dbg", "fin")) or self.nadd in DEBUG.get("skip", ()):
            return Op(eng, None, reads, writes, dma, sem)
        if dma and sem is None:
            sem = "dma_" + str(writes[0])
        if not dma:
            sem = "eng_" + eng
        op = Op(eng, fn(_REC), reads, writes, dma, sem)
        if DEBUG.get("trace_ops"):
            print(len(self.ops), eng, op.fn[0], reads, writes)
        op.id = len(self.ops)
        self.ops.append(op)
        self.last_by_sem[sem] = op
        return op

    def pe(self, fn, reads=(), writes=()):
        return self.add("pe", fn, reads, writes)

    def act(self, fn, reads=(), writes=()):
        return self.add("act", fn, reads, writes)

    def dve(self, fn, reads=(), writes=()):
        return self.add("dve", fn, reads, writes)

    def pool(self, fn, reads=(), writes=()):
        return self.add("pool", fn, reads, writes)

    def dma(self, out, in_, reads=(), writes=(), sem=None, q="sp", **kw):
        return self.add(q, lambda e: e.dma_start(out=out, in_=in_, **kw), reads, writes, dma=True, sem=sem)

    def barrier(self):
        lasts = list(self.last_by_sem.values())
        for eng in self.ENGS:
            op = self.add(eng, lambda e: e.nop())
            op.extra = tuple(lasts)
        self.last_by_sem = {k: v for k, v in self.last_by_sem.items() if k.startswith("eng_")}

    def analyze(self):
        state = {}
        for op in self.ops:
            deps = set(op.extra)
            for k in op.reads:
                st = state.get(k)
                if st:
                    deps.update(st[0])
                    if k in self.psum_keys:
                        deps.update(r for r in st[1] if r.eng != op.eng)
            for k in op.writes:
                st = state.get(k)
                if st is None:
                    st = state[k] = [[], []]
                if st[1]:
                    deps.update(st[1])
                    deps.update(st[0])
                    st[0] = [op]
                    st[1] = []
                else:
                    same_group = op.dma and all(w.dma and w.sem == op.sem for w in st[0])
                    if same_group:
                        st[0].append(op)
                    else:
                        deps.update(st[0])
                        st[0] = [op]
            for k in op.reads:
                st = state.get(k)
                if st is None:
                    st = state[k] = [[], []]
                st[1].append(op)
            deps.discard(op)
            red = {}
            for d in deps:
                if (not d.dma) and (not op.dma) and d.eng == op.eng:
                    if op.eng == "pe" or not self.same_eng_sync:
                        continue
                cur = red.get(d.sem)
                if cur is None or d.id > cur.id:
                    red[d.sem] = d
            op.deps = list(red.values())
            for d in op.deps:
                d.needs_inc = True
        cnt = {}
        for op in self.ops:
            if op.needs_inc:
                cnt[op.sem] = cnt.get(op.sem, 0) + 1
                op.idx = cnt[op.sem]
        self.sem_names = sorted(cnt.keys())
        return cnt

    def emit(self, stack):
        nc = self.nc
        cnt = self.analyze()
        sems = {}
        for name in self.sem_names:
            sems[name] = stack.enter_context(nc.semaphore(name))
        block = stack.enter_context(nc.Block())
        per_eng = {e: [o for o in self.ops if o.eng == e] for e in self.ENGS}

        def run(eng_obj, ops):
            known = {}
            for op in ops:
                for d in op.deps:
                    val = d.idx * (16 if d.dma else 1)
                    if known.get(d.sem, 0) < val:
                        eng_obj.wait_ge(sems[d.sem], val)
                        known[d.sem] = val
                name, a, k = op.fn
                inst = getattr(eng_obj, name)(*a, **k)
                if op.needs_inc:
                    inst.then_inc(sems[op.sem], 16 if op.dma else 1)

        @block.sync
        def _(e):
            run(e, per_eng["sp"])

        @block.tensor
        def _(e):
            run(e, per_eng["pe"])

        @block.scalar
        def _(e):
            run(e, per_eng["act"])

        @block.vector
        def _(e):
            run(e, per_eng["dve"])

        @block.gpsimd
        def _(e):
            run(e, per_eng["pool"])
        return cnt


class Ring:
    def __init__(self, items):
        self.items = items
        self.i = 0

    def next(self):
        it = self.items[self.i % len(self.items)]
        self.i += 1
        return it


def tile_w(w):
    K, N = w.shape
    return np.ascontiguousarray(w.reshape(K // 128, 128, N).transpose(1, 0, 2).reshape(128, -1))


def rope_tabs(pos, d, scale):
    half = d // 2
    inv = 10000.0 ** (-np.arange(half, dtype=np.float64) * 2.0 / d)
    ang = pos.astype(np.float64)[None, :] * inv[:, None]
    cos = np.cos(ang) * scale
    sin = np.sin(ang) * scale
    reps = 128 // half
    return (np.tile(cos, (reps, 1)).astype(np.float32), np.tile(sin, (reps, 1)).astype(np.float32))


def rot_lhsT(d):
    half = d // 2
    Pm = np.zeros((128, 128), np.float32)
    for blk in range(128 // d):
        o = blk * d
        for m in range(half):
            Pm[o + m, o + m + half] = -1.0
            Pm[o + m + half, o + m] = 1.0
    return np.ascontiguousarray(Pm.T)


_CONST_CACHE = {}


def make_consts(c):
    if c in _CONST_CACHE:
        return _CONST_CACHE[c]
    cs = {}
    own_pos = np.concatenate([np.arange(128) + (2 * i + c) * 128 for i in range(16)])
    allpos = np.arange(T)
    cs["cosK"], cs["sinK"] = rope_tabs(allpos, 64, 1.0)
    cs["cosQ"], cs["sinQ"] = rope_tabs(own_pos, 64, 0.125)
    cs["cosRK"], cs["sinRK"] = rope_tabs(allpos, 128, 128.0 ** -0.5)
    cs["cosRQ"], cs["sinRQ"] = rope_tabs(own_pos, 128, 1.0)
    cend = np.arange(256) * 16 + 31
    cs["cosC"], cs["sinC"] = rope_tabs(cend, 64, 1.0)
    cs["pt64"] = rot_lhsT(64).astype(NPBF)
    cs["pt128"] = rot_lhsT(128).astype(NPBF)
    cs["identb"] = np.eye(128, dtype=np.float32).astype(NPBF)
    E = np.zeros((128, 32, 128), np.float32)
    for j in range(32):
        for k in range(128):
            E[2 * j + k // 64, j, k] = 1.0
            E[64 + 2 * j + k // 64, j, k] = 1.0
    cs["eall"] = E.reshape(128, -1).astype(NPBF)
    wm = np.zeros((128, 6, 128), np.float32)
    kk = np.arange(128)[:, None]
    tt = np.arange(128)[None, :]
    for r in range(6):
        dj = (r - 4) - c
        tk = dj * 128 + kk
        ok = (tk <= tt) & (tt - tk < 512)
        wm[:, r, :] = np.where(ok, 0.0, NEGM)
    cs["wmask"] = wm.reshape(128, -1).astype(NPBF)
    cm = np.zeros((128, 2, 16, 128), np.float32)
    for a in range(2):
        for i in range(16):
            G = 2 * i + c
            n = a * 128 + kk
            t = G * 128 + tt
            cm[:, a, i, :] = np.where(16 * n + 31 <= t, 0.0, NEGM)
    cs["cmpmask"] = cm.reshape(128, -1).astype(NPBF)
    cstart = np.arange(255) * 16
    sstart = np.arange(64) * 64
    ov = np.clip(np.minimum(cstart[None, :] + 32, sstart[:, None] + 64) - np.maximum(cstart[None, :], sstart[:, None]), 0, None) / 16.0
    ovT = np.zeros((256, 64), np.float32)
    ovT[:255] = ov.T
    cs["ovT"] = np.ascontiguousarray(ovT.reshape(2, 128, 64).transpose(1, 0, 2).reshape(128, -1)).astype(NPBF)
    tkm = np.zeros((128, 16, 64), np.float32)
    tkb = np.zeros((128, 16, 64), np.float32)
    for i in range(16):
        G = 2 * i + c
        for p in range(128):
            bt = (G * 128 + p) // 64
            for s in range(64):
                if s == 0:
                    tkb[p, i, s] = 1e9
                elif s == bt:
                    tkb[p, i, s] = 2e9
                elif s == bt - 1:
                    tkb[p, i, s] = 3e9
                elif s <= bt:
                    tkm[p, i, s] = 1.0
                else:
                    tkb[p, i, s] = -1e9 - 1e6 * s
    cs["tkm"] = tkm.reshape(128, -1)
    cs["tkb"] = tkb.reshape(128, -1)
    gam = 1.0 - 2.0 ** (-5.0 - np.arange(4, dtype=np.float64))
    lg = np.log(gam)
    m = np.arange(256)[:, None]
    cq = np.arange(128)[None, :]
    qq = 128 * c + cq
    Dc = np.zeros((128, 2, 4, 128), np.float32)
    for h in range(4):
        dd = np.where(qq >= m, np.exp(np.maximum(qq - m, 0) * lg[h]), 0.0)
        Dc[:, :, h, :] = dd.reshape(2, 128, 128).transpose(1, 0, 2)
    cs["Dc"] = Dc.reshape(128, -1)
    xi = np.zeros((128, 4, 128), np.float32)
    for h in range(4):
        xi[:, h, :] = np.exp((qq + 1.0) * lg[h])
    cs["xi"] = xi.reshape(128, -1)
    zt = np.zeros((128, 2, 4), np.float32)
    for h in range(4):
        zt[:, :, h] = np.exp((255.0 - np.arange(256)) * lg[h]).reshape(2, 128).T
    cs["zeta"] = zt.reshape(128, -1)
    cs["_decay256"] = [float(np.exp(256.0 * lg[h])) for h in range(4)]
    _CONST_CACHE[c] = cs
    return cs


CONST_SHAPES = None


def build_program(n_stage=6, debug=()):
    nc = bass.Bass("TRN2", target_bir_lowering=False)
    cs0 = make_consts(0)
    dram = {}

    def din(name, shape, dt=F32):
        dram[name] = nc.dram_tensor(name, list(shape), dt, kind="ExternalInput").ap()
        return dram[name]

    xT = din("xT", [1024, T])
    xTo = din("xTo", [1024, TO])
    xo = din("xo", [TO, 1024])
    w1t = din("w1t", [128, 8 * 1304])
    w4t = din("w4t", [128, 8 * 2048])
    wmgt = din("wmgt", [128, 8 * 2048])
    cw1 = {kv: din("cw1" + kv, [128, 32 * 256]) for kv in "kv"}
    cpos = {kv: din("cpos" + kv, [128, 32]) for kv in "kv"}
    cb1 = {kv: din("cb1" + kv, [128, 2]) for kv in "kv"}
    cw2k = din("cw2k", [128, 2 * 128])
    cw2v = din("cw2v", [128, 2 * 64])
    cb2k = din("cb2k", [128, 1])
    cb2v = din("cb2v", [64])
    gng8 = din("gng8", [128, 8])
    gnb8 = din("gnb8", [128, 8])
    wgtt = din("wgtt", [128, 8 * 1024])
    wat = din("wat", [128, 4 * 1024])
    wrt = din("wrt", [128, 8 * 1024])
    wot = din("wot", [128, 8 * 1024])
    ln1g = din("ln1g", [1024])
    ln1b = din("ln1b", [1024])
    ln2g = din("ln2g", [1024])
    ln2b = din("ln2b", [1024])
    wrout = din("wrout", [128, 8 * 36])
    brout = din("brout", [36])
    wexp = din("wexp", [32, 128, 12288])
    cdr = {}
    for k, v in cs0.items():
        if k.startswith("_"):
            continue
        cdr[k] = din("c_" + k, v.shape, BF16 if v.dtype == NPBF else F32)
    out = nc.dram_tensor("out", [TO, 1024], F32, kind="ExternalOutput").ap()
    dbg_out = {}

    decay256 = cs0["_decay256"]

    with ExitStack() as G:
        P = Prog(nc)

        def sb(stack, name, shape, dt):
            return stack.enter_context(nc.sbuf_tensor(name, list(shape), dt))

        def ps(stack, name, shape, dt=F32):
            P.psum_keys.add(name)
            ncol = 512 if dt == F32 else 1024
            full = stack.enter_context(nc.psum_tensor(name, [128, ncol], dt))
            n = 1
            for d_ in shape[1:]:
                n *= d_
            v = full[0:shape[0], 0:n]
            if len(shape) == 3:
                v = v.rearrange("p (a b) -> p a b", a=shape[1])
            return v

        def dump(name, ap, shape, key):
            if name in debug:
                t = nc.dram_tensor("dbg_" + name, list(shape), ap.dtype, kind="ExternalOutput").ap()
                dbg_out[name] = t
                P.dma(t, ap, reads=[key], writes=["dbg_" + name], sem="dbg")

        identb = sb(G, "identb", [128, 128], BF16)
        P.dma(identb[:], cdr["identb"], writes=["identb"])
        wst = sb(G, "wst", [128, 4096], F32)
        cast_rr = [0]

        def load_cast(dst_ap, src_ap, n, dst_key, shape3=None):
            o = 0
            while o < n:
                m = min(4096, n - o)
                P.dma(wst[:, 0:m], src_ap[:, o:o + m], writes=["wst"])
                d = dst_ap[:, o:o + m]
                if cast_rr[0] % 2 == 0:
                    P.act(lambda e, d=d, m=m: e.copy(out=d, in_=wst[:, 0:m]), ["wst"], [dst_key])
                else:
                    P.dve(lambda e, d=d, m=m: e.tensor_copy(out=d, in_=wst[:, 0:m]), ["wst"], [dst_key])
                cast_rr[0] += 1
                o += m

        x1T = sb(G, "x1T", [128, 8, TO], BF16)
        wst3 = wst[:].rearrange("p (k n) -> p k n", k=8)
        A_ = ExitStack()
        oattnT = sb(A_, "oattnT", [128, 4, TO], BF16)

        with ExitStack() as SN:
            QT = sb(SN, "QT", [128, 4, TO], BF16)
            slckT = sb(SN, "slckT", [128, T], BF16)
            winkT = sb(SN, "winkT", [128, T], BF16)
            slcv1 = sb(SN, "slcv1", [128, 32, 2, 65], BF16)
            winv1 = sb(SN, "winv1", [128, 32, 2, 65], BF16)
            gates = sb(SN, "gates", [128, 16, 24], F32)
            kcmpT = sb(SN, "kcmpT", [128, 256], BF16)
            vcmp1 = sb(SN, "vcmp1", [128, 2, 2, 65], BF16)
            pt64 = sb(SN, "pt64", [128, 128], BF16)
            P.dma(pt64[:], cdr["pt64"], writes=["pt64"])
            P.dve(lambda e: e.memset(slcv1[:].rearrange("p a g d -> p (a g d)"), 1.0), [], ["slcv1"])
            P.dve(lambda e: e.memset(winv1[:].rearrange("p a g d -> p (a g d)"), 1.0), [], ["winv1"])
            P.dve(lambda e: e.memset(kcmpT[:], 0.0), [], ["kcmpT"])
            P.dve(lambda e: e.memset(vcmp1[:].rearrange("p a g d -> p (a g d)"), 0.0), [], ["vcmp1"])
            P.dve(lambda e: e.memset(vcmp1[:, :, :, 64:65], 1.0), [], ["vcmp1"])

            with ExitStack() as S12:
                cmpT = {"k": sb(S12, "cmpkT", [128, T], BF16), "v": sb(S12, "cmpvT", [128, T], BF16)}
                with ExitStack() as S1:
                    Wn = sb(S1, "Wn", [128, 8, 1304], BF16)
                    load_cast(Wn[:].rearrange("p k n -> p (k n)"), w1t, 8 * 1304, "Wn")
                    xb = [sb(S1, "xb%d" % i, [128, 8, 512], BF16) for i in range(2)]
                    tabs = [sb(S1, "tab%d" % i, [128, 2, 512], F32) for i in range(2)]
                    ybf = [sb(S1, "ybf%d" % i, [128, 512], BF16) for i in range(2)]
                    t1 = [sb(S1, "t1_%d" % i, [128, 512], F32) for i in range(2)]
                    t2 = [sb(S1, "t2_%d" % i, [128, 512], F32) for i in range(2)]
                    pj = [ps(S1, "pj%d" % i, [128, 512]) for i in range(3)]
                    prot = [ps(S1, "prot%d" % i, [128, 512]) for i in range(2)]
                    pv = [ps(S1, "pv%d" % i, [128, 256]) for i in range(2)]
                    pjr = Ring(list(range(3)))
                    rr = Ring(list(range(2)))
                    pvr = Ring(list(range(2)))
                    xTv = xT.rearrange("(k p) t -> p k t", p=128)
                    xTov = xTo.rearrange("(k p) t -> p k t", p=128)

                    def load_x(src_view, c0, n, slot):
                        P.dma(wst3[:, :, 0:n], src_view[:, :, c0:c0 + n], writes=["wst"])
                        P.act(lambda e: e.copy(out=xb[slot][:, 0:4, 0:n], in_=wst3[:, 0:4, 0:n]), ["wst"], ["xb%d" % slot])
                        P.dve(lambda e: e.tensor_copy(out=xb[slot][:, 4:8, 0:n], in_=wst3[:, 4:8, 0:n]), ["wst"], ["xb%d" % slot])

                    def proj_fm(col0, slot, n=512):
                        pi = pjr.next()
                        for k in range(8):
                            P.pe(lambda e, k=k, pi=pi: e.matmul(pj[pi][:, 0:n], lhsT=Wn[:, k, col0:col0 + 128], rhs=xb[slot][:, k, 0:n],
                                                                 start=(k == 0), stop=(k == 7)), ["Wn", "xb%d" % slot], ["pj%d" % pi])
                        return pi

                    def rope_fm(pi, tslot, dst_ap, dst_key, ptm, ptkey, n=512, src=None, srckey=None):
                        r = rr.next()
                        srcap = pj[pi][:, 0:n] if src is None else src
                        sk = ("pj%d" % pi) if srckey is None else srckey
                        P.act(lambda e: e.copy(out=ybf[r][:, 0:n], in_=srcap), [sk], ["ybf%d" % r])
                        P.pe(lambda e: e.matmul(prot[r][:, 0:n], lhsT=ptm[:], rhs=ybf[r][:, 0:n], start=True, stop=True),
                             [ptkey, "ybf%d" % r], ["prot%d" % r])
                        P.dve(lambda e: e.tensor_tensor(out=t1[r][:, 0:n], in0=srcap, in1=tabs[tslot][:, 0, 0:n], op=ALU.mult),
                              [sk, "tab%d" % tslot], ["t1_%d" % r])
                        P.dve(lambda e: e.tensor_tensor(out=t2[r][:, 0:n], in0=prot[r][:, 0:n], in1=tabs[tslot][:, 1, 0:n], op=ALU.mult),
                              ["prot%d" % r, "tab%d" % tslot], ["t2_%d" % r])
                        P.pool(lambda e: e.tensor_tensor(out=dst_ap, in0=t1[r][:, 0:n], in1=t2[r][:, 0:n], op=ALU.add),
                               ["t1_%d" % r, "t2_%d" % r], [dst_key])

                    for ch in range(8):
                        slot = ch % 2
                        c0 = ch * 512
                        load_x(xTv, c0, 512, slot)
                        P.dma(tabs[slot][:, 0, :], cdr["cosK"][:, c0:c0 + 512], writes=["tab%d" % slot])
                        P.dma(tabs[slot][:, 1, :], cdr["sinK"][:, c0:c0 + 512], writes=["tab%d" % slot])
                        for col0, kv in ((512, "k"), (640, "v")):
                            pi = proj_fm(col0, slot)
                            P.act(lambda e, pi=pi, kv=kv: e.copy(out=cmpT[kv][:, c0:c0 + 512], in_=pj[pi][:]), ["pj%d" % pi], ["cmp" + kv + "T"])
                        for col0, dst, dk in ((768, slckT, "slckT"), (896, winkT, "winkT")):
                            pi = proj_fm(col0, slot)
                            rope_fm(pi, slot, dst[:, c0:c0 + 512], dk, pt64, "pt64")
                        for tt in range(4):
                            vi = pvr.next()
                            for k in range(8):
                                P.pe(lambda e, k=k, vi=vi, tt=tt: e.matmul(pv[vi][:], lhsT=xb[slot][:, k, tt * 128:(tt + 1) * 128], rhs=Wn[:, k, 1024:1280],
                                                                            start=(k == 0), stop=(k == 7)), ["Wn", "xb%d" % slot], ["pv%d" % vi])
                            tg = ch * 4 + tt
                            P.act(lambda e, vi=vi, tg=tg: e.copy(out=slcv1[:, tg, :, 0:64], in_=pv[vi][:, 0:128].rearrange("p (g d) -> p g d", g=2)),
                                  ["pv%d" % vi], ["slcv1"])
                            P.dve(lambda e, vi=vi, tg=tg: e.tensor_copy(out=winv1[:, tg, :, 0:64], in_=pv[vi][:, 128:256].rearrange("p (g d) -> p g d", g=2)),
                                  ["pv%d" % vi], ["winv1"])
                    for oc in range(4):
                        slot = oc % 2
                        c0 = oc * 512
                        load_x(xTov, c0, 512, slot)
                        P.dma(tabs[slot][:, 0, :], cdr["cosQ"][:, c0:c0 + 512], writes=["tab%d" % slot])
                        P.dma(tabs[slot][:, 1, :], cdr["sinQ"][:, c0:c0 + 512], writes=["tab%d" % slot])
                        for hh in range(4):
                            pi = proj_fm(hh * 128, slot)
                            rope_fm(pi, slot, QT[:, hh, c0:c0 + 512], "QT", pt64, "pt64")
                        for tt in range(4):
                            vi = pvr.next()
                            for k in range(8):
                                P.pe(lambda e, k=k, vi=vi, tt=tt: e.matmul(pv[vi][:, 0:24], lhsT=xb[slot][:, k, tt * 128:(tt + 1) * 128], rhs=Wn[:, k, 1280:1304],
                                                                            start=(k == 0), stop=(k == 7)), ["Wn", "xb%d" % slot], ["pv%d" % vi])
                            tg = oc * 4 + tt
                            P.act(lambda e, vi=vi, tg=tg: e.activation(out=gates[:, tg, :], in_=pv[vi][:, 0:24], func=AF.Sigmoid), ["pv%d" % vi], ["gates"])
                    dump("QT", QT[:].rearrange("p a t -> p (a t)"), [128, 4 * TO], "QT")
                    dump("slckT", slckT[:], [128, T], "slckT")
                    dump("cmpkT", cmpT["k"][:], [128, T], "cmpkT")
                    dump("slcv1", slcv1[:].rearrange("p a g d -> p (a g d)"), [128, 32 * 130], "slcv1")
                    dump("gates", gates[:].rearrange("p a g -> p (a g)"), [128, 16 * 24], "gates")
                P.barrier()
                if n_stage >= 2:
                    with ExitStack() as S2:
                        w1b = sb(S2, "w1b", [128, 32, 256], BF16)
                        posT = sb(S2, "posT", [128, 32], F32)
                        posTb = sb(S2, "posTb", [128, 32], BF16)
                        b1 = sb(S2, "b1", [128, 2], F32)
                        bias1 = sb(S2, "bias1", [128, 2], F32)
                        w2kf = sb(S2, "w2kf", [128, 2, 128], F32)
                        w2k = sb(S2, "w2k", [128, 2, 128], BF16)
                        w2vf = sb(S2, "w2vf", [128, 2, 64], F32)
                        w2v = sb(S2, "w2v", [128, 2, 64], BF16)
                        b2k = sb(S2, "b2k", [128, 1], F32)
                        b2v = sb(S2, "b2v", [128, 64], F32)
                        tabC = sb(S2, "tabC", [128, 2, 256], F32)
                        h1 = sb(S2, "h1", [128, 2, 256], BF16)
                        xg = sb(S2, "xg", [128, 256], F32)
                        ug = sb(S2, "ug", [128, 256], F32)
                        sg_ = sb(S2, "sg_", [128, 256], F32)
                        yk = sb(S2, "yk", [128, 256], F32)
                        ykb = sb(S2, "ykb", [128, 256], BF16)
                        tk1 = sb(S2, "tk1", [128, 256], F32)
                        tk2 = sb(S2, "tk2", [128, 256], F32)
                        ph = [ps(S2, "ph%d" % i, [128, 256]) for i in range(2)]
                        pcv = ps(S2, "pcv", [128, 2])
                        pkc = ps(S2, "pkc", [128, 256])
                        prk = ps(S2, "prk", [128, 256])
                        pvc = ps(S2, "pvc", [128, 64])
                        P.dma(w2kf[:].rearrange("p a n -> p (a n)"), cw2k, writes=["w2kf"])
                        P.dve(lambda e: e.tensor_copy(out=w2k[:], in_=w2kf[:]), ["w2kf"], ["w2k"])
                        P.dma(w2vf[:].rearrange("p a n -> p (a n)"), cw2v, writes=["w2vf"])
                        P.dve(lambda e: e.tensor_copy(out=w2v[:], in_=w2vf[:]), ["w2vf"], ["w2v"])
                        P.dma(b2k[:], cb2k, writes=["b2k"])
                        P.dma(b2v[:], cb2v.partition_broadcast(128), writes=["b2v"])
                        P.dma(tabC[:, 0, :], cdr["cosC"], writes=["tabC"])
                        P.dma(tabC[:, 1, :], cdr["sinC"], writes=["tabC"])
                        for kv in "kv":
                            load_cast(w1b[:].rearrange("p l n -> p (l n)"), cw1[kv], 32 * 256, "w1b")
                            P.dma(posT[:], cpos[kv], writes=["posT"])
                            P.dve(lambda e: e.tensor_copy(out=posTb[:], in_=posT[:]), ["posT"], ["posTb"])
                            P.dma(b1[:], cb1[kv], writes=["b1"])
                            for ht in range(2):
                                for l in range(32):
                                    P.pe(lambda e, ht=ht, l=l: e.matmul(pcv[:, ht:ht + 1], lhsT=w1b[0:64, l, ht * 128:(ht + 1) * 128], rhs=posTb[0:64, l:l + 1],
                                                                         start=(l == 0), stop=(l == 31)), ["w1b", "posTb"], ["pcv"])
                            P.dve(lambda e: e.tensor_tensor(out=bias1[:], in0=pcv[:], in1=b1[:], op=ALU.add), ["pcv", "b1"], ["bias1"])
                            for g in range(2):
                                gp = slice(g * 64, (g + 1) * 64)
                                for ht in range(2):
                                    for l in range(32):
                                        P.pe(lambda e, ht=ht, l=l, gp=gp, kv=kv: e.matmul(ph[ht][:, 0:255], lhsT=w1b[gp, l, ht * 128:(ht + 1) * 128],
                                                                                        rhs=cmpT[kv][gp, l:l + 16 * 254 + 1:16],
                                                                                        start=(l == 0), stop=(l == 31)), ["w1b", "cmp" + kv + "T"], ["ph%d" % ht])
                                    P.act(lambda e, ht=ht: e.activation(out=xg[:, 0:255], in_=ph[ht][:, 0:255], func=AF.Identity, bias=bias1[:, ht:ht + 1], scale=1.0),
                                          ["ph%d" % ht, "bias1"], ["xg"])
                                    P.dve(lambda e: e.tensor_tensor(out=ug[:, 0:255], in0=xg[:, 0:255], in1=xg[:, 0:255], op=ALU.mult), ["xg"], ["ug"])
                                    P.dve(lambda e: e.tensor_scalar(out=ug[:, 0:255], in0=ug[:, 0:255], scalar1=0.044715, scalar2=1.0, op0=ALU.mult, op1=ALU.add), ["ug"], ["ug"])
                                    P.dve(lambda e: e.tensor_tensor(out=ug[:, 0:255], in0=ug[:, 0:255], in1=xg[:, 0:255], op=ALU.mult), ["ug", "xg"], ["ug"])
                                    P.act(lambda e: e.activation(out=sg_[:, 0:255], in_=ug[:, 0:255], func=AF.Sigmoid, scale=1.5957691216057308), ["ug"], ["sg_"])
                                    P.dve(lambda e, ht=ht: e.tensor_tensor(out=h1[:, ht, 0:255], in0=xg[:, 0:255], in1=sg_[:, 0:255], op=ALU.mult), ["xg", "sg_"], ["h1"])
                                if kv == "k":
                                    for ht in range(2):
                                        P.pe(lambda e, ht=ht: e.matmul(pkc[:, 0:255], lhsT=w2k[:, ht, :], rhs=h1[:, ht, 0:255], start=(ht == 0), stop=(ht == 1)),
                                             ["w2k", "h1"], ["pkc"])
                                    P.act(lambda e: e.activation(out=yk[:, 0:255], in_=pkc[:, 0:255], func=AF.Identity, bias=b2k[:, 0:1], scale=1.0), ["pkc", "b2k"], ["yk"])
                                    P.act(lambda e: e.copy(out=ykb[:, 0:255], in_=yk[:, 0:255]), ["yk"], ["ykb"])
                                    P.pe(lambda e: e.matmul(prk[:, 0:255], lhsT=pt64[:], rhs=ykb[:, 0:255], start=True, stop=True), ["pt64", "ykb"], ["prk"])
                                    P.dve(lambda e: e.tensor_tensor(out=tk1[:, 0:255], in0=yk[:, 0:255], in1=tabC[:, 0, 0:255], op=ALU.mult), ["yk", "tabC"], ["tk1"])
                                    P.dve(lambda e: e.tensor_tensor(out=tk2[:, 0:255], in0=prk[:, 0:255], in1=tabC[:, 1, 0:255], op=ALU.mult), ["prk", "tabC"], ["tk2"])
                                    P.dve(lambda e, gp=gp: e.tensor_tensor(out=kcmpT[gp, 0:255], in0=tk1[gp, 0:255], in1=tk2[gp, 0:255], op=ALU.add), ["tk1", "tk2"], ["kcmpT"])
                                else:
                                    for a in range(2):
                                        cntn = 128 if a == 0 else 127
                                        for ht in range(2):
                                            P.pe(lambda e, ht=ht, a=a, cntn=cntn: e.matmul(pvc[0:cntn, :], lhsT=h1[:, ht, a * 128:a * 128 + cntn], rhs=w2v[:, ht, :],
                                                                                            start=(ht == 0), stop=(ht == 1)), ["w2v", "h1"], ["pvc"])
                                        P.dve(lambda e, a=a, cntn=cntn, g=g: e.tensor_tensor(out=vcmp1[0:cntn, a, g, 0:64], in0=pvc[0:cntn, :], in1=b2v[0:cntn, :], op=ALU.add),
                                              ["pvc", "b2v"], ["vcmp1"])
                        dump("kcmpT", kcmpT[:], [128, 256], "kcmpT")
                        dump("vcmp1", vcmp1[:].rearrange("p a g d -> p (a g d)"), [128, 260], "vcmp1")
                    P.barrier()
            P.barrier()
            if n_stage >= 3:
                with ExitStack() as S3:
                    eall = sb(S3, "eall", [128, 32, 128], BF16)
                    wmask = sb(S3, "wmask", [128, 6, 128], BF16)
                    cmpmask = sb(S3, "cmpmask", [128, 2, 16, 128], BF16)
                    ovT = sb(S3, "ovT", [128, 2, 64], BF16)
                    tkm = sb(S3, "tkm", [128, 16, 64], F32)
                    tkb = sb(S3, "tkb", [128, 16, 64], F32)
                    P.dma(eall[:].rearrange("p a k -> p (a k)"), cdr["eall"], writes=["eall"])
                    P.dma(wmask[:].rearrange("p a k -> p (a k)"), cdr["wmask"], writes=["wmask"])
                    P.dma(cmpmask[:].rearrange("p a i k -> p (a i k)"), cdr["cmpmask"], writes=["cmpmask"])
                    P.dma(ovT[:].rearrange("p a k -> p (a k)"), cdr["ovT"], writes=["ovT"])
                    P.dma(tkm[:].rearrange("p a k -> p (a k)"), cdr["tkm"], writes=["tkm"])
                    P.dma(tkb[:].rearrange("p a k -> p (a k)"), cdr["tkb"], writes=["tkb"])
                    eT = [sb(S3, "eT%d" % i, [128, 512], BF16) for i in range(3)]
                    oacc = sb(S3, "oacc", [128, 512], F32)
                    oab = sb(S3, "oab", [128, 512], BF16)
                    rz = sb(S3, "rz", [128, 4], F32)
                    coef = sb(S3, "coef", [128, 4], F32)
                    imp = sb(S3, "imp", [128, 64], F32)
                    score = sb(S3, "score", [128, 64], F32)
                    work = sb(S3, "work", [128, 64], F32)
                    m8 = sb(S3, "m8", [128, 16], F32)
                    nmk = [sb(S3, "nmk%d" % i_, [128, 2, 64], BF16) for i_ in range(2)]
                    nmT = sb(S3, "nmT", [128, 128], BF16)
                    pST = [ps(S3, "pST%d" % i, [128, 512]) for i in range(2)]
                    pA = ps(S3, "pA", [128, 4, 65])
                    pB = ps(S3, "pB", [128, 4, 64])
                    pS = ps(S3, "pS", [128, 4, 65])
                    pW = ps(S3, "pW", [128, 4, 65])
                    pTr = ps(S3, "pTr", [128, 128], BF16)
                    str_ = Ring([0, 1])
                    etr = Ring([0, 1, 2])

                    def scores(kT_ap, kkey, g, i, masks):
                        gp = slice(g * 64, (g + 1) * 64)
                        si = str_.next()
                        ei = etr.next()
                        nm = len(masks)
                        P.pe(lambda e: e.matmul(pST[si][:].rearrange("p (a t) -> p a t", a=4), lhsT=kT_ap, rhs=QT[gp, :, i * 128:(i + 1) * 128],
                                                start=True, stop=(nm == 0)), [kkey, "QT"], ["pST%d" % si])
                        for mi, (ml, mr, mkeys) in enumerate(masks):
                            P.pe(lambda e, ml=ml, mr=mr, mi=mi: e.matmul(pST[si][:].rearrange("p (a t) -> p a t", a=4), lhsT=ml, rhs=mr,
                                                                           start=False, stop=(mi == nm - 1)), mkeys, ["pST%d" % si])
                        P.act(lambda e: e.activation(out=eT[ei][:], in_=pST[si][:], func=AF.Exp), ["pST%d" % si], ["eT%d" % ei])
                        return ei

                    def bc4(ap):
                        return ap.unsqueeze(1).broadcast_to([ap.shape[0], 4, ap.shape[1]])

                    def finish_branch(pacc, pkey, i, g, br, first):
                        P.dve(lambda e: e.tensor_scalar(out=rz[:], in0=pacc[:, :, 64], scalar1=1e-30, scalar2=None, op0=ALU.max), [pkey], ["rz"])
                        P.dve(lambda e: e.reciprocal(out=rz[:], in_=rz[:]), ["rz"], ["rz"])
                        P.dve(lambda e: e.tensor_tensor(out=coef[:], in0=rz[:], in1=gates[:, i, g * 12 + br:g * 12 + 12:3], op=ALU.mult), ["rz", "gates"], ["coef"])
                        for hh in range(4):
                            o = oacc[:, g * 256 + hh * 64:g * 256 + (hh + 1) * 64]
                            if first:
                                P.dve(lambda e, hh=hh, o=o: e.tensor_scalar(out=o, in0=pacc[:, hh, 0:64], scalar1=coef[:, hh:hh + 1], scalar2=None, op0=ALU.mult),
                                      [pkey, "coef"], ["oacc"])
                            else:
                                P.dve(lambda e, hh=hh, o=o: e.scalar_tensor_tensor(out=o, in0=pacc[:, hh, 0:64], scalar=coef[:, hh:hh + 1], in1=o, op0=ALU.mult, op1=ALU.add),
                                      [pkey, "coef", "oacc"], ["oacc"])

                    tasks = []

                    def mk_cmp(i, g, a, na):
                        gp = slice(g * 64, (g + 1) * 64)

                        def sc():
                            return scores(kcmpT[gp, a * 128:(a + 1) * 128], "kcmpT", g, i,
                                          [(identb[:], bc4(cmpmask[:, a, i, :]), ["identb", "cmpmask"])])

                        def pvf(ei):
                            for hh in range(4):
                                P.pe(lambda e: e.matmul(pA[:, hh, :], lhsT=eT[ei][:, hh * 128:(hh + 1) * 128], rhs=vcmp1[:, a, g, :],
                                                        start=(a == 0 and hh == 0), stop=(a == na - 1 and hh == 3)), ["eT%d" % ei, "vcmp1"], ["pA"])
                                P.pe(lambda e: e.matmul(pB[:, hh, :], lhsT=eT[ei][:, hh * 128:(hh + 1) * 128], rhs=ovT[:, a, :],
                                                        start=(a == 0 and hh == 0), stop=(a == na - 1 and hh == 3)), ["eT%d" % ei, "ovT"], ["pB"])

                        def post():
                            finish_branch(pA, "pA", i, g, 0, True)
                            P.dve(lambda e: e.tensor_scalar(out=imp[:], in0=pB[:, 0, :], scalar1=rz[:, 0:1], scalar2=None, op0=ALU.mult), ["pB", "rz"], ["imp"])
                            for hh in range(1, 4):
                                P.dve(lambda e: e.scalar_tensor_tensor(out=imp[:], in0=pB[:, hh, :], scalar=rz[:, hh:hh + 1], in1=imp[:], op0=ALU.mult, op1=ALU.add),
                                      ["pB", "rz", "imp"], ["imp"])
                            P.dve(lambda e: e.tensor_tensor(out=score[:], in0=imp[:], in1=tkm[:, i, :], op=ALU.mult), ["imp", "tkm"], ["score"])
                            P.dve(lambda e: e.tensor_tensor(out=score[:], in0=score[:], in1=tkb[:, i, :], op=ALU.add), ["score", "tkb"], ["score"])
                            P.dve(lambda e: e.max(out=m8[:, 0:8], in_=score[:]), ["score"], ["m8"])
                            P.dve(lambda e: e.match_replace(out=work[:], in_to_replace=m8[:, 0:8], in_values=score[:], imm_value=-3.0e38), ["score", "m8"], ["work"])
                            P.dve(lambda e: e.max(out=m8[:, 8:16], in_=work[:]), ["work"], ["m8"])
                            P.dve(lambda e: e.tensor_scalar(out=nmk[g][:], in0=score[:].unsqueeze(1).broadcast_to([128, 2, 64]), scalar1=m8[:, 15:16], scalar2=NEGM,
                                                            op0=ALU.is_lt, op1=ALU.mult), ["score", "m8"], ["nmk%d" % g])
                            if ("imp%d_%d" % (i, g)) in debug:
                                dump("imp%d_%d" % (i, g), imp[:], [128, 64], "imp")
                                dump("score%d_%d" % (i, g), score[:], [128, 64], "score")
                                dump("m8%d_%d" % (i, g), m8[:], [128, 16], "m8")
                        return [None, sc, pvf, post if a == na - 1 else None]

                    def mk_win(i, g, idx, r, j, nw):
                        gp = slice(g * 64, (g + 1) * 64)

                        def sc():
                            return scores(winkT[gp, j * 128:(j + 1) * 128], "winkT", g, i,
                                          [(identb[:], bc4(wmask[:, r, :]), ["identb", "wmask"])])

                        def pvf(ei):
                            for hh in range(4):
                                P.pe(lambda e: e.matmul(pW[:, hh, :], lhsT=eT[ei][:, hh * 128:(hh + 1) * 128], rhs=winv1[:, j, g, :],
                                                        start=(idx == 0 and hh == 0), stop=(idx == nw - 1 and hh == 3)), ["eT%d" % ei, "winv1"], ["pW"])

                        def post():
                            finish_branch(pW, "pW", i, g, 2, False)
                        return [None, sc, pvf, post if idx == nw - 1 else None]

                    def tile_end_pe(i):
                        for ct in range(4):
                            P.pe(lambda e: e.transpose(out=pTr[:], in_=oab[:, ct * 128:(ct + 1) * 128], identity=identb[:]), ["oab", "identb"], ["pTr"])
                            P.dve(lambda e: e.tensor_copy(out=oattnT[:, ct, i * 128:(i + 1) * 128], in_=pTr[:]), ["pTr"], ["oattnT"])

                    def mk_slc(i, g, j, nj):
                        gp = slice(g * 64, (g + 1) * 64)

                        def pre():
                            P.pe(lambda e: e.transpose(out=pTr[:], in_=nmk[g][:].rearrange("p a s -> p (a s)"), identity=identb[:]), ["nmk%d" % g, "identb"], ["pTr"])
                            P.act(lambda e: e.copy(out=nmT[gp, :], in_=pTr[gp, :]), ["pTr"], ["nmT%d" % g])
                            if g == 0 and i > 0:
                                tile_end_pe(i - 1)

                        def sc():
                            masks = [(eall[gp, j, :], bc4(nmT[gp, :]), ["eall", "nmT%d" % g])]
                            if j >= 2 * i:
                                masks.append((identb[:], bc4(wmask[:, 4 + (j - 2 * i), :]), ["identb", "wmask"]))
                            return scores(slckT[gp, j * 128:(j + 1) * 128], "slckT", g, i, masks)

                        def pvf(ei):
                            for hh in range(4):
                                P.pe(lambda e: e.matmul(pS[:, hh, :], lhsT=eT[ei][:, hh * 128:(hh + 1) * 128], rhs=slcv1[:, j, g, :],
                                                        start=(j == 0 and hh == 0), stop=(j == nj - 1 and hh == 3)), ["eT%d" % ei, "slcv1"], ["pS"])

                        def post():
                            finish_branch(pS, "pS", i, g, 1, False)
                            if g == 1:
                                if ("oacc%d" % i) in debug:
                                    dump("oacc%d" % i, oacc[:], [128, 512], "oacc")
                                P.pool(lambda e: e.tensor_copy(out=oab[:], in_=oacc[:]), ["oacc"], ["oab"])
                        return [pre if j == 0 else None, sc, pvf, post if j == nj - 1 else None]

                    for i in range(16):
                        for g in range(2):
                            na = 1 if i < 8 else 2
                            for a in range(na):
                                tasks.append(mk_cmp(i, g, a, na))
                            js = [(r, 2 * i - 4 + r) for r in range(6) if 2 * i - 4 + r >= 0]
                            for idx, (r, j) in enumerate(js):
                                tasks.append(mk_win(i, g, idx, r, j, len(js)))
                            nj = 2 * i + 2
                            for j in range(nj):
                                tasks.append(mk_slc(i, g, j, nj))
                    nt = len(tasks)
                    eis = [None] * nt

                    def emit_score(k):
                        if tasks[k][0] is not None:
                            tasks[k][0]()
                        eis[k] = tasks[k][1]()

                    emit_score(0)
                    for k in range(nt):
                        if k + 1 < nt:
                            emit_score(k + 1)
                        tasks[k][2](eis[k])
                        if tasks[k][3] is not None:
                            tasks[k][3]()
                    tile_end_pe(15)
                    dump("oattnT", oattnT[:].rearrange("p a t -> p (a t)"), [128, 4 * TO], "oattnT")
                P.barrier()
        P.barrier()

        B_ = ExitStack()
        oretT = sb(B_, "oretT", [128, 8, TO], BF16)
        if n_stage >= 4:
            with ExitStack() as S4:
                W4 = sb(S4, "W4", [128, 8, 2048], BF16)
                load_cast(W4[:].rearrange("p k n -> p (k n)"), w4t, 8 * 2048, "W4")
                pt128 = sb(S4, "pt128", [128, 128], BF16)
                P.dma(pt128[:], cdr["pt128"], writes=["pt128"])
                Dc = sb(S4, "Dc", [128, 2, 4, 128], F32)
                xi = sb(S4, "xi", [128, 4, 128], F32)
                zeta = sb(S4, "zeta", [128, 2, 4], F32)
                P.dma(Dc[:].rearrange("p a h c -> p (a h c)"), cdr["Dc"], writes=["Dc"])
                P.dma(xi[:].rearrange("p h c -> p (h c)"), cdr["xi"], writes=["xi"])
                P.dma(zeta[:].rearrange("p a h -> p (a h)"), cdr["zeta"], writes=["zeta"])
                xst = wst3
                xb = sb(S4, "xb4", [128, 8, 512], BF16)
                xob = sb(S4, "xob4", [128, 8, 256], BF16)
                tabs = sb(S4, "tab4", [128, 2, 512], F32)
                tabq = sb(S4, "tabq4", [128, 2, 256], F32)
                ybf = sb(S4, "ybf4", [128, 512], BF16)
                t1 = sb(S4, "t1_4", [128, 512], F32)
                t2 = sb(S4, "t2_4", [128, 512], F32)
                kT = sb(S4, "kT4", [128, 4, 512], BF16)
                qT = sb(S4, "qT4", [128, 4, 256], BF16)
                qxT = sb(S4, "qxT4", [128, 4, 256], BF16)
                vtok = sb(S4, "vtok", [128, 4, 1024], BF16)
                kz = sb(S4, "kz", [128, 4, 4, 128], BF16)
                R = sb(S4, "R", [128, 4, 256], F32)
                Rb = sb(S4, "Rb", [128, 4, 256], BF16)
                sc = [sb(S4, "sc%d" % i_, [128, 2, 128], BF16) for i_ in range(2)]
                epsT = sb(S4, "epsT", [128, 1], F32)
                P.dve(lambda e: e.memset(epsT[:], LN_EPS), [], ["epsT"])
                pending4 = []
                st6 = sb(S4, "st6", [128, 6], F32)
                mv = sb(S4, "mv", [128, 2], F32)
                rstd = sb(S4, "rstd", [128, 1], F32)
                oretb = [sb(S4, "oretb%d" % i_, [128, 1024], BF16) for i_ in range(2)]
                pj = [ps(S4, "pj4_%d" % i, [128, 512]) for i in range(3)]
                psc = [ps(S4, "psc%d" % i_, [128, 2, 128]) for i_ in range(2)]
                po = [ps(S4, "po%d" % i_, [128, 256]) for i_ in range(2)]
                pTr = ps(S4, "pTr4", [128, 128], BF16)
                pjr = Ring([0, 1, 2])
                P.dve(lambda e: e.memset(R[:].rearrange("p h e -> p (h e)"), 0.0), [], ["R%d" % h_ for h_ in range(4)])
                P.dve(lambda e: e.memset(Rb[:].rearrange("p h e -> p (h e)"), 0.0), [], ["Rb%d" % h_ for h_ in range(4)])
                xTv = xT.rearrange("(k p) t -> p k t", p=128)
                xTov = xTo.rearrange("(k p) t -> p k t", p=128)

                def rope4(pi, n, tab, tabkey, dst_ap, dst_key):
                    ri = pjr.next()
                    prot = pj[ri]
                    P.act(lambda e: e.copy(out=ybf[:, 0:n], in_=pj[pi][:, 0:n]), ["pj4_%d" % pi], ["ybf4"])
                    P.pe(lambda e: e.matmul(prot[:, 0:n], lhsT=pt128[:], rhs=ybf[:, 0:n], start=True, stop=True), ["pt128", "ybf4"], ["pj4_%d" % ri])
                    P.dve(lambda e: e.tensor_tensor(out=t1[:, 0:n], in0=pj[pi][:, 0:n], in1=tab[:, 0, 0:n], op=ALU.mult), ["pj4_%d" % pi, tabkey], ["t1_4"])
                    P.dve(lambda e: e.tensor_tensor(out=t2[:, 0:n], in0=prot[:, 0:n], in1=tab[:, 1, 0:n], op=ALU.mult), ["pj4_%d" % ri, tabkey], ["t2_4"])
                    P.pool(lambda e: e.tensor_tensor(out=dst_ap, in0=t1[:, 0:n], in1=t2[:, 0:n], op=ALU.add), ["t1_4", "t2_4"], [dst_key])

                for gch in range(8):
                    c0 = gch * 512
                    o0 = gch * 256
                    P.dma(xst[:], xTv[:, :, c0:c0 + 512], writes=["wst"])
                    P.act(lambda e: e.copy(out=xb[:, 0:4, :], in_=xst[:, 0:4, :]), ["wst"], ["xb4"])
                    P.dve(lambda e: e.tensor_copy(out=xb[:, 4:8, :], in_=xst[:, 4:8, :]), ["wst"], ["xb4"])
                    P.dma(xst[:, :, 0:256], xTov[:, :, o0:o0 + 256], writes=["wst"])
                    P.act(lambda e: e.copy(out=xob[:, 0:4, :], in_=xst[:, 0:4, 0:256]), ["wst"], ["xob4"])
                    P.dve(lambda e: e.tensor_copy(out=xob[:, 4:8, :], in_=xst[:, 4:8, 0:256]), ["wst"], ["xob4"])
                    P.dma(tabs[:, 0, :], cdr["cosRK"][:, c0:c0 + 512], writes=["tab4"])
                    P.dma(tabs[:, 1, :], cdr["sinRK"][:, c0:c0 + 512], writes=["tab4"])
                    P.dma(tabq[:, 0, :], cdr["cosRQ"][:, o0:o0 + 256], writes=["tabq4"])
                    P.dma(tabq[:, 1, :], cdr["sinRQ"][:, o0:o0 + 256], writes=["tabq4"])
                    for h in range(4):
                        pi = pjr.next()
                        for k in range(8):
                            P.pe(lambda e, k=k, pi=pi, h=h: e.matmul(pj[pi][:], lhsT=W4[:, k, 512 + h * 128:512 + (h + 1) * 128], rhs=xb[:, k, :],
                                                                      start=(k == 0), stop=(k == 7)), ["W4", "xb4"], ["pj4_%d" % pi])
                        rope4(pi, 512, tabs, "tab4", kT[:, h, :], "kT4")
                    for h in range(4):
                        pi = pjr.next()
                        for k in range(8):
                            P.pe(lambda e, k=k, pi=pi, h=h: e.matmul(pj[pi][:, 0:256], lhsT=W4[:, k, h * 128:(h + 1) * 128], rhs=xob[:, k, :],
                                                                      start=(k == 0), stop=(k == 7)), ["W4", "xob4"], ["pj4_%d" % pi])
                        rope4(pi, 256, tabq, "tabq4", qT[:, h, :], "qT4")
                    for pp in range(2):
                        P.dve(lambda e, pp=pp: e.tensor_tensor(out=qxT[:, :, pp * 128:(pp + 1) * 128], in0=qT[:, :, pp * 128:(pp + 1) * 128], in1=xi[:], op=ALU.mult),
                              ["qT4", "xi"], ["qxT4"])
                    for tt in range(4):
                        for hf in range(2):
                            pi = pjr.next()
                            for k in range(8):
                                P.pe(lambda e, k=k, pi=pi, tt=tt, hf=hf: e.matmul(pj[pi][:], lhsT=xb[:, k, tt * 128:(tt + 1) * 128],
                                                                                   rhs=W4[:, k, 1024 + hf * 512:1024 + (hf + 1) * 512],
                                                                                   start=(k == 0), stop=(k == 7)), ["W4", "xb4"], ["pj4_%d" % pi])
                            P.act(lambda e, pi=pi, tt=tt, hf=hf: e.copy(out=vtok[:, tt, hf * 512:(hf + 1) * 512], in_=pj[pi][:]), ["pj4_%d" % pi], ["vtok"])
                    for tt in range(4):
                        for h in range(4):
                            P.pe(lambda e, tt=tt, h=h: e.transpose(out=pTr[:], in_=kT[:, h, tt * 128:(tt + 1) * 128], identity=identb[:]), ["kT4", "identb"], ["pTr4"])
                            P.dve(lambda e, tt=tt, h=h: e.tensor_scalar(out=kz[:, tt, h, :], in0=pTr[:], scalar1=zeta[:, tt % 2, h:h + 1], scalar2=None, op0=ALU.mult),
                                  ["pTr4", "zeta"], ["kz"])
                    units = [(pp, h) for pp in range(2) for h in range(4)]

                    def phaseA(pp, h, ub):
                        qs = slice(pp * 128, (pp + 1) * 128)
                        for mt in range(2):
                            tt = pp * 2 + mt
                            P.pe(lambda e: e.matmul(psc[ub][:, mt, :], lhsT=kT[:, h, tt * 128:(tt + 1) * 128], rhs=qT[:, h, qs], start=True, stop=True),
                                 ["kT4", "qT4"], ["psc%d" % ub])
                        P.dve(lambda e: e.tensor_tensor(out=sc[ub][:], in0=psc[ub][:], in1=Dc[:, :, h, :], op=ALU.mult), ["psc%d" % ub, "Dc"], ["sc%d" % ub])

                    def phaseBC(pp, h, ub):
                        i = gch * 2 + pp
                        qs = slice(pp * 128, (pp + 1) * 128)
                        hs = slice(h * 256, (h + 1) * 256)
                        ob = oretb[pp]
                        for mt in range(2):
                            tt = pp * 2 + mt
                            P.pe(lambda e: e.matmul(po[ub][:], lhsT=sc[ub][:, mt, :], rhs=vtok[:, tt, hs], start=(mt == 0), stop=False), ["sc%d" % ub, "vtok"], ["po%d" % ub])
                        P.pe(lambda e: e.matmul(po[ub][:], lhsT=qxT[:, h, qs], rhs=Rb[:, h, :], start=False, stop=True), ["qxT4", "Rb%d" % h], ["po%d" % ub])
                        ri = pjr.next()
                        for mt in range(2):
                            tt = pp * 2 + mt
                            P.pe(lambda e: e.matmul(pj[ri][:, 0:256], lhsT=kz[:, tt, h, :], rhs=vtok[:, tt, hs], start=(mt == 0), stop=(mt == 1)),
                                 ["kz", "vtok"], ["pj4_%d" % ri])
                        P.dve(lambda e: e.bn_stats(out=st6[:], in_=po[ub][:]), ["po%d" % ub], ["st6"])
                        P.dve(lambda e: e.bn_aggr(out=mv[:], in_=st6[:]), ["st6"], ["mv"])
                        P.act(lambda e: e.activation(out=rstd[:], in_=mv[:, 1:2], func=AF.Sqrt, bias=epsT[:, 0:1], scale=1.0), ["mv", "epsT"], ["rstd"])
                        P.dve(lambda e: e.reciprocal(out=rstd[:], in_=rstd[:]), ["rstd"], ["rstd"])
                        P.dve(lambda e: e.tensor_scalar(out=ob[:, hs], in0=po[ub][:], scalar1=mv[:, 0:1], scalar2=rstd[:, 0:1], op0=ALU.subtract, op1=ALU.mult),
                              ["po%d" % ub, "mv", "rstd"], ["oretb%d" % pp])
                        P.dve(lambda e: e.scalar_tensor_tensor(out=R[:, h, :], in0=R[:, h, :], scalar=decay256[h], in1=pj[ri][:, 0:256], op0=ALU.mult, op1=ALU.add),
                              ["R%d" % h, "pj4_%d" % ri], ["R%d" % h])
                        P.act(lambda e: e.copy(out=Rb[:, h, :], in_=R[:, h, :]), ["R%d" % h], ["Rb%d" % h])

                    def pair_end(pp, i):
                        ob = oretb[pp]
                        for et in range(8):
                            P.pe(lambda e: e.transpose(out=pTr[:], in_=ob[:, et * 128:(et + 1) * 128], identity=identb[:]), ["oretb%d" % pp, "identb"], ["pTr4"])
                            P.act(lambda e: e.copy(out=oretT[:, et, i * 128:(i + 1) * 128], in_=pTr[:]), ["pTr4"], ["oretT"])

                    phaseA(units[0][0], units[0][1], 0)
                    for u, (pp, h) in enumerate(units):
                        if u + 1 < len(units):
                            phaseA(units[u + 1][0], units[u + 1][1], (u + 1) % 2)
                        phaseBC(pp, h, u % 2)
                        if pending4:
                            pending4.pop(0)()
                        if h == 3:
                            pending4.append(lambda pp=pp, i=gch * 2 + pp: pair_end(pp, i))
                while pending4:
                    pending4.pop(0)()
                dump("oretT", oretT[:].rearrange("p a t -> p (a t)"), [128, 8 * TO], "oretT")
            P.barrier()

        if n_stage >= 5:
            with ExitStack() as S5a:
                Wmg = sb(S5a, "Wmg", [128, 8, 2048], BF16)
                Wa = sb(S5a, "Wa", [128, 4, 1024], BF16)
                Wr = sb(S5a, "Wr", [128, 8, 1024], BF16)
                Wgt = sb(S5a, "Wgt", [128, 8, 1024], BF16)
                load_cast(Wmg[:].rearrange("p k n -> p (k n)"), wmgt, 8 * 2048, "Wmg")
                load_cast(Wa[:].rearrange("p k n -> p (k n)"), wat, 4 * 1024, "Wa")
                load_cast(Wr[:].rearrange("p k n -> p (k n)"), wrt, 8 * 1024, "Wr")
                load_cast(Wgt[:].rearrange("p k n -> p (k n)"), wgtt, 8 * 1024, "Wgt")
                gg8 = sb(S5a, "gg8", [128, 8], F32)
                gb8 = sb(S5a, "gb8", [128, 8], F32)
                P.dma(gg8[:], gng8, writes=["gg8"])
                P.dma(gb8[:], gnb8, writes=["gb8"])
                xb = sb(S5a, "xb5", [128, 8, 512], BF16)
                og = sb(S5a, "og", [128, 8, 512], BF16)
                sgt = [sb(S5a, "sgt%d" % i, [128, 512], F32) for i in range(2)]
                yn = [sb(S5a, "yn%d" % i, [128, 512], F32) for i in range(2)]
                ga2 = [sb(S5a, "ga%d" % i, [128, 512], F32) for i in range(2)]
                gr2 = [sb(S5a, "gr%d" % i, [128, 512], F32) for i in range(2)]
                ma2 = [sb(S5a, "ma%d" % i, [128, 512], F32) for i in range(2)]
                bk = [ps(S5a, "bk%d" % i, [128, 512]) for i in range(8)]
                pgt = [bk[4], bk[5]]
                xTov = xTo.rearrange("(k p) t -> p k t", p=128)
                for oc in range(4):
                    c0 = oc * 512
                    cs_ = slice(c0, c0 + 512)
                    P.dma(wst3[:], xTov[:, :, cs_], writes=["wst"])
                    P.act(lambda e: e.copy(out=xb[:, 0:4, :], in_=wst3[:, 0:4, :]), ["wst"], ["xb5"])
                    P.dve(lambda e: e.tensor_copy(out=xb[:, 4:8, :], in_=wst3[:, 4:8, :]), ["wst"], ["xb5"])
                    for et in range(8):
                        b_ = et % 2
                        for k in range(8):
                            P.pe(lambda e: e.matmul(pgt[b_][:], lhsT=Wgt[:, k, et * 128:(et + 1) * 128], rhs=xb[:, k, :], start=(k == 0), stop=(k == 7)),
                                 ["Wgt", "xb5"], ["bk%d" % (4 + b_)])
                        P.act(lambda e: e.activation(out=sgt[b_][:], in_=pgt[b_][:], func=AF.Silu), ["bk%d" % (4 + b_)], ["sgt%d" % b_])
                        P.act(lambda e: e.activation(out=yn[b_][:], in_=oretT[:, et, cs_], func=AF.Identity, scale=gg8[:, et:et + 1], bias=gb8[:, et:et + 1]),
                              ["oretT", "gg8", "gb8"], ["yn%d" % b_])
                        P.dve(lambda e: e.tensor_tensor(out=og[:, et, :], in0=yn[b_][:], in1=sgt[b_][:], op=ALU.mult), ["yn%d" % b_, "sgt%d" % b_], ["og"])
                    for ct in range(8):
                        cb = (ct % 2) * 4
                        cp = ct % 2
                        pg0, pg1, pu0, pu1 = bk[cb], bk[cb + 1], bk[cb + 2], bk[cb + 3]
                        kg0, kg1, ku0, ku1 = ["bk%d" % (cb + q_) for q_ in range(4)]
                        ga, gr, ma = ga2[cp], gr2[cp], ma2[cp]
                        for k in range(8):
                            P.pe(lambda e: e.matmul(pg0[:], lhsT=Wmg[:, k, ct * 128:(ct + 1) * 128], rhs=xb[:, k, :], start=(k == 0), stop=(k == 7)),
                                 ["Wmg", "xb5"], [kg0])
                        for k in range(8):
                            P.pe(lambda e: e.matmul(pg1[:], lhsT=Wmg[:, k, 1024 + ct * 128:1024 + (ct + 1) * 128], rhs=xb[:, k, :], start=(k == 0), stop=(k == 7)),
                                 ["Wmg", "xb5"], [kg1])
                        for k in range(4):
                            P.pe(lambda e: e.matmul(pu0[:], lhsT=Wa[:, k, ct * 128:(ct + 1) * 128], rhs=oattnT[:, k, cs_], start=(k == 0), stop=(k == 3)),
                                 ["Wa", "oattnT"], [ku0])
                        for k in range(8):
                            P.pe(lambda e: e.matmul(pu1[:], lhsT=Wr[:, k, ct * 128:(ct + 1) * 128], rhs=og[:, k, :], start=(k == 0), stop=(k == 7)),
                                 ["Wr", "og"], [ku1])
                        P.act(lambda e: e.activation(out=ga[:], in_=pg0[:], func=AF.Sigmoid), [kg0], ["ga%d" % cp])
                        P.act(lambda e: e.activation(out=gr[:], in_=pg1[:], func=AF.Sigmoid), [kg1], ["gr%d" % cp])
                        P.dve(lambda e: e.tensor_tensor(out=ma[:], in0=pu0[:], in1=ga[:], op=ALU.mult), [ku0, "ga%d" % cp], ["ma%d" % cp])
                        P.dve(lambda e: e.tensor_tensor(out=gr[:], in0=pu1[:], in1=gr[:], op=ALU.mult), [ku1, "gr%d" % cp], ["gr%d" % cp])
                        P.pool(lambda e: e.tensor_tensor(out=x1T[:, ct, cs_], in0=ma[:], in1=gr[:], op=ALU.add), ["ma%d" % cp, "gr%d" % cp], ["mx%d" % (oc * 4 + t_) for t_ in range(4)])
                dump("mergedT", x1T[:].rearrange("p a t -> p (a t)"), [128, 8 * TO], "mx0")
            P.barrier()
        B_.close()
        A_.close()
        if n_stage >= 5:
            with ExitStack() as S56:
                acc = sb(S56, "acc", [128, 16, 1024], F32)
                lng = sb(S56, "lng", [128, 1024], F32)
                lnb = sb(S56, "lnb", [128, 1024], F32)
                st12 = sb(S56, "st12", [128, 2, 6], F32)
                mv = sb(S56, "mv5", [128, 2], F32)
                rstd = sb(S56, "rstd5", [128, 1], F32)

                def layer_norm(src_ap, src_key, dst_ap, dst_key, tmp_ap, tmp_key):
                    for hf in range(2):
                        P.dve(lambda e: e.bn_stats(out=st12[:, hf, :], in_=src_ap[:, hf * 512:(hf + 1) * 512]), [src_key], ["st12"])
                    P.dve(lambda e: e.bn_aggr(out=mv[:], in_=st12[:].rearrange("p a s -> p (a s)")), ["st12"], ["mv5"])
                    P.dve(lambda e: e.tensor_scalar(out=rstd[:], in0=mv[:, 1:2], scalar1=LN_EPS, scalar2=None, op0=ALU.add), ["mv5"], ["rstd5"])
                    P.act(lambda e: e.activation(out=rstd[:], in_=rstd[:], func=AF.Sqrt), ["rstd5"], ["rstd5"])
                    P.dve(lambda e: e.reciprocal(out=rstd[:], in_=rstd[:]), ["rstd5"], ["rstd5"])
                    P.dve(lambda e: e.tensor_scalar(out=tmp_ap, in0=src_ap, scalar1=mv[:, 0:1], scalar2=rstd[:, 0:1], op0=ALU.subtract, op1=ALU.mult),
                          [src_key, "mv5", "rstd5"], [tmp_key])
                    P.pool(lambda e: e.tensor_tensor(out=tmp_ap, in0=tmp_ap, in1=lng[:], op=ALU.mult), [tmp_key, "lng"], [tmp_key])
                    P.pool(lambda e: e.tensor_tensor(out=dst_ap, in0=tmp_ap, in1=lnb[:], op=ALU.add), [tmp_key, "lnb"], [dst_key])

                with ExitStack() as S5b:
                    Wo = sb(S5b, "Wo", [128, 8, 1024], BF16)
                    load_cast(Wo[:].rearrange("p k n -> p (k n)"), wot, 8 * 1024, "Wo")
                    P.dma(lng[:], ln1g.partition_broadcast(128), writes=["lng"])
                    P.dma(lnb[:], ln1b.partition_broadcast(128), writes=["lnb"])
                    xres2 = [sb(S5b, "xres%d" % i_, [128, 1024], F32) for i_ in range(2)]
                    yt2 = [sb(S5b, "yt%d" % i_, [128, 1024], F32) for i_ in range(2)]
                    x12 = [sb(S5b, "x1_%d" % i_, [128, 1024], F32) for i_ in range(2)]
                    x1b2 = [sb(S5b, "x1b%d" % i_, [128, 1024], BF16) for i_ in range(2)]
                    pm2 = [[ps(S5b, "pm%d_%d" % (q_, i_), [128, 512]) for i_ in range(2)] for q_ in range(2)]
                    pTr = ps(S5b, "pTr5", [128, 128], BF16)
                    pend5 = []
                    for i in range(16):
                        q_ = i % 2
                        xres, yt, x1, x1b, pm = xres2[q_], yt2[q_], x12[q_], x1b2[q_], pm2[q_]
                        ts_ = slice(i * 128, (i + 1) * 128)
                        P.dma(xres[:], xo[ts_, :], writes=["xres%d" % q_])
                        for hf in range(2):
                            for k in range(8):
                                P.pe(lambda e: e.matmul(pm[hf][:], lhsT=x1T[:, k, ts_], rhs=Wo[:, k, hf * 512:(hf + 1) * 512], start=(k == 0), stop=(k == 7)),
                                     ["mx%d" % i, "Wo"], ["pm%d_%d" % (q_, hf)])
                            P.dve(lambda e: e.scalar_tensor_tensor(out=yt[:, hf * 512:(hf + 1) * 512], in0=xres[:, hf * 512:(hf + 1) * 512], scalar=ALPHA,
                                                                   in1=pm[hf][:], op0=ALU.mult, op1=ALU.add), ["xres%d" % q_, "pm%d_%d" % (q_, hf)], ["yt%d" % q_])
                        layer_norm(yt[:], "yt%d" % q_, x1[:], "x1_%d" % q_, yt[:], "yt%d" % q_)
                        if ("x1_%d" % i) in debug:
                            dump("x1_%d" % i, x1[:], [128, 1024], "x1_%d" % q_)
                        P.dve(lambda e: e.tensor_scalar(out=acc[:, i, :], in0=x1[:], scalar1=ALPHA, scalar2=None, op0=ALU.mult), ["x1_%d" % q_], ["acc%d" % i])
                        P.act(lambda e: e.copy(out=x1b[:], in_=x1[:]), ["x1_%d" % q_], ["x1b%d" % q_])
                        if pend5:
                            pend5.pop(0)()

                        def tr5(i=i, q_=q_, x1b=x1b, ts_=ts_):
                            for dt_ in range(8):
                                P.pe(lambda e: e.transpose(out=pTr[:], in_=x1b[:, dt_ * 128:(dt_ + 1) * 128], identity=identb[:]), ["x1b%d" % q_, "identb"], ["pTr5"])
                                P.act(lambda e: e.copy(out=x1T[:, dt_, ts_], in_=pTr[:]), ["pTr5"], ["mx%d" % i])
                        pend5.append(tr5)
                    while pend5:
                        pend5.pop(0)()
                P.barrier()
                if n_stage >= 6:
                    with ExitStack() as S6:
                        P.dma(lng[:], ln2g.partition_broadcast(128), writes=["lng"])
                        P.dma(lnb[:], ln2b.partition_broadcast(128), writes=["lnb"])
                        comb = sb(S6, "comb", [128, 16, 32], F32)
                        wrf = sb(S6, "wrf", [128, 8, 36], F32)
                        wrb = sb(S6, "wrb", [128, 8, 36], BF16)
                        brb = sb(S6, "brb", [128, 36], F32)
                        P.dma(wrf[:].rearrange("p k n -> p (k n)"), wrout, writes=["wrf"])
                        P.dve(lambda e: e.tensor_copy(out=wrb[:], in_=wrf[:]), ["wrf"], ["wrb"])
                        P.dma(brb[:], brout.partition_broadcast(128), writes=["brb"])
                        lg = sb(S6, "lg", [128, 36], F32)
                        gmx = sb(S6, "gmx", [128, 1], F32)
                        ngmx = sb(S6, "ngmx", [128, 1], F32)
                        gex = sb(S6, "gex", [128, 4], F32)
                        gsum = sb(S6, "gsum", [128, 1], F32)
                        gprob = sb(S6, "gprob", [128, 1], F32)
                        ohg = sb(S6, "ohg", [128, 4], F32)
                        tmp48 = sb(S6, "tmp48", [128, 4, 8], F32)
                        isel = sb(S6, "isel", [128, 8], F32)
                        m8 = sb(S6, "m8r", [128, 8], F32)
                        dlt = sb(S6, "dlt", [128, 1], F32)
                        w2e = sb(S6, "w2e", [128, 1], F32)
                        wsum = sb(S6, "wsum", [128, 1], F32)
                        wt1 = sb(S6, "wt1", [128, 1], F32)
                        wt2 = sb(S6, "wt2", [128, 1], F32)
                        ce = sb(S6, "ce", [128, 8], F32)
                        ce2 = sb(S6, "ce2", [128, 8], F32)
                        plg = ps(S6, "plg", [128, 36])
                        AXX = mybir.AxisListType.X
                        for i in range(16):
                            ts_ = slice(i * 128, (i + 1) * 128)
                            for k in range(8):
                                P.pe(lambda e: e.matmul(plg[:], lhsT=x1T[:, k, ts_], rhs=wrb[:, k, :], start=(k == 0), stop=(k == 7)), ["mx%d" % i, "wrb"], ["plg"])
                            P.dve(lambda e: e.tensor_tensor(out=lg[:], in0=plg[:], in1=brb[:], op=ALU.add), ["plg", "brb"], ["lg"])
                            P.dve(lambda e: e.tensor_reduce(out=gmx[:], in_=lg[:, 0:4], axis=AXX, op=ALU.max), ["lg"], ["gmx"])
                            P.dve(lambda e: e.tensor_scalar(out=ngmx[:], in0=gmx[:], scalar1=-1.0, scalar2=None, op0=ALU.mult), ["gmx"], ["ngmx"])
                            P.act(lambda e: e.activation(out=gex[:], in_=lg[:, 0:4], func=AF.Exp, bias=ngmx[:, 0:1], scale=1.0), ["lg", "ngmx"], ["gex"])
                            P.dve(lambda e: e.tensor_reduce(out=gsum[:], in_=gex[:], axis=AXX, op=ALU.add), ["gex"], ["gsum"])
                            P.dve(lambda e: e.reciprocal(out=gprob[:], in_=gsum[:]), ["gsum"], ["gprob"])
                            P.dve(lambda e: e.tensor_scalar(out=ohg[:], in0=lg[:, 0:4], scalar1=gmx[:, 0:1], scalar2=None, op0=ALU.is_ge), ["lg", "gmx"], ["ohg"])
                            P.dve(lambda e: e.tensor_tensor(out=tmp48[:], in0=lg[:, 4:36].rearrange("p (g e) -> p g e", g=4),
                                                            in1=ohg[:].unsqueeze(2).broadcast_to([128, 4, 8]), op=ALU.mult), ["lg", "ohg"], ["tmp48"])
                            P.dve(lambda e: e.tensor_reduce(out=isel[:], in_=tmp48[:].rearrange("p g e -> p e g"), axis=AXX, op=ALU.add), ["tmp48"], ["isel"])
                            P.dve(lambda e: e.max(out=m8[:], in_=isel[:]), ["isel"], ["m8r"])
                            P.dve(lambda e: e.tensor_tensor(out=dlt[:], in0=m8[:, 1:2], in1=m8[:, 0:1], op=ALU.subtract), ["m8r"], ["dlt"])
                            P.act(lambda e: e.activation(out=w2e[:], in_=dlt[:], func=AF.Exp), ["dlt"], ["w2e"])
                            P.dve(lambda e: e.tensor_scalar(out=wsum[:], in0=w2e[:], scalar1=1.0, scalar2=None, op0=ALU.add), ["w2e"], ["wsum"])
                            P.dve(lambda e: e.reciprocal(out=wsum[:], in_=wsum[:]), ["wsum"], ["wsum"])
                            P.dve(lambda e: e.tensor_tensor(out=wt1[:], in0=wsum[:], in1=gprob[:], op=ALU.mult), ["wsum", "gprob"], ["wt1"])
                            P.dve(lambda e: e.tensor_tensor(out=wt2[:], in0=wt1[:], in1=w2e[:], op=ALU.mult), ["wt1", "w2e"], ["wt2"])
                            P.dve(lambda e: e.tensor_scalar(out=ce[:], in0=isel[:], scalar1=m8[:, 0:1], scalar2=wt1[:, 0:1], op0=ALU.is_equal, op1=ALU.mult), ["isel", "m8r", "wt1"], ["ce"])
                            P.dve(lambda e: e.tensor_scalar(out=ce2[:], in0=isel[:], scalar1=m8[:, 1:2], scalar2=wt2[:, 0:1], op0=ALU.is_equal, op1=ALU.mult), ["isel", "m8r", "wt2"], ["ce2"])
                            P.dve(lambda e: e.tensor_tensor(out=ce[:], in0=ce[:], in1=ce2[:], op=ALU.add), ["ce", "ce2"], ["ce"])
                            P.dve(lambda e: e.tensor_tensor(out=comb[:, i, :].rearrange("p (g e) -> p g e", g=4), in0=ce[:].unsqueeze(1).broadcast_to([128, 4, 8]),
                                                            in1=ohg[:].unsqueeze(2).broadcast_to([128, 4, 8]), op=ALU.mult), ["ce", "ohg"], ["comb"])
                        dump("comb", comb[:].rearrange("p a e -> p (a e)"), [128, 512], "comb")
                        wstE = [wst[:, 0:2048], wst[:, 2048:4096]]
                        wE = [sb(S6, "wE%d" % i, [128, 12288], BF16) for i in range(2)]
                        sgE = [sb(S6, "sgE%d" % i, [128, 512], F32) for i in range(2)]
                        hT = [sb(S6, "hT%d" % i, [128, 4, 512], BF16) for i in range(2)]
                        pG = [ps(S6, "pG%d" % i, [128, 512]) for i in range(2)]
                        pU = [ps(S6, "pU%d" % i, [128, 512]) for i in range(2)]
                        pO = [ps(S6, "pO%d" % i, [128, 512]) for i in range(3)]
                        wsr = Ring([0, 1])
                        gr_ = Ring([0, 1])
                        or_ = Ring([0, 1, 2])
                        crr = [0]
                        n_exp = DEBUG.get("n_exp", 32)

                        def load_expert(ex):
                            ws = ex % 2
                            for pc in range(6):
                                si = wsr.next()
                                P.dma(wstE[si], wexp[ex, :, pc * 2048:(pc + 1) * 2048], writes=["wstE%d" % si])
                                d = wE[ws][:, pc * 2048:(pc + 1) * 2048]
                                P.pool(lambda e: e.tensor_copy(out=d, in_=wstE[si]), ["wstE%d" % si], ["wE%d" % ws])

                        def gu_phase(ex, cth):
                            ws = ex % 2
                            Wg = wE[ws][:, 0:4096].rearrange("p (k n) -> p k n", k=8)
                            Wu = wE[ws][:, 4096:8192].rearrange("p (k n) -> p k n", k=8)
                            wkey = "wE%d" % ws
                            cs_ = slice(cth * 512, (cth + 1) * 512)
                            xkeys = ["mx%d" % (cth * 4 + t_) for t_ in range(4)]
                            hs_ = cth % 2
                            for ft in range(4):
                                gi = gr_.next()
                                for k in range(8):
                                    P.pe(lambda e: e.matmul(pG[gi][:], lhsT=Wg[:, k, ft * 128:(ft + 1) * 128], rhs=x1T[:, k, cs_], start=(k == 0), stop=(k == 7)),
                                         [wkey] + xkeys, ["pG%d" % gi])
                                for k in range(8):
                                    P.pe(lambda e: e.matmul(pU[gi][:], lhsT=Wu[:, k, ft * 128:(ft + 1) * 128], rhs=x1T[:, k, cs_], start=(k == 0), stop=(k == 7)),
                                         [wkey] + xkeys, ["pU%d" % gi])
                                P.act(lambda e: e.activation(out=sgE[gi][:], in_=pG[gi][:], func=AF.Silu), ["pG%d" % gi], ["sgE%d" % gi])
                                P.dve(lambda e: e.tensor_tensor(out=hT[hs_][:, ft, :], in0=pU[gi][:], in1=sgE[gi][:], op=ALU.mult),
                                      ["pU%d" % gi, "sgE%d" % gi], ["hT%d" % hs_])

                        def d_phase(ex, cth):
                            ws = ex % 2
                            Wd = wE[ws][:, 8192:12288].rearrange("p (k n) -> p k n", k=4)
                            wkey = "wE%d" % ws
                            hs_ = cth % 2
                            for tt in range(4):
                                ti = cth * 4 + tt
                                for hf in range(2):
                                    oi = or_.next()
                                    for ft in range(4):
                                        P.pe(lambda e: e.matmul(pO[oi][:], lhsT=hT[hs_][:, ft, tt * 128:(tt + 1) * 128],
                                                                rhs=Wd[:, ft, hf * 512:(hf + 1) * 512], start=(ft == 0), stop=(ft == 3)),
                                             [wkey, "hT%d" % hs_], ["pO%d" % oi])
                                    a_ = acc[:, ti, hf * 512:(hf + 1) * 512]
                                    P.dve(lambda e: e.scalar_tensor_tensor(out=a_, in0=pO[oi][:], scalar=comb[:, ti, ex:ex + 1], in1=a_, op0=ALU.mult, op1=ALU.add),
                                          ["pO%d" % oi, "comb", "acc%d" % ti], ["acc%d" % ti])

                        units = [(ex, cth) for ex in range(n_exp) for cth in range(4)]
                        load_expert(0)
                        if n_exp > 1:
                            load_expert(1)
                        gu_phase(*units[0])
                        for u, (ex, cth) in enumerate(units):
                            if u + 1 < len(units):
                                gu_phase(*units[u + 1])
                            d_phase(ex, cth)
                            if cth == 3 and ex + 2 < n_exp:
                                load_expert(ex + 2)
                        yo = [sb(S6, "yo%d" % i, [128, 1024], F32) for i in range(2)]
                        for i in range(16):
                            yi = i % 2
                            layer_norm(acc[:, i, :], "acc%d" % i, yo[yi][:], "yo%d" % yi, yo[yi][:], "yo%d" % yi)
                            P.dma(out[i * 128:(i + 1) * 128, :], yo[yi][:], reads=["yo%d" % yi], writes=["out%d" % yi], sem="outd%d" % yi)
        fin_reads = ["out0", "out1"] + ["dbg_" + n for n in dbg_out]
        P.add("sp", lambda e: e.nop(), reads=fin_reads, sem="fin")
        if n_stage < 6:
            pass
        cnt = P.emit(G)
        nsem = len(cnt)
    return nc, dbg_out, nsem


def prep_shared(inp):
    f = lambda a: np.ascontiguousarray(np.asarray(a, dtype=np.float32))
    w_in = f(inp["w_in"])[0]
    sh = {}
    q = w_in[:, 0:512].reshape(1024, 2, 4, 64).transpose(0, 2, 1, 3).reshape(1024, 512)
    w1 = np.concatenate([q, w_in[:, 512:640], w_in[:, 640:768], w_in[:, 768:896], w_in[:, 1024:1152],
                         w_in[:, 896:1024], w_in[:, 1152:1280], w_in[:, 1280:1304]], axis=1)
    sh["w1t"] = tile_w(w1)
    sh["w4t"] = tile_w(w_in[:, 1304:3352])
    sh["wgtt"] = tile_w(w_in[:, 3352:4376])
    sh["wmgt"] = tile_w(w_in[:, 4376:6424])
    lit = {"k": (inp["cmp_k_w1"], inp["cmp_k_b1"], inp["cmp_pos_k"]), "v": (inp["cmp_v_w1"], inp["cmp_v_b1"], inp["cmp_pos_v"])}
    for kv in "kv":
        cw1 = f(lit[kv][0])[0]
        r = cw1.reshape(32, 64, 256).transpose(1, 0, 2).reshape(64, 32 * 256)
        sh["cw1" + kv] = np.ascontiguousarray(np.concatenate([r, r], axis=0))
        pos = f(lit[kv][2])[0]
        sh["cpos" + kv] = np.ascontiguousarray(np.concatenate([pos.T, pos.T], axis=0))
        sh["cb1" + kv] = np.ascontiguousarray(f(lit[kv][1])[0].reshape(2, 128).T)
    w2k = f(inp["cmp_k_w2"])[0]
    sh["cw2k"] = tile_w(np.concatenate([w2k, w2k], axis=1))
    sh["cw2v"] = tile_w(f(inp["cmp_v_w2"])[0])
    b2k = f(inp["cmp_k_b2"])[0]
    sh["cb2k"] = np.ascontiguousarray(np.concatenate([b2k, b2k])[:, None])
    sh["cb2v"] = f(inp["cmp_v_b2"])[0]
    sh["gng8"] = np.ascontiguousarray(f(inp["ret_gn_g"])[0].reshape(8, 128).T)
    sh["gnb8"] = np.ascontiguousarray(f(inp["ret_gn_b"])[0].reshape(8, 128).T)
    sh["wat"] = tile_w(f(inp["w_up_attn"])[0])
    sh["wrt"] = tile_w(f(inp["w_up_ret"])[0])
    sh["wot"] = tile_w(f(inp["w_out"])[0])
    for n in ("ln1_g", "ln1_b", "ln2_g", "ln2_b"):
        sh[n.replace("_", "")] = f(inp[n])[0]
    rg = f(inp["router_group_w"])[0]
    ri = f(inp["router_inner_w"])[0]
    sh["wrout"] = tile_w(np.concatenate([rg, ri.transpose(1, 0, 2).reshape(1024, 32)], axis=1))
    sh["brout"] = np.ascontiguousarray(np.concatenate([f(inp["router_group_b"])[0], f(inp["router_inner_b"])[0].reshape(32)]))
    wg = f(inp["expert_w_gate"])[0]
    wu = f(inp["expert_w_up"])[0]
    wd = f(inp["expert_w_down"])[0]
    we = np.empty((32, 128, 12288), np.float32)
    we[:, :, 0:4096] = wg.reshape(32, 8, 128, 512).transpose(0, 2, 1, 3).reshape(32, 128, 4096)
    we[:, :, 4096:8192] = wu.reshape(32, 8, 128, 512).transpose(0, 2, 1, 3).reshape(32, 128, 4096)
    we[:, :, 8192:12288] = wd.reshape(32, 4, 128, 1024).transpose(0, 2, 1, 3).reshape(32, 128, 4096)
    sh["wexp"] = we
    return sh


def make_in_maps(inp):
    sh = prep_shared(inp)
    x = np.asarray(inp["x"], dtype=np.float32)
    maps = []
    for core in range(8):
        b, c = core // 2, core % 2
        m = dict(sh)
        xb = x[b]
        own = xb.reshape(16, 2, 128, 1024)[:, c].reshape(TO, 1024)
        m["xT"] = np.ascontiguousarray(xb.T)
        m["xTo"] = np.ascontiguousarray(own.T)
        m["xo"] = np.ascontiguousarray(own)
        for k, v in make_consts(c).items():
            if not k.startswith("_"):
                m["c_" + k] = v
        maps.append(m)
    return maps


_PROG_CACHE = {}


def kernel(**inputs):
    if "prog" not in _PROG_CACHE:
        _PROG_CACHE["prog"] = build_program()
    nc, _, _ = _PROG_CACHE["prog"]
    maps = make_in_maps(inputs)
    res = run_bass_kernel_spmd(nc, maps, core_ids=list(range(8)))
    outp = np.empty((4, 16, 2, 128, 1024), np.float32)
    for core in range(8):
        b, c = core // 2, core % 2
        outp[b, :, c] = res.results[core]["out"].reshape(16, 128, 1024)
    return outp.reshape(4, T, 1024)
```

```python
import numpy as np
import ml_dtypes
import concourse.bass as bass
import concourse.mybir as mybir
from concourse.bass_utils import run_bass_kernel_spmd
from contextlib import ExitStack

F32 = mybir.dt.float32
BF16 = mybir.dt.bfloat16
AF = mybir.ActivationFunctionType
ALU = mybir.AluOpType
NPBF = ml_dtypes.bfloat16

T = 4096
D = 1024
TO = 2048
NEGM = -30000.0
LN_EPS = 1e-5
ALPHA = 2.0 ** 0.25
DEBUG = {}


class Op:
    __slots__ = ("eng", "fn", "reads", "writes", "dma", "sem", "deps", "needs_inc", "idx", "id", "extra")

    def __init__(self, eng, fn, reads, writes, dma, sem):
        self.eng = eng
        self.fn = fn
        self.reads = tuple(reads)
        self.writes = tuple(writes)
        self.dma = dma
        self.sem = sem
        self.deps = []
        self.needs_inc = dma
        self.idx = 0
        self.extra = ()


class _Rec:
    def __getattr__(self, name):
        return lambda *a, **k: (name, a, k)


_REC = _Rec()


class Prog:
    ENGS = ("pe", "act", "dve", "pool", "sp")

    def __init__(self, nc, same_eng_sync=True):
        self.nc = nc
        self.ops = []
        self.same_eng_sync = same_eng_sync
        self.last_by_sem = {}
        self.psum_keys = set()

    def add(self, eng, fn, reads=(), writes=(), dma=False, sem=None):
        lim = DEBUG.get("max_ops")
        self.nadd = getattr(self, "nadd", -1) + 1
        if (lim is not None and self.nadd >= lim and sem not in ("dbg", "fin")) or self.nadd in DEBUG.get("skip", ()):
            return Op(eng, None, reads, writes, dma, sem)
        if dma and sem is None:
            sem = "dma_" + str(writes[0])
        if not dma:
            sem = "eng_" + eng
        op = Op(eng, fn(_REC), reads, writes, dma, sem)
        if DEBUG.get("trace_ops"):
            print(len(self.ops), eng, op.fn[0], reads, writes)
        op.id = len(self.ops)
        self.ops.append(op)
        self.last_by_sem[sem] = op
        return op

    def pe(self, fn, reads=(), writes=()):
        return self.add("pe", fn, reads, writes)

    def act(self, fn, reads=(), writes=()):
        return self.add("act", fn, reads, writes)

    def dve(self, fn, reads=(), writes=()):
        return self.add("dve", fn, reads, writes)

    def pool(self, fn, reads=(), writes=()):
        return self.add("pool", fn, reads, writes)

    def dma(self, out, in_, reads=(), writes=(), sem=None, q="sp", **kw):
        return self.add(q, lambda e: e.dma_start(out=out, in_=in_, **kw), reads, writes, dma=True, sem=sem)

    def barrier(self):
        lasts = list(self.last_by_sem.values())
        for eng in self.ENGS:
            op = self.add(eng, lambda e: e.nop())
            op.extra = tuple(lasts)
        self.last_by_sem = {k: v for k, v in self.last_by_sem.items() if k.startswith("eng_")}

    def analyze(self):
        state = {}
        for op in self.ops:
            deps = set(op.extra)
            for k in op.reads:
                st = state.get(k)
                if st:
                    deps.update(st[0])
                    if k in self.psum_keys:
                        deps.update(r for r in st[1] if r.eng != op.eng)
            for k in op.writes:
                st = state.get(k)
                if st is None:
                    st = state[k] = [[], []]
                if st[1]:
                    deps.update(st[1])
                    deps.update(st[0])
                    st[0] = [op]
                    st[1] = []
                else:
                    same_group = op.dma and all(w.dma and w.sem == op.sem for w in st[0])
                    if same_group:
                        st[0].append(op)
                    else:
                        deps.update(st[0])
                        st[0] = [op]
            for k in op.reads:
                st = state.get(k)
                if st is None:
                    st = state[k] = [[], []]
                st[1].append(op)
            deps.discard(op)
            red = {}
            for d in deps:
                if (not d.dma) and (not op.dma) and d.eng == op.eng:
                    if op.eng == "pe" or not self.same_eng_sync:
                        continue
                cur = red.get(d.sem)
                if cur is None or d.id > cur.id:
                    red[d.sem] = d
            op.deps = list(red.values())
            for d in op.deps:
                d.needs_inc = True
        cnt = {}
        for op in self.ops:
            if op.needs_inc:
                cnt[op.sem] = cnt.get(op.sem, 0) + 1
                op.idx = cnt[op.sem]
        self.sem_names = sorted(cnt.keys())
        return cnt

    def emit(self, stack):
        nc = self.nc
        cnt = self.analyze()
        sems = {}
        for name in self.sem_names:
            sems[name] = stack.enter_context(nc.semaphore(name))
        block = stack.enter_context(nc.Block())
        per_eng = {e: [o for o in self.ops if o.eng == e] for e in self.ENGS}

        def run(eng_obj, ops):
            known = {}
            for op in ops:
                for d in op.deps:
                    val = d.idx * (16 if d.dma else 1)
                    if known.get(d.sem, 0) < val:
                        eng_obj.wait_ge(sems[d.sem], val)
                        known[d.sem] = val
                name, a, k = op.fn
                inst = getattr(eng_obj, name)(*a, **k)
                if op.needs_inc:
                    inst.then_inc(sems[op.sem], 16 if op.dma else 1)

        @block.sync
        def _(e):
            run(e, per_eng["sp"])

        @block.tensor
        def _(e):
            run(e, per_eng["pe"])

        @block.scalar
        def _(e):
            run(e, per_eng["act"])

        @block.vector
        def _(e):
            run(e, per_eng["dve"])

        @block.gpsimd
        def _(e):
            run(e, per_eng["pool"])
        return cnt


class Ring:
    def __init__(self, items):
        self.items = items
        self.i = 0

    def next(self):
        it = self.items[self.i % len(self.items)]
        self.i += 1
        return it


def tile_w(w):
    K, N = w.shape
    return np.ascontiguousarray(w.reshape(K // 128, 128, N).transpose(1, 0, 2).reshape(128, -1))


def rope_tabs(pos, d, scale):
    half = d // 2
    inv = 10000.0 ** (-np.arange(half, dtype=np.float64) * 2.0 / d)
    ang = pos.astype(np.float64)[None, :] * inv[:, None]
    cos = np.cos(ang) * scale
    sin = np.sin(ang) * scale
    reps = 128 // half
    return (np.tile(cos, (reps, 1)).astype(np.float32), np.tile(sin, (reps, 1)).astype(np.float32))


def rot_lhsT(d):
    half = d // 2
    Pm = np.zeros((128, 128), np.float32)
    for blk in range(128 // d):
        o = blk * d
        for m in range(half):
            Pm[o + m, o + m + half] = -1.0
            Pm[o + m + half, o + m] = 1.0
    return np.ascontiguousarray(Pm.T)


_CONST_CACHE = {}


def make_consts(c):
    if c in _CONST_CACHE:
        return _CONST_CACHE[c]
    cs = {}
    own_pos = np.concatenate([np.arange(128) + (2 * i + c) * 128 for i in range(16)])
    allpos = np.arange(T)
    cs["cosK"], cs["sinK"] = rope_tabs(allpos, 64, 1.0)
    cs["cosQ"], cs["sinQ"] = rope_tabs(own_pos, 64, 0.125)
    cs["cosRK"], cs["sinRK"] = rope_tabs(allpos, 128, 128.0 ** -0.5)
    cs["cosRQ"], cs["sinRQ"] = rope_tabs(own_pos, 128, 1.0)
    cend = np.arange(256) * 16 + 31
    cs["cosC"], cs["sinC"] = rope_tabs(cend, 64, 1.0)
    cs["pt64"] = rot_lhsT(64).astype(NPBF)
    cs["pt128"] = rot_lhsT(128).astype(NPBF)
    cs["identb"] = np.eye(128, dtype=np.float32).astype(NPBF)
    E = np.zeros((128, 32, 128), np.float32)
    for j in range(32):
        for k in range(128):
            E[2 * j + k // 64, j, k] = 1.0
            E[64 + 2 * j + k // 64, j, k] = 1.0
    cs["eall"] = E.reshape(128, -1).astype(NPBF)
    wm = np.zeros((128, 6, 128), np.float32)
    kk = np.arange(128)[:, None]
    tt = np.arange(128)[None, :]
    for r in range(6):
        dj = (r - 4) - c
        tk = dj * 128 + kk
        ok = (tk <= tt) & (tt - tk < 512)
        wm[:, r, :] = np.where(ok, 0.0, NEGM)
    cs["wmask"] = wm.reshape(128, -1).astype(NPBF)
    cm = np.zeros((128, 2, 16, 128), np.float32)
    for a in range(2):
        for i in range(16):
            G = 2 * i + c
            n = a * 128 + kk
            t = G * 128 + tt
            cm[:, a, i, :] = np.where(16 * n + 31 <= t, 0.0, NEGM)
    cs["cmpmask"] = cm.reshape(128, -1).astype(NPBF)
    cstart = np.arange(255) * 16
    sstart = np.arange(64) * 64
    ov = np.clip(np.minimum(cstart[None, :] + 32, sstart[:, None] + 64) - np.maximum(cstart[None, :], sstart[:, None]), 0, None) / 16.0
    ovT = np.zeros((256, 64), np.float32)
    ovT[:255] = ov.T
    cs["ovT"] = np.ascontiguousarray(ovT.reshape(2, 128, 64).transpose(1, 0, 2).reshape(128, -1)).astype(NPBF)
    tkm = np.zeros((128, 16, 64), np.float32)
    tkb = np.zeros((128, 16, 64), np.float32)
    for i in range(16):
        G = 2 * i + c
        for p in range(128):
            bt = (G * 128 + p) // 64
            for s in range(64):
                if s == 0:
                    tkb[p, i, s] = 1e9
                elif s == bt:
                    tkb[p, i, s] = 2e9
                elif s == bt - 1:
                    tkb[p, i, s] = 3e9
                elif s <= bt:
                    tkm[p, i, s] = 1.0
                else:
                    tkb[p, i, s] = -1e9 - 1e6 * s
    cs["tkm"] = tkm.reshape(128, -1)
    cs["tkb"] = tkb.reshape(128, -1)
    gam = 1.0 - 2.0 ** (-5.0 - np.arange(4, dtype=np.float64))
    lg = np.log(gam)
    m = np.arange(256)[:, None]
    cq = np.arange(128)[None, :]
    qq = 128 * c + cq
    Dc = np.zeros((128, 2, 4, 128), np.float32)
    for h in range(4):
        dd = np.where(qq >= m, np.exp(np.maximum(qq - m, 0) * lg[h]), 0.0)
        Dc[:, :, h, :] = dd.reshape(2, 128, 128).transpose(1, 0, 2)
    cs["Dc"] = Dc.reshape(128, -1)
    xi = np.zeros((128, 4, 128), np.float32)
    for h in range(4):
        xi[:, h, :] = np.exp((qq + 1.0) * lg[h])
    cs["xi"] = xi.reshape(128, -1)
    zt = np.zeros((128, 2, 4), np.float32)
    for h in range(4):
        zt[:, :, h] = np.exp((255.0 - np.arange(256)) * lg[h]).reshape(2, 128).T
    cs["zeta"] = zt.reshape(128, -1)
    cs["_decay256"] = [float(np.exp(256.0 * lg[h])) for h in range(4)]
    _CONST_CACHE[c] = cs
    return cs


CONST_SHAPES = None


def build_program(n_stage=6, debug=()):
    nc = bass.Bass("TRN2", target_bir_lowering=False)
    cs0 = make_consts(0)
    dram = {}

    def din(name, shape, dt=F32):
        dram[name] = nc.dram_tensor(name, list(shape), dt, kind="ExternalInput").ap()
        return dram[name]

    xT = din("xT", [1024, T])
    xTo = din("xTo", [1024, TO])
    xo = din("xo", [TO, 1024])
    w1t = din("w1t", [128, 8 * 1304])
    w4t = din("w4t", [128, 8 * 2048])
    wmgt = din("wmgt", [128, 8 * 2048])
    cw1 = {kv: din("cw1" + kv, [128, 32 * 256]) for kv in "kv"}
    cpos = {kv: din("cpos" + kv, [128, 32]) for kv in "kv"}
    cb1 = {kv: din("cb1" + kv, [128, 2]) for kv in "kv"}
    cw2k = din("cw2k", [128, 2 * 128])
    cw2v = din("cw2v", [128, 2 * 64])
    cb2k = din("cb2k", [128, 1])
    cb2v = din("cb2v", [64])
    gng8 = din("gng8", [128, 8])
    gnb8 = din("gnb8", [128, 8])
    wgtt = din("wgtt", [128, 8 * 1024])
    wat = din("wat", [128, 4 * 1024])
    wrt = din("wrt", [128, 8 * 1024])
    wot = din("wot", [128, 8 * 1024])
    ln1g = din("ln1g", [1024])
    ln1b = din("ln1b", [1024])
    ln2g = din("ln2g", [1024])
    ln2b = din("ln2b", [1024])
    wrout = din("wrout", [128, 8 * 36])
    brout = din("brout", [36])
    wexp = din("wexp", [32, 128, 12288])
    cdr = {}
    for k, v in cs0.items():
        if k.startswith("_"):
            continue
        cdr[k] = din("c_" + k, v.shape, BF16 if v.dtype == NPBF else F32)
    out = nc.dram_tensor("out", [TO, 1024], F32, kind="ExternalOutput").ap()
    dbg_out = {}

    decay256 = cs0["_decay256"]

    with ExitStack() as G:
        P = Prog(nc)

        def sb(stack, name, shape, dt):
            return stack.enter_context(nc.sbuf_tensor(name, list(shape), dt))

        def ps(stack, name, shape, dt=F32):
            P.psum_keys.add(name)
            ncol = 512 if dt == F32 else 1024
            full = stack.enter_context(nc.psum_tensor(name, [128, ncol], dt))
            n = 1
            for d_ in shape[1:]:
                n *= d_
            v = full[0:shape[0], 0:n]
            if len(shape) == 3:
                v = v.rearrange("p (a b) -> p a b", a=shape[1])
            return v

        def dump(name, ap, shape, key):
            if name in debug:
                t = nc.dram_tensor("dbg_" + name, list(shape), ap.dtype, kind="ExternalOutput").ap()
                dbg_out[name] = t
                P.dma(t, ap, reads=[key], writes=["dbg_" + name], sem="dbg")

        identb = sb(G, "identb", [128, 128], BF16)
        P.dma(identb[:], cdr["identb"], writes=["identb"])
        wst = sb(G, "wst", [128, 4096], F32)
        cast_rr = [0]

        def load_cast(dst_ap, src_ap, n, dst_key, shape3=None):
            o = 0
            while o < n:
                m = min(4096, n - o)
                P.dma(wst[:, 0:m], src_ap[:, o:o + m], writes=["wst"])
                d = dst_ap[:, o:o + m]
                if cast_rr[0] % 2 == 0:
                    P.act(lambda e, d=d, m=m: e.copy(out=d, in_=wst[:, 0:m]), ["wst"], [dst_key])
                else:
                    P.dve(lambda e, d=d, m=m: e.tensor_copy(out=d, in_=wst[:, 0:m]), ["wst"], [dst_key])
                cast_rr[0] += 1
                o += m

        x1T = sb(G, "x1T", [128, 8, TO], BF16)
        wst3 = wst[:].rearrange("p (k n) -> p k n", k=8)
        A_ = ExitStack()
        oattnT = sb(A_, "oattnT", [128, 4, TO], BF16)

        with ExitStack() as SN:
            QT = sb(SN, "QT", [128, 16, 4, 128], BF16)
            KE = [sb(SN, "KE%d" % i_, [128, T], BF16) for i_ in range(2)]
            P.dma(KE[0][64:128, :], cdr["eall"][64:128, :], writes=["KE0"])
            P.dma(KE[1][0:64, :], cdr["eall"][0:64, :], writes=["KE1"])
            winkT = sb(SN, "winkT", [128, T], BF16)
            slcv1 = sb(SN, "slcv1", [128, 32, 2, 65], BF16)
            winv1 = sb(SN, "winv1", [128, 32, 2, 65], BF16)
            gates = sb(SN, "gates", [128, 16, 24], F32)
            kcmpT = sb(SN, "kcmpT", [128, 256], BF16)
            vcmp1 = sb(SN, "vcmp1", [128, 2, 2, 65], BF16)
            pt64 = sb(SN, "pt64", [128, 128], BF16)
            P.dma(pt64[:], cdr["pt64"], writes=["pt64"])
            P.dve(lambda e: e.memset(slcv1[:].rearrange("p a g d -> p (a g d)"), 1.0), [], ["slcv1"])
            P.dve(lambda e: e.memset(winv1[:].rearrange("p a g d -> p (a g d)"), 1.0), [], ["winv1"])
            P.dve(lambda e: e.memset(kcmpT[:], 0.0), [], ["kcmpT"])
            P.dve(lambda e: e.memset(vcmp1[:].rearrange("p a g d -> p (a g d)"), 0.0), [], ["vcmp1"])
            P.dve(lambda e: e.memset(vcmp1[:, :, :, 64:65], 1.0), [], ["vcmp1"])

            with ExitStack() as S12:
                cmpT = {"k": sb(S12, "cmpkT", [128, T], BF16), "v": sb(S12, "cmpvT", [128, T], BF16)}
                with ExitStack() as S1:
                    Wn = sb(S1, "Wn", [128, 8, 1304], BF16)
                    load_cast(Wn[:].rearrange("p k n -> p (k n)"), w1t, 8 * 1304, "Wn")
                    xb = [sb(S1, "xb%d" % i, [128, 8, 512], BF16) for i in range(2)]
                    tabs = [sb(S1, "tab%d" % i, [128, 2, 512], F32) for i in range(2)]
                    ybf = [sb(S1, "ybf%d" % i, [128, 512], BF16) for i in range(2)]
                    t1 = [sb(S1, "t1_%d" % i, [128, 512], F32) for i in range(2)]
                    t2 = [sb(S1, "t2_%d" % i, [128, 512], F32) for i in range(2)]
                    pj = [ps(S1, "pj%d" % i, [128, 512]) for i in range(3)]
                    prot = [ps(S1, "prot%d" % i, [128, 512]) for i in range(2)]
                    pv = [ps(S1, "pv%d" % i, [128, 256]) for i in range(2)]
                    pjr = Ring(list(range(3)))
                    rr = Ring(list(range(2)))
                    pvr = Ring(list(range(2)))
                    xTv = xT.rearrange("(k p) t -> p k t", p=128)
                    xTov = xTo.rearrange("(k p) t -> p k t", p=128)

                    def load_x(src_view, c0, n, slot):
                        P.dma(wst3[:, :, 0:n], src_view[:, :, c0:c0 + n], writes=["wst"])
                        P.act(lambda e: e.copy(out=xb[slot][:, 0:4, 0:n], in_=wst3[:, 0:4, 0:n]), ["wst"], ["xb%d" % slot])
                        P.dve(lambda e: e.tensor_copy(out=xb[slot][:, 4:8, 0:n], in_=wst3[:, 4:8, 0:n]), ["wst"], ["xb%d" % slot])

                    def proj_fm(col0, slot, n=512):
                        pi = pjr.next()
                        for k in range(8):
                            P.pe(lambda e, k=k, pi=pi: e.matmul(pj[pi][:, 0:n], lhsT=Wn[:, k, col0:col0 + 128], rhs=xb[slot][:, k, 0:n],
                                                                 start=(k == 0), stop=(k == 7)), ["Wn", "xb%d" % slot], ["pj%d" % pi])
                        return pi

                    def rope_fm(pi, tslot, dst_ap, dst_key, ptm, ptkey, n=512, src=None, srckey=None):
                        r = rr.next()
                        srcap = pj[pi][:, 0:n] if src is None else src
                        sk = ("pj%d" % pi) if srckey is None else srckey
                        P.act(lambda e: e.copy(out=ybf[r][:, 0:n], in_=srcap), [sk], ["ybf%d" % r])
                        P.pe(lambda e: e.matmul(prot[r][:, 0:n], lhsT=ptm[:], rhs=ybf[r][:, 0:n], start=True, stop=True),
                             [ptkey, "ybf%d" % r], ["prot%d" % r])
                        P.dve(lambda e: e.tensor_tensor(out=t1[r][:, 0:n], in0=srcap, in1=tabs[tslot][:, 0, 0:n], op=ALU.mult),
                              [sk, "tab%d" % tslot], ["t1_%d" % r])
                        P.dve(lambda e: e.tensor_tensor(out=t2[r][:, 0:n], in0=prot[r][:, 0:n], in1=tabs[tslot][:, 1, 0:n], op=ALU.mult),
                              ["prot%d" % r, "tab%d" % tslot], ["t2_%d" % r])
                        if isinstance(dst_ap, list):
                            for (d_ap, rows, dkey) in dst_ap:
                                P.pool(lambda e: e.tensor_tensor(out=d_ap, in0=t1[r][rows, 0:n], in1=t2[r][rows, 0:n], op=ALU.add),
                                       ["t1_%d" % r, "t2_%d" % r], [dkey])
                        elif dst_key == "QT":
                            P.pool(lambda e: e.tensor_tensor(out=dst_ap, in0=t1[r][:, 0:n].rearrange("p (a t) -> p a t", a=4),
                                                             in1=t2[r][:, 0:n].rearrange("p (a t) -> p a t", a=4), op=ALU.add),
                                   ["t1_%d" % r, "t2_%d" % r], [dst_key])
                        else:
                            P.pool(lambda e: e.tensor_tensor(out=dst_ap, in0=t1[r][:, 0:n], in1=t2[r][:, 0:n], op=ALU.add),
                                   ["t1_%d" % r, "t2_%d" % r], [dst_key])

                    for ch in range(8):
                        slot = ch % 2
                        c0 = ch * 512
                        load_x(xTv, c0, 512, slot)
                        P.dma(tabs[slot][:, 0, :], cdr["cosK"][:, c0:c0 + 512], writes=["tab%d" % slot])
                        P.dma(tabs[slot][:, 1, :], cdr["sinK"][:, c0:c0 + 512], writes=["tab%d" % slot])
                        for col0, kv in ((512, "k"), (640, "v")):
                            pi = proj_fm(col0, slot)
                            P.act(lambda e, pi=pi, kv=kv: e.copy(out=cmpT[kv][:, c0:c0 + 512], in_=pj[pi][:]), ["pj%d" % pi], ["cmp" + kv + "T"])
                        pi = proj_fm(768, slot)
                        rope_fm(pi, slot, [(KE[0][0:64, c0:c0 + 512], slice(0, 64), "KE0"), (KE[1][64:128, c0:c0 + 512], slice(64, 128), "KE1")], None, pt64, "pt64")
                        pi = proj_fm(896, slot)
                        rope_fm(pi, slot, winkT[:, c0:c0 + 512], "winkT", pt64, "pt64")
                        for tt in range(4):
                            vi = pvr.next()
                            for k in range(8):
                                P.pe(lambda e, k=k, vi=vi, tt=tt: e.matmul(pv[vi][:], lhsT=xb[slot][:, k, tt * 128:(tt + 1) * 128], rhs=Wn[:, k, 1024:1280],
                                                                            start=(k == 0), stop=(k == 7)), ["Wn", "xb%d" % slot], ["pv%d" % vi])
                            tg = ch * 4 + tt
                            P.act(lambda e, vi=vi, tg=tg: e.copy(out=slcv1[:, tg, :, 0:64], in_=pv[vi][:, 0:128].rearrange("p (g d) -> p g d", g=2)),
                                  ["pv%d" % vi], ["slcv1"])
                            P.dve(lambda e, vi=vi, tg=tg: e.tensor_copy(out=winv1[:, tg, :, 0:64], in_=pv[vi][:, 128:256].rearrange("p (g d) -> p g d", g=2)),
                                  ["pv%d" % vi], ["winv1"])
                    for oc in range(4):
                        slot = oc % 2
                        c0 = oc * 512
                        load_x(xTov, c0, 512, slot)
                        P.dma(tabs[slot][:, 0, :], cdr["cosQ"][:, c0:c0 + 512], writes=["tab%d" % slot])
                        P.dma(tabs[slot][:, 1, :], cdr["sinQ"][:, c0:c0 + 512], writes=["tab%d" % slot])
                        for hh in range(4):
                            pi = proj_fm(hh * 128, slot)
                            rope_fm(pi, slot, QT[:, oc * 4:(oc + 1) * 4, hh, :], "QT", pt64, "pt64")
                        for tt in range(4):
                            vi = pvr.next()
                            for k in range(8):
                                P.pe(lambda e, k=k, vi=vi, tt=tt: e.matmul(pv[vi][:, 0:24], lhsT=xb[slot][:, k, tt * 128:(tt + 1) * 128], rhs=Wn[:, k, 1280:1304],
                                                                            start=(k == 0), stop=(k == 7)), ["Wn", "xb%d" % slot], ["pv%d" % vi])
                            tg = oc * 4 + tt
                            P.act(lambda e, vi=vi, tg=tg: e.activation(out=gates[:, tg, :], in_=pv[vi][:, 0:24], func=AF.Sigmoid), ["pv%d" % vi], ["gates"])
                    dump("QT", QT[:].rearrange("p i a t -> p (i a t)"), [128, 4 * TO], "QT")
                    dump("cmpkT", cmpT["k"][:], [128, T], "cmpkT")
                    dump("slcv1", slcv1[:].rearrange("p a g d -> p (a g d)"), [128, 32 * 130], "slcv1")
                    dump("gates", gates[:].rearrange("p a g -> p (a g)"), [128, 16 * 24], "gates")
                P.barrier()
                if n_stage >= 2:
                    with ExitStack() as S2:
                        w1b = sb(S2, "w1b", [128, 32, 256], BF16)
                        posT = sb(S2, "posT", [128, 32], F32)
                        posTb = sb(S2, "posTb", [128, 32], BF16)
                        b1 = sb(S2, "b1", [128, 2], F32)
                        bias1 = sb(S2, "bias1", [128, 2], F32)
                        w2kf = sb(S2, "w2kf", [128, 2, 128], F32)
                        w2k = sb(S2, "w2k", [128, 2, 128], BF16)
                        w2vf = sb(S2, "w2vf", [128, 2, 64], F32)
                        w2v = sb(S2, "w2v", [128, 2, 64], BF16)
                        b2k = sb(S2, "b2k", [128, 1], F32)
                        b2v = sb(S2, "b2v", [128, 64], F32)
                        tabC = sb(S2, "tabC", [128, 2, 256], F32)
                        h1 = sb(S2, "h1", [128, 2, 256], BF16)
                        xg = sb(S2, "xg", [128, 256], F32)
                        ug = sb(S2, "ug", [128, 256], F32)
                        sg_ = sb(S2, "sg_", [128, 256], F32)
                        yk = sb(S2, "yk", [128, 256], F32)
                        ykb = sb(S2, "ykb", [128, 256], BF16)
                        tk1 = sb(S2, "tk1", [128, 256], F32)
                        tk2 = sb(S2, "tk2", [128, 256], F32)
                        ph = [ps(S2, "ph%d" % i, [128, 256]) for i in range(2)]
                        pcv = ps(S2, "pcv", [128, 2])
                        pkc = ps(S2, "pkc", [128, 256])
                        prk = ps(S2, "prk", [128, 256])
                        pvc = ps(S2, "pvc", [128, 64])
                        P.dma(w2kf[:].rearrange("p a n -> p (a n)"), cw2k, writes=["w2kf"])
                        P.dve(lambda e: e.tensor_copy(out=w2k[:], in_=w2kf[:]), ["w2kf"], ["w2k"])
                        P.dma(w2vf[:].rearrange("p a n -> p (a n)"), cw2v, writes=["w2vf"])
                        P.dve(lambda e: e.tensor_copy(out=w2v[:], in_=w2vf[:]), ["w2vf"], ["w2v"])
                        P.dma(b2k[:], cb2k, writes=["b2k"])
                        P.dma(b2v[:], cb2v.partition_broadcast(128), writes=["b2v"])
                        P.dma(tabC[:, 0, :], cdr["cosC"], writes=["tabC"])
                        P.dma(tabC[:, 1, :], cdr["sinC"], writes=["tabC"])
                        for kv in "kv":
                            load_cast(w1b[:].rearrange("p l n -> p (l n)"), cw1[kv], 32 * 256, "w1b")
                            P.dma(posT[:], cpos[kv], writes=["posT"])
                            P.dve(lambda e: e.tensor_copy(out=posTb[:], in_=posT[:]), ["posT"], ["posTb"])
                            P.dma(b1[:], cb1[kv], writes=["b1"])
                            for ht in range(2):
                                for l in range(32):
                                    P.pe(lambda e, ht=ht, l=l: e.matmul(pcv[:, ht:ht + 1], lhsT=w1b[0:64, l, ht * 128:(ht + 1) * 128], rhs=posTb[0:64, l:l + 1],
                                                                         start=(l == 0), stop=(l == 31)), ["w1b", "posTb"], ["pcv"])
                            P.dve(lambda e: e.tensor_tensor(out=bias1[:], in0=pcv[:], in1=b1[:], op=ALU.add), ["pcv", "b1"], ["bias1"])
                            for g in range(2):
                                gp = slice(g * 64, (g + 1) * 64)
                                for ht in range(2):
                                    for l in range(32):
                                        P.pe(lambda e, ht=ht, l=l, gp=gp, kv=kv: e.matmul(ph[ht][:, 0:255], lhsT=w1b[gp, l, ht * 128:(ht + 1) * 128],
                                                                                        rhs=cmpT[kv][gp, l:l + 16 * 254 + 1:16],
                                                                                        start=(l == 0), stop=(l == 31)), ["w1b", "cmp" + kv + "T"], ["ph%d" % ht])
                                    P.act(lambda e, ht=ht: e.activation(out=xg[:, 0:255], in_=ph[ht][:, 0:255], func=AF.Identity, bias=bias1[:, ht:ht + 1], scale=1.0),
                                          ["ph%d" % ht, "bias1"], ["xg"])
                                    P.dve(lambda e: e.tensor_tensor(out=ug[:, 0:255], in0=xg[:, 0:255], in1=xg[:, 0:255], op=ALU.mult), ["xg"], ["ug"])
                                    P.dve(lambda e: e.tensor_scalar(out=ug[:, 0:255], in0=ug[:, 0:255], scalar1=0.044715, scalar2=1.0, op0=ALU.mult, op1=ALU.add), ["ug"], ["ug"])
                                    P.dve(lambda e: e.tensor_tensor(out=ug[:, 0:255], in0=ug[:, 0:255], in1=xg[:, 0:255], op=ALU.mult), ["ug", "xg"], ["ug"])
                                    P.act(lambda e: e.activation(out=sg_[:, 0:255], in_=ug[:, 0:255], func=AF.Sigmoid, scale=1.5957691216057308), ["ug"], ["sg_"])
                                    P.dve(lambda e, ht=ht: e.tensor_tensor(out=h1[:, ht, 0:255], in0=xg[:, 0:255], in1=sg_[:, 0:255], op=ALU.mult), ["xg", "sg_"], ["h1"])
                                if kv == "k":
                                    for ht in range(2):
                                        P.pe(lambda e, ht=ht: e.matmul(pkc[:, 0:255], lhsT=w2k[:, ht, :], rhs=h1[:, ht, 0:255], start=(ht == 0), stop=(ht == 1)),
                                             ["w2k", "h1"], ["pkc"])
                                    P.act(lambda e: e.activation(out=yk[:, 0:255], in_=pkc[:, 0:255], func=AF.Identity, bias=b2k[:, 0:1], scale=1.0), ["pkc", "b2k"], ["yk"])
                                    P.act(lambda e: e.copy(out=ykb[:, 0:255], in_=yk[:, 0:255]), ["yk"], ["ykb"])
                                    P.pe(lambda e: e.matmul(prk[:, 0:255], lhsT=pt64[:], rhs=ykb[:, 0:255], start=True, stop=True), ["pt64", "ykb"], ["prk"])
                                    P.dve(lambda e: e.tensor_tensor(out=tk1[:, 0:255], in0=yk[:, 0:255], in1=tabC[:, 0, 0:255], op=ALU.mult), ["yk", "tabC"], ["tk1"])
                                    P.dve(lambda e: e.tensor_tensor(out=tk2[:, 0:255], in0=prk[:, 0:255], in1=tabC[:, 1, 0:255], op=ALU.mult), ["prk", "tabC"], ["tk2"])
                                    P.dve(lambda e, gp=gp: e.tensor_tensor(out=kcmpT[gp, 0:255], in0=tk1[gp, 0:255], in1=tk2[gp, 0:255], op=ALU.add), ["tk1", "tk2"], ["kcmpT"])
                                else:
                                    for a in range(2):
                                        cntn = 128 if a == 0 else 127
                                        for ht in range(2):
                                            P.pe(lambda e, ht=ht, a=a, cntn=cntn: e.matmul(pvc[0:cntn, :], lhsT=h1[:, ht, a * 128:a * 128 + cntn], rhs=w2v[:, ht, :],
                                                                                            start=(ht == 0), stop=(ht == 1)), ["w2v", "h1"], ["pvc"])
                                        P.dve(lambda e, a=a, cntn=cntn, g=g: e.tensor_tensor(out=vcmp1[0:cntn, a, g, 0:64], in0=pvc[0:cntn, :], in1=b2v[0:cntn, :], op=ALU.add),
                                              ["pvc", "b2v"], ["vcmp1"])
                        dump("kcmpT", kcmpT[:], [128, 256], "kcmpT")
                        dump("vcmp1", vcmp1[:].rearrange("p a g d -> p (a g d)"), [128, 260], "vcmp1")
                    P.barrier()
            P.barrier()
            if n_stage >= 3:
                with ExitStack() as S3:
                    def bc4(ap):
                        return ap.unsqueeze(1).broadcast_to([ap.shape[0], 4, ap.shape[1]])

                    wmask = sb(S3, "wmask", [128, 6, 128], BF16)
                    cmpmask = sb(S3, "cmpmask", [128, 2, 16, 128], BF16)
                    ovT = sb(S3, "ovT", [128, 2, 64], BF16)
                    tkm = sb(S3, "tkm", [128, 16, 64], F32)
                    tkb = sb(S3, "tkb", [128, 16, 64], F32)
                    wmask4 = sb(S3, "wmask4", [128, 6, 512], BF16)
                    cm4 = [sb(S3, "cm4_%d" % i_, [128, 2, 512], BF16) for i_ in range(2)]
                    QN = [sb(S3, "QN%d" % i_, [128, 512], BF16) for i_ in range(4)]
                    P.dma(wmask[:].rearrange("p a k -> p (a k)"), cdr["wmask"], writes=["wmask"])
                    P.dma(cmpmask[:].rearrange("p a i k -> p (a i k)"), cdr["cmpmask"], writes=["cmpmask"])
                    P.dma(ovT[:].rearrange("p a k -> p (a k)"), cdr["ovT"], writes=["ovT"])
                    P.dma(tkm[:].rearrange("p a k -> p (a k)"), cdr["tkm"], writes=["tkm"])
                    P.dma(tkb[:].rearrange("p a k -> p (a k)"), cdr["tkb"], writes=["tkb"])
                    for r_ in range(6):
                        P.pool(lambda e: e.tensor_copy(out=wmask4[:, r_, :].rearrange("p (a t) -> p a t", a=4), in_=bc4(wmask[:, r_, :])), ["wmask"], ["wmask4"])
                    eT = [sb(S3, "eT%d" % i, [128, 512], BF16) for i in range(4)]
                    oacc = sb(S3, "oacc", [128, 512], F32)
                    oab = sb(S3, "oab", [128, 512], BF16)
                    rz = sb(S3, "rz", [128, 4], F32)
                    coef = sb(S3, "coef", [128, 4], F32)
                    imp = sb(S3, "imp", [128, 64], F32)
                    score = sb(S3, "score", [128, 64], F32)
                    work = sb(S3, "work", [128, 64], F32)
                    m8 = sb(S3, "m8", [128, 16], F32)
                    nmk = [sb(S3, "nmk%d" % i_, [128, 2, 64], BF16) for i_ in range(2)]
                    pST = [ps(S3, "pST%d" % i, [128, 512]) for i in range(3)]
                    pA = ps(S3, "pA", [128, 4, 65])
                    pB = ps(S3, "pB", [128, 4, 64])
                    pS = ps(S3, "pS", [128, 4, 65])
                    pW = ps(S3, "pW", [128, 4, 65])
                    pTr = ps(S3, "pTr", [128, 128], BF16)
                    str_ = Ring([0, 1, 2])
                    etr = Ring([0, 1, 2, 3])

                    def scores(kT_ap, kkey, g, i, masks, q_ap=None, qkey="QT"):
                        gp = slice(g * 64, (g + 1) * 64)
                        si = str_.next()
                        ei = etr.next()
                        nm = len(masks)
                        if q_ap is None:
                            q_ap = QT[gp, i, :, :].rearrange("p a t -> p (a t)")
                        P.pe(lambda e: e.matmul(pST[si][:], lhsT=kT_ap, rhs=q_ap, start=True, stop=(nm == 0)), [kkey, qkey], ["pST%d" % si])
                        for mi, (ml, mr, mkeys) in enumerate(masks):
                            P.pe(lambda e: e.matmul(pST[si][:], lhsT=ml, rhs=mr, start=False, stop=(mi == nm - 1)), mkeys, ["pST%d" % si])
                        P.act(lambda e: e.activation(out=eT[ei][:], in_=pST[si][:], func=AF.Exp), ["pST%d" % si], ["eT%d" % ei])
                        return ei

                    def finish_branch(pacc, pkey, i, g, br, first):
                        P.dve(lambda e: e.tensor_scalar(out=rz[:], in0=pacc[:, :, 64], scalar1=1e-30, scalar2=None, op0=ALU.max), [pkey], ["rz"])
                        P.dve(lambda e: e.reciprocal(out=rz[:], in_=rz[:]), ["rz"], ["rz"])
                        P.dve(lambda e: e.tensor_tensor(out=coef[:], in0=rz[:], in1=gates[:, i, g * 12 + br:g * 12 + 12:3], op=ALU.mult), ["rz", "gates"], ["coef"])
                        for hh in range(4):
                            o = oacc[:, g * 256 + hh * 64:g * 256 + (hh + 1) * 64]
                            if first:
                                P.dve(lambda e, hh=hh, o=o: e.tensor_scalar(out=o, in0=pacc[:, hh, 0:64], scalar1=coef[:, hh:hh + 1], scalar2=None, op0=ALU.mult),
                                      [pkey, "coef"], ["oacc"])
                            else:
                                P.dve(lambda e, hh=hh, o=o: e.scalar_tensor_tensor(out=o, in0=pacc[:, hh, 0:64], scalar=coef[:, hh:hh + 1], in1=o, op0=ALU.mult, op1=ALU.add),
                                      [pkey, "coef", "oacc"], ["oacc"])

                    tasks = []

                    def mk_cmp(i, g, a, na):
                        gp = slice(g * 64, (g + 1) * 64)

                        def sc():
                            if g == 0:
                                P.pool(lambda e: e.tensor_copy(out=cm4[i % 2][:, a, :].rearrange("p (h t) -> p h t", h=4), in_=bc4(cmpmask[:, a, i, :])),
                                       ["cmpmask"], ["cm4_%d" % (i % 2)])
                            return scores(kcmpT[gp, a * 128:(a + 1) * 128], "kcmpT", g, i,
                                          [(identb[:], cm4[i % 2][:, a, :], ["identb", "cm4_%d" % (i % 2)])])

                        def pvf(ei):
                            for hh in range(4):
                                P.pe(lambda e: e.matmul(pA[:, hh, :], lhsT=eT[ei][:, hh * 128:(hh + 1) * 128], rhs=vcmp1[:, a, g, :],
                                                        start=(a == 0 and hh == 0), stop=(a == na - 1 and hh == 3)), ["eT%d" % ei, "vcmp1"], ["pA"])
                                P.pe(lambda e: e.matmul(pB[:, hh, :], lhsT=eT[ei][:, hh * 128:(hh + 1) * 128], rhs=ovT[:, a, :],
                                                        start=(a == 0 and hh == 0), stop=(a == na - 1 and hh == 3)), ["eT%d" % ei, "ovT"], ["pB"])

                        def post():
                            finish_branch(pA, "pA", i, g, 0, True)
                            P.dve(lambda e: e.tensor_scalar(out=imp[:], in0=pB[:, 0, :], scalar1=rz[:, 0:1], scalar2=None, op0=ALU.mult), ["pB", "rz"], ["imp"])
                            for hh in range(1, 4):
                                P.dve(lambda e: e.scalar_tensor_tensor(out=imp[:], in0=pB[:, hh, :], scalar=rz[:, hh:hh + 1], in1=imp[:], op0=ALU.mult, op1=ALU.add),
                                      ["pB", "rz", "imp"], ["imp"])
                            P.dve(lambda e: e.tensor_tensor(out=score[:], in0=imp[:], in1=tkm[:, i, :], op=ALU.mult), ["imp", "tkm"], ["score"])
                            P.dve(lambda e: e.tensor_tensor(out=score[:], in0=score[:], in1=tkb[:, i, :], op=ALU.add), ["score", "tkb"], ["score"])
                            P.dve(lambda e: e.max(out=m8[:, 0:8], in_=score[:]), ["score"], ["m8"])
                            P.dve(lambda e: e.match_replace(out=work[:], in_to_replace=m8[:, 0:8], in_values=score[:], imm_value=-3.0e38), ["score", "m8"], ["work"])
                            P.dve(lambda e: e.max(out=m8[:, 8:16], in_=work[:]), ["work"], ["m8"])
                            P.dve(lambda e: e.tensor_scalar(out=nmk[g][:], in0=score[:].unsqueeze(1).broadcast_to([128, 2, 64]), scalar1=m8[:, 15:16], scalar2=NEGM,
                                                            op0=ALU.is_lt, op1=ALU.mult), ["score", "m8"], ["nmk%d" % g])
                            if ("imp%d_%d" % (i, g)) in debug:
                                dump("imp%d_%d" % (i, g), imp[:], [128, 64], "imp")
                                dump("score%d_%d" % (i, g), score[:], [128, 64], "score")
                                dump("m8%d_%d" % (i, g), m8[:], [128, 16], "m8")
                        return [None, sc, pvf, post if a == na - 1 else None]

                    def mk_win(i, g, idx, r, j, nw):
                        gp = slice(g * 64, (g + 1) * 64)

                        def sc():
                            return scores(winkT[gp, j * 128:(j + 1) * 128], "winkT", g, i,
                                          [(identb[:], wmask4[:, r, :], ["identb", "wmask4"])])

                        def pvf(ei):
                            for hh in range(4):
                                P.pe(lambda e: e.matmul(pW[:, hh, :], lhsT=eT[ei][:, hh * 128:(hh + 1) * 128], rhs=winv1[:, j, g, :],
                                                        start=(idx == 0 and hh == 0), stop=(idx == nw - 1 and hh == 3)), ["eT%d" % ei, "winv1"], ["pW"])

                        def post():
                            finish_branch(pW, "pW", i, g, 2, False)
                        return [None, sc, pvf, post if idx == nw - 1 else None]

                    def tile_end_pe(i):
                        for ct in range(4):
                            P.pe(lambda e: e.transpose(out=pTr[:], in_=oab[:, ct * 128:(ct + 1) * 128], identity=identb[:]), ["oab", "identb"], ["pTr"])
                            P.dve(lambda e: e.tensor_copy(out=oattnT[:, ct, i * 128:(i + 1) * 128], in_=pTr[:]), ["pTr"], ["oattnT"])

                    def mk_slc(i, g, j, nj):
                        gp = slice(g * 64, (g + 1) * 64)

                        qn_i = (2 * i + g) % 4
                        oh = slice((1 - g) * 64, (2 - g) * 64)

                        def pre():
                            P.pool(lambda e: e.tensor_copy(out=QN[qn_i][gp, :], in_=QT[gp, i, :, :].rearrange("p a t -> p (a t)")), ["QT"], ["QN%d" % qn_i])
                            P.pe(lambda e: e.transpose(out=pTr[:], in_=nmk[g][:].rearrange("p a s -> p (a s)"), identity=identb[:]), ["nmk%d" % g, "identb"], ["pTr"])
                            P.act(lambda e: e.copy(out=QN[qn_i][oh, :].rearrange("p (a t) -> p a t", a=4), in_=bc4(pTr[oh, :])), ["pTr"], ["QN%d" % qn_i])
                            if g == 0 and i > 0:
                                tile_end_pe(i - 1)

                        def sc():
                            masks = []
                            if j >= 2 * i:
                                masks.append((identb[:], wmask4[:, 4 + (j - 2 * i), :], ["identb", "wmask4"]))
                            return scores(KE[g][:, j * 128:(j + 1) * 128], "KE%d" % g, g, i, masks, q_ap=QN[qn_i][:], qkey="QN%d" % qn_i)

                        def pvf(ei):
                            for hh in range(4):
                                P.pe(lambda e: e.matmul(pS[:, hh, :], lhsT=eT[ei][:, hh * 128:(hh + 1) * 128], rhs=slcv1[:, j, g, :],
                                                        start=(j == 0 and hh == 0), stop=(j == nj - 1 and hh == 3)), ["eT%d" % ei, "slcv1"], ["pS"])

                        def post():
                            finish_branch(pS, "pS", i, g, 1, False)
                            if g == 1:
                                if ("oacc%d" % i) in debug:
                                    dump("oacc%d" % i, oacc[:], [128, 512], "oacc")
                                P.pool(lambda e: e.tensor_copy(out=oab[:], in_=oacc[:]), ["oacc"], ["oab"])
                        return [pre if j == 0 else None, sc, pvf, post if j == nj - 1 else None]

                    for i in range(16):
                        for g in range(2):
                            na = 1 if i < 8 else 2
                            for a in range(na):
                                tasks.append(mk_cmp(i, g, a, na))
                            js = [(r, 2 * i - 4 + r) for r in range(6) if 2 * i - 4 + r >= 0]
                            for idx, (r, j) in enumerate(js):
                                tasks.append(mk_win(i, g, idx, r, j, len(js)))
                            nj = 2 * i + 2
                            for j in range(nj):
                                tasks.append(mk_slc(i, g, j, nj))
                    nt = len(tasks)
                    eis = [None] * nt

                    def emit_score(k):
                        if tasks[k][0] is not None:
                            tasks[k][0]()
                        eis[k] = tasks[k][1]()

                    emit_score(0)
                    emit_score(1)
                    for k in range(nt):
                        if k + 2 < nt:
                            emit_score(k + 2)
                        tasks[k][2](eis[k])
                        if tasks[k][3] is not None:
                            tasks[k][3]()
                    tile_end_pe(15)
                    dump("oattnT", oattnT[:].rearrange("p a t -> p (a t)"), [128, 4 * TO], "oattnT")
                P.barrier()
        P.barrier()

        B_ = ExitStack()
        oretT = sb(B_, "oretT", [128, 8, TO], BF16)
        if n_stage >= 4:
            with ExitStack() as S4:
                W4 = sb(S4, "W4", [128, 8, 2048], BF16)
                load_cast(W4[:].rearrange("p k n -> p (k n)"), w4t, 8 * 2048, "W4")
                pt128 = sb(S4, "pt128", [128, 128], BF16)
                P.dma(pt128[:], cdr["pt128"], writes=["pt128"])
                Dc = sb(S4, "Dc", [128, 2, 4, 128], F32)
                xi = sb(S4, "xi", [128, 4, 128], F32)
                zeta = sb(S4, "zeta", [128, 2, 4], F32)
                P.dma(Dc[:].rearrange("p a h c -> p (a h c)"), cdr["Dc"], writes=["Dc"])
                P.dma(xi[:].rearrange("p h c -> p (h c)"), cdr["xi"], writes=["xi"])
                P.dma(zeta[:].rearrange("p a h -> p (a h)"), cdr["zeta"], writes=["zeta"])
                xst = wst3
                xb = sb(S4, "xb4", [128, 8, 512], BF16)
                xob = sb(S4, "xob4", [128, 8, 256], BF16)
                tabs = sb(S4, "tab4", [128, 2, 512], F32)
                tabq = sb(S4, "tabq4", [128, 2, 256], F32)
                ybf2 = [sb(S4, "ybf4_%d" % i_, [128, 512], BF16) for i_ in range(2)]
                t12 = [sb(S4, "t1_4_%d" % i_, [128, 512], F32) for i_ in range(2)]
                t22 = [sb(S4, "t2_4_%d" % i_, [128, 512], F32) for i_ in range(2)]
                rr4 = Ring([0, 1])
                kT = sb(S4, "kT4", [128, 4, 512], BF16)
                qT = sb(S4, "qT4", [128, 4, 256], BF16)
                qxT = sb(S4, "qxT4", [128, 4, 256], BF16)
                vtok = sb(S4, "vtok", [128, 4, 1024], BF16)
                kz = sb(S4, "kz", [128, 4, 4, 128], BF16)
                R = sb(S4, "R", [128, 4, 256], F32)
                Rb = sb(S4, "Rb", [128, 4, 256], BF16)
                sc = [sb(S4, "sc%d" % i_, [128, 2, 128], BF16) for i_ in range(2)]
                epsT = sb(S4, "epsT", [128, 1], F32)
                P.dve(lambda e: e.memset(epsT[:], LN_EPS), [], ["epsT"])
                pending4 = []
                st6 = sb(S4, "st6", [128, 6], F32)
                mv = sb(S4, "mv", [128, 2], F32)
                rstd = sb(S4, "rstd", [128, 1], F32)
                oretb = [sb(S4, "oretb%d" % i_, [128, 1024], BF16) for i_ in range(2)]
                pj = [ps(S4, "pj4_%d" % i, [128, 512]) for i in range(3)]
                psc = [ps(S4, "psc%d" % i_, [128, 2, 128]) for i_ in range(2)]
                po = [ps(S4, "po%d" % i_, [128, 256]) for i_ in range(2)]
                pTrw = ps(S4, "pTr4", [128, 512], BF16)
                pTr = pTrw[:, 0:128]
                pjr = Ring([0, 1, 2])
                P.dve(lambda e: e.memset(R[:].rearrange("p h e -> p (h e)"), 0.0), [], ["R%d" % h_ for h_ in range(4)])
                P.dve(lambda e: e.memset(Rb[:].rearrange("p h e -> p (h e)"), 0.0), [], ["Rb%d" % h_ for h_ in range(4)])
                xTv = xT.rearrange("(k p) t -> p k t", p=128)
                xTov = xTo.rearrange("(k p) t -> p k t", p=128)

                def rope4(pi, n, tab, tabkey, dst_ap, dst_key):
                    ri = pjr.next()
                    prot = pj[ri]
                    rb = rr4.next()
                    ybf, t1, t2 = ybf2[rb], t12[rb], t22[rb]
                    P.act(lambda e: e.copy(out=ybf[:, 0:n], in_=pj[pi][:, 0:n]), ["pj4_%d" % pi], ["ybf4_%d" % rb])
                    P.pe(lambda e: e.matmul(prot[:, 0:n], lhsT=pt128[:], rhs=ybf[:, 0:n], start=True, stop=True), ["pt128", "ybf4_%d" % rb], ["pj4_%d" % ri])
                    P.dve(lambda e: e.tensor_tensor(out=t1[:, 0:n], in0=pj[pi][:, 0:n], in1=tab[:, 0, 0:n], op=ALU.mult), ["pj4_%d" % pi, tabkey], ["t1_4_%d" % rb])
                    P.dve(lambda e: e.tensor_tensor(out=t2[:, 0:n], in0=prot[:, 0:n], in1=tab[:, 1, 0:n], op=ALU.mult), ["pj4_%d" % ri, tabkey], ["t2_4_%d" % rb])
                    P.pool(lambda e: e.tensor_tensor(out=dst_ap, in0=t1[:, 0:n], in1=t2[:, 0:n], op=ALU.add), ["t1_4_%d" % rb, "t2_4_%d" % rb], [dst_key])

                for gch in range(8):
                    c0 = gch * 512
                    o0 = gch * 256
                    P.dma(xst[:], xTv[:, :, c0:c0 + 512], writes=["wst"])
                    P.act(lambda e: e.copy(out=xb[:, 0:4, :], in_=xst[:, 0:4, :]), ["wst"], ["xb4"])
                    P.dve(lambda e: e.tensor_copy(out=xb[:, 4:8, :], in_=xst[:, 4:8, :]), ["wst"], ["xb4"])
                    P.dma(xst[:, :, 0:256], xTov[:, :, o0:o0 + 256], writes=["wst"])
                    P.act(lambda e: e.copy(out=xob[:, 0:4, :], in_=xst[:, 0:4, 0:256]), ["wst"], ["xob4"])
                    P.dve(lambda e: e.tensor_copy(out=xob[:, 4:8, :], in_=xst[:, 4:8, 0:256]), ["wst"], ["xob4"])
                    P.dma(tabs[:, 0, :], cdr["cosRK"][:, c0:c0 + 512], writes=["tab4"])
                    P.dma(tabs[:, 1, :], cdr["sinRK"][:, c0:c0 + 512], writes=["tab4"])
                    P.dma(tabq[:, 0, :], cdr["cosRQ"][:, o0:o0 + 256], writes=["tabq4"])
                    P.dma(tabq[:, 1, :], cdr["sinRQ"][:, o0:o0 + 256], writes=["tabq4"])
                    for h in range(4):
                        pi = pjr.next()
                        for k in range(8):
                            P.pe(lambda e, k=k, pi=pi, h=h: e.matmul(pj[pi][:], lhsT=W4[:, k, 512 + h * 128:512 + (h + 1) * 128], rhs=xb[:, k, :],
                                                                      start=(k == 0), stop=(k == 7)), ["W4", "xb4"], ["pj4_%d" % pi])
                        rope4(pi, 512, tabs, "tab4", kT[:, h, :], "kT4")
                    for h in range(4):
                        pi = pjr.next()
                        for k in range(8):
                            P.pe(lambda e, k=k, pi=pi, h=h: e.matmul(pj[pi][:, 0:256], lhsT=W4[:, k, h * 128:(h + 1) * 128], rhs=xob[:, k, :],
                                                                      start=(k == 0), stop=(k == 7)), ["W4", "xob4"], ["pj4_%d" % pi])
                        rope4(pi, 256, tabq, "tabq4", qT[:, h, :], "qT4")
                    for pp in range(2):
                        P.dve(lambda e, pp=pp: e.tensor_tensor(out=qxT[:, :, pp * 128:(pp + 1) * 128], in0=qT[:, :, pp * 128:(pp + 1) * 128], in1=xi[:], op=ALU.mult),
                              ["qT4", "xi"], ["qxT4"])
                    for tt in range(4):
                        for hf in range(2):
                            pi = pjr.next()
                            for k in range(8):
                                P.pe(lambda e, k=k, pi=pi, tt=tt, hf=hf: e.matmul(pj[pi][:], lhsT=xb[:, k, tt * 128:(tt + 1) * 128],
                                                                                   rhs=W4[:, k, 1024 + hf * 512:1024 + (hf + 1) * 512],
                                                                                   start=(k == 0), stop=(k == 7)), ["W4", "xb4"], ["pj4_%d" % pi])
                            P.act(lambda e, pi=pi, tt=tt, hf=hf: e.copy(out=vtok[:, tt, hf * 512:(hf + 1) * 512], in_=pj[pi][:]), ["pj4_%d" % pi], ["vtok"])
                    for tt in range(4):
                        for h in range(4):
                            P.pe(lambda e: e.transpose(out=pTrw[:, h * 128:(h + 1) * 128], in_=kT[:, h, tt * 128:(tt + 1) * 128], identity=identb[:]), ["kT4", "identb"], ["pTr4"])
                        P.dve(lambda e: e.tensor_tensor(out=kz[:, tt, :, :], in0=pTrw[:].rearrange("p (h d) -> p h d", h=4),
                                                        in1=zeta[:, tt % 2, :].unsqueeze(2).broadcast_to([128, 4, 128]), op=ALU.mult), ["pTr4", "zeta"], ["kz"])
                    units = [(pp, h) for pp in range(2) for h in range(4)]

                    def phaseA(pp, h, ub):
                        qs = slice(pp * 128, (pp + 1) * 128)
                        for mt in range(2):
                            tt = pp * 2 + mt
                            P.pe(lambda e: e.matmul(psc[ub][:, mt, :], lhsT=kT[:, h, tt * 128:(tt + 1) * 128], rhs=qT[:, h, qs], start=True, stop=True),
                                 ["kT4", "qT4"], ["psc%d" % ub])
                        P.dve(lambda e: e.tensor_tensor(out=sc[ub][:], in0=psc[ub][:], in1=Dc[:, :, h, :], op=ALU.mult), ["psc%d" % ub, "Dc"], ["sc%d" % ub])

                    def phaseBC(pp, h, ub):
                        i = gch * 2 + pp
                        qs = slice(pp * 128, (pp + 1) * 128)
                        hs = slice(h * 256, (h + 1) * 256)
                        ob = oretb[pp]
                        for mt in range(2):
                            tt = pp * 2 + mt
                            P.pe(lambda e: e.matmul(po[ub][:], lhsT=sc[ub][:, mt, :], rhs=vtok[:, tt, hs], start=(mt == 0), stop=False), ["sc%d" % ub, "vtok"], ["po%d" % ub])
                        P.pe(lambda e: e.matmul(po[ub][:], lhsT=qxT[:, h, qs], rhs=Rb[:, h, :], start=False, stop=True), ["qxT4", "Rb%d" % h], ["po%d" % ub])
                        ri = pjr.next()
                        for mt in range(2):
                            tt = pp * 2 + mt
                            P.pe(lambda e: e.matmul(pj[ri][:, 0:256], lhsT=kz[:, tt, h, :], rhs=vtok[:, tt, hs], start=(mt == 0), stop=(mt == 1)),
                                 ["kz", "vtok"], ["pj4_%d" % ri])
                        P.dve(lambda e: e.bn_stats(out=st6[:], in_=po[ub][:]), ["po%d" % ub], ["st6"])
                        P.dve(lambda e: e.bn_aggr(out=mv[:], in_=st6[:]), ["st6"], ["mv"])
                        P.act(lambda e: e.activation(out=rstd[:], in_=mv[:, 1:2], func=AF.Sqrt, bias=epsT[:, 0:1], scale=1.0), ["mv", "epsT"], ["rstd"])
                        P.dve(lambda e: e.reciprocal(out=rstd[:], in_=rstd[:]), ["rstd"], ["rstd"])
                        P.dve(lambda e: e.tensor_scalar(out=ob[:, hs], in0=po[ub][:], scalar1=mv[:, 0:1], scalar2=rstd[:, 0:1], op0=ALU.subtract, op1=ALU.mult),
                              ["po%d" % ub, "mv", "rstd"], ["oretb%d" % pp])
                        P.dve(lambda e: e.scalar_tensor_tensor(out=R[:, h, :], in0=R[:, h, :], scalar=decay256[h], in1=pj[ri][:, 0:256], op0=ALU.mult, op1=ALU.add),
                              ["R%d" % h, "pj4_%d" % ri], ["R%d" % h])
                        P.act(lambda e: e.copy(out=Rb[:, h, :], in_=R[:, h, :]), ["R%d" % h], ["Rb%d" % h])

                    def pair_end(pp, i):
                        ob = oretb[pp]
                        for et in range(8):
                            P.pe(lambda e: e.transpose(out=pTr[:], in_=ob[:, et * 128:(et + 1) * 128], identity=identb[:]), ["oretb%d" % pp, "identb"], ["pTr4"])
                            P.act(lambda e: e.copy(out=oretT[:, et, i * 128:(i + 1) * 128], in_=pTr[:]), ["pTr4"], ["oretT"])

                    phaseA(units[0][0], units[0][1], 0)
                    for u, (pp, h) in enumerate(units):
                        if u + 1 < len(units):
                            phaseA(units[u + 1][0], units[u + 1][1], (u + 1) % 2)
                        phaseBC(pp, h, u % 2)
                        if pending4:
                            pending4.pop(0)()
                        if h == 3:
                            pending4.append(lambda pp=pp, i=gch * 2 + pp: pair_end(pp, i))
                while pending4:
                    pending4.pop(0)()
                dump("oretT", oretT[:].rearrange("p a t -> p (a t)"), [128, 8 * TO], "oretT")
            P.barrier()

        if n_stage >= 5:
            with ExitStack() as S5a:
                Wmg = sb(S5a, "Wmg", [128, 8, 2048], BF16)
                Wa = sb(S5a, "Wa", [128, 4, 1024], BF16)
                Wr = sb(S5a, "Wr", [128, 8, 1024], BF16)
                Wgt = sb(S5a, "Wgt", [128, 8, 1024], BF16)
                load_cast(Wmg[:].rearrange("p k n -> p (k n)"), wmgt, 8 * 2048, "Wmg")
                load_cast(Wa[:].rearrange("p k n -> p (k n)"), wat, 4 * 1024, "Wa")
                load_cast(Wr[:].rearrange("p k n -> p (k n)"), wrt, 8 * 1024, "Wr")
                load_cast(Wgt[:].rearrange("p k n -> p (k n)"), wgtt, 8 * 1024, "Wgt")
                gg8 = sb(S5a, "gg8", [128, 8], F32)
                gb8 = sb(S5a, "gb8", [128, 8], F32)
                P.dma(gg8[:], gng8, writes=["gg8"])
                P.dma(gb8[:], gnb8, writes=["gb8"])
                xb = sb(S5a, "xb5", [128, 8, 512], BF16)
                og = sb(S5a, "og", [128, 8, 512], BF16)
                sgt = [sb(S5a, "sgt%d" % i, [128, 512], F32) for i in range(2)]
                yn = [sb(S5a, "yn%d" % i, [128, 512], F32) for i in range(2)]
                ga2 = [sb(S5a, "ga%d" % i, [128, 512], F32) for i in range(2)]
                gr2 = [sb(S5a, "gr%d" % i, [128, 512], F32) for i in range(2)]
                ma2 = [sb(S5a, "ma%d" % i, [128, 512], F32) for i in range(2)]
                bk = [ps(S5a, "bk%d" % i, [128, 512]) for i in range(8)]
                pgt = [bk[4], bk[5]]
                xTov = xTo.rearrange("(k p) t -> p k t", p=128)
                for oc in range(4):
                    c0 = oc * 512
                    cs_ = slice(c0, c0 + 512)
                    P.dma(wst3[:], xTov[:, :, cs_], writes=["wst"])
                    P.act(lambda e: e.copy(out=xb[:, 0:4, :], in_=wst3[:, 0:4, :]), ["wst"], ["xb5"])
                    P.dve(lambda e: e.tensor_copy(out=xb[:, 4:8, :], in_=wst3[:, 4:8, :]), ["wst"], ["xb5"])
                    for et in range(8):
                        b_ = et % 2
                        for k in range(8):
                            P.pe(lambda e: e.matmul(pgt[b_][:], lhsT=Wgt[:, k, et * 128:(et + 1) * 128], rhs=xb[:, k, :], start=(k == 0), stop=(k == 7)),
                                 ["Wgt", "xb5"], ["bk%d" % (4 + b_)])
                        P.act(lambda e: e.activation(out=sgt[b_][:], in_=pgt[b_][:], func=AF.Silu), ["bk%d" % (4 + b_)], ["sgt%d" % b_])
                        P.act(lambda e: e.activation(out=yn[b_][:], in_=oretT[:, et, cs_], func=AF.Identity, scale=gg8[:, et:et + 1], bias=gb8[:, et:et + 1]),
                              ["oretT", "gg8", "gb8"], ["yn%d" % b_])
                        P.dve(lambda e: e.tensor_tensor(out=og[:, et, :], in0=yn[b_][:], in1=sgt[b_][:], op=ALU.mult), ["yn%d" % b_, "sgt%d" % b_], ["og"])
                    for ct in range(8):
                        cb = (ct % 2) * 4
                        cp = ct % 2
                        pg0, pg1, pu0, pu1 = bk[cb], bk[cb + 1], bk[cb + 2], bk[cb + 3]
                        kg0, kg1, ku0, ku1 = ["bk%d" % (cb + q_) for q_ in range(4)]
                        ga, gr, ma = ga2[cp], gr2[cp], ma2[cp]
                        for k in range(8):
                            P.pe(lambda e: e.matmul(pg0[:], lhsT=Wmg[:, k, ct * 128:(ct + 1) * 128], rhs=xb[:, k, :], start=(k == 0), stop=(k == 7)),
                                 ["Wmg", "xb5"], [kg0])
                        for k in range(8):
                            P.pe(lambda e: e.matmul(pg1[:], lhsT=Wmg[:, k, 1024 + ct * 128:1024 + (ct + 1) * 128], rhs=xb[:, k, :], start=(k == 0), stop=(k == 7)),
                                 ["Wmg", "xb5"], [kg1])
                        for k in range(4):
                            P.pe(lambda e: e.matmul(pu0[:], lhsT=Wa[:, k, ct * 128:(ct + 1) * 128], rhs=oattnT[:, k, cs_], start=(k == 0), stop=(k == 3)),
                                 ["Wa", "oattnT"], [ku0])
                        for k in range(8):
                            P.pe(lambda e: e.matmul(pu1[:], lhsT=Wr[:, k, ct * 128:(ct + 1) * 128], rhs=og[:, k, :], start=(k == 0), stop=(k == 7)),
                                 ["Wr", "og"], [ku1])
                        P.act(lambda e: e.activation(out=ga[:], in_=pg0[:], func=AF.Sigmoid), [kg0], ["ga%d" % cp])
                        P.act(lambda e: e.activation(out=gr[:], in_=pg1[:], func=AF.Sigmoid), [kg1], ["gr%d" % cp])
                        P.dve(lambda e: e.tensor_tensor(out=ma[:], in0=pu0[:], in1=ga[:], op=ALU.mult), [ku0, "ga%d" % cp], ["ma%d" % cp])
                        P.dve(lambda e: e.tensor_tensor(out=gr[:], in0=pu1[:], in1=gr[:], op=ALU.mult), [ku1, "gr%d" % cp], ["gr%d" % cp])
                        P.pool(lambda e: e.tensor_tensor(out=x1T[:, ct, cs_], in0=ma[:], in1=gr[:], op=ALU.add), ["ma%d" % cp, "gr%d" % cp], ["mx%d" % (oc * 4 + t_) for t_ in range(4)])
                dump("mergedT", x1T[:].rearrange("p a t -> p (a t)"), [128, 8 * TO], "mx0")
            P.barrier()
        B_.close()
        A_.close()
        if n_stage >= 5:
            with ExitStack() as S56:
                acc = sb(S56, "acc", [128, 16, 1024], F32)
                lng = sb(S56, "lng", [128, 1024], F32)
                lnb = sb(S56, "lnb", [128, 1024], F32)
                st12 = sb(S56, "st12", [128, 2, 6], F32)
                mv = sb(S56, "mv5", [128, 2], F32)
                rstd = sb(S56, "rstd5", [128, 1], F32)

                def layer_norm(src_ap, src_key, dst_ap, dst_key, tmp_ap, tmp_key):
                    for hf in range(2):
                        P.dve(lambda e: e.bn_stats(out=st12[:, hf, :], in_=src_ap[:, hf * 512:(hf + 1) * 512]), [src_key], ["st12"])
                    P.dve(lambda e: e.bn_aggr(out=mv[:], in_=st12[:].rearrange("p a s -> p (a s)")), ["st12"], ["mv5"])
                    P.dve(lambda e: e.tensor_scalar(out=rstd[:], in0=mv[:, 1:2], scalar1=LN_EPS, scalar2=None, op0=ALU.add), ["mv5"], ["rstd5"])
                    P.act(lambda e: e.activation(out=rstd[:], in_=rstd[:], func=AF.Sqrt), ["rstd5"], ["rstd5"])
                    P.dve(lambda e: e.reciprocal(out=rstd[:], in_=rstd[:]), ["rstd5"], ["rstd5"])
                    P.dve(lambda e: e.tensor_scalar(out=tmp_ap, in0=src_ap, scalar1=mv[:, 0:1], scalar2=rstd[:, 0:1], op0=ALU.subtract, op1=ALU.mult),
                          [src_key, "mv5", "rstd5"], [tmp_key])
                    P.pool(lambda e: e.tensor_tensor(out=tmp_ap, in0=tmp_ap, in1=lng[:], op=ALU.mult), [tmp_key, "lng"], [tmp_key])
                    P.pool(lambda e: e.tensor_tensor(out=dst_ap, in0=tmp_ap, in1=lnb[:], op=ALU.add), [tmp_key, "lnb"], [dst_key])

                with ExitStack() as S5b:
                    Wo = sb(S5b, "Wo", [128, 8, 1024], BF16)
                    load_cast(Wo[:].rearrange("p k n -> p (k n)"), wot, 8 * 1024, "Wo")
                    P.dma(lng[:], ln1g.partition_broadcast(128), writes=["lng"])
                    P.dma(lnb[:], ln1b.partition_broadcast(128), writes=["lnb"])
                    xres2 = [sb(S5b, "xres%d" % i_, [128, 1024], F32) for i_ in range(2)]
                    yt2 = [sb(S5b, "yt%d" % i_, [128, 1024], F32) for i_ in range(2)]
                    x12 = [sb(S5b, "x1_%d" % i_, [128, 1024], F32) for i_ in range(2)]
                    x1b2 = [sb(S5b, "x1b%d" % i_, [128, 1024], BF16) for i_ in range(2)]
                    pm2 = [[ps(S5b, "pm%d_%d" % (q_, i_), [128, 512]) for i_ in range(2)] for q_ in range(2)]
                    pTr = ps(S5b, "pTr5", [128, 128], BF16)
                    pend5 = []
                    for i in range(16):
                        q_ = i % 2
                        xres, yt, x1, x1b, pm = xres2[q_], yt2[q_], x12[q_], x1b2[q_], pm2[q_]
                        ts_ = slice(i * 128, (i + 1) * 128)
                        P.dma(xres[:], xo[ts_, :], writes=["xres%d" % q_])
                        for hf in range(2):
                            for k in range(8):
                                P.pe(lambda e: e.matmul(pm[hf][:], lhsT=x1T[:, k, ts_], rhs=Wo[:, k, hf * 512:(hf + 1) * 512], start=(k == 0), stop=(k == 7)),
                                     ["mx%d" % i, "Wo"], ["pm%d_%d" % (q_, hf)])
                            P.dve(lambda e: e.scalar_tensor_tensor(out=yt[:, hf * 512:(hf + 1) * 512], in0=xres[:, hf * 512:(hf + 1) * 512], scalar=ALPHA,
                                                                   in1=pm[hf][:], op0=ALU.mult, op1=ALU.add), ["xres%d" % q_, "pm%d_%d" % (q_, hf)], ["yt%d" % q_])
                        layer_norm(yt[:], "yt%d" % q_, x1[:], "x1_%d" % q_, yt[:], "yt%d" % q_)
                        if ("x1_%d" % i) in debug:
                            dump("x1_%d" % i, x1[:], [128, 1024], "x1_%d" % q_)
                        P.dve(lambda e: e.tensor_scalar(out=acc[:, i, :], in0=x1[:], scalar1=ALPHA, scalar2=None, op0=ALU.mult), ["x1_%d" % q_], ["acc%d" % i])
                        P.act(lambda e: e.copy(out=x1b[:], in_=x1[:]), ["x1_%d" % q_], ["x1b%d" % q_])
                        if pend5:
                            pend5.pop(0)()

                        def tr5(i=i, q_=q_, x1b=x1b, ts_=ts_):
                            for dt_ in range(8):
                                P.pe(lambda e: e.transpose(out=pTr[:], in_=x1b[:, dt_ * 128:(dt_ + 1) * 128], identity=identb[:]), ["x1b%d" % q_, "identb"], ["pTr5"])
                                P.act(lambda e: e.copy(out=x1T[:, dt_, ts_], in_=pTr[:]), ["pTr5"], ["mx%d" % i])
                        pend5.append(tr5)
                    while pend5:
                        pend5.pop(0)()
                P.barrier()
                if n_stage >= 6:
                    with ExitStack() as S6:
                        P.dma(lng[:], ln2g.partition_broadcast(128), writes=["lng"])
                        P.dma(lnb[:], ln2b.partition_broadcast(128), writes=["lnb"])
                        comb = sb(S6, "comb", [128, 16, 32], F32)
                        wrf = sb(S6, "wrf", [128, 8, 36], F32)
                        wrb = sb(S6, "wrb", [128, 8, 36], BF16)
                        brb = sb(S6, "brb", [128, 36], F32)
                        P.dma(wrf[:].rearrange("p k n -> p (k n)"), wrout, writes=["wrf"])
                        P.dve(lambda e: e.tensor_copy(out=wrb[:], in_=wrf[:]), ["wrf"], ["wrb"])
                        P.dma(brb[:], brout.partition_broadcast(128), writes=["brb"])
                        lg = sb(S6, "lg", [128, 36], F32)
                        gmx = sb(S6, "gmx", [128, 1], F32)
                        ngmx = sb(S6, "ngmx", [128, 1], F32)
                        gex = sb(S6, "gex", [128, 4], F32)
                        gsum = sb(S6, "gsum", [128, 1], F32)
                        gprob = sb(S6, "gprob", [128, 1], F32)
                        ohg = sb(S6, "ohg", [128, 4], F32)
                        tmp48 = sb(S6, "tmp48", [128, 4, 8], F32)
                        isel = sb(S6, "isel", [128, 8], F32)
                        m8 = sb(S6, "m8r", [128, 8], F32)
                        dlt = sb(S6, "dlt", [128, 1], F32)
                        w2e = sb(S6, "w2e", [128, 1], F32)
                        wsum = sb(S6, "wsum", [128, 1], F32)
                        wt1 = sb(S6, "wt1", [128, 1], F32)
                        wt2 = sb(S6, "wt2", [128, 1], F32)
                        ce = sb(S6, "ce", [128, 8], F32)
                        ce2 = sb(S6, "ce2", [128, 8], F32)
                        plg = ps(S6, "plg", [128, 36])
                        AXX = mybir.AxisListType.X
                        for i in range(16):
                            ts_ = slice(i * 128, (i + 1) * 128)
                            for k in range(8):
                                P.pe(lambda e: e.matmul(plg[:], lhsT=x1T[:, k, ts_], rhs=wrb[:, k, :], start=(k == 0), stop=(k == 7)), ["mx%d" % i, "wrb"], ["plg"])
                            P.dve(lambda e: e.tensor_tensor(out=lg[:], in0=plg[:], in1=brb[:], op=ALU.add), ["plg", "brb"], ["lg"])
                            P.dve(lambda e: e.tensor_reduce(out=gmx[:], in_=lg[:, 0:4], axis=AXX, op=ALU.max), ["lg"], ["gmx"])
                            P.dve(lambda e: e.tensor_scalar(out=ngmx[:], in0=gmx[:], scalar1=-1.0, scalar2=None, op0=ALU.mult), ["gmx"], ["ngmx"])
                            P.act(lambda e: e.activation(out=gex[:], in_=lg[:, 0:4], func=AF.Exp, bias=ngmx[:, 0:1], scale=1.0), ["lg", "ngmx"], ["gex"])
                            P.dve(lambda e: e.tensor_reduce(out=gsum[:], in_=gex[:], axis=AXX, op=ALU.add), ["gex"], ["gsum"])
                            P.dve(lambda e: e.reciprocal(out=gprob[:], in_=gsum[:]), ["gsum"], ["gprob"])
                            P.dve(lambda e: e.tensor_scalar(out=ohg[:], in0=lg[:, 0:4], scalar1=gmx[:, 0:1], scalar2=None, op0=ALU.is_ge), ["lg", "gmx"], ["ohg"])
                            P.dve(lambda e: e.tensor_tensor(out=tmp48[:], in0=lg[:, 4:36].rearrange("p (g e) -> p g e", g=4),
                                                            in1=ohg[:].unsqueeze(2).broadcast_to([128, 4, 8]), op=ALU.mult), ["lg", "ohg"], ["tmp48"])
                            P.dve(lambda e: e.tensor_reduce(out=isel[:], in_=tmp48[:].rearrange("p g e -> p e g"), axis=AXX, op=ALU.add), ["tmp48"], ["isel"])
                            P.dve(lambda e: e.max(out=m8[:], in_=isel[:]), ["isel"], ["m8r"])
                            P.dve(lambda e: e.tensor_tensor(out=dlt[:], in0=m8[:, 1:2], in1=m8[:, 0:1], op=ALU.subtract), ["m8r"], ["dlt"])
                            P.act(lambda e: e.activation(out=w2e[:], in_=dlt[:], func=AF.Exp), ["dlt"], ["w2e"])
                            P.dve(lambda e: e.tensor_scalar(out=wsum[:], in0=w2e[:], scalar1=1.0, scalar2=None, op0=ALU.add), ["w2e"], ["wsum"])
                            P.dve(lambda e: e.reciprocal(out=wsum[:], in_=wsum[:]), ["wsum"], ["wsum"])
                            P.dve(lambda e: e.tensor_tensor(out=wt1[:], in0=wsum[:], in1=gprob[:], op=ALU.mult), ["wsum", "gprob"], ["wt1"])
                            P.dve(lambda e: e.tensor_tensor(out=wt2[:], in0=wt1[:], in1=w2e[:], op=ALU.mult), ["wt1", "w2e"], ["wt2"])
                            P.dve(lambda e: e.tensor_scalar(out=ce[:], in0=isel[:], scalar1=m8[:, 0:1], scalar2=wt1[:, 0:1], op0=ALU.is_equal, op1=ALU.mult), ["isel", "m8r", "wt1"], ["ce"])
                            P.dve(lambda e: e.tensor_scalar(out=ce2[:], in0=isel[:], scalar1=m8[:, 1:2], scalar2=wt2[:, 0:1], op0=ALU.is_equal, op1=ALU.mult), ["isel", "m8r", "wt2"], ["ce2"])
                            P.dve(lambda e: e.tensor_tensor(out=ce[:], in0=ce[:], in1=ce2[:], op=ALU.add), ["ce", "ce2"], ["ce"])
                            P.dve(lambda e: e.tensor_tensor(out=comb[:, i, :].rearrange("p (g e) -> p g e", g=4), in0=ce[:].unsqueeze(1).broadcast_to([128, 4, 8]),
                                                            in1=ohg[:].unsqueeze(2).broadcast_to([128, 4, 8]), op=ALU.mult), ["ce", "ohg"], ["comb"])
                        dump("comb", comb[:].rearrange("p a e -> p (a e)"), [128, 512], "comb")
                        wstE = [wst[:, 0:2048], wst[:, 2048:4096]]
                        wE = [sb(S6, "wE%d" % i, [128, 12288], BF16) for i in range(2)]
                        sgE = [sb(S6, "sgE%d" % i, [128, 512], F32) for i in range(2)]
                        hT = [sb(S6, "hT%d" % i, [128, 4, 512], BF16) for i in range(2)]
                        pG = [ps(S6, "pG%d" % i, [128, 512]) for i in range(2)]
                        pU = [ps(S6, "pU%d" % i, [128, 512]) for i in range(2)]
                        pO = [ps(S6, "pO%d" % i, [128, 512]) for i in range(3)]
                        wsr = Ring([0, 1])
                        gr_ = Ring([0, 1])
                        or_ = Ring([0, 1, 2])
                        crr = [0]
                        n_exp = DEBUG.get("n_exp", 32)

                        def load_expert(ex):
                            ws = ex % 2
                            for pc in range(6):
                                si = wsr.next()
                                P.dma(wstE[si], wexp[ex, :, pc * 2048:(pc + 1) * 2048], writes=["wstE%d" % si])
                                d = wE[ws][:, pc * 2048:(pc + 1) * 2048]
                                P.pool(lambda e: e.tensor_copy(out=d, in_=wstE[si]), ["wstE%d" % si], ["wE%d" % ws])

                        def gu_phase(ex, cth):
                            ws = ex % 2
                            Wg = wE[ws][:, 0:4096].rearrange("p (k n) -> p k n", k=8)
                            Wu = wE[ws][:, 4096:8192].rearrange("p (k n) -> p k n", k=8)
                            wkey = "wE%d" % ws
                            cs_ = slice(cth * 512, (cth + 1) * 512)
                            xkeys = ["mx%d" % (cth * 4 + t_) for t_ in range(4)]
                            hs_ = cth % 2
                            for ft in range(4):
                                gi = gr_.next()
                                for k in range(8):
                                    P.pe(lambda e: e.matmul(pG[gi][:], lhsT=Wg[:, k, ft * 128:(ft + 1) * 128], rhs=x1T[:, k, cs_], start=(k == 0), stop=(k == 7)),
                                         [wkey] + xkeys, ["pG%d" % gi])
                                for k in range(8):
                                    P.pe(lambda e: e.matmul(pU[gi][:], lhsT=Wu[:, k, ft * 128:(ft + 1) * 128], rhs=x1T[:, k, cs_], start=(k == 0), stop=(k == 7)),
                                         [wkey] + xkeys, ["pU%d" % gi])
                                P.act(lambda e: e.activation(out=sgE[gi][:], in_=pG[gi][:], func=AF.Silu), ["pG%d" % gi], ["sgE%d" % gi])
                                P.dve(lambda e: e.tensor_tensor(out=hT[hs_][:, ft, :], in0=pU[gi][:], in1=sgE[gi][:], op=ALU.mult),
                                      ["pU%d" % gi, "sgE%d" % gi], ["hT%d" % hs_])

                        def d_phase(ex, cth):
                            ws = ex % 2
                            Wd = wE[ws][:, 8192:12288].rearrange("p (k n) -> p k n", k=4)
                            wkey = "wE%d" % ws
                            hs_ = cth % 2
                            for tt in range(4):
                                ti = cth * 4 + tt
                                for hf in range(2):
                                    oi = or_.next()
                                    for ft in range(4):
                                        P.pe(lambda e: e.matmul(pO[oi][:], lhsT=hT[hs_][:, ft, tt * 128:(tt + 1) * 128],
                                                                rhs=Wd[:, ft, hf * 512:(hf + 1) * 512], start=(ft == 0), stop=(ft == 3)),
                                             [wkey, "hT%d" % hs_], ["pO%d" % oi])
                                    a_ = acc[:, ti, hf * 512:(hf + 1) * 512]
                                    P.dve(lambda e: e.scalar_tensor_tensor(out=a_, in0=pO[oi][:], scalar=comb[:, ti, ex:ex + 1], in1=a_, op0=ALU.mult, op1=ALU.add),
                                          ["pO%d" % oi, "comb", "acc%d" % ti], ["acc%d" % ti])

                        units = [(ex, cth) for ex in range(n_exp) for cth in range(4)]
                        load_expert(0)
                        if n_exp > 1:
                            load_expert(1)
                        gu_phase(*units[0])
                        for u, (ex, cth) in enumerate(units):
                            if u + 1 < len(units):
                                gu_phase(*units[u + 1])
                            d_phase(ex, cth)
                            if cth == 3 and ex + 2 < n_exp:
                                load_expert(ex + 2)
                        yo = [sb(S6, "yo%d" % i, [128, 1024], F32) for i in range(2)]
                        for i in range(16):
                            yi = i % 2
                            layer_norm(acc[:, i, :], "acc%d" % i, yo[yi][:], "yo%d" % yi, yo[yi][:], "yo%d" % yi)
                            P.dma(out[i * 128:(i + 1) * 128, :], yo[yi][:], reads=["yo%d" % yi], writes=["out%d" % yi], sem="outd%d" % yi)
        fin_reads = ["out0", "out1"] + ["dbg_" + n for n in dbg_out]
        P.add("sp", lambda e: e.nop(), reads=fin_reads, sem="fin")
        if n_stage < 6:
            pass
        cnt = P.emit(G)
        nsem = len(cnt)
    return nc, dbg_out, nsem


def prep_shared(inp):
    f = lambda a: np.ascontiguousarray(np.asarray(a, dtype=np.float32))
    w_in = f(inp["w_in"])[0]
    sh = {}
    q = w_in[:, 0:512].reshape(1024, 2, 4, 64).transpose(0, 2, 1, 3).reshape(1024, 512)
    w1 = np.concatenate([q, w_in[:, 512:640], w_in[:, 640:768], w_in[:, 768:896], w_in[:, 1024:1152],
                         w_in[:, 896:1024], w_in[:, 1152:1280], w_in[:, 1280:1304]], axis=1)
    sh["w1t"] = tile_w(w1)
    sh["w4t"] = tile_w(w_in[:, 1304:3352])
    sh["wgtt"] = tile_w(w_in[:, 3352:4376])
    sh["wmgt"] = tile_w(w_in[:, 4376:6424])
    lit = {"k": (inp["cmp_k_w1"], inp["cmp_k_b1"], inp["cmp_pos_k"]), "v": (inp["cmp_v_w1"], inp["cmp_v_b1"], inp["cmp_pos_v"])}
    for kv in "kv":
        cw1 = f(lit[kv][0])[0]
        r = cw1.reshape(32, 64, 256).transpose(1, 0, 2).reshape(64, 32 * 256)
        sh["cw1" + kv] = np.ascontiguousarray(np.concatenate([r, r], axis=0))
        pos = f(lit[kv][2])[0]
        sh["cpos" + kv] = np.ascontiguousarray(np.concatenate([pos.T, pos.T], axis=0))
        sh["cb1" + kv] = np.ascontiguousarray(f(lit[kv][1])[0].reshape(2, 128).T)
    w2k = f(inp["cmp_k_w2"])[0]
    sh["cw2k"] = tile_w(np.concatenate([w2k, w2k], axis=1))
    sh["cw2v"] = tile_w(f(inp["cmp_v_w2"])[0])
    b2k = f(inp["cmp_k_b2"])[0]
    sh["cb2k"] = np.ascontiguousarray(np.concatenate([b2k, b2k])[:, None])
    sh["cb2v"] = f(inp["cmp_v_b2"])[0]
    sh["gng8"] = np.ascontiguousarray(f(inp["ret_gn_g"])[0].reshape(8, 128).T)
    sh["gnb8"] = np.ascontiguousarray(f(inp["ret_gn_b"])[0].reshape(8, 128).T)
    sh["wat"] = tile_w(f(inp["w_up_attn"])[0])
    sh["wrt"] = tile_w(f(inp["w_up_ret"])[0])
    sh["wot"] = tile_w(f(inp["w_out"])[0])
    for n in ("ln1_g", "ln1_b", "ln2_g", "ln2_b"):
        sh[n.replace("_", "")] = f(inp[n])[0]
    rg = f(inp["router_group_w"])[0]
    ri = f(inp["router_inner_w"])[0]
    sh["wrout"] = tile_w(np.concatenate([rg, ri.transpose(1, 0, 2).reshape(1024, 32)], axis=1))
    sh["brout"] = np.ascontiguousarray(np.concatenate([f(inp["router_group_b"])[0], f(inp["router_inner_b"])[0].reshape(32)]))
    wg = f(inp["expert_w_gate"])[0]
    wu = f(inp["expert_w_up"])[0]
    wd = f(inp["expert_w_down"])[0]
    we = np.empty((32, 128, 12288), np.float32)
    we[:, :, 0:4096] = wg.reshape(32, 8, 128, 512).transpose(0, 2, 1, 3).reshape(32, 128, 4096)
    we[:, :, 4096:8192] = wu.reshape(32, 8, 128, 512).transpose(0, 2, 1, 3).reshape(32, 128, 4096)
    we[:, :, 8192:12288] = wd.reshape(32, 4, 128, 1024).transpose(0, 2, 1, 3).reshape(32, 128, 4096)
    sh["wexp"] = we
    return sh


def make_in_maps(inp):
    sh = prep_shared(inp)
    x = np.asarray(inp["x"], dtype=np.float32)
    maps = []
    for core in range(8):
        b, c = core // 2, core % 2
        m = dict(sh)
        xb = x[b]
        own = xb.reshape(16, 2, 128, 1024)[:, c].reshape(TO, 1024)
        m["xT"] = np.ascontiguousarray(xb.T)
        m["xTo"] = np.ascontiguousarray(own.T)
        m["xo"] = np.ascontiguousarray(own)
        for k, v in make_consts(c).items():
            if not k.startswith("_"):
                m["c_" + k] = v
        maps.append(m)
    return maps


_PROG_CACHE = {}


def kernel(**inputs):
    if "prog" not in _PROG_CACHE:
        _PROG_CACHE["prog"] = build_program()
    nc, _, _ = _PROG_CACHE["prog"]
    maps = make_in_maps(inputs)
    res = run_bass_kernel_spmd(nc, maps, core_ids=list(range(8)))
    outp = np.empty((4, 16, 2, 128, 1024), np.float32)
    for core in range(8):
        b, c = core // 2, core % 2
        outp[b, :, c] = res.results[core]["out"].reshape(16, 128, 1024)
    return outp.reshape(4, T, 1024)
```

```python
import numpy as np
import ml_dtypes
import concourse.bass as bass
import concourse.mybir as mybir
from concourse.bass_utils import run_bass_kernel_spmd
from contextlib import ExitStack

F32 = mybir.dt.float32
BF16 = mybir.dt.bfloat16
AF = mybir.ActivationFunctionType
ALU = mybir.AluOpType
NPBF = ml_dtypes.bfloat16

T = 4096
D = 1024
TO = 2048
NEGM = -30000.0
LN_EPS = 1e-5
ALPHA = 2.0 ** 0.25
DEBUG = {}


class Op:
    __slots__ = ("eng", "fn", "reads", "writes", "dma", "sem", "deps", "needs_inc", "idx", "id", "extra")

    def __init__(self, eng, fn, reads, writes, dma, sem):
        self.eng = eng
        self.fn = fn
        self.reads = tuple(reads)
        self.writes = tuple(writes)
        self.dma = dma
        self.sem = sem
        self.deps = []
        self.needs_inc = dma
        self.idx = 0
        self.extra = ()


class _Rec:
    def __getattr__(self, name):
        return lambda *a, **k: (name, a, k)


_REC = _Rec()


class Prog:
    ENGS = ("pe", "act", "dve", "pool", "sp")

    def __init__(self, nc, same_eng_sync=True):
        self.nc = nc
        self.ops = []
        self.same_eng_sync = same_eng_sync
        self.last_by_sem = {}
        self.psum_keys = set()

    def add(self, eng, fn, reads=(), writes=(), dma=False, sem=None):
        lim = DEBUG.get("max_ops")
        self.nadd = getattr(self, "nadd", -1) + 1
        if (lim is not None and self.nadd >= lim and sem not in ("dbg", "fin")) or self.nadd in DEBUG.get("skip", ()):
            return Op(eng, None, reads, writes, dma, sem)
        if dma and sem is None:
            sem = "dma_" + str(writes[0])
        if not dma:
            sem = "eng_" + eng
        op = Op(eng, fn(_REC), reads, writes, dma, sem)
        if DEBUG.get("trace_ops"):
            print(len(self.ops), eng, op.fn[0], reads, writes)
        op.id = len(self.ops)
        self.ops.append(op)
        self.last_by_sem[sem] = op
        return op

    def pe(self, fn, reads=(), writes=()):
        return self.add("pe", fn, reads, writes)

    def act(self, fn, reads=(), writes=()):
        return self.add("act", fn, reads, writes)

    def dve(self, fn, reads=(), writes=()):
        return self.add("dve", fn, reads, writes)

    def pool(self, fn, reads=(), writes=()):
        return self.add("pool", fn, reads, writes)

    def dma(self, out, in_, reads=(), writes=(), sem=None, q="sp", **kw):
        return self.add(q, lambda e: e.dma_start(out=out, in_=in_, **kw), reads, writes, dma=True, sem=sem)

    def barrier(self):
        lasts = list(self.last_by_sem.values())
        for eng in self.ENGS:
            op = self.add(eng, lambda e: e.nop())
            op.extra = tuple(lasts)
        self.last_by_sem = {k: v for k, v in self.last_by_sem.items() if k.startswith("eng_")}

    def analyze(self):
        state = {}
        for op in self.ops:
            deps = set(op.extra)
            for k in op.reads:
                st = state.get(k)
                if st:
                    deps.update(st[0])
                    if k in self.psum_keys:
                        deps.update(r for r in st[1] if r.eng != op.eng)
            for k in op.writes:
                st = state.get(k)
                if st is None:
                    st = state[k] = [[], []]
                if st[1]:
                    deps.update(st[1])
                    deps.update(st[0])
                    st[0] = [op]
                    st[1] = []
                else:
                    same_group = op.dma and all(w.dma and w.sem == op.sem for w in st[0])
                    if same_group:
                        st[0].append(op)
                    else:
                        deps.update(st[0])
                        st[0] = [op]
            for k in op.reads:
                st = state.get(k)
                if st is None:
                    st = state[k] = [[], []]
                st[1].append(op)
            deps.discard(op)
            red = {}
            for d in deps:
                if (not d.dma) and (not op.dma) and d.eng == op.eng:
                    if op.eng == "pe" or not self.same_eng_sync:
                        continue
                cur = red.get(d.sem)
                if cur is None or d.id > cur.id:
                    red[d.sem] = d
            op.deps = list(red.values())
            for d in op.deps:
                d.needs_inc = True
        cnt = {}
        for op in self.ops:
            if op.needs_inc:
                cnt[op.sem] = cnt.get(op.sem, 0) + 1
                op.idx = cnt[op.sem]
        self.sem_names = sorted(cnt.keys())
        return cnt

    def emit(self, stack):
        nc = self.nc
        cnt = self.analyze()
        sems = {}
        for name in self.sem_names:
            sems[name] = stack.enter_context(nc.semaphore(name))
        block = stack.enter_context(nc.Block())
        per_eng = {e: [o for o in self.ops if o.eng == e] for e in self.ENGS}

        def run(eng_obj, ops):
            known = {}
            for op in ops:
                for d in op.deps:
                    val = d.idx * (16 if d.dma else 1)
                    if known.get(d.sem, 0) < val:
                        eng_obj.wait_ge(sems[d.sem], val)
                        known[d.sem] = val
                name, a, k = op.fn
                inst = getattr(eng_obj, name)(*a, **k)
                if op.needs_inc:
                    inst.then_inc(sems[op.sem], 16 if op.dma else 1)

        @block.sync
        def _(e):
            run(e, per_eng["sp"])

        @block.tensor
        def _(e):
            run(e, per_eng["pe"])

        @block.scalar
        def _(e):
            run(e, per_eng["act"])

        @block.vector
        def _(e):
            run(e, per_eng["dve"])

        @block.gpsimd
        def _(e):
            run(e, per_eng["pool"])
        return cnt


class Ring:
    def __init__(self, items):
        self.items = items
        self.i = 0

    def next(self):
        it = self.items[self.i % len(self.items)]
        self.i += 1
        return it


def tile_w(w):
    K, N = w.shape
    return np.ascontiguousarray(w.reshape(K // 128, 128, N).transpose(1, 0, 2).reshape(128, -1))


def rope_tabs(pos, d, scale):
    half = d // 2
    inv = 10000.0 ** (-np.arange(half, dtype=np.float64) * 2.0 / d)
    ang = pos.astype(np.float64)[None, :] * inv[:, None]
    cos = np.cos(ang) * scale
    sin = np.sin(ang) * scale
    reps = 128 // half
    return (np.tile(cos, (reps, 1)).astype(np.float32), np.tile(sin, (reps, 1)).astype(np.float32))


def rot_lhsT(d):
    half = d // 2
    Pm = np.zeros((128, 128), np.float32)
    for blk in range(128 // d):
        o = blk * d
        for m in range(half):
            Pm[o + m, o + m + half] = -1.0
            Pm[o + m + half, o + m] = 1.0
    return np.ascontiguousarray(Pm.T)


_CONST_CACHE = {}


def make_consts(c):
    if c in _CONST_CACHE:
        return _CONST_CACHE[c]
    cs = {}
    own_pos = np.concatenate([np.arange(128) + (2 * i + c) * 128 for i in range(16)])
    allpos = np.arange(T)
    cs["cosK"], cs["sinK"] = rope_tabs(allpos, 64, 1.0)
    cs["cosQ"], cs["sinQ"] = rope_tabs(own_pos, 64, 0.125)
    cs["cosRK"], cs["sinRK"] = rope_tabs(allpos, 128, 128.0 ** -0.5)
    cs["cosRQ"], cs["sinRQ"] = rope_tabs(own_pos, 128, 1.0)
    cend = np.arange(256) * 16 + 31
    cs["cosC"], cs["sinC"] = rope_tabs(cend, 64, 1.0)
    cs["pt64"] = rot_lhsT(64).astype(NPBF)
    cs["pt128"] = rot_lhsT(128).astype(NPBF)
    cs["identb"] = np.eye(128, dtype=np.float32).astype(NPBF)
    E = np.zeros((128, 32, 128), np.float32)
    for j in range(32):
        for k in range(128):
            E[2 * j + k // 64, j, k] = 1.0
            E[64 + 2 * j + k // 64, j, k] = 1.0
    cs["eall"] = E.reshape(128, -1).astype(NPBF)
    wm = np.zeros((128, 6, 128), np.float32)
    kk = np.arange(128)[:, None]
    tt = np.arange(128)[None, :]
    for r in range(6):
        dj = (r - 4) - c
        tk = dj * 128 + kk
        ok = (tk <= tt) & (tt - tk < 512)
        wm[:, r, :] = np.where(ok, 0.0, NEGM)
    cs["wmask"] = wm.reshape(128, -1).astype(NPBF)
    cm = np.zeros((128, 2, 16, 128), np.float32)
    for a in range(2):
        for i in range(16):
            G = 2 * i + c
            n = a * 128 + kk
            t = G * 128 + tt
            cm[:, a, i, :] = np.where(16 * n + 31 <= t, 0.0, NEGM)
    cs["cmpmask"] = cm.reshape(128, -1).astype(NPBF)
    cstart = np.arange(255) * 16
    sstart = np.arange(64) * 64
    ov = np.clip(np.minimum(cstart[None, :] + 32, sstart[:, None] + 64) - np.maximum(cstart[None, :], sstart[:, None]), 0, None) / 16.0
    ovT = np.zeros((256, 64), np.float32)
    ovT[:255] = ov.T
    cs["ovT"] = np.ascontiguousarray(ovT.reshape(2, 128, 64).transpose(1, 0, 2).reshape(128, -1)).astype(NPBF)
    tkm = np.zeros((128, 16, 64), np.float32)
    tkb = np.zeros((128, 16, 64), np.float32)
    for i in range(16):
        G = 2 * i + c
        for p in range(128):
            bt = (G * 128 + p) // 64
            for s in range(64):
                if s == 0:
                    tkb[p, i, s] = 1e9
                elif s == bt:
                    tkb[p, i, s] = 2e9
                elif s == bt - 1:
                    tkb[p, i, s] = 3e9
                elif s <= bt:
                    tkm[p, i, s] = 1.0
                else:
                    tkb[p, i, s] = -1e9 - 1e6 * s
    cs["tkm"] = tkm.reshape(128, -1)
    cs["tkb"] = tkb.reshape(128, -1)
    gam = 1.0 - 2.0 ** (-5.0 - np.arange(4, dtype=np.float64))
    lg = np.log(gam)
    m = np.arange(256)[:, None]
    cq = np.arange(128)[None, :]
    qq = 128 * c + cq
    Dc = np.zeros((128, 2, 4, 128), np.float32)
    for h in range(4):
        dd = np.where(qq >= m, np.exp(np.maximum(qq - m, 0) * lg[h]), 0.0)
        Dc[:, :, h, :] = dd.reshape(2, 128, 128).transpose(1, 0, 2)
    cs["Dc"] = Dc.reshape(128, -1)
    xi = np.zeros((128, 4, 128), np.float32)
    for h in range(4):
        xi[:, h, :] = np.exp((qq + 1.0) * lg[h])
    cs["xi"] = xi.reshape(128, -1)
    zt = np.zeros((128, 2, 4), np.float32)
    for h in range(4):
        zt[:, :, h] = np.exp((255.0 - np.arange(256)) * lg[h]).reshape(2, 128).T
    cs["zeta"] = zt.reshape(128, -1)
    cs["_decay256"] = [float(np.exp(256.0 * lg[h])) for h in range(4)]
    _CONST_CACHE[c] = cs
    return cs


CONST_SHAPES = None


def build_program(n_stage=6, debug=()):
    nc = bass.Bass("TRN2", target_bir_lowering=False)
    cs0 = make_consts(0)
    dram = {}

    def din(name, shape, dt=F32):
        dram[name] = nc.dram_tensor(name, list(shape), dt, kind="ExternalInput").ap()
        return dram[name]

    xT = din("xT", [1024, T])
    xTo = din("xTo", [1024, TO])
    xo = din("xo", [TO, 1024])
    w1t = din("w1t", [128, 8 * 1304])
    w4t = din("w4t", [128, 8 * 2048])
    wmgt = din("wmgt", [128, 8 * 2048])
    cw1 = {kv: din("cw1" + kv, [128, 32 * 256]) for kv in "kv"}
    cpos = {kv: din("cpos" + kv, [128, 32]) for kv in "kv"}
    cb1 = {kv: din("cb1" + kv, [128, 2]) for kv in "kv"}
    cw2k = din("cw2k", [128, 2 * 128])
    cw2v = din("cw2v", [128, 2 * 64])
    cb2k = din("cb2k", [128, 1])
    cb2v = din("cb2v", [64])
    gng8 = din("gng8", [128, 8])
    gnb8 = din("gnb8", [128, 8])
    wgtt = din("wgtt", [128, 8 * 1024])
    wat = din("wat", [128, 4 * 1024])
    wrt = din("wrt", [128, 8 * 1024])
    wot = din("wot", [128, 8 * 1024])
    ln1g = din("ln1g", [1024])
    ln1b = din("ln1b", [1024])
    ln2g = din("ln2g", [1024])
    ln2b = din("ln2b", [1024])
    wrout = din("wrout", [128, 8 * 36])
    brout = din("brout", [36])
    wexp = din("wexp", [32, 128, 12288])
    cdr = {}
    for k, v in cs0.items():
        if k.startswith("_"):
            continue
        cdr[k] = din("c_" + k, v.shape, BF16 if v.dtype == NPBF else F32)
    out = nc.dram_tensor("out", [TO, 1024], F32, kind="ExternalOutput").ap()
    dbg_out = {}

    decay256 = cs0["_decay256"]

    with ExitStack() as G:
        P = Prog(nc)

        def sb(stack, name, shape, dt):
            return stack.enter_context(nc.sbuf_tensor(name, list(shape), dt))

        def ps(stack, name, shape, dt=F32):
            P.psum_keys.add(name)
            ncol = 512 if dt == F32 else 1024
            full = stack.enter_context(nc.psum_tensor(name, [128, ncol], dt))
            n = 1
            for d_ in shape[1:]:
                n *= d_
            v = full[0:shape[0], 0:n]
            if len(shape) == 3:
                v = v.rearrange("p (a b) -> p a b", a=shape[1])
            return v

        def dump(name, ap, shape, key):
            if name in debug:
                t = nc.dram_tensor("dbg_" + name, list(shape), ap.dtype, kind="ExternalOutput").ap()
                dbg_out[name] = t
                P.dma(t, ap, reads=[key], writes=["dbg_" + name], sem="dbg")

        identb = sb(G, "identb", [128, 128], BF16)
        P.dma(identb[:], cdr["identb"], writes=["identb"])
        wst = sb(G, "wst", [128, 4096], F32)
        cast_rr = [0]

        def load_cast(dst_ap, src_ap, n, dst_key, shape3=None):
            o = 0
            while o < n:
                m = min(4096, n - o)
                P.dma(wst[:, 0:m], src_ap[:, o:o + m], writes=["wst"])
                d = dst_ap[:, o:o + m]
                if cast_rr[0] % 2 == 0:
                    P.act(lambda e, d=d, m=m: e.copy(out=d, in_=wst[:, 0:m]), ["wst"], [dst_key])
                else:
                    P.dve(lambda e, d=d, m=m: e.tensor_copy(out=d, in_=wst[:, 0:m]), ["wst"], [dst_key])
                cast_rr[0] += 1
                o += m

        x1T = sb(G, "x1T", [128, 8, TO], BF16)
        wst3 = wst[:].rearrange("p (k n) -> p k n", k=8)
        A_ = ExitStack()
        oattnT = sb(A_, "oattnT", [128, 4, TO], BF16)

        with ExitStack() as SN:
            QT = sb(SN, "QT", [128, 16, 4, 128], BF16)
            KE = [sb(SN, "KE%d" % i_, [128, T], BF16) for i_ in range(2)]
            P.dma(KE[0][64:128, :], cdr["eall"][64:128, :], writes=["KE0"])
            P.dma(KE[1][0:64, :], cdr["eall"][0:64, :], writes=["KE1"])
            winkT = sb(SN, "winkT", [128, T], BF16)
            slcv1 = sb(SN, "slcv1", [128, 32, 2, 65], BF16)
            winv1 = sb(SN, "winv1", [128, 32, 2, 65], BF16)
            gates = sb(SN, "gates", [128, 16, 24], F32)
            kcmpT = sb(SN, "kcmpT", [128, 256], BF16)
            vcmp1 = sb(SN, "vcmp1", [128, 2, 2, 65], BF16)
            pt64 = sb(SN, "pt64", [128, 128], BF16)
            P.dma(pt64[:], cdr["pt64"], writes=["pt64"])
            P.dve(lambda e: e.memset(slcv1[:].rearrange("p a g d -> p (a g d)"), 1.0), [], ["slcv1"])
            P.dve(lambda e: e.memset(winv1[:].rearrange("p a g d -> p (a g d)"), 1.0), [], ["winv1"])
            P.dve(lambda e: e.memset(kcmpT[:], 0.0), [], ["kcmpT"])
            P.dve(lambda e: e.memset(vcmp1[:].rearrange("p a g d -> p (a g d)"), 0.0), [], ["vcmp1"])
            P.dve(lambda e: e.memset(vcmp1[:, :, :, 64:65], 1.0), [], ["vcmp1"])

            with ExitStack() as S12:
                cmpT = {"k": sb(S12, "cmpkT", [128, T], BF16), "v": sb(S12, "cmpvT", [128, T], BF16)}
                with ExitStack() as S1:
                    Wn = sb(S1, "Wn", [128, 8, 1304], BF16)
                    load_cast(Wn[:].rearrange("p k n -> p (k n)"), w1t, 8 * 1304, "Wn")
                    xb = [sb(S1, "xb%d" % i, [128, 8, 512], BF16) for i in range(2)]
                    tabs = [sb(S1, "tab%d" % i, [128, 2, 512], F32) for i in range(2)]
                    ybf = [sb(S1, "ybf%d" % i, [128, 512], BF16) for i in range(2)]
                    t1 = [sb(S1, "t1_%d" % i, [128, 512], F32) for i in range(2)]
                    t2 = [sb(S1, "t2_%d" % i, [128, 512], F32) for i in range(2)]
                    pj = [ps(S1, "pj%d" % i, [128, 512]) for i in range(3)]
                    prot = [ps(S1, "prot%d" % i, [128, 512]) for i in range(2)]
                    pv = [ps(S1, "pv%d" % i, [128, 256]) for i in range(2)]
                    pjr = Ring(list(range(3)))
                    rr = Ring(list(range(2)))
                    pvr = Ring(list(range(2)))
                    xTv = xT.rearrange("(k p) t -> p k t", p=128)
                    xTov = xTo.rearrange("(k p) t -> p k t", p=128)

                    def load_x(src_view, c0, n, slot):
                        P.dma(wst3[:, :, 0:n], src_view[:, :, c0:c0 + n], writes=["wst"])
                        P.act(lambda e: e.copy(out=xb[slot][:, 0:4, 0:n], in_=wst3[:, 0:4, 0:n]), ["wst"], ["xb%d" % slot])
                        P.dve(lambda e: e.tensor_copy(out=xb[slot][:, 4:8, 0:n], in_=wst3[:, 4:8, 0:n]), ["wst"], ["xb%d" % slot])

                    def proj_fm(col0, slot, n=512):
                        pi = pjr.next()
                        for k in range(8):
                            P.pe(lambda e, k=k, pi=pi: e.matmul(pj[pi][:, 0:n], lhsT=Wn[:, k, col0:col0 + 128], rhs=xb[slot][:, k, 0:n],
                                                                 start=(k == 0), stop=(k == 7)), ["Wn", "xb%d" % slot], ["pj%d" % pi])
                        return pi

                    def rope_fm(pi, tslot, dst_ap, dst_key, ptm, ptkey, n=512, src=None, srckey=None):
                        r = rr.next()
                        srcap = pj[pi][:, 0:n] if src is None else src
                        sk = ("pj%d" % pi) if srckey is None else srckey
                        P.act(lambda e: e.copy(out=ybf[r][:, 0:n], in_=srcap), [sk], ["ybf%d" % r])
                        P.pe(lambda e: e.matmul(prot[r][:, 0:n], lhsT=ptm[:], rhs=ybf[r][:, 0:n], start=True, stop=True),
                             [ptkey, "ybf%d" % r], ["prot%d" % r])
                        P.dve(lambda e: e.tensor_tensor(out=t1[r][:, 0:n], in0=srcap, in1=tabs[tslot][:, 0, 0:n], op=ALU.mult),
                              [sk, "tab%d" % tslot], ["t1_%d" % r])
                        P.dve(lambda e: e.tensor_tensor(out=t2[r][:, 0:n], in0=prot[r][:, 0:n], in1=tabs[tslot][:, 1, 0:n], op=ALU.mult),
                              ["prot%d" % r, "tab%d" % tslot], ["t2_%d" % r])
                        if isinstance(dst_ap, list):
                            for (d_ap, rows, dkey) in dst_ap:
                                P.pool(lambda e: e.tensor_tensor(out=d_ap, in0=t1[r][rows, 0:n], in1=t2[r][rows, 0:n], op=ALU.add),
                                       ["t1_%d" % r, "t2_%d" % r], [dkey])
                        elif dst_key == "QT":
                            P.pool(lambda e: e.tensor_tensor(out=dst_ap, in0=t1[r][:, 0:n].rearrange("p (a t) -> p a t", a=4),
                                                             in1=t2[r][:, 0:n].rearrange("p (a t) -> p a t", a=4), op=ALU.add),
                                   ["t1_%d" % r, "t2_%d" % r], [dst_key])
                        else:
                            P.pool(lambda e: e.tensor_tensor(out=dst_ap, in0=t1[r][:, 0:n], in1=t2[r][:, 0:n], op=ALU.add),
                                   ["t1_%d" % r, "t2_%d" % r], [dst_key])

                    for ch in range(8):
                        slot = ch % 2
                        c0 = ch * 512
                        load_x(xTv, c0, 512, slot)
                        P.dma(tabs[slot][:, 0, :], cdr["cosK"][:, c0:c0 + 512], writes=["tab%d" % slot])
                        P.dma(tabs[slot][:, 1, :], cdr["sinK"][:, c0:c0 + 512], writes=["tab%d" % slot])
                        for col0, kv in ((512, "k"), (640, "v")):
                            pi = proj_fm(col0, slot)
                            P.act(lambda e, pi=pi, kv=kv: e.copy(out=cmpT[kv][:, c0:c0 + 512], in_=pj[pi][:]), ["pj%d" % pi], ["cmp" + kv + "T"])
                        pi = proj_fm(768, slot)
                        rope_fm(pi, slot, [(KE[0][0:64, c0:c0 + 512], slice(0, 64), "KE0"), (KE[1][64:128, c0:c0 + 512], slice(64, 128), "KE1")], None, pt64, "pt64")
                        pi = proj_fm(896, slot)
                        rope_fm(pi, slot, winkT[:, c0:c0 + 512], "winkT", pt64, "pt64")
                        for tt in range(4):
                            vi = pvr.next()
                            for k in range(8):
                                P.pe(lambda e, k=k, vi=vi, tt=tt: e.matmul(pv[vi][:], lhsT=xb[slot][:, k, tt * 128:(tt + 1) * 128], rhs=Wn[:, k, 1024:1280],
                                                                            start=(k == 0), stop=(k == 7)), ["Wn", "xb%d" % slot], ["pv%d" % vi])
                            tg = ch * 4 + tt
                            P.act(lambda e, vi=vi, tg=tg: e.copy(out=slcv1[:, tg, :, 0:64], in_=pv[vi][:, 0:128].rearrange("p (g d) -> p g d", g=2)),
                                  ["pv%d" % vi], ["slcv1"])
                            P.dve(lambda e, vi=vi, tg=tg: e.tensor_copy(out=winv1[:, tg, :, 0:64], in_=pv[vi][:, 128:256].rearrange("p (g d) -> p g d", g=2)),
                                  ["pv%d" % vi], ["winv1"])
                    for oc in range(4):
                        slot = oc % 2
                        c0 = oc * 512
                        load_x(xTov, c0, 512, slot)
                        P.dma(tabs[slot][:, 0, :], cdr["cosQ"][:, c0:c0 + 512], writes=["tab%d" % slot])
                        P.dma(tabs[slot][:, 1, :], cdr["sinQ"][:, c0:c0 + 512], writes=["tab%d" % slot])
                        for hh in range(4):
                            pi = proj_fm(hh * 128, slot)
                            rope_fm(pi, slot, QT[:, oc * 4:(oc + 1) * 4, hh, :], "QT", pt64, "pt64")
                        for tt in range(4):
                            vi = pvr.next()
                            for k in range(8):
                                P.pe(lambda e, k=k, vi=vi, tt=tt: e.matmul(pv[vi][:, 0:24], lhsT=xb[slot][:, k, tt * 128:(tt + 1) * 128], rhs=Wn[:, k, 1280:1304],
                                                                            start=(k == 0), stop=(k == 7)), ["Wn", "xb%d" % slot], ["pv%d" % vi])
                            tg = oc * 4 + tt
                            P.act(lambda e, vi=vi, tg=tg: e.activation(out=gates[:, tg, :], in_=pv[vi][:, 0:24], func=AF.Sigmoid), ["pv%d" % vi], ["gates"])
                    dump("QT", QT[:].rearrange("p i a t -> p (i a t)"), [128, 4 * TO], "QT")
                    dump("cmpkT", cmpT["k"][:], [128, T], "cmpkT")
                    dump("slcv1", slcv1[:].rearrange("p a g d -> p (a g d)"), [128, 32 * 130], "slcv1")
                    dump("gates", gates[:].rearrange("p a g -> p (a g)"), [128, 16 * 24], "gates")
                P.barrier()
                if n_stage >= 2:
                    with ExitStack() as S2:
                        w1b = sb(S2, "w1b", [128, 32, 256], BF16)
                        posT = sb(S2, "posT", [128, 32], F32)
                        posTb = sb(S2, "posTb", [128, 32], BF16)
                        b1 = sb(S2, "b1", [128, 2], F32)
                        bias1 = sb(S2, "bias1", [128, 2], F32)
                        w2kf = sb(S2, "w2kf", [128, 2, 128], F32)
                        w2k = sb(S2, "w2k", [128, 2, 128], BF16)
                        w2vf = sb(S2, "w2vf", [128, 2, 64], F32)
                        w2v = sb(S2, "w2v", [128, 2, 64], BF16)
                        b2k = sb(S2, "b2k", [128, 1], F32)
                        b2v = sb(S2, "b2v", [128, 64], F32)
                        tabC = sb(S2, "tabC", [128, 2, 256], F32)
                        h1 = sb(S2, "h1", [128, 2, 256], BF16)
                        xg = sb(S2, "xg", [128, 256], F32)
                        ug = sb(S2, "ug", [128, 256], F32)
                        sg_ = sb(S2, "sg_", [128, 256], F32)
                        yk = sb(S2, "yk", [128, 256], F32)
                        ykb = sb(S2, "ykb", [128, 256], BF16)
                        tk1 = sb(S2, "tk1", [128, 256], F32)
                        tk2 = sb(S2, "tk2", [128, 256], F32)
                        ph = [ps(S2, "ph%d" % i, [128, 256]) for i in range(2)]
                        pcv = ps(S2, "pcv", [128, 2])
                        pkc = ps(S2, "pkc", [128, 256])
                        prk = ps(S2, "prk", [128, 256])
                        pvc = ps(S2, "pvc", [128, 64])
                        P.dma(w2kf[:].rearrange("p a n -> p (a n)"), cw2k, writes=["w2kf"])
                        P.dve(lambda e: e.tensor_copy(out=w2k[:], in_=w2kf[:]), ["w2kf"], ["w2k"])
                        P.dma(w2vf[:].rearrange("p a n -> p (a n)"), cw2v, writes=["w2vf"])
                        P.dve(lambda e: e.tensor_copy(out=w2v[:], in_=w2vf[:]), ["w2vf"], ["w2v"])
                        P.dma(b2k[:], cb2k, writes=["b2k"])
                        P.dma(b2v[:], cb2v.partition_broadcast(128), writes=["b2v"])
                        P.dma(tabC[:, 0, :], cdr["cosC"], writes=["tabC"])
                        P.dma(tabC[:, 1, :], cdr["sinC"], writes=["tabC"])
                        for kv in "kv":
                            load_cast(w1b[:].rearrange("p l n -> p (l n)"), cw1[kv], 32 * 256, "w1b")
                            P.dma(posT[:], cpos[kv], writes=["posT"])
                            P.dve(lambda e: e.tensor_copy(out=posTb[:], in_=posT[:]), ["posT"], ["posTb"])
                            P.dma(b1[:], cb1[kv], writes=["b1"])
                            for ht in range(2):
                                for l in range(32):
                                    P.pe(lambda e, ht=ht, l=l: e.matmul(pcv[:, ht:ht + 1], lhsT=w1b[0:64, l, ht * 128:(ht + 1) * 128], rhs=posTb[0:64, l:l + 1],
                                                                         start=(l == 0), stop=(l == 31)), ["w1b", "posTb"], ["pcv"])
                            P.dve(lambda e: e.tensor_tensor(out=bias1[:], in0=pcv[:], in1=b1[:], op=ALU.add), ["pcv", "b1"], ["bias1"])
                            for g in range(2):
                                gp = slice(g * 64, (g + 1) * 64)
                                for ht in range(2):
                                    for l in range(32):
                                        P.pe(lambda e, ht=ht, l=l, gp=gp, kv=kv: e.matmul(ph[ht][:, 0:255], lhsT=w1b[gp, l, ht * 128:(ht + 1) * 128],
                                                                                        rhs=cmpT[kv][gp, l:l + 16 * 254 + 1:16],
                                                                                        start=(l == 0), stop=(l == 31)), ["w1b", "cmp" + kv + "T"], ["ph%d" % ht])
                                    P.act(lambda e, ht=ht: e.activation(out=xg[:, 0:255], in_=ph[ht][:, 0:255], func=AF.Identity, bias=bias1[:, ht:ht + 1], scale=1.0),
                                          ["ph%d" % ht, "bias1"], ["xg"])
                                    P.dve(lambda e: e.tensor_tensor(out=ug[:, 0:255], in0=xg[:, 0:255], in1=xg[:, 0:255], op=ALU.mult), ["xg"], ["ug"])
                                    P.dve(lambda e: e.tensor_scalar(out=ug[:, 0:255], in0=ug[:, 0:255], scalar1=0.044715, scalar2=1.0, op0=ALU.mult, op1=ALU.add), ["ug"], ["ug"])
                                    P.dve(lambda e: e.tensor_tensor(out=ug[:, 0:255], in0=ug[:, 0:255], in1=xg[:, 0:255], op=ALU.mult), ["ug", "xg"], ["ug"])
                                    P.act(lambda e: e.activation(out=sg_[:, 0:255], in_=ug[:, 0:255], func=AF.Sigmoid, scale=1.5957691216057308), ["ug"], ["sg_"])
                                    P.dve(lambda e, ht=ht: e.tensor_tensor(out=h1[:, ht, 0:255], in0=xg[:, 0:255], in1=sg_[:, 0:255], op=ALU.mult), ["xg", "sg_"], ["h1"])
                                if kv == "k":
                                    for ht in range(2):
                                        P.pe(lambda e, ht=ht: e.matmul(pkc[:, 0:255], lhsT=w2k[:, ht, :], rhs=h1[:, ht, 0:255], start=(ht == 0), stop=(ht == 1)),
                                             ["w2k", "h1"], ["pkc"])
                                    P.act(lambda e: e.activation(out=yk[:, 0:255], in_=pkc[:, 0:255], func=AF.Identity, bias=b2k[:, 0:1], scale=1.0), ["pkc", "b2k"], ["yk"])
                                    P.act(lambda e: e.copy(out=ykb[:, 0:255], in_=yk[:, 0:255]), ["yk"], ["ykb"])
                                    P.pe(lambda e: e.matmul(prk[:, 0:255], lhsT=pt64[:], rhs=ykb[:, 0:255], start=True, stop=True), ["pt64", "ykb"], ["prk"])
                                    P.dve(lambda e: e.tensor_tensor(out=tk1[:, 0:255], in0=yk[:, 0:255], in1=tabC[:, 0, 0:255], op=ALU.mult), ["yk", "tabC"], ["tk1"])
                                    P.dve(lambda e: e.tensor_tensor(out=tk2[:, 0:255], in0=prk[:, 0:255], in1=tabC[:, 1, 0:255], op=ALU.mult), ["prk", "tabC"], ["tk2"])
                                    P.dve(lambda e, gp=gp: e.tensor_tensor(out=kcmpT[gp, 0:255], in0=tk1[gp, 0:255], in1=tk2[gp, 0:255], op=ALU.add), ["tk1", "tk2"], ["kcmpT"])
                                else:
                                    for a in range(2):
                                        cntn = 128 if a == 0 else 127
                                        for ht in range(2):
                                            P.pe(lambda e, ht=ht, a=a, cntn=cntn: e.matmul(pvc[0:cntn, :], lhsT=h1[:, ht, a * 128:a * 128 + cntn], rhs=w2v[:, ht, :],
                                                                                            start=(ht == 0), stop=(ht == 1)), ["w2v", "h1"], ["pvc"])
                                        P.dve(lambda e, a=a, cntn=cntn, g=g: e.tensor_tensor(out=vcmp1[0:cntn, a, g, 0:64], in0=pvc[0:cntn, :], in1=b2v[0:cntn, :], op=ALU.add),
                                              ["pvc", "b2v"], ["vcmp1"])
                        dump("kcmpT", kcmpT[:], [128, 256], "kcmpT")
                        dump("vcmp1", vcmp1[:].rearrange("p a g d -> p (a g d)"), [128, 260], "vcmp1")
                    P.barrier()
            P.barrier()
            if n_stage >= 3:
                with ExitStack() as S3:
                    def bc4(ap):
                        return ap.unsqueeze(1).broadcast_to([ap.shape[0], 4, ap.shape[1]])

                    wmask = sb(S3, "wmask", [128, 6, 128], BF16)
                    cmpmask = sb(S3, "cmpmask", [128, 2, 16, 128], BF16)
                    ovT = sb(S3, "ovT", [128, 2, 64], BF16)
                    tkm = sb(S3, "tkm", [128, 16, 64], F32)
                    tkb = sb(S3, "tkb", [128, 16, 64], F32)
                    wmask4 = sb(S3, "wmask4", [128, 6, 512], BF16)
                    cm4 = [sb(S3, "cm4_%d" % i_, [128, 2, 512], BF16) for i_ in range(2)]
                    QN = [sb(S3, "QN%d" % i_, [128, 512], BF16) for i_ in range(4)]
                    P.dma(wmask[:].rearrange("p a k -> p (a k)"), cdr["wmask"], writes=["wmask"])
                    P.dma(cmpmask[:].rearrange("p a i k -> p (a i k)"), cdr["cmpmask"], writes=["cmpmask"])
                    P.dma(ovT[:].rearrange("p a k -> p (a k)"), cdr["ovT"], writes=["ovT"])
                    P.dma(tkm[:].rearrange("p a k -> p (a k)"), cdr["tkm"], writes=["tkm"])
                    P.dma(tkb[:].rearrange("p a k -> p (a k)"), cdr["tkb"], writes=["tkb"])
                    for r_ in range(6):
                        P.pool(lambda e: e.tensor_copy(out=wmask4[:, r_, :].rearrange("p (a t) -> p a t", a=4), in_=bc4(wmask[:, r_, :])), ["wmask"], ["wmask4"])
                    eT = [sb(S3, "eT%d" % i, [128, 512], BF16) for i in range(4)]
                    oacc = sb(S3, "oacc", [128, 512], F32)
                    oab = sb(S3, "oab", [128, 512], BF16)
                    rz = sb(S3, "rz", [128, 4], F32)
                    coef = sb(S3, "coef", [128, 4], F32)
                    imp = sb(S3, "imp", [128, 64], F32)
                    score = sb(S3, "score", [128, 64], F32)
                    work = sb(S3, "work", [128, 64], F32)
                    m8 = sb(S3, "m8", [128, 16], F32)
                    nmk = [sb(S3, "nmk%d" % i_, [128, 2, 64], BF16) for i_ in range(2)]
                    pST = [ps(S3, "pST%d" % i, [128, 512]) for i in range(3)]
                    pA = ps(S3, "pA", [128, 4, 65])
                    pB = ps(S3, "pB", [128, 4, 64])
                    pS = ps(S3, "pS", [128, 4, 65])
                    pW = ps(S3, "pW", [128, 4, 65])
                    pTr = ps(S3, "pTr", [128, 128], BF16)
                    str_ = Ring([0, 1, 2])
                    etr = Ring([0, 1, 2, 3])

                    def scores(kT_ap, kkey, g, i, masks, q_ap=None, qkey="QT"):
                        gp = slice(g * 64, (g + 1) * 64)
                        si = str_.next()
                        ei = etr.next()
                        nm = len(masks)
                        if q_ap is None:
                            q_ap = QT[gp, i, :, :].rearrange("p a t -> p (a t)")
                        P.pe(lambda e: e.matmul(pST[si][:], lhsT=kT_ap, rhs=q_ap, start=True, stop=(nm == 0)), [kkey, qkey], ["pST%d" % si])
                        for mi, (ml, mr, mkeys) in enumerate(masks):
                            P.pe(lambda e: e.matmul(pST[si][:], lhsT=ml, rhs=mr, start=False, stop=(mi == nm - 1)), mkeys, ["pST%d" % si])
                        P.act(lambda e: e.activation(out=eT[ei][:], in_=pST[si][:], func=AF.Exp), ["pST%d" % si], ["eT%d" % ei])
                        return ei

                    def finish_branch(pacc, pkey, i, g, br, first):
                        P.dve(lambda e: e.tensor_scalar(out=rz[:], in0=pacc[:, :, 64], scalar1=1e-30, scalar2=None, op0=ALU.max), [pkey], ["rz"])
                        P.dve(lambda e: e.reciprocal(out=rz[:], in_=rz[:]), ["rz"], ["rz"])
                        P.dve(lambda e: e.tensor_tensor(out=coef[:], in0=rz[:], in1=gates[:, i, g * 12 + br:g * 12 + 12:3], op=ALU.mult), ["rz", "gates"], ["coef"])
                        for hh in range(4):
                            o = oacc[:, g * 256 + hh * 64:g * 256 + (hh + 1) * 64]
                            if first:
                                P.dve(lambda e, hh=hh, o=o: e.tensor_scalar(out=o, in0=pacc[:, hh, 0:64], scalar1=coef[:, hh:hh + 1], scalar2=None, op0=ALU.mult),
                                      [pkey, "coef"], ["oacc"])
                            else:
                                P.dve(lambda e, hh=hh, o=o: e.scalar_tensor_tensor(out=o, in0=pacc[:, hh, 0:64], scalar=coef[:, hh:hh + 1], in1=o, op0=ALU.mult, op1=ALU.add),
                                      [pkey, "coef", "oacc"], ["oacc"])

                    tasks = []

                    def mk_cmp(i, g, a, na):
                        gp = slice(g * 64, (g + 1) * 64)

                        def sc():
                            if g == 0:
                                P.pool(lambda e: e.tensor_copy(out=cm4[i % 2][:, a, :].rearrange("p (h t) -> p h t", h=4), in_=bc4(cmpmask[:, a, i, :])),
                                       ["cmpmask"], ["cm4_%d" % (i % 2)])
                            return scores(kcmpT[gp, a * 128:(a + 1) * 128], "kcmpT", g, i,
                                          [(identb[:], cm4[i % 2][:, a, :], ["identb", "cm4_%d" % (i % 2)])])

                        def pvf(ei):
                            for hh in range(4):
                                P.pe(lambda e: e.matmul(pA[:, hh, :], lhsT=eT[ei][:, hh * 128:(hh + 1) * 128], rhs=vcmp1[:, a, g, :],
                                                        start=(a == 0 and hh == 0), stop=(a == na - 1 and hh == 3)), ["eT%d" % ei, "vcmp1"], ["pA"])
                                P.pe(lambda e: e.matmul(pB[:, hh, :], lhsT=eT[ei][:, hh * 128:(hh + 1) * 128], rhs=ovT[:, a, :],
                                                        start=(a == 0 and hh == 0), stop=(a == na - 1 and hh == 3)), ["eT%d" % ei, "ovT"], ["pB"])

                        def post():
                            finish_branch(pA, "pA", i, g, 0, True)
                            P.dve(lambda e: e.tensor_scalar(out=imp[:], in0=pB[:, 0, :], scalar1=rz[:, 0:1], scalar2=None, op0=ALU.mult), ["pB", "rz"], ["imp"])
                            for hh in range(1, 4):
                                P.dve(lambda e: e.scalar_tensor_tensor(out=imp[:], in0=pB[:, hh, :], scalar=rz[:, hh:hh + 1], in1=imp[:], op0=ALU.mult, op1=ALU.add),
                                      ["pB", "rz", "imp"], ["imp"])
                            P.dve(lambda e: e.tensor_tensor(out=score[:], in0=imp[:], in1=tkm[:, i, :], op=ALU.mult), ["imp", "tkm"], ["score"])
                            P.dve(lambda e: e.tensor_tensor(out=score[:], in0=score[:], in1=tkb[:, i, :], op=ALU.add), ["score", "tkb"], ["score"])
                            P.dve(lambda e: e.max(out=m8[:, 0:8], in_=score[:]), ["score"], ["m8"])
                            P.dve(lambda e: e.match_replace(out=work[:], in_to_replace=m8[:, 0:8], in_values=score[:], imm_value=-3.0e38), ["score", "m8"], ["work"])
                            P.dve(lambda e: e.max(out=m8[:, 8:16], in_=work[:]), ["work"], ["m8"])
                            P.dve(lambda e: e.tensor_scalar(out=nmk[g][:], in0=score[:].unsqueeze(1).broadcast_to([128, 2, 64]), scalar1=m8[:, 15:16], scalar2=NEGM,
                                                            op0=ALU.is_lt, op1=ALU.mult), ["score", "m8"], ["nmk%d" % g])
                            if ("imp%d_%d" % (i, g)) in debug:
                                dump("imp%d_%d" % (i, g), imp[:], [128, 64], "imp")
                                dump("score%d_%d" % (i, g), score[:], [128, 64], "score")
                                dump("m8%d_%d" % (i, g), m8[:], [128, 16], "m8")
                        return [None, sc, pvf, post if a == na - 1 else None]

                    def mk_win(i, g, idx, r, j, nw):
                        gp = slice(g * 64, (g + 1) * 64)

                        def sc():
                            return scores(winkT[gp, j * 128:(j + 1) * 128], "winkT", g, i,
                                          [(identb[:], wmask4[:, r, :], ["identb", "wmask4"])])

                        def pvf(ei):
                            for hh in range(4):
                                P.pe(lambda e: e.matmul(pW[:, hh, :], lhsT=eT[ei][:, hh * 128:(hh + 1) * 128], rhs=winv1[:, j, g, :],
                                                        start=(idx == 0 and hh == 0), stop=(idx == nw - 1 and hh == 3)), ["eT%d" % ei, "winv1"], ["pW"])

                        def post():
                            finish_branch(pW, "pW", i, g, 2, False)
                        return [None, sc, pvf, post if idx == nw - 1 else None]

                    def tile_end_pe(i):
                        for ct in range(4):
                            P.pe(lambda e: e.transpose(out=pTr[:], in_=oab[:, ct * 128:(ct + 1) * 128], identity=identb[:]), ["oab", "identb"], ["pTr"])
                            P.dve(lambda e: e.tensor_copy(out=oattnT[:, ct, i * 128:(i + 1) * 128], in_=pTr[:]), ["pTr"], ["oattnT"])

                    def mk_slc(i, g, j, nj):
                        gp = slice(g * 64, (g + 1) * 64)

                        qn_i = (2 * i + g) % 4
                        oh = slice((1 - g) * 64, (2 - g) * 64)

                        def pre():
                            P.pool(lambda e: e.tensor_copy(out=QN[qn_i][gp, :], in_=QT[gp, i, :, :].rearrange("p a t -> p (a t)")), ["QT"], ["QN%d" % qn_i])
                            P.pe(lambda e: e.transpose(out=pTr[:], in_=nmk[g][:].rearrange("p a s -> p (a s)"), identity=identb[:]), ["nmk%d" % g, "identb"], ["pTr"])
                            P.dve(lambda e: e.tensor_copy(out=QN[qn_i][oh, :].rearrange("p (a t) -> p a t", a=4), in_=bc4(pTr[oh, :])), ["pTr"], ["QN%d" % qn_i])
                            if g == 0 and i > 0:
                                tile_end_pe(i - 1)

                        def sc():
                            masks = []
                            if j >= 2 * i:
                                masks.append((identb[:], wmask4[:, 4 + (j - 2 * i), :], ["identb", "wmask4"]))
                            return scores(KE[g][:, j * 128:(j + 1) * 128], "KE%d" % g, g, i, masks, q_ap=QN[qn_i][:], qkey="QN%d" % qn_i)

                        def pvf(ei):
                            for hh in range(4):
                                P.pe(lambda e: e.matmul(pS[:, hh, :], lhsT=eT[ei][:, hh * 128:(hh + 1) * 128], rhs=slcv1[:, j, g, :],
                                                        start=(j == 0 and hh == 0), stop=(j == nj - 1 and hh == 3)), ["eT%d" % ei, "slcv1"], ["pS"])

                        def post():
                            finish_branch(pS, "pS", i, g, 1, False)
                            if g == 1:
                                if ("oacc%d" % i) in debug:
                                    dump("oacc%d" % i, oacc[:], [128, 512], "oacc")
                                P.pool(lambda e: e.tensor_copy(out=oab[:], in_=oacc[:]), ["oacc"], ["oab"])
                        return [pre if j == 0 else None, sc, pvf, post if j == nj - 1 else None]

                    for i in range(16):
                        for g in range(2):
                            na = 1 if i < 8 else 2
                            for a in range(na):
                                tasks.append(mk_cmp(i, g, a, na))
                            js = [(r, 2 * i - 4 + r) for r in range(6) if 2 * i - 4 + r >= 0]
                            for idx, (r, j) in enumerate(js):
                                tasks.append(mk_win(i, g, idx, r, j, len(js)))
                            nj = 2 * i + 2
                            for j in range(nj):
                                tasks.append(mk_slc(i, g, j, nj))
                    nt = len(tasks)
                    eis = [None] * nt

                    def emit_score(k):
                        if tasks[k][0] is not None:
                            tasks[k][0]()
                        eis[k] = tasks[k][1]()

                    emit_score(0)
                    emit_score(1)
                    for k in range(nt):
                        if k + 2 < nt:
                            emit_score(k + 2)
                        tasks[k][2](eis[k])
                        if tasks[k][3] is not None:
                            tasks[k][3]()
                    tile_end_pe(15)
                    dump("oattnT", oattnT[:].rearrange("p a t -> p (a t)"), [128, 4 * TO], "oattnT")
                P.barrier()
        P.barrier()

        B_ = ExitStack()
        oretT = sb(B_, "oretT", [128, 8, TO], BF16)
        if n_stage >= 4:
            with ExitStack() as S4:
                W4 = sb(S4, "W4", [128, 8, 2048], BF16)
                load_cast(W4[:].rearrange("p k n -> p (k n)"), w4t, 8 * 2048, "W4")
                pt128 = sb(S4, "pt128", [128, 128], BF16)
                P.dma(pt128[:], cdr["pt128"], writes=["pt128"])
                Dc = sb(S4, "Dc", [128, 2, 4, 128], F32)
                xi = sb(S4, "xi", [128, 4, 128], F32)
                zeta = sb(S4, "zeta", [128, 2, 4], F32)
                P.dma(Dc[:].rearrange("p a h c -> p (a h c)"), cdr["Dc"], writes=["Dc"])
                P.dma(xi[:].rearrange("p h c -> p (h c)"), cdr["xi"], writes=["xi"])
                P.dma(zeta[:].rearrange("p a h -> p (a h)"), cdr["zeta"], writes=["zeta"])
                xst = wst3
                xb = sb(S4, "xb4", [128, 8, 512], BF16)
                xob = sb(S4, "xob4", [128, 8, 256], BF16)
                tabs = sb(S4, "tab4", [128, 2, 512], F32)
                tabq = sb(S4, "tabq4", [128, 2, 256], F32)
                ybf2 = [sb(S4, "ybf4_%d" % i_, [128, 512], BF16) for i_ in range(2)]
                t12 = [sb(S4, "t1_4_%d" % i_, [128, 512], F32) for i_ in range(2)]
                t22 = [sb(S4, "t2_4_%d" % i_, [128, 512], F32) for i_ in range(2)]
                rr4 = Ring([0, 1])
                kT = sb(S4, "kT4", [128, 4, 512], BF16)
                qT = sb(S4, "qT4", [128, 4, 256], BF16)
                qxT = sb(S4, "qxT4", [128, 4, 256], BF16)
                vtok = sb(S4, "vtok", [128, 4, 1024], BF16)
                kz = sb(S4, "kz", [128, 4, 4, 128], BF16)
                R = sb(S4, "R", [128, 4, 256], F32)
                Rb = sb(S4, "Rb", [128, 4, 256], BF16)
                sc = [sb(S4, "sc%d" % i_, [128, 2, 128], BF16) for i_ in range(2)]
                epsT = sb(S4, "epsT", [128, 1], F32)
                P.dve(lambda e: e.memset(epsT[:], LN_EPS), [], ["epsT"])
                pending4 = []
                st6 = sb(S4, "st6", [128, 6], F32)
                mv = sb(S4, "mv", [128, 2], F32)
                rstd = sb(S4, "rstd", [128, 1], F32)
                oretb = [sb(S4, "oretb%d" % i_, [128, 1024], BF16) for i_ in range(2)]
                pj = [ps(S4, "pj4_%d" % i, [128, 512]) for i in range(3)]
                psc = [ps(S4, "psc%d" % i_, [128, 2, 128]) for i_ in range(2)]
                po = [ps(S4, "po%d" % i_, [128, 256]) for i_ in range(2)]
                pTrw = ps(S4, "pTr4", [128, 512], BF16)
                pTr = pTrw[:, 0:128]
                pjr = Ring([0, 1, 2])
                P.dve(lambda e: e.memset(R[:].rearrange("p h e -> p (h e)"), 0.0), [], ["R%d" % h_ for h_ in range(4)])
                P.dve(lambda e: e.memset(Rb[:].rearrange("p h e -> p (h e)"), 0.0), [], ["Rb%d" % h_ for h_ in range(4)])
                xTv = xT.rearrange("(k p) t -> p k t", p=128)
                xTov = xTo.rearrange("(k p) t -> p k t", p=128)

                def rope4(pi, n, tab, tabkey, dst_ap, dst_key):
                    ri = pjr.next()
                    prot = pj[ri]
                    rb = rr4.next()
                    ybf, t1, t2 = ybf2[rb], t12[rb], t22[rb]
                    P.act(lambda e: e.copy(out=ybf[:, 0:n], in_=pj[pi][:, 0:n]), ["pj4_%d" % pi], ["ybf4_%d" % rb])
                    P.pe(lambda e: e.matmul(prot[:, 0:n], lhsT=pt128[:], rhs=ybf[:, 0:n], start=True, stop=True), ["pt128", "ybf4_%d" % rb], ["pj4_%d" % ri])
                    P.dve(lambda e: e.tensor_tensor(out=t1[:, 0:n], in0=pj[pi][:, 0:n], in1=tab[:, 0, 0:n], op=ALU.mult), ["pj4_%d" % pi, tabkey], ["t1_4_%d" % rb])
                    P.dve(lambda e: e.tensor_tensor(out=t2[:, 0:n], in0=prot[:, 0:n], in1=tab[:, 1, 0:n], op=ALU.mult), ["pj4_%d" % ri, tabkey], ["t2_4_%d" % rb])
                    P.pool(lambda e: e.tensor_tensor(out=dst_ap, in0=t1[:, 0:n], in1=t2[:, 0:n], op=ALU.add), ["t1_4_%d" % rb, "t2_4_%d" % rb], [dst_key])

                for gch in range(8):
                    c0 = gch * 512
                    o0 = gch * 256
                    P.dma(xst[:], xTv[:, :, c0:c0 + 512], writes=["wst"])
                    P.act(lambda e: e.copy(out=xb[:, 0:4, :], in_=xst[:, 0:4, :]), ["wst"], ["xb4"])
                    P.dve(lambda e: e.tensor_copy(out=xb[:, 4:8, :], in_=xst[:, 4:8, :]), ["wst"], ["xb4"])
                    P.dma(xst[:, :, 0:256], xTov[:, :, o0:o0 + 256], writes=["wst"])
                    P.act(lambda e: e.copy(out=xob[:, 0:4, :], in_=xst[:, 0:4, 0:256]), ["wst"], ["xob4"])
                    P.dve(lambda e: e.tensor_copy(out=xob[:, 4:8, :], in_=xst[:, 4:8, 0:256]), ["wst"], ["xob4"])
                    P.dma(tabs[:, 0, :], cdr["cosRK"][:, c0:c0 + 512], writes=["tab4"])
                    P.dma(tabs[:, 1, :], cdr["sinRK"][:, c0:c0 + 512], writes=["tab4"])
                    P.dma(tabq[:, 0, :], cdr["cosRQ"][:, o0:o0 + 256], writes=["tabq4"])
                    P.dma(tabq[:, 1, :], cdr["sinRQ"][:, o0:o0 + 256], writes=["tabq4"])
                    for h in range(4):
                        pi = pjr.next()
                        for k in range(8):
                            P.pe(lambda e, k=k, pi=pi, h=h: e.matmul(pj[pi][:], lhsT=W4[:, k, 512 + h * 128:512 + (h + 1) * 128], rhs=xb[:, k, :],
                                                                      start=(k == 0), stop=(k == 7)), ["W4", "xb4"], ["pj4_%d" % pi])
                        rope4(pi, 512, tabs, "tab4", kT[:, h, :], "kT4")
                    for h in range(4):
                        pi = pjr.next()
                        for k in range(8):
                            P.pe(lambda e, k=k, pi=pi, h=h: e.matmul(pj[pi][:, 0:256], lhsT=W4[:, k, h * 128:(h + 1) * 128], rhs=xob[:, k, :],
                                                                      start=(k == 0), stop=(k == 7)), ["W4", "xob4"], ["pj4_%d" % pi])
                        rope4(pi, 256, tabq, "tabq4", qT[:, h, :], "qT4")
                    for pp in range(2):
                        P.dve(lambda e, pp=pp: e.tensor_tensor(out=qxT[:, :, pp * 128:(pp + 1) * 128], in0=qT[:, :, pp * 128:(pp + 1) * 128], in1=xi[:], op=ALU.mult),
                              ["qT4", "xi"], ["qxT4"])
                    for tt in range(4):
                        for hf in range(2):
                            pi = pjr.next()
                            for k in range(8):
                                P.pe(lambda e, k=k, pi=pi, tt=tt, hf=hf: e.matmul(pj[pi][:], lhsT=xb[:, k, tt * 128:(tt + 1) * 128],
                                                                                   rhs=W4[:, k, 1024 + hf * 512:1024 + (hf + 1) * 512],
                                                                                   start=(k == 0), stop=(k == 7)), ["W4", "xb4"], ["pj4_%d" % pi])
                            P.act(lambda e, pi=pi, tt=tt, hf=hf: e.copy(out=vtok[:, tt, hf * 512:(hf + 1) * 512], in_=pj[pi][:]), ["pj4_%d" % pi], ["vtok"])
                    for tt in range(4):
                        for h in range(4):
                            P.pe(lambda e: e.transpose(out=pTrw[:, h * 128:(h + 1) * 128], in_=kT[:, h, tt * 128:(tt + 1) * 128], identity=identb[:]), ["kT4", "identb"], ["pTr4"])
                        P.dve(lambda e: e.tensor_tensor(out=kz[:, tt, :, :], in0=pTrw[:].rearrange("p (h d) -> p h d", h=4),
                                                        in1=zeta[:, tt % 2, :].unsqueeze(2).broadcast_to([128, 4, 128]), op=ALU.mult), ["pTr4", "zeta"], ["kz"])
                    units = [(pp, h) for pp in range(2) for h in range(4)]

                    def phaseA(pp, h, ub):
                        qs = slice(pp * 128, (pp + 1) * 128)
                        for mt in range(2):
                            tt = pp * 2 + mt
                            P.pe(lambda e: e.matmul(psc[ub][:, mt, :], lhsT=kT[:, h, tt * 128:(tt + 1) * 128], rhs=qT[:, h, qs], start=True, stop=True),
                                 ["kT4", "qT4"], ["psc%d" % ub])
                        P.dve(lambda e: e.tensor_tensor(out=sc[ub][:], in0=psc[ub][:], in1=Dc[:, :, h, :], op=ALU.mult), ["psc%d" % ub, "Dc"], ["sc%d" % ub])

                    def phaseBC(pp, h, ub):
                        i = gch * 2 + pp
                        qs = slice(pp * 128, (pp + 1) * 128)
                        hs = slice(h * 256, (h + 1) * 256)
                        ob = oretb[pp]
                        for mt in range(2):
                            tt = pp * 2 + mt
                            P.pe(lambda e: e.matmul(po[ub][:], lhsT=sc[ub][:, mt, :], rhs=vtok[:, tt, hs], start=(mt == 0), stop=False), ["sc%d" % ub, "vtok"], ["po%d" % ub])
                        P.pe(lambda e: e.matmul(po[ub][:], lhsT=qxT[:, h, qs], rhs=Rb[:, h, :], start=False, stop=True), ["qxT4", "Rb%d" % h], ["po%d" % ub])
                        ri = pjr.next()
                        for mt in range(2):
                            tt = pp * 2 + mt
                            P.pe(lambda e: e.matmul(pj[ri][:, 0:256], lhsT=kz[:, tt, h, :], rhs=vtok[:, tt, hs], start=(mt == 0), stop=(mt == 1)),
                                 ["kz", "vtok"], ["pj4_%d" % ri])
                        P.dve(lambda e: e.bn_stats(out=st6[:], in_=po[ub][:]), ["po%d" % ub], ["st6"])
                        P.dve(lambda e: e.bn_aggr(out=mv[:], in_=st6[:]), ["st6"], ["mv"])
                        P.act(lambda e: e.activation(out=rstd[:], in_=mv[:, 1:2], func=AF.Sqrt, bias=epsT[:, 0:1], scale=1.0), ["mv", "epsT"], ["rstd"])
                        P.dve(lambda e: e.reciprocal(out=rstd[:], in_=rstd[:]), ["rstd"], ["rstd"])
                        P.dve(lambda e: e.tensor_scalar(out=ob[:, hs], in0=po[ub][:], scalar1=mv[:, 0:1], scalar2=rstd[:, 0:1], op0=ALU.subtract, op1=ALU.mult),
                              ["po%d" % ub, "mv", "rstd"], ["oretb%d" % pp])
                        P.dve(lambda e: e.scalar_tensor_tensor(out=R[:, h, :], in0=R[:, h, :], scalar=decay256[h], in1=pj[ri][:, 0:256], op0=ALU.mult, op1=ALU.add),
                              ["R%d" % h, "pj4_%d" % ri], ["R%d" % h])
                        P.act(lambda e: e.copy(out=Rb[:, h, :], in_=R[:, h, :]), ["R%d" % h], ["Rb%d" % h])

                    def pair_end(pp, i):
                        ob = oretb[pp]
                        for et in range(8):
                            P.pe(lambda e: e.transpose(out=pTr[:], in_=ob[:, et * 128:(et + 1) * 128], identity=identb[:]), ["oretb%d" % pp, "identb"], ["pTr4"])
                            P.act(lambda e: e.copy(out=oretT[:, et, i * 128:(i + 1) * 128], in_=pTr[:]), ["pTr4"], ["oretT"])

                    phaseA(units[0][0], units[0][1], 0)
                    for u, (pp, h) in enumerate(units):
                        if u + 1 < len(units):
                            phaseA(units[u + 1][0], units[u + 1][1], (u + 1) % 2)
                        phaseBC(pp, h, u % 2)
                        if pending4:
                            pending4.pop(0)()
                        if h == 3:
                            pending4.append(lambda pp=pp, i=gch * 2 + pp: pair_end(pp, i))
                while pending4:
                    pending4.pop(0)()
                dump("oretT", oretT[:].rearrange("p a t -> p (a t)"), [128, 8 * TO], "oretT")
            P.barrier()

        if n_stage >= 5:
            with ExitStack() as S5a:
                Wmg = sb(S5a, "Wmg", [128, 8, 2048], BF16)
                Wa = sb(S5a, "Wa", [128, 4, 1024], BF16)
                Wr = sb(S5a, "Wr", [128, 8, 1024], BF16)
                Wgt = sb(S5a, "Wgt", [128, 8, 1024], BF16)
                load_cast(Wmg[:].rearrange("p k n -> p (k n)"), wmgt, 8 * 2048, "Wmg")
                load_cast(Wa[:].rearrange("p k n -> p (k n)"), wat, 4 * 1024, "Wa")
                load_cast(Wr[:].rearrange("p k n -> p (k n)"), wrt, 8 * 1024, "Wr")
                load_cast(Wgt[:].rearrange("p k n -> p (k n)"), wgtt, 8 * 1024, "Wgt")
                gg8 = sb(S5a, "gg8", [128, 8], F32)
                gb8 = sb(S5a, "gb8", [128, 8], F32)
                P.dma(gg8[:], gng8, writes=["gg8"])
                P.dma(gb8[:], gnb8, writes=["gb8"])
                xb = sb(S5a, "xb5", [128, 8, 512], BF16)
                og = sb(S5a, "og", [128, 8, 512], BF16)
                sgt = [sb(S5a, "sgt%d" % i, [128, 512], F32) for i in range(2)]
                yn = [sb(S5a, "yn%d" % i, [128, 512], F32) for i in range(2)]
                ga2 = [sb(S5a, "ga%d" % i, [128, 512], F32) for i in range(2)]
                gr2 = [sb(S5a, "gr%d" % i, [128, 512], F32) for i in range(2)]
                ma2 = [sb(S5a, "ma%d" % i, [128, 512], F32) for i in range(2)]
                bk = [ps(S5a, "bk%d" % i, [128, 512]) for i in range(8)]
                pgt = [bk[4], bk[5]]
                xTov = xTo.rearrange("(k p) t -> p k t", p=128)
                for oc in range(4):
                    c0 = oc * 512
                    cs_ = slice(c0, c0 + 512)
                    P.dma(wst3[:], xTov[:, :, cs_], writes=["wst"])
                    P.act(lambda e: e.copy(out=xb[:, 0:4, :], in_=wst3[:, 0:4, :]), ["wst"], ["xb5"])
                    P.dve(lambda e: e.tensor_copy(out=xb[:, 4:8, :], in_=wst3[:, 4:8, :]), ["wst"], ["xb5"])
                    for et in range(8):
                        b_ = et % 2
                        for k in range(8):
                            P.pe(lambda e: e.matmul(pgt[b_][:], lhsT=Wgt[:, k, et * 128:(et + 1) * 128], rhs=xb[:, k, :], start=(k == 0), stop=(k == 7)),
                                 ["Wgt", "xb5"], ["bk%d" % (4 + b_)])
                        P.act(lambda e: e.activation(out=sgt[b_][:], in_=pgt[b_][:], func=AF.Silu), ["bk%d" % (4 + b_)], ["sgt%d" % b_])
                        P.act(lambda e: e.activation(out=yn[b_][:], in_=oretT[:, et, cs_], func=AF.Identity, scale=gg8[:, et:et + 1], bias=gb8[:, et:et + 1]),
                              ["oretT", "gg8", "gb8"], ["yn%d" % b_])
                        P.dve(lambda e: e.tensor_tensor(out=og[:, et, :], in0=yn[b_][:], in1=sgt[b_][:], op=ALU.mult), ["yn%d" % b_, "sgt%d" % b_], ["og"])
                    for ct in range(8):
                        cb = (ct % 2) * 4
                        cp = ct % 2
                        pg0, pg1, pu0, pu1 = bk[cb], bk[cb + 1], bk[cb + 2], bk[cb + 3]
                        kg0, kg1, ku0, ku1 = ["bk%d" % (cb + q_) for q_ in range(4)]
                        ga, gr, ma = ga2[cp], gr2[cp], ma2[cp]
                        for k in range(8):
                            P.pe(lambda e: e.matmul(pg0[:], lhsT=Wmg[:, k, ct * 128:(ct + 1) * 128], rhs=xb[:, k, :], start=(k == 0), stop=(k == 7)),
                                 ["Wmg", "xb5"], [kg0])
                        for k in range(8):
                            P.pe(lambda e: e.matmul(pg1[:], lhsT=Wmg[:, k, 1024 + ct * 128:1024 + (ct + 1) * 128], rhs=xb[:, k, :], start=(k == 0), stop=(k == 7)),
                                 ["Wmg", "xb5"], [kg1])
                        for k in range(4):
                            P.pe(lambda e: e.matmul(pu0[:], lhsT=Wa[:, k, ct * 128:(ct + 1) * 128], rhs=oattnT[:, k, cs_], start=(k == 0), stop=(k == 3)),
                                 ["Wa", "oattnT"], [ku0])
                        for k in range(8):
                            P.pe(lambda e: e.matmul(pu1[:], lhsT=Wr[:, k, ct * 128:(ct + 1) * 128], rhs=og[:, k, :], start=(k == 0), stop=(k == 7)),
                                 ["Wr", "og"], [ku1])
                        P.act(lambda e: e.activation(out=ga[:], in_=pg0[:], func=AF.Sigmoid), [kg0], ["ga%d" % cp])
                        P.act(lambda e: e.activation(out=gr[:], in_=pg1[:], func=AF.Sigmoid), [kg1], ["gr%d" % cp])
                        P.dve(lambda e: e.tensor_tensor(out=ma[:], in0=pu0[:], in1=ga[:], op=ALU.mult), [ku0, "ga%d" % cp], ["ma%d" % cp])
                        P.dve(lambda e: e.tensor_tensor(out=gr[:], in0=pu1[:], in1=gr[:], op=ALU.mult), [ku1, "gr%d" % cp], ["gr%d" % cp])
                        P.pool(lambda e: e.tensor_tensor(out=x1T[:, ct, cs_], in0=ma[:], in1=gr[:], op=ALU.add), ["ma%d" % cp, "gr%d" % cp], ["mx%d" % (oc * 4 + t_) for t_ in range(4)])
                dump("mergedT", x1T[:].rearrange("p a t -> p (a t)"), [128, 8 * TO], "mx0")
            P.barrier()
        B_.close()
        A_.close()
        if n_stage >= 5:
            with ExitStack() as S56:
                acc = sb(S56, "acc", [128, 16, 1024], F32)
                lng = sb(S56, "lng", [128, 1024], F32)
                lnb = sb(S56, "lnb", [128, 1024], F32)
                st12 = sb(S56, "st12", [128, 2, 6], F32)
                mv = sb(S56, "mv5", [128, 2], F32)
                rstd = sb(S56, "rstd5", [128, 1], F32)

                def layer_norm(src_ap, src_key, dst_ap, dst_key, tmp_ap, tmp_key):
                    for hf in range(2):
                        P.dve(lambda e: e.bn_stats(out=st12[:, hf, :], in_=src_ap[:, hf * 512:(hf + 1) * 512]), [src_key], ["st12"])
                    P.dve(lambda e: e.bn_aggr(out=mv[:], in_=st12[:].rearrange("p a s -> p (a s)")), ["st12"], ["mv5"])
                    P.dve(lambda e: e.tensor_scalar(out=rstd[:], in0=mv[:, 1:2], scalar1=LN_EPS, scalar2=None, op0=ALU.add), ["mv5"], ["rstd5"])
                    P.act(lambda e: e.activation(out=rstd[:], in_=rstd[:], func=AF.Sqrt), ["rstd5"], ["rstd5"])
                    P.dve(lambda e: e.reciprocal(out=rstd[:], in_=rstd[:]), ["rstd5"], ["rstd5"])
                    P.dve(lambda e: e.tensor_scalar(out=tmp_ap, in0=src_ap, scalar1=mv[:, 0:1], scalar2=rstd[:, 0:1], op0=ALU.subtract, op1=ALU.mult),
                          [src_key, "mv5", "rstd5"], [tmp_key])
                    P.dve(lambda e: e.tensor_tensor(out=tmp_ap, in0=tmp_ap, in1=lng[:], op=ALU.mult), [tmp_key, "lng"], [tmp_key])
                    P.pool(lambda e: e.tensor_tensor(out=dst_ap, in0=tmp_ap, in1=lnb[:], op=ALU.add), [tmp_key, "lnb"], [dst_key])

                with ExitStack() as S5b:
                    Wo = sb(S5b, "Wo", [128, 8, 1024], BF16)
                    load_cast(Wo[:].rearrange("p k n -> p (k n)"), wot, 8 * 1024, "Wo")
                    P.dma(lng[:], ln1g.partition_broadcast(128), writes=["lng"])
                    P.dma(lnb[:], ln1b.partition_broadcast(128), writes=["lnb"])
                    xres2 = [sb(S5b, "xres%d" % i_, [128, 1024], F32) for i_ in range(2)]
                    yt2 = [sb(S5b, "yt%d" % i_, [128, 1024], F32) for i_ in range(2)]
                    x12 = [sb(S5b, "x1_%d" % i_, [128, 1024], F32) for i_ in range(2)]
                    x1b2 = [sb(S5b, "x1b%d" % i_, [128, 1024], BF16) for i_ in range(2)]
                    pm2 = [[ps(S5b, "pm%d_%d" % (q_, i_), [128, 512]) for i_ in range(2)] for q_ in range(2)]
                    pTr = ps(S5b, "pTr5", [128, 128], BF16)
                    pend5 = []
                    for i in range(16):
                        q_ = i % 2
                        xres, yt, x1, x1b, pm = xres2[q_], yt2[q_], x12[q_], x1b2[q_], pm2[q_]
                        ts_ = slice(i * 128, (i + 1) * 128)
                        if len(pend5) >= 2:
                            pend5.pop(0)()
                        P.dma(xres[:], xo[ts_, :], writes=["xres%d" % q_])
                        for hf in range(2):
                            for k in range(8):
                                P.pe(lambda e: e.matmul(pm[hf][:], lhsT=x1T[:, k, ts_], rhs=Wo[:, k, hf * 512:(hf + 1) * 512], start=(k == 0), stop=(k == 7)),
                                     ["mx%d" % i, "Wo"], ["pm%d_%d" % (q_, hf)])
                            P.dve(lambda e: e.scalar_tensor_tensor(out=yt[:, hf * 512:(hf + 1) * 512], in0=xres[:, hf * 512:(hf + 1) * 512], scalar=ALPHA,
                                                                   in1=pm[hf][:], op0=ALU.mult, op1=ALU.add), ["xres%d" % q_, "pm%d_%d" % (q_, hf)], ["yt%d" % q_])
                        layer_norm(yt[:], "yt%d" % q_, x1[:], "x1_%d" % q_, yt[:], "yt%d" % q_)
                        if ("x1_%d" % i) in debug:
                            dump("x1_%d" % i, x1[:], [128, 1024], "x1_%d" % q_)
                        P.pool(lambda e: e.tensor_copy(out=x1b[:], in_=x1[:]), ["x1_%d" % q_], ["x1b%d" % q_])
                        P.pool(lambda e: e.tensor_scalar(out=acc[:, i, :], in0=x1[:], scalar1=ALPHA, scalar2=None, op0=ALU.mult), ["x1_%d" % q_], ["acc%d" % i])

                        def tr5(i=i, q_=q_, x1b=x1b, ts_=ts_):
                            for dt_ in range(8):
                                P.pe(lambda e: e.transpose(out=pTr[:], in_=x1b[:, dt_ * 128:(dt_ + 1) * 128], identity=identb[:]), ["x1b%d" % q_, "identb"], ["pTr5"])
                                P.act(lambda e: e.copy(out=x1T[:, dt_, ts_], in_=pTr[:]), ["pTr5"], ["mx%d" % i])
                        pend5.append(tr5)
                    while pend5:
                        pend5.pop(0)()
                P.barrier()
                if n_stage >= 6:
                    with ExitStack() as S6:
                        P.dma(lng[:], ln2g.partition_broadcast(128), writes=["lng"])
                        P.dma(lnb[:], ln2b.partition_broadcast(128), writes=["lnb"])
                        comb = sb(S6, "comb", [128, 16, 32], F32)
                        wrf = sb(S6, "wrf", [128, 8, 36], F32)
                        wrb = sb(S6, "wrb", [128, 8, 36], BF16)
                        brb = sb(S6, "brb", [128, 36], F32)
                        P.dma(wrf[:].rearrange("p k n -> p (k n)"), wrout, writes=["wrf"])
                        P.dve(lambda e: e.tensor_copy(out=wrb[:], in_=wrf[:]), ["wrf"], ["wrb"])
                        P.dma(brb[:], brout.partition_broadcast(128), writes=["brb"])
                        lg = sb(S6, "lg", [128, 16, 36], F32)
                        gmx = sb(S6, "gmx", [128, 16], F32)
                        gsh = sb(S6, "gsh", [128, 16, 4], F32)
                        gex = sb(S6, "gex", [128, 16, 4], F32)
                        gsum = sb(S6, "gsum", [128, 16], F32)
                        gprob = sb(S6, "gprob", [128, 16], F32)
                        ohg = sb(S6, "ohg", [128, 16, 4], F32)
                        tmp48 = sb(S6, "tmp48", [128, 16, 4, 8], F32)
                        isel = sb(S6, "isel", [128, 16, 8], F32)
                        isel2 = sb(S6, "isel2", [128, 16, 8], F32)
                        eq0 = sb(S6, "eq0", [128, 16, 8], F32)
                        eq1 = sb(S6, "eq1", [128, 16, 8], F32)
                        m0 = sb(S6, "m0r", [128, 16], F32)
                        m1 = sb(S6, "m1r", [128, 16], F32)
                        dlt = sb(S6, "dlt", [128, 16], F32)
                        w2e = sb(S6, "w2e", [128, 16], F32)
                        wsum = sb(S6, "wsum", [128, 16], F32)
                        wt1 = sb(S6, "wt1", [128, 16], F32)
                        wt2 = sb(S6, "wt2", [128, 16], F32)
                        ce = sb(S6, "ce", [128, 16, 8], F32)
                        ce2 = sb(S6, "ce2", [128, 16, 8], F32)
                        SR = ExitStack()
                        plg = [ps(SR, "plg%d" % i_, [128, 8, 36]) for i_ in range(2)]
                        AXX = mybir.AxisListType.X

                        def b3(ap, n):
                            return ap.unsqueeze(2).broadcast_to([128, 16, n])
                        for i in range(16):
                            ts_ = slice(i * 128, (i + 1) * 128)
                            for k in range(8):
                                P.pe(lambda e: e.matmul(plg[i // 8][:, i % 8, :], lhsT=x1T[:, k, ts_], rhs=wrb[:, k, :], start=(k == 0), stop=(k == 7)),
                                     ["mx%d" % i, "wrb"], ["plg%d" % (i // 8)])
                        for hf in range(2):
                            P.dve(lambda e: e.tensor_tensor(out=lg[:, hf * 8:(hf + 1) * 8, :], in0=plg[hf][:], in1=brb[:].unsqueeze(1).broadcast_to([128, 8, 36]), op=ALU.add),
                                  ["plg%d" % hf, "brb"], ["lg"])
                        P.dve(lambda e: e.tensor_reduce(out=gmx[:], in_=lg[:, :, 0:4], axis=AXX, op=ALU.max), ["lg"], ["gmx"])
                        P.dve(lambda e: e.tensor_tensor(out=gsh[:], in0=lg[:, :, 0:4], in1=b3(gmx[:], 4), op=ALU.subtract), ["lg", "gmx"], ["gsh"])
                        P.act(lambda e: e.activation(out=gex[:].rearrange("p t g -> p (t g)"), in_=gsh[:].rearrange("p t g -> p (t g)"), func=AF.Exp), ["gsh"], ["gex"])
                        P.dve(lambda e: e.tensor_reduce(out=gsum[:], in_=gex[:], axis=AXX, op=ALU.add), ["gex"], ["gsum"])
                        P.dve(lambda e: e.reciprocal(out=gprob[:], in_=gsum[:]), ["gsum"], ["gprob"])
                        P.dve(lambda e: e.tensor_scalar(out=ohg[:].rearrange("p t g -> p (t g)"), in0=gsh[:].rearrange("p t g -> p (t g)"), scalar1=0.0, scalar2=None, op0=ALU.is_ge),
                              ["gsh"], ["ohg"])
                        P.dve(lambda e: e.tensor_tensor(out=tmp48[:], in0=lg[:, :, 4:36].rearrange("p t (g e) -> p t g e", g=4),
                                                        in1=ohg[:].unsqueeze(3).broadcast_to([128, 16, 4, 8]), op=ALU.mult), ["lg", "ohg"], ["tmp48"])
                        P.dve(lambda e: e.tensor_reduce(out=isel[:], in_=tmp48[:].rearrange("p t g e -> p t e g"), axis=AXX, op=ALU.add), ["tmp48"], ["isel"])
                        P.dve(lambda e: e.tensor_reduce(out=m0[:], in_=isel[:], axis=AXX, op=ALU.max), ["isel"], ["m0r"])
                        P.dve(lambda e: e.tensor_tensor(out=eq0[:], in0=isel[:], in1=b3(m0[:], 8), op=ALU.is_equal), ["isel", "m0r"], ["eq0"])
                        P.dve(lambda e: e.scalar_tensor_tensor(out=isel2[:].rearrange("p t e -> p (t e)"), in0=eq0[:].rearrange("p t e -> p (t e)"), scalar=-1.0e30,
                                                               in1=isel[:].rearrange("p t e -> p (t e)"), op0=ALU.mult, op1=ALU.add), ["eq0", "isel"], ["isel2"])
                        P.dve(lambda e: e.tensor_reduce(out=m1[:], in_=isel2[:], axis=AXX, op=ALU.max), ["isel2"], ["m1r"])
                        P.dve(lambda e: e.tensor_tensor(out=eq1[:], in0=isel2[:], in1=b3(m1[:], 8), op=ALU.is_equal), ["isel2", "m1r"], ["eq1"])
                        P.dve(lambda e: e.tensor_tensor(out=dlt[:], in0=m1[:], in1=m0[:], op=ALU.subtract), ["m1r", "m0r"], ["dlt"])
                        P.act(lambda e: e.activation(out=w2e[:], in_=dlt[:], func=AF.Exp), ["dlt"], ["w2e"])
                        P.dve(lambda e: e.tensor_scalar(out=wsum[:], in0=w2e[:], scalar1=1.0, scalar2=None, op0=ALU.add), ["w2e"], ["wsum"])
                        P.dve(lambda e: e.reciprocal(out=wsum[:], in_=wsum[:]), ["wsum"], ["wsum"])
                        P.dve(lambda e: e.tensor_tensor(out=wt1[:], in0=wsum[:], in1=gprob[:], op=ALU.mult), ["wsum", "gprob"], ["wt1"])
                        P.dve(lambda e: e.tensor_tensor(out=wt2[:], in0=wt1[:], in1=w2e[:], op=ALU.mult), ["wt1", "w2e"], ["wt2"])
                        P.dve(lambda e: e.tensor_tensor(out=ce[:], in0=eq0[:], in1=b3(wt1[:], 8), op=ALU.mult), ["eq0", "wt1"], ["ce"])
                        P.dve(lambda e: e.tensor_tensor(out=ce2[:], in0=eq1[:], in1=b3(wt2[:], 8), op=ALU.mult), ["eq1", "wt2"], ["ce2"])
                        P.dve(lambda e: e.tensor_tensor(out=ce[:], in0=ce[:], in1=ce2[:], op=ALU.add), ["ce", "ce2"], ["ce"])
                        P.dve(lambda e: e.tensor_tensor(out=comb[:].rearrange("p t (g e) -> p t g e", g=4), in0=ce[:].unsqueeze(2).broadcast_to([128, 16, 4, 8]),
                                                        in1=ohg[:].unsqueeze(3).broadcast_to([128, 16, 4, 8]), op=ALU.mult), ["ce", "ohg"], ["comb"])
                        dump("comb", comb[:].rearrange("p a e -> p (a e)"), [128, 512], "comb")
                        SR.close()
                        P.barrier()
                        wstE = [wst[:, 0:2048], wst[:, 2048:4096]]
                        wE = [sb(S6, "wE%d" % i, [128, 12288], BF16) for i in range(2)]
                        sgE = [sb(S6, "sgE%d" % i, [128, 512], F32) for i in range(2)]
                        hT = [sb(S6, "hT%d" % i, [128, 4, 512], BF16) for i in range(2)]
                        pG = [ps(S6, "pG%d" % i, [128, 512]) for i in range(2)]
                        pU = [ps(S6, "pU%d" % i, [128, 512]) for i in range(2)]
                        pO = [ps(S6, "pO%d" % i, [128, 512]) for i in range(3)]
                        wsr = Ring([0, 1])
                        gr_ = Ring([0, 1])
                        or_ = Ring([0, 1, 2])
                        crr = [0]
                        n_exp = DEBUG.get("n_exp", 32)

                        def load_expert(ex):
                            ws = ex % 2
                            for pc in range(6):
                                si = wsr.next()
                                P.dma(wstE[si], wexp[ex, :, pc * 2048:(pc + 1) * 2048], writes=["wstE%d" % si])
                                d = wE[ws][:, pc * 2048:(pc + 1) * 2048]
                                P.pool(lambda e: e.tensor_copy(out=d, in_=wstE[si]), ["wstE%d" % si], ["wE%d" % ws])

                        def gu_phase(ex, cth):
                            ws = ex % 2
                            Wg = wE[ws][:, 0:4096].rearrange("p (k n) -> p k n", k=8)
                            Wu = wE[ws][:, 4096:8192].rearrange("p (k n) -> p k n", k=8)
                            wkey = "wE%d" % ws
                            cs_ = slice(cth * 512, (cth + 1) * 512)
                            xkeys = ["mx%d" % (cth * 4 + t_) for t_ in range(4)]
                            hs_ = cth % 2
                            for ft in range(4):
                                gi = gr_.next()
                                for k in range(8):
                                    P.pe(lambda e: e.matmul(pG[gi][:], lhsT=Wg[:, k, ft * 128:(ft + 1) * 128], rhs=x1T[:, k, cs_], start=(k == 0), stop=(k == 7)),
                                         [wkey] + xkeys, ["pG%d" % gi])
                                for k in range(8):
                                    P.pe(lambda e: e.matmul(pU[gi][:], lhsT=Wu[:, k, ft * 128:(ft + 1) * 128], rhs=x1T[:, k, cs_], start=(k == 0), stop=(k == 7)),
                                         [wkey] + xkeys, ["pU%d" % gi])
                                P.act(lambda e: e.activation(out=sgE[gi][:], in_=pG[gi][:], func=AF.Silu), ["pG%d" % gi], ["sgE%d" % gi])
                                P.dve(lambda e: e.tensor_tensor(out=hT[hs_][:, ft, :], in0=pU[gi][:], in1=sgE[gi][:], op=ALU.mult),
                                      ["pU%d" % gi, "sgE%d" % gi], ["hT%d" % hs_])

                        def d_phase(ex, cth):
                            ws = ex % 2
                            Wd = wE[ws][:, 8192:12288].rearrange("p (k n) -> p k n", k=4)
                            wkey = "wE%d" % ws
                            hs_ = cth % 2
                            for tt in range(4):
                                ti = cth * 4 + tt
                                for hf in range(2):
                                    oi = or_.next()
                                    for ft in range(4):
                                        P.pe(lambda e: e.matmul(pO[oi][:], lhsT=hT[hs_][:, ft, tt * 128:(tt + 1) * 128],
                                                                rhs=Wd[:, ft, hf * 512:(hf + 1) * 512], start=(ft == 0), stop=(ft == 3)),
                                             [wkey, "hT%d" % hs_], ["pO%d" % oi])
                                    a_ = acc[:, ti, hf * 512:(hf + 1) * 512]
                                    P.dve(lambda e: e.scalar_tensor_tensor(out=a_, in0=pO[oi][:], scalar=comb[:, ti, ex:ex + 1], in1=a_, op0=ALU.mult, op1=ALU.add),
                                          ["pO%d" % oi, "comb", "acc%d" % ti], ["acc%d" % ti])

                        units = [(ex, cth) for ex in range(n_exp) for cth in range(4)]
                        load_expert(0)
                        if n_exp > 1:
                            load_expert(1)
                        gu_phase(*units[0])
                        for u, (ex, cth) in enumerate(units):
                            if u + 1 < len(units):
                                gu_phase(*units[u + 1])
                            d_phase(ex, cth)
                            if cth == 3 and ex + 2 < n_exp:
                                load_expert(ex + 2)
                        yo = [sb(S6, "yo%d" % i, [128, 1024], F32) for i in range(2)]
                        for i in range(16):
                            yi = i % 2
                            layer_norm(acc[:, i, :], "acc%d" % i, yo[yi][:], "yo%d" % yi, yo[yi][:], "yo%d" % yi)
                            P.dma(out[i * 128:(i + 1) * 128, :], yo[yi][:], reads=["yo%d" % yi], writes=["out%d" % yi], sem="outd%d" % yi)
        fin_reads = ["out0", "out1"] + ["dbg_" + n for n in dbg_out]
        P.add("sp", lambda e: e.nop(), reads=fin_reads, sem="fin")
        if n_stage < 6:
            pass
        cnt = P.emit(G)
        nsem = len(cnt)
    return nc, dbg_out, nsem


def prep_shared(inp):
    f = lambda a: np.ascontiguousarray(np.asarray(a, dtype=np.float32))
    w_in = f(inp["w_in"])[0]
    sh = {}
    q = w_in[:, 0:512].reshape(1024, 2, 4, 64).transpose(0, 2, 1, 3).reshape(1024, 512)
    w1 = np.concatenate([q, w_in[:, 512:640], w_in[:, 640:768], w_in[:, 768:896], w_in[:, 1024:1152],
                         w_in[:, 896:1024], w_in[:, 1152:1280], w_in[:, 1280:1304]], axis=1)
    sh["w1t"] = tile_w(w1)
    sh["w4t"] = tile_w(w_in[:, 1304:3352])
    sh["wgtt"] = tile_w(w_in[:, 3352:4376])
    sh["wmgt"] = tile_w(w_in[:, 4376:6424])
    lit = {"k": (inp["cmp_k_w1"], inp["cmp_k_b1"], inp["cmp_pos_k"]), "v": (inp["cmp_v_w1"], inp["cmp_v_b1"], inp["cmp_pos_v"])}
    for kv in "kv":
        cw1 = f(lit[kv][0])[0]
        r = cw1.reshape(32, 64, 256).transpose(1, 0, 2).reshape(64, 32 * 256)
        sh["cw1" + kv] = np.ascontiguousarray(np.concatenate([r, r], axis=0))
        pos = f(lit[kv][2])[0]
        sh["cpos" + kv] = np.ascontiguousarray(np.concatenate([pos.T, pos.T], axis=0))
        sh["cb1" + kv] = np.ascontiguousarray(f(lit[kv][1])[0].reshape(2, 128).T)
    w2k = f(inp["cmp_k_w2"])[0]
    sh["cw2k"] = tile_w(np.concatenate([w2k, w2k], axis=1))
    sh["cw2v"] = tile_w(f(inp["cmp_v_w2"])[0])
    b2k = f(inp["cmp_k_b2"])[0]
    sh["cb2k"] = np.ascontiguousarray(np.concatenate([b2k, b2k])[:, None])
    sh["cb2v"] = f(inp["cmp_v_b2"])[0]
    sh["gng8"] = np.ascontiguousarray(f(inp["ret_gn_g"])[0].reshape(8, 128).T)
    sh["gnb8"] = np.ascontiguousarray(f(inp["ret_gn_b"])[0].reshape(8, 128).T)
    sh["wat"] = tile_w(f(inp["w_up_attn"])[0])
    sh["wrt"] = tile_w(f(inp["w_up_ret"])[0])
    sh["wot"] = tile_w(f(inp["w_out"])[0])
    for n in ("ln1_g", "ln1_b", "ln2_g", "ln2_b"):
        sh[n.replace("_", "")] = f(inp[n])[0]
    rg = f(inp["router_group_w"])[0]
    ri = f(inp["router_inner_w"])[0]
    sh["wrout"] = tile_w(np.concatenate([rg, ri.transpose(1, 0, 2).reshape(1024, 32)], axis=1))
    sh["brout"] = np.ascontiguousarray(np.concatenate([f(inp["router_group_b"])[0], f(inp["router_inner_b"])[0].reshape(32)]))
    wg = f(inp["expert_w_gate"])[0]
    wu = f(inp["expert_w_up"])[0]
    wd = f(inp["expert_w_down"])[0]
    we = np.empty((32, 128, 12288), np.float32)
    we[:, :, 0:4096] = wg.reshape(32, 8, 128, 512).transpose(0, 2, 1, 3).reshape(32, 128, 4096)
    we[:, :, 4096:8192] = wu.reshape(32, 8, 128, 512).transpose(0, 2, 1, 3).reshape(32, 128, 4096)
    we[:, :, 8192:12288] = wd.reshape(32, 4, 128, 1024).transpose(0, 2, 1, 3).reshape(32, 128, 4096)
    sh["wexp"] = we
    return sh


def make_in_maps(inp):
    sh = prep_shared(inp)
    x = np.asarray(inp["x"], dtype=np.float32)
    maps = []
    for core in range(8):
        b, c = core // 2, core % 2
        m = dict(sh)
        xb = x[b]
        own = xb.reshape(16, 2, 128, 1024)[:, c].reshape(TO, 1024)
        m["xT"] = np.ascontiguousarray(xb.T)
        m["xTo"] = np.ascontiguousarray(own.T)
        m["xo"] = np.ascontiguousarray(own)
        for k, v in make_consts(c).items():
            if not k.startswith("_"):
                m["c_" + k] = v
        maps.append(m)
    return maps


_PROG_CACHE = {}


def kernel(**inputs):
    if "prog" not in _PROG_CACHE:
        _PROG_CACHE["prog"] = build_program()
    nc, _, _ = _PROG_CACHE["prog"]
    maps = make_in_maps(inputs)
    res = run_bass_kernel_spmd(nc, maps, core_ids=list(range(8)))
    outp = np.empty((4, 16, 2, 128, 1024), np.float32)
    for core in range(8):
        b, c = core // 2, core % 2
        outp[b, :, c] = res.results[core]["out"].reshape(16, 128, 1024)
    return outp.reshape(4, T, 1024)
```

```python
import numpy as np
import ml_dtypes
import concourse.bass as bass
import concourse.mybir as mybir
from concourse.bass_utils import run_bass_kernel_spmd
from contextlib import ExitStack

F32 = mybir.dt.float32
BF16 = mybir.dt.bfloat16
AF = mybir.ActivationFunctionType
ALU = mybir.AluOpType
NPBF = ml_dtypes.bfloat16

T = 4096
D = 1024
TO = 2048
NEGM = -30000.0
LN_EPS = 1e-5
ALPHA = 2.0 ** 0.25
DEBUG = {}


class Op:
    __slots__ = ("eng", "fn", "reads", "writes", "dma", "sem", "deps", "needs_inc", "idx", "id", "extra")

    def __init__(self, eng, fn, reads, writes, dma, sem):
        self.eng = eng
        self.fn = fn
        self.reads = tuple(reads)
        self.writes = tuple(writes)
        self.dma = dma
        self.sem = sem
        self.deps = []
        self.needs_inc = dma
        self.idx = 0
        self.extra = ()


class _Rec:
    def __getattr__(self, name):
        return lambda *a, **k: (name, a, k)


_REC = _Rec()


class Prog:
    ENGS = ("pe", "act", "dve", "pool", "sp")

    def __init__(self, nc, same_eng_sync=True):
        self.nc = nc
        self.ops = []
        self.same_eng_sync = same_eng_sync
        self.last_by_sem = {}
        self.psum_keys = set()

    def add(self, eng, fn, reads=(), writes=(), dma=False, sem=None):
        lim = DEBUG.get("max_ops")
        self.nadd = getattr(self, "nadd", -1) + 1
        if (lim is not None and self.nadd >= lim and sem not in ("dbg", "fin")) or self.nadd in DEBUG.get("skip", ()):
            return Op(eng, None, reads, writes, dma, sem)
        if dma and sem is None:
            sem = "dma_" + str(writes[0])
        if not dma:
            sem = "eng_" + eng
        op = Op(eng, fn(_REC), reads, writes, dma, sem)
        if DEBUG.get("trace_ops"):
            print(len(self.ops), eng, op.fn[0], reads, writes)
        op.id = len(self.ops)
        self.ops.append(op)
        self.last_by_sem[sem] = op
        return op

    def pe(self, fn, reads=(), writes=()):
        return self.add("pe", fn, reads, writes)

    def act(self, fn, reads=(), writes=()):
        return self.add("act", fn, reads, writes)

    def dve(self, fn, reads=(), writes=()):
        return self.add("dve", fn, reads, writes)

    def pool(self, fn, reads=(), writes=()):
        return self.add("pool", fn, reads, writes)

    def dma(self, out, in_, reads=(), writes=(), sem=None, q="sp", **kw):
        return self.add(q, lambda e: e.dma_start(out=out, in_=in_, **kw), reads, writes, dma=True, sem=sem)

    def barrier(self):
        lasts = list(self.last_by_sem.values())
        for eng in self.ENGS:
            op = self.add(eng, lambda e: e.nop())
            op.extra = tuple(lasts)
        self.last_by_sem = {k: v for k, v in self.last_by_sem.items() if k.startswith("eng_")}

    def analyze(self):
        state = {}
        for op in self.ops:
            deps = set(op.extra)
            for k in op.reads:
                st = state.get(k)
                if st:
                    deps.update(st[0])
                    if k in self.psum_keys:
                        deps.update(r for r in st[1] if r.eng != op.eng)
            for k in op.writes:
                st = state.get(k)
                if st is None:
                    st = state[k] = [[], []]
                if st[1]:
                    deps.update(st[1])
                    deps.update(st[0])
                    st[0] = [op]
                    st[1] = []
                else:
                    same_group = op.dma and all(w.dma and w.sem == op.sem for w in st[0])
                    if same_group:
                        st[0].append(op)
                    else:
                        deps.update(st[0])
                        st[0] = [op]
            for k in op.reads:
                st = state.get(k)
                if st is None:
                    st = state[k] = [[], []]
                st[1].append(op)
            deps.discard(op)
            red = {}
            for d in deps:
                if (not d.dma) and (not op.dma) and d.eng == op.eng:
                    if op.eng == "pe" or not self.same_eng_sync:
                        continue
                cur = red.get(d.sem)
                if cur is None or d.id > cur.id:
                    red[d.sem] = d
            op.deps = list(red.values())
            for d in op.deps:
                d.needs_inc = True
        cnt = {}
        for op in self.ops:
            if op.needs_inc:
                cnt[op.sem] = cnt.get(op.sem, 0) + 1
                op.idx = cnt[op.sem]
        self.sem_names = sorted(cnt.keys())
        return cnt

    def emit(self, stack):
        nc = self.nc
        cnt = self.analyze()
        sems = {}
        for name in self.sem_names:
            sems[name] = stack.enter_context(nc.semaphore(name))
        block = stack.enter_context(nc.Block())
        per_eng = {e: [o for o in self.ops if o.eng == e] for e in self.ENGS}

        def run(eng_obj, ops):
            known = {}
            for op in ops:
                for d in op.deps:
                    val = d.idx * (16 if d.dma else 1)
                    if known.get(d.sem, 0) < val:
                        eng_obj.wait_ge(sems[d.sem], val)
                        known[d.sem] = val
                name, a, k = op.fn
                inst = getattr(eng_obj, name)(*a, **k)
                if op.needs_inc:
                    inst.then_inc(sems[op.sem], 16 if op.dma else 1)

        @block.sync
        def _(e):
            run(e, per_eng["sp"])

        @block.tensor
        def _(e):
            run(e, per_eng["pe"])

        @block.scalar
        def _(e):
            run(e, per_eng["act"])

        @block.vector
        def _(e):
            run(e, per_eng["dve"])

        @block.gpsimd
        def _(e):
            run(e, per_eng["pool"])
        return cnt


class Ring:
    def __init__(self, items):
        self.items = items
        self.i = 0

    def next(self):
        it = self.items[self.i % len(self.items)]
        self.i += 1
        return it


def tile_w(w):
    K, N = w.shape
    return np.ascontiguousarray(w.reshape(K // 128, 128, N).transpose(1, 0, 2).reshape(128, -1))


def rope_tabs(pos, d, scale):
    half = d // 2
    inv = 10000.0 ** (-np.arange(half, dtype=np.float64) * 2.0 / d)
    ang = pos.astype(np.float64)[None, :] * inv[:, None]
    cos = np.cos(ang) * scale
    sin = np.sin(ang) * scale
    reps = 128 // half
    return (np.tile(cos, (reps, 1)).astype(np.float32), np.tile(sin, (reps, 1)).astype(np.float32))


def rot_lhsT(d):
    half = d // 2
    Pm = np.zeros((128, 128), np.float32)
    for blk in range(128 // d):
        o = blk * d
        for m in range(half):
            Pm[o + m, o + m + half] = -1.0
            Pm[o + m + half, o + m] = 1.0
    return np.ascontiguousarray(Pm.T)


_CONST_CACHE = {}


def make_consts(c):
    if c in _CONST_CACHE:
        return _CONST_CACHE[c]
    cs = {}
    own_pos = np.concatenate([np.arange(128) + (2 * i + c) * 128 for i in range(16)])
    allpos = np.arange(T)
    cs["cosK"], cs["sinK"] = rope_tabs(allpos, 64, 1.0)
    cs["cosQ"], cs["sinQ"] = rope_tabs(own_pos, 64, 0.125)
    cs["cosRK"], cs["sinRK"] = rope_tabs(allpos, 128, 128.0 ** -0.5)
    cs["cosRQ"], cs["sinRQ"] = rope_tabs(own_pos, 128, 1.0)
    cend = np.arange(256) * 16 + 31
    cs["cosC"], cs["sinC"] = rope_tabs(cend, 64, 1.0)
    cs["pt64"] = rot_lhsT(64).astype(NPBF)
    cs["pt128"] = rot_lhsT(128).astype(NPBF)
    cs["identb"] = np.eye(128, dtype=np.float32).astype(NPBF)
    E = np.zeros((128, 32, 128), np.float32)
    for j in range(32):
        for k in range(128):
            E[2 * j + k // 64, j, k] = 1.0
            E[64 + 2 * j + k // 64, j, k] = 1.0
    cs["eall"] = E.reshape(128, -1).astype(NPBF)
    wm = np.zeros((128, 6, 128), np.float32)
    kk = np.arange(128)[:, None]
    tt = np.arange(128)[None, :]
    for r in range(6):
        dj = (r - 4) - c
        tk = dj * 128 + kk
        ok = (tk <= tt) & (tt - tk < 512)
        wm[:, r, :] = np.where(ok, 0.0, NEGM)
    cs["wmask"] = wm.reshape(128, -1).astype(NPBF)
    cm = np.zeros((128, 2, 16, 128), np.float32)
    for a in range(2):
        for i in range(16):
            G = 2 * i + c
            n = a * 128 + kk
            t = G * 128 + tt
            cm[:, a, i, :] = np.where(16 * n + 31 <= t, 0.0, NEGM)
    cs["cmpmask"] = cm.reshape(128, -1).astype(NPBF)
    cstart = np.arange(255) * 16
    sstart = np.arange(64) * 64
    ov = np.clip(np.minimum(cstart[None, :] + 32, sstart[:, None] + 64) - np.maximum(cstart[None, :], sstart[:, None]), 0, None) / 16.0
    ovT = np.zeros((256, 64), np.float32)
    ovT[:255] = ov.T
    cs["ovT"] = np.ascontiguousarray(ovT.reshape(2, 128, 64).transpose(1, 0, 2).reshape(128, -1)).astype(NPBF)
    tkm = np.zeros((128, 16, 64), np.float32)
    tkb = np.zeros((128, 16, 64), np.float32)
    for i in range(16):
        G = 2 * i + c
        for p in range(128):
            bt = (G * 128 + p) // 64
            for s in range(64):
                if s == 0:
                    tkb[p, i, s] = 1e9
                elif s == bt:
                    tkb[p, i, s] = 2e9
                elif s == bt - 1:
                    tkb[p, i, s] = 3e9
                elif s <= bt:
                    tkm[p, i, s] = 1.0
                else:
                    tkb[p, i, s] = -1e9 - 1e6 * s
    cs["tkm"] = tkm.reshape(128, -1)
    cs["tkb"] = tkb.reshape(128, -1)
    gam = 1.0 - 2.0 ** (-5.0 - np.arange(4, dtype=np.float64))
    lg = np.log(gam)
    m = np.arange(256)[:, None]
    cq = np.arange(128)[None, :]
    qq = 128 * c + cq
    Dc = np.zeros((128, 2, 4, 128), np.float32)
    for h in range(4):
        dd = np.where(qq >= m, np.exp(np.maximum(qq - m, 0) * lg[h]), 0.0)
        Dc[:, :, h, :] = dd.reshape(2, 128, 128).transpose(1, 0, 2)
    cs["Dc"] = Dc.reshape(128, -1)
    xi = np.zeros((128, 4, 128), np.float32)
    for h in range(4):
        xi[:, h, :] = np.exp((qq + 1.0) * lg[h])
    cs["xi"] = xi.reshape(128, -1)
    zt = np.zeros((128, 2, 4), np.float32)
    for h in range(4):
        zt[:, :, h] = np.exp((255.0 - np.arange(256)) * lg[h]).reshape(2, 128).T
    cs["zeta"] = zt.reshape(128, -1)
    cs["_decay256"] = [float(np.exp(256.0 * lg[h])) for h in range(4)]
    _CONST_CACHE[c] = cs
    return cs


CONST_SHAPES = None


def build_program(n_stage=6, debug=()):
    nc = bass.Bass("TRN2", target_bir_lowering=False)
    cs0 = make_consts(0)
    dram = {}

    def din(name, shape, dt=F32):
        dram[name] = nc.dram_tensor(name, list(shape), dt, kind="ExternalInput").ap()
        return dram[name]

    xT = din("xT", [1024, T])
    xTo = din("xTo", [1024, TO])
    xo = din("xo", [TO, 1024])
    w1t = din("w1t", [128, 8 * 1304])
    w4t = din("w4t", [128, 8 * 2048])
    wmgt = din("wmgt", [128, 8 * 2048])
    cw1 = {kv: din("cw1" + kv, [128, 32 * 256]) for kv in "kv"}
    cpos = {kv: din("cpos" + kv, [128, 32]) for kv in "kv"}
    cb1 = {kv: din("cb1" + kv, [128, 2]) for kv in "kv"}
    cw2k = din("cw2k", [128, 2 * 128])
    cw2v = din("cw2v", [128, 2 * 64])
    cb2k = din("cb2k", [128, 1])
    cb2v = din("cb2v", [64])
    gng8 = din("gng8", [128, 8])
    gnb8 = din("gnb8", [128, 8])
    wgtt = din("wgtt", [128, 8 * 1024])
    wat = din("wat", [128, 4 * 1024])
    wrt = din("wrt", [128, 8 * 1024])
    wot = din("wot", [128, 8 * 1024])
    ln1g = din("ln1g", [1024])
    ln1b = din("ln1b", [1024])
    ln2g = din("ln2g", [1024])
    ln2b = din("ln2b", [1024])
    wrout = din("wrout", [128, 8 * 36])
    brout = din("brout", [36])
    wexp = din("wexp", [32, 128, 12288])
    cdr = {}
    for k, v in cs0.items():
        if k.startswith("_"):
            continue
        cdr[k] = din("c_" + k, v.shape, BF16 if v.dtype == NPBF else F32)
    out = nc.dram_tensor("out", [TO, 1024], F32, kind="ExternalOutput").ap()
    dbg_out = {}

    decay256 = cs0["_decay256"]

    with ExitStack() as G:
        P = Prog(nc)

        def sb(stack, name, shape, dt):
            return stack.enter_context(nc.sbuf_tensor(name, list(shape), dt))

        def ps(stack, name, shape, dt=F32):
            P.psum_keys.add(name)
            ncol = 512 if dt == F32 else 1024
            full = stack.enter_context(nc.psum_tensor(name, [128, ncol], dt))
            n = 1
            for d_ in shape[1:]:
                n *= d_
            v = full[0:shape[0], 0:n]
            if len(shape) == 3:
                v = v.rearrange("p (a b) -> p a b", a=shape[1])
            return v

        def dump(name, ap, shape, key):
            if name in debug:
                t = nc.dram_tensor("dbg_" + name, list(shape), ap.dtype, kind="ExternalOutput").ap()
                dbg_out[name] = t
                P.dma(t, ap, reads=[key], writes=["dbg_" + name], sem="dbg")

        identb = sb(G, "identb", [128, 128], BF16)
        P.dma(identb[:], cdr["identb"], writes=["identb"])
        wst = sb(G, "wst", [128, 4096], F32)
        cast_rr = [0]

        def load_cast(dst_ap, src_ap, n, dst_key, shape3=None):
            o = 0
            while o < n:
                m = min(4096, n - o)
                P.dma(wst[:, 0:m], src_ap[:, o:o + m], writes=["wst"])
                d = dst_ap[:, o:o + m]
                if cast_rr[0] % 2 == 0:
                    P.act(lambda e, d=d, m=m: e.copy(out=d, in_=wst[:, 0:m]), ["wst"], [dst_key])
                else:
                    P.dve(lambda e, d=d, m=m: e.tensor_copy(out=d, in_=wst[:, 0:m]), ["wst"], [dst_key])
                cast_rr[0] += 1
                o += m

        x1T = sb(G, "x1T", [128, 8, TO], BF16)
        wst3 = wst[:].rearrange("p (k n) -> p k n", k=8)
        A_ = ExitStack()
        oattnT = sb(A_, "oattnT", [128, 4, TO], BF16)

        with ExitStack() as SN:
            QT = sb(SN, "QT", [128, 16, 4, 128], BF16)
            KE = [sb(SN, "KE%d" % i_, [128, T], BF16) for i_ in range(2)]
            P.dma(KE[0][64:128, :], cdr["eall"][64:128, :], writes=["KE0"])
            P.dma(KE[1][0:64, :], cdr["eall"][0:64, :], writes=["KE1"])
            winkT = sb(SN, "winkT", [128, T], BF16)
            slcv1 = sb(SN, "slcv1", [128, 32, 2, 65], BF16)
            winv1 = sb(SN, "winv1", [128, 32, 2, 65], BF16)
            gates = sb(SN, "gates", [128, 16, 24], F32)
            kcmpT = sb(SN, "kcmpT", [128, 256], BF16)
            vcmp1 = sb(SN, "vcmp1", [128, 2, 2, 65], BF16)
            pt64 = sb(SN, "pt64", [128, 128], BF16)
            P.dma(pt64[:], cdr["pt64"], writes=["pt64"])
            P.dve(lambda e: e.memset(slcv1[:].rearrange("p a g d -> p (a g d)"), 1.0), [], ["slcv1"])
            P.dve(lambda e: e.memset(winv1[:].rearrange("p a g d -> p (a g d)"), 1.0), [], ["winv1"])
            P.dve(lambda e: e.memset(kcmpT[:], 0.0), [], ["kcmpT"])
            P.dve(lambda e: e.memset(vcmp1[:].rearrange("p a g d -> p (a g d)"), 0.0), [], ["vcmp1"])
            P.dve(lambda e: e.memset(vcmp1[:, :, :, 64:65], 1.0), [], ["vcmp1"])

            with ExitStack() as S12:
                cmpT = {"k": sb(S12, "cmpkT", [128, T], BF16), "v": sb(S12, "cmpvT", [128, T], BF16)}
                with ExitStack() as S1:
                    Wn = sb(S1, "Wn", [128, 8, 1304], BF16)
                    load_cast(Wn[:].rearrange("p k n -> p (k n)"), w1t, 8 * 1304, "Wn")
                    xb = [sb(S1, "xb%d" % i, [128, 8, 512], BF16) for i in range(2)]
                    tabs = [sb(S1, "tab%d" % i, [128, 2, 512], F32) for i in range(2)]
                    ybf = [sb(S1, "ybf%d" % i, [128, 512], BF16) for i in range(2)]
                    t1 = [sb(S1, "t1_%d" % i, [128, 512], F32) for i in range(2)]
                    t2 = [sb(S1, "t2_%d" % i, [128, 512], F32) for i in range(2)]
                    pj = [ps(S1, "pj%d" % i, [128, 512]) for i in range(3)]
                    prot = [ps(S1, "prot%d" % i, [128, 512]) for i in range(2)]
                    pv = [ps(S1, "pv%d" % i, [128, 256]) for i in range(2)]
                    pjr = Ring(list(range(3)))
                    rr = Ring(list(range(2)))
                    pvr = Ring(list(range(2)))
                    xTv = xT.rearrange("(k p) t -> p k t", p=128)
                    xTov = xTo.rearrange("(k p) t -> p k t", p=128)

                    def load_x(src_view, c0, n, slot):
                        P.dma(wst3[:, :, 0:n], src_view[:, :, c0:c0 + n], writes=["wst"])
                        P.act(lambda e: e.copy(out=xb[slot][:, 0:4, 0:n], in_=wst3[:, 0:4, 0:n]), ["wst"], ["xb%d" % slot])
                        P.dve(lambda e: e.tensor_copy(out=xb[slot][:, 4:8, 0:n], in_=wst3[:, 4:8, 0:n]), ["wst"], ["xb%d" % slot])

                    def proj_fm(col0, slot, n=512):
                        pi = pjr.next()
                        for k in range(8):
                            P.pe(lambda e, k=k, pi=pi: e.matmul(pj[pi][:, 0:n], lhsT=Wn[:, k, col0:col0 + 128], rhs=xb[slot][:, k, 0:n],
                                                                 start=(k == 0), stop=(k == 7)), ["Wn", "xb%d" % slot], ["pj%d" % pi])
                        return pi

                    def rope_fm(pi, tslot, dst_ap, dst_key, ptm, ptkey, n=512, src=None, srckey=None):
                        r = rr.next()
                        srcap = pj[pi][:, 0:n] if src is None else src
                        sk = ("pj%d" % pi) if srckey is None else srckey
                        P.act(lambda e: e.copy(out=ybf[r][:, 0:n], in_=srcap), [sk], ["ybf%d" % r])
                        P.pe(lambda e: e.matmul(prot[r][:, 0:n], lhsT=ptm[:], rhs=ybf[r][:, 0:n], start=True, stop=True),
                             [ptkey, "ybf%d" % r], ["prot%d" % r])
                        P.dve(lambda e: e.tensor_tensor(out=t1[r][:, 0:n], in0=srcap, in1=tabs[tslot][:, 0, 0:n], op=ALU.mult),
                              [sk, "tab%d" % tslot], ["t1_%d" % r])
                        P.dve(lambda e: e.tensor_tensor(out=t2[r][:, 0:n], in0=prot[r][:, 0:n], in1=tabs[tslot][:, 1, 0:n], op=ALU.mult),
                              ["prot%d" % r, "tab%d" % tslot], ["t2_%d" % r])
                        if isinstance(dst_ap, list):
                            for (d_ap, rows, dkey) in dst_ap:
                                P.pool(lambda e: e.tensor_tensor(out=d_ap, in0=t1[r][rows, 0:n], in1=t2[r][rows, 0:n], op=ALU.add),
                                       ["t1_%d" % r, "t2_%d" % r], [dkey])
                        elif dst_key == "QT":
                            P.pool(lambda e: e.tensor_tensor(out=dst_ap, in0=t1[r][:, 0:n].rearrange("p (a t) -> p a t", a=4),
                                                             in1=t2[r][:, 0:n].rearrange("p (a t) -> p a t", a=4), op=ALU.add),
                                   ["t1_%d" % r, "t2_%d" % r], [dst_key])
                        else:
                            P.pool(lambda e: e.tensor_tensor(out=dst_ap, in0=t1[r][:, 0:n], in1=t2[r][:, 0:n], op=ALU.add),
                                   ["t1_%d" % r, "t2_%d" % r], [dst_key])

                    for ch in range(8):
                        slot = ch % 2
                        c0 = ch * 512
                        load_x(xTv, c0, 512, slot)
                        P.dma(tabs[slot][:, 0, :], cdr["cosK"][:, c0:c0 + 512], writes=["tab%d" % slot])
                        P.dma(tabs[slot][:, 1, :], cdr["sinK"][:, c0:c0 + 512], writes=["tab%d" % slot])
                        for col0, kv in ((512, "k"), (640, "v")):
                            pi = proj_fm(col0, slot)
                            P.act(lambda e, pi=pi, kv=kv: e.copy(out=cmpT[kv][:, c0:c0 + 512], in_=pj[pi][:]), ["pj%d" % pi], ["cmp" + kv + "T"])
                        pi = proj_fm(768, slot)
                        rope_fm(pi, slot, [(KE[0][0:64, c0:c0 + 512], slice(0, 64), "KE0"), (KE[1][64:128, c0:c0 + 512], slice(64, 128), "KE1")], None, pt64, "pt64")
                        pi = proj_fm(896, slot)
                        rope_fm(pi, slot, winkT[:, c0:c0 + 512], "winkT", pt64, "pt64")
                        for tt in range(4):
                            vi = pvr.next()
                            for k in range(8):
                                P.pe(lambda e, k=k, vi=vi, tt=tt: e.matmul(pv[vi][:], lhsT=xb[slot][:, k, tt * 128:(tt + 1) * 128], rhs=Wn[:, k, 1024:1280],
                                                                            start=(k == 0), stop=(k == 7)), ["Wn", "xb%d" % slot], ["pv%d" % vi])
                            tg = ch * 4 + tt
                            P.act(lambda e, vi=vi, tg=tg: e.copy(out=slcv1[:, tg, :, 0:64], in_=pv[vi][:, 0:128].rearrange("p (g d) -> p g d", g=2)),
                                  ["pv%d" % vi], ["slcv1"])
                            P.dve(lambda e, vi=vi, tg=tg: e.tensor_copy(out=winv1[:, tg, :, 0:64], in_=pv[vi][:, 128:256].rearrange("p (g d) -> p g d", g=2)),
                                  ["pv%d" % vi], ["winv1"])
                    for oc in range(4):
                        slot = oc % 2
                        c0 = oc * 512
                        load_x(xTov, c0, 512, slot)
                        P.dma(tabs[slot][:, 0, :], cdr["cosQ"][:, c0:c0 + 512], writes=["tab%d" % slot])
                        P.dma(tabs[slot][:, 1, :], cdr["sinQ"][:, c0:c0 + 512], writes=["tab%d" % slot])
                        for hh in range(4):
                            pi = proj_fm(hh * 128, slot)
                            rope_fm(pi, slot, QT[:, oc * 4:(oc + 1) * 4, hh, :], "QT", pt64, "pt64")
                        for tt in range(4):
                            vi = pvr.next()
                            for k in range(8):
                                P.pe(lambda e, k=k, vi=vi, tt=tt: e.matmul(pv[vi][:, 0:24], lhsT=xb[slot][:, k, tt * 128:(tt + 1) * 128], rhs=Wn[:, k, 1280:1304],
                                                                            start=(k == 0), stop=(k == 7)), ["Wn", "xb%d" % slot], ["pv%d" % vi])
                            tg = oc * 4 + tt
                            P.act(lambda e, vi=vi, tg=tg: e.activation(out=gates[:, tg, :], in_=pv[vi][:, 0:24], func=AF.Sigmoid), ["pv%d" % vi], ["gates"])
                    dump("QT", QT[:].rearrange("p i a t -> p (i a t)"), [128, 4 * TO], "QT")
                    dump("cmpkT", cmpT["k"][:], [128, T], "cmpkT")
                    dump("slcv1", slcv1[:].rearrange("p a g d -> p (a g d)"), [128, 32 * 130], "slcv1")
                    dump("gates", gates[:].rearrange("p a g -> p (a g)"), [128, 16 * 24], "gates")
                P.barrier()
                if n_stage >= 2:
                    with ExitStack() as S2:
                        w1b = sb(S2, "w1b", [128, 32, 256], BF16)
                        posT = sb(S2, "posT", [128, 32], F32)
                        posTb = sb(S2, "posTb", [128, 32], BF16)
                        b1 = sb(S2, "b1", [128, 2], F32)
                        bias1 = sb(S2, "bias1", [128, 2], F32)
                        w2kf = sb(S2, "w2kf", [128, 2, 128], F32)
                        w2k = sb(S2, "w2k", [128, 2, 128], BF16)
                        w2vf = sb(S2, "w2vf", [128, 2, 64], F32)
                        w2v = sb(S2, "w2v", [128, 2, 64], BF16)
                        b2k = sb(S2, "b2k", [128, 1], F32)
                        b2v = sb(S2, "b2v", [128, 64], F32)
                        tabC = sb(S2, "tabC", [128, 2, 256], F32)
                        h1 = sb(S2, "h1", [128, 2, 256], BF16)
                        xg = sb(S2, "xg", [128, 256], F32)
                        ug = sb(S2, "ug", [128, 256], F32)
                        sg_ = sb(S2, "sg_", [128, 256], F32)
                        yk = sb(S2, "yk", [128, 256], F32)
                        ykb = sb(S2, "ykb", [128, 256], BF16)
                        tk1 = sb(S2, "tk1", [128, 256], F32)
                        tk2 = sb(S2, "tk2", [128, 256], F32)
                        ph = [ps(S2, "ph%d" % i, [128, 256]) for i in range(2)]
                        pcv = ps(S2, "pcv", [128, 2])
                        pkc = ps(S2, "pkc", [128, 256])
                        prk = ps(S2, "prk", [128, 256])
                        pvc = ps(S2, "pvc", [128, 64])
                        P.dma(w2kf[:].rearrange("p a n -> p (a n)"), cw2k, writes=["w2kf"])
                        P.dve(lambda e: e.tensor_copy(out=w2k[:], in_=w2kf[:]), ["w2kf"], ["w2k"])
                        P.dma(w2vf[:].rearrange("p a n -> p (a n)"), cw2v, writes=["w2vf"])
                        P.dve(lambda e: e.tensor_copy(out=w2v[:], in_=w2vf[:]), ["w2vf"], ["w2v"])
                        P.dma(b2k[:], cb2k, writes=["b2k"])
                        P.dma(b2v[:], cb2v.partition_broadcast(128), writes=["b2v"])
                        P.dma(tabC[:, 0, :], cdr["cosC"], writes=["tabC"])
                        P.dma(tabC[:, 1, :], cdr["sinC"], writes=["tabC"])
                        for kv in "kv":
                            load_cast(w1b[:].rearrange("p l n -> p (l n)"), cw1[kv], 32 * 256, "w1b")
                            P.dma(posT[:], cpos[kv], writes=["posT"])
                            P.dve(lambda e: e.tensor_copy(out=posTb[:], in_=posT[:]), ["posT"], ["posTb"])
                            P.dma(b1[:], cb1[kv], writes=["b1"])
                            for ht in range(2):
                                for l in range(32):
                                    P.pe(lambda e, ht=ht, l=l: e.matmul(pcv[:, ht:ht + 1], lhsT=w1b[0:64, l, ht * 128:(ht + 1) * 128], rhs=posTb[0:64, l:l + 1],
                                                                         start=(l == 0), stop=(l == 31)), ["w1b", "posTb"], ["pcv"])
                            P.dve(lambda e: e.tensor_tensor(out=bias1[:], in0=pcv[:], in1=b1[:], op=ALU.add), ["pcv", "b1"], ["bias1"])
                            for g in range(2):
                                gp = slice(g * 64, (g + 1) * 64)
                                for ht in range(2):
                                    for l in range(32):
                                        P.pe(lambda e, ht=ht, l=l, gp=gp, kv=kv: e.matmul(ph[ht][:, 0:255], lhsT=w1b[gp, l, ht * 128:(ht + 1) * 128],
                                                                                        rhs=cmpT[kv][gp, l:l + 16 * 254 + 1:16],
                                                                                        start=(l == 0), stop=(l == 31)), ["w1b", "cmp" + kv + "T"], ["ph%d" % ht])
                                    P.act(lambda e, ht=ht: e.activation(out=xg[:, 0:255], in_=ph[ht][:, 0:255], func=AF.Identity, bias=bias1[:, ht:ht + 1], scale=1.0),
                                          ["ph%d" % ht, "bias1"], ["xg"])
                                    P.dve(lambda e: e.tensor_tensor(out=ug[:, 0:255], in0=xg[:, 0:255], in1=xg[:, 0:255], op=ALU.mult), ["xg"], ["ug"])
                                    P.dve(lambda e: e.tensor_scalar(out=ug[:, 0:255], in0=ug[:, 0:255], scalar1=0.044715, scalar2=1.0, op0=ALU.mult, op1=ALU.add), ["ug"], ["ug"])
                                    P.dve(lambda e: e.tensor_tensor(out=ug[:, 0:255], in0=ug[:, 0:255], in1=xg[:, 0:255], op=ALU.mult), ["ug", "xg"], ["ug"])
                                    P.act(lambda e: e.activation(out=sg_[:, 0:255], in_=ug[:, 0:255], func=AF.Sigmoid, scale=1.5957691216057308), ["ug"], ["sg_"])
                                    P.dve(lambda e, ht=ht: e.tensor_tensor(out=h1[:, ht, 0:255], in0=xg[:, 0:255], in1=sg_[:, 0:255], op=ALU.mult), ["xg", "sg_"], ["h1"])
                                if kv == "k":
                                    for ht in range(2):
                                        P.pe(lambda e, ht=ht: e.matmul(pkc[:, 0:255], lhsT=w2k[:, ht, :], rhs=h1[:, ht, 0:255], start=(ht == 0), stop=(ht == 1)),
                                             ["w2k", "h1"], ["pkc"])
                                    P.act(lambda e: e.activation(out=yk[:, 0:255], in_=pkc[:, 0:255], func=AF.Identity, bias=b2k[:, 0:1], scale=1.0), ["pkc", "b2k"], ["yk"])
                                    P.act(lambda e: e.copy(out=ykb[:, 0:255], in_=yk[:, 0:255]), ["yk"], ["ykb"])
                                    P.pe(lambda e: e.matmul(prk[:, 0:255], lhsT=pt64[:], rhs=ykb[:, 0:255], start=True, stop=True), ["pt64", "ykb"], ["prk"])
                                    P.dve(lambda e: e.tensor_tensor(out=tk1[:, 0:255], in0=yk[:, 0:255], in1=tabC[:, 0, 0:255], op=ALU.mult), ["yk", "tabC"], ["tk1"])
                                    P.dve(lambda e: e.tensor_tensor(out=tk2[:, 0:255], in0=prk[:, 0:255], in1=tabC[:, 1, 0:255], op=ALU.mult), ["prk", "tabC"], ["tk2"])
                                    P.dve(lambda e, gp=gp: e.tensor_tensor(out=kcmpT[gp, 0:255], in0=tk1[gp, 0:255], in1=tk2[gp, 0:255], op=ALU.add), ["tk1", "tk2"], ["kcmpT"])
                                else:
                                    for a in range(2):
                                        cntn = 128 if a == 0 else 127
                                        for ht in range(2):
                                            P.pe(lambda e, ht=ht, a=a, cntn=cntn: e.matmul(pvc[0:cntn, :], lhsT=h1[:, ht, a * 128:a * 128 + cntn], rhs=w2v[:, ht, :],
                                                                                            start=(ht == 0), stop=(ht == 1)), ["w2v", "h1"], ["pvc"])
                                        P.dve(lambda e, a=a, cntn=cntn, g=g: e.tensor_tensor(out=vcmp1[0:cntn, a, g, 0:64], in0=pvc[0:cntn, :], in1=b2v[0:cntn, :], op=ALU.add),
                                              ["pvc", "b2v"], ["vcmp1"])
                        dump("kcmpT", kcmpT[:], [128, 256], "kcmpT")
                        dump("vcmp1", vcmp1[:].rearrange("p a g d -> p (a g d)"), [128, 260], "vcmp1")
                    P.barrier()
            P.barrier()
            if n_stage >= 3:
                with ExitStack() as S3:
                    def bc4(ap):
                        return ap.unsqueeze(1).broadcast_to([ap.shape[0], 4, ap.shape[1]])

                    wmask = sb(S3, "wmask", [128, 6, 128], BF16)
                    cmpmask = sb(S3, "cmpmask", [128, 2, 16, 128], BF16)
                    ovT = sb(S3, "ovT", [128, 2, 64], BF16)
                    tkm = sb(S3, "tkm", [128, 16, 64], F32)
                    tkb = sb(S3, "tkb", [128, 16, 64], F32)
                    wmask4 = sb(S3, "wmask4", [128, 6, 512], BF16)
                    cm4 = [sb(S3, "cm4_%d" % i_, [128, 2, 512], BF16) for i_ in range(2)]
                    QN = [sb(S3, "QN%d" % i_, [128, 512], BF16) for i_ in range(4)]
                    P.dma(wmask[:].rearrange("p a k -> p (a k)"), cdr["wmask"], writes=["wmask"])
                    P.dma(cmpmask[:].rearrange("p a i k -> p (a i k)"), cdr["cmpmask"], writes=["cmpmask"])
                    P.dma(ovT[:].rearrange("p a k -> p (a k)"), cdr["ovT"], writes=["ovT"])
                    P.dma(tkm[:].rearrange("p a k -> p (a k)"), cdr["tkm"], writes=["tkm"])
                    P.dma(tkb[:].rearrange("p a k -> p (a k)"), cdr["tkb"], writes=["tkb"])
                    for r_ in range(6):
                        P.pool(lambda e: e.tensor_copy(out=wmask4[:, r_, :].rearrange("p (a t) -> p a t", a=4), in_=bc4(wmask[:, r_, :])), ["wmask"], ["wmask4"])
                    eT = [sb(S3, "eT%d" % i, [128, 512], BF16) for i in range(4)]
                    oacc = sb(S3, "oacc", [128, 512], F32)
                    oab = sb(S3, "oab", [128, 512], BF16)
                    rz = sb(S3, "rz", [128, 4], F32)
                    coef = sb(S3, "coef", [128, 4], F32)
                    imp = sb(S3, "imp", [128, 64], F32)
                    score = sb(S3, "score", [128, 64], F32)
                    work = sb(S3, "work", [128, 64], F32)
                    m8 = sb(S3, "m8", [128, 16], F32)
                    nmk = [sb(S3, "nmk%d" % i_, [128, 2, 64], BF16) for i_ in range(2)]
                    pST = [ps(S3, "pST%d" % i, [128, 512]) for i in range(3)]
                    pA = ps(S3, "pA", [128, 4, 65])
                    pB = ps(S3, "pB", [128, 4, 64])
                    pS = ps(S3, "pS", [128, 4, 65])
                    pW = ps(S3, "pW", [128, 4, 65])
                    pTr = ps(S3, "pTr", [128, 128], BF16)
                    str_ = Ring([0, 1, 2])
                    etr = Ring([0, 1, 2, 3])

                    def scores(kT_ap, kkey, g, i, masks, q_ap=None, qkey="QT"):
                        gp = slice(g * 64, (g + 1) * 64)
                        si = str_.next()
                        ei = etr.next()
                        nm = len(masks)
                        if q_ap is None:
                            q_ap = QT[gp, i, :, :].rearrange("p a t -> p (a t)")
                        P.pe(lambda e: e.matmul(pST[si][:], lhsT=kT_ap, rhs=q_ap, start=True, stop=(nm == 0)), [kkey, qkey], ["pST%d" % si])
                        for mi, (ml, mr, mkeys) in enumerate(masks):
                            P.pe(lambda e: e.matmul(pST[si][:], lhsT=ml, rhs=mr, start=False, stop=(mi == nm - 1)), mkeys, ["pST%d" % si])
                        P.act(lambda e: e.activation(out=eT[ei][:], in_=pST[si][:], func=AF.Exp), ["pST%d" % si], ["eT%d" % ei])
                        return ei

                    def finish_branch(pacc, pkey, i, g, br, first):
                        P.dve(lambda e: e.tensor_scalar(out=rz[:], in0=pacc[:, :, 64], scalar1=1e-30, scalar2=None, op0=ALU.max), [pkey], ["rz"])
                        P.dve(lambda e: e.reciprocal(out=rz[:], in_=rz[:]), ["rz"], ["rz"])
                        P.dve(lambda e: e.tensor_tensor(out=coef[:], in0=rz[:], in1=gates[:, i, g * 12 + br:g * 12 + 12:3], op=ALU.mult), ["rz", "gates"], ["coef"])
                        for hh in range(4):
                            o = oacc[:, g * 256 + hh * 64:g * 256 + (hh + 1) * 64]
                            if first:
                                P.dve(lambda e, hh=hh, o=o: e.tensor_scalar(out=o, in0=pacc[:, hh, 0:64], scalar1=coef[:, hh:hh + 1], scalar2=None, op0=ALU.mult),
                                      [pkey, "coef"], ["oacc"])
                            else:
                                P.dve(lambda e, hh=hh, o=o: e.scalar_tensor_tensor(out=o, in0=pacc[:, hh, 0:64], scalar=coef[:, hh:hh + 1], in1=o, op0=ALU.mult, op1=ALU.add),
                                      [pkey, "coef", "oacc"], ["oacc"])

                    tasks = []

                    def mk_cmp(i, g, a, na):
                        gp = slice(g * 64, (g + 1) * 64)

                        def sc():
                            if g == 0:
                                P.pool(lambda e: e.tensor_copy(out=cm4[i % 2][:, a, :].rearrange("p (h t) -> p h t", h=4), in_=bc4(cmpmask[:, a, i, :])),
                                       ["cmpmask"], ["cm4_%d" % (i % 2)])
                            return scores(kcmpT[gp, a * 128:(a + 1) * 128], "kcmpT", g, i,
                                          [(identb[:], cm4[i % 2][:, a, :], ["identb", "cm4_%d" % (i % 2)])])

                        def pvf(ei):
                            for hh in range(4):
                                P.pe(lambda e: e.matmul(pA[:, hh, :], lhsT=eT[ei][:, hh * 128:(hh + 1) * 128], rhs=vcmp1[:, a, g, :],
                                                        start=(a == 0 and hh == 0), stop=(a == na - 1 and hh == 3)), ["eT%d" % ei, "vcmp1"], ["pA"])
                                P.pe(lambda e: e.matmul(pB[:, hh, :], lhsT=eT[ei][:, hh * 128:(hh + 1) * 128], rhs=ovT[:, a, :],
                                                        start=(a == 0 and hh == 0), stop=(a == na - 1 and hh == 3)), ["eT%d" % ei, "ovT"], ["pB"])

                        def post():
                            finish_branch(pA, "pA", i, g, 0, True)
                            P.dve(lambda e: e.tensor_scalar(out=imp[:], in0=pB[:, 0, :], scalar1=rz[:, 0:1], scalar2=None, op0=ALU.mult), ["pB", "rz"], ["imp"])
                            for hh in range(1, 4):
                                P.dve(lambda e: e.scalar_tensor_tensor(out=imp[:], in0=pB[:, hh, :], scalar=rz[:, hh:hh + 1], in1=imp[:], op0=ALU.mult, op1=ALU.add),
                                      ["pB", "rz", "imp"], ["imp"])
                            P.dve(lambda e: e.tensor_tensor(out=score[:], in0=imp[:], in1=tkm[:, i, :], op=ALU.mult), ["imp", "tkm"], ["score"])
                            P.dve(lambda e: e.tensor_tensor(out=score[:], in0=score[:], in1=tkb[:, i, :], op=ALU.add), ["score", "tkb"], ["score"])
                            P.dve(lambda e: e.max(out=m8[:, 0:8], in_=score[:]), ["score"], ["m8"])
                            P.dve(lambda e: e.match_replace(out=work[:], in_to_replace=m8[:, 0:8], in_values=score[:], imm_value=-3.0e38), ["score", "m8"], ["work"])
                            P.dve(lambda e: e.max(out=m8[:, 8:16], in_=work[:]), ["work"], ["m8"])
                            P.dve(lambda e: e.tensor_scalar(out=nmk[g][:], in0=score[:].unsqueeze(1).broadcast_to([128, 2, 64]), scalar1=m8[:, 15:16], scalar2=NEGM,
                                                            op0=ALU.is_lt, op1=ALU.mult), ["score", "m8"], ["nmk%d" % g])
                            if ("imp%d_%d" % (i, g)) in debug:
                                dump("imp%d_%d" % (i, g), imp[:], [128, 64], "imp")
                                dump("score%d_%d" % (i, g), score[:], [128, 64], "score")
                                dump("m8%d_%d" % (i, g), m8[:], [128, 16], "m8")
                        return [None, sc, pvf, post if a == na - 1 else None]

                    def mk_win(i, g, idx, r, j, nw):
                        gp = slice(g * 64, (g + 1) * 64)

                        def sc():
                            return scores(winkT[gp, j * 128:(j + 1) * 128], "winkT", g, i,
                                          [(identb[:], wmask4[:, r, :], ["identb", "wmask4"])])

                        def pvf(ei):
                            for hh in range(4):
                                P.pe(lambda e: e.matmul(pW[:, hh, :], lhsT=eT[ei][:, hh * 128:(hh + 1) * 128], rhs=winv1[:, j, g, :],
                                                        start=(idx == 0 and hh == 0), stop=(idx == nw - 1 and hh == 3)), ["eT%d" % ei, "winv1"], ["pW"])

                        def post():
                            finish_branch(pW, "pW", i, g, 2, False)
                        return [None, sc, pvf, post if idx == nw - 1 else None]

                    def tile_end_pe(i):
                        for ct in range(4):
                            P.pe(lambda e: e.transpose(out=pTr[:], in_=oab[:, ct * 128:(ct + 1) * 128], identity=identb[:]), ["oab", "identb"], ["pTr"])
                            P.dve(lambda e: e.tensor_copy(out=oattnT[:, ct, i * 128:(i + 1) * 128], in_=pTr[:]), ["pTr"], ["oattnT"])

                    def mk_slc(i, g, j, nj):
                        gp = slice(g * 64, (g + 1) * 64)

                        qn_i = (2 * i + g) % 4
                        oh = slice((1 - g) * 64, (2 - g) * 64)

                        def pre():
                            P.pool(lambda e: e.tensor_copy(out=QN[qn_i][gp, :], in_=QT[gp, i, :, :].rearrange("p a t -> p (a t)")), ["QT"], ["QN%d" % qn_i])
                            P.pe(lambda e: e.transpose(out=pTr[:], in_=nmk[g][:].rearrange("p a s -> p (a s)"), identity=identb[:]), ["nmk%d" % g, "identb"], ["pTr"])
                            P.dve(lambda e: e.tensor_copy(out=QN[qn_i][oh, :].rearrange("p (a t) -> p a t", a=4), in_=bc4(pTr[oh, :])), ["pTr"], ["QN%d" % qn_i])
                            if g == 0 and i > 0:
                                tile_end_pe(i - 1)

                        def sc():
                            masks = []
                            if j >= 2 * i:
                                masks.append((identb[:], wmask4[:, 4 + (j - 2 * i), :], ["identb", "wmask4"]))
                            return scores(KE[g][:, j * 128:(j + 1) * 128], "KE%d" % g, g, i, masks, q_ap=QN[qn_i][:], qkey="QN%d" % qn_i)

                        def pvf(ei):
                            for hh in range(4):
                                P.pe(lambda e: e.matmul(pS[:, hh, :], lhsT=eT[ei][:, hh * 128:(hh + 1) * 128], rhs=slcv1[:, j, g, :],
                                                        start=(j == 0 and hh == 0), stop=(j == nj - 1 and hh == 3)), ["eT%d" % ei, "slcv1"], ["pS"])

                        def post():
                            finish_branch(pS, "pS", i, g, 1, False)
                            if g == 1:
                                if ("oacc%d" % i) in debug:
                                    dump("oacc%d" % i, oacc[:], [128, 512], "oacc")
                                P.pool(lambda e: e.tensor_copy(out=oab[:], in_=oacc[:]), ["oacc"], ["oab"])
                        return [pre if j == 0 else None, sc, pvf, post if j == nj - 1 else None]

                    for i in range(16):
                        for g in range(2):
                            na = 1 if i < 8 else 2
                            for a in range(na):
                                tasks.append(mk_cmp(i, g, a, na))
                            js = [(r, 2 * i - 4 + r) for r in range(6) if 2 * i - 4 + r >= 0]
                            for idx, (r, j) in enumerate(js):
                                tasks.append(mk_win(i, g, idx, r, j, len(js)))
                            nj = 2 * i + 2
                            for j in range(nj):
                                tasks.append(mk_slc(i, g, j, nj))
                    nt = len(tasks)
                    eis = [None] * nt

                    def emit_score(k):
                        if tasks[k][0] is not None:
                            tasks[k][0]()
                        eis[k] = tasks[k][1]()

                    emit_score(0)
                    emit_score(1)
                    for k in range(nt):
                        if k + 2 < nt:
                            emit_score(k + 2)
                        tasks[k][2](eis[k])
                        if tasks[k][3] is not None:
                            tasks[k][3]()
                    tile_end_pe(15)
                    dump("oattnT", oattnT[:].rearrange("p a t -> p (a t)"), [128, 4 * TO], "oattnT")
                P.barrier()
        P.barrier()

        B_ = ExitStack()
        oretT = sb(B_, "oretT", [128, 8, TO], BF16)
        if n_stage >= 4:
            with ExitStack() as S4:
                W4 = sb(S4, "W4", [128, 8, 2048], BF16)
                load_cast(W4[:].rearrange("p k n -> p (k n)"), w4t, 8 * 2048, "W4")
                pt128 = sb(S4, "pt128", [128, 128], BF16)
                P.dma(pt128[:], cdr["pt128"], writes=["pt128"])
                Dc = sb(S4, "Dc", [128, 2, 4, 128], F32)
                xi = sb(S4, "xi", [128, 4, 128], F32)
                zeta = sb(S4, "zeta", [128, 2, 4], F32)
                P.dma(Dc[:].rearrange("p a h c -> p (a h c)"), cdr["Dc"], writes=["Dc"])
                P.dma(xi[:].rearrange("p h c -> p (h c)"), cdr["xi"], writes=["xi"])
                P.dma(zeta[:].rearrange("p a h -> p (a h)"), cdr["zeta"], writes=["zeta"])
                xst = wst3
                xb = sb(S4, "xb4", [128, 8, 512], BF16)
                xob = sb(S4, "xob4", [128, 8, 256], BF16)
                tabs = sb(S4, "tab4", [128, 2, 512], F32)
                tabq = sb(S4, "tabq4", [128, 2, 256], F32)
                ybf2 = [sb(S4, "ybf4_%d" % i_, [128, 512], BF16) for i_ in range(2)]
                t12 = [sb(S4, "t1_4_%d" % i_, [128, 512], F32) for i_ in range(2)]
                t22 = [sb(S4, "t2_4_%d" % i_, [128, 512], F32) for i_ in range(2)]
                rr4 = Ring([0, 1])
                kT = sb(S4, "kT4", [128, 4, 512], BF16)
                qT = sb(S4, "qT4", [128, 4, 256], BF16)
                qxT = sb(S4, "qxT4", [128, 4, 256], BF16)
                vtok = sb(S4, "vtok", [128, 4, 1024], BF16)
                kz = sb(S4, "kz", [128, 4, 4, 128], BF16)
                R = sb(S4, "R", [128, 4, 256], F32)
                Rb = sb(S4, "Rb", [128, 4, 256], BF16)
                sc = [sb(S4, "sc%d" % i_, [128, 2, 128], BF16) for i_ in range(2)]
                epsT = sb(S4, "epsT", [128, 1], F32)
                P.dve(lambda e: e.memset(epsT[:], LN_EPS), [], ["epsT"])
                pending4 = []
                st6 = sb(S4, "st6", [128, 6], F32)
                mv = sb(S4, "mv", [128, 2], F32)
                rstd = sb(S4, "rstd", [128, 1], F32)
                oretb = [sb(S4, "oretb%d" % i_, [128, 1024], BF16) for i_ in range(2)]
                pj = [ps(S4, "pj4_%d" % i, [128, 512]) for i in range(3)]
                psc = [ps(S4, "psc%d" % i_, [128, 2, 128]) for i_ in range(2)]
                po = [ps(S4, "po%d" % i_, [128, 256]) for i_ in range(2)]
                pTrw = ps(S4, "pTr4", [128, 512], BF16)
                pTr = pTrw[:, 0:128]
                pjr = Ring([0, 1, 2])
                P.dve(lambda e: e.memset(R[:].rearrange("p h e -> p (h e)"), 0.0), [], ["R%d" % h_ for h_ in range(4)])
                P.dve(lambda e: e.memset(Rb[:].rearrange("p h e -> p (h e)"), 0.0), [], ["Rb%d" % h_ for h_ in range(4)])
                xTv = xT.rearrange("(k p) t -> p k t", p=128)
                xTov = xTo.rearrange("(k p) t -> p k t", p=128)

                def rope4(pi, n, tab, tabkey, dst_ap, dst_key):
                    ri = pjr.next()
                    prot = pj[ri]
                    rb = rr4.next()
                    ybf, t1, t2 = ybf2[rb], t12[rb], t22[rb]
                    P.act(lambda e: e.copy(out=ybf[:, 0:n], in_=pj[pi][:, 0:n]), ["pj4_%d" % pi], ["ybf4_%d" % rb])
                    P.pe(lambda e: e.matmul(prot[:, 0:n], lhsT=pt128[:], rhs=ybf[:, 0:n], start=True, stop=True), ["pt128", "ybf4_%d" % rb], ["pj4_%d" % ri])
                    P.dve(lambda e: e.tensor_tensor(out=t1[:, 0:n], in0=pj[pi][:, 0:n], in1=tab[:, 0, 0:n], op=ALU.mult), ["pj4_%d" % pi, tabkey], ["t1_4_%d" % rb])
                    P.dve(lambda e: e.tensor_tensor(out=t2[:, 0:n], in0=prot[:, 0:n], in1=tab[:, 1, 0:n], op=ALU.mult), ["pj4_%d" % ri, tabkey], ["t2_4_%d" % rb])
                    P.pool(lambda e: e.tensor_tensor(out=dst_ap, in0=t1[:, 0:n], in1=t2[:, 0:n], op=ALU.add), ["t1_4_%d" % rb, "t2_4_%d" % rb], [dst_key])

                for gch in range(8):
                    c0 = gch * 512
                    o0 = gch * 256
                    P.dma(xst[:], xTv[:, :, c0:c0 + 512], writes=["wst"])
                    P.act(lambda e: e.copy(out=xb[:, 0:4, :], in_=xst[:, 0:4, :]), ["wst"], ["xb4"])
                    P.dve(lambda e: e.tensor_copy(out=xb[:, 4:8, :], in_=xst[:, 4:8, :]), ["wst"], ["xb4"])
                    P.dma(xst[:, :, 0:256], xTov[:, :, o0:o0 + 256], writes=["wst"])
                    P.act(lambda e: e.copy(out=xob[:, 0:4, :], in_=xst[:, 0:4, 0:256]), ["wst"], ["xob4"])
                    P.dve(lambda e: e.tensor_copy(out=xob[:, 4:8, :], in_=xst[:, 4:8, 0:256]), ["wst"], ["xob4"])
                    P.dma(tabs[:, 0, :], cdr["cosRK"][:, c0:c0 + 512], writes=["tab4"])
                    P.dma(tabs[:, 1, :], cdr["sinRK"][:, c0:c0 + 512], writes=["tab4"])
                    P.dma(tabq[:, 0, :], cdr["cosRQ"][:, o0:o0 + 256], writes=["tabq4"])
                    P.dma(tabq[:, 1, :], cdr["sinRQ"][:, o0:o0 + 256], writes=["tabq4"])
                    for h in range(4):
                        pi = pjr.next()
                        for k in range(8):
                            P.pe(lambda e, k=k, pi=pi, h=h: e.matmul(pj[pi][:], lhsT=W4[:, k, 512 + h * 128:512 + (h + 1) * 128], rhs=xb[:, k, :],
                                                                      start=(k == 0), stop=(k == 7)), ["W4", "xb4"], ["pj4_%d" % pi])
                        rope4(pi, 512, tabs, "tab4", kT[:, h, :], "kT4")
                    for h in range(4):
                        pi = pjr.next()
                        for k in range(8):
                            P.pe(lambda e, k=k, pi=pi, h=h: e.matmul(pj[pi][:, 0:256], lhsT=W4[:, k, h * 128:(h + 1) * 128], rhs=xob[:, k, :],
                                                                      start=(k == 0), stop=(k == 7)), ["W4", "xob4"], ["pj4_%d" % pi])
                        rope4(pi, 256, tabq, "tabq4", qT[:, h, :], "qT4")
                    for pp in range(2):
                        P.dve(lambda e, pp=pp: e.tensor_tensor(out=qxT[:, :, pp * 128:(pp + 1) * 128], in0=qT[:, :, pp * 128:(pp + 1) * 128], in1=xi[:], op=ALU.mult),
                              ["qT4", "xi"], ["qxT4"])
                    for tt in range(4):
                        for hf in range(2):
                            pi = pjr.next()
                            for k in range(8):
                                P.pe(lambda e, k=k, pi=pi, tt=tt, hf=hf: e.matmul(pj[pi][:], lhsT=xb[:, k, tt * 128:(tt + 1) * 128],
                                                                                   rhs=W4[:, k, 1024 + hf * 512:1024 + (hf + 1) * 512],
                                                                                   start=(k == 0), stop=(k == 7)), ["W4", "xb4"], ["pj4_%d" % pi])
                            P.act(lambda e, pi=pi, tt=tt, hf=hf: e.copy(out=vtok[:, tt, hf * 512:(hf + 1) * 512], in_=pj[pi][:]), ["pj4_%d" % pi], ["vtok"])
                    for tt in range(4):
                        for h in range(4):
                            P.pe(lambda e: e.transpose(out=pTrw[:, h * 128:(h + 1) * 128], in_=kT[:, h, tt * 128:(tt + 1) * 128], identity=identb[:]), ["kT4", "identb"], ["pTr4"])
                        P.dve(lambda e: e.tensor_tensor(out=kz[:, tt, :, :], in0=pTrw[:].rearrange("p (h d) -> p h d", h=4),
                                                        in1=zeta[:, tt % 2, :].unsqueeze(2).broadcast_to([128, 4, 128]), op=ALU.mult), ["pTr4", "zeta"], ["kz"])
                    units = [(pp, h) for pp in range(2) for h in range(4)]

                    def phaseA(pp, h, ub):
                        qs = slice(pp * 128, (pp + 1) * 128)
                        for mt in range(2):
                            tt = pp * 2 + mt
                            P.pe(lambda e: e.matmul(psc[ub][:, mt, :], lhsT=kT[:, h, tt * 128:(tt + 1) * 128], rhs=qT[:, h, qs], start=True, stop=True),
                                 ["kT4", "qT4"], ["psc%d" % ub])
                        P.dve(lambda e: e.tensor_tensor(out=sc[ub][:], in0=psc[ub][:], in1=Dc[:, :, h, :], op=ALU.mult), ["psc%d" % ub, "Dc"], ["sc%d" % ub])

                    def phaseBC(pp, h, ub):
                        i = gch * 2 + pp
                        qs = slice(pp * 128, (pp + 1) * 128)
                        hs = slice(h * 256, (h + 1) * 256)
                        ob = oretb[pp]
                        for mt in range(2):
                            tt = pp * 2 + mt
                            P.pe(lambda e: e.matmul(po[ub][:], lhsT=sc[ub][:, mt, :], rhs=vtok[:, tt, hs], start=(mt == 0), stop=False), ["sc%d" % ub, "vtok"], ["po%d" % ub])
                        P.pe(lambda e: e.matmul(po[ub][:], lhsT=qxT[:, h, qs], rhs=Rb[:, h, :], start=False, stop=True), ["qxT4", "Rb%d" % h], ["po%d" % ub])
                        ri = pjr.next()
                        for mt in range(2):
                            tt = pp * 2 + mt
                            P.pe(lambda e: e.matmul(pj[ri][:, 0:256], lhsT=kz[:, tt, h, :], rhs=vtok[:, tt, hs], start=(mt == 0), stop=(mt == 1)),
                                 ["kz", "vtok"], ["pj4_%d" % ri])
                        P.dve(lambda e: e.bn_stats(out=st6[:], in_=po[ub][:]), ["po%d" % ub], ["st6"])
                        P.dve(lambda e: e.bn_aggr(out=mv[:], in_=st6[:]), ["st6"], ["mv"])
                        P.act(lambda e: e.activation(out=rstd[:], in_=mv[:, 1:2], func=AF.Sqrt, bias=epsT[:, 0:1], scale=1.0), ["mv", "epsT"], ["rstd"])
                        P.dve(lambda e: e.reciprocal(out=rstd[:], in_=rstd[:]), ["rstd"], ["rstd"])
                        P.dve(lambda e: e.tensor_scalar(out=ob[:, hs], in0=po[ub][:], scalar1=mv[:, 0:1], scalar2=rstd[:, 0:1], op0=ALU.subtract, op1=ALU.mult),
                              ["po%d" % ub, "mv", "rstd"], ["oretb%d" % pp])
                        P.dve(lambda e: e.scalar_tensor_tensor(out=R[:, h, :], in0=R[:, h, :], scalar=decay256[h], in1=pj[ri][:, 0:256], op0=ALU.mult, op1=ALU.add),
                              ["R%d" % h, "pj4_%d" % ri], ["R%d" % h])
                        P.act(lambda e: e.copy(out=Rb[:, h, :], in_=R[:, h, :]), ["R%d" % h], ["Rb%d" % h])

                    def pair_end(pp, i):
                        ob = oretb[pp]
                        for et in range(8):
                            P.pe(lambda e: e.transpose(out=pTr[:], in_=ob[:, et * 128:(et + 1) * 128], identity=identb[:]), ["oretb%d" % pp, "identb"], ["pTr4"])
                            P.act(lambda e: e.copy(out=oretT[:, et, i * 128:(i + 1) * 128], in_=pTr[:]), ["pTr4"], ["oretT"])

                    phaseA(units[0][0], units[0][1], 0)
                    for u, (pp, h) in enumerate(units):
                        if u + 1 < len(units):
                            phaseA(units[u + 1][0], units[u + 1][1], (u + 1) % 2)
                        phaseBC(pp, h, u % 2)
                        if pending4:
                            pending4.pop(0)()
                        if h == 3:
                            pending4.append(lambda pp=pp, i=gch * 2 + pp: pair_end(pp, i))
                while pending4:
                    pending4.pop(0)()
                dump("oretT", oretT[:].rearrange("p a t -> p (a t)"), [128, 8 * TO], "oretT")
            P.barrier()

        if n_stage >= 5:
            with ExitStack() as S5a:
                Wmg = sb(S5a, "Wmg", [128, 8, 2048], BF16)
                Wa = sb(S5a, "Wa", [128, 4, 1024], BF16)
                Wr = sb(S5a, "Wr", [128, 8, 1024], BF16)
                Wgt = sb(S5a, "Wgt", [128, 8, 1024], BF16)
                load_cast(Wmg[:].rearrange("p k n -> p (k n)"), wmgt, 8 * 2048, "Wmg")
                load_cast(Wa[:].rearrange("p k n -> p (k n)"), wat, 4 * 1024, "Wa")
                load_cast(Wr[:].rearrange("p k n -> p (k n)"), wrt, 8 * 1024, "Wr")
                load_cast(Wgt[:].rearrange("p k n -> p (k n)"), wgtt, 8 * 1024, "Wgt")
                gg8 = sb(S5a, "gg8", [128, 8], F32)
                gb8 = sb(S5a, "gb8", [128, 8], F32)
                P.dma(gg8[:], gng8, writes=["gg8"])
                P.dma(gb8[:], gnb8, writes=["gb8"])
                xb = sb(S5a, "xb5", [128, 8, 512], BF16)
                og = sb(S5a, "og", [128, 8, 512], BF16)
                sgt = [sb(S5a, "sgt%d" % i, [128, 512], F32) for i in range(2)]
                yn = [sb(S5a, "yn%d" % i, [128, 512], F32) for i in range(2)]
                ga2 = [sb(S5a, "ga%d" % i, [128, 512], F32) for i in range(2)]
                gr2 = [sb(S5a, "gr%d" % i, [128, 512], F32) for i in range(2)]
                ma2 = [sb(S5a, "ma%d" % i, [128, 512], F32) for i in range(2)]
                bk = [ps(S5a, "bk%d" % i, [128, 512]) for i in range(8)]
                pgt = [bk[4], bk[5]]
                xTov = xTo.rearrange("(k p) t -> p k t", p=128)
                for oc in range(4):
                    c0 = oc * 512
                    cs_ = slice(c0, c0 + 512)
                    P.dma(wst3[:], xTov[:, :, cs_], writes=["wst"])
                    P.act(lambda e: e.copy(out=xb[:, 0:4, :], in_=wst3[:, 0:4, :]), ["wst"], ["xb5"])
                    P.dve(lambda e: e.tensor_copy(out=xb[:, 4:8, :], in_=wst3[:, 4:8, :]), ["wst"], ["xb5"])
                    for et in range(8):
                        b_ = et % 2
                        for k in range(8):
                            P.pe(lambda e: e.matmul(pgt[b_][:], lhsT=Wgt[:, k, et * 128:(et + 1) * 128], rhs=xb[:, k, :], start=(k == 0), stop=(k == 7)),
                                 ["Wgt", "xb5"], ["bk%d" % (4 + b_)])
                        P.act(lambda e: e.activation(out=sgt[b_][:], in_=pgt[b_][:], func=AF.Silu), ["bk%d" % (4 + b_)], ["sgt%d" % b_])
                        P.act(lambda e: e.activation(out=yn[b_][:], in_=oretT[:, et, cs_], func=AF.Identity, scale=gg8[:, et:et + 1], bias=gb8[:, et:et + 1]),
                              ["oretT", "gg8", "gb8"], ["yn%d" % b_])
                        P.dve(lambda e: e.tensor_tensor(out=og[:, et, :], in0=yn[b_][:], in1=sgt[b_][:], op=ALU.mult), ["yn%d" % b_, "sgt%d" % b_], ["og"])
                    for ct in range(8):
                        cb = (ct % 2) * 4
                        cp = ct % 2
                        pg0, pg1, pu0, pu1 = bk[cb], bk[cb + 1], bk[cb + 2], bk[cb + 3]
                        kg0, kg1, ku0, ku1 = ["bk%d" % (cb + q_) for q_ in range(4)]
                        ga, gr, ma = ga2[cp], gr2[cp], ma2[cp]
                        for k in range(8):
                            P.pe(lambda e: e.matmul(pg0[:], lhsT=Wmg[:, k, ct * 128:(ct + 1) * 128], rhs=xb[:, k, :], start=(k == 0), stop=(k == 7)),
                                 ["Wmg", "xb5"], [kg0])
                        for k in range(8):
                            P.pe(lambda e: e.matmul(pg1[:], lhsT=Wmg[:, k, 1024 + ct * 128:1024 + (ct + 1) * 128], rhs=xb[:, k, :], start=(k == 0), stop=(k == 7)),
                                 ["Wmg", "xb5"], [kg1])
                        for k in range(4):
                            P.pe(lambda e: e.matmul(pu0[:], lhsT=Wa[:, k, ct * 128:(ct + 1) * 128], rhs=oattnT[:, k, cs_], start=(k == 0), stop=(k == 3)),
                                 ["Wa", "oattnT"], [ku0])
                        for k in range(8):
                            P.pe(lambda e: e.matmul(pu1[:], lhsT=Wr[:, k, ct * 128:(ct + 1) * 128], rhs=og[:, k, :], start=(k == 0), stop=(k == 7)),
                                 ["Wr", "og"], [ku1])
                        P.act(lambda e: e.activation(out=ga[:], in_=pg0[:], func=AF.Sigmoid), [kg0], ["ga%d" % cp])
                        P.act(lambda e: e.activation(out=gr[:], in_=pg1[:], func=AF.Sigmoid), [kg1], ["gr%d" % cp])
                        P.dve(lambda e: e.tensor_tensor(out=ma[:], in0=pu0[:], in1=ga[:], op=ALU.mult), [ku0, "ga%d" % cp], ["ma%d" % cp])
                        P.dve(lambda e: e.tensor_tensor(out=gr[:], in0=pu1[:], in1=gr[:], op=ALU.mult), [ku1, "gr%d" % cp], ["gr%d" % cp])
                        P.pool(lambda e: e.tensor_tensor(out=x1T[:, ct, cs_], in0=ma[:], in1=gr[:], op=ALU.add), ["ma%d" % cp, "gr%d" % cp], ["mx%d" % (oc * 4 + t_) for t_ in range(4)])
                dump("mergedT", x1T[:].rearrange("p a t -> p (a t)"), [128, 8 * TO], "mx0")
            P.barrier()
        B_.close()
        A_.close()
        if n_stage >= 5:
            with ExitStack() as S56:
                acc = sb(S56, "acc", [128, 16, 1024], F32)
                lng = sb(S56, "lng", [128, 1024], F32)
                lnb = sb(S56, "lnb", [128, 1024], F32)
                st12 = sb(S56, "st12", [128, 2, 6], F32)
                mv = sb(S56, "mv5", [128, 2], F32)
                rstd = sb(S56, "rstd5", [128, 1], F32)

                def layer_norm(src_ap, src_key, dst_ap, dst_key, tmp_ap, tmp_key):
                    for hf in range(2):
                        P.dve(lambda e: e.bn_stats(out=st12[:, hf, :], in_=src_ap[:, hf * 512:(hf + 1) * 512]), [src_key], ["st12"])
                    P.dve(lambda e: e.bn_aggr(out=mv[:], in_=st12[:].rearrange("p a s -> p (a s)")), ["st12"], ["mv5"])
                    P.dve(lambda e: e.tensor_scalar(out=rstd[:], in0=mv[:, 1:2], scalar1=LN_EPS, scalar2=None, op0=ALU.add), ["mv5"], ["rstd5"])
                    P.act(lambda e: e.activation(out=rstd[:], in_=rstd[:], func=AF.Sqrt), ["rstd5"], ["rstd5"])
                    P.dve(lambda e: e.reciprocal(out=rstd[:], in_=rstd[:]), ["rstd5"], ["rstd5"])
                    P.dve(lambda e: e.scalar_tensor_tensor(out=tmp_ap, in0=src_ap, scalar=mv[:, 0:1], in1=lng[:], op0=ALU.subtract, op1=ALU.mult),
                          [src_key, "mv5", "lng"], [tmp_key])
                    P.dve(lambda e: e.scalar_tensor_tensor(out=dst_ap, in0=tmp_ap, scalar=rstd[:, 0:1], in1=lnb[:], op0=ALU.mult, op1=ALU.add),
                          [tmp_key, "rstd5", "lnb"], [dst_key])

                with ExitStack() as S5b:
                    Wo = sb(S5b, "Wo", [128, 8, 1024], BF16)
                    load_cast(Wo[:].rearrange("p k n -> p (k n)"), wot, 8 * 1024, "Wo")
                    P.dma(lng[:], ln1g.partition_broadcast(128), writes=["lng"])
                    P.dma(lnb[:], ln1b.partition_broadcast(128), writes=["lnb"])
                    xres2 = [sb(S5b, "xres%d" % i_, [128, 1024], F32) for i_ in range(2)]
                    yt2 = [sb(S5b, "yt%d" % i_, [128, 1024], F32) for i_ in range(2)]
                    x12 = [sb(S5b, "x1_%d" % i_, [128, 1024], F32) for i_ in range(2)]
                    x1b2 = [sb(S5b, "x1b%d" % i_, [128, 1024], BF16) for i_ in range(2)]
                    pm2 = [[ps(S5b, "pm%d_%d" % (q_, i_), [128, 512]) for i_ in range(2)] for q_ in range(2)]
                    pTr = ps(S5b, "pTr5", [128, 128], BF16)
                    pend5 = []
                    for i in range(16):
                        q_ = i % 2
                        xres, yt, x1, x1b, pm = xres2[q_], yt2[q_], x12[q_], x1b2[q_], pm2[q_]
                        ts_ = slice(i * 128, (i + 1) * 128)
                        if len(pend5) >= 2:
                            pend5.pop(0)()
                        P.dma(xres[:], xo[ts_, :], writes=["xres%d" % q_])
                        for hf in range(2):
                            for k in range(8):
                                P.pe(lambda e: e.matmul(pm[hf][:], lhsT=x1T[:, k, ts_], rhs=Wo[:, k, hf * 512:(hf + 1) * 512], start=(k == 0), stop=(k == 7)),
                                     ["mx%d" % i, "Wo"], ["pm%d_%d" % (q_, hf)])
                            P.dve(lambda e: e.scalar_tensor_tensor(out=yt[:, hf * 512:(hf + 1) * 512], in0=xres[:, hf * 512:(hf + 1) * 512], scalar=ALPHA,
                                                                   in1=pm[hf][:], op0=ALU.mult, op1=ALU.add), ["xres%d" % q_, "pm%d_%d" % (q_, hf)], ["yt%d" % q_])
                        layer_norm(yt[:], "yt%d" % q_, x1[:], "x1_%d" % q_, yt[:], "yt%d" % q_)
                        if ("x1_%d" % i) in debug:
                            dump("x1_%d" % i, x1[:], [128, 1024], "x1_%d" % q_)
                        P.pool(lambda e: e.tensor_copy(out=x1b[:], in_=x1[:]), ["x1_%d" % q_], ["x1b%d" % q_])
                        P.pool(lambda e: e.tensor_scalar(out=acc[:, i, :], in0=x1[:], scalar1=ALPHA, scalar2=None, op0=ALU.mult), ["x1_%d" % q_], ["acc%d" % i])

                        def tr5(i=i, q_=q_, x1b=x1b, ts_=ts_):
                            for dt_ in range(8):
                                P.pe(lambda e: e.transpose(out=pTr[:], in_=x1b[:, dt_ * 128:(dt_ + 1) * 128], identity=identb[:]), ["x1b%d" % q_, "identb"], ["pTr5"])
                                P.act(lambda e: e.copy(out=x1T[:, dt_, ts_], in_=pTr[:]), ["pTr5"], ["mx%d" % i])
                        pend5.append(tr5)
                    while pend5:
                        pend5.pop(0)()
                P.barrier()
                if n_stage >= 6:
                    with ExitStack() as S6:
                        P.dma(lng[:], ln2g.partition_broadcast(128), writes=["lng"])
                        P.dma(lnb[:], ln2b.partition_broadcast(128), writes=["lnb"])
                        comb = sb(S6, "comb", [128, 16, 32], F32)
                        wrf = sb(S6, "wrf", [128, 8, 36], F32)
                        wrb = sb(S6, "wrb", [128, 8, 36], BF16)
                        brb = sb(S6, "brb", [128, 36], F32)
                        P.dma(wrf[:].rearrange("p k n -> p (k n)"), wrout, writes=["wrf"])
                        P.dve(lambda e: e.tensor_copy(out=wrb[:], in_=wrf[:]), ["wrf"], ["wrb"])
                        P.dma(brb[:], brout.partition_broadcast(128), writes=["brb"])
                        lg = sb(S6, "lg", [128, 16, 36], F32)
                        gmx = sb(S6, "gmx", [128, 16], F32)
                        gsh = sb(S6, "gsh", [128, 16, 4], F32)
                        gex = sb(S6, "gex", [128, 16, 4], F32)
                        gsum = sb(S6, "gsum", [128, 16], F32)
                        gprob = sb(S6, "gprob", [128, 16], F32)
                        ohg = sb(S6, "ohg", [128, 16, 4], F32)
                        tmp48 = sb(S6, "tmp48", [128, 16, 4, 8], F32)
                        isel = sb(S6, "isel", [128, 16, 8], F32)
                        isel2 = sb(S6, "isel2", [128, 16, 8], F32)
                        eq0 = sb(S6, "eq0", [128, 16, 8], F32)
                        eq1 = sb(S6, "eq1", [128, 16, 8], F32)
                        m0 = sb(S6, "m0r", [128, 16], F32)
                        m1 = sb(S6, "m1r", [128, 16], F32)
                        dlt = sb(S6, "dlt", [128, 16], F32)
                        w2e = sb(S6, "w2e", [128, 16], F32)
                        wsum = sb(S6, "wsum", [128, 16], F32)
                        wt1 = sb(S6, "wt1", [128, 16], F32)
                        wt2 = sb(S6, "wt2", [128, 16], F32)
                        ce = sb(S6, "ce", [128, 16, 8], F32)
                        ce2 = sb(S6, "ce2", [128, 16, 8], F32)
                        SR = ExitStack()
                        plg = [ps(SR, "plg%d" % i_, [128, 8, 36]) for i_ in range(2)]
                        AXX = mybir.AxisListType.X

                        def b3(ap, n):
                            return ap.unsqueeze(2).broadcast_to([128, 16, n])
                        for i in range(16):
                            ts_ = slice(i * 128, (i + 1) * 128)
                            for k in range(8):
                                P.pe(lambda e: e.matmul(plg[i // 8][:, i % 8, :], lhsT=x1T[:, k, ts_], rhs=wrb[:, k, :], start=(k == 0), stop=(k == 7)),
                                     ["mx%d" % i, "wrb"], ["plg%d" % (i // 8)])
                        for hf in range(2):
                            P.dve(lambda e: e.tensor_tensor(out=lg[:, hf * 8:(hf + 1) * 8, :], in0=plg[hf][:], in1=brb[:].unsqueeze(1).broadcast_to([128, 8, 36]), op=ALU.add),
                                  ["plg%d" % hf, "brb"], ["lg"])
                        P.dve(lambda e: e.tensor_reduce(out=gmx[:], in_=lg[:, :, 0:4], axis=AXX, op=ALU.max), ["lg"], ["gmx"])
                        P.dve(lambda e: e.tensor_tensor(out=gsh[:], in0=lg[:, :, 0:4], in1=b3(gmx[:], 4), op=ALU.subtract), ["lg", "gmx"], ["gsh"])
                        P.act(lambda e: e.activation(out=gex[:].rearrange("p t g -> p (t g)"), in_=gsh[:].rearrange("p t g -> p (t g)"), func=AF.Exp), ["gsh"], ["gex"])
                        P.dve(lambda e: e.tensor_reduce(out=gsum[:], in_=gex[:], axis=AXX, op=ALU.add), ["gex"], ["gsum"])
                        P.dve(lambda e: e.reciprocal(out=gprob[:], in_=gsum[:]), ["gsum"], ["gprob"])
                        P.dve(lambda e: e.tensor_scalar(out=ohg[:].rearrange("p t g -> p (t g)"), in0=gsh[:].rearrange("p t g -> p (t g)"), scalar1=0.0, scalar2=None, op0=ALU.is_ge),
                              ["gsh"], ["ohg"])
                        P.dve(lambda e: e.tensor_tensor(out=tmp48[:], in0=lg[:, :, 4:36].rearrange("p t (g e) -> p t g e", g=4),
                                                        in1=ohg[:].unsqueeze(3).broadcast_to([128, 16, 4, 8]), op=ALU.mult), ["lg", "ohg"], ["tmp48"])
                        P.dve(lambda e: e.tensor_reduce(out=isel[:], in_=tmp48[:].rearrange("p t g e -> p t e g"), axis=AXX, op=ALU.add), ["tmp48"], ["isel"])
                        P.dve(lambda e: e.tensor_reduce(out=m0[:], in_=isel[:], axis=AXX, op=ALU.max), ["isel"], ["m0r"])
                        P.dve(lambda e: e.tensor_tensor(out=eq0[:], in0=isel[:], in1=b3(m0[:], 8), op=ALU.is_equal), ["isel", "m0r"], ["eq0"])
                        P.dve(lambda e: e.scalar_tensor_tensor(out=isel2[:].rearrange("p t e -> p (t e)"), in0=eq0[:].rearrange("p t e -> p (t e)"), scalar=-1.0e30,
                                                               in1=isel[:].rearrange("p t e -> p (t e)"), op0=ALU.mult, op1=ALU.add), ["eq0", "isel"], ["isel2"])
                        P.dve(lambda e: e.tensor_reduce(out=m1[:], in_=isel2[:], axis=AXX, op=ALU.max), ["isel2"], ["m1r"])
                        P.dve(lambda e: e.tensor_tensor(out=eq1[:], in0=isel2[:], in1=b3(m1[:], 8), op=ALU.is_equal), ["isel2", "m1r"], ["eq1"])
                        P.dve(lambda e: e.tensor_tensor(out=dlt[:], in0=m1[:], in1=m0[:], op=ALU.subtract), ["m1r", "m0r"], ["dlt"])
                        P.act(lambda e: e.activation(out=w2e[:], in_=dlt[:], func=AF.Exp), ["dlt"], ["w2e"])
                        P.dve(lambda e: e.tensor_scalar(out=wsum[:], in0=w2e[:], scalar1=1.0, scalar2=None, op0=ALU.add), ["w2e"], ["wsum"])
                        P.dve(lambda e: e.reciprocal(out=wsum[:], in_=wsum[:]), ["wsum"], ["wsum"])
                        P.dve(lambda e: e.tensor_tensor(out=wt1[:], in0=wsum[:], in1=gprob[:], op=ALU.mult), ["wsum", "gprob"], ["wt1"])
                        P.dve(lambda e: e.tensor_tensor(out=wt2[:], in0=wt1[:], in1=w2e[:], op=ALU.mult), ["wt1", "w2e"], ["wt2"])
                        P.dve(lambda e: e.tensor_tensor(out=ce[:], in0=eq0[:], in1=b3(wt1[:], 8), op=ALU.mult), ["eq0", "wt1"], ["ce"])
                        P.dve(lambda e: e.tensor_tensor(out=ce2[:], in0=eq1[:], in1=b3(wt2[:], 8), op=ALU.mult), ["eq1", "wt2"], ["ce2"])
                        P.dve(lambda e: e.tensor_tensor(out=ce[:], in0=ce[:], in1=ce2[:], op=ALU.add), ["ce", "ce2"], ["ce"])
                        P.dve(lambda e: e.tensor_tensor(out=comb[:].rearrange("p t (g e) -> p t g e", g=4), in0=ce[:].unsqueeze(2).broadcast_to([128, 16, 4, 8]),
                                                        in1=ohg[:].unsqueeze(3).broadcast_to([128, 16, 4, 8]), op=ALU.mult), ["ce", "ohg"], ["comb"])
                        dump("comb", comb[:].rearrange("p a e -> p (a e)"), [128, 512], "comb")
                        SR.close()
                        P.barrier()
                        wstE = [wst[:, 0:2048], wst[:, 2048:4096]]
                        wE = [sb(S6, "wE%d" % i, [128, 12288], BF16) for i in range(2)]
                        sgE = [sb(S6, "sgE%d" % i, [128, 512], F32) for i in range(2)]
                        hT = [sb(S6, "hT%d" % i, [128, 4, 512], BF16) for i in range(2)]
                        pG = [ps(S6, "pG%d" % i, [128, 512]) for i in range(2)]
                        pU = [ps(S6, "pU%d" % i, [128, 512]) for i in range(2)]
                        pO = [ps(S6, "pO%d" % i, [128, 512]) for i in range(3)]
                        wsr = Ring([0, 1])
                        gr_ = Ring([0, 1])
                        or_ = Ring([0, 1, 2])
                        crr = [0]
                        n_exp = DEBUG.get("n_exp", 32)

                        def load_expert(ex):
                            ws = ex % 2
                            for pc in range(6):
                                si = wsr.next()
                                P.dma(wstE[si], wexp[ex, :, pc * 2048:(pc + 1) * 2048], writes=["wstE%d" % si])
                                d = wE[ws][:, pc * 2048:(pc + 1) * 2048]
                                P.pool(lambda e: e.tensor_copy(out=d, in_=wstE[si]), ["wstE%d" % si], ["wE%d" % ws])

                        def gu_phase(ex, cth):
                            ws = ex % 2
                            Wg = wE[ws][:, 0:4096].rearrange("p (k n) -> p k n", k=8)
                            Wu = wE[ws][:, 4096:8192].rearrange("p (k n) -> p k n", k=8)
                            wkey = "wE%d" % ws
                            cs_ = slice(cth * 512, (cth + 1) * 512)
                            xkeys = ["mx%d" % (cth * 4 + t_) for t_ in range(4)]
                            hs_ = cth % 2
                            for ft in range(4):
                                gi = gr_.next()
                                for k in range(8):
                                    P.pe(lambda e: e.matmul(pG[gi][:], lhsT=Wg[:, k, ft * 128:(ft + 1) * 128], rhs=x1T[:, k, cs_], start=(k == 0), stop=(k == 7)),
                                         [wkey] + xkeys, ["pG%d" % gi])
                                for k in range(8):
                                    P.pe(lambda e: e.matmul(pU[gi][:], lhsT=Wu[:, k, ft * 128:(ft + 1) * 128], rhs=x1T[:, k, cs_], start=(k == 0), stop=(k == 7)),
                                         [wkey] + xkeys, ["pU%d" % gi])
                                P.act(lambda e: e.activation(out=sgE[gi][:], in_=pG[gi][:], func=AF.Silu), ["pG%d" % gi], ["sgE%d" % gi])
                                P.dve(lambda e: e.tensor_tensor(out=hT[hs_][:, ft, :], in0=pU[gi][:], in1=sgE[gi][:], op=ALU.mult),
                                      ["pU%d" % gi, "sgE%d" % gi], ["hT%d" % hs_])

                        def d_phase(ex, cth):
                            ws = ex % 2
                            Wd = wE[ws][:, 8192:12288].rearrange("p (k n) -> p k n", k=4)
                            wkey = "wE%d" % ws
                            hs_ = cth % 2
                            for tt in range(4):
                                ti = cth * 4 + tt
                                for hf in range(2):
                                    oi = or_.next()
                                    for ft in range(4):
                                        P.pe(lambda e: e.matmul(pO[oi][:], lhsT=hT[hs_][:, ft, tt * 128:(tt + 1) * 128],
                                                                rhs=Wd[:, ft, hf * 512:(hf + 1) * 512], start=(ft == 0), stop=(ft == 3)),
                                             [wkey, "hT%d" % hs_], ["pO%d" % oi])
                                    a_ = acc[:, ti, hf * 512:(hf + 1) * 512]
                                    P.dve(lambda e: e.scalar_tensor_tensor(out=a_, in0=pO[oi][:], scalar=comb[:, ti, ex:ex + 1], in1=a_, op0=ALU.mult, op1=ALU.add),
                                          ["pO%d" % oi, "comb", "acc%d" % ti], ["acc%d" % ti])

                        units = [(ex, cth) for ex in range(n_exp) for cth in range(4)]
                        load_expert(0)
                        if n_exp > 1:
                            load_expert(1)
                        gu_phase(*units[0])
                        for u, (ex, cth) in enumerate(units):
                            if u + 1 < len(units):
                                gu_phase(*units[u + 1])
                            d_phase(ex, cth)
                            if cth == 3 and ex + 2 < n_exp:
                                load_expert(ex + 2)
                        yo = [sb(S6, "yo%d" % i, [128, 1024], F32) for i in range(2)]
                        for i in range(16):
                            yi = i % 2
                            layer_norm(acc[:, i, :], "acc%d" % i, yo[yi][:], "yo%d" % yi, yo[yi][:], "yo%d" % yi)
                            P.dma(out[i * 128:(i + 1) * 128, :], yo[yi][:], reads=["yo%d" % yi], writes=["out%d" % yi], sem="outd%d" % yi)
        fin_reads = ["out0", "out1"] + ["dbg_" + n for n in dbg_out]
        P.add("sp", lambda e: e.nop(), reads=fin_reads, sem="fin")
        if n_stage < 6:
            pass
        cnt = P.emit(G)
        nsem = len(cnt)
    return nc, dbg_out, nsem


def prep_shared(inp):
    f = lambda a: np.ascontiguousarray(np.asarray(a, dtype=np.float32))
    w_in = f(inp["w_in"])[0]
    sh = {}
    q = w_in[:, 0:512].reshape(1024, 2, 4, 64).transpose(0, 2, 1, 3).reshape(1024, 512)
    w1 = np.concatenate([q, w_in[:, 512:640], w_in[:, 640:768], w_in[:, 768:896], w_in[:, 1024:1152],
                         w_in[:, 896:1024], w_in[:, 1152:1280], w_in[:, 1280:1304]], axis=1)
    sh["w1t"] = tile_w(w1)
    sh["w4t"] = tile_w(w_in[:, 1304:3352])
    sh["wgtt"] = tile_w(w_in[:, 3352:4376])
    sh["wmgt"] = tile_w(w_in[:, 4376:6424])
    lit = {"k": (inp["cmp_k_w1"], inp["cmp_k_b1"], inp["cmp_pos_k"]), "v": (inp["cmp_v_w1"], inp["cmp_v_b1"], inp["cmp_pos_v"])}
    for kv in "kv":
        cw1 = f(lit[kv][0])[0]
        r = cw1.reshape(32, 64, 256).transpose(1, 0, 2).reshape(64, 32 * 256)
        sh["cw1" + kv] = np.ascontiguousarray(np.concatenate([r, r], axis=0))
        pos = f(lit[kv][2])[0]
        sh["cpos" + kv] = np.ascontiguousarray(np.concatenate([pos.T, pos.T], axis=0))
        sh["cb1" + kv] = np.ascontiguousarray(f(lit[kv][1])[0].reshape(2, 128).T)
    w2k = f(inp["cmp_k_w2"])[0]
    sh["cw2k"] = tile_w(np.concatenate([w2k, w2k], axis=1))
    sh["cw2v"] = tile_w(f(inp["cmp_v_w2"])[0])
    b2k = f(inp["cmp_k_b2"])[0]
    sh["cb2k"] = np.ascontiguousarray(np.concatenate([b2k, b2k])[:, None])
    sh["cb2v"] = f(inp["cmp_v_b2"])[0]
    sh["gng8"] = np.ascontiguousarray(f(inp["ret_gn_g"])[0].reshape(8, 128).T)
    sh["gnb8"] = np.ascontiguousarray(f(inp["ret_gn_b"])[0].reshape(8, 128).T)
    sh["wat"] = tile_w(f(inp["w_up_attn"])[0])
    sh["wrt"] = tile_w(f(inp["w_up_ret"])[0])
    sh["wot"] = tile_w(f(inp["w_out"])[0])
    for n in ("ln1_g", "ln1_b", "ln2_g", "ln2_b"):
        sh[n.replace("_", "")] = f(inp[n])[0]
    rg = f(inp["router_group_w"])[0]
    ri = f(inp["router_inner_w"])[0]
    sh["wrout"] = tile_w(np.concatenate([rg, ri.transpose(1, 0, 2).reshape(1024, 32)], axis=1))
    sh["brout"] = np.ascontiguousarray(np.concatenate([f(inp["router_group_b"])[0], f(inp["router_inner_b"])[0].reshape(32)]))
    wg = f(inp["expert_w_gate"])[0]
    wu = f(inp["expert_w_up"])[0]
    wd = f(inp["expert_w_down"])[0]
    we = np.empty((32, 128, 12288), np.float32)
    we[:, :, 0:4096] = wg.reshape(32, 8, 128, 512).transpose(0, 2, 1, 3).reshape(32, 128, 4096)
    we[:, :, 4096:8192] = wu.reshape(32, 8, 128, 512).transpose(0, 2, 1, 3).reshape(32, 128, 4096)
    we[:, :, 8192:12288] = wd.reshape(32, 4, 128, 1024).transpose(0, 2, 1, 3).reshape(32, 128, 4096)
    sh["wexp"] = we
    return sh


def make_in_maps(inp):
    sh = prep_shared(inp)
    x = np.asarray(inp["x"], dtype=np.float32)
    maps = []
    for core in range(8):
        b, c = core // 2, core % 2
        m = dict(sh)
        xb = x[b]
        own = xb.reshape(16, 2, 128, 1024)[:, c].reshape(TO, 1024)
        m["xT"] = np.ascontiguousarray(xb.T)
        m["xTo"] = np.ascontiguousarray(own.T)
        m["xo"] = np.ascontiguousarray(own)
        for k, v in make_consts(c).items():
            if not k.startswith("_"):
                m["c_" + k] = v
        maps.append(m)
    return maps


_PROG_CACHE = {}


def kernel(**inputs):
    if "prog" not in _PROG_CACHE:
        _PROG_CACHE["prog"] = build_program()
    nc, _, _ = _PROG_CACHE["prog"]
    maps = make_in_maps(inputs)
    res = run_bass_kernel_spmd(nc, maps, core_ids=list(range(8)))
    outp = np.empty((4, 16, 2, 128, 1024), np.float32)
    for core in range(8):
        b, c = core // 2, core % 2
        outp[b, :, c] = res.results[core]["out"].reshape(16, 128, 1024)
    return outp.reshape(4, T, 1024)
```

```python
import numpy as np
import ml_dtypes
import concourse.bass as bass
import concourse.mybir as mybir
from concourse.bass_utils import run_bass_kernel_spmd
from contextlib import ExitStack

F32 = mybir.dt.float32
BF16 = mybir.dt.bfloat16
AF = mybir.ActivationFunctionType
ALU = mybir.AluOpType
NPBF = ml_dtypes.bfloat16

T = 4096
D = 1024
TO = 2048
NEGM = -30000.0
LN_EPS = 1e-5
ALPHA = 2.0 ** 0.25
DEBUG = {}


class Op:
    __slots__ = ("eng", "fn", "reads", "writes", "dma", "sem", "deps", "needs_inc", "idx", "id", "extra")

    def __init__(self, eng, fn, reads, writes, dma, sem):
        self.eng = eng
        self.fn = fn
        self.reads = tuple(reads)
        self.writes = tuple(writes)
        self.dma = dma
        self.sem = sem
        self.deps = []
        self.needs_inc = dma
        self.idx = 0
        self.extra = ()


class _Rec:
    def __getattr__(self, name):
        return lambda *a, **k: (name, a, k)


_REC = _Rec()


class Prog:
    ENGS = ("pe", "act", "dve", "pool", "sp")

    def __init__(self, nc, same_eng_sync=True):
        self.nc = nc
        self.ops = []
        self.same_eng_sync = same_eng_sync
        self.last_by_sem = {}
        self.psum_keys = set()

    def add(self, eng, fn, reads=(), writes=(), dma=False, sem=None):
        lim = DEBUG.get("max_ops")
        self.nadd = getattr(self, "nadd", -1) + 1
        if (lim is not None and self.nadd >= lim and sem not in ("dbg", "fin")) or self.nadd in DEBUG.get("skip", ()):
            return Op(eng, None, reads, writes, dma, sem)
        if dma and sem is None:
            sem = "dma_" + str(writes[0])
        if not dma:
            sem = "eng_" + eng
        op = Op(eng, fn(_REC), reads, writes, dma, sem)
        if DEBUG.get("trace_ops"):
            print(len(self.ops), eng, op.fn[0], reads, writes)
        op.id = len(self.ops)
        self.ops.append(op)
        self.last_by_sem[sem] = op
        return op

    def pe(self, fn, reads=(), writes=()):
        return self.add("pe", fn, reads, writes)

    def act(self, fn, reads=(), writes=()):
        return self.add("act", fn, reads, writes)

    def dve(self, fn, reads=(), writes=()):
        return self.add("dve", fn, reads, writes)

    def pool(self, fn, reads=(), writes=()):
        return self.add("pool", fn, reads, writes)

    def dma(self, out, in_, reads=(), writes=(), sem=None, q="sp", **kw):
        return self.add(q, lambda e: e.dma_start(out=out, in_=in_, **kw), reads, writes, dma=True, sem=sem)

    def barrier(self):
        lasts = list(self.last_by_sem.values())
        for eng in self.ENGS:
            op = self.add(eng, lambda e: e.nop())
            op.extra = tuple(lasts)
        self.last_by_sem = {k: v for k, v in self.last_by_sem.items() if k.startswith("eng_")}

    def analyze(self):
        state = {}
        for op in self.ops:
            deps = set(op.extra)
            for k in op.reads:
                st = state.get(k)
                if st:
                    deps.update(st[0])
                    if k in self.psum_keys:
                        deps.update(r for r in st[1] if r.eng != op.eng)
            for k in op.writes:
                st = state.get(k)
                if st is None:
                    st = state[k] = [[], []]
                if st[1]:
                    deps.update(st[1])
                    deps.update(st[0])
                    st[0] = [op]
                    st[1] = []
                else:
                    same_group = op.dma and all(w.dma and w.sem == op.sem for w in st[0])
                    if same_group:
                        st[0].append(op)
                    else:
                        deps.update(st[0])
                        st[0] = [op]
            for k in op.reads:
                st = state.get(k)
                if st is None:
                    st = state[k] = [[], []]
                st[1].append(op)
            deps.discard(op)
            red = {}
            for d in deps:
                if (not d.dma) and (not op.dma) and d.eng == op.eng:
                    if op.eng == "pe" or not self.same_eng_sync:
                        continue
                cur = red.get(d.sem)
                if cur is None or d.id > cur.id:
                    red[d.sem] = d
            op.deps = list(red.values())
            for d in op.deps:
                d.needs_inc = True
        cnt = {}
        for op in self.ops:
            if op.needs_inc:
                cnt[op.sem] = cnt.get(op.sem, 0) + 1
                op.idx = cnt[op.sem]
        self.sem_names = sorted(cnt.keys())
        return cnt

    def emit(self, stack):
        nc = self.nc
        cnt = self.analyze()
        sems = {}
        for name in self.sem_names:
            sems[name] = stack.enter_context(nc.semaphore(name))
        block = stack.enter_context(nc.Block())
        per_eng = {e: [o for o in self.ops if o.eng == e] for e in self.ENGS}

        def run(eng_obj, ops):
            known = {}
            for op in ops:
                for d in op.deps:
                    val = d.idx * (16 if d.dma else 1)
                    if known.get(d.sem, 0) < val:
                        eng_obj.wait_ge(sems[d.sem], val)
                        known[d.sem] = val
                name, a, k = op.fn
                inst = getattr(eng_obj, name)(*a, **k)
                if op.needs_inc:
                    inst.then_inc(sems[op.sem], 16 if op.dma else 1)

        @block.sync
        def _(e):
            run(e, per_eng["sp"])

        @block.tensor
        def _(e):
            run(e, per_eng["pe"])

        @block.scalar
        def _(e):
            run(e, per_eng["act"])

        @block.vector
        def _(e):
            run(e, per_eng["dve"])

        @block.gpsimd
        def _(e):
            run(e, per_eng["pool"])
        return cnt


class Ring:
    def __init__(self, items):
        self.items = items
        self.i = 0

    def next(self):
        it = self.items[self.i % len(self.items)]
        self.i += 1
        return it


def tile_w(w):
    K, N = w.shape
    return np.ascontiguousarray(w.reshape(K // 128, 128, N).transpose(1, 0, 2).reshape(128, -1))


def rope_tabs(pos, d, scale):
    half = d // 2
    inv = 10000.0 ** (-np.arange(half, dtype=np.float64) * 2.0 / d)
    ang = pos.astype(np.float64)[None, :] * inv[:, None]
    cos = np.cos(ang) * scale
    sin = np.sin(ang) * scale
    reps = 128 // half
    return (np.tile(cos, (reps, 1)).astype(np.float32), np.tile(sin, (reps, 1)).astype(np.float32))


def rot_lhsT(d):
    half = d // 2
    Pm = np.zeros((128, 128), np.float32)
    for blk in range(128 // d):
        o = blk * d
        for m in range(half):
            Pm[o + m, o + m + half] = -1.0
            Pm[o + m + half, o + m] = 1.0
    return np.ascontiguousarray(Pm.T)


_CONST_CACHE = {}


def make_consts(c):
    if c in _CONST_CACHE:
        return _CONST_CACHE[c]
    cs = {}
    own_pos = np.concatenate([np.arange(128) + (2 * i + c) * 128 for i in range(16)])
    allpos = np.arange(T)
    cs["cosK"], cs["sinK"] = rope_tabs(allpos, 64, 1.0)
    cs["cosQ"], cs["sinQ"] = rope_tabs(own_pos, 64, 0.125)
    cs["cosRK"], cs["sinRK"] = rope_tabs(allpos, 128, 128.0 ** -0.5)
    cs["cosRQ"], cs["sinRQ"] = rope_tabs(own_pos, 128, 1.0)
    cend = np.arange(256) * 16 + 31
    cs["cosC"], cs["sinC"] = rope_tabs(cend, 64, 1.0)
    cs["pt64"] = rot_lhsT(64).astype(NPBF)
    cs["pt128"] = rot_lhsT(128).astype(NPBF)
    cs["identb"] = np.eye(128, dtype=np.float32).astype(NPBF)
    E = np.zeros((128, 32, 128), np.float32)
    for j in range(32):
        for k in range(128):
            E[2 * j + k // 64, j, k] = 1.0
            E[64 + 2 * j + k // 64, j, k] = 1.0
    cs["eall"] = E.reshape(128, -1).astype(NPBF)
    wm = np.zeros((128, 6, 128), np.float32)
    kk = np.arange(128)[:, None]
    tt = np.arange(128)[None, :]
    for r in range(6):
        dj = (r - 4) - c
        tk = dj * 128 + kk
        ok = (tk <= tt) & (tt - tk < 512)
        wm[:, r, :] = np.where(ok, 0.0, NEGM)
    cs["wmask"] = wm.reshape(128, -1).astype(NPBF)
    cm = np.zeros((128, 2, 16, 128), np.float32)
    for a in range(2):
        for i in range(16):
            G = 2 * i + c
            n = a * 128 + kk
            t = G * 128 + tt
            cm[:, a, i, :] = np.where(16 * n + 31 <= t, 0.0, NEGM)
    cs["cmpmask"] = cm.reshape(128, -1).astype(NPBF)
    cstart = np.arange(255) * 16
    sstart = np.arange(64) * 64
    ov = np.clip(np.minimum(cstart[None, :] + 32, sstart[:, None] + 64) - np.maximum(cstart[None, :], sstart[:, None]), 0, None) / 16.0
    ovT = np.zeros((256, 64), np.float32)
    ovT[:255] = ov.T
    cs["ovT"] = np.ascontiguousarray(ovT.reshape(2, 128, 64).transpose(1, 0, 2).reshape(128, -1)).astype(NPBF)
    tkm = np.zeros((128, 16, 64), np.float32)
    tkb = np.zeros((128, 16, 64), np.float32)
    for i in range(16):
        G = 2 * i + c
        for p in range(128):
            bt = (G * 128 + p) // 64
            for s in range(64):
                if s == 0:
                    tkb[p, i, s] = 1e9
                elif s == bt:
                    tkb[p, i, s] = 2e9
                elif s == bt - 1:
                    tkb[p, i, s] = 3e9
                elif s <= bt:
                    tkm[p, i, s] = 1.0
                else:
                    tkb[p, i, s] = -1e9 - 1e6 * s
    cs["tkm"] = tkm.reshape(128, -1)
    cs["tkb"] = tkb.reshape(128, -1)
    gam = 1.0 - 2.0 ** (-5.0 - np.arange(4, dtype=np.float64))
    lg = np.log(gam)
    m = np.arange(256)[:, None]
    cq = np.arange(128)[None, :]
    qq = 128 * c + cq
    Dc = np.zeros((128, 2, 4, 128), np.float32)
    for h in range(4):
        dd = np.where(qq >= m, np.exp(np.maximum(qq - m, 0) * lg[h]), 0.0)
        Dc[:, :, h, :] = dd.reshape(2, 128, 128).transpose(1, 0, 2)
    cs["Dc"] = Dc.reshape(128, -1)
    xi = np.zeros((128, 4, 128), np.float32)
    for h in range(4):
        xi[:, h, :] = np.exp((qq + 1.0) * lg[h])
    cs["xi"] = xi.reshape(128, -1)
    zt = np.zeros((128, 2, 4), np.float32)
    for h in range(4):
        zt[:, :, h] = np.exp((255.0 - np.arange(256)) * lg[h]).reshape(2, 128).T
    cs["zeta"] = zt.reshape(128, -1)
    cs["_decay256"] = [float(np.exp(256.0 * lg[h])) for h in range(4)]
    _CONST_CACHE[c] = cs
    return cs


CONST_SHAPES = None


def build_program(n_stage=6, debug=()):
    nc = bass.Bass("TRN2", target_bir_lowering=False)
    cs0 = make_consts(0)
    dram = {}

    def din(name, shape, dt=F32):
        dram[name] = nc.dram_tensor(name, list(shape), dt, kind="ExternalInput").ap()
        return dram[name]

    xT = din("xT", [1024, T])
    xTo = din("xTo", [1024, TO])
    xo = din("xo", [TO, 1024])
    w1t = din("w1t", [128, 8 * 1304])
    w4t = din("w4t", [128, 8 * 2048])
    wmgt = din("wmgt", [128, 8 * 2048])
    cw1 = {kv: din("cw1" + kv, [128, 32 * 256]) for kv in "kv"}
    cpos = {kv: din("cpos" + kv, [128, 32]) for kv in "kv"}
    cb1 = {kv: din("cb1" + kv, [128, 2]) for kv in "kv"}
    cw2k = din("cw2k", [128, 2 * 128])
    cw2v = din("cw2v", [128, 2 * 64])
    cb2k = din("cb2k", [128, 1])
    cb2v = din("cb2v", [64])
    gng8 = din("gng8", [128, 8])
    gnb8 = din("gnb8", [128, 8])
    wgtt = din("wgtt", [128, 8 * 1024])
    wat = din("wat", [128, 4 * 1024])
    wrt = din("wrt", [128, 8 * 1024])
    wot = din("wot", [128, 8 * 1024])
    ln1g = din("ln1g", [1024])
    ln1b = din("ln1b", [1024])
    ln2g = din("ln2g", [1024])
    ln2b = din("ln2b", [1024])
    wrout = din("wrout", [128, 8 * 36])
    brout = din("brout", [36])
    wexp = din("wexp", [32, 128, 12288])
    cdr = {}
    for k, v in cs0.items():
        if k.startswith("_"):
            continue
        cdr[k] = din("c_" + k, v.shape, BF16 if v.dtype == NPBF else F32)
    out = nc.dram_tensor("out", [TO, 1024], F32, kind="ExternalOutput").ap()
    dbg_out = {}

    decay256 = cs0["_decay256"]

    with ExitStack() as G:
        P = Prog(nc)

        def sb(stack, name, shape, dt):
            return stack.enter_context(nc.sbuf_tensor(name, list(shape), dt))

        def ps(stack, name, shape, dt=F32):
            P.psum_keys.add(name)
            ncol = 512 if dt == F32 else 1024
            full = stack.enter_context(nc.psum_tensor(name, [128, ncol], dt))
            n = 1
            for d_ in shape[1:]:
                n *= d_
            v = full[0:shape[0], 0:n]
            if len(shape) == 3:
                v = v.rearrange("p (a b) -> p a b", a=shape[1])
            return v

        def dump(name, ap, shape, key):
            if name in debug:
                t = nc.dram_tensor("dbg_" + name, list(shape), ap.dtype, kind="ExternalOutput").ap()
                dbg_out[name] = t
                P.dma(t, ap, reads=[key], writes=["dbg_" + name], sem="dbg")

        identb = sb(G, "identb", [128, 128], BF16)
        P.dma(identb[:], cdr["identb"], writes=["identb"])
        wst = sb(G, "wst", [128, 4096], F32)
        cast_rr = [0]

        def load_cast(dst_ap, src_ap, n, dst_key, shape3=None):
            o = 0
            while o < n:
                m = min(4096, n - o)
                P.dma(wst[:, 0:m], src_ap[:, o:o + m], writes=["wst"])
                d = dst_ap[:, o:o + m]
                if cast_rr[0] % 2 == 0:
                    P.act(lambda e, d=d, m=m: e.copy(out=d, in_=wst[:, 0:m]), ["wst"], [dst_key])
                else:
                    P.dve(lambda e, d=d, m=m: e.tensor_copy(out=d, in_=wst[:, 0:m]), ["wst"], [dst_key])
                cast_rr[0] += 1
                o += m

        x1T = sb(G, "x1T", [128, 8, TO], BF16)
        wst3 = wst[:].rearrange("p (k n) -> p k n", k=8)
        A_ = ExitStack()
        oattnT = sb(A_, "oattnT", [128, 4, TO], BF16)

        with ExitStack() as SN:
            QT = sb(SN, "QT", [128, 16, 4, 128], BF16)
            KE = [sb(SN, "KE%d" % i_, [128, T], BF16) for i_ in range(2)]
            P.dma(KE[0][64:128, :], cdr["eall"][64:128, :], writes=["KE0"])
            P.dma(KE[1][0:64, :], cdr["eall"][0:64, :], writes=["KE1"])
            winkT = sb(SN, "winkT", [128, T], BF16)
            slcv1 = sb(SN, "slcv1", [128, 32, 2, 65], BF16)
            winv1 = sb(SN, "winv1", [128, 32, 2, 65], BF16)
            gates = sb(SN, "gates", [128, 16, 24], F32)
            kcmpT = sb(SN, "kcmpT", [128, 256], BF16)
            vcmp1 = sb(SN, "vcmp1", [128, 2, 2, 65], BF16)
            pt64 = sb(SN, "pt64", [128, 128], BF16)
            P.dma(pt64[:], cdr["pt64"], writes=["pt64"])
            P.dve(lambda e: e.memset(slcv1[:].rearrange("p a g d -> p (a g d)"), 1.0), [], ["slcv1"])
            P.dve(lambda e: e.memset(winv1[:].rearrange("p a g d -> p (a g d)"), 1.0), [], ["winv1"])
            P.dve(lambda e: e.memset(kcmpT[:], 0.0), [], ["kcmpT"])
            P.dve(lambda e: e.memset(vcmp1[:].rearrange("p a g d -> p (a g d)"), 0.0), [], ["vcmp1"])
            P.dve(lambda e: e.memset(vcmp1[:, :, :, 64:65], 1.0), [], ["vcmp1"])

            with ExitStack() as S12:
                cmpT = {"k": sb(S12, "cmpkT", [128, T], BF16), "v": sb(S12, "cmpvT", [128, T], BF16)}
                with ExitStack() as S1:
                    Wn = sb(S1, "Wn", [128, 8, 1304], BF16)
                    load_cast(Wn[:].rearrange("p k n -> p (k n)"), w1t, 8 * 1304, "Wn")
                    xb = [sb(S1, "xb%d" % i, [128, 8, 512], BF16) for i in range(2)]
                    tabs = [sb(S1, "tab%d" % i, [128, 2, 512], F32) for i in range(2)]
                    ybf = [sb(S1, "ybf%d" % i, [128, 512], BF16) for i in range(2)]
                    t1 = [sb(S1, "t1_%d" % i, [128, 512], F32) for i in range(2)]
                    t2 = [sb(S1, "t2_%d" % i, [128, 512], F32) for i in range(2)]
                    pj = [ps(S1, "pj%d" % i, [128, 512]) for i in range(3)]
                    prot = [ps(S1, "prot%d" % i, [128, 512]) for i in range(2)]
                    pv = [ps(S1, "pv%d" % i, [128, 256]) for i in range(2)]
                    pjr = Ring(list(range(3)))
                    rr = Ring(list(range(2)))
                    pvr = Ring(list(range(2)))
                    xTv = xT.rearrange("(k p) t -> p k t", p=128)
                    xTov = xTo.rearrange("(k p) t -> p k t", p=128)

                    def load_x(src_view, c0, n, slot):
                        P.dma(wst3[:, :, 0:n], src_view[:, :, c0:c0 + n], writes=["wst"])
                        P.act(lambda e: e.copy(out=xb[slot][:, 0:4, 0:n], in_=wst3[:, 0:4, 0:n]), ["wst"], ["xb%d" % slot])
                        P.dve(lambda e: e.tensor_copy(out=xb[slot][:, 4:8, 0:n], in_=wst3[:, 4:8, 0:n]), ["wst"], ["xb%d" % slot])

                    def proj_fm(col0, slot, n=512):
                        pi = pjr.next()
                        for k in range(8):
                            P.pe(lambda e, k=k, pi=pi: e.matmul(pj[pi][:, 0:n], lhsT=Wn[:, k, col0:col0 + 128], rhs=xb[slot][:, k, 0:n],
                                                                 start=(k == 0), stop=(k == 7)), ["Wn", "xb%d" % slot], ["pj%d" % pi])
                        return pi

                    def rope_fm(pi, tslot, dst_ap, dst_key, ptm, ptkey, n=512, src=None, srckey=None):
                        r = rr.next()
                        srcap = pj[pi][:, 0:n] if src is None else src
                        sk = ("pj%d" % pi) if srckey is None else srckey
                        P.act(lambda e: e.copy(out=ybf[r][:, 0:n], in_=srcap), [sk], ["ybf%d" % r])
                        P.pe(lambda e: e.matmul(prot[r][:, 0:n], lhsT=ptm[:], rhs=ybf[r][:, 0:n], start=True, stop=True),
                             [ptkey, "ybf%d" % r], ["prot%d" % r])
                        P.dve(lambda e: e.tensor_tensor(out=t1[r][:, 0:n], in0=srcap, in1=tabs[tslot][:, 0, 0:n], op=ALU.mult),
                              [sk, "tab%d" % tslot], ["t1_%d" % r])
                        P.dve(lambda e: e.tensor_tensor(out=t2[r][:, 0:n], in0=prot[r][:, 0:n], in1=tabs[tslot][:, 1, 0:n], op=ALU.mult),
                              ["prot%d" % r, "tab%d" % tslot], ["t2_%d" % r])
                        if isinstance(dst_ap, list):
                            for (d_ap, rows, dkey) in dst_ap:
                                P.pool(lambda e: e.tensor_tensor(out=d_ap, in0=t1[r][rows, 0:n], in1=t2[r][rows, 0:n], op=ALU.add),
                                       ["t1_%d" % r, "t2_%d" % r], [dkey])
                        elif dst_key == "QT":
                            P.pool(lambda e: e.tensor_tensor(out=dst_ap, in0=t1[r][:, 0:n].rearrange("p (a t) -> p a t", a=4),
                                                             in1=t2[r][:, 0:n].rearrange("p (a t) -> p a t", a=4), op=ALU.add),
                                   ["t1_%d" % r, "t2_%d" % r], [dst_key])
                        else:
                            P.pool(lambda e: e.tensor_tensor(out=dst_ap, in0=t1[r][:, 0:n], in1=t2[r][:, 0:n], op=ALU.add),
                                   ["t1_%d" % r, "t2_%d" % r], [dst_key])

                    for ch in range(8):
                        slot = ch % 2
                        c0 = ch * 512
                        load_x(xTv, c0, 512, slot)
                        P.dma(tabs[slot][:, 0, :], cdr["cosK"][:, c0:c0 + 512], writes=["tab%d" % slot])
                        P.dma(tabs[slot][:, 1, :], cdr["sinK"][:, c0:c0 + 512], writes=["tab%d" % slot])
                        for col0, kv in ((512, "k"), (640, "v")):
                            pi = proj_fm(col0, slot)
                            P.act(lambda e, pi=pi, kv=kv: e.copy(out=cmpT[kv][:, c0:c0 + 512], in_=pj[pi][:]), ["pj%d" % pi], ["cmp" + kv + "T"])
                        pi = proj_fm(768, slot)
                        rope_fm(pi, slot, [(KE[0][0:64, c0:c0 + 512], slice(0, 64), "KE0"), (KE[1][64:128, c0:c0 + 512], slice(64, 128), "KE1")], None, pt64, "pt64")
                        pi = proj_fm(896, slot)
                        rope_fm(pi, slot, winkT[:, c0:c0 + 512], "winkT", pt64, "pt64")
                        for tt in range(4):
                            vi = pvr.next()
                            for k in range(8):
                                P.pe(lambda e, k=k, vi=vi, tt=tt: e.matmul(pv[vi][:], lhsT=xb[slot][:, k, tt * 128:(tt + 1) * 128], rhs=Wn[:, k, 1024:1280],
                                                                            start=(k == 0), stop=(k == 7)), ["Wn", "xb%d" % slot], ["pv%d" % vi])
                            tg = ch * 4 + tt
                            P.act(lambda e, vi=vi, tg=tg: e.copy(out=slcv1[:, tg, :, 0:64], in_=pv[vi][:, 0:128].rearrange("p (g d) -> p g d", g=2)),
                                  ["pv%d" % vi], ["slcv1"])
                            P.dve(lambda e, vi=vi, tg=tg: e.tensor_copy(out=winv1[:, tg, :, 0:64], in_=pv[vi][:, 128:256].rearrange("p (g d) -> p g d", g=2)),
                                  ["pv%d" % vi], ["winv1"])
                    for oc in range(4):
                        slot = oc % 2
                        c0 = oc * 512
                        load_x(xTov, c0, 512, slot)
                        P.dma(tabs[slot][:, 0, :], cdr["cosQ"][:, c0:c0 + 512], writes=["tab%d" % slot])
                        P.dma(tabs[slot][:, 1, :], cdr["sinQ"][:, c0:c0 + 512], writes=["tab%d" % slot])
                        for hh in range(4):
                            pi = proj_fm(hh * 128, slot)
                            rope_fm(pi, slot, QT[:, oc * 4:(oc + 1) * 4, hh, :], "QT", pt64, "pt64")
                        for tt in range(4):
                            vi = pvr.next()
                            for k in range(8):
                                P.pe(lambda e, k=k, vi=vi, tt=tt: e.matmul(pv[vi][:, 0:24], lhsT=xb[slot][:, k, tt * 128:(tt + 1) * 128], rhs=Wn[:, k, 1280:1304],
                                                                            start=(k == 0), stop=(k == 7)), ["Wn", "xb%d" % slot], ["pv%d" % vi])
                            tg = oc * 4 + tt
                            P.act(lambda e, vi=vi, tg=tg: e.activation(out=gates[:, tg, :], in_=pv[vi][:, 0:24], func=AF.Sigmoid), ["pv%d" % vi], ["gates"])
                    dump("QT", QT[:].rearrange("p i a t -> p (i a t)"), [128, 4 * TO], "QT")
                    dump("cmpkT", cmpT["k"][:], [128, T], "cmpkT")
                    dump("slcv1", slcv1[:].rearrange("p a g d -> p (a g d)"), [128, 32 * 130], "slcv1")
                    dump("gates", gates[:].rearrange("p a g -> p (a g)"), [128, 16 * 24], "gates")
                P.barrier()
                if n_stage >= 2:
                    with ExitStack() as S2:
                        w1b = sb(S2, "w1b", [128, 32, 256], BF16)
                        posT = sb(S2, "posT", [128, 32], F32)
                        posTb = sb(S2, "posTb", [128, 32], BF16)
                        b1 = sb(S2, "b1", [128, 2], F32)
                        bias1 = sb(S2, "bias1", [128, 2], F32)
                        w2kf = sb(S2, "w2kf", [128, 2, 128], F32)
                        w2k = sb(S2, "w2k", [128, 2, 128], BF16)
                        w2vf = sb(S2, "w2vf", [128, 2, 64], F32)
                        w2v = sb(S2, "w2v", [128, 2, 64], BF16)
                        b2k = sb(S2, "b2k", [128, 1], F32)
                        b2v = sb(S2, "b2v", [128, 64], F32)
                        tabC = sb(S2, "tabC", [128, 2, 256], F32)
                        h1 = sb(S2, "h1", [128, 2, 256], BF16)
                        xg = sb(S2, "xg", [128, 256], F32)
                        ug = sb(S2, "ug", [128, 256], F32)
                        sg_ = sb(S2, "sg_", [128, 256], F32)
                        yk = sb(S2, "yk", [128, 256], F32)
                        ykb = sb(S2, "ykb", [128, 256], BF16)
                        tk1 = sb(S2, "tk1", [128, 256], F32)
                        tk2 = sb(S2, "tk2", [128, 256], F32)
                        ph = [ps(S2, "ph%d" % i, [128, 256]) for i in range(2)]
                        pcv = ps(S2, "pcv", [128, 2])
                        pkc = ps(S2, "pkc", [128, 256])
                        prk = ps(S2, "prk", [128, 256])
                        pvc = ps(S2, "pvc", [128, 64])
                        P.dma(w2kf[:].rearrange("p a n -> p (a n)"), cw2k, writes=["w2kf"])
                        P.dve(lambda e: e.tensor_copy(out=w2k[:], in_=w2kf[:]), ["w2kf"], ["w2k"])
                        P.dma(w2vf[:].rearrange("p a n -> p (a n)"), cw2v, writes=["w2vf"])
                        P.dve(lambda e: e.tensor_copy(out=w2v[:], in_=w2vf[:]), ["w2vf"], ["w2v"])
                        P.dma(b2k[:], cb2k, writes=["b2k"])
                        P.dma(b2v[:], cb2v.partition_broadcast(128), writes=["b2v"])
                        P.dma(tabC[:, 0, :], cdr["cosC"], writes=["tabC"])
                        P.dma(tabC[:, 1, :], cdr["sinC"], writes=["tabC"])
                        for kv in "kv":
                            load_cast(w1b[:].rearrange("p l n -> p (l n)"), cw1[kv], 32 * 256, "w1b")
                            P.dma(posT[:], cpos[kv], writes=["posT"])
                            P.dve(lambda e: e.tensor_copy(out=posTb[:], in_=posT[:]), ["posT"], ["posTb"])
                            P.dma(b1[:], cb1[kv], writes=["b1"])
                            for ht in range(2):
                                for l in range(32):
                                    P.pe(lambda e, ht=ht, l=l: e.matmul(pcv[:, ht:ht + 1], lhsT=w1b[0:64, l, ht * 128:(ht + 1) * 128], rhs=posTb[0:64, l:l + 1],
                                                                         start=(l == 0), stop=(l == 31)), ["w1b", "posTb"], ["pcv"])
                            P.dve(lambda e: e.tensor_tensor(out=bias1[:], in0=pcv[:], in1=b1[:], op=ALU.add), ["pcv", "b1"], ["bias1"])
                            for g in range(2):
                                gp = slice(g * 64, (g + 1) * 64)
                                for ht in range(2):
                                    for l in range(32):
                                        P.pe(lambda e, ht=ht, l=l, gp=gp, kv=kv: e.matmul(ph[ht][:, 0:255], lhsT=w1b[gp, l, ht * 128:(ht + 1) * 128],
                                                                                        rhs=cmpT[kv][gp, l:l + 16 * 254 + 1:16],
                                                                                        start=(l == 0), stop=(l == 31)), ["w1b", "cmp" + kv + "T"], ["ph%d" % ht])
                                    P.act(lambda e, ht=ht: e.activation(out=xg[:, 0:255], in_=ph[ht][:, 0:255], func=AF.Identity, bias=bias1[:, ht:ht + 1], scale=1.0),
                                          ["ph%d" % ht, "bias1"], ["xg"])
                                    P.dve(lambda e: e.tensor_tensor(out=ug[:, 0:255], in0=xg[:, 0:255], in1=xg[:, 0:255], op=ALU.mult), ["xg"], ["ug"])
                                    P.dve(lambda e: e.tensor_scalar(out=ug[:, 0:255], in0=ug[:, 0:255], scalar1=0.044715, scalar2=1.0, op0=ALU.mult, op1=ALU.add), ["ug"], ["ug"])
                                    P.dve(lambda e: e.tensor_tensor(out=ug[:, 0:255], in0=ug[:, 0:255], in1=xg[:, 0:255], op=ALU.mult), ["ug", "xg"], ["ug"])
                                    P.act(lambda e: e.activation(out=sg_[:, 0:255], in_=ug[:, 0:255], func=AF.Sigmoid, scale=1.5957691216057308), ["ug"], ["sg_"])
                                    P.dve(lambda e, ht=ht: e.tensor_tensor(out=h1[:, ht, 0:255], in0=xg[:, 0:255], in1=sg_[:, 0:255], op=ALU.mult), ["xg", "sg_"], ["h1"])
                                if kv == "k":
                                    for ht in range(2):
                                        P.pe(lambda e, ht=ht: e.matmul(pkc[:, 0:255], lhsT=w2k[:, ht, :], rhs=h1[:, ht, 0:255], start=(ht == 0), stop=(ht == 1)),
                                             ["w2k", "h1"], ["pkc"])
                                    P.act(lambda e: e.activation(out=yk[:, 0:255], in_=pkc[:, 0:255], func=AF.Identity, bias=b2k[:, 0:1], scale=1.0), ["pkc", "b2k"], ["yk"])
                                    P.act(lambda e: e.copy(out=ykb[:, 0:255], in_=yk[:, 0:255]), ["yk"], ["ykb"])
                                    P.pe(lambda e: e.matmul(prk[:, 0:255], lhsT=pt64[:], rhs=ykb[:, 0:255], start=True, stop=True), ["pt64", "ykb"], ["prk"])
                                    P.dve(lambda e: e.tensor_tensor(out=tk1[:, 0:255], in0=yk[:, 0:255], in1=tabC[:, 0, 0:255], op=ALU.mult), ["yk", "tabC"], ["tk1"])
                                    P.dve(lambda e: e.tensor_tensor(out=tk2[:, 0:255], in0=prk[:, 0:255], in1=tabC[:, 1, 0:255], op=ALU.mult), ["prk", "tabC"], ["tk2"])
                                    P.dve(lambda e, gp=gp: e.tensor_tensor(out=kcmpT[gp, 0:255], in0=tk1[gp, 0:255], in1=tk2[gp, 0:255], op=ALU.add), ["tk1", "tk2"], ["kcmpT"])
                                else:
                                    for a in range(2):
                                        cntn = 128 if a == 0 else 127
                                        for ht in range(2):
                                            P.pe(lambda e, ht=ht, a=a, cntn=cntn: e.matmul(pvc[0:cntn, :], lhsT=h1[:, ht, a * 128:a * 128 + cntn], rhs=w2v[:, ht, :],
                                                                                            start=(ht == 0), stop=(ht == 1)), ["w2v", "h1"], ["pvc"])
                                        P.dve(lambda e, a=a, cntn=cntn, g=g: e.tensor_tensor(out=vcmp1[0:cntn, a, g, 0:64], in0=pvc[0:cntn, :], in1=b2v[0:cntn, :], op=ALU.add),
                                              ["pvc", "b2v"], ["vcmp1"])
                        dump("kcmpT", kcmpT[:], [128, 256], "kcmpT")
                        dump("vcmp1", vcmp1[:].rearrange("p a g d -> p (a g d)"), [128, 260], "vcmp1")
                    P.barrier()
            P.barrier()
            if n_stage >= 3:
                with ExitStack() as S3:
                    def bc4(ap):
                        return ap.unsqueeze(1).broadcast_to([ap.shape[0], 4, ap.shape[1]])

                    wmask = sb(S3, "wmask", [128, 6, 128], BF16)
                    cmpmask = sb(S3, "cmpmask", [128, 2, 16, 128], BF16)
                    ovT = sb(S3, "ovT", [128, 2, 64], BF16)
                    tkm = sb(S3, "tkm", [128, 16, 64], F32)
                    tkb = sb(S3, "tkb", [128, 16, 64], F32)
                    wmask4 = sb(S3, "wmask4", [128, 6, 512], BF16)
                    cm4 = [sb(S3, "cm4_%d" % i_, [128, 2, 512], BF16) for i_ in range(2)]
                    QN = [sb(S3, "QN%d" % i_, [128, 512], BF16) for i_ in range(4)]
                    P.dma(wmask[:].rearrange("p a k -> p (a k)"), cdr["wmask"], writes=["wmask"])
                    P.dma(cmpmask[:].rearrange("p a i k -> p (a i k)"), cdr["cmpmask"], writes=["cmpmask"])
                    P.dma(ovT[:].rearrange("p a k -> p (a k)"), cdr["ovT"], writes=["ovT"])
                    P.dma(tkm[:].rearrange("p a k -> p (a k)"), cdr["tkm"], writes=["tkm"])
                    P.dma(tkb[:].rearrange("p a k -> p (a k)"), cdr["tkb"], writes=["tkb"])
                    for r_ in range(6):
                        P.pool(lambda e: e.tensor_copy(out=wmask4[:, r_, :].rearrange("p (a t) -> p a t", a=4), in_=bc4(wmask[:, r_, :])), ["wmask"], ["wmask4"])
                    eT = [sb(S3, "eT%d" % i, [128, 512], BF16) for i in range(4)]
                    oacc = sb(S3, "oacc", [128, 512], F32)
                    oab = sb(S3, "oab", [128, 512], BF16)
                    rz = sb(S3, "rz", [128, 4], F32)
                    coef = sb(S3, "coef", [128, 4], F32)
                    imp = sb(S3, "imp", [128, 64], F32)
                    score = sb(S3, "score", [128, 64], F32)
                    work = sb(S3, "work", [128, 64], F32)
                    m8 = sb(S3, "m8", [128, 16], F32)
                    nmk = [sb(S3, "nmk%d" % i_, [128, 2, 64], BF16) for i_ in range(2)]
                    pST = [ps(S3, "pST%d" % i, [128, 512]) for i in range(3)]
                    pA = ps(S3, "pA", [128, 4, 65])
                    pB = ps(S3, "pB", [128, 4, 64])
                    pS = ps(S3, "pS", [128, 4, 65])
                    pW = ps(S3, "pW", [128, 4, 65])
                    pTr = ps(S3, "pTr", [128, 128], BF16)
                    str_ = Ring([0, 1, 2])
                    etr = Ring([0, 1, 2, 3])

                    def scores(kT_ap, kkey, g, i, masks, q_ap=None, qkey="QT"):
                        gp = slice(g * 64, (g + 1) * 64)
                        si = str_.next()
                        ei = etr.next()
                        nm = len(masks)
                        if q_ap is None:
                            q_ap = QT[gp, i, :, :].rearrange("p a t -> p (a t)")
                        P.pe(lambda e: e.matmul(pST[si][:], lhsT=kT_ap, rhs=q_ap, start=True, stop=(nm == 0)), [kkey, qkey], ["pST%d" % si])
                        for mi, (ml, mr, mkeys) in enumerate(masks):
                            P.pe(lambda e: e.matmul(pST[si][:], lhsT=ml, rhs=mr, start=False, stop=(mi == nm - 1)), mkeys, ["pST%d" % si])
                        P.act(lambda e: e.activation(out=eT[ei][:], in_=pST[si][:], func=AF.Exp), ["pST%d" % si], ["eT%d" % ei])
                        return ei

                    def finish_branch(pacc, pkey, i, g, br, first):
                        P.dve(lambda e: e.tensor_scalar(out=rz[:], in0=pacc[:, :, 64], scalar1=1e-30, scalar2=None, op0=ALU.max), [pkey], ["rz"])
                        P.dve(lambda e: e.reciprocal(out=rz[:], in_=rz[:]), ["rz"], ["rz"])
                        P.dve(lambda e: e.tensor_tensor(out=coef[:], in0=rz[:], in1=gates[:, i, g * 12 + br:g * 12 + 12:3], op=ALU.mult), ["rz", "gates"], ["coef"])
                        for hh in range(4):
                            o = oacc[:, g * 256 + hh * 64:g * 256 + (hh + 1) * 64]
                            if first:
                                P.dve(lambda e, hh=hh, o=o: e.tensor_scalar(out=o, in0=pacc[:, hh, 0:64], scalar1=coef[:, hh:hh + 1], scalar2=None, op0=ALU.mult),
                                      [pkey, "coef"], ["oacc"])
                            else:
                                P.dve(lambda e, hh=hh, o=o: e.scalar_tensor_tensor(out=o, in0=pacc[:, hh, 0:64], scalar=coef[:, hh:hh + 1], in1=o, op0=ALU.mult, op1=ALU.add),
                                      [pkey, "coef", "oacc"], ["oacc"])

                    tasks = []

                    def mk_cmp(i, g, a, na):
                        gp = slice(g * 64, (g + 1) * 64)

                        def sc():
                            if g == 0:
                                P.pool(lambda e: e.tensor_copy(out=cm4[i % 2][:, a, :].rearrange("p (h t) -> p h t", h=4), in_=bc4(cmpmask[:, a, i, :])),
                                       ["cmpmask"], ["cm4_%d" % (i % 2)])
                            return scores(kcmpT[gp, a * 128:(a + 1) * 128], "kcmpT", g, i,
                                          [(identb[:], cm4[i % 2][:, a, :], ["identb", "cm4_%d" % (i % 2)])])

                        def pvf(ei):
                            for hh in range(4):
                                P.pe(lambda e: e.matmul(pA[:, hh, :], lhsT=eT[ei][:, hh * 128:(hh + 1) * 128], rhs=vcmp1[:, a, g, :],
                                                        start=(a == 0 and hh == 0), stop=(a == na - 1 and hh == 3)), ["eT%d" % ei, "vcmp1"], ["pA"])
                                P.pe(lambda e: e.matmul(pB[:, hh, :], lhsT=eT[ei][:, hh * 128:(hh + 1) * 128], rhs=ovT[:, a, :],
                                                        start=(a == 0 and hh == 0), stop=(a == na - 1 and hh == 3)), ["eT%d" % ei, "ovT"], ["pB"])

                        def post():
                            finish_branch(pA, "pA", i, g, 0, True)
                            P.dve(lambda e: e.tensor_scalar(out=imp[:], in0=pB[:, 0, :], scalar1=rz[:, 0:1], scalar2=None, op0=ALU.mult), ["pB", "rz"], ["imp"])
                            for hh in range(1, 4):
                                P.dve(lambda e: e.scalar_tensor_tensor(out=imp[:], in0=pB[:, hh, :], scalar=rz[:, hh:hh + 1], in1=imp[:], op0=ALU.mult, op1=ALU.add),
                                      ["pB", "rz", "imp"], ["imp"])
                            P.dve(lambda e: e.tensor_tensor(out=score[:], in0=imp[:], in1=tkm[:, i, :], op=ALU.mult), ["imp", "tkm"], ["score"])
                            P.dve(lambda e: e.tensor_tensor(out=score[:], in0=score[:], in1=tkb[:, i, :], op=ALU.add), ["score", "tkb"], ["score"])
                            P.dve(lambda e: e.max(out=m8[:, 0:8], in_=score[:]), ["score"], ["m8"])
                            P.dve(lambda e: e.match_replace(out=work[:], in_to_replace=m8[:, 0:8], in_values=score[:], imm_value=-3.0e38), ["score", "m8"], ["work"])
                            P.dve(lambda e: e.max(out=m8[:, 8:16], in_=work[:]), ["work"], ["m8"])
                            P.dve(lambda e: e.tensor_scalar(out=nmk[g][:], in0=score[:].unsqueeze(1).broadcast_to([128, 2, 64]), scalar1=m8[:, 15:16], scalar2=NEGM,
                                                            op0=ALU.is_lt, op1=ALU.mult), ["score", "m8"], ["nmk%d" % g])
                            if ("imp%d_%d" % (i, g)) in debug:
                                dump("imp%d_%d" % (i, g), imp[:], [128, 64], "imp")
                                dump("score%d_%d" % (i, g), score[:], [128, 64], "score")
                                dump("m8%d_%d" % (i, g), m8[:], [128, 16], "m8")
                        return [None, sc, pvf, post if a == na - 1 else None]

                    def mk_win(i, g, idx, r, j, nw):
                        gp = slice(g * 64, (g + 1) * 64)

                        def sc():
                            return scores(winkT[gp, j * 128:(j + 1) * 128], "winkT", g, i,
                                          [(identb[:], wmask4[:, r, :], ["identb", "wmask4"])])

                        def pvf(ei):
                            for hh in range(4):
                                P.pe(lambda e: e.matmul(pW[:, hh, :], lhsT=eT[ei][:, hh * 128:(hh + 1) * 128], rhs=winv1[:, j, g, :],
                                                        start=(idx == 0 and hh == 0), stop=(idx == nw - 1 and hh == 3)), ["eT%d" % ei, "winv1"], ["pW"])

                        def post():
                            finish_branch(pW, "pW", i, g, 2, False)
                        return [None, sc, pvf, post if idx == nw - 1 else None]

                    def tile_end_pe(i):
                        for ct in range(4):
                            P.pe(lambda e: e.transpose(out=pTr[:], in_=oab[:, ct * 128:(ct + 1) * 128], identity=identb[:]), ["oab", "identb"], ["pTr"])
                            P.dve(lambda e: e.tensor_copy(out=oattnT[:, ct, i * 128:(i + 1) * 128], in_=pTr[:]), ["pTr"], ["oattnT"])

                    def mk_slc(i, g, j, nj):
                        gp = slice(g * 64, (g + 1) * 64)

                        qn_i = (2 * i + g) % 4
                        oh = slice((1 - g) * 64, (2 - g) * 64)

                        def pre():
                            P.pool(lambda e: e.tensor_copy(out=QN[qn_i][gp, :], in_=QT[gp, i, :, :].rearrange("p a t -> p (a t)")), ["QT"], ["QN%d" % qn_i])
                            P.pe(lambda e: e.transpose(out=pTr[:], in_=nmk[g][:].rearrange("p a s -> p (a s)"), identity=identb[:]), ["nmk%d" % g, "identb"], ["pTr"])
                            P.dve(lambda e: e.tensor_copy(out=QN[qn_i][oh, :].rearrange("p (a t) -> p a t", a=4), in_=bc4(pTr[oh, :])), ["pTr"], ["QN%d" % qn_i])
                            if g == 0 and i > 0:
                                tile_end_pe(i - 1)

                        def sc():
                            masks = []
                            if j >= 2 * i:
                                masks.append((identb[:], wmask4[:, 4 + (j - 2 * i), :], ["identb", "wmask4"]))
                            return scores(KE[g][:, j * 128:(j + 1) * 128], "KE%d" % g, g, i, masks, q_ap=QN[qn_i][:], qkey="QN%d" % qn_i)

                        def pvf(ei):
                            for hh in range(4):
                                P.pe(lambda e: e.matmul(pS[:, hh, :], lhsT=eT[ei][:, hh * 128:(hh + 1) * 128], rhs=slcv1[:, j, g, :],
                                                        start=(j == 0 and hh == 0), stop=(j == nj - 1 and hh == 3)), ["eT%d" % ei, "slcv1"], ["pS"])

                        def post():
                            finish_branch(pS, "pS", i, g, 1, False)
                            if g == 1:
                                if ("oacc%d" % i) in debug:
                                    dump("oacc%d" % i, oacc[:], [128, 512], "oacc")
                                P.pool(lambda e: e.tensor_copy(out=oab[:], in_=oacc[:]), ["oacc"], ["oab"])
                        return [pre if j == 0 else None, sc, pvf, post if j == nj - 1 else None]

                    for i in range(16):
                        for g in range(2):
                            na = 1 if i < 8 else 2
                            for a in range(na):
                                tasks.append(mk_cmp(i, g, a, na))
                            js = [(r, 2 * i - 4 + r) for r in range(6) if 2 * i - 4 + r >= 0]
                            for idx, (r, j) in enumerate(js):
                                tasks.append(mk_win(i, g, idx, r, j, len(js)))
                            nj = 2 * i + 2
                            for j in range(nj):
                                tasks.append(mk_slc(i, g, j, nj))
                    nt = len(tasks)
                    eis = [None] * nt

                    def emit_score(k):
                        if tasks[k][0] is not None:
                            tasks[k][0]()
                        eis[k] = tasks[k][1]()

                    emit_score(0)
                    emit_score(1)
                    for k in range(nt):
                        if k + 2 < nt:
                            emit_score(k + 2)
                        tasks[k][2](eis[k])
                        if tasks[k][3] is not None:
                            tasks[k][3]()
                    tile_end_pe(15)
                    dump("oattnT", oattnT[:].rearrange("p a t -> p (a t)"), [128, 4 * TO], "oattnT")
                P.barrier()
        P.barrier()

        B_ = ExitStack()
        oretT = sb(B_, "oretT", [128, 8, TO], BF16)
        if n_stage >= 4:
            with ExitStack() as S4:
                W4 = sb(S4, "W4", [128, 8, 2048], BF16)
                load_cast(W4[:].rearrange("p k n -> p (k n)"), w4t, 8 * 2048, "W4")
                pt128 = sb(S4, "pt128", [128, 128], BF16)
                P.dma(pt128[:], cdr["pt128"], writes=["pt128"])
                Dc = sb(S4, "Dc", [128, 2, 4, 128], F32)
                xi = sb(S4, "xi", [128, 4, 128], F32)
                zeta = sb(S4, "zeta", [128, 2, 4], F32)
                P.dma(Dc[:].rearrange("p a h c -> p (a h c)"), cdr["Dc"], writes=["Dc"])
                P.dma(xi[:].rearrange("p h c -> p (h c)"), cdr["xi"], writes=["xi"])
                P.dma(zeta[:].rearrange("p a h -> p (a h)"), cdr["zeta"], writes=["zeta"])
                xst = wst3
                xb = sb(S4, "xb4", [128, 8, 512], BF16)
                xob = sb(S4, "xob4", [128, 8, 256], BF16)
                tabs = sb(S4, "tab4", [128, 2, 512], F32)
                tabq = sb(S4, "tabq4", [128, 2, 256], F32)
                ybf2 = [sb(S4, "ybf4_%d" % i_, [128, 512], BF16) for i_ in range(2)]
                t12 = [sb(S4, "t1_4_%d" % i_, [128, 512], F32) for i_ in range(2)]
                t22 = [sb(S4, "t2_4_%d" % i_, [128, 512], F32) for i_ in range(2)]
                rr4 = Ring([0, 1])
                kT = sb(S4, "kT4", [128, 4, 512], BF16)
                qT = sb(S4, "qT4", [128, 4, 256], BF16)
                qxT = sb(S4, "qxT4", [128, 4, 256], BF16)
                vtok = sb(S4, "vtok", [128, 4, 1024], BF16)
                kz = sb(S4, "kz", [128, 4, 4, 128], BF16)
                R = sb(S4, "R", [128, 4, 256], F32)
                Rb = sb(S4, "Rb", [128, 4, 256], BF16)
                sc = [sb(S4, "sc%d" % i_, [128, 2, 128], BF16) for i_ in range(2)]
                epsT = sb(S4, "epsT", [128, 1], F32)
                P.dve(lambda e: e.memset(epsT[:], LN_EPS), [], ["epsT"])
                pending4 = []
                st6 = sb(S4, "st6", [128, 6], F32)
                mv = sb(S4, "mv", [128, 2], F32)
                rstd = sb(S4, "rstd", [128, 1], F32)
                oretb = [sb(S4, "oretb%d" % i_, [128, 1024], BF16) for i_ in range(2)]
                pj = [ps(S4, "pj4_%d" % i, [128, 512]) for i in range(3)]
                psc = [ps(S4, "psc%d" % i_, [128, 2, 128]) for i_ in range(2)]
                po = [ps(S4, "po%d" % i_, [128, 256]) for i_ in range(2)]
                pTrw = ps(S4, "pTr4", [128, 512], BF16)
                pTr = pTrw[:, 0:128]
                pjr = Ring([0, 1, 2])
                P.dve(lambda e: e.memset(R[:].rearrange("p h e -> p (h e)"), 0.0), [], ["R%d" % h_ for h_ in range(4)])
                P.dve(lambda e: e.memset(Rb[:].rearrange("p h e -> p (h e)"), 0.0), [], ["Rb%d" % h_ for h_ in range(4)])
                xTv = xT.rearrange("(k p) t -> p k t", p=128)
                xTov = xTo.rearrange("(k p) t -> p k t", p=128)

                def rope4(pi, n, tab, tabkey, dst_ap, dst_key):
                    ri = pjr.next()
                    prot = pj[ri]
                    rb = rr4.next()
                    ybf, t1, t2 = ybf2[rb], t12[rb], t22[rb]
                    P.act(lambda e: e.copy(out=ybf[:, 0:n], in_=pj[pi][:, 0:n]), ["pj4_%d" % pi], ["ybf4_%d" % rb])
                    P.pe(lambda e: e.matmul(prot[:, 0:n], lhsT=pt128[:], rhs=ybf[:, 0:n], start=True, stop=True), ["pt128", "ybf4_%d" % rb], ["pj4_%d" % ri])
                    P.dve(lambda e: e.tensor_tensor(out=t1[:, 0:n], in0=pj[pi][:, 0:n], in1=tab[:, 0, 0:n], op=ALU.mult), ["pj4_%d" % pi, tabkey], ["t1_4_%d" % rb])
                    P.dve(lambda e: e.tensor_tensor(out=t2[:, 0:n], in0=prot[:, 0:n], in1=tab[:, 1, 0:n], op=ALU.mult), ["pj4_%d" % ri, tabkey], ["t2_4_%d" % rb])
                    P.pool(lambda e: e.tensor_tensor(out=dst_ap, in0=t1[:, 0:n], in1=t2[:, 0:n], op=ALU.add), ["t1_4_%d" % rb, "t2_4_%d" % rb], [dst_key])

                for gch in range(8):
                    c0 = gch * 512
                    o0 = gch * 256
                    P.dma(xst[:], xTv[:, :, c0:c0 + 512], writes=["wst"])
                    P.act(lambda e: e.copy(out=xb[:, 0:4, :], in_=xst[:, 0:4, :]), ["wst"], ["xb4"])
                    P.dve(lambda e: e.tensor_copy(out=xb[:, 4:8, :], in_=xst[:, 4:8, :]), ["wst"], ["xb4"])
                    P.dma(xst[:, :, 0:256], xTov[:, :, o0:o0 + 256], writes=["wst"])
                    P.act(lambda e: e.copy(out=xob[:, 0:4, :], in_=xst[:, 0:4, 0:256]), ["wst"], ["xob4"])
                    P.dve(lambda e: e.tensor_copy(out=xob[:, 4:8, :], in_=xst[:, 4:8, 0:256]), ["wst"], ["xob4"])
                    P.dma(tabs[:, 0, :], cdr["cosRK"][:, c0:c0 + 512], writes=["tab4"])
                    P.dma(tabs[:, 1, :], cdr["sinRK"][:, c0:c0 + 512], writes=["tab4"])
                    P.dma(tabq[:, 0, :], cdr["cosRQ"][:, o0:o0 + 256], writes=["tabq4"])
                    P.dma(tabq[:, 1, :], cdr["sinRQ"][:, o0:o0 + 256], writes=["tabq4"])
                    for h in range(4):
                        pi = pjr.next()
                        for k in range(8):
                            P.pe(lambda e, k=k, pi=pi, h=h: e.matmul(pj[pi][:], lhsT=W4[:, k, 512 + h * 128:512 + (h + 1) * 128], rhs=xb[:, k, :],
                                                                      start=(k == 0), stop=(k == 7)), ["W4", "xb4"], ["pj4_%d" % pi])
                        rope4(pi, 512, tabs, "tab4", kT[:, h, :], "kT4")
                    for h in range(4):
                        pi = pjr.next()
                        for k in range(8):
                            P.pe(lambda e, k=k, pi=pi, h=h: e.matmul(pj[pi][:, 0:256], lhsT=W4[:, k, h * 128:(h + 1) * 128], rhs=xob[:, k, :],
                                                                      start=(k == 0), stop=(k == 7)), ["W4", "xob4"], ["pj4_%d" % pi])
                        rope4(pi, 256, tabq, "tabq4", qT[:, h, :], "qT4")
                    for pp in range(2):
                        P.dve(lambda e, pp=pp: e.tensor_tensor(out=qxT[:, :, pp * 128:(pp + 1) * 128], in0=qT[:, :, pp * 128:(pp + 1) * 128], in1=xi[:], op=ALU.mult),
                              ["qT4", "xi"], ["qxT4"])
                    for tt in range(4):
                        for hf in range(2):
                            pi = pjr.next()
                            for k in range(8):
                                P.pe(lambda e, k=k, pi=pi, tt=tt, hf=hf: e.matmul(pj[pi][:], lhsT=xb[:, k, tt * 128:(tt + 1) * 128],
                                                                                   rhs=W4[:, k, 1024 + hf * 512:1024 + (hf + 1) * 512],
                                                                                   start=(k == 0), stop=(k == 7)), ["W4", "xb4"], ["pj4_%d" % pi])
                            P.act(lambda e, pi=pi, tt=tt, hf=hf: e.copy(out=vtok[:, tt, hf * 512:(hf + 1) * 512], in_=pj[pi][:]), ["pj4_%d" % pi], ["vtok"])
                    for tt in range(4):
                        for h in range(4):
                            P.pe(lambda e: e.transpose(out=pTrw[:, h * 128:(h + 1) * 128], in_=kT[:, h, tt * 128:(tt + 1) * 128], identity=identb[:]), ["kT4", "identb"], ["pTr4"])
                        P.dve(lambda e: e.tensor_tensor(out=kz[:, tt, :, :], in0=pTrw[:].rearrange("p (h d) -> p h d", h=4),
                                                        in1=zeta[:, tt % 2, :].unsqueeze(2).broadcast_to([128, 4, 128]), op=ALU.mult), ["pTr4", "zeta"], ["kz"])
                    units = [(pp, h) for pp in range(2) for h in range(4)]

                    def phaseA(pp, h, ub):
                        qs = slice(pp * 128, (pp + 1) * 128)
                        for mt in range(2):
                            tt = pp * 2 + mt
                            P.pe(lambda e: e.matmul(psc[ub][:, mt, :], lhsT=kT[:, h, tt * 128:(tt + 1) * 128], rhs=qT[:, h, qs], start=True, stop=True),
                                 ["kT4", "qT4"], ["psc%d" % ub])
                        P.dve(lambda e: e.tensor_tensor(out=sc[ub][:], in0=psc[ub][:], in1=Dc[:, :, h, :], op=ALU.mult), ["psc%d" % ub, "Dc"], ["sc%d" % ub])

                    def phaseBC(pp, h, ub):
                        i = gch * 2 + pp
                        qs = slice(pp * 128, (pp + 1) * 128)
                        hs = slice(h * 256, (h + 1) * 256)
                        ob = oretb[pp]
                        for mt in range(2):
                            tt = pp * 2 + mt
                            P.pe(lambda e: e.matmul(po[ub][:], lhsT=sc[ub][:, mt, :], rhs=vtok[:, tt, hs], start=(mt == 0), stop=False), ["sc%d" % ub, "vtok"], ["po%d" % ub])
                        P.pe(lambda e: e.matmul(po[ub][:], lhsT=qxT[:, h, qs], rhs=Rb[:, h, :], start=False, stop=True), ["qxT4", "Rb%d" % h], ["po%d" % ub])
                        ri = pjr.next()
                        for mt in range(2):
                            tt = pp * 2 + mt
                            P.pe(lambda e: e.matmul(pj[ri][:, 0:256], lhsT=kz[:, tt, h, :], rhs=vtok[:, tt, hs], start=(mt == 0), stop=(mt == 1)),
                                 ["kz", "vtok"], ["pj4_%d" % ri])
                        P.dve(lambda e: e.bn_stats(out=st6[:], in_=po[ub][:]), ["po%d" % ub], ["st6"])
                        P.dve(lambda e: e.bn_aggr(out=mv[:], in_=st6[:]), ["st6"], ["mv"])
                        P.act(lambda e: e.activation(out=rstd[:], in_=mv[:, 1:2], func=AF.Sqrt, bias=epsT[:, 0:1], scale=1.0), ["mv", "epsT"], ["rstd"])
                        P.dve(lambda e: e.reciprocal(out=rstd[:], in_=rstd[:]), ["rstd"], ["rstd"])
                        P.dve(lambda e: e.tensor_scalar(out=ob[:, hs], in0=po[ub][:], scalar1=mv[:, 0:1], scalar2=rstd[:, 0:1], op0=ALU.subtract, op1=ALU.mult),
                              ["po%d" % ub, "mv", "rstd"], ["oretb%d" % pp])
                        P.dve(lambda e: e.scalar_tensor_tensor(out=R[:, h, :], in0=R[:, h, :], scalar=decay256[h], in1=pj[ri][:, 0:256], op0=ALU.mult, op1=ALU.add),
                              ["R%d" % h, "pj4_%d" % ri], ["R%d" % h])
                        P.act(lambda e: e.copy(out=Rb[:, h, :], in_=R[:, h, :]), ["R%d" % h], ["Rb%d" % h])

                    def pair_end(pp, i):
                        ob = oretb[pp]
                        for et in range(8):
                            P.pe(lambda e: e.transpose(out=pTr[:], in_=ob[:, et * 128:(et + 1) * 128], identity=identb[:]), ["oretb%d" % pp, "identb"], ["pTr4"])
                            P.act(lambda e: e.copy(out=oretT[:, et, i * 128:(i + 1) * 128], in_=pTr[:]), ["pTr4"], ["oretT"])

                    phaseA(units[0][0], units[0][1], 0)
                    for u, (pp, h) in enumerate(units):
                        if u + 1 < len(units):
                            phaseA(units[u + 1][0], units[u + 1][1], (u + 1) % 2)
                        phaseBC(pp, h, u % 2)
                        if pending4:
                            pending4.pop(0)()
                        if h == 3:
                            pending4.append(lambda pp=pp, i=gch * 2 + pp: pair_end(pp, i))
                while pending4:
                    pending4.pop(0)()
                dump("oretT", oretT[:].rearrange("p a t -> p (a t)"), [128, 8 * TO], "oretT")
            P.barrier()

        if n_stage >= 5:
            with ExitStack() as S5a:
                Wmg = sb(S5a, "Wmg", [128, 8, 2048], BF16)
                Wa = sb(S5a, "Wa", [128, 4, 1024], BF16)
                Wr = sb(S5a, "Wr", [128, 8, 1024], BF16)
                Wgt = sb(S5a, "Wgt", [128, 8, 1024], BF16)
                load_cast(Wmg[:].rearrange("p k n -> p (k n)"), wmgt, 8 * 2048, "Wmg")
                load_cast(Wa[:].rearrange("p k n -> p (k n)"), wat, 4 * 1024, "Wa")
                load_cast(Wr[:].rearrange("p k n -> p (k n)"), wrt, 8 * 1024, "Wr")
                load_cast(Wgt[:].rearrange("p k n -> p (k n)"), wgtt, 8 * 1024, "Wgt")
                gg8 = sb(S5a, "gg8", [128, 8], F32)
                gb8 = sb(S5a, "gb8", [128, 8], F32)
                P.dma(gg8[:], gng8, writes=["gg8"])
                P.dma(gb8[:], gnb8, writes=["gb8"])
                xb = sb(S5a, "xb5", [128, 8, 512], BF16)
                og = sb(S5a, "og", [128, 8, 512], BF16)
                sgt = [sb(S5a, "sgt%d" % i, [128, 512], F32) for i in range(2)]
                yn = [sb(S5a, "yn%d" % i, [128, 512], F32) for i in range(2)]
                ga2 = [sb(S5a, "ga%d" % i, [128, 512], F32) for i in range(2)]
                gr2 = [sb(S5a, "gr%d" % i, [128, 512], F32) for i in range(2)]
                ma2 = [sb(S5a, "ma%d" % i, [128, 512], F32) for i in range(2)]
                bk = [ps(S5a, "bk%d" % i, [128, 512]) for i in range(8)]
                pgt = [bk[4], bk[5]]
                xTov = xTo.rearrange("(k p) t -> p k t", p=128)
                for oc in range(4):
                    c0 = oc * 512
                    cs_ = slice(c0, c0 + 512)
                    P.dma(wst3[:], xTov[:, :, cs_], writes=["wst"])
                    P.act(lambda e: e.copy(out=xb[:, 0:4, :], in_=wst3[:, 0:4, :]), ["wst"], ["xb5"])
                    P.dve(lambda e: e.tensor_copy(out=xb[:, 4:8, :], in_=wst3[:, 4:8, :]), ["wst"], ["xb5"])
                    for et in range(8):
                        b_ = et % 2
                        for k in range(8):
                            P.pe(lambda e: e.matmul(pgt[b_][:], lhsT=Wgt[:, k, et * 128:(et + 1) * 128], rhs=xb[:, k, :], start=(k == 0), stop=(k == 7)),
                                 ["Wgt", "xb5"], ["bk%d" % (4 + b_)])
                        P.act(lambda e: e.activation(out=sgt[b_][:], in_=pgt[b_][:], func=AF.Silu), ["bk%d" % (4 + b_)], ["sgt%d" % b_])
                        P.act(lambda e: e.activation(out=yn[b_][:], in_=oretT[:, et, cs_], func=AF.Identity, scale=gg8[:, et:et + 1], bias=gb8[:, et:et + 1]),
                              ["oretT", "gg8", "gb8"], ["yn%d" % b_])
                        P.dve(lambda e: e.tensor_tensor(out=og[:, et, :], in0=yn[b_][:], in1=sgt[b_][:], op=ALU.mult), ["yn%d" % b_, "sgt%d" % b_], ["og"])
                    for ct in range(8):
                        cb = (ct % 2) * 4
                        cp = ct % 2
                        pg0, pg1, pu0, pu1 = bk[cb], bk[cb + 1], bk[cb + 2], bk[cb + 3]
                        kg0, kg1, ku0, ku1 = ["bk%d" % (cb + q_) for q_ in range(4)]
                        ga, gr, ma = ga2[cp], gr2[cp], ma2[cp]
                        for k in range(8):
                            P.pe(lambda e: e.matmul(pg0[:], lhsT=Wmg[:, k, ct * 128:(ct + 1) * 128], rhs=xb[:, k, :], start=(k == 0), stop=(k == 7)),
                                 ["Wmg", "xb5"], [kg0])
                        for k in range(8):
                            P.pe(lambda e: e.matmul(pg1[:], lhsT=Wmg[:, k, 1024 + ct * 128:1024 + (ct + 1) * 128], rhs=xb[:, k, :], start=(k == 0), stop=(k == 7)),
                                 ["Wmg", "xb5"], [kg1])
                        for k in range(4):
                            P.pe(lambda e: e.matmul(pu0[:], lhsT=Wa[:, k, ct * 128:(ct + 1) * 128], rhs=oattnT[:, k, cs_], start=(k == 0), stop=(k == 3)),
                                 ["Wa", "oattnT"], [ku0])
                        for k in range(8):
                            P.pe(lambda e: e.matmul(pu1[:], lhsT=Wr[:, k, ct * 128:(ct + 1) * 128], rhs=og[:, k, :], start=(k == 0), stop=(k == 7)),
                                 ["Wr", "og"], [ku1])
                        P.act(lambda e: e.activation(out=ga[:], in_=pg0[:], func=AF.Sigmoid), [kg0], ["ga%d" % cp])
                        P.act(lambda e: e.activation(out=gr[:], in_=pg1[:], func=AF.Sigmoid), [kg1], ["gr%d" % cp])
                        P.dve(lambda e: e.tensor_tensor(out=ma[:], in0=pu0[:], in1=ga[:], op=ALU.mult), [ku0, "ga%d" % cp], ["ma%d" % cp])
                        P.dve(lambda e: e.tensor_tensor(out=gr[:], in0=pu1[:], in1=gr[:], op=ALU.mult), [ku1, "gr%d" % cp], ["gr%d" % cp])
                        P.pool(lambda e: e.tensor_tensor(out=x1T[:, ct, cs_], in0=ma[:], in1=gr[:], op=ALU.add), ["ma%d" % cp, "gr%d" % cp], ["mx%d" % (oc * 4 + t_) for t_ in range(4)])
                dump("mergedT", x1T[:].rearrange("p a t -> p (a t)"), [128, 8 * TO], "mx0")
            P.barrier()
        B_.close()
        A_.close()
        if n_stage >= 5:
            with ExitStack() as S56:
                acc = sb(S56, "acc", [128, 16, 1024], F32)
                lng = sb(S56, "lng", [128, 1024], F32)
                lnb = sb(S56, "lnb", [128, 1024], F32)
                st12 = sb(S56, "st12", [128, 2, 6], F32)
                mv = sb(S56, "mv5", [128, 2], F32)
                rstd = sb(S56, "rstd5", [128, 1], F32)

                def layer_norm(src_ap, src_key, dst_ap, dst_key, tmp_ap, tmp_key, eps=LN_EPS):
                    for hf in range(2):
                        P.dve(lambda e: e.bn_stats(out=st12[:, hf, :], in_=src_ap[:, hf * 512:(hf + 1) * 512]), [src_key], ["st12"])
                    P.dve(lambda e: e.bn_aggr(out=mv[:], in_=st12[:].rearrange("p a s -> p (a s)")), ["st12"], ["mv5"])
                    P.dve(lambda e: e.tensor_scalar(out=rstd[:], in0=mv[:, 1:2], scalar1=eps, scalar2=None, op0=ALU.add), ["mv5"], ["rstd5"])
                    P.act(lambda e: e.activation(out=rstd[:], in_=rstd[:], func=AF.Sqrt), ["rstd5"], ["rstd5"])
                    P.dve(lambda e: e.reciprocal(out=rstd[:], in_=rstd[:]), ["rstd5"], ["rstd5"])
                    P.dve(lambda e: e.scalar_tensor_tensor(out=tmp_ap, in0=src_ap, scalar=mv[:, 0:1], in1=lng[:], op0=ALU.subtract, op1=ALU.mult),
                          [src_key, "mv5", "lng"], [tmp_key])
                    P.dve(lambda e: e.scalar_tensor_tensor(out=dst_ap, in0=tmp_ap, scalar=rstd[:, 0:1], in1=lnb[:], op0=ALU.mult, op1=ALU.add),
                          [tmp_key, "rstd5", "lnb"], [dst_key])

                with ExitStack() as S5b:
                    Wo = sb(S5b, "Wo", [128, 8, 1024], BF16)
                    load_cast(Wo[:].rearrange("p k n -> p (k n)"), wot, 8 * 1024, "Wo")
                    P.dma(lng[:], ln1g.partition_broadcast(128), writes=["lng"])
                    P.dma(lnb[:], ln1b.partition_broadcast(128), writes=["lnb"])
                    xres2 = [sb(S5b, "xres%d" % i_, [128, 1024], F32) for i_ in range(2)]
                    yt2 = [sb(S5b, "yt%d" % i_, [128, 1024], F32) for i_ in range(2)]
                    x1b2 = [sb(S5b, "x1b%d" % i_, [128, 1024], BF16) for i_ in range(2)]
                    pm2 = [[ps(S5b, "pm%d_%d" % (q_, i_), [128, 512]) for i_ in range(2)] for q_ in range(2)]
                    pTr = ps(S5b, "pTr5", [128, 128], BF16)
                    pend5 = []
                    for i in range(16):
                        q_ = i % 2
                        xres, yt, x1b, pm = xres2[q_], yt2[q_], x1b2[q_], pm2[q_]
                        ts_ = slice(i * 128, (i + 1) * 128)
                        if len(pend5) >= 2:
                            pend5.pop(0)()
                        P.dma(xres[:], xo[ts_, :], writes=["xres%d" % q_])
                        for hf in range(2):
                            for k in range(8):
                                P.pe(lambda e: e.matmul(pm[hf][:], lhsT=x1T[:, k, ts_], rhs=Wo[:, k, hf * 512:(hf + 1) * 512], start=(k == 0), stop=(k == 7)),
                                     ["mx%d" % i, "Wo"], ["pm%d_%d" % (q_, hf)])
                            P.dve(lambda e: e.scalar_tensor_tensor(out=yt[:, hf * 512:(hf + 1) * 512], in0=xres[:, hf * 512:(hf + 1) * 512], scalar=ALPHA,
                                                                   in1=pm[hf][:], op0=ALU.mult, op1=ALU.add), ["xres%d" % q_, "pm%d_%d" % (q_, hf)], ["yt%d" % q_])
                        layer_norm(yt[:], "yt%d" % q_, acc[:, i, :], "acc%d" % i, yt[:], "yt%d" % q_)
                        if ("x1_%d" % i) in debug:
                            dump("x1_%d" % i, acc[:, i, :], [128, 1024], "acc%d" % i)
                        P.pool(lambda e: e.tensor_copy(out=x1b[:], in_=acc[:, i, :]), ["acc%d" % i], ["x1b%d" % q_])

                        def tr5(i=i, q_=q_, x1b=x1b, ts_=ts_):
                            for dt_ in range(8):
                                P.pe(lambda e: e.transpose(out=pTr[:], in_=x1b[:, dt_ * 128:(dt_ + 1) * 128], identity=identb[:]), ["x1b%d" % q_, "identb"], ["pTr5"])
                                P.act(lambda e: e.copy(out=x1T[:, dt_, ts_], in_=pTr[:]), ["pTr5"], ["mx%d" % i])
                        pend5.append(tr5)
                    while pend5:
                        pend5.pop(0)()
                P.barrier()
                if n_stage >= 6:
                    with ExitStack() as S6:
                        P.dma(lng[:], ln2g.partition_broadcast(128), writes=["lng"])
                        P.dma(lnb[:], ln2b.partition_broadcast(128), writes=["lnb"])
                        comb = sb(S6, "comb", [128, 16, 32], F32)
                        wrf = sb(S6, "wrf", [128, 8, 36], F32)
                        wrb = sb(S6, "wrb", [128, 8, 36], BF16)
                        brb = sb(S6, "brb", [128, 36], F32)
                        P.dma(wrf[:].rearrange("p k n -> p (k n)"), wrout, writes=["wrf"])
                        P.dve(lambda e: e.tensor_copy(out=wrb[:], in_=wrf[:]), ["wrf"], ["wrb"])
                        P.dma(brb[:], brout.partition_broadcast(128), writes=["brb"])
                        lg = sb(S6, "lg", [128, 16, 36], F32)
                        gmx = sb(S6, "gmx", [128, 16], F32)
                        gsh = sb(S6, "gsh", [128, 16, 4], F32)
                        gex = sb(S6, "gex", [128, 16, 4], F32)
                        gsum = sb(S6, "gsum", [128, 16], F32)
                        gprob = sb(S6, "gprob", [128, 16], F32)
                        ohg = sb(S6, "ohg", [128, 16, 4], F32)
                        tmp48 = sb(S6, "tmp48", [128, 16, 4, 8], F32)
                        isel = sb(S6, "isel", [128, 16, 8], F32)
                        isel2 = sb(S6, "isel2", [128, 16, 8], F32)
                        eq0 = sb(S6, "eq0", [128, 16, 8], F32)
                        eq1 = sb(S6, "eq1", [128, 16, 8], F32)
                        m0 = sb(S6, "m0r", [128, 16], F32)
                        m1 = sb(S6, "m1r", [128, 16], F32)
                        dlt = sb(S6, "dlt", [128, 16], F32)
                        w2e = sb(S6, "w2e", [128, 16], F32)
                        wsum = sb(S6, "wsum", [128, 16], F32)
                        wt1 = sb(S6, "wt1", [128, 16], F32)
                        wt2 = sb(S6, "wt2", [128, 16], F32)
                        ce = sb(S6, "ce", [128, 16, 8], F32)
                        ce2 = sb(S6, "ce2", [128, 16, 8], F32)
                        SR = ExitStack()
                        plg = [ps(SR, "plg%d" % i_, [128, 8, 36]) for i_ in range(2)]
                        AXX = mybir.AxisListType.X

                        def b3(ap, n):
                            return ap.unsqueeze(2).broadcast_to([128, 16, n])
                        for i in range(16):
                            ts_ = slice(i * 128, (i + 1) * 128)
                            for k in range(8):
                                P.pe(lambda e: e.matmul(plg[i // 8][:, i % 8, :], lhsT=x1T[:, k, ts_], rhs=wrb[:, k, :], start=(k == 0), stop=(k == 7)),
                                     ["mx%d" % i, "wrb"], ["plg%d" % (i // 8)])
                        for hf in range(2):
                            P.dve(lambda e: e.tensor_tensor(out=lg[:, hf * 8:(hf + 1) * 8, :], in0=plg[hf][:], in1=brb[:].unsqueeze(1).broadcast_to([128, 8, 36]), op=ALU.add),
                                  ["plg%d" % hf, "brb"], ["lg"])
                        P.dve(lambda e: e.tensor_reduce(out=gmx[:], in_=lg[:, :, 0:4], axis=AXX, op=ALU.max), ["lg"], ["gmx"])
                        P.dve(lambda e: e.tensor_tensor(out=gsh[:], in0=lg[:, :, 0:4], in1=b3(gmx[:], 4), op=ALU.subtract), ["lg", "gmx"], ["gsh"])
                        P.act(lambda e: e.activation(out=gex[:].rearrange("p t g -> p (t g)"), in_=gsh[:].rearrange("p t g -> p (t g)"), func=AF.Exp), ["gsh"], ["gex"])
                        P.dve(lambda e: e.tensor_reduce(out=gsum[:], in_=gex[:], axis=AXX, op=ALU.add), ["gex"], ["gsum"])
                        P.dve(lambda e: e.reciprocal(out=gprob[:], in_=gsum[:]), ["gsum"], ["gprob"])
                        P.dve(lambda e: e.tensor_scalar(out=ohg[:].rearrange("p t g -> p (t g)"), in0=gsh[:].rearrange("p t g -> p (t g)"), scalar1=0.0, scalar2=None, op0=ALU.is_ge),
                              ["gsh"], ["ohg"])
                        P.dve(lambda e: e.tensor_tensor(out=tmp48[:], in0=lg[:, :, 4:36].rearrange("p t (g e) -> p t g e", g=4),
                                                        in1=ohg[:].unsqueeze(3).broadcast_to([128, 16, 4, 8]), op=ALU.mult), ["lg", "ohg"], ["tmp48"])
                        P.dve(lambda e: e.tensor_reduce(out=isel[:], in_=tmp48[:].rearrange("p t g e -> p t e g"), axis=AXX, op=ALU.add), ["tmp48"], ["isel"])
                        P.dve(lambda e: e.tensor_reduce(out=m0[:], in_=isel[:], axis=AXX, op=ALU.max), ["isel"], ["m0r"])
                        P.dve(lambda e: e.tensor_tensor(out=eq0[:], in0=isel[:], in1=b3(m0[:], 8), op=ALU.is_equal), ["isel", "m0r"], ["eq0"])
                        P.dve(lambda e: e.scalar_tensor_tensor(out=isel2[:].rearrange("p t e -> p (t e)"), in0=eq0[:].rearrange("p t e -> p (t e)"), scalar=-1.0e30,
                                                               in1=isel[:].rearrange("p t e -> p (t e)"), op0=ALU.mult, op1=ALU.add), ["eq0", "isel"], ["isel2"])
                        P.dve(lambda e: e.tensor_reduce(out=m1[:], in_=isel2[:], axis=AXX, op=ALU.max), ["isel2"], ["m1r"])
                        P.dve(lambda e: e.tensor_tensor(out=eq1[:], in0=isel2[:], in1=b3(m1[:], 8), op=ALU.is_equal), ["isel2", "m1r"], ["eq1"])
                        P.dve(lambda e: e.tensor_tensor(out=dlt[:], in0=m1[:], in1=m0[:], op=ALU.subtract), ["m1r", "m0r"], ["dlt"])
                        P.act(lambda e: e.activation(out=w2e[:], in_=dlt[:], func=AF.Exp), ["dlt"], ["w2e"])
                        P.dve(lambda e: e.tensor_scalar(out=wsum[:], in0=w2e[:], scalar1=1.0, scalar2=None, op0=ALU.add), ["w2e"], ["wsum"])
                        P.dve(lambda e: e.reciprocal(out=wsum[:], in_=wsum[:]), ["wsum"], ["wsum"])
                        P.dve(lambda e: e.tensor_tensor(out=wt1[:], in0=wsum[:], in1=gprob[:], op=ALU.mult), ["wsum", "gprob"], ["wt1"])
                        P.dve(lambda e: e.tensor_tensor(out=wt2[:], in0=wt1[:], in1=w2e[:], op=ALU.mult), ["wt1", "w2e"], ["wt2"])
                        P.dve(lambda e: e.tensor_tensor(out=ce[:], in0=eq0[:], in1=b3(wt1[:], 8), op=ALU.mult), ["eq0", "wt1"], ["ce"])
                        P.dve(lambda e: e.tensor_tensor(out=ce2[:], in0=eq1[:], in1=b3(wt2[:], 8), op=ALU.mult), ["eq1", "wt2"], ["ce2"])
                        P.dve(lambda e: e.tensor_tensor(out=ce[:], in0=ce[:], in1=ce2[:], op=ALU.add), ["ce", "ce2"], ["ce"])
                        P.dve(lambda e: e.tensor_tensor(out=comb[:].rearrange("p t (g e) -> p t g e", g=4), in0=ce[:].unsqueeze(2).broadcast_to([128, 16, 4, 8]),
                                                        in1=ohg[:].unsqueeze(3).broadcast_to([128, 16, 4, 8]), op=ALU.mult), ["ce", "ohg"], ["comb"])
                        P.dve(lambda e: e.tensor_scalar(out=comb[:].rearrange("p a e -> p (a e)"), in0=comb[:].rearrange("p a e -> p (a e)"), scalar1=1.0 / ALPHA, scalar2=None, op0=ALU.mult),
                              ["comb"], ["comb"])
                        dump("comb", comb[:].rearrange("p a e -> p (a e)"), [128, 512], "comb")
                        SR.close()
                        P.barrier()
                        wstE = [wst[:, 0:2048], wst[:, 2048:4096]]
                        wE = [sb(S6, "wE%d" % i, [128, 12288], BF16) for i in range(2)]
                        sgE = [sb(S6, "sgE%d" % i, [128, 512], F32) for i in range(2)]
                        hT = [sb(S6, "hT%d" % i, [128, 4, 512], BF16) for i in range(2)]
                        pG = [ps(S6, "pG%d" % i, [128, 512]) for i in range(2)]
                        pU = [ps(S6, "pU%d" % i, [128, 512]) for i in range(2)]
                        pO = [ps(S6, "pO%d" % i, [128, 512]) for i in range(3)]
                        wsr = Ring([0, 1])
                        gr_ = Ring([0, 1])
                        or_ = Ring([0, 1, 2])
                        crr = [0]
                        n_exp = DEBUG.get("n_exp", 32)

                        def load_expert(ex):
                            ws = ex % 2
                            for pc in range(6):
                                si = wsr.next()
                                P.dma(wstE[si], wexp[ex, :, pc * 2048:(pc + 1) * 2048], writes=["wstE%d" % si])
                                d = wE[ws][:, pc * 2048:(pc + 1) * 2048]
                                P.pool(lambda e: e.tensor_copy(out=d, in_=wstE[si]), ["wstE%d" % si], ["wE%d" % ws])

                        def gu_phase(ex, cth):
                            ws = ex % 2
                            Wg = wE[ws][:, 0:4096].rearrange("p (k n) -> p k n", k=8)
                            Wu = wE[ws][:, 4096:8192].rearrange("p (k n) -> p k n", k=8)
                            wkey = "wE%d" % ws
                            cs_ = slice(cth * 512, (cth + 1) * 512)
                            xkeys = ["mx%d" % (cth * 4 + t_) for t_ in range(4)]
                            hs_ = cth % 2
                            for ft in range(4):
                                gi = gr_.next()
                                for k in range(8):
                                    P.pe(lambda e: e.matmul(pG[gi][:], lhsT=Wg[:, k, ft * 128:(ft + 1) * 128], rhs=x1T[:, k, cs_], start=(k == 0), stop=(k == 7)),
                                         [wkey] + xkeys, ["pG%d" % gi])
                                for k in range(8):
                                    P.pe(lambda e: e.matmul(pU[gi][:], lhsT=Wu[:, k, ft * 128:(ft + 1) * 128], rhs=x1T[:, k, cs_], start=(k == 0), stop=(k == 7)),
                                         [wkey] + xkeys, ["pU%d" % gi])
                                P.act(lambda e: e.activation(out=sgE[gi][:], in_=pG[gi][:], func=AF.Silu), ["pG%d" % gi], ["sgE%d" % gi])
                                P.dve(lambda e: e.tensor_tensor(out=hT[hs_][:, ft, :], in0=pU[gi][:], in1=sgE[gi][:], op=ALU.mult),
                                      ["pU%d" % gi, "sgE%d" % gi], ["hT%d" % hs_])

                        def d_phase(ex, cth):
                            ws = ex % 2
                            Wd = wE[ws][:, 8192:12288].rearrange("p (k n) -> p k n", k=4)
                            wkey = "wE%d" % ws
                            hs_ = cth % 2
                            for tt in range(4):
                                ti = cth * 4 + tt
                                for hf in range(2):
                                    oi = or_.next()
                                    for ft in range(4):
                                        P.pe(lambda e: e.matmul(pO[oi][:], lhsT=hT[hs_][:, ft, tt * 128:(tt + 1) * 128],
                                                                rhs=Wd[:, ft, hf * 512:(hf + 1) * 512], start=(ft == 0), stop=(ft == 3)),
                                             [wkey, "hT%d" % hs_], ["pO%d" % oi])
                                    a_ = acc[:, ti, hf * 512:(hf + 1) * 512]
                                    P.dve(lambda e: e.scalar_tensor_tensor(out=a_, in0=pO[oi][:], scalar=comb[:, ti, ex:ex + 1], in1=a_, op0=ALU.mult, op1=ALU.add),
                                          ["pO%d" % oi, "comb", "acc%d" % ti], ["acc%d" % ti])

                        units = [(ex, cth) for ex in range(n_exp) for cth in range(4)]
                        load_expert(0)
                        if n_exp > 1:
                            load_expert(1)
                        gu_phase(*units[0])
                        for u, (ex, cth) in enumerate(units):
                            if u + 1 < len(units):
                                gu_phase(*units[u + 1])
                            d_phase(ex, cth)
                            if cth == 3 and ex + 2 < n_exp:
                                load_expert(ex + 2)
                        yo = [sb(S6, "yo%d" % i, [128, 1024], F32) for i in range(2)]
                        for i in range(16):
                            yi = i % 2
                            layer_norm(acc[:, i, :], "acc%d" % i, yo[yi][:], "yo%d" % yi, yo[yi][:], "yo%d" % yi, eps=LN_EPS / (ALPHA * ALPHA))
                            P.dma(out[i * 128:(i + 1) * 128, :], yo[yi][:], reads=["yo%d" % yi], writes=["out%d" % yi], sem="outd%d" % yi)
        fin_reads = ["out0", "out1"] + ["dbg_" + n for n in dbg_out]
        P.add("sp", lambda e: e.nop(), reads=fin_reads, sem="fin")
        if n_stage < 6:
            pass
        cnt = P.emit(G)
        nsem = len(cnt)
    return nc, dbg_out, nsem


def prep_shared(inp):
    f = lambda a: np.ascontiguousarray(np.asarray(a, dtype=np.float32))
    w_in = f(inp["w_in"])[0]
    sh = {}
    q = w_in[:, 0:512].reshape(1024, 2, 4, 64).transpose(0, 2, 1, 3).reshape(1024, 512)
    w1 = np.concatenate([q, w_in[:, 512:640], w_in[:, 640:768], w_in[:, 768:896], w_in[:, 1024:1152],
                         w_in[:, 896:1024], w_in[:, 1152:1280], w_in[:, 1280:1304]], axis=1)
    sh["w1t"] = tile_w(w1)
    sh["w4t"] = tile_w(w_in[:, 1304:3352])
    sh["wgtt"] = tile_w(w_in[:, 3352:4376])
    sh["wmgt"] = tile_w(w_in[:, 4376:6424])
    lit = {"k": (inp["cmp_k_w1"], inp["cmp_k_b1"], inp["cmp_pos_k"]), "v": (inp["cmp_v_w1"], inp["cmp_v_b1"], inp["cmp_pos_v"])}
    for kv in "kv":
        cw1 = f(lit[kv][0])[0]
        r = cw1.reshape(32, 64, 256).transpose(1, 0, 2).reshape(64, 32 * 256)
        sh["cw1" + kv] = np.ascontiguousarray(np.concatenate([r, r], axis=0))
        pos = f(lit[kv][2])[0]
        sh["cpos" + kv] = np.ascontiguousarray(np.concatenate([pos.T, pos.T], axis=0))
        sh["cb1" + kv] = np.ascontiguousarray(f(lit[kv][1])[0].reshape(2, 128).T)
    w2k = f(inp["cmp_k_w2"])[0]
    sh["cw2k"] = tile_w(np.concatenate([w2k, w2k], axis=1))
    sh["cw2v"] = tile_w(f(inp["cmp_v_w2"])[0])
    b2k = f(inp["cmp_k_b2"])[0]
    sh["cb2k"] = np.ascontiguousarray(np.concatenate([b2k, b2k])[:, None])
    sh["cb2v"] = f(inp["cmp_v_b2"])[0]
    sh["gng8"] = np.ascontiguousarray(f(inp["ret_gn_g"])[0].reshape(8, 128).T)
    sh["gnb8"] = np.ascontiguousarray(f(inp["ret_gn_b"])[0].reshape(8, 128).T)
    sh["wat"] = tile_w(f(inp["w_up_attn"])[0])
    sh["wrt"] = tile_w(f(inp["w_up_ret"])[0])
    sh["wot"] = tile_w(f(inp["w_out"])[0])
    for n in ("ln1_g", "ln1_b", "ln2_g", "ln2_b"):
        sh[n.replace("_", "")] = f(inp[n])[0]
    rg = f(inp["router_group_w"])[0]
    ri = f(inp["router_inner_w"])[0]
    sh["wrout"] = tile_w(np.concatenate([rg, ri.transpose(1, 0, 2).reshape(1024, 32)], axis=1))
    sh["brout"] = np.ascontiguousarray(np.concatenate([f(inp["router_group_b"])[0], f(inp["router_inner_b"])[0].reshape(32)]))
    wg = f(inp["expert_w_gate"])[0]
    wu = f(inp["expert_w_up"])[0]
    wd = f(inp["expert_w_down"])[0]
    we = np.empty((32, 128, 12288), np.float32)
    we[:, :, 0:4096] = wg.reshape(32, 8, 128, 512).transpose(0, 2, 1, 3).reshape(32, 128, 4096)
    we[:, :, 4096:8192] = wu.reshape(32, 8, 128, 512).transpose(0, 2, 1, 3).reshape(32, 128, 4096)
    we[:, :, 8192:12288] = wd.reshape(32, 4, 128, 1024).transpose(0, 2, 1, 3).reshape(32, 128, 4096)
    sh["wexp"] = we
    return sh


def make_in_maps(inp):
    sh = prep_shared(inp)
    x = np.asarray(inp["x"], dtype=np.float32)
    maps = []
    for core in range(8):
        b, c = core // 2, core % 2
        m = dict(sh)
        xb = x[b]
        own = xb.reshape(16, 2, 128, 1024)[:, c].reshape(TO, 1024)
        m["xT"] = np.ascontiguousarray(xb.T)
        m["xTo"] = np.ascontiguousarray(own.T)
        m["xo"] = np.ascontiguousarray(own)
        for k, v in make_consts(c).items():
            if not k.startswith("_"):
                m["c_" + k] = v
        maps.append(m)
    return maps


_PROG_CACHE = {}


def kernel(**inputs):
    if "prog" not in _PROG_CACHE:
        _PROG_CACHE["prog"] = build_program()
    nc, _, _ = _PROG_CACHE["prog"]
    maps = make_in_maps(inputs)
    res = run_bass_kernel_spmd(nc, maps, core_ids=list(range(8)))
    outp = np.empty((4, 16, 2, 128, 1024), np.float32)
    for core in range(8):
        b, c = core // 2, core % 2
        outp[b, :, c] = res.results[core]["out"].reshape(16, 128, 1024)
    return outp.reshape(4, T, 1024)
```

```python
import numpy as np
import ml_dtypes
import concourse.bass as bass
import concourse.mybir as mybir
from concourse.bass_utils import run_bass_kernel_spmd
from contextlib import ExitStack

F32 = mybir.dt.float32
BF16 = mybir.dt.bfloat16
AF = mybir.ActivationFunctionType
ALU = mybir.AluOpType
NPBF = ml_dtypes.bfloat16

T = 4096
D = 1024
TO = 2048
NEGM = -30000.0
LN_EPS = 1e-5
ALPHA = 2.0 ** 0.25
DEBUG = {}


class Op:
    __slots__ = ("eng", "fn", "reads", "writes", "dma", "sem", "deps", "needs_inc", "idx", "id", "extra")

    def __init__(self, eng, fn, reads, writes, dma, sem):
        self.eng = eng
        self.fn = fn
        self.reads = tuple(reads)
        self.writes = tuple(writes)
        self.dma = dma
        self.sem = sem
        self.deps = []
        self.needs_inc = dma
        self.idx = 0
        self.extra = ()


class _Rec:
    def __getattr__(self, name):
        return lambda *a, **k: (name, a, k)


_REC = _Rec()


class Prog:
    ENGS = ("pe", "act", "dve", "pool", "sp")

    def __init__(self, nc, same_eng_sync=True):
        self.nc = nc
        self.ops = []
        self.same_eng_sync = same_eng_sync
        self.last_by_sem = {}
        self.psum_keys = set()

    def add(self, eng, fn, reads=(), writes=(), dma=False, sem=None):
        lim = DEBUG.get("max_ops")
        self.nadd = getattr(self, "nadd", -1) + 1
        if (lim is not None and self.nadd >= lim and sem not in ("dbg", "fin")) or self.nadd in DEBUG.get("skip", ()):
            return Op(eng, None, reads, writes, dma, sem)
        if dma and sem is None:
            sem = "dma_" + str(writes[0])
        if not dma:
            sem = "eng_" + eng
        op = Op(eng, fn(_REC), reads, writes, dma, sem)
        if DEBUG.get("trace_ops"):
            print(len(self.ops), eng, op.fn[0], reads, writes)
        op.id = len(self.ops)
        self.ops.append(op)
        self.last_by_sem[sem] = op
        return op

    def pe(self, fn, reads=(), writes=()):
        return self.add("pe", fn, reads, writes)

    def act(self, fn, reads=(), writes=()):
        return self.add("act", fn, reads, writes)

    def dve(self, fn, reads=(), writes=()):
        return self.add("dve", fn, reads, writes)

    def pool(self, fn, reads=(), writes=()):
        return self.add("pool", fn, reads, writes)

    def dma(self, out, in_, reads=(), writes=(), sem=None, q="sp", **kw):
        return self.add(q, lambda e: e.dma_start(out=out, in_=in_, **kw), reads, writes, dma=True, sem=sem)

    def barrier(self):
        lasts = list(self.last_by_sem.values())
        for eng in self.ENGS:
            op = self.add(eng, lambda e: e.nop())
            op.extra = tuple(lasts)
        self.last_by_sem = {k: v for k, v in self.last_by_sem.items() if k.startswith("eng_")}

    def analyze(self):
        state = {}
        for op in self.ops:
            deps = set(op.extra)
            for k in op.reads:
                st = state.get(k)
                if st:
                    deps.update(st[0])
                    if k in self.psum_keys:
                        deps.update(r for r in st[1] if r.eng != op.eng)
            for k in op.writes:
                st = state.get(k)
                if st is None:
                    st = state[k] = [[], []]
                if st[1]:
                    deps.update(st[1])
                    deps.update(st[0])
                    st[0] = [op]
                    st[1] = []
                else:
                    same_group = op.dma and all(w.dma and w.sem == op.sem for w in st[0])
                    if same_group:
                        st[0].append(op)
                    else:
                        deps.update(st[0])
                        st[0] = [op]
            for k in op.reads:
                st = state.get(k)
                if st is None:
                    st = state[k] = [[], []]
                st[1].append(op)
            deps.discard(op)
            red = {}
            for d in deps:
                if (not d.dma) and (not op.dma) and d.eng == op.eng:
                    if op.eng == "pe" or not self.same_eng_sync:
                        continue
                cur = red.get(d.sem)
                if cur is None or d.id > cur.id:
                    red[d.sem] = d
            op.deps = list(red.values())
            for d in op.deps:
                d.needs_inc = True
        cnt = {}
        for op in self.ops:
            if op.needs_inc:
                cnt[op.sem] = cnt.get(op.sem, 0) + 1
                op.idx = cnt[op.sem]
        self.sem_names = sorted(cnt.keys())
        return cnt

    def emit(self, stack):
        nc = self.nc
        cnt = self.analyze()
        sems = {}
        for name in self.sem_names:
            sems[name] = stack.enter_context(nc.semaphore(name))
        block = stack.enter_context(nc.Block())
        per_eng = {e: [o for o in self.ops if o.eng == e] for e in self.ENGS}

        def run(eng_obj, ops):
            known = {}
            for op in ops:
                for d in op.deps:
                    val = d.idx * (16 if d.dma else 1)
                    if known.get(d.sem, 0) < val:
                        eng_obj.wait_ge(sems[d.sem], val)
                        known[d.sem] = val
                name, a, k = op.fn
                inst = getattr(eng_obj, name)(*a, **k)
                if op.needs_inc:
                    inst.then_inc(sems[op.sem], 16 if op.dma else 1)

        @block.sync
        def _(e):
            run(e, per_eng["sp"])

        @block.tensor
        def _(e):
            run(e, per_eng["pe"])

        @block.scalar
        def _(e):
            run(e, per_eng["act"])

        @block.vector
        def _(e):
            run(e, per_eng["dve"])

        @block.gpsimd
        def _(e):
            run(e, per_eng["pool"])
        return cnt


class Ring:
    def __init__(self, items):
        self.items = items
        self.i = 0

    def next(self):
        it = self.items[self.i % len(self.items)]
        self.i += 1
        return it


def tile_w(w):
    K, N = w.shape
    return np.ascontiguousarray(w.reshape(K // 128, 128, N).transpose(1, 0, 2).reshape(128, -1))


def rope_tabs(pos, d, scale):
    half = d // 2
    inv = 10000.0 ** (-np.arange(half, dtype=np.float64) * 2.0 / d)
    ang = pos.astype(np.float64)[None, :] * inv[:, None]
    cos = np.cos(ang) * scale
    sin = np.sin(ang) * scale
    reps = 128 // half
    return (np.tile(cos, (reps, 1)).astype(np.float32), np.tile(sin, (reps, 1)).astype(np.float32))


def rot_lhsT(d):
    half = d // 2
    Pm = np.zeros((128, 128), np.float32)
    for blk in range(128 // d):
        o = blk * d
        for m in range(half):
            Pm[o + m, o + m + half] = -1.0
            Pm[o + m + half, o + m] = 1.0
    return np.ascontiguousarray(Pm.T)


_CONST_CACHE = {}


def make_consts(c):
    if c in _CONST_CACHE:
        return _CONST_CACHE[c]
    cs = {}
    own_pos = np.concatenate([np.arange(128) + (2 * i + c) * 128 for i in range(16)])
    allpos = np.arange(T)
    cs["cosK"], cs["sinK"] = rope_tabs(allpos, 64, 1.0)
    cs["cosQ"], cs["sinQ"] = rope_tabs(own_pos, 64, 0.125)
    cs["cosRK"], cs["sinRK"] = rope_tabs(allpos, 128, 128.0 ** -0.5)
    cs["cosRQ"], cs["sinRQ"] = rope_tabs(own_pos, 128, 1.0)
    cend = np.arange(256) * 16 + 31
    cs["cosC"], cs["sinC"] = rope_tabs(cend, 64, 1.0)
    cs["pt64"] = rot_lhsT(64).astype(NPBF)
    cs["pt128"] = rot_lhsT(128).astype(NPBF)
    cs["identb"] = np.eye(128, dtype=np.float32).astype(NPBF)
    E = np.zeros((128, 32, 128), np.float32)
    for j in range(32):
        for k in range(128):
            E[2 * j + k // 64, j, k] = 1.0
            E[64 + 2 * j + k // 64, j, k] = 1.0
    cs["eall"] = E.reshape(128, -1).astype(NPBF)
    wm = np.zeros((128, 6, 128), np.float32)
    kk = np.arange(128)[:, None]
    tt = np.arange(128)[None, :]
    for r in range(6):
        dj = (r - 4) - c
        tk = dj * 128 + kk
        ok = (tk <= tt) & (tt - tk < 512)
        wm[:, r, :] = np.where(ok, 0.0, NEGM)
    cs["wmask"] = wm.reshape(128, -1).astype(NPBF)
    cm = np.zeros((128, 2, 16, 128), np.float32)
    for a in range(2):
        for i in range(16):
            G = 2 * i + c
            n = a * 128 + kk
            t = G * 128 + tt
            cm[:, a, i, :] = np.where(16 * n + 31 <= t, 0.0, NEGM)
    cs["cmpmask"] = cm.reshape(128, -1).astype(NPBF)
    cstart = np.arange(255) * 16
    sstart = np.arange(64) * 64
    ov = np.clip(np.minimum(cstart[None, :] + 32, sstart[:, None] + 64) - np.maximum(cstart[None, :], sstart[:, None]), 0, None) / 16.0
    ovT = np.zeros((256, 64), np.float32)
    ovT[:255] = ov.T
    cs["ovT"] = np.ascontiguousarray(ovT.reshape(2, 128, 64).transpose(1, 0, 2).reshape(128, -1)).astype(NPBF)
    tkm = np.zeros((128, 16, 64), np.float32)
    tkb = np.zeros((128, 16, 64), np.float32)
    for i in range(16):
        G = 2 * i + c
        for p in range(128):
            bt = (G * 128 + p) // 64
            for s in range(64):
                if s == 0:
                    tkb[p, i, s] = 1e9
                elif s == bt:
                    tkb[p, i, s] = 2e9
                elif s == bt - 1:
                    tkb[p, i, s] = 3e9
                elif s <= bt:
                    tkm[p, i, s] = 1.0
                else:
                    tkb[p, i, s] = -1e9 - 1e6 * s
    cs["tkm"] = tkm.reshape(128, -1)
    cs["tkb"] = tkb.reshape(128, -1)
    gam = 1.0 - 2.0 ** (-5.0 - np.arange(4, dtype=np.float64))
    lg = np.log(gam)
    m = np.arange(256)[:, None]
    cq = np.arange(128)[None, :]
    qq = 128 * c + cq
    Dc = np.zeros((128, 2, 4, 128), np.float32)
    for h in range(4):
        dd = np.where(qq >= m, np.exp(np.maximum(qq - m, 0) * lg[h]), 0.0)
        Dc[:, :, h, :] = dd.reshape(2, 128, 128).transpose(1, 0, 2)
    cs["Dc"] = Dc.reshape(128, -1)
    xi = np.zeros((128, 4, 128), np.float32)
    for h in range(4):
        xi[:, h, :] = np.exp((qq + 1.0) * lg[h])
    cs["xi"] = xi.reshape(128, -1)
    zt = np.zeros((128, 2, 4), np.float32)
    for h in range(4):
        zt[:, :, h] = np.exp((255.0 - np.arange(256)) * lg[h]).reshape(2, 128).T
    cs["zeta"] = zt.reshape(128, -1)
    cs["_decay256"] = [float(np.exp(256.0 * lg[h])) for h in range(4)]
    _CONST_CACHE[c] = cs
    return cs


CONST_SHAPES = None


def build_program(n_stage=6, debug=()):
    nc = bass.Bass("TRN2", target_bir_lowering=False)
    cs0 = make_consts(0)
    dram = {}

    def din(name, shape, dt=F32):
        dram[name] = nc.dram_tensor(name, list(shape), dt, kind="ExternalInput").ap()
        return dram[name]

    xT = din("xT", [1024, T])
    xTo = din("xTo", [1024, TO])
    xo = din("xo", [TO, 1024])
    w1t = din("w1t", [128, 8 * 1304])
    w4t = din("w4t", [128, 8 * 2048])
    wmgt = din("wmgt", [128, 8 * 2048])
    cw1 = {kv: din("cw1" + kv, [128, 32 * 256]) for kv in "kv"}
    cpos = {kv: din("cpos" + kv, [128, 32]) for kv in "kv"}
    cb1 = {kv: din("cb1" + kv, [128, 2]) for kv in "kv"}
    cw2k = din("cw2k", [128, 2 * 128])
    cw2v = din("cw2v", [128, 2 * 64])
    cb2k = din("cb2k", [128, 1])
    cb2v = din("cb2v", [64])
    gng8 = din("gng8", [128, 8])
    gnb8 = din("gnb8", [128, 8])
    wgtt = din("wgtt", [128, 8 * 1024])
    wat = din("wat", [128, 4 * 1024])
    wrt = din("wrt", [128, 8 * 1024])
    wot = din("wot", [128, 8 * 1024])
    ln1g = din("ln1g", [1024])
    ln1b = din("ln1b", [1024])
    ln2g = din("ln2g", [1024])
    ln2b = din("ln2b", [1024])
    wrout = din("wrout", [128, 8 * 36])
    brout = din("brout", [36])
    wexp = din("wexp", [32, 128, 12288])
    cdr = {}
    for k, v in cs0.items():
        if k.startswith("_"):
            continue
        cdr[k] = din("c_" + k, v.shape, BF16 if v.dtype == NPBF else F32)
    out = nc.dram_tensor("out", [TO, 1024], F32, kind="ExternalOutput").ap()
    dbg_out = {}

    decay256 = cs0["_decay256"]

    with ExitStack() as G:
        P = Prog(nc)

        def sb(stack, name, shape, dt):
            return stack.enter_context(nc.sbuf_tensor(name, list(shape), dt))

        def ps(stack, name, shape, dt=F32):
            P.psum_keys.add(name)
            ncol = 512 if dt == F32 else 1024
            full = stack.enter_context(nc.psum_tensor(name, [128, ncol], dt))
            n = 1
            for d_ in shape[1:]:
                n *= d_
            v = full[0:shape[0], 0:n]
            if len(shape) == 3:
                v = v.rearrange("p (a b) -> p a b", a=shape[1])
            return v

        def dump(name, ap, shape, key):
            if name in debug:
                t = nc.dram_tensor("dbg_" + name, list(shape), ap.dtype, kind="ExternalOutput").ap()
                dbg_out[name] = t
                P.dma(t, ap, reads=[key], writes=["dbg_" + name], sem="dbg")

        identb = sb(G, "identb", [128, 128], BF16)
        P.dma(identb[:], cdr["identb"], writes=["identb"])
        wst = sb(G, "wst", [128, 4096], F32)
        cast_rr = [0]

        def load_cast(dst_ap, src_ap, n, dst_key, shape3=None):
            o = 0
            while o < n:
                m = min(4096, n - o)
                P.dma(wst[:, 0:m], src_ap[:, o:o + m], writes=["wst"])
                d = dst_ap[:, o:o + m]
                if cast_rr[0] % 2 == 0:
                    P.act(lambda e, d=d, m=m: e.copy(out=d, in_=wst[:, 0:m]), ["wst"], [dst_key])
                else:
                    P.dve(lambda e, d=d, m=m: e.tensor_copy(out=d, in_=wst[:, 0:m]), ["wst"], [dst_key])
                cast_rr[0] += 1
                o += m

        x1T = sb(G, "x1T", [128, 8, TO], BF16)
        wst3 = wst[:].rearrange("p (k n) -> p k n", k=8)
        A_ = ExitStack()
        oattnT = sb(A_, "oattnT", [128, 4, TO], BF16)

        with ExitStack() as SN:
            QT = sb(SN, "QT", [128, 16, 4, 128], BF16)
            KE = [sb(SN, "KE%d" % i_, [128, T], BF16) for i_ in range(2)]
            P.dma(KE[0][64:128, :], cdr["eall"][64:128, :], writes=["KE0"])
            P.dma(KE[1][0:64, :], cdr["eall"][0:64, :], writes=["KE1"])
            winkT = sb(SN, "winkT", [128, T], BF16)
            slcv1 = sb(SN, "slcv1", [128, 32, 2, 65], BF16)
            winv1 = sb(SN, "winv1", [128, 32, 2, 65], BF16)
            gates = sb(SN, "gates", [128, 16, 24], F32)
            kcmpT = sb(SN, "kcmpT", [128, 256], BF16)
            vcmp1 = sb(SN, "vcmp1", [128, 2, 2, 65], BF16)
            pt64 = sb(SN, "pt64", [128, 128], BF16)
            P.dma(pt64[:], cdr["pt64"], writes=["pt64"])
            P.dve(lambda e: e.memset(slcv1[:].rearrange("p a g d -> p (a g d)"), 1.0), [], ["slcv1"])
            P.dve(lambda e: e.memset(winv1[:].rearrange("p a g d -> p (a g d)"), 1.0), [], ["winv1"])
            P.dve(lambda e: e.memset(kcmpT[:], 0.0), [], ["kcmpT"])
            P.dve(lambda e: e.memset(vcmp1[:].rearrange("p a g d -> p (a g d)"), 0.0), [], ["vcmp1"])
            P.dve(lambda e: e.memset(vcmp1[:, :, :, 64:65], 1.0), [], ["vcmp1"])

            with ExitStack() as S12:
                cmpT = {"k": sb(S12, "cmpkT", [128, T], BF16), "v": sb(S12, "cmpvT", [128, T], BF16)}
                with ExitStack() as S1:
                    Wn = sb(S1, "Wn", [128, 8, 1304], BF16)
                    load_cast(Wn[:].rearrange("p k n -> p (k n)"), w1t, 8 * 1304, "Wn")
                    xb = [sb(S1, "xb%d" % i, [128, 8, 512], BF16) for i in range(2)]
                    tabs = [sb(S1, "tab%d" % i, [128, 2, 512], F32) for i in range(2)]
                    ybf = [sb(S1, "ybf%d" % i, [128, 512], BF16) for i in range(2)]
                    t1 = [sb(S1, "t1_%d" % i, [128, 512], F32) for i in range(2)]
                    t2 = [sb(S1, "t2_%d" % i, [128, 512], F32) for i in range(2)]
                    pj = [ps(S1, "pj%d" % i, [128, 512]) for i in range(3)]
                    prot = [ps(S1, "prot%d" % i, [128, 512]) for i in range(2)]
                    pv = [ps(S1, "pv%d" % i, [128, 256]) for i in range(2)]
                    pjr = Ring(list(range(3)))
                    rr = Ring(list(range(2)))
                    pvr = Ring(list(range(2)))
                    xTv = xT.rearrange("(k p) t -> p k t", p=128)
                    xTov = xTo.rearrange("(k p) t -> p k t", p=128)

                    def load_x(src_view, c0, n, slot):
                        P.dma(wst3[:, :, 0:n], src_view[:, :, c0:c0 + n], writes=["wst"])
                        P.act(lambda e: e.copy(out=xb[slot][:, 0:4, 0:n], in_=wst3[:, 0:4, 0:n]), ["wst"], ["xb%d" % slot])
                        P.dve(lambda e: e.tensor_copy(out=xb[slot][:, 4:8, 0:n], in_=wst3[:, 4:8, 0:n]), ["wst"], ["xb%d" % slot])

                    def proj_fm(col0, slot, n=512):
                        pi = pjr.next()
                        for k in range(8):
                            P.pe(lambda e, k=k, pi=pi: e.matmul(pj[pi][:, 0:n], lhsT=Wn[:, k, col0:col0 + 128], rhs=xb[slot][:, k, 0:n],
                                                                 start=(k == 0), stop=(k == 7)), ["Wn", "xb%d" % slot], ["pj%d" % pi])
                        return pi

                    def rope_fm(pi, tslot, dst_ap, dst_key, ptm, ptkey, n=512, src=None, srckey=None):
                        r = rr.next()
                        srcap = pj[pi][:, 0:n] if src is None else src
                        sk = ("pj%d" % pi) if srckey is None else srckey
                        P.act(lambda e: e.copy(out=ybf[r][:, 0:n], in_=srcap), [sk], ["ybf%d" % r])
                        P.pe(lambda e: e.matmul(prot[r][:, 0:n], lhsT=ptm[:], rhs=ybf[r][:, 0:n], start=True, stop=True),
                             [ptkey, "ybf%d" % r], ["prot%d" % r])
                        P.dve(lambda e: e.tensor_tensor(out=t1[r][:, 0:n], in0=srcap, in1=tabs[tslot][:, 0, 0:n], op=ALU.mult),
                              [sk, "tab%d" % tslot], ["t1_%d" % r])
                        P.dve(lambda e: e.tensor_tensor(out=t2[r][:, 0:n], in0=prot[r][:, 0:n], in1=tabs[tslot][:, 1, 0:n], op=ALU.mult),
                              ["prot%d" % r, "tab%d" % tslot], ["t2_%d" % r])
                        if isinstance(dst_ap, list):
                            for (d_ap, rows, dkey) in dst_ap:
                                P.pool(lambda e: e.tensor_tensor(out=d_ap, in0=t1[r][rows, 0:n], in1=t2[r][rows, 0:n], op=ALU.add),
                                       ["t1_%d" % r, "t2_%d" % r], [dkey])
                        elif dst_key == "QT":
                            P.pool(lambda e: e.tensor_tensor(out=dst_ap, in0=t1[r][:, 0:n].rearrange("p (a t) -> p a t", a=4),
                                                             in1=t2[r][:, 0:n].rearrange("p (a t) -> p a t", a=4), op=ALU.add),
                                   ["t1_%d" % r, "t2_%d" % r], [dst_key])
                        else:
                            P.pool(lambda e: e.tensor_tensor(out=dst_ap, in0=t1[r][:, 0:n], in1=t2[r][:, 0:n], op=ALU.add),
                                   ["t1_%d" % r, "t2_%d" % r], [dst_key])

                    for ch in range(8):
                        slot = ch % 2
                        c0 = ch * 512
                        load_x(xTv, c0, 512, slot)
                        P.dma(tabs[slot][:, 0, :], cdr["cosK"][:, c0:c0 + 512], writes=["tab%d" % slot])
                        P.dma(tabs[slot][:, 1, :], cdr["sinK"][:, c0:c0 + 512], writes=["tab%d" % slot])
                        for col0, kv in ((512, "k"), (640, "v")):
                            pi = proj_fm(col0, slot)
                            P.act(lambda e, pi=pi, kv=kv: e.copy(out=cmpT[kv][:, c0:c0 + 512], in_=pj[pi][:]), ["pj%d" % pi], ["cmp" + kv + "T"])
                        pi = proj_fm(768, slot)
                        rope_fm(pi, slot, [(KE[0][0:64, c0:c0 + 512], slice(0, 64), "KE0"), (KE[1][64:128, c0:c0 + 512], slice(64, 128), "KE1")], None, pt64, "pt64")
                        pi = proj_fm(896, slot)
                        rope_fm(pi, slot, winkT[:, c0:c0 + 512], "winkT", pt64, "pt64")
                        for tt in range(4):
                            vi = pvr.next()
                            for k in range(8):
                                P.pe(lambda e, k=k, vi=vi, tt=tt: e.matmul(pv[vi][:], lhsT=xb[slot][:, k, tt * 128:(tt + 1) * 128], rhs=Wn[:, k, 1024:1280],
                                                                            start=(k == 0), stop=(k == 7)), ["Wn", "xb%d" % slot], ["pv%d" % vi])
                            tg = ch * 4 + tt
                            P.act(lambda e, vi=vi, tg=tg: e.copy(out=slcv1[:, tg, :, 0:64], in_=pv[vi][:, 0:128].rearrange("p (g d) -> p g d", g=2)),
                                  ["pv%d" % vi], ["slcv1"])
                            P.dve(lambda e, vi=vi, tg=tg: e.tensor_copy(out=winv1[:, tg, :, 0:64], in_=pv[vi][:, 128:256].rearrange("p (g d) -> p g d", g=2)),
                                  ["pv%d" % vi], ["winv1"])
                    for oc in range(4):
                        slot = oc % 2
                        c0 = oc * 512
                        load_x(xTov, c0, 512, slot)
                        P.dma(tabs[slot][:, 0, :], cdr["cosQ"][:, c0:c0 + 512], writes=["tab%d" % slot])
                        P.dma(tabs[slot][:, 1, :], cdr["sinQ"][:, c0:c0 + 512], writes=["tab%d" % slot])
                        for hh in range(4):
                            pi = proj_fm(hh * 128, slot)
                            rope_fm(pi, slot, QT[:, oc * 4:(oc + 1) * 4, hh, :], "QT", pt64, "pt64")
                        for tt in range(4):
                            vi = pvr.next()
                            for k in range(8):
                                P.pe(lambda e, k=k, vi=vi, tt=tt: e.matmul(pv[vi][:, 0:24], lhsT=xb[slot][:, k, tt * 128:(tt + 1) * 128], rhs=Wn[:, k, 1280:1304],
                                                                            start=(k == 0), stop=(k == 7)), ["Wn", "xb%d" % slot], ["pv%d" % vi])
                            tg = oc * 4 + tt
                            P.act(lambda e, vi=vi, tg=tg: e.activation(out=gates[:, tg, :], in_=pv[vi][:, 0:24], func=AF.Sigmoid), ["pv%d" % vi], ["gates"])
                    dump("QT", QT[:].rearrange("p i a t -> p (i a t)"), [128, 4 * TO], "QT")
                    dump("cmpkT", cmpT["k"][:], [128, T], "cmpkT")
                    dump("slcv1", slcv1[:].rearrange("p a g d -> p (a g d)"), [128, 32 * 130], "slcv1")
                    dump("gates", gates[:].rearrange("p a g -> p (a g)"), [128, 16 * 24], "gates")
                P.barrier()
                if n_stage >= 2:
                    with ExitStack() as S2:
                        w1b = sb(S2, "w1b", [128, 32, 256], BF16)
                        posT = sb(S2, "posT", [128, 32], F32)
                        posTb = sb(S2, "posTb", [128, 32], BF16)
                        b1 = sb(S2, "b1", [128, 2], F32)
                        bias1 = sb(S2, "bias1", [128, 2], F32)
                        w2kf = sb(S2, "w2kf", [128, 2, 128], F32)
                        w2k = sb(S2, "w2k", [128, 2, 128], BF16)
                        w2vf = sb(S2, "w2vf", [128, 2, 64], F32)
                        w2v = sb(S2, "w2v", [128, 2, 64], BF16)
                        b2k = sb(S2, "b2k", [128, 1], F32)
                        b2v = sb(S2, "b2v", [128, 64], F32)
                        tabC = sb(S2, "tabC", [128, 2, 256], F32)
                        h1 = sb(S2, "h1", [128, 2, 256], BF16)
                        xg = sb(S2, "xg", [128, 256], F32)
                        ug = sb(S2, "ug", [128, 256], F32)
                        sg_ = sb(S2, "sg_", [128, 256], F32)
                        yk = sb(S2, "yk", [128, 256], F32)
                        ykb = sb(S2, "ykb", [128, 256], BF16)
                        tk1 = sb(S2, "tk1", [128, 256], F32)
                        tk2 = sb(S2, "tk2", [128, 256], F32)
                        ph = [ps(S2, "ph%d" % i, [128, 256]) for i in range(2)]
                        pcv = ps(S2, "pcv", [128, 2])
                        pkc = ps(S2, "pkc", [128, 256])
                        prk = ps(S2, "prk", [128, 256])
                        pvc = ps(S2, "pvc", [128, 64])
                        P.dma(w2kf[:].rearrange("p a n -> p (a n)"), cw2k, writes=["w2kf"])
                        P.dve(lambda e: e.tensor_copy(out=w2k[:], in_=w2kf[:]), ["w2kf"], ["w2k"])
                        P.dma(w2vf[:].rearrange("p a n -> p (a n)"), cw2v, writes=["w2vf"])
                        P.dve(lambda e: e.tensor_copy(out=w2v[:], in_=w2vf[:]), ["w2vf"], ["w2v"])
                        P.dma(b2k[:], cb2k, writes=["b2k"])
                        P.dma(b2v[:], cb2v.partition_broadcast(128), writes=["b2v"])
                        P.dma(tabC[:, 0, :], cdr["cosC"], writes=["tabC"])
                        P.dma(tabC[:, 1, :], cdr["sinC"], writes=["tabC"])
                        for kv in "kv":
                            load_cast(w1b[:].rearrange("p l n -> p (l n)"), cw1[kv], 32 * 256, "w1b")
                            P.dma(posT[:], cpos[kv], writes=["posT"])
                            P.dve(lambda e: e.tensor_copy(out=posTb[:], in_=posT[:]), ["posT"], ["posTb"])
                            P.dma(b1[:], cb1[kv], writes=["b1"])
                            for ht in range(2):
                                for l in range(32):
                                    P.pe(lambda e, ht=ht, l=l: e.matmul(pcv[:, ht:ht + 1], lhsT=w1b[0:64, l, ht * 128:(ht + 1) * 128], rhs=posTb[0:64, l:l + 1],
                                                                         start=(l == 0), stop=(l == 31)), ["w1b", "posTb"], ["pcv"])
                            P.dve(lambda e: e.tensor_tensor(out=bias1[:], in0=pcv[:], in1=b1[:], op=ALU.add), ["pcv", "b1"], ["bias1"])
                            for g in range(2):
                                gp = slice(g * 64, (g + 1) * 64)
                                for ht in range(2):
                                    for l in range(32):
                                        P.pe(lambda e, ht=ht, l=l, gp=gp, kv=kv: e.matmul(ph[ht][:, 0:255], lhsT=w1b[gp, l, ht * 128:(ht + 1) * 128],
                                                                                        rhs=cmpT[kv][gp, l:l + 16 * 254 + 1:16],
                                                                                        start=(l == 0), stop=(l == 31)), ["w1b", "cmp" + kv + "T"], ["ph%d" % ht])
                                    P.act(lambda e, ht=ht: e.activation(out=xg[:, 0:255], in_=ph[ht][:, 0:255], func=AF.Identity, bias=bias1[:, ht:ht + 1], scale=1.0),
                                          ["ph%d" % ht, "bias1"], ["xg"])
                                    P.dve(lambda e: e.tensor_tensor(out=ug[:, 0:255], in0=xg[:, 0:255], in1=xg[:, 0:255], op=ALU.mult), ["xg"], ["ug"])
                                    P.dve(lambda e: e.tensor_scalar(out=ug[:, 0:255], in0=ug[:, 0:255], scalar1=0.044715, scalar2=1.0, op0=ALU.mult, op1=ALU.add), ["ug"], ["ug"])
                                    P.dve(lambda e: e.tensor_tensor(out=ug[:, 0:255], in0=ug[:, 0:255], in1=xg[:, 0:255], op=ALU.mult), ["ug", "xg"], ["ug"])
                                    P.act(lambda e: e.activation(out=sg_[:, 0:255], in_=ug[:, 0:255], func=AF.Sigmoid, scale=1.5957691216057308), ["ug"], ["sg_"])
                                    P.dve(lambda e, ht=ht: e.tensor_tensor(out=h1[:, ht, 0:255], in0=xg[:, 0:255], in1=sg_[:, 0:255], op=ALU.mult), ["xg", "sg_"], ["h1"])
                                if kv == "k":
                                    for ht in range(2):
                                        P.pe(lambda e, ht=ht: e.matmul(pkc[:, 0:255], lhsT=w2k[:, ht, :], rhs=h1[:, ht, 0:255], start=(ht == 0), stop=(ht == 1)),
                                             ["w2k", "h1"], ["pkc"])
                                    P.act(lambda e: e.activation(out=yk[:, 0:255], in_=pkc[:, 0:255], func=AF.Identity, bias=b2k[:, 0:1], scale=1.0), ["pkc", "b2k"], ["yk"])
                                    P.act(lambda e: e.copy(out=ykb[:, 0:255], in_=yk[:, 0:255]), ["yk"], ["ykb"])
                                    P.pe(lambda e: e.matmul(prk[:, 0:255], lhsT=pt64[:], rhs=ykb[:, 0:255], start=True, stop=True), ["pt64", "ykb"], ["prk"])
                                    P.dve(lambda e: e.tensor_tensor(out=tk1[:, 0:255], in0=yk[:, 0:255], in1=tabC[:, 0, 0:255], op=ALU.mult), ["yk", "tabC"], ["tk1"])
                                    P.dve(lambda e: e.tensor_tensor(out=tk2[:, 0:255], in0=prk[:, 0:255], in1=tabC[:, 1, 0:255], op=ALU.mult), ["prk", "tabC"], ["tk2"])
                                    P.dve(lambda e, gp=gp: e.tensor_tensor(out=kcmpT[gp, 0:255], in0=tk1[gp, 0:255], in1=tk2[gp, 0:255], op=ALU.add), ["tk1", "tk2"], ["kcmpT"])
                                else:
                                    for a in range(2):
                                        cntn = 128 if a == 0 else 127
                                        for ht in range(2):
                                            P.pe(lambda e, ht=ht, a=a, cntn=cntn: e.matmul(pvc[0:cntn, :], lhsT=h1[:, ht, a * 128:a * 128 + cntn], rhs=w2v[:, ht, :],
                                                                                            start=(ht == 0), stop=(ht == 1)), ["w2v", "h1"], ["pvc"])
                                        P.dve(lambda e, a=a, cntn=cntn, g=g: e.tensor_tensor(out=vcmp1[0:cntn, a, g, 0:64], in0=pvc[0:cntn, :], in1=b2v[0:cntn, :], op=ALU.add),
                                              ["pvc", "b2v"], ["vcmp1"])
                        dump("kcmpT", kcmpT[:], [128, 256], "kcmpT")
                        dump("vcmp1", vcmp1[:].rearrange("p a g d -> p (a g d)"), [128, 260], "vcmp1")
                    P.barrier()
            P.barrier()
            if n_stage >= 3:
                with ExitStack() as S3:
                    def bc4(ap):
                        return ap.unsqueeze(1).broadcast_to([ap.shape[0], 4, ap.shape[1]])

                    wmask = sb(S3, "wmask", [128, 6, 128], BF16)
                    cmpmask = sb(S3, "cmpmask", [128, 2, 16, 128], BF16)
                    ovT = sb(S3, "ovT", [128, 2, 64], BF16)
                    tkm = sb(S3, "tkm", [128, 16, 64], F32)
                    tkb = sb(S3, "tkb", [128, 16, 64], F32)
                    wmask4 = sb(S3, "wmask4", [128, 6, 512], BF16)
                    cm4 = [sb(S3, "cm4_%d" % i_, [128, 2, 512], BF16) for i_ in range(2)]
                    QN = [sb(S3, "QN%d" % i_, [128, 512], BF16) for i_ in range(4)]
                    P.dma(wmask[:].rearrange("p a k -> p (a k)"), cdr["wmask"], writes=["wmask"])
                    P.dma(cmpmask[:].rearrange("p a i k -> p (a i k)"), cdr["cmpmask"], writes=["cmpmask"])
                    P.dma(ovT[:].rearrange("p a k -> p (a k)"), cdr["ovT"], writes=["ovT"])
                    P.dma(tkm[:].rearrange("p a k -> p (a k)"), cdr["tkm"], writes=["tkm"])
                    P.dma(tkb[:].rearrange("p a k -> p (a k)"), cdr["tkb"], writes=["tkb"])
                    for r_ in range(6):
                        P.pool(lambda e: e.tensor_copy(out=wmask4[:, r_, :].rearrange("p (a t) -> p a t", a=4), in_=bc4(wmask[:, r_, :])), ["wmask"], ["wmask4"])
                    eT = [sb(S3, "eT%d" % i, [128, 512], BF16) for i in range(4)]
                    oacc = sb(S3, "oacc", [128, 512], F32)
                    oab = sb(S3, "oab", [128, 512], BF16)
                    rz = sb(S3, "rz", [128, 4], F32)
                    coef = sb(S3, "coef", [128, 4], F32)
                    imp = sb(S3, "imp", [128, 64], F32)
                    score = sb(S3, "score", [128, 64], F32)
                    work = sb(S3, "work", [128, 64], F32)
                    m8 = sb(S3, "m8", [128, 16], F32)
                    nmk = [sb(S3, "nmk%d" % i_, [128, 2, 64], BF16) for i_ in range(2)]
                    pST = [ps(S3, "pST%d" % i, [128, 512]) for i in range(3)]
                    pA = ps(S3, "pA", [128, 4, 65])
                    pB = ps(S3, "pB", [128, 4, 64])
                    pS = ps(S3, "pS", [128, 4, 65])
                    pW = ps(S3, "pW", [128, 4, 65])
                    pTr = ps(S3, "pTr", [128, 128], BF16)
                    str_ = Ring([0, 1, 2])
                    etr = Ring([0, 1, 2, 3])

                    def scores(kT_ap, kkey, g, i, masks, q_ap=None, qkey="QT"):
                        gp = slice(g * 64, (g + 1) * 64)
                        si = str_.next()
                        ei = etr.next()
                        nm = len(masks)
                        if q_ap is None:
                            q_ap = QT[gp, i, :, :].rearrange("p a t -> p (a t)")
                        P.pe(lambda e: e.matmul(pST[si][:], lhsT=kT_ap, rhs=q_ap, start=True, stop=(nm == 0)), [kkey, qkey], ["pST%d" % si])
                        for mi, (ml, mr, mkeys) in enumerate(masks):
                            P.pe(lambda e: e.matmul(pST[si][:], lhsT=ml, rhs=mr, start=False, stop=(mi == nm - 1)), mkeys, ["pST%d" % si])
                        P.act(lambda e: e.activation(out=eT[ei][:], in_=pST[si][:], func=AF.Exp), ["pST%d" % si], ["eT%d" % ei])
                        return ei

                    def finish_branch(pacc, pkey, i, g, br, first):
                        P.dve(lambda e: e.tensor_scalar(out=rz[:], in0=pacc[:, :, 64], scalar1=1e-30, scalar2=None, op0=ALU.max), [pkey], ["rz"])
                        P.dve(lambda e: e.reciprocal(out=rz[:], in_=rz[:]), ["rz"], ["rz"])
                        P.dve(lambda e: e.tensor_tensor(out=coef[:], in0=rz[:], in1=gates[:, i, g * 12 + br:g * 12 + 12:3], op=ALU.mult), ["rz", "gates"], ["coef"])
                        for hh in range(4):
                            o = oacc[:, g * 256 + hh * 64:g * 256 + (hh + 1) * 64]
                            if first:
                                P.dve(lambda e, hh=hh, o=o: e.tensor_scalar(out=o, in0=pacc[:, hh, 0:64], scalar1=coef[:, hh:hh + 1], scalar2=None, op0=ALU.mult),
                                      [pkey, "coef"], ["oacc"])
                            else:
                                P.dve(lambda e, hh=hh, o=o: e.scalar_tensor_tensor(out=o, in0=pacc[:, hh, 0:64], scalar=coef[:, hh:hh + 1], in1=o, op0=ALU.mult, op1=ALU.add),
                                      [pkey, "coef", "oacc"], ["oacc"])

                    tasks = []

                    def mk_cmp(i, g, a, na):
                        gp = slice(g * 64, (g + 1) * 64)

                        def sc():
                            if g == 0:
                                P.pool(lambda e: e.tensor_copy(out=cm4[i % 2][:, a, :].rearrange("p (h t) -> p h t", h=4), in_=bc4(cmpmask[:, a, i, :])),
                                       ["cmpmask"], ["cm4_%d" % (i % 2)])
                            return scores(kcmpT[gp, a * 128:(a + 1) * 128], "kcmpT", g, i,
                                          [(identb[:], cm4[i % 2][:, a, :], ["identb", "cm4_%d" % (i % 2)])])

                        def pvf(ei):
                            for hh in range(4):
                                P.pe(lambda e: e.matmul(pA[:, hh, :], lhsT=eT[ei][:, hh * 128:(hh + 1) * 128], rhs=vcmp1[:, a, g, :],
                                                        start=(a == 0 and hh == 0), stop=(a == na - 1 and hh == 3)), ["eT%d" % ei, "vcmp1"], ["pA"])
                                P.pe(lambda e: e.matmul(pB[:, hh, :], lhsT=eT[ei][:, hh * 128:(hh + 1) * 128], rhs=ovT[:, a, :],
                                                        start=(a == 0 and hh == 0), stop=(a == na - 1 and hh == 3)), ["eT%d" % ei, "ovT"], ["pB"])

                        def post():
                            finish_branch(pA, "pA", i, g, 0, True)
                            P.dve(lambda e: e.tensor_scalar(out=imp[:], in0=pB[:, 0, :], scalar1=rz[:, 0:1], scalar2=None, op0=ALU.mult), ["pB", "rz"], ["imp"])
                            for hh in range(1, 4):
                                P.dve(lambda e: e.scalar_tensor_tensor(out=imp[:], in0=pB[:, hh, :], scalar=rz[:, hh:hh + 1], in1=imp[:], op0=ALU.mult, op1=ALU.add),
                                      ["pB", "rz", "imp"], ["imp"])
                            P.dve(lambda e: e.tensor_tensor(out=score[:], in0=imp[:], in1=tkm[:, i, :], op=ALU.mult), ["imp", "tkm"], ["score"])
                            P.dve(lambda e: e.tensor_tensor(out=score[:], in0=score[:], in1=tkb[:, i, :], op=ALU.add), ["score", "tkb"], ["score"])
                            P.dve(lambda e: e.max(out=m8[:, 0:8], in_=score[:]), ["score"], ["m8"])
                            P.dve(lambda e: e.match_replace(out=work[:], in_to_replace=m8[:, 0:8], in_values=score[:], imm_value=-3.0e38), ["score", "m8"], ["work"])
                            P.dve(lambda e: e.max(out=m8[:, 8:16], in_=work[:]), ["work"], ["m8"])
                            P.dve(lambda e: e.tensor_scalar(out=nmk[g][:], in0=score[:].unsqueeze(1).broadcast_to([128, 2, 64]), scalar1=m8[:, 15:16], scalar2=NEGM,
                                                            op0=ALU.is_lt, op1=ALU.mult), ["score", "m8"], ["nmk%d" % g])
                            if ("imp%d_%d" % (i, g)) in debug:
                                dump("imp%d_%d" % (i, g), imp[:], [128, 64], "imp")
                                dump("score%d_%d" % (i, g), score[:], [128, 64], "score")
                                dump("m8%d_%d" % (i, g), m8[:], [128, 16], "m8")
                        return [None, sc, pvf, post if a == na - 1 else None]

                    def mk_win(i, g, idx, r, j, nw):
                        gp = slice(g * 64, (g + 1) * 64)

                        def sc():
                            return scores(winkT[gp, j * 128:(j + 1) * 128], "winkT", g, i,
                                          [(identb[:], wmask4[:, r, :], ["identb", "wmask4"])])

                        def pvf(ei):
                            for hh in range(4):
                                P.pe(lambda e: e.matmul(pW[:, hh, :], lhsT=eT[ei][:, hh * 128:(hh + 1) * 128], rhs=winv1[:, j, g, :],
                                                        start=(idx == 0 and hh == 0), stop=(idx == nw - 1 and hh == 3)), ["eT%d" % ei, "winv1"], ["pW"])

                        def post():
                            finish_branch(pW, "pW", i, g, 2, False)
                        return [None, sc, pvf, post if idx == nw - 1 else None]

                    def tile_end_pe(i):
                        for ct in range(4):
                            P.pe(lambda e: e.transpose(out=pTr[:], in_=oab[:, ct * 128:(ct + 1) * 128], identity=identb[:]), ["oab", "identb"], ["pTr"])
                            P.dve(lambda e: e.tensor_copy(out=oattnT[:, ct, i * 128:(i + 1) * 128], in_=pTr[:]), ["pTr"], ["oattnT"])

                    def mk_slc(i, g, j, nj):
                        gp = slice(g * 64, (g + 1) * 64)

                        qn_i = (2 * i + g) % 4
                        oh = slice((1 - g) * 64, (2 - g) * 64)

                        def pre():
                            P.pool(lambda e: e.tensor_copy(out=QN[qn_i][gp, :], in_=QT[gp, i, :, :].rearrange("p a t -> p (a t)")), ["QT"], ["QN%d" % qn_i])
                            P.pe(lambda e: e.transpose(out=pTr[:], in_=nmk[g][:].rearrange("p a s -> p (a s)"), identity=identb[:]), ["nmk%d" % g, "identb"], ["pTr"])
                            P.dve(lambda e: e.tensor_copy(out=QN[qn_i][oh, :].rearrange("p (a t) -> p a t", a=4), in_=bc4(pTr[oh, :])), ["pTr"], ["QN%d" % qn_i])
                            if g == 0 and i > 0:
                                tile_end_pe(i - 1)

                        def sc():
                            masks = []
                            if j >= 2 * i:
                                masks.append((identb[:], wmask4[:, 4 + (j - 2 * i), :], ["identb", "wmask4"]))
                            return scores(KE[g][:, j * 128:(j + 1) * 128], "KE%d" % g, g, i, masks, q_ap=QN[qn_i][:], qkey="QN%d" % qn_i)

                        def pvf(ei):
                            for hh in range(4):
                                P.pe(lambda e: e.matmul(pS[:, hh, :], lhsT=eT[ei][:, hh * 128:(hh + 1) * 128], rhs=slcv1[:, j, g, :],
                                                        start=(j == 0 and hh == 0), stop=(j == nj - 1 and hh == 3)), ["eT%d" % ei, "slcv1"], ["pS"])

                        def post():
                            finish_branch(pS, "pS", i, g, 1, False)
                            if g == 1:
                                if ("oacc%d" % i) in debug:
                                    dump("oacc%d" % i, oacc[:], [128, 512], "oacc")
                                P.pool(lambda e: e.tensor_copy(out=oab[:], in_=oacc[:]), ["oacc"], ["oab"])
                        return [pre if j == 0 else None, sc, pvf, post if j == nj - 1 else None]

                    for i in range(16):
                        for g in range(2):
                            na = 1 if i < 8 else 2
                            for a in range(na):
                                tasks.append(mk_cmp(i, g, a, na))
                            js = [(r, 2 * i - 4 + r) for r in range(6) if 2 * i - 4 + r >= 0]
                            for idx, (r, j) in enumerate(js):
                                tasks.append(mk_win(i, g, idx, r, j, len(js)))
                            nj = 2 * i + 2
                            for j in range(nj):
                                tasks.append(mk_slc(i, g, j, nj))
                    nt = len(tasks)
                    eis = [None] * nt

                    def emit_score(k):
                        if tasks[k][0] is not None:
                            tasks[k][0]()
                        eis[k] = tasks[k][1]()

                    emit_score(0)
                    emit_score(1)
                    for k in range(nt):
                        if k + 2 < nt:
                            emit_score(k + 2)
                        tasks[k][2](eis[k])
                        if tasks[k][3] is not None:
                            tasks[k][3]()
                    tile_end_pe(15)
                    dump("oattnT", oattnT[:].rearrange("p a t -> p (a t)"), [128, 4 * TO], "oattnT")
                P.barrier()
        P.barrier()

        B_ = ExitStack()
        oretT = sb(B_, "oretT", [128, 8, TO], BF16)
        if n_stage >= 4:
            with ExitStack() as S4:
                W4 = sb(S4, "W4", [128, 8, 2048], BF16)
                load_cast(W4[:].rearrange("p k n -> p (k n)"), w4t, 8 * 2048, "W4")
                pt128 = sb(S4, "pt128", [128, 128], BF16)
                P.dma(pt128[:], cdr["pt128"], writes=["pt128"])
                Dc = sb(S4, "Dc", [128, 2, 4, 128], F32)
                xi = sb(S4, "xi", [128, 4, 128], F32)
                zeta = sb(S4, "zeta", [128, 2, 4], F32)
                P.dma(Dc[:].rearrange("p a h c -> p (a h c)"), cdr["Dc"], writes=["Dc"])
                P.dma(xi[:].rearrange("p h c -> p (h c)"), cdr["xi"], writes=["xi"])
                P.dma(zeta[:].rearrange("p a h -> p (a h)"), cdr["zeta"], writes=["zeta"])
                xst = wst3
                xb = sb(S4, "xb4", [128, 8, 512], BF16)
                xob = sb(S4, "xob4", [128, 8, 256], BF16)
                tabs = sb(S4, "tab4", [128, 2, 512], F32)
                tabq = sb(S4, "tabq4", [128, 2, 256], F32)
                ybf2 = [sb(S4, "ybf4_%d" % i_, [128, 512], BF16) for i_ in range(2)]
                t12 = [sb(S4, "t1_4_%d" % i_, [128, 512], F32) for i_ in range(2)]
                t22 = [sb(S4, "t2_4_%d" % i_, [128, 512], F32) for i_ in range(2)]
                rr4 = Ring([0, 1])
                kT = sb(S4, "kT4", [128, 4, 512], BF16)
                qT = sb(S4, "qT4", [128, 4, 256], BF16)
                qxT = sb(S4, "qxT4", [128, 4, 256], BF16)
                vtok = sb(S4, "vtok", [128, 4, 1024], BF16)
                kz = sb(S4, "kz", [128, 4, 4, 128], BF16)
                R = sb(S4, "R", [128, 4, 256], F32)
                Rb = sb(S4, "Rb", [128, 4, 256], BF16)
                sc = [sb(S4, "sc%d" % i_, [128, 2, 128], BF16) for i_ in range(2)]
                epsT = sb(S4, "epsT", [128, 1], F32)
                P.dve(lambda e: e.memset(epsT[:], LN_EPS), [], ["epsT"])
                pending4 = []
                st6 = sb(S4, "st6", [128, 6], F32)
                mv = sb(S4, "mv", [128, 2], F32)
                rstd = sb(S4, "rstd", [128, 1], F32)
                oretb = [sb(S4, "oretb%d" % i_, [128, 1024], BF16) for i_ in range(2)]
                pj = [ps(S4, "pj4_%d" % i, [128, 512]) for i in range(3)]
                psc = [ps(S4, "psc%d" % i_, [128, 2, 128]) for i_ in range(2)]
                po = [ps(S4, "po%d" % i_, [128, 256]) for i_ in range(2)]
                pTrw = ps(S4, "pTr4", [128, 512], BF16)
                pTr = pTrw[:, 0:128]
                pjr = Ring([0, 1, 2])
                P.dve(lambda e: e.memset(R[:].rearrange("p h e -> p (h e)"), 0.0), [], ["R%d" % h_ for h_ in range(4)])
                P.dve(lambda e: e.memset(Rb[:].rearrange("p h e -> p (h e)"), 0.0), [], ["Rb%d" % h_ for h_ in range(4)])
                xTv = xT.rearrange("(k p) t -> p k t", p=128)
                xTov = xTo.rearrange("(k p) t -> p k t", p=128)

                def rope4(pi, n, tab, tabkey, dst_ap, dst_key):
                    ri = pjr.next()
                    prot = pj[ri]
                    rb = rr4.next()
                    ybf, t1, t2 = ybf2[rb], t12[rb], t22[rb]
                    P.act(lambda e: e.copy(out=ybf[:, 0:n], in_=pj[pi][:, 0:n]), ["pj4_%d" % pi], ["ybf4_%d" % rb])
                    P.pe(lambda e: e.matmul(prot[:, 0:n], lhsT=pt128[:], rhs=ybf[:, 0:n], start=True, stop=True), ["pt128", "ybf4_%d" % rb], ["pj4_%d" % ri])
                    P.dve(lambda e: e.tensor_tensor(out=t1[:, 0:n], in0=pj[pi][:, 0:n], in1=tab[:, 0, 0:n], op=ALU.mult), ["pj4_%d" % pi, tabkey], ["t1_4_%d" % rb])
                    P.dve(lambda e: e.tensor_tensor(out=t2[:, 0:n], in0=prot[:, 0:n], in1=tab[:, 1, 0:n], op=ALU.mult), ["pj4_%d" % ri, tabkey], ["t2_4_%d" % rb])
                    P.pool(lambda e: e.tensor_tensor(out=dst_ap, in0=t1[:, 0:n], in1=t2[:, 0:n], op=ALU.add), ["t1_4_%d" % rb, "t2_4_%d" % rb], [dst_key])

                for gch in range(8):
                    c0 = gch * 512
                    o0 = gch * 256
                    P.dma(xst[:], xTv[:, :, c0:c0 + 512], writes=["wst"])
                    P.act(lambda e: e.copy(out=xb[:, 0:4, :], in_=xst[:, 0:4, :]), ["wst"], ["xb4"])
                    P.dve(lambda e: e.tensor_copy(out=xb[:, 4:8, :], in_=xst[:, 4:8, :]), ["wst"], ["xb4"])
                    P.dma(xst[:, :, 0:256], xTov[:, :, o0:o0 + 256], writes=["wst"])
                    P.act(lambda e: e.copy(out=xob[:, 0:4, :], in_=xst[:, 0:4, 0:256]), ["wst"], ["xob4"])
                    P.dve(lambda e: e.tensor_copy(out=xob[:, 4:8, :], in_=xst[:, 4:8, 0:256]), ["wst"], ["xob4"])
                    P.dma(tabs[:, 0, :], cdr["cosRK"][:, c0:c0 + 512], writes=["tab4"])
                    P.dma(tabs[:, 1, :], cdr["sinRK"][:, c0:c0 + 512], writes=["tab4"])
                    P.dma(tabq[:, 0, :], cdr["cosRQ"][:, o0:o0 + 256], writes=["tabq4"])
                    P.dma(tabq[:, 1, :], cdr["sinRQ"][:, o0:o0 + 256], writes=["tabq4"])
                    for h in range(4):
                        pi = pjr.next()
                        for k in range(8):
                            P.pe(lambda e, k=k, pi=pi, h=h: e.matmul(pj[pi][:], lhsT=W4[:, k, 512 + h * 128:512 + (h + 1) * 128], rhs=xb[:, k, :],
                                                                      start=(k == 0), stop=(k == 7)), ["W4", "xb4"], ["pj4_%d" % pi])
                        rope4(pi, 512, tabs, "tab4", kT[:, h, :], "kT4")
                    for h in range(4):
                        pi = pjr.next()
                        for k in range(8):
                            P.pe(lambda e, k=k, pi=pi, h=h: e.matmul(pj[pi][:, 0:256], lhsT=W4[:, k, h * 128:(h + 1) * 128], rhs=xob[:, k, :],
                                                                      start=(k == 0), stop=(k == 7)), ["W4", "xob4"], ["pj4_%d" % pi])
                        rope4(pi, 256, tabq, "tabq4", qT[:, h, :], "qT4")
                    for pp in range(2):
                        P.dve(lambda e, pp=pp: e.tensor_tensor(out=qxT[:, :, pp * 128:(pp + 1) * 128], in0=qT[:, :, pp * 128:(pp + 1) * 128], in1=xi[:], op=ALU.mult),
                              ["qT4", "xi"], ["qxT4"])
                    for tt in range(4):
                        for hf in range(2):
                            pi = pjr.next()
                            for k in range(8):
                                P.pe(lambda e, k=k, pi=pi, tt=tt, hf=hf: e.matmul(pj[pi][:], lhsT=xb[:, k, tt * 128:(tt + 1) * 128],
                                                                                   rhs=W4[:, k, 1024 + hf * 512:1024 + (hf + 1) * 512],
                                                                                   start=(k == 0), stop=(k == 7)), ["W4", "xb4"], ["pj4_%d" % pi])
                            P.act(lambda e, pi=pi, tt=tt, hf=hf: e.copy(out=vtok[:, tt, hf * 512:(hf + 1) * 512], in_=pj[pi][:]), ["pj4_%d" % pi], ["vtok"])
                    for tt in range(4):
                        for h in range(4):
                            P.pe(lambda e: e.transpose(out=pTrw[:, h * 128:(h + 1) * 128], in_=kT[:, h, tt * 128:(tt + 1) * 128], identity=identb[:]), ["kT4", "identb"], ["pTr4"])
                        P.dve(lambda e: e.tensor_tensor(out=kz[:, tt, :, :], in0=pTrw[:].rearrange("p (h d) -> p h d", h=4),
                                                        in1=zeta[:, tt % 2, :].unsqueeze(2).broadcast_to([128, 4, 128]), op=ALU.mult), ["pTr4", "zeta"], ["kz"])
                    units = [(pp, h) for pp in range(2) for h in range(4)]

                    def phaseA(pp, h, ub):
                        qs = slice(pp * 128, (pp + 1) * 128)
                        for mt in range(2):
                            tt = pp * 2 + mt
                            P.pe(lambda e: e.matmul(psc[ub][:, mt, :], lhsT=kT[:, h, tt * 128:(tt + 1) * 128], rhs=qT[:, h, qs], start=True, stop=True),
                                 ["kT4", "qT4"], ["psc%d" % ub])
                        P.dve(lambda e: e.tensor_tensor(out=sc[ub][:], in0=psc[ub][:], in1=Dc[:, :, h, :], op=ALU.mult), ["psc%d" % ub, "Dc"], ["sc%d" % ub])

                    def phaseBC(pp, h, ub):
                        i = gch * 2 + pp
                        qs = slice(pp * 128, (pp + 1) * 128)
                        hs = slice(h * 256, (h + 1) * 256)
                        ob = oretb[pp]
                        for mt in range(2):
                            tt = pp * 2 + mt
                            P.pe(lambda e: e.matmul(po[ub][:], lhsT=sc[ub][:, mt, :], rhs=vtok[:, tt, hs], start=(mt == 0), stop=False), ["sc%d" % ub, "vtok"], ["po%d" % ub])
                        P.pe(lambda e: e.matmul(po[ub][:], lhsT=qxT[:, h, qs], rhs=Rb[:, h, :], start=False, stop=True), ["qxT4", "Rb%d" % h], ["po%d" % ub])
                        ri = pjr.next()
                        for mt in range(2):
                            tt = pp * 2 + mt
                            P.pe(lambda e: e.matmul(pj[ri][:, 0:256], lhsT=kz[:, tt, h, :], rhs=vtok[:, tt, hs], start=(mt == 0), stop=(mt == 1)),
                                 ["kz", "vtok"], ["pj4_%d" % ri])
                        P.dve(lambda e: e.bn_stats(out=st6[:], in_=po[ub][:]), ["po%d" % ub], ["st6"])
                        P.dve(lambda e: e.bn_aggr(out=mv[:], in_=st6[:]), ["st6"], ["mv"])
                        P.act(lambda e: e.activation(out=rstd[:], in_=mv[:, 1:2], func=AF.Sqrt, bias=epsT[:, 0:1], scale=1.0), ["mv", "epsT"], ["rstd"])
                        P.dve(lambda e: e.reciprocal(out=rstd[:], in_=rstd[:]), ["rstd"], ["rstd"])
                        P.dve(lambda e: e.tensor_scalar(out=ob[:, hs], in0=po[ub][:], scalar1=mv[:, 0:1], scalar2=rstd[:, 0:1], op0=ALU.subtract, op1=ALU.mult),
                              ["po%d" % ub, "mv", "rstd"], ["oretb%d" % pp])
                        P.dve(lambda e: e.scalar_tensor_tensor(out=R[:, h, :], in0=R[:, h, :], scalar=decay256[h], in1=pj[ri][:, 0:256], op0=ALU.mult, op1=ALU.add),
                              ["R%d" % h, "pj4_%d" % ri], ["R%d" % h])
                        P.act(lambda e: e.copy(out=Rb[:, h, :], in_=R[:, h, :]), ["R%d" % h], ["Rb%d" % h])

                    def pair_end(pp, i):
                        ob = oretb[pp]
                        for et in range(8):
                            P.pe(lambda e: e.transpose(out=pTr[:], in_=ob[:, et * 128:(et + 1) * 128], identity=identb[:]), ["oretb%d" % pp, "identb"], ["pTr4"])
                            P.act(lambda e: e.copy(out=oretT[:, et, i * 128:(i + 1) * 128], in_=pTr[:]), ["pTr4"], ["oretT"])

                    phaseA(units[0][0], units[0][1], 0)
                    for u, (pp, h) in enumerate(units):
                        if u + 1 < len(units):
                            phaseA(units[u + 1][0], units[u + 1][1], (u + 1) % 2)
                        phaseBC(pp, h, u % 2)
                        if pending4:
                            pending4.pop(0)()
                        if h == 3:
                            pending4.append(lambda pp=pp, i=gch * 2 + pp: pair_end(pp, i))
                while pending4:
                    pending4.pop(0)()
                dump("oretT", oretT[:].rearrange("p a t -> p (a t)"), [128, 8 * TO], "oretT")
            P.barrier()

        if n_stage >= 5:
            with ExitStack() as S5a:
                Wmg = sb(S5a, "Wmg", [128, 8, 2048], BF16)
                Wa = sb(S5a, "Wa", [128, 4, 1024], BF16)
                Wr = sb(S5a, "Wr", [128, 8, 1024], BF16)
                Wgt = sb(S5a, "Wgt", [128, 8, 1024], BF16)
                load_cast(Wmg[:].rearrange("p k n -> p (k n)"), wmgt, 8 * 2048, "Wmg")
                load_cast(Wa[:].rearrange("p k n -> p (k n)"), wat, 4 * 1024, "Wa")
                load_cast(Wr[:].rearrange("p k n -> p (k n)"), wrt, 8 * 1024, "Wr")
                load_cast(Wgt[:].rearrange("p k n -> p (k n)"), wgtt, 8 * 1024, "Wgt")
                gg8 = sb(S5a, "gg8", [128, 8], F32)
                gb8 = sb(S5a, "gb8", [128, 8], F32)
                P.dma(gg8[:], gng8, writes=["gg8"])
                P.dma(gb8[:], gnb8, writes=["gb8"])
                xb = sb(S5a, "xb5", [128, 8, 512], BF16)
                og = sb(S5a, "og", [128, 8, 512], BF16)
                sgt = [sb(S5a, "sgt%d" % i, [128, 512], F32) for i in range(2)]
                yn = [sb(S5a, "yn%d" % i, [128, 512], F32) for i in range(2)]
                ga2 = [sb(S5a, "ga%d" % i, [128, 512], F32) for i in range(2)]
                gr2 = [sb(S5a, "gr%d" % i, [128, 512], F32) for i in range(2)]
                ma2 = [sb(S5a, "ma%d" % i, [128, 512], F32) for i in range(2)]
                bk = [ps(S5a, "bk%d" % i, [128, 512]) for i in range(8)]
                pgt = [bk[4], bk[5]]
                xTov = xTo.rearrange("(k p) t -> p k t", p=128)
                for oc in range(4):
                    c0 = oc * 512
                    cs_ = slice(c0, c0 + 512)
                    P.dma(wst3[:], xTov[:, :, cs_], writes=["wst"])
                    P.act(lambda e: e.copy(out=xb[:, 0:4, :], in_=wst3[:, 0:4, :]), ["wst"], ["xb5"])
                    P.dve(lambda e: e.tensor_copy(out=xb[:, 4:8, :], in_=wst3[:, 4:8, :]), ["wst"], ["xb5"])
                    for et in range(8):
                        b_ = et % 2
                        for k in range(8):
                            P.pe(lambda e: e.matmul(pgt[b_][:], lhsT=Wgt[:, k, et * 128:(et + 1) * 128], rhs=xb[:, k, :], start=(k == 0), stop=(k == 7)),
                                 ["Wgt", "xb5"], ["bk%d" % (4 + b_)])
                        P.act(lambda e: e.activation(out=sgt[b_][:], in_=pgt[b_][:], func=AF.Silu), ["bk%d" % (4 + b_)], ["sgt%d" % b_])
                        P.act(lambda e: e.activation(out=yn[b_][:], in_=oretT[:, et, cs_], func=AF.Identity, scale=gg8[:, et:et + 1], bias=gb8[:, et:et + 1]),
                              ["oretT", "gg8", "gb8"], ["yn%d" % b_])
                        P.dve(lambda e: e.tensor_tensor(out=og[:, et, :], in0=yn[b_][:], in1=sgt[b_][:], op=ALU.mult), ["yn%d" % b_, "sgt%d" % b_], ["og"])
                    for ct in range(8):
                        cb = (ct % 2) * 4
                        cp = ct % 2
                        pg0, pg1, pu0, pu1 = bk[cb], bk[cb + 1], bk[cb + 2], bk[cb + 3]
                        kg0, kg1, ku0, ku1 = ["bk%d" % (cb + q_) for q_ in range(4)]
                        ga, gr, ma = ga2[cp], gr2[cp], ma2[cp]
                        for k in range(8):
                            P.pe(lambda e: e.matmul(pg0[:], lhsT=Wmg[:, k, ct * 128:(ct + 1) * 128], rhs=xb[:, k, :], start=(k == 0), stop=(k == 7)),
                                 ["Wmg", "xb5"], [kg0])
                        for k in range(8):
                            P.pe(lambda e: e.matmul(pg1[:], lhsT=Wmg[:, k, 1024 + ct * 128:1024 + (ct + 1) * 128], rhs=xb[:, k, :], start=(k == 0), stop=(k == 7)),
                                 ["Wmg", "xb5"], [kg1])
                        for k in range(4):
                            P.pe(lambda e: e.matmul(pu0[:], lhsT=Wa[:, k, ct * 128:(ct + 1) * 128], rhs=oattnT[:, k, cs_], start=(k == 0), stop=(k == 3)),
                                 ["Wa", "oattnT"], [ku0])
                        for k in range(8):
                            P.pe(lambda e: e.matmul(pu1[:], lhsT=Wr[:, k, ct * 128:(ct + 1) * 128], rhs=og[:, k, :], start=(k == 0), stop=(k == 7)),
                                 ["Wr", "og"], [ku1])
                        P.act(lambda e: e.activation(out=ga[:], in_=pg0[:], func=AF.Sigmoid), [kg0], ["ga%d" % cp])
                        P.act(lambda e: e.activation(out=gr[:], in_=pg1[:], func=AF.Sigmoid), [kg1], ["gr%d" % cp])
                        P.dve(lambda e: e.tensor_tensor(out=ma[:], in0=pu0[:], in1=ga[:], op=ALU.mult), [ku0, "ga%d" % cp], ["ma%d" % cp])
                        P.dve(lambda e: e.tensor_tensor(out=gr[:], in0=pu1[:], in1=gr[:], op=ALU.mult), [ku1, "gr%d" % cp], ["gr%d" % cp])
                        P.pool(lambda e: e.tensor_tensor(out=x1T[:, ct, cs_], in0=ma[:], in1=gr[:], op=ALU.add), ["ma%d" % cp, "gr%d" % cp], ["mx%d" % (oc * 4 + t_) for t_ in range(4)])
                dump("mergedT", x1T[:].rearrange("p a t -> p (a t)"), [128, 8 * TO], "mx0")
            P.barrier()
        B_.close()
        A_.close()
        if n_stage >= 5:
            with ExitStack() as S56:
                acc = sb(S56, "acc", [128, 16, 1024], F32)
                lng = sb(S56, "lng", [128, 1024], F32)
                lnb = sb(S56, "lnb", [128, 1024], F32)
                st12 = sb(S56, "st12", [128, 2, 6], F32)
                mv = sb(S56, "mv5", [128, 2], F32)
                rstd = sb(S56, "rstd5", [128, 1], F32)

                def layer_norm(src_ap, src_key, dst_ap, dst_key, tmp_ap, tmp_key, eps=LN_EPS):
                    for hf in range(2):
                        P.dve(lambda e: e.bn_stats(out=st12[:, hf, :], in_=src_ap[:, hf * 512:(hf + 1) * 512]), [src_key], ["st12"])
                    P.dve(lambda e: e.bn_aggr(out=mv[:], in_=st12[:].rearrange("p a s -> p (a s)")), ["st12"], ["mv5"])
                    P.dve(lambda e: e.tensor_scalar(out=rstd[:], in0=mv[:, 1:2], scalar1=eps, scalar2=None, op0=ALU.add), ["mv5"], ["rstd5"])
                    P.act(lambda e: e.activation(out=rstd[:], in_=rstd[:], func=AF.Sqrt), ["rstd5"], ["rstd5"])
                    P.dve(lambda e: e.reciprocal(out=rstd[:], in_=rstd[:]), ["rstd5"], ["rstd5"])
                    P.dve(lambda e: e.scalar_tensor_tensor(out=tmp_ap, in0=src_ap, scalar=mv[:, 0:1], in1=lng[:], op0=ALU.subtract, op1=ALU.mult),
                          [src_key, "mv5", "lng"], [tmp_key])
                    P.dve(lambda e: e.scalar_tensor_tensor(out=dst_ap, in0=tmp_ap, scalar=rstd[:, 0:1], in1=lnb[:], op0=ALU.mult, op1=ALU.add),
                          [tmp_key, "rstd5", "lnb"], [dst_key])

                with ExitStack() as S5b:
                    Wo = sb(S5b, "Wo", [128, 8, 1024], BF16)
                    load_cast(Wo[:].rearrange("p k n -> p (k n)"), wot, 8 * 1024, "Wo")
                    P.dma(lng[:], ln1g.partition_broadcast(128), writes=["lng"])
                    P.dma(lnb[:], ln1b.partition_broadcast(128), writes=["lnb"])
                    xres2 = [sb(S5b, "xres%d" % i_, [128, 1024], F32) for i_ in range(2)]
                    yt2 = [sb(S5b, "yt%d" % i_, [128, 1024], F32) for i_ in range(2)]
                    x1b2 = [sb(S5b, "x1b%d" % i_, [128, 1024], BF16) for i_ in range(2)]
                    pm2 = [[ps(S5b, "pm%d_%d" % (q_, i_), [128, 512]) for i_ in range(2)] for q_ in range(2)]
                    pTr = ps(S5b, "pTr5", [128, 128], BF16)
                    pend5 = []
                    for i in range(16):
                        q_ = i % 2
                        xres, yt, x1b, pm = xres2[q_], yt2[q_], x1b2[q_], pm2[q_]
                        ts_ = slice(i * 128, (i + 1) * 128)
                        if len(pend5) >= 2:
                            pend5.pop(0)()
                        P.dma(xres[:], xo[ts_, :], writes=["xres%d" % q_])
                        for hf in range(2):
                            for k in range(8):
                                P.pe(lambda e: e.matmul(pm[hf][:], lhsT=x1T[:, k, ts_], rhs=Wo[:, k, hf * 512:(hf + 1) * 512], start=(k == 0), stop=(k == 7)),
                                     ["mx%d" % i, "Wo"], ["pm%d_%d" % (q_, hf)])
                            P.dve(lambda e: e.scalar_tensor_tensor(out=yt[:, hf * 512:(hf + 1) * 512], in0=xres[:, hf * 512:(hf + 1) * 512], scalar=ALPHA,
                                                                   in1=pm[hf][:], op0=ALU.mult, op1=ALU.add), ["xres%d" % q_, "pm%d_%d" % (q_, hf)], ["yt%d" % q_])
                        layer_norm(yt[:], "yt%d" % q_, acc[:, i, :], "acc%d" % i, yt[:], "yt%d" % q_)
                        if ("x1_%d" % i) in debug:
                            dump("x1_%d" % i, acc[:, i, :], [128, 1024], "acc%d" % i)
                        P.pool(lambda e: e.tensor_copy(out=x1b[:], in_=acc[:, i, :]), ["acc%d" % i], ["x1b%d" % q_])

                        def tr5(i=i, q_=q_, x1b=x1b, ts_=ts_):
                            for dt_ in range(8):
                                P.pe(lambda e: e.transpose(out=pTr[:], in_=x1b[:, dt_ * 128:(dt_ + 1) * 128], identity=identb[:]), ["x1b%d" % q_, "identb"], ["pTr5"])
                                P.act(lambda e: e.copy(out=x1T[:, dt_, ts_], in_=pTr[:]), ["pTr5"], ["mx%d" % i])
                        pend5.append(tr5)
                    while pend5:
                        pend5.pop(0)()
                P.barrier()
                if n_stage >= 6:
                    with ExitStack() as S6:
                        P.dma(lng[:], ln2g.partition_broadcast(128), writes=["lng"])
                        P.dma(lnb[:], ln2b.partition_broadcast(128), writes=["lnb"])
                        comb = sb(S6, "comb", [128, 16, 32], F32)
                        wrf = sb(S6, "wrf", [128, 8, 36], F32)
                        wrb = sb(S6, "wrb", [128, 8, 36], BF16)
                        brb = sb(S6, "brb", [128, 36], F32)
                        P.dma(wrf[:].rearrange("p k n -> p (k n)"), wrout, writes=["wrf"])
                        P.dve(lambda e: e.tensor_copy(out=wrb[:], in_=wrf[:]), ["wrf"], ["wrb"])
                        P.dma(brb[:], brout.partition_broadcast(128), writes=["brb"])
                        lg = sb(S6, "lg", [128, 16, 36], F32)
                        gmx = sb(S6, "gmx", [128, 16], F32)
                        gsh = sb(S6, "gsh", [128, 16, 4], F32)
                        gex = sb(S6, "gex", [128, 16, 4], F32)
                        gsum = sb(S6, "gsum", [128, 16], F32)
                        gprob = sb(S6, "gprob", [128, 16], F32)
                        ohg = sb(S6, "ohg", [128, 16, 4], F32)
                        tmp48 = sb(S6, "tmp48", [128, 16, 4, 8], F32)
                        isel = sb(S6, "isel", [128, 16, 8], F32)
                        isel2 = sb(S6, "isel2", [128, 16, 8], F32)
                        eq0 = sb(S6, "eq0", [128, 16, 8], F32)
                        eq1 = sb(S6, "eq1", [128, 16, 8], F32)
                        m0 = sb(S6, "m0r", [128, 16], F32)
                        m1 = sb(S6, "m1r", [128, 16], F32)
                        dlt = sb(S6, "dlt", [128, 16], F32)
                        w2e = sb(S6, "w2e", [128, 16], F32)
                        wsum = sb(S6, "wsum", [128, 16], F32)
                        wt1 = sb(S6, "wt1", [128, 16], F32)
                        wt2 = sb(S6, "wt2", [128, 16], F32)
                        ce = sb(S6, "ce", [128, 16, 8], F32)
                        ce2 = sb(S6, "ce2", [128, 16, 8], F32)
                        SR = ExitStack()
                        plg = [ps(SR, "plg%d" % i_, [128, 8, 36]) for i_ in range(2)]
                        AXX = mybir.AxisListType.X

                        def b3(ap, n):
                            return ap.unsqueeze(2).broadcast_to([128, 16, n])
                        for i in range(16):
                            ts_ = slice(i * 128, (i + 1) * 128)
                            for k in range(8):
                                P.pe(lambda e: e.matmul(plg[i // 8][:, i % 8, :], lhsT=x1T[:, k, ts_], rhs=wrb[:, k, :], start=(k == 0), stop=(k == 7)),
                                     ["mx%d" % i, "wrb"], ["plg%d" % (i // 8)])
                        for hf in range(2):
                            P.dve(lambda e: e.tensor_tensor(out=lg[:, hf * 8:(hf + 1) * 8, :], in0=plg[hf][:], in1=brb[:].unsqueeze(1).broadcast_to([128, 8, 36]), op=ALU.add),
                                  ["plg%d" % hf, "brb"], ["lg"])
                        P.dve(lambda e: e.tensor_reduce(out=gmx[:], in_=lg[:, :, 0:4], axis=AXX, op=ALU.max), ["lg"], ["gmx"])
                        P.dve(lambda e: e.tensor_tensor(out=gsh[:], in0=lg[:, :, 0:4], in1=b3(gmx[:], 4), op=ALU.subtract), ["lg", "gmx"], ["gsh"])
                        P.act(lambda e: e.activation(out=gex[:].rearrange("p t g -> p (t g)"), in_=gsh[:].rearrange("p t g -> p (t g)"), func=AF.Exp), ["gsh"], ["gex"])
                        P.dve(lambda e: e.tensor_reduce(out=gsum[:], in_=gex[:], axis=AXX, op=ALU.add), ["gex"], ["gsum"])
                        P.dve(lambda e: e.reciprocal(out=gprob[:], in_=gsum[:]), ["gsum"], ["gprob"])
                        P.dve(lambda e: e.tensor_scalar(out=ohg[:].rearrange("p t g -> p (t g)"), in0=gsh[:].rearrange("p t g -> p (t g)"), scalar1=0.0, scalar2=None, op0=ALU.is_ge),
                              ["gsh"], ["ohg"])
                        P.dve(lambda e: e.tensor_tensor(out=tmp48[:], in0=lg[:, :, 4:36].rearrange("p t (g e) -> p t g e", g=4),
                                                        in1=ohg[:].unsqueeze(3).broadcast_to([128, 16, 4, 8]), op=ALU.mult), ["lg", "ohg"], ["tmp48"])
                        P.dve(lambda e: e.tensor_reduce(out=isel[:], in_=tmp48[:].rearrange("p t g e -> p t e g"), axis=AXX, op=ALU.add), ["tmp48"], ["isel"])
                        P.dve(lambda e: e.tensor_reduce(out=m0[:], in_=isel[:], axis=AXX, op=ALU.max), ["isel"], ["m0r"])
                        P.dve(lambda e: e.tensor_tensor(out=eq0[:], in0=isel[:], in1=b3(m0[:], 8), op=ALU.is_equal), ["isel", "m0r"], ["eq0"])
                        P.dve(lambda e: e.scalar_tensor_tensor(out=isel2[:].rearrange("p t e -> p (t e)"), in0=eq0[:].rearrange("p t e -> p (t e)"), scalar=-1.0e30,
                                                               in1=isel[:].rearrange("p t e -> p (t e)"), op0=ALU.mult, op1=ALU.add), ["eq0", "isel"], ["isel2"])
                        P.dve(lambda e: e.tensor_reduce(out=m1[:], in_=isel2[:], axis=AXX, op=ALU.max), ["isel2"], ["m1r"])
                        P.dve(lambda e: e.tensor_tensor(out=eq1[:], in0=isel2[:], in1=b3(m1[:], 8), op=ALU.is_equal), ["isel2", "m1r"], ["eq1"])
                        P.dve(lambda e: e.tensor_tensor(out=dlt[:], in0=m1[:], in1=m0[:], op=ALU.subtract), ["m1r", "m0r"], ["dlt"])
                        P.act(lambda e: e.activation(out=w2e[:], in_=dlt[:], func=AF.Exp), ["dlt"], ["w2e"])
                        P.dve(lambda e: e.tensor_scalar(out=wsum[:], in0=w2e[:], scalar1=1.0, scalar2=None, op0=ALU.add), ["w2e"], ["wsum"])
                        P.dve(lambda e: e.reciprocal(out=wsum[:], in_=wsum[:]), ["wsum"], ["wsum"])
                        P.dve(lambda e: e.tensor_tensor(out=wt1[:], in0=wsum[:], in1=gprob[:], op=ALU.mult), ["wsum", "gprob"], ["wt1"])
                        P.dve(lambda e: e.tensor_tensor(out=wt2[:], in0=wt1[:], in1=w2e[:], op=ALU.mult), ["wt1", "w2e"], ["wt2"])
                        P.dve(lambda e: e.tensor_tensor(out=ce[:], in0=eq0[:], in1=b3(wt1[:], 8), op=ALU.mult), ["eq0", "wt1"], ["ce"])
                        P.dve(lambda e: e.tensor_tensor(out=ce2[:], in0=eq1[:], in1=b3(wt2[:], 8), op=ALU.mult), ["eq1", "wt2"], ["ce2"])
                        P.dve(lambda e: e.tensor_tensor(out=ce[:], in0=ce[:], in1=ce2[:], op=ALU.add), ["ce", "ce2"], ["ce"])
                        P.dve(lambda e: e.tensor_tensor(out=comb[:].rearrange("p t (g e) -> p t g e", g=4), in0=ce[:].unsqueeze(2).broadcast_to([128, 16, 4, 8]),
                                                        in1=ohg[:].unsqueeze(3).broadcast_to([128, 16, 4, 8]), op=ALU.mult), ["ce", "ohg"], ["comb"])
                        P.dve(lambda e: e.tensor_scalar(out=comb[:].rearrange("p a e -> p (a e)"), in0=comb[:].rearrange("p a e -> p (a e)"), scalar1=1.0 / ALPHA, scalar2=None, op0=ALU.mult),
                              ["comb"], ["comb"])
                        dump("comb", comb[:].rearrange("p a e -> p (a e)"), [128, 512], "comb")
                        SR.close()
                        P.barrier()
                        wstE = [wst[:, 0:2048], wst[:, 2048:4096]]
                        wE = [sb(S6, "wE%d" % i, [128, 12288], BF16) for i in range(2)]
                        sgE = [sb(S6, "sgE%d" % i, [128, 512], F32) for i in range(2)]
                        hT = [sb(S6, "hT%d" % i, [128, 4, 512], BF16) for i in range(2)]
                        pG = [ps(S6, "pG%d" % i, [128, 512]) for i in range(2)]
                        pU = [ps(S6, "pU%d" % i, [128, 512]) for i in range(2)]
                        pO = [ps(S6, "pO%d" % i, [128, 512]) for i in range(3)]
                        wsr = Ring([0, 1])
                        gr_ = Ring([0, 1])
                        or_ = Ring([0, 1, 2])
                        crr = [0]
                        n_exp = DEBUG.get("n_exp", 32)

                        def load_expert(ex):
                            ws = ex % 2
                            for pc in range(6):
                                si = wsr.next()
                                P.dma(wstE[si], wexp[ex, :, pc * 2048:(pc + 1) * 2048], writes=["wstE%d" % si])
                                d = wE[ws][:, pc * 2048:(pc + 1) * 2048]
                                P.pool(lambda e: e.tensor_copy(out=d, in_=wstE[si]), ["wstE%d" % si], ["wE%d" % ws])

                        def gu_phase(ex, cth):
                            ws = ex % 2
                            Wg = wE[ws][:, 0:4096].rearrange("p (k n) -> p k n", k=8)
                            Wu = wE[ws][:, 4096:8192].rearrange("p (k n) -> p k n", k=8)
                            wkey = "wE%d" % ws
                            cs_ = slice(cth * 512, (cth + 1) * 512)
                            xkeys = ["mx%d" % (cth * 4 + t_) for t_ in range(4)]
                            hs_ = cth % 2
                            for ft in range(4):
                                gi = gr_.next()
                                for k in range(8):
                                    P.pe(lambda e: e.matmul(pG[gi][:], lhsT=Wg[:, k, ft * 128:(ft + 1) * 128], rhs=x1T[:, k, cs_], start=(k == 0), stop=(k == 7)),
                                         [wkey] + xkeys, ["pG%d" % gi])
                                for k in range(8):
                                    P.pe(lambda e: e.matmul(pU[gi][:], lhsT=Wu[:, k, ft * 128:(ft + 1) * 128], rhs=x1T[:, k, cs_], start=(k == 0), stop=(k == 7)),
                                         [wkey] + xkeys, ["pU%d" % gi])
                                P.act(lambda e: e.activation(out=sgE[gi][:], in_=pG[gi][:], func=AF.Silu), ["pG%d" % gi], ["sgE%d" % gi])
                                P.dve(lambda e: e.tensor_tensor(out=hT[hs_][:, ft, :], in0=pU[gi][:], in1=sgE[gi][:], op=ALU.mult),
                                      ["pU%d" % gi, "sgE%d" % gi], ["hT%d" % hs_])

                        def d_phase(ex, cth):
                            ws = ex % 2
                            Wd = wE[ws][:, 8192:12288].rearrange("p (k n) -> p k n", k=4)
                            wkey = "wE%d" % ws
                            hs_ = cth % 2
                            for tt in range(4):
                                ti = cth * 4 + tt
                                for hf in range(2):
                                    oi = or_.next()
                                    for ft in range(4):
                                        P.pe(lambda e: e.matmul(pO[oi][:], lhsT=hT[hs_][:, ft, tt * 128:(tt + 1) * 128],
                                                                rhs=Wd[:, ft, hf * 512:(hf + 1) * 512], start=(ft == 0), stop=(ft == 3)),
                                             [wkey, "hT%d" % hs_], ["pO%d" % oi])
                                    a_ = acc[:, ti, hf * 512:(hf + 1) * 512]
                                    P.dve(lambda e: e.scalar_tensor_tensor(out=a_, in0=pO[oi][:], scalar=comb[:, ti, ex:ex + 1], in1=a_, op0=ALU.mult, op1=ALU.add),
                                          ["pO%d" % oi, "comb", "acc%d" % ti], ["acc%d" % ti])

                        yo = [sb(S6, "yo%d" % i, [128, 1024], F32) for i in range(2)]

                        def ln2_out(i):
                            yi = i % 2
                            layer_norm(acc[:, i, :], "acc%d" % i, yo[yi][:], "yo%d" % yi, yo[yi][:], "yo%d" % yi, eps=LN_EPS / (ALPHA * ALPHA))
                            P.dma(out[i * 128:(i + 1) * 128, :], yo[yi][:], reads=["yo%d" % yi], writes=["out%d" % yi], sem="outd%d" % yi)

                        units = [(ex, cth) for ex in range(n_exp) for cth in range(4)]
                        load_expert(0)
                        if n_exp > 1:
                            load_expert(1)
                        gu_phase(*units[0])
                        for u, (ex, cth) in enumerate(units):
                            if u + 1 < len(units):
                                gu_phase(*units[u + 1])
                            d_phase(ex, cth)
                            if cth == 3 and ex + 2 < n_exp:
                                load_expert(ex + 2)
                            if ex == n_exp - 1:
                                for t_ in range(4):
                                    ln2_out(cth * 4 + t_)
        fin_reads = ["out0", "out1"] + ["dbg_" + n for n in dbg_out]
        P.add("sp", lambda e: e.nop(), reads=fin_reads, sem="fin")
        if n_stage < 6:
            pass
        cnt = P.emit(G)
        nsem = len(cnt)
    return nc, dbg_out, nsem


def prep_shared(inp):
    f = lambda a: np.ascontiguousarray(np.asarray(a, dtype=np.float32))
    w_in = f(inp["w_in"])[0]
    sh = {}
    q = w_in[:, 0:512].reshape(1024, 2, 4, 64).transpose(0, 2, 1, 3).reshape(1024, 512)
    w1 = np.concatenate([q, w_in[:, 512:640], w_in[:, 640:768], w_in[:, 768:896], w_in[:, 1024:1152],
                         w_in[:, 896:1024], w_in[:, 1152:1280], w_in[:, 1280:1304]], axis=1)
    sh["w1t"] = tile_w(w1)
    sh["w4t"] = tile_w(w_in[:, 1304:3352])
    sh["wgtt"] = tile_w(w_in[:, 3352:4376])
    sh["wmgt"] = tile_w(w_in[:, 4376:6424])
    lit = {"k": (inp["cmp_k_w1"], inp["cmp_k_b1"], inp["cmp_pos_k"]), "v": (inp["cmp_v_w1"], inp["cmp_v_b1"], inp["cmp_pos_v"])}
    for kv in "kv":
        cw1 = f(lit[kv][0])[0]
        r = cw1.reshape(32, 64, 256).transpose(1, 0, 2).reshape(64, 32 * 256)
        sh["cw1" + kv] = np.ascontiguousarray(np.concatenate([r, r], axis=0))
        pos = f(lit[kv][2])[0]
        sh["cpos" + kv] = np.ascontiguousarray(np.concatenate([pos.T, pos.T], axis=0))
        sh["cb1" + kv] = np.ascontiguousarray(f(lit[kv][1])[0].reshape(2, 128).T)
    w2k = f(inp["cmp_k_w2"])[0]
    sh["cw2k"] = tile_w(np.concatenate([w2k, w2k], axis=1))
    sh["cw2v"] = tile_w(f(inp["cmp_v_w2"])[0])
    b2k = f(inp["cmp_k_b2"])[0]
    sh["cb2k"] = np.ascontiguousarray(np.concatenate([b2k, b2k])[:, None])
    sh["cb2v"] = f(inp["cmp_v_b2"])[0]
    sh["gng8"] = np.ascontiguousarray(f(inp["ret_gn_g"])[0].reshape(8, 128).T)
    sh["gnb8"] = np.ascontiguousarray(f(inp["ret_gn_b"])[0].reshape(8, 128).T)
    sh["wat"] = tile_w(f(inp["w_up_attn"])[0])
    sh["wrt"] = tile_w(f(inp["w_up_ret"])[0])
    sh["wot"] = tile_w(f(inp["w_out"])[0])
    for n in ("ln1_g", "ln1_b", "ln2_g", "ln2_b"):
        sh[n.replace("_", "")] = f(inp[n])[0]
    rg = f(inp["router_group_w"])[0]
    ri = f(inp["router_inner_w"])[0]
    sh["wrout"] = tile_w(np.concatenate([rg, ri.transpose(1, 0, 2).reshape(1024, 32)], axis=1))
    sh["brout"] = np.ascontiguousarray(np.concatenate([f(inp["router_group_b"])[0], f(inp["router_inner_b"])[0].reshape(32)]))
    wg = f(inp["expert_w_gate"])[0]
    wu = f(inp["expert_w_up"])[0]
    wd = f(inp["expert_w_down"])[0]
    we = np.empty((32, 128, 12288), np.float32)
    we[:, :, 0:4096] = wg.reshape(32, 8, 128, 512).transpose(0, 2, 1, 3).reshape(32, 128, 4096)
    we[:, :, 4096:8192] = wu.reshape(32, 8, 128, 512).transpose(0, 2, 1, 3).reshape(32, 128, 4096)
    we[:, :, 8192:12288] = wd.reshape(32, 4, 128, 1024).transpose(0, 2, 1, 3).reshape(32, 128, 4096)
    sh["wexp"] = we
    return sh


def make_in_maps(inp):
    sh = prep_shared(inp)
    x = np.asarray(inp["x"], dtype=np.float32)
    maps = []
    for core in range(8):
        b, c = core // 2, core % 2
        m = dict(sh)
        xb = x[b]
        own = xb.reshape(16, 2, 128, 1024)[:, c].reshape(TO, 1024)
        m["xT"] = np.ascontiguousarray(xb.T)
        m["xTo"] = np.ascontiguousarray(own.T)
        m["xo"] = np.ascontiguousarray(own)
        for k, v in make_consts(c).items():
            if not k.startswith("_"):
                m["c_" + k] = v
        maps.append(m)
    return maps


_PROG_CACHE = {}


def kernel(**inputs):
    if "prog" not in _PROG_CACHE:
        _PROG_CACHE["prog"] = build_program()
    nc, _, _ = _PROG_CACHE["prog"]
    maps = make_in_maps(inputs)
    res = run_bass_kernel_spmd(nc, maps, core_ids=list(range(8)))
    outp = np.empty((4, 16, 2, 128, 1024), np.float32)
    for core in range(8):
        b, c = core // 2, core % 2
        outp[b, :, c] = res.results[core]["out"].reshape(16, 128, 1024)
    return outp.reshape(4, T, 1024)
```

```python
import numpy as np
import ml_dtypes
import concourse.bass as bass
import concourse.mybir as mybir
from concourse.bass_utils import run_bass_kernel_spmd
from contextlib import ExitStack

F32 = mybir.dt.float32
BF16 = mybir.dt.bfloat16
AF = mybir.ActivationFunctionType
ALU = mybir.AluOpType
NPBF = ml_dtypes.bfloat16

T = 4096
D = 1024
TO = 2048
NEGM = -30000.0
LN_EPS = 1e-5
ALPHA = 2.0 ** 0.25
DEBUG = {}


class Op:
    __slots__ = ("eng", "fn", "reads", "writes", "dma", "sem", "deps", "needs_inc", "idx", "id", "extra")

    def __init__(self, eng, fn, reads, writes, dma, sem):
        self.eng = eng
        self.fn = fn
        self.reads = tuple(reads)
        self.writes = tuple(writes)
        self.dma = dma
        self.sem = sem
        self.deps = []
        self.needs_inc = dma
        self.idx = 0
        self.extra = ()


class _Rec:
    def __getattr__(self, name):
        return lambda *a, **k: (name, a, k)


_REC = _Rec()


class Prog:
    ENGS = ("pe", "act", "dve", "pool", "sp")

    def __init__(self, nc, same_eng_sync=True):
        self.nc = nc
        self.ops = []
        self.same_eng_sync = same_eng_sync
        self.last_by_sem = {}
        self.psum_keys = set()

    def add(self, eng, fn, reads=(), writes=(), dma=False, sem=None):
        lim = DEBUG.get("max_ops")
        self.nadd = getattr(self, "nadd", -1) + 1
        if (lim is not None and self.nadd >= lim and sem not in ("dbg", "fin")) or self.nadd in DEBUG.get("skip", ()):
            return Op(eng, None, reads, writes, dma, sem)
        if dma and sem is None:
            sem = "dma_" + str(writes[0])
        if not dma:
            sem = "eng_" + eng
        op = Op(eng, fn(_REC), reads, writes, dma, sem)
        if DEBUG.get("trace_ops"):
            print(len(self.ops), eng, op.fn[0], reads, writes)
        op.id = len(self.ops)
        self.ops.append(op)
        self.last_by_sem[sem] = op
        return op

    def pe(self, fn, reads=(), writes=()):
        return self.add("pe", fn, reads, writes)

    def act(self, fn, reads=(), writes=()):
        return self.add("act", fn, reads, writes)

    def dve(self, fn, reads=(), writes=()):
        return self.add("dve", fn, reads, writes)

    def pool(self, fn, reads=(), writes=()):
        return self.add("pool", fn, reads, writes)

    def dma(self, out, in_, reads=(), writes=(), sem=None, q="sp", **kw):
        return self.add(q, lambda e: e.dma_start(out=out, in_=in_, **kw), reads, writes, dma=True, sem=sem)

    def barrier(self):
        lasts = list(self.last_by_sem.values())
        for eng in self.ENGS:
            op = self.add(eng, lambda e: e.nop())
            op.extra = tuple(lasts)
        self.last_by_sem = {k: v for k, v in self.last_by_sem.items() if k.startswith("eng_")}

    def analyze(self):
        state = {}
        for op in self.ops:
            deps = set(op.extra)
            for k in op.reads:
                st = state.get(k)
                if st:
                    deps.update(st[0])
                    if k in self.psum_keys:
                        deps.update(r for r in st[1] if r.eng != op.eng)
            for k in op.writes:
                st = state.get(k)
                if st is None:
                    st = state[k] = [[], []]
                if st[1]:
                    deps.update(st[1])
                    deps.update(st[0])
                    st[0] = [op]
                    st[1] = []
                else:
                    same_group = op.dma and all(w.dma and w.sem == op.sem for w in st[0])
                    if same_group:
                        st[0].append(op)
                    else:
                        deps.update(st[0])
                        st[0] = [op]
            for k in op.reads:
                st = state.get(k)
                if st is None:
                    st = state[k] = [[], []]
                st[1].append(op)
            deps.discard(op)
            red = {}
            for d in deps:
                if (not d.dma) and (not op.dma) and d.eng == op.eng:
                    if op.eng == "pe" or not self.same_eng_sync:
                        continue
                cur = red.get(d.sem)
                if cur is None or d.id > cur.id:
                    red[d.sem] = d
            op.deps = list(red.values())
            for d in op.deps:
                d.needs_inc = True
        cnt = {}
        for op in self.ops:
            if op.needs_inc:
                cnt[op.sem] = cnt.get(op.sem, 0) + 1
                op.idx = cnt[op.sem]
        self.sem_names = sorted(cnt.keys())
        return cnt

    def emit(self, stack):
        nc = self.nc
        cnt = self.analyze()
        sems = {}
        for name in self.sem_names:
            sems[name] = stack.enter_context(nc.semaphore(name))
        block = stack.enter_context(nc.Block())
        per_eng = {e: [o for o in self.ops if o.eng == e] for e in self.ENGS}

        def run(eng_obj, ops):
            known = {}
            for op in ops:
                for d in op.deps:
                    val = d.idx * (16 if d.dma else 1)
                    if known.get(d.sem, 0) < val:
                        eng_obj.wait_ge(sems[d.sem], val)
                        known[d.sem] = val
                name, a, k = op.fn
                inst = getattr(eng_obj, name)(*a, **k)
                if op.needs_inc:
                    inst.then_inc(sems[op.sem], 16 if op.dma else 1)

        @block.sync
        def _(e):
            run(e, per_eng["sp"])

        @block.tensor
        def _(e):
            run(e, per_eng["pe"])

        @block.scalar
        def _(e):
            run(e, per_eng["act"])

        @block.vector
        def _(e):
            run(e, per_eng["dve"])

        @block.gpsimd
        def _(e):
            run(e, per_eng["pool"])
        return cnt


class Ring:
    def __init__(self, items):
        self.items = items
        self.i = 0

    def next(self):
        it = self.items[self.i % len(self.items)]
        self.i += 1
        return it


def tile_w(w):
    K, N = w.shape
    return np.ascontiguousarray(w.reshape(K // 128, 128, N).transpose(1, 0, 2).reshape(128, -1))


def rope_tabs(pos, d, scale):
    half = d // 2
    inv = 10000.0 ** (-np.arange(half, dtype=np.float64) * 2.0 / d)
    ang = pos.astype(np.float64)[None, :] * inv[:, None]
    cos = np.cos(ang) * scale
    sin = np.sin(ang) * scale
    reps = 128 // half
    return (np.tile(cos, (reps, 1)).astype(np.float32), np.tile(sin, (reps, 1)).astype(np.float32))


def rot_lhsT(d):
    half = d // 2
    Pm = np.zeros((128, 128), np.float32)
    for blk in range(128 // d):
        o = blk * d
        for m in range(half):
            Pm[o + m, o + m + half] = -1.0
            Pm[o + m + half, o + m] = 1.0
    return np.ascontiguousarray(Pm.T)


_CONST_CACHE = {}


def make_consts(c):
    if c in _CONST_CACHE:
        return _CONST_CACHE[c]
    cs = {}
    own_pos = np.concatenate([np.arange(128) + (2 * i + c) * 128 for i in range(16)])
    allpos = np.arange(T)
    cs["cosK"], cs["sinK"] = rope_tabs(allpos, 64, 1.0)
    cs["cosQ"], cs["sinQ"] = rope_tabs(own_pos, 64, 0.125)
    cs["cosRK"], cs["sinRK"] = rope_tabs(allpos, 128, 128.0 ** -0.5)
    cs["cosRQ"], cs["sinRQ"] = rope_tabs(own_pos, 128, 1.0)
    cend = np.arange(256) * 16 + 31
    cs["cosC"], cs["sinC"] = rope_tabs(cend, 64, 1.0)
    cs["pt64"] = rot_lhsT(64).astype(NPBF)
    cs["pt128"] = rot_lhsT(128).astype(NPBF)
    cs["identb"] = np.eye(128, dtype=np.float32).astype(NPBF)
    E = np.zeros((128, 32, 128), np.float32)
    for j in range(32):
        for k in range(128):
            E[2 * j + k // 64, j, k] = 1.0
            E[64 + 2 * j + k // 64, j, k] = 1.0
    cs["eall"] = E.reshape(128, -1).astype(NPBF)
    wm = np.zeros((128, 6, 128), np.float32)
    kk = np.arange(128)[:, None]
    tt = np.arange(128)[None, :]
    for r in range(6):
        dj = (r - 4) - c
        tk = dj * 128 + kk
        ok = (tk <= tt) & (tt - tk < 512)
        wm[:, r, :] = np.where(ok, 0.0, NEGM)
    cs["wmask"] = wm.reshape(128, -1).astype(NPBF)
    cm = np.zeros((128, 2, 16, 128), np.float32)
    for a in range(2):
        for i in range(16):
            G = 2 * i + c
            n = a * 128 + kk
            t = G * 128 + tt
            cm[:, a, i, :] = np.where(16 * n + 31 <= t, 0.0, NEGM)
    cs["cmpmask"] = cm.reshape(128, -1).astype(NPBF)
    cstart = np.arange(255) * 16
    sstart = np.arange(64) * 64
    ov = np.clip(np.minimum(cstart[None, :] + 32, sstart[:, None] + 64) - np.maximum(cstart[None, :], sstart[:, None]), 0, None) / 16.0
    ovT = np.zeros((256, 64), np.float32)
    ovT[:255] = ov.T
    cs["ovT"] = np.ascontiguousarray(ovT.reshape(2, 128, 64).transpose(1, 0, 2).reshape(128, -1)).astype(NPBF)
    tkm = np.zeros((128, 16, 64), np.float32)
    tkb = np.zeros((128, 16, 64), np.float32)
    for i in range(16):
        G = 2 * i + c
        for p in range(128):
            bt = (G * 128 + p) // 64
            for s in range(64):
                if s == 0:
                    tkb[p, i, s] = 1e9
                elif s == bt:
                    tkb[p, i, s] = 2e9
                elif s == bt - 1:
                    tkb[p, i, s] = 3e9
                elif s <= bt:
                    tkm[p, i, s] = 1.0
                else:
                    tkb[p, i, s] = -1e9 - 1e6 * s
    cs["tkm"] = tkm.reshape(128, -1)
    cs["tkb"] = tkb.reshape(128, -1)
    gam = 1.0 - 2.0 ** (-5.0 - np.arange(4, dtype=np.float64))
    lg = np.log(gam)
    m = np.arange(256)[:, None]
    cq = np.arange(128)[None, :]
    qq = 128 * c + cq
    Dc = np.zeros((128, 2, 4, 128), np.float32)
    for h in range(4):
        dd = np.where(qq >= m, np.exp(np.maximum(qq - m, 0) * lg[h]), 0.0)
        Dc[:, :, h, :] = dd.reshape(2, 128, 128).transpose(1, 0, 2)
    cs["Dc"] = Dc.reshape(128, -1)
    xi = np.zeros((128, 4, 128), np.float32)
    for h in range(4):
        xi[:, h, :] = np.exp((qq + 1.0) * lg[h])
    cs["xi"] = xi.reshape(128, -1)
    zt = np.zeros((128, 2, 4), np.float32)
    for h in range(4):
        zt[:, :, h] = np.exp((255.0 - np.arange(256)) * lg[h]).reshape(2, 128).T
    cs["zeta"] = zt.reshape(128, -1)
    cs["_decay256"] = [float(np.exp(256.0 * lg[h])) for h in range(4)]
    _CONST_CACHE[c] = cs
    return cs


CONST_SHAPES = None


def build_program(n_stage=6, debug=()):
    nc = bass.Bass("TRN2", target_bir_lowering=False)
    cs0 = make_consts(0)
    dram = {}

    def din(name, shape, dt=F32):
        dram[name] = nc.dram_tensor(name, list(shape), dt, kind="ExternalInput").ap()
        return dram[name]

    xT = din("xT", [1024, T])
    xTo = din("xTo", [1024, TO])
    xo = din("xo", [TO, 1024])
    w1t = din("w1t", [128, 8 * 1304])
    w4t = din("w4t", [128, 8 * 2048])
    wmgt = din("wmgt", [128, 8 * 2048])
    cw1 = {kv: din("cw1" + kv, [128, 32 * 256]) for kv in "kv"}
    cpos = {kv: din("cpos" + kv, [128, 32]) for kv in "kv"}
    cb1 = {kv: din("cb1" + kv, [128, 2]) for kv in "kv"}
    cw2k = din("cw2k", [128, 2 * 128])
    cw2v = din("cw2v", [128, 2 * 64])
    cb2k = din("cb2k", [128, 1])
    cb2v = din("cb2v", [64])
    gng8 = din("gng8", [128, 8])
    gnb8 = din("gnb8", [128, 8])
    wgtt = din("wgtt", [128, 8 * 1024])
    wat = din("wat", [128, 4 * 1024])
    wrt = din("wrt", [128, 8 * 1024])
    wot = din("wot", [128, 8 * 1024])
    ln1g = din("ln1g", [1024])
    ln1b = din("ln1b", [1024])
    ln2g = din("ln2g", [1024])
    ln2b = din("ln2b", [1024])
    wrout = din("wrout", [128, 8 * 36])
    brout = din("brout", [36])
    wexp = din("wexp", [32, 128, 12288])
    cdr = {}
    for k, v in cs0.items():
        if k.startswith("_"):
            continue
        cdr[k] = din("c_" + k, v.shape, BF16 if v.dtype == NPBF else F32)
    out = nc.dram_tensor("out", [TO, 1024], F32, kind="ExternalOutput").ap()
    dbg_out = {}

    decay256 = cs0["_decay256"]

    with ExitStack() as G:
        P = Prog(nc)

        def sb(stack, name, shape, dt):
            return stack.enter_context(nc.sbuf_tensor(name, list(shape), dt))

        def ps(stack, name, shape, dt=F32):
            P.psum_keys.add(name)
            ncol = 512 if dt == F32 else 1024
            full = stack.enter_context(nc.psum_tensor(name, [128, ncol], dt))
            n = 1
            for d_ in shape[1:]:
                n *= d_
            v = full[0:shape[0], 0:n]
            if len(shape) == 3:
                v = v.rearrange("p (a b) -> p a b", a=shape[1])
            return v

        def dump(name, ap, shape, key):
            if name in debug:
                t = nc.dram_tensor("dbg_" + name, list(shape), ap.dtype, kind="ExternalOutput").ap()
                dbg_out[name] = t
                P.dma(t, ap, reads=[key], writes=["dbg_" + name], sem="dbg")

        identb = sb(G, "identb", [128, 128], BF16)
        P.dma(identb[:], cdr["identb"], writes=["identb"])
        wst = sb(G, "wst", [128, 4096], F32)
        cast_rr = [0]

        def load_cast(dst_ap, src_ap, n, dst_key, shape3=None):
            o = 0
            while o < n:
                m = min(4096, n - o)
                P.dma(wst[:, 0:m], src_ap[:, o:o + m], writes=["wst"])
                d = dst_ap[:, o:o + m]
                if cast_rr[0] % 2 == 0:
                    P.act(lambda e, d=d, m=m: e.copy(out=d, in_=wst[:, 0:m]), ["wst"], [dst_key])
                else:
                    P.dve(lambda e, d=d, m=m: e.tensor_copy(out=d, in_=wst[:, 0:m]), ["wst"], [dst_key])
                cast_rr[0] += 1
                o += m

        x1T = sb(G, "x1T", [128, 8, TO], BF16)
        wst3 = wst[:].rearrange("p (k n) -> p k n", k=8)
        A_ = ExitStack()
        oattnT = sb(A_, "oattnT", [128, 4, TO], BF16)

        with ExitStack() as SN:
            QT = sb(SN, "QT", [128, 16, 4, 128], BF16)
            KE = [sb(SN, "KE%d" % i_, [128, T], BF16) for i_ in range(2)]
            P.dma(KE[0][64:128, :], cdr["eall"][64:128, :], writes=["KE0"])
            P.dma(KE[1][0:64, :], cdr["eall"][0:64, :], writes=["KE1"])
            winkT = sb(SN, "winkT", [128, T], BF16)
            slcv1 = sb(SN, "slcv1", [128, 32, 2, 65], BF16)
            winv1 = sb(SN, "winv1", [128, 32, 2, 65], BF16)
            gates = sb(SN, "gates", [128, 16, 24], F32)
            kcmpT = sb(SN, "kcmpT", [128, 256], BF16)
            vcmp1 = sb(SN, "vcmp1", [128, 2, 2, 65], BF16)
            pt64 = sb(SN, "pt64", [128, 128], BF16)
            P.dma(pt64[:], cdr["pt64"], writes=["pt64"])
            P.dve(lambda e: e.memset(slcv1[:].rearrange("p a g d -> p (a g d)"), 1.0), [], ["slcv1"])
            P.dve(lambda e: e.memset(winv1[:].rearrange("p a g d -> p (a g d)"), 1.0), [], ["winv1"])
            P.dve(lambda e: e.memset(kcmpT[:], 0.0), [], ["kcmpT"])
            P.dve(lambda e: e.memset(vcmp1[:].rearrange("p a g d -> p (a g d)"), 0.0), [], ["vcmp1"])
            P.dve(lambda e: e.memset(vcmp1[:, :, :, 64:65], 1.0), [], ["vcmp1"])

            with ExitStack() as S12:
                cmpT = {"k": sb(S12, "cmpkT", [128, T], BF16), "v": sb(S12, "cmpvT", [128, T], BF16)}
                with ExitStack() as S1:
                    Wn = sb(S1, "Wn", [128, 8, 1304], BF16)
                    load_cast(Wn[:].rearrange("p k n -> p (k n)"), w1t, 8 * 1304, "Wn")
                    xb = [sb(S1, "xb%d" % i, [128, 8, 512], BF16) for i in range(2)]
                    tabs = [sb(S1, "tab%d" % i, [128, 2, 512], F32) for i in range(2)]
                    ybf = [sb(S1, "ybf%d" % i, [128, 512], BF16) for i in range(2)]
                    t1 = [sb(S1, "t1_%d" % i, [128, 512], F32) for i in range(2)]
                    t2 = [sb(S1, "t2_%d" % i, [128, 512], F32) for i in range(2)]
                    pj = [ps(S1, "pj%d" % i, [128, 512]) for i in range(3)]
                    prot = [ps(S1, "prot%d" % i, [128, 512]) for i in range(2)]
                    pv = [ps(S1, "pv%d" % i, [128, 256]) for i in range(2)]
                    pjr = Ring(list(range(3)))
                    rr = Ring(list(range(2)))
                    pvr = Ring(list(range(2)))
                    xTv = xT.rearrange("(k p) t -> p k t", p=128)
                    xTov = xTo.rearrange("(k p) t -> p k t", p=128)

                    def load_x(src_view, c0, n, slot):
                        P.dma(wst3[:, :, 0:n], src_view[:, :, c0:c0 + n], writes=["wst"])
                        P.act(lambda e: e.copy(out=xb[slot][:, 0:4, 0:n], in_=wst3[:, 0:4, 0:n]), ["wst"], ["xb%d" % slot])
                        P.dve(lambda e: e.tensor_copy(out=xb[slot][:, 4:8, 0:n], in_=wst3[:, 4:8, 0:n]), ["wst"], ["xb%d" % slot])

                    def proj_fm(col0, slot, n=512):
                        pi = pjr.next()
                        for k in range(8):
                            P.pe(lambda e, k=k, pi=pi: e.matmul(pj[pi][:, 0:n], lhsT=Wn[:, k, col0:col0 + 128], rhs=xb[slot][:, k, 0:n],
                                                                 start=(k == 0), stop=(k == 7)), ["Wn", "xb%d" % slot], ["pj%d" % pi])
                        return pi

                    def rope_fm(pi, tslot, dst_ap, dst_key, ptm, ptkey, n=512, src=None, srckey=None):
                        r = rr.next()
                        srcap = pj[pi][:, 0:n] if src is None else src
                        sk = ("pj%d" % pi) if srckey is None else srckey
                        P.act(lambda e: e.copy(out=ybf[r][:, 0:n], in_=srcap), [sk], ["ybf%d" % r])
                        P.pe(lambda e: e.matmul(prot[r][:, 0:n], lhsT=ptm[:], rhs=ybf[r][:, 0:n], start=True, stop=True),
                             [ptkey, "ybf%d" % r], ["prot%d" % r])
                        P.dve(lambda e: e.tensor_tensor(out=t1[r][:, 0:n], in0=srcap, in1=tabs[tslot][:, 0, 0:n], op=ALU.mult),
                              [sk, "tab%d" % tslot], ["t1_%d" % r])
                        P.dve(lambda e: e.tensor_tensor(out=t2[r][:, 0:n], in0=prot[r][:, 0:n], in1=tabs[tslot][:, 1, 0:n], op=ALU.mult),
                              ["prot%d" % r, "tab%d" % tslot], ["t2_%d" % r])
                        if isinstance(dst_ap, list):
                            for (d_ap, rows, dkey) in dst_ap:
                                P.pool(lambda e: e.tensor_tensor(out=d_ap, in0=t1[r][rows, 0:n], in1=t2[r][rows, 0:n], op=ALU.add),
                                       ["t1_%d" % r, "t2_%d" % r], [dkey])
                        elif dst_key == "QT":
                            P.pool(lambda e: e.tensor_tensor(out=dst_ap, in0=t1[r][:, 0:n].rearrange("p (a t) -> p a t", a=4),
                                                             in1=t2[r][:, 0:n].rearrange("p (a t) -> p a t", a=4), op=ALU.add),
                                   ["t1_%d" % r, "t2_%d" % r], [dst_key])
                        else:
                            P.pool(lambda e: e.tensor_tensor(out=dst_ap, in0=t1[r][:, 0:n], in1=t2[r][:, 0:n], op=ALU.add),
                                   ["t1_%d" % r, "t2_%d" % r], [dst_key])

                    for ch in range(8):
                        slot = ch % 2
                        c0 = ch * 512
                        load_x(xTv, c0, 512, slot)
                        P.dma(tabs[slot][:, 0, :], cdr["cosK"][:, c0:c0 + 512], writes=["tab%d" % slot])
                        P.dma(tabs[slot][:, 1, :], cdr["sinK"][:, c0:c0 + 512], writes=["tab%d" % slot])
                        for col0, kv in ((512, "k"), (640, "v")):
                            pi = proj_fm(col0, slot)
                            P.act(lambda e, pi=pi, kv=kv: e.copy(out=cmpT[kv][:, c0:c0 + 512], in_=pj[pi][:]), ["pj%d" % pi], ["cmp" + kv + "T"])
                        pi = proj_fm(768, slot)
                        rope_fm(pi, slot, [(KE[0][0:64, c0:c0 + 512], slice(0, 64), "KE0"), (KE[1][64:128, c0:c0 + 512], slice(64, 128), "KE1")], None, pt64, "pt64")
                        pi = proj_fm(896, slot)
                        rope_fm(pi, slot, winkT[:, c0:c0 + 512], "winkT", pt64, "pt64")
                        for tt in range(4):
                            vi = pvr.next()
                            for k in range(8):
                                P.pe(lambda e, k=k, vi=vi, tt=tt: e.matmul(pv[vi][:], lhsT=xb[slot][:, k, tt * 128:(tt + 1) * 128], rhs=Wn[:, k, 1024:1280],
                                                                            start=(k == 0), stop=(k == 7)), ["Wn", "xb%d" % slot], ["pv%d" % vi])
                            tg = ch * 4 + tt
                            P.act(lambda e, vi=vi, tg=tg: e.copy(out=slcv1[:, tg, :, 0:64], in_=pv[vi][:, 0:128].rearrange("p (g d) -> p g d", g=2)),
                                  ["pv%d" % vi], ["slcv1"])
                            P.dve(lambda e, vi=vi, tg=tg: e.tensor_copy(out=winv1[:, tg, :, 0:64], in_=pv[vi][:, 128:256].rearrange("p (g d) -> p g d", g=2)),
                                  ["pv%d" % vi], ["winv1"])
                    for oc in range(4):
                        slot = oc % 2
                        c0 = oc * 512
                        load_x(xTov, c0, 512, slot)
                        P.dma(tabs[slot][:, 0, :], cdr["cosQ"][:, c0:c0 + 512], writes=["tab%d" % slot])
                        P.dma(tabs[slot][:, 1, :], cdr["sinQ"][:, c0:c0 + 512], writes=["tab%d" % slot])
                        for hh in range(4):
                            pi = proj_fm(hh * 128, slot)
                            rope_fm(pi, slot, QT[:, oc * 4:(oc + 1) * 4, hh, :], "QT", pt64, "pt64")
                        for tt in range(4):
                            vi = pvr.next()
                            for k in range(8):
                                P.pe(lambda e, k=k, vi=vi, tt=tt: e.matmul(pv[vi][:, 0:24], lhsT=xb[slot][:, k, tt * 128:(tt + 1) * 128], rhs=Wn[:, k, 1280:1304],
                                                                            start=(k == 0), stop=(k == 7)), ["Wn", "xb%d" % slot], ["pv%d" % vi])
                            tg = oc * 4 + tt
                            P.act(lambda e, vi=vi, tg=tg: e.activation(out=gates[:, tg, :], in_=pv[vi][:, 0:24], func=AF.Sigmoid), ["pv%d" % vi], ["gates"])
                    dump("QT", QT[:].rearrange("p i a t -> p (i a t)"), [128, 4 * TO], "QT")
                    dump("cmpkT", cmpT["k"][:], [128, T], "cmpkT")
                    dump("slcv1", slcv1[:].rearrange("p a g d -> p (a g d)"), [128, 32 * 130], "slcv1")
                    dump("gates", gates[:].rearrange("p a g -> p (a g)"), [128, 16 * 24], "gates")
                P.barrier()
                if n_stage >= 2:
                    with ExitStack() as S2:
                        w1b = sb(S2, "w1b", [128, 32, 256], BF16)
                        posT = sb(S2, "posT", [128, 32], F32)
                        posTb = sb(S2, "posTb", [128, 32], BF16)
                        b1 = sb(S2, "b1", [128, 2], F32)
                        bias1 = sb(S2, "bias1", [128, 2], F32)
                        w2kf = sb(S2, "w2kf", [128, 2, 128], F32)
                        w2k = sb(S2, "w2k", [128, 2, 128], BF16)
                        w2vf = sb(S2, "w2vf", [128, 2, 64], F32)
                        w2v = sb(S2, "w2v", [128, 2, 64], BF16)
                        b2k = sb(S2, "b2k", [128, 1], F32)
                        b2v = sb(S2, "b2v", [128, 64], F32)
                        tabC = sb(S2, "tabC", [128, 2, 256], F32)
                        h1 = sb(S2, "h1", [128, 2, 256], BF16)
                        xg = sb(S2, "xg", [128, 256], F32)
                        ug = sb(S2, "ug", [128, 256], F32)
                        sg_ = sb(S2, "sg_", [128, 256], F32)
                        yk = sb(S2, "yk", [128, 256], F32)
                        ykb = sb(S2, "ykb", [128, 256], BF16)
                        tk1 = sb(S2, "tk1", [128, 256], F32)
                        tk2 = sb(S2, "tk2", [128, 256], F32)
                        ph = [ps(S2, "ph%d" % i, [128, 256]) for i in range(2)]
                        pcv = ps(S2, "pcv", [128, 2])
                        pkc = ps(S2, "pkc", [128, 256])
                        prk = ps(S2, "prk", [128, 256])
                        pvc = ps(S2, "pvc", [128, 64])
                        P.dma(w2kf[:].rearrange("p a n -> p (a n)"), cw2k, writes=["w2kf"])
                        P.dve(lambda e: e.tensor_copy(out=w2k[:], in_=w2kf[:]), ["w2kf"], ["w2k"])
                        P.dma(w2vf[:].rearrange("p a n -> p (a n)"), cw2v, writes=["w2vf"])
                        P.dve(lambda e: e.tensor_copy(out=w2v[:], in_=w2vf[:]), ["w2vf"], ["w2v"])
                        P.dma(b2k[:], cb2k, writes=["b2k"])
                        P.dma(b2v[:], cb2v.partition_broadcast(128), writes=["b2v"])
                        P.dma(tabC[:, 0, :], cdr["cosC"], writes=["tabC"])
                        P.dma(tabC[:, 1, :], cdr["sinC"], writes=["tabC"])
                        for kv in "kv":
                            load_cast(w1b[:].rearrange("p l n -> p (l n)"), cw1[kv], 32 * 256, "w1b")
                            P.dma(posT[:], cpos[kv], writes=["posT"])
                            P.dve(lambda e: e.tensor_copy(out=posTb[:], in_=posT[:]), ["posT"], ["posTb"])
                            P.dma(b1[:], cb1[kv], writes=["b1"])
                            for ht in range(2):
                                for l in range(32):
                                    P.pe(lambda e, ht=ht, l=l: e.matmul(pcv[:, ht:ht + 1], lhsT=w1b[0:64, l, ht * 128:(ht + 1) * 128], rhs=posTb[0:64, l:l + 1],
                                                                         start=(l == 0), stop=(l == 31)), ["w1b", "posTb"], ["pcv"])
                            P.dve(lambda e: e.tensor_tensor(out=bias1[:], in0=pcv[:], in1=b1[:], op=ALU.add), ["pcv", "b1"], ["bias1"])
                            for g in range(2):
                                gp = slice(g * 64, (g + 1) * 64)
                                for ht in range(2):
                                    for l in range(32):
                                        P.pe(lambda e, ht=ht, l=l, gp=gp, kv=kv: e.matmul(ph[ht][:, 0:255], lhsT=w1b[gp, l, ht * 128:(ht + 1) * 128],
                                                                                        rhs=cmpT[kv][gp, l:l + 16 * 254 + 1:16],
                                                                                        start=(l == 0), stop=(l == 31)), ["w1b", "cmp" + kv + "T"], ["ph%d" % ht])
                                    P.act(lambda e, ht=ht: e.activation(out=xg[:, 0:255], in_=ph[ht][:, 0:255], func=AF.Identity, bias=bias1[:, ht:ht + 1], scale=1.0),
                                          ["ph%d" % ht, "bias1"], ["xg"])
                                    P.dve(lambda e: e.tensor_tensor(out=ug[:, 0:255], in0=xg[:, 0:255], in1=xg[:, 0:255], op=ALU.mult), ["xg"], ["ug"])
                                    P.dve(lambda e: e.tensor_scalar(out=ug[:, 0:255], in0=ug[:, 0:255], scalar1=0.044715, scalar2=1.0, op0=ALU.mult, op1=ALU.add), ["ug"], ["ug"])
                                    P.dve(lambda e: e.tensor_tensor(out=ug[:, 0:255], in0=ug[:, 0:255], in1=xg[:, 0:255], op=ALU.mult), ["ug", "xg"], ["ug"])
                                    P.act(lambda e: e.activation(out=sg_[:, 0:255], in_=ug[:, 0:255], func=AF.Sigmoid, scale=1.5957691216057308), ["ug"], ["sg_"])
                                    P.dve(lambda e, ht=ht: e.tensor_tensor(out=h1[:, ht, 0:255], in0=xg[:, 0:255], in1=sg_[:, 0:255], op=ALU.mult), ["xg", "sg_"], ["h1"])
                                if kv == "k":
                                    for ht in range(2):
                                        P.pe(lambda e, ht=ht: e.matmul(pkc[:, 0:255], lhsT=w2k[:, ht, :], rhs=h1[:, ht, 0:255], start=(ht == 0), stop=(ht == 1)),
                                             ["w2k", "h1"], ["pkc"])
                                    P.act(lambda e: e.activation(out=yk[:, 0:255], in_=pkc[:, 0:255], func=AF.Identity, bias=b2k[:, 0:1], scale=1.0), ["pkc", "b2k"], ["yk"])
                                    P.act(lambda e: e.copy(out=ykb[:, 0:255], in_=yk[:, 0:255]), ["yk"], ["ykb"])
                                    P.pe(lambda e: e.matmul(prk[:, 0:255], lhsT=pt64[:], rhs=ykb[:, 0:255], start=True, stop=True), ["pt64", "ykb"], ["prk"])
                                    P.dve(lambda e: e.tensor_tensor(out=tk1[:, 0:255], in0=yk[:, 0:255], in1=tabC[:, 0, 0:255], op=ALU.mult), ["yk", "tabC"], ["tk1"])
                                    P.dve(lambda e: e.tensor_tensor(out=tk2[:, 0:255], in0=prk[:, 0:255], in1=tabC[:, 1, 0:255], op=ALU.mult), ["prk", "tabC"], ["tk2"])
                                    P.dve(lambda e, gp=gp: e.tensor_tensor(out=kcmpT[gp, 0:255], in0=tk1[gp, 0:255], in1=tk2[gp, 0:255], op=ALU.add), ["tk1", "tk2"], ["kcmpT"])
                                else:
                                    for a in range(2):
                                        cntn = 128 if a == 0 else 127
                                        for ht in range(2):
                                            P.pe(lambda e, ht=ht, a=a, cntn=cntn: e.matmul(pvc[0:cntn, :], lhsT=h1[:, ht, a * 128:a * 128 + cntn], rhs=w2v[:, ht, :],
                                                                                            start=(ht == 0), stop=(ht == 1)), ["w2v", "h1"], ["pvc"])
                                        P.dve(lambda e, a=a, cntn=cntn, g=g: e.tensor_tensor(out=vcmp1[0:cntn, a, g, 0:64], in0=pvc[0:cntn, :], in1=b2v[0:cntn, :], op=ALU.add),
                                              ["pvc", "b2v"], ["vcmp1"])
                        dump("kcmpT", kcmpT[:], [128, 256], "kcmpT")
                        dump("vcmp1", vcmp1[:].rearrange("p a g d -> p (a g d)"), [128, 260], "vcmp1")
                    P.barrier()
            P.barrier()
            if n_stage >= 3:
                with ExitStack() as S3:
                    def bc4(ap):
                        return ap.unsqueeze(1).broadcast_to([ap.shape[0], 4, ap.shape[1]])

                    wmask = sb(S3, "wmask", [128, 6, 128], BF16)
                    cmpmask = sb(S3, "cmpmask", [128, 2, 16, 128], BF16)
                    ovT = sb(S3, "ovT", [128, 2, 64], BF16)
                    tkm = sb(S3, "tkm", [128, 16, 64], F32)
                    tkb = sb(S3, "tkb", [128, 16, 64], F32)
                    wmask4 = sb(S3, "wmask4", [128, 6, 512], BF16)
                    cm4 = [sb(S3, "cm4_%d" % i_, [128, 2, 512], BF16) for i_ in range(2)]
                    QN = [sb(S3, "QN%d" % i_, [128, 512], BF16) for i_ in range(4)]
                    P.dma(wmask[:].rearrange("p a k -> p (a k)"), cdr["wmask"], writes=["wmask"])
                    P.dma(cmpmask[:].rearrange("p a i k -> p (a i k)"), cdr["cmpmask"], writes=["cmpmask"])
                    P.dma(ovT[:].rearrange("p a k -> p (a k)"), cdr["ovT"], writes=["ovT"])
                    P.dma(tkm[:].rearrange("p a k -> p (a k)"), cdr["tkm"], writes=["tkm"])
                    P.dma(tkb[:].rearrange("p a k -> p (a k)"), cdr["tkb"], writes=["tkb"])
                    for r_ in range(6):
                        P.pool(lambda e: e.tensor_copy(out=wmask4[:, r_, :].rearrange("p (a t) -> p a t", a=4), in_=bc4(wmask[:, r_, :])), ["wmask"], ["wmask4"])
                    eT = [sb(S3, "eT%d" % i, [128, 512], BF16) for i in range(4)]
                    oacc = sb(S3, "oacc", [128, 512], F32)
                    oab = sb(S3, "oab", [128, 512], BF16)
                    rz = sb(S3, "rz", [128, 4], F32)
                    coef = sb(S3, "coef", [128, 4], F32)
                    imp = sb(S3, "imp", [128, 64], F32)
                    score = sb(S3, "score", [128, 64], F32)
                    work = sb(S3, "work", [128, 64], F32)
                    m8 = sb(S3, "m8", [128, 16], F32)
                    nmk = [sb(S3, "nmk%d" % i_, [128, 2, 64], BF16) for i_ in range(2)]
                    pST = [ps(S3, "pST%d" % i, [128, 512]) for i in range(3)]
                    pA = ps(S3, "pA", [128, 4, 65])
                    pB = ps(S3, "pB", [128, 4, 64])
                    pS = ps(S3, "pS", [128, 4, 65])
                    pW = ps(S3, "pW", [128, 4, 65])
                    pTr = ps(S3, "pTr", [128, 128], BF16)
                    str_ = Ring([0, 1, 2])
                    etr = Ring([0, 1, 2, 3])

                    def scores(kT_ap, kkey, g, i, masks, q_ap=None, qkey="QT"):
                        gp = slice(g * 64, (g + 1) * 64)
                        si = str_.next()
                        ei = etr.next()
                        nm = len(masks)
                        if q_ap is None:
                            q_ap = QT[gp, i, :, :].rearrange("p a t -> p (a t)")
                        P.pe(lambda e: e.matmul(pST[si][:], lhsT=kT_ap, rhs=q_ap, start=True, stop=(nm == 0)), [kkey, qkey], ["pST%d" % si])
                        for mi, (ml, mr, mkeys) in enumerate(masks):
                            P.pe(lambda e: e.matmul(pST[si][:], lhsT=ml, rhs=mr, start=False, stop=(mi == nm - 1)), mkeys, ["pST%d" % si])
                        P.act(lambda e: e.activation(out=eT[ei][:], in_=pST[si][:], func=AF.Exp), ["pST%d" % si], ["eT%d" % ei])
                        return ei

                    def finish_branch(pacc, pkey, i, g, br, first):
                        P.dve(lambda e: e.tensor_scalar(out=rz[:], in0=pacc[:, :, 64], scalar1=1e-30, scalar2=None, op0=ALU.max), [pkey], ["rz"])
                        P.dve(lambda e: e.reciprocal(out=rz[:], in_=rz[:]), ["rz"], ["rz"])
                        P.dve(lambda e: e.tensor_tensor(out=coef[:], in0=rz[:], in1=gates[:, i, g * 12 + br:g * 12 + 12:3], op=ALU.mult), ["rz", "gates"], ["coef"])
                        for hh in range(4):
                            o = oacc[:, g * 256 + hh * 64:g * 256 + (hh + 1) * 64]
                            if first:
                                P.dve(lambda e, hh=hh, o=o: e.tensor_scalar(out=o, in0=pacc[:, hh, 0:64], scalar1=coef[:, hh:hh + 1], scalar2=None, op0=ALU.mult),
                                      [pkey, "coef"], ["oacc"])
                            else:
                                P.dve(lambda e, hh=hh, o=o: e.scalar_tensor_tensor(out=o, in0=pacc[:, hh, 0:64], scalar=coef[:, hh:hh + 1], in1=o, op0=ALU.mult, op1=ALU.add),
                                      [pkey, "coef", "oacc"], ["oacc"])

                    tasks = []

                    def mk_cmp(i, g, a, na):
                        gp = slice(g * 64, (g + 1) * 64)

                        def sc():
                            if g == 0:
                                P.pool(lambda e: e.tensor_copy(out=cm4[i % 2][:, a, :].rearrange("p (h t) -> p h t", h=4), in_=bc4(cmpmask[:, a, i, :])),
                                       ["cmpmask"], ["cm4_%d" % (i % 2)])
                            return scores(kcmpT[gp, a * 128:(a + 1) * 128], "kcmpT", g, i,
                                          [(identb[:], cm4[i % 2][:, a, :], ["identb", "cm4_%d" % (i % 2)])])

                        def pvf(ei):
                            for hh in range(4):
                                P.pe(lambda e: e.matmul(pA[:, hh, :], lhsT=eT[ei][:, hh * 128:(hh + 1) * 128], rhs=vcmp1[:, a, g, :],
                                                        start=(a == 0 and hh == 0), stop=(a == na - 1 and hh == 3)), ["eT%d" % ei, "vcmp1"], ["pA"])
                                P.pe(lambda e: e.matmul(pB[:, hh, :], lhsT=eT[ei][:, hh * 128:(hh + 1) * 128], rhs=ovT[:, a, :],
                                                        start=(a == 0 and hh == 0), stop=(a == na - 1 and hh == 3)), ["eT%d" % ei, "ovT"], ["pB"])

                        def post():
                            finish_branch(pA, "pA", i, g, 0, True)
                            P.dve(lambda e: e.tensor_scalar(out=imp[:], in0=pB[:, 0, :], scalar1=rz[:, 0:1], scalar2=None, op0=ALU.mult), ["pB", "rz"], ["imp"])
                            for hh in range(1, 4):
                                P.dve(lambda e: e.scalar_tensor_tensor(out=imp[:], in0=pB[:, hh, :], scalar=rz[:, hh:hh + 1], in1=imp[:], op0=ALU.mult, op1=ALU.add),
                                      ["pB", "rz", "imp"], ["imp"])
                            P.dve(lambda e: e.tensor_tensor(out=score[:], in0=imp[:], in1=tkm[:, i, :], op=ALU.mult), ["imp", "tkm"], ["score"])
                            P.dve(lambda e: e.tensor_tensor(out=score[:], in0=score[:], in1=tkb[:, i, :], op=ALU.add), ["score", "tkb"], ["score"])
                            P.dve(lambda e: e.max(out=m8[:, 0:8], in_=score[:]), ["score"], ["m8"])
                            P.dve(lambda e: e.match_replace(out=work[:], in_to_replace=m8[:, 0:8], in_values=score[:], imm_value=-3.0e38), ["score", "m8"], ["work"])
                            P.dve(lambda e: e.max(out=m8[:, 8:16], in_=work[:]), ["work"], ["m8"])
                            P.dve(lambda e: e.tensor_scalar(out=nmk[g][:], in0=score[:].unsqueeze(1).broadcast_to([128, 2, 64]), scalar1=m8[:, 15:16], scalar2=NEGM,
                                                            op0=ALU.is_lt, op1=ALU.mult), ["score", "m8"], ["nmk%d" % g])
                            if ("imp%d_%d" % (i, g)) in debug:
                                dump("imp%d_%d" % (i, g), imp[:], [128, 64], "imp")
                                dump("score%d_%d" % (i, g), score[:], [128, 64], "score")
                                dump("m8%d_%d" % (i, g), m8[:], [128, 16], "m8")
                        return [None, sc, pvf, post if a == na - 1 else None]

                    def mk_win(i, g, idx, r, j, nw):
                        gp = slice(g * 64, (g + 1) * 64)

                        def sc():
                            return scores(winkT[gp, j * 128:(j + 1) * 128], "winkT", g, i,
                                          [(identb[:], wmask4[:, r, :], ["identb", "wmask4"])])

                        def pvf(ei):
                            for hh in range(4):
                                P.pe(lambda e: e.matmul(pW[:, hh, :], lhsT=eT[ei][:, hh * 128:(hh + 1) * 128], rhs=winv1[:, j, g, :],
                                                        start=(idx == 0 and hh == 0), stop=(idx == nw - 1 and hh == 3)), ["eT%d" % ei, "winv1"], ["pW"])

                        def post():
                            finish_branch(pW, "pW", i, g, 2, False)
                        return [None, sc, pvf, post if idx == nw - 1 else None]

                    def tile_end_pe(i):
                        for ct in range(4):
                            P.pe(lambda e: e.transpose(out=pTr[:], in_=oab[:, ct * 128:(ct + 1) * 128], identity=identb[:]), ["oab", "identb"], ["pTr"])
                            P.dve(lambda e: e.tensor_copy(out=oattnT[:, ct, i * 128:(i + 1) * 128], in_=pTr[:]), ["pTr"], ["oattnT"])

                    def mk_slc(i, g, j, nj):
                        gp = slice(g * 64, (g + 1) * 64)

                        qn_i = (2 * i + g) % 4
                        oh = slice((1 - g) * 64, (2 - g) * 64)

                        def pre():
                            P.pool(lambda e: e.tensor_copy(out=QN[qn_i][gp, :], in_=QT[gp, i, :, :].rearrange("p a t -> p (a t)")), ["QT"], ["QN%d" % qn_i])
                            P.pe(lambda e: e.transpose(out=pTr[:], in_=nmk[g][:].rearrange("p a s -> p (a s)"), identity=identb[:]), ["nmk%d" % g, "identb"], ["pTr"])
                            P.dve(lambda e: e.tensor_copy(out=QN[qn_i][oh, :].rearrange("p (a t) -> p a t", a=4), in_=bc4(pTr[oh, :])), ["pTr"], ["QN%d" % qn_i])
                            if g == 0 and i > 0:
                                tile_end_pe(i - 1)

                        def sc():
                            masks = []
                            if j >= 2 * i:
                                masks.append((identb[:], wmask4[:, 4 + (j - 2 * i), :], ["identb", "wmask4"]))
                            return scores(KE[g][:, j * 128:(j + 1) * 128], "KE%d" % g, g, i, masks, q_ap=QN[qn_i][:], qkey="QN%d" % qn_i)

                        def pvf(ei):
                            for hh in range(4):
                                P.pe(lambda e: e.matmul(pS[:, hh, :], lhsT=eT[ei][:, hh * 128:(hh + 1) * 128], rhs=slcv1[:, j, g, :],
                                                        start=(j == 0 and hh == 0), stop=(j == nj - 1 and hh == 3)), ["eT%d" % ei, "slcv1"], ["pS"])

                        def post():
                            finish_branch(pS, "pS", i, g, 1, False)
                            if g == 1:
                                if ("oacc%d" % i) in debug:
                                    dump("oacc%d" % i, oacc[:], [128, 512], "oacc")
                                P.pool(lambda e: e.tensor_copy(out=oab[:], in_=oacc[:]), ["oacc"], ["oab"])
                        return [pre if j == 0 else None, sc, pvf, post if j == nj - 1 else None]

                    for i in range(16):
                        for g in range(2):
                            na = 1 if i < 8 else 2
                            for a in range(na):
                                tasks.append(mk_cmp(i, g, a, na))
                            js = [(r, 2 * i - 4 + r) for r in range(6) if 2 * i - 4 + r >= 0]
                            for idx, (r, j) in enumerate(js):
                                tasks.append(mk_win(i, g, idx, r, j, len(js)))
                            nj = 2 * i + 2
                            for j in range(nj):
                                tasks.append(mk_slc(i, g, j, nj))
                    nt = len(tasks)
                    eis = [None] * nt

                    def emit_score(k):
                        if tasks[k][0] is not None:
                            tasks[k][0]()
                        eis[k] = tasks[k][1]()

                    emit_score(0)
                    emit_score(1)
                    for k in range(nt):
                        if k + 2 < nt:
                            emit_score(k + 2)
                        tasks[k][2](eis[k])
                        if tasks[k][3] is not None:
                            tasks[k][3]()
                    tile_end_pe(15)
                    dump("oattnT", oattnT[:].rearrange("p a t -> p (a t)"), [128, 4 * TO], "oattnT")
                P.barrier()
        P.barrier()

        B_ = ExitStack()
        oretT = sb(B_, "oretT", [128, 8, TO], BF16)
        if n_stage >= 4:
            with ExitStack() as S4:
                W4 = sb(S4, "W4", [128, 8, 2048], BF16)
                load_cast(W4[:].rearrange("p k n -> p (k n)"), w4t, 8 * 2048, "W4")
                pt128 = sb(S4, "pt128", [128, 128], BF16)
                P.dma(pt128[:], cdr["pt128"], writes=["pt128"])
                Dc = sb(S4, "Dc", [128, 2, 4, 128], F32)
                xi = sb(S4, "xi", [128, 4, 128], F32)
                zeta = sb(S4, "zeta", [128, 2, 4], F32)
                P.dma(Dc[:].rearrange("p a h c -> p (a h c)"), cdr["Dc"], writes=["Dc"])
                P.dma(xi[:].rearrange("p h c -> p (h c)"), cdr["xi"], writes=["xi"])
                P.dma(zeta[:].rearrange("p a h -> p (a h)"), cdr["zeta"], writes=["zeta"])
                xst = wst3
                xb = sb(S4, "xb4", [128, 8, 512], BF16)
                xob = sb(S4, "xob4", [128, 8, 256], BF16)
                tabs = sb(S4, "tab4", [128, 2, 512], F32)
                tabq = sb(S4, "tabq4", [128, 2, 256], F32)
                ybf2 = [sb(S4, "ybf4_%d" % i_, [128, 512], BF16) for i_ in range(2)]
                t12 = [sb(S4, "t1_4_%d" % i_, [128, 512], F32) for i_ in range(2)]
                t22 = [sb(S4, "t2_4_%d" % i_, [128, 512], F32) for i_ in range(2)]
                rr4 = Ring([0, 1])
                kT = sb(S4, "kT4", [128, 4, 512], BF16)
                qT = sb(S4, "qT4", [128, 4, 256], BF16)
                qxT = sb(S4, "qxT4", [128, 4, 256], BF16)
                vtok = sb(S4, "vtok", [128, 4, 1024], BF16)
                kz = sb(S4, "kz", [128, 4, 4, 128], BF16)
                R = sb(S4, "R", [128, 4, 256], F32)
                Rb = sb(S4, "Rb", [128, 4, 256], BF16)
                sc = [sb(S4, "sc%d" % i_, [128, 2, 128], BF16) for i_ in range(2)]
                epsT = sb(S4, "epsT", [128, 1], F32)
                P.dve(lambda e: e.memset(epsT[:], LN_EPS), [], ["epsT"])
                pending4 = []
                st6 = sb(S4, "st6", [128, 6], F32)
                mv = sb(S4, "mv", [128, 2], F32)
                rstd = sb(S4, "rstd", [128, 1], F32)
                oretb = [sb(S4, "oretb%d" % i_, [128, 1024], BF16) for i_ in range(2)]
                pj = [ps(S4, "pj4_%d" % i, [128, 512]) for i in range(3)]
                psc = [ps(S4, "psc%d" % i_, [128, 2, 128]) for i_ in range(2)]
                po = [ps(S4, "po%d" % i_, [128, 256]) for i_ in range(2)]
                pTrw = ps(S4, "pTr4", [128, 1024], BF16)
                pTr = pTrw[:, 0:128]
                pjr = Ring([0, 1, 2])
                P.dve(lambda e: e.memset(R[:].rearrange("p h e -> p (h e)"), 0.0), [], ["R%d" % h_ for h_ in range(4)])
                P.dve(lambda e: e.memset(Rb[:].rearrange("p h e -> p (h e)"), 0.0), [], ["Rb%d" % h_ for h_ in range(4)])
                xTv = xT.rearrange("(k p) t -> p k t", p=128)
                xTov = xTo.rearrange("(k p) t -> p k t", p=128)

                def rope4(pi, n, tab, tabkey, dst_ap, dst_key):
                    ri = pjr.next()
                    prot = pj[ri]
                    rb = rr4.next()
                    ybf, t1, t2 = ybf2[rb], t12[rb], t22[rb]
                    P.act(lambda e: e.copy(out=ybf[:, 0:n], in_=pj[pi][:, 0:n]), ["pj4_%d" % pi], ["ybf4_%d" % rb])
                    P.pe(lambda e: e.matmul(prot[:, 0:n], lhsT=pt128[:], rhs=ybf[:, 0:n], start=True, stop=True), ["pt128", "ybf4_%d" % rb], ["pj4_%d" % ri])
                    P.dve(lambda e: e.tensor_tensor(out=t1[:, 0:n], in0=pj[pi][:, 0:n], in1=tab[:, 0, 0:n], op=ALU.mult), ["pj4_%d" % pi, tabkey], ["t1_4_%d" % rb])
                    P.dve(lambda e: e.tensor_tensor(out=t2[:, 0:n], in0=prot[:, 0:n], in1=tab[:, 1, 0:n], op=ALU.mult), ["pj4_%d" % ri, tabkey], ["t2_4_%d" % rb])
                    P.pool(lambda e: e.tensor_tensor(out=dst_ap, in0=t1[:, 0:n], in1=t2[:, 0:n], op=ALU.add), ["t1_4_%d" % rb, "t2_4_%d" % rb], [dst_key])

                for gch in range(8):
                    c0 = gch * 512
                    o0 = gch * 256
                    P.dma(xst[:], xTv[:, :, c0:c0 + 512], writes=["wst"])
                    P.act(lambda e: e.copy(out=xb[:, 0:4, :], in_=xst[:, 0:4, :]), ["wst"], ["xb4"])
                    P.dve(lambda e: e.tensor_copy(out=xb[:, 4:8, :], in_=xst[:, 4:8, :]), ["wst"], ["xb4"])
                    P.dma(xst[:, :, 0:256], xTov[:, :, o0:o0 + 256], writes=["wst"])
                    P.act(lambda e: e.copy(out=xob[:, 0:4, :], in_=xst[:, 0:4, 0:256]), ["wst"], ["xob4"])
                    P.dve(lambda e: e.tensor_copy(out=xob[:, 4:8, :], in_=xst[:, 4:8, 0:256]), ["wst"], ["xob4"])
                    P.dma(tabs[:, 0, :], cdr["cosRK"][:, c0:c0 + 512], writes=["tab4"])
                    P.dma(tabs[:, 1, :], cdr["sinRK"][:, c0:c0 + 512], writes=["tab4"])
                    P.dma(tabq[:, 0, :], cdr["cosRQ"][:, o0:o0 + 256], writes=["tabq4"])
                    P.dma(tabq[:, 1, :], cdr["sinRQ"][:, o0:o0 + 256], writes=["tabq4"])
                    for h in range(4):
                        pi = pjr.next()
                        for k in range(8):
                            P.pe(lambda e, k=k, pi=pi, h=h: e.matmul(pj[pi][:], lhsT=W4[:, k, 512 + h * 128:512 + (h + 1) * 128], rhs=xb[:, k, :],
                                                                      start=(k == 0), stop=(k == 7)), ["W4", "xb4"], ["pj4_%d" % pi])
                        rope4(pi, 512, tabs, "tab4", kT[:, h, :], "kT4")
                    for h in range(4):
                        pi = pjr.next()
                        for k in range(8):
                            P.pe(lambda e, k=k, pi=pi, h=h: e.matmul(pj[pi][:, 0:256], lhsT=W4[:, k, h * 128:(h + 1) * 128], rhs=xob[:, k, :],
                                                                      start=(k == 0), stop=(k == 7)), ["W4", "xob4"], ["pj4_%d" % pi])
                        rope4(pi, 256, tabq, "tabq4", qT[:, h, :], "qT4")
                    for pp in range(2):
                        P.dve(lambda e, pp=pp: e.tensor_tensor(out=qxT[:, :, pp * 128:(pp + 1) * 128], in0=qT[:, :, pp * 128:(pp + 1) * 128], in1=xi[:], op=ALU.mult),
                              ["qT4", "xi"], ["qxT4"])
                    for tt in range(4):
                        for hf in range(2):
                            pi = pjr.next()
                            for k in range(8):
                                P.pe(lambda e, k=k, pi=pi, tt=tt, hf=hf: e.matmul(pj[pi][:], lhsT=xb[:, k, tt * 128:(tt + 1) * 128],
                                                                                   rhs=W4[:, k, 1024 + hf * 512:1024 + (hf + 1) * 512],
                                                                                   start=(k == 0), stop=(k == 7)), ["W4", "xb4"], ["pj4_%d" % pi])
                            P.act(lambda e, pi=pi, tt=tt, hf=hf: e.copy(out=vtok[:, tt, hf * 512:(hf + 1) * 512], in_=pj[pi][:]), ["pj4_%d" % pi], ["vtok"])
                    for tt in range(4):
                        for h in range(4):
                            P.pe(lambda e: e.transpose(out=pTrw[:, h * 128:(h + 1) * 128], in_=kT[:, h, tt * 128:(tt + 1) * 128], identity=identb[:]), ["kT4", "identb"], ["pTr4"])
                        P.dve(lambda e: e.tensor_tensor(out=kz[:, tt, :, :], in0=pTrw[:, 0:512].rearrange("p (h d) -> p h d", h=4),
                                                        in1=zeta[:, tt % 2, :].unsqueeze(2).broadcast_to([128, 4, 128]), op=ALU.mult), ["pTr4", "zeta"], ["kz"])
                    units = [(pp, h) for pp in range(2) for h in range(4)]

                    def phaseA(pp, h, ub):
                        qs = slice(pp * 128, (pp + 1) * 128)
                        for mt in range(2):
                            tt = pp * 2 + mt
                            P.pe(lambda e: e.matmul(psc[ub][:, mt, :], lhsT=kT[:, h, tt * 128:(tt + 1) * 128], rhs=qT[:, h, qs], start=True, stop=True),
                                 ["kT4", "qT4"], ["psc%d" % ub])
                        P.dve(lambda e: e.tensor_tensor(out=sc[ub][:], in0=psc[ub][:], in1=Dc[:, :, h, :], op=ALU.mult), ["psc%d" % ub, "Dc"], ["sc%d" % ub])

                    def phaseBC(pp, h, ub):
                        i = gch * 2 + pp
                        qs = slice(pp * 128, (pp + 1) * 128)
                        hs = slice(h * 256, (h + 1) * 256)
                        ob = oretb[pp]
                        for mt in range(2):
                            tt = pp * 2 + mt
                            P.pe(lambda e: e.matmul(po[ub][:], lhsT=sc[ub][:, mt, :], rhs=vtok[:, tt, hs], start=(mt == 0), stop=False), ["sc%d" % ub, "vtok"], ["po%d" % ub])
                        P.pe(lambda e: e.matmul(po[ub][:], lhsT=qxT[:, h, qs], rhs=Rb[:, h, :], start=False, stop=True), ["qxT4", "Rb%d" % h], ["po%d" % ub])
                        ri = pjr.next()
                        for mt in range(2):
                            tt = pp * 2 + mt
                            P.pe(lambda e: e.matmul(pj[ri][:, 0:256], lhsT=kz[:, tt, h, :], rhs=vtok[:, tt, hs], start=(mt == 0), stop=(mt == 1)),
                                 ["kz", "vtok"], ["pj4_%d" % ri])
                        P.dve(lambda e: e.bn_stats(out=st6[:], in_=po[ub][:]), ["po%d" % ub], ["st6"])
                        P.dve(lambda e: e.bn_aggr(out=mv[:], in_=st6[:]), ["st6"], ["mv"])
                        P.act(lambda e: e.activation(out=rstd[:], in_=mv[:, 1:2], func=AF.Sqrt, bias=epsT[:, 0:1], scale=1.0), ["mv", "epsT"], ["rstd"])
                        P.dve(lambda e: e.reciprocal(out=rstd[:], in_=rstd[:]), ["rstd"], ["rstd"])
                        P.dve(lambda e: e.tensor_scalar(out=ob[:, hs], in0=po[ub][:], scalar1=mv[:, 0:1], scalar2=rstd[:, 0:1], op0=ALU.subtract, op1=ALU.mult),
                              ["po%d" % ub, "mv", "rstd"], ["oretb%d" % pp])
                        P.dve(lambda e: e.scalar_tensor_tensor(out=R[:, h, :], in0=R[:, h, :], scalar=decay256[h], in1=pj[ri][:, 0:256], op0=ALU.mult, op1=ALU.add),
                              ["R%d" % h, "pj4_%d" % ri], ["R%d" % h])
                        P.act(lambda e: e.copy(out=Rb[:, h, :], in_=R[:, h, :]), ["R%d" % h], ["Rb%d" % h])

                    def pair_end(pp, i):
                        ob = oretb[pp]
                        for et in range(8):
                            P.pe(lambda e: e.transpose(out=pTrw[:, et * 128:(et + 1) * 128], in_=ob[:, et * 128:(et + 1) * 128], identity=identb[:]), ["oretb%d" % pp, "identb"], ["pTr4"])
                        P.act(lambda e: e.copy(out=oretT[:, :, i * 128:(i + 1) * 128], in_=pTrw[:].rearrange("p (a t) -> p a t", a=8)), ["pTr4"], ["oretT"])

                    phaseA(units[0][0], units[0][1], 0)
                    for u, (pp, h) in enumerate(units):
                        if u + 1 < len(units):
                            phaseA(units[u + 1][0], units[u + 1][1], (u + 1) % 2)
                        phaseBC(pp, h, u % 2)
                        if pending4:
                            pending4.pop(0)()
                        if h == 3:
                            pending4.append(lambda pp=pp, i=gch * 2 + pp: pair_end(pp, i))
                while pending4:
                    pending4.pop(0)()
                dump("oretT", oretT[:].rearrange("p a t -> p (a t)"), [128, 8 * TO], "oretT")
            P.barrier()

        if n_stage >= 5:
            with ExitStack() as S5a:
                Wmg = sb(S5a, "Wmg", [128, 8, 2048], BF16)
                Wa = sb(S5a, "Wa", [128, 4, 1024], BF16)
                Wr = sb(S5a, "Wr", [128, 8, 1024], BF16)
                Wgt = sb(S5a, "Wgt", [128, 8, 1024], BF16)
                load_cast(Wmg[:].rearrange("p k n -> p (k n)"), wmgt, 8 * 2048, "Wmg")
                load_cast(Wa[:].rearrange("p k n -> p (k n)"), wat, 4 * 1024, "Wa")
                load_cast(Wr[:].rearrange("p k n -> p (k n)"), wrt, 8 * 1024, "Wr")
                load_cast(Wgt[:].rearrange("p k n -> p (k n)"), wgtt, 8 * 1024, "Wgt")
                gg8 = sb(S5a, "gg8", [128, 8], F32)
                gb8 = sb(S5a, "gb8", [128, 8], F32)
                P.dma(gg8[:], gng8, writes=["gg8"])
                P.dma(gb8[:], gnb8, writes=["gb8"])
                xb = sb(S5a, "xb5", [128, 8, 512], BF16)
                og = sb(S5a, "og", [128, 8, 512], BF16)
                sgt = [sb(S5a, "sgt%d" % i, [128, 512], F32) for i in range(2)]
                yn = [sb(S5a, "yn%d" % i, [128, 512], F32) for i in range(2)]
                ga2 = [sb(S5a, "ga%d" % i, [128, 512], F32) for i in range(2)]
                gr2 = [sb(S5a, "gr%d" % i, [128, 512], F32) for i in range(2)]
                ma2 = [sb(S5a, "ma%d" % i, [128, 512], F32) for i in range(2)]
                bk = [ps(S5a, "bk%d" % i, [128, 512]) for i in range(8)]
                pgt = [bk[4], bk[5]]
                xTov = xTo.rearrange("(k p) t -> p k t", p=128)
                for oc in range(4):
                    c0 = oc * 512
                    cs_ = slice(c0, c0 + 512)
                    P.dma(wst3[:], xTov[:, :, cs_], writes=["wst"])
                    P.act(lambda e: e.copy(out=xb[:, 0:4, :], in_=wst3[:, 0:4, :]), ["wst"], ["xb5"])
                    P.dve(lambda e: e.tensor_copy(out=xb[:, 4:8, :], in_=wst3[:, 4:8, :]), ["wst"], ["xb5"])
                    for et in range(8):
                        b_ = et % 2
                        for k in range(8):
                            P.pe(lambda e: e.matmul(pgt[b_][:], lhsT=Wgt[:, k, et * 128:(et + 1) * 128], rhs=xb[:, k, :], start=(k == 0), stop=(k == 7)),
                                 ["Wgt", "xb5"], ["bk%d" % (4 + b_)])
                        P.act(lambda e: e.activation(out=sgt[b_][:], in_=pgt[b_][:], func=AF.Silu), ["bk%d" % (4 + b_)], ["sgt%d" % b_])
                        P.act(lambda e: e.activation(out=yn[b_][:], in_=oretT[:, et, cs_], func=AF.Identity, scale=gg8[:, et:et + 1], bias=gb8[:, et:et + 1]),
                              ["oretT", "gg8", "gb8"], ["yn%d" % b_])
                        P.dve(lambda e: e.tensor_tensor(out=og[:, et, :], in0=yn[b_][:], in1=sgt[b_][:], op=ALU.mult), ["yn%d" % b_, "sgt%d" % b_], ["og"])
                    for ct in range(8):
                        cb = (ct % 2) * 4
                        cp = ct % 2
                        pg0, pg1, pu0, pu1 = bk[cb], bk[cb + 1], bk[cb + 2], bk[cb + 3]
                        kg0, kg1, ku0, ku1 = ["bk%d" % (cb + q_) for q_ in range(4)]
                        ga, gr, ma = ga2[cp], gr2[cp], ma2[cp]
                        for k in range(8):
                            P.pe(lambda e: e.matmul(pg0[:], lhsT=Wmg[:, k, ct * 128:(ct + 1) * 128], rhs=xb[:, k, :], start=(k == 0), stop=(k == 7)),
                                 ["Wmg", "xb5"], [kg0])
                        for k in range(8):
                            P.pe(lambda e: e.matmul(pg1[:], lhsT=Wmg[:, k, 1024 + ct * 128:1024 + (ct + 1) * 128], rhs=xb[:, k, :], start=(k == 0), stop=(k == 7)),
                                 ["Wmg", "xb5"], [kg1])
                        for k in range(4):
                            P.pe(lambda e: e.matmul(pu0[:], lhsT=Wa[:, k, ct * 128:(ct + 1) * 128], rhs=oattnT[:, k, cs_], start=(k == 0), stop=(k == 3)),
                                 ["Wa", "oattnT"], [ku0])
                        for k in range(8):
                            P.pe(lambda e: e.matmul(pu1[:], lhsT=Wr[:, k, ct * 128:(ct + 1) * 128], rhs=og[:, k, :], start=(k == 0), stop=(k == 7)),
                                 ["Wr", "og"], [ku1])
                        P.act(lambda e: e.activation(out=ga[:], in_=pg0[:], func=AF.Sigmoid), [kg0], ["ga%d" % cp])
                        P.act(lambda e: e.activation(out=gr[:], in_=pg1[:], func=AF.Sigmoid), [kg1], ["gr%d" % cp])
                        P.dve(lambda e: e.tensor_tensor(out=ma[:], in0=pu0[:], in1=ga[:], op=ALU.mult), [ku0, "ga%d" % cp], ["ma%d" % cp])
                        P.dve(lambda e: e.tensor_tensor(out=gr[:], in0=pu1[:], in1=gr[:], op=ALU.mult), [ku1, "gr%d" % cp], ["gr%d" % cp])
                        P.pool(lambda e: e.tensor_tensor(out=x1T[:, ct, cs_], in0=ma[:], in1=gr[:], op=ALU.add), ["ma%d" % cp, "gr%d" % cp], ["mx%d" % (oc * 4 + t_) for t_ in range(4)])
                dump("mergedT", x1T[:].rearrange("p a t -> p (a t)"), [128, 8 * TO], "mx0")
            P.barrier()
        B_.close()
        A_.close()
        if n_stage >= 5:
            with ExitStack() as S56:
                acc = sb(S56, "acc", [128, 16, 1024], F32)
                lng = sb(S56, "lng", [128, 1024], F32)
                lnb = sb(S56, "lnb", [128, 1024], F32)
                st12 = sb(S56, "st12", [128, 2, 6], F32)
                mv = sb(S56, "mv5", [128, 2], F32)
                rstd = sb(S56, "rstd5", [128, 1], F32)

                def layer_norm(src_ap, src_key, dst_ap, dst_key, tmp_ap, tmp_key, eps=LN_EPS):
                    for hf in range(2):
                        P.dve(lambda e: e.bn_stats(out=st12[:, hf, :], in_=src_ap[:, hf * 512:(hf + 1) * 512]), [src_key], ["st12"])
                    P.dve(lambda e: e.bn_aggr(out=mv[:], in_=st12[:].rearrange("p a s -> p (a s)")), ["st12"], ["mv5"])
                    P.dve(lambda e: e.tensor_scalar(out=rstd[:], in0=mv[:, 1:2], scalar1=eps, scalar2=None, op0=ALU.add), ["mv5"], ["rstd5"])
                    P.act(lambda e: e.activation(out=rstd[:], in_=rstd[:], func=AF.Sqrt), ["rstd5"], ["rstd5"])
                    P.dve(lambda e: e.reciprocal(out=rstd[:], in_=rstd[:]), ["rstd5"], ["rstd5"])
                    P.dve(lambda e: e.scalar_tensor_tensor(out=tmp_ap, in0=src_ap, scalar=mv[:, 0:1], in1=lng[:], op0=ALU.subtract, op1=ALU.mult),
                          [src_key, "mv5", "lng"], [tmp_key])
                    P.dve(lambda e: e.scalar_tensor_tensor(out=dst_ap, in0=tmp_ap, scalar=rstd[:, 0:1], in1=lnb[:], op0=ALU.mult, op1=ALU.add),
                          [tmp_key, "rstd5", "lnb"], [dst_key])

                with ExitStack() as S5b:
                    Wo = sb(S5b, "Wo", [128, 8, 1024], BF16)
                    load_cast(Wo[:].rearrange("p k n -> p (k n)"), wot, 8 * 1024, "Wo")
                    P.dma(lng[:], ln1g.partition_broadcast(128), writes=["lng"])
                    P.dma(lnb[:], ln1b.partition_broadcast(128), writes=["lnb"])
                    xres2 = [sb(S5b, "xres%d" % i_, [128, 1024], F32) for i_ in range(2)]
                    yt2 = [sb(S5b, "yt%d" % i_, [128, 1024], F32) for i_ in range(2)]
                    x1b2 = [sb(S5b, "x1b%d" % i_, [128, 1024], BF16) for i_ in range(2)]
                    pm2 = [[ps(S5b, "pm%d_%d" % (q_, i_), [128, 512]) for i_ in range(2)] for q_ in range(2)]
                    pTr = ps(S5b, "pTr5", [128, 1024], BF16)
                    pend5 = []
                    for i in range(16):
                        q_ = i % 2
                        xres, yt, x1b, pm = xres2[q_], yt2[q_], x1b2[q_], pm2[q_]
                        ts_ = slice(i * 128, (i + 1) * 128)
                        if len(pend5) >= 2:
                            pend5.pop(0)()
                        P.dma(xres[:], xo[ts_, :], writes=["xres%d" % q_])
                        for hf in range(2):
                            for k in range(8):
                                P.pe(lambda e: e.matmul(pm[hf][:], lhsT=x1T[:, k, ts_], rhs=Wo[:, k, hf * 512:(hf + 1) * 512], start=(k == 0), stop=(k == 7)),
                                     ["mx%d" % i, "Wo"], ["pm%d_%d" % (q_, hf)])
                            P.dve(lambda e: e.scalar_tensor_tensor(out=yt[:, hf * 512:(hf + 1) * 512], in0=xres[:, hf * 512:(hf + 1) * 512], scalar=ALPHA,
                                                                   in1=pm[hf][:], op0=ALU.mult, op1=ALU.add), ["xres%d" % q_, "pm%d_%d" % (q_, hf)], ["yt%d" % q_])
                        layer_norm(yt[:], "yt%d" % q_, acc[:, i, :], "acc%d" % i, yt[:], "yt%d" % q_)
                        if ("x1_%d" % i) in debug:
                            dump("x1_%d" % i, acc[:, i, :], [128, 1024], "acc%d" % i)
                        P.pool(lambda e: e.tensor_copy(out=x1b[:], in_=acc[:, i, :]), ["acc%d" % i], ["x1b%d" % q_])

                        def tr5(i=i, q_=q_, x1b=x1b, ts_=ts_):
                            for dt_ in range(8):
                                P.pe(lambda e: e.transpose(out=pTr[:, dt_ * 128:(dt_ + 1) * 128], in_=x1b[:, dt_ * 128:(dt_ + 1) * 128], identity=identb[:]), ["x1b%d" % q_, "identb"], ["pTr5"])
                            P.act(lambda e: e.copy(out=x1T[:, :, ts_], in_=pTr[:].rearrange("p (a t) -> p a t", a=8)), ["pTr5"], ["mx%d" % i])
                        pend5.append(tr5)
                    while pend5:
                        pend5.pop(0)()
                P.barrier()
                if n_stage >= 6:
                    with ExitStack() as S6:
                        P.dma(lng[:], ln2g.partition_broadcast(128), writes=["lng"])
                        P.dma(lnb[:], ln2b.partition_broadcast(128), writes=["lnb"])
                        comb = sb(S6, "comb", [128, 16, 32], F32)
                        wrf = sb(S6, "wrf", [128, 8, 36], F32)
                        wrb = sb(S6, "wrb", [128, 8, 36], BF16)
                        brb = sb(S6, "brb", [128, 36], F32)
                        P.dma(wrf[:].rearrange("p k n -> p (k n)"), wrout, writes=["wrf"])
                        P.dve(lambda e: e.tensor_copy(out=wrb[:], in_=wrf[:]), ["wrf"], ["wrb"])
                        P.dma(brb[:], brout.partition_broadcast(128), writes=["brb"])
                        lg = sb(S6, "lg", [128, 16, 36], F32)
                        gmx = sb(S6, "gmx", [128, 16], F32)
                        gsh = sb(S6, "gsh", [128, 16, 4], F32)
                        gex = sb(S6, "gex", [128, 16, 4], F32)
                        gsum = sb(S6, "gsum", [128, 16], F32)
                        gprob = sb(S6, "gprob", [128, 16], F32)
                        ohg = sb(S6, "ohg", [128, 16, 4], F32)
                        tmp48 = sb(S6, "tmp48", [128, 16, 4, 8], F32)
                        isel = sb(S6, "isel", [128, 16, 8], F32)
                        isel2 = sb(S6, "isel2", [128, 16, 8], F32)
                        eq0 = sb(S6, "eq0", [128, 16, 8], F32)
                        eq1 = sb(S6, "eq1", [128, 16, 8], F32)
                        m0 = sb(S6, "m0r", [128, 16], F32)
                        m1 = sb(S6, "m1r", [128, 16], F32)
                        dlt = sb(S6, "dlt", [128, 16], F32)
                        w2e = sb(S6, "w2e", [128, 16], F32)
                        wsum = sb(S6, "wsum", [128, 16], F32)
                        wt1 = sb(S6, "wt1", [128, 16], F32)
                        wt2 = sb(S6, "wt2", [128, 16], F32)
                        ce = sb(S6, "ce", [128, 16, 8], F32)
                        ce2 = sb(S6, "ce2", [128, 16, 8], F32)
                        SR = ExitStack()
                        plg = [ps(SR, "plg%d" % i_, [128, 8, 36]) for i_ in range(2)]
                        AXX = mybir.AxisListType.X

                        def b3(ap, n):
                            return ap.unsqueeze(2).broadcast_to([128, 16, n])
                        for i in range(16):
                            ts_ = slice(i * 128, (i + 1) * 128)
                            for k in range(8):
                                P.pe(lambda e: e.matmul(plg[i // 8][:, i % 8, :], lhsT=x1T[:, k, ts_], rhs=wrb[:, k, :], start=(k == 0), stop=(k == 7)),
                                     ["mx%d" % i, "wrb"], ["plg%d" % (i // 8)])
                        for hf in range(2):
                            P.dve(lambda e: e.tensor_tensor(out=lg[:, hf * 8:(hf + 1) * 8, :], in0=plg[hf][:], in1=brb[:].unsqueeze(1).broadcast_to([128, 8, 36]), op=ALU.add),
                                  ["plg%d" % hf, "brb"], ["lg"])
                        P.dve(lambda e: e.tensor_reduce(out=gmx[:], in_=lg[:, :, 0:4], axis=AXX, op=ALU.max), ["lg"], ["gmx"])
                        P.dve(lambda e: e.tensor_tensor(out=gsh[:], in0=lg[:, :, 0:4], in1=b3(gmx[:], 4), op=ALU.subtract), ["lg", "gmx"], ["gsh"])
                        P.act(lambda e: e.activation(out=gex[:].rearrange("p t g -> p (t g)"), in_=gsh[:].rearrange("p t g -> p (t g)"), func=AF.Exp), ["gsh"], ["gex"])
                        P.dve(lambda e: e.tensor_reduce(out=gsum[:], in_=gex[:], axis=AXX, op=ALU.add), ["gex"], ["gsum"])
                        P.dve(lambda e: e.reciprocal(out=gprob[:], in_=gsum[:]), ["gsum"], ["gprob"])
                        P.dve(lambda e: e.tensor_scalar(out=ohg[:].rearrange("p t g -> p (t g)"), in0=gsh[:].rearrange("p t g -> p (t g)"), scalar1=0.0, scalar2=None, op0=ALU.is_ge),
                              ["gsh"], ["ohg"])
                        P.dve(lambda e: e.tensor_tensor(out=tmp48[:], in0=lg[:, :, 4:36].rearrange("p t (g e) -> p t g e", g=4),
                                                        in1=ohg[:].unsqueeze(3).broadcast_to([128, 16, 4, 8]), op=ALU.mult), ["lg", "ohg"], ["tmp48"])
                        P.dve(lambda e: e.tensor_reduce(out=isel[:], in_=tmp48[:].rearrange("p t g e -> p t e g"), axis=AXX, op=ALU.add), ["tmp48"], ["isel"])
                        P.dve(lambda e: e.tensor_reduce(out=m0[:], in_=isel[:], axis=AXX, op=ALU.max), ["isel"], ["m0r"])
                        P.dve(lambda e: e.tensor_tensor(out=eq0[:], in0=isel[:], in1=b3(m0[:], 8), op=ALU.is_equal), ["isel", "m0r"], ["eq0"])
                        P.dve(lambda e: e.scalar_tensor_tensor(out=isel2[:].rearrange("p t e -> p (t e)"), in0=eq0[:].rearrange("p t e -> p (t e)"), scalar=-1.0e30,
                                                               in1=isel[:].rearrange("p t e -> p (t e)"), op0=ALU.mult, op1=ALU.add), ["eq0", "isel"], ["isel2"])
                        P.dve(lambda e: e.tensor_reduce(out=m1[:], in_=isel2[:], axis=AXX, op=ALU.max), ["isel2"], ["m1r"])
                        P.dve(lambda e: e.tensor_tensor(out=eq1[:], in0=isel2[:], in1=b3(m1[:], 8), op=ALU.is_equal), ["isel2", "m1r"], ["eq1"])
                        P.dve(lambda e: e.tensor_tensor(out=dlt[:], in0=m1[:], in1=m0[:], op=ALU.subtract), ["m1r", "m0r"], ["dlt"])
                        P.act(lambda e: e.activation(out=w2e[:], in_=dlt[:], func=AF.Exp), ["dlt"], ["w2e"])
                        P.dve(lambda e: e.tensor_scalar(out=wsum[:], in0=w2e[:], scalar1=1.0, scalar2=None, op0=ALU.add), ["w2e"], ["wsum"])
                        P.dve(lambda e: e.reciprocal(out=wsum[:], in_=wsum[:]), ["wsum"], ["wsum"])
                        P.dve(lambda e: e.tensor_tensor(out=wt1[:], in0=wsum[:], in1=gprob[:], op=ALU.mult), ["wsum", "gprob"], ["wt1"])
                        P.dve(lambda e: e.tensor_tensor(out=wt2[:], in0=wt1[:], in1=w2e[:], op=ALU.mult), ["wt1", "w2e"], ["wt2"])
                        P.dve(lambda e: e.tensor_tensor(out=ce[:], in0=eq0[:], in1=b3(wt1[:], 8), op=ALU.mult), ["eq0", "wt1"], ["ce"])
                        P.dve(lambda e: e.tensor_tensor(out=ce2[:], in0=eq1[:], in1=b3(wt2[:], 8), op=ALU.mult), ["eq1", "wt2"], ["ce2"])
                        P.dve(lambda e: e.tensor_tensor(out=ce[:], in0=ce[:], in1=ce2[:], op=ALU.add), ["ce", "ce2"], ["ce"])
                        P.dve(lambda e: e.tensor_tensor(out=comb[:].rearrange("p t (g e) -> p t g e", g=4), in0=ce[:].unsqueeze(2).broadcast_to([128, 16, 4, 8]),
                                                        in1=ohg[:].unsqueeze(3).broadcast_to([128, 16, 4, 8]), op=ALU.mult), ["ce", "ohg"], ["comb"])
                        P.dve(lambda e: e.tensor_scalar(out=comb[:].rearrange("p a e -> p (a e)"), in0=comb[:].rearrange("p a e -> p (a e)"), scalar1=1.0 / ALPHA, scalar2=None, op0=ALU.mult),
                              ["comb"], ["comb"])
                        dump("comb", comb[:].rearrange("p a e -> p (a e)"), [128, 512], "comb")
                        SR.close()
                        P.barrier()
                        wstE = [wst[:, 0:2048], wst[:, 2048:4096]]
                        wE = [sb(S6, "wE%d" % i, [128, 12288], BF16) for i in range(2)]
                        sgE = [sb(S6, "sgE%d" % i, [128, 512], F32) for i in range(2)]
                        hT = [sb(S6, "hT%d" % i, [128, 4, 512], BF16) for i in range(2)]
                        pG = [ps(S6, "pG%d" % i, [128, 512]) for i in range(2)]
                        pU = [ps(S6, "pU%d" % i, [128, 512]) for i in range(2)]
                        pO = [ps(S6, "pO%d" % i, [128, 512]) for i in range(3)]
                        wsr = Ring([0, 1])
                        gr_ = Ring([0, 1])
                        or_ = Ring([0, 1, 2])
                        crr = [0]
                        n_exp = DEBUG.get("n_exp", 32)

                        def load_expert(ex):
                            ws = ex % 2
                            for pc in range(6):
                                si = wsr.next()
                                P.dma(wstE[si], wexp[ex, :, pc * 2048:(pc + 1) * 2048], writes=["wstE%d" % si])
                                d = wE[ws][:, pc * 2048:(pc + 1) * 2048]
                                P.pool(lambda e: e.tensor_copy(out=d, in_=wstE[si]), ["wstE%d" % si], ["wE%d" % ws])

                        def gu_phase(ex, cth):
                            ws = ex % 2
                            Wg = wE[ws][:, 0:4096].rearrange("p (k n) -> p k n", k=8)
                            Wu = wE[ws][:, 4096:8192].rearrange("p (k n) -> p k n", k=8)
                            wkey = "wE%d" % ws
                            cs_ = slice(cth * 512, (cth + 1) * 512)
                            xkeys = ["mx%d" % (cth * 4 + t_) for t_ in range(4)]
                            hs_ = cth % 2
                            for ft in range(4):
                                gi = gr_.next()
                                for k in range(8):
                                    P.pe(lambda e: e.matmul(pG[gi][:], lhsT=Wg[:, k, ft * 128:(ft + 1) * 128], rhs=x1T[:, k, cs_], start=(k == 0), stop=(k == 7)),
                                         [wkey] + xkeys, ["pG%d" % gi])
                                for k in range(8):
                                    P.pe(lambda e: e.matmul(pU[gi][:], lhsT=Wu[:, k, ft * 128:(ft + 1) * 128], rhs=x1T[:, k, cs_], start=(k == 0), stop=(k == 7)),
                                         [wkey] + xkeys, ["pU%d" % gi])
                                P.act(lambda e: e.activation(out=sgE[gi][:], in_=pG[gi][:], func=AF.Silu), ["pG%d" % gi], ["sgE%d" % gi])
                                P.dve(lambda e: e.tensor_tensor(out=hT[hs_][:, ft, :], in0=pU[gi][:], in1=sgE[gi][:], op=ALU.mult),
                                      ["pU%d" % gi, "sgE%d" % gi], ["hT%d" % hs_])

                        def d_phase(ex, cth):
                            ws = ex % 2
                            Wd = wE[ws][:, 8192:12288].rearrange("p (k n) -> p k n", k=4)
                            wkey = "wE%d" % ws
                            hs_ = cth % 2
                            for tt in range(4):
                                ti = cth * 4 + tt
                                for hf in range(2):
                                    oi = or_.next()
                                    for ft in range(4):
                                        P.pe(lambda e: e.matmul(pO[oi][:], lhsT=hT[hs_][:, ft, tt * 128:(tt + 1) * 128],
                                                                rhs=Wd[:, ft, hf * 512:(hf + 1) * 512], start=(ft == 0), stop=(ft == 3)),
                                             [wkey, "hT%d" % hs_], ["pO%d" % oi])
                                    a_ = acc[:, ti, hf * 512:(hf + 1) * 512]
                                    P.dve(lambda e: e.scalar_tensor_tensor(out=a_, in0=pO[oi][:], scalar=comb[:, ti, ex:ex + 1], in1=a_, op0=ALU.mult, op1=ALU.add),
                                          ["pO%d" % oi, "comb", "acc%d" % ti], ["acc%d" % ti])

                        units = [(ex, cth) for ex in range(n_exp) for cth in range(4)]
                        load_expert(0)
                        if n_exp > 1:
                            load_expert(1)
                        gu_phase(*units[0])
                        for u, (ex, cth) in enumerate(units):
                            if u + 1 < len(units):
                                gu_phase(*units[u + 1])
                            d_phase(ex, cth)
                            if cth == 3 and ex + 2 < n_exp:
                                load_expert(ex + 2)
                        yo = [sb(S6, "yo%d" % i, [128, 1024], F32) for i in range(2)]
                        for i in range(16):
                            yi = i % 2
                            layer_norm(acc[:, i, :], "acc%d" % i, yo[yi][:], "yo%d" % yi, yo[yi][:], "yo%d" % yi, eps=LN_EPS / (ALPHA * ALPHA))
                            P.dma(out[i * 128:(i + 1) * 128, :], yo[yi][:], reads=["yo%d" % yi], writes=["out%d" % yi], sem="outd%d" % yi)
        fin_reads = ["out0", "out1"] + ["dbg_" + n for n in dbg_out]
        P.add("sp", lambda e: e.nop(), reads=fin_reads, sem="fin")
        if n_stage < 6:
            pass
        cnt = P.emit(G)
        nsem = len(cnt)
    return nc, dbg_out, nsem


def prep_shared(inp):
    f = lambda a: np.ascontiguousarray(np.asarray(a, dtype=np.float32))
    w_in = f(inp["w_in"])[0]
    sh = {}
    q = w_in[:, 0:512].reshape(1024, 2, 4, 64).transpose(0, 2, 1, 3).reshape(1024, 512)
    w1 = np.concatenate([q, w_in[:, 512:640], w_in[:, 640:768], w_in[:, 768:896], w_in[:, 1024:1152],
                         w_in[:, 896:1024], w_in[:, 1152:1280], w_in[:, 1280:1304]], axis=1)
    sh["w1t"] = tile_w(w1)
    sh["w4t"] = tile_w(w_in[:, 1304:3352])
    sh["wgtt"] = tile_w(w_in[:, 3352:4376])
    sh["wmgt"] = tile_w(w_in[:, 4376:6424])
    lit = {"k": (inp["cmp_k_w1"], inp["cmp_k_b1"], inp["cmp_pos_k"]), "v": (inp["cmp_v_w1"], inp["cmp_v_b1"], inp["cmp_pos_v"])}
    for kv in "kv":
        cw1 = f(lit[kv][0])[0]
        r = cw1.reshape(32, 64, 256).transpose(1, 0, 2).reshape(64, 32 * 256)
        sh["cw1" + kv] = np.ascontiguousarray(np.concatenate([r, r], axis=0))
        pos = f(lit[kv][2])[0]
        sh["cpos" + kv] = np.ascontiguousarray(np.concatenate([pos.T, pos.T], axis=0))
        sh["cb1" + kv] = np.ascontiguousarray(f(lit[kv][1])[0].reshape(2, 128).T)
    w2k = f(inp["cmp_k_w2"])[0]
    sh["cw2k"] = tile_w(np.concatenate([w2k, w2k], axis=1))
    sh["cw2v"] = tile_w(f(inp["cmp_v_w2"])[0])
    b2k = f(inp["cmp_k_b2"])[0]
    sh["cb2k"] = np.ascontiguousarray(np.concatenate([b2k, b2k])[:, None])
    sh["cb2v"] = f(inp["cmp_v_b2"])[0]
    sh["gng8"] = np.ascontiguousarray(f(inp["ret_gn_g"])[0].reshape(8, 128).T)
    sh["gnb8"] = np.ascontiguousarray(f(inp["ret_gn_b"])[0].reshape(8, 128).T)
    sh["wat"] = tile_w(f(inp["w_up_attn"])[0])
    sh["wrt"] = tile_w(f(inp["w_up_ret"])[0])
    sh["wot"] = tile_w(f(inp["w_out"])[0])
    for n in ("ln1_g", "ln1_b", "ln2_g", "ln2_b"):
        sh[n.replace("_", "")] = f(inp[n])[0]
    rg = f(inp["router_group_w"])[0]
    ri = f(inp["router_inner_w"])[0]
    sh["wrout"] = tile_w(np.concatenate([rg, ri.transpose(1, 0, 2).reshape(1024, 32)], axis=1))
    sh["brout"] = np.ascontiguousarray(np.concatenate([f(inp["router_group_b"])[0], f(inp["router_inner_b"])[0].reshape(32)]))
    wg = f(inp["expert_w_gate"])[0]
    wu = f(inp["expert_w_up"])[0]
    wd = f(inp["expert_w_down"])[0]
    we = np.empty((32, 128, 12288), np.float32)
    we[:, :, 0:4096] = wg.reshape(32, 8, 128, 512).transpose(0, 2, 1, 3).reshape(32, 128, 4096)
    we[:, :, 4096:8192] = wu.reshape(32, 8, 128, 512).transpose(0, 2, 1, 3).reshape(32, 128, 4096)
    we[:, :, 8192:12288] = wd.reshape(32, 4, 128, 1024).transpose(0, 2, 1, 3).reshape(32, 128, 4096)
    sh["wexp"] = we
    return sh


def make_in_maps(inp):
    sh = prep_shared(inp)
    x = np.asarray(inp["x"], dtype=np.float32)
    maps = []
    for core in range(8):
        b, c = core // 2, core % 2
        m = dict(sh)
        xb = x[b]
        own = xb.reshape(16, 2, 128, 1024)[:, c].reshape(TO, 1024)
        m["xT"] = np.ascontiguousarray(xb.T)
        m["xTo"] = np.ascontiguousarray(own.T)
        m["xo"] = np.ascontiguousarray(own)
        for k, v in make_consts(c).items():
            if not k.startswith("_"):
                m["c_" + k] = v
        maps.append(m)
    return maps


_PROG_CACHE = {}


def kernel(**inputs):
    if "prog" not in _PROG_CACHE:
        _PROG_CACHE["prog"] = build_program()
    nc, _, _ = _PROG_CACHE["prog"]
    maps = make_in_maps(inputs)
    res = run_bass_kernel_spmd(nc, maps, core_ids=list(range(8)))
    outp = np.empty((4, 16, 2, 128, 1024), np.float32)
    for core in range(8):
        b, c = core // 2, core % 2
        outp[b, :, c] = res.results[core]["out"].reshape(16, 128, 1024)
    return outp.reshape(4, T, 1024)
```

```python
import numpy as np
import ml_dtypes
import concourse.bass as bass
import concourse.mybir as mybir
from concourse.bass_utils import run_bass_kernel_spmd
from contextlib import ExitStack

F32 = mybir.dt.float32
BF16 = mybir.dt.bfloat16
AF = mybir.ActivationFunctionType
ALU = mybir.AluOpType
NPBF = ml_dtypes.bfloat16

T = 4096
D = 1024
TO = 2048
NEGM = -30000.0
LN_EPS = 1e-5
ALPHA = 2.0 ** 0.25
DEBUG = {}


class Op:
    __slots__ = ("eng", "fn", "reads", "writes", "dma", "sem", "deps", "needs_inc", "idx", "id", "extra")

    def __init__(self, eng, fn, reads, writes, dma, sem):
        self.eng = eng
        self.fn = fn
        self.reads = tuple(reads)
        self.writes = tuple(writes)
        self.dma = dma
        self.sem = sem
        self.deps = []
        self.needs_inc = dma
        self.idx = 0
        self.extra = ()


class _Rec:
    def __getattr__(self, name):
        return lambda *a, **k: (name, a, k)


_REC = _Rec()


class Prog:
    ENGS = ("pe", "act", "dve", "pool", "sp")

    def __init__(self, nc, same_eng_sync=True):
        self.nc = nc
        self.ops = []
        self.same_eng_sync = same_eng_sync
        self.last_by_sem = {}
        self.psum_keys = set()

    def add(self, eng, fn, reads=(), writes=(), dma=False, sem=None):
        lim = DEBUG.get("max_ops")
        self.nadd = getattr(self, "nadd", -1) + 1
        if (lim is not None and self.nadd >= lim and sem not in ("dbg", "fin")) or self.nadd in DEBUG.get("skip", ()):
            return Op(eng, None, reads, writes, dma, sem)
        if dma and sem is None:
            sem = "dma_" + str(writes[0])
        if not dma:
            sem = "eng_" + eng
        op = Op(eng, fn(_REC), reads, writes, dma, sem)
        if DEBUG.get("trace_ops"):
            print(len(self.ops), eng, op.fn[0], reads, writes)
        op.id = len(self.ops)
        self.ops.append(op)
        self.last_by_sem[sem] = op
        return op

    def pe(self, fn, reads=(), writes=()):
        return self.add("pe", fn, reads, writes)

    def act(self, fn, reads=(), writes=()):
        return self.add("act", fn, reads, writes)

    def dve(self, fn, reads=(), writes=()):
        return self.add("dve", fn, reads, writes)

    def pool(self, fn, reads=(), writes=()):
        return self.add("pool", fn, reads, writes)

    def dma(self, out, in_, reads=(), writes=(), sem=None, q="sp", **kw):
        return self.add(q, lambda e: e.dma_start(out=out, in_=in_, **kw), reads, writes, dma=True, sem=sem)

    def barrier(self):
        lasts = list(self.last_by_sem.values())
        for eng in self.ENGS:
            op = self.add(eng, lambda e: e.nop())
            op.extra = tuple(lasts)
        self.last_by_sem = {k: v for k, v in self.last_by_sem.items() if k.startswith("eng_")}

    def analyze(self):
        state = {}
        for op in self.ops:
            deps = set(op.extra)
            for k in op.reads:
                st = state.get(k)
                if st:
                    deps.update(st[0])
                    if k in self.psum_keys:
                        deps.update(r for r in st[1] if r.eng != op.eng)
            for k in op.writes:
                st = state.get(k)
                if st is None:
                    st = state[k] = [[], []]
                if st[1]:
                    deps.update(st[1])
                    deps.update(st[0])
                    st[0] = [op]
                    st[1] = []
                else:
                    same_group = op.dma and all(w.dma and w.sem == op.sem for w in st[0])
                    if same_group:
                        st[0].append(op)
                    else:
                        deps.update(st[0])
                        st[0] = [op]
            for k in op.reads:
                st = state.get(k)
                if st is None:
                    st = state[k] = [[], []]
                st[1].append(op)
            deps.discard(op)
            red = {}
            for d in deps:
                if (not d.dma) and (not op.dma) and d.eng == op.eng:
                    if op.eng == "pe" or not self.same_eng_sync:
                        continue
                cur = red.get(d.sem)
                if cur is None or d.id > cur.id:
                    red[d.sem] = d
            op.deps = list(red.values())
            for d in op.deps:
                d.needs_inc = True
        cnt = {}
        for op in self.ops:
            if op.needs_inc:
                cnt[op.sem] = cnt.get(op.sem, 0) + 1
                op.idx = cnt[op.sem]
        self.sem_names = sorted(cnt.keys())
        return cnt

    def emit(self, stack):
        nc = self.nc
        cnt = self.analyze()
        sems = {}
        for name in self.sem_names:
            sems[name] = stack.enter_context(nc.semaphore(name))
        block = stack.enter_context(nc.Block())
        per_eng = {e: [o for o in self.ops if o.eng == e] for e in self.ENGS}

        def run(eng_obj, ops):
            known = {}
            for op in ops:
                for d in op.deps:
                    val = d.idx * (16 if d.dma else 1)
                    if known.get(d.sem, 0) < val:
                        eng_obj.wait_ge(sems[d.sem], val)
                        known[d.sem] = val
                name, a, k = op.fn
                inst = getattr(eng_obj, name)(*a, **k)
                if op.needs_inc:
                    inst.then_inc(sems[op.sem], 16 if op.dma else 1)

        @block.sync
        def _(e):
            run(e, per_eng["sp"])

        @block.tensor
        def _(e):
            run(e, per_eng["pe"])

        @block.scalar
        def _(e):
            run(e, per_eng["act"])

        @block.vector
        def _(e):
            run(e, per_eng["dve"])

        @block.gpsimd
        def _(e):
            run(e, per_eng["pool"])
        return cnt


class Ring:
    def __init__(self, items):
        self.items = items
        self.i = 0

    def next(self):
        it = self.items[self.i % len(self.items)]
        self.i += 1
        return it


def tile_w(w):
    K, N = w.shape
    return np.ascontiguousarray(w.reshape(K // 128, 128, N).transpose(1, 0, 2).reshape(128, -1))


def rope_tabs(pos, d, scale):
    half = d // 2
    inv = 10000.0 ** (-np.arange(half, dtype=np.float64) * 2.0 / d)
    ang = pos.astype(np.float64)[None, :] * inv[:, None]
    cos = np.cos(ang) * scale
    sin = np.sin(ang) * scale
    reps = 128 // half
    return (np.tile(cos, (reps, 1)).astype(np.float32), np.tile(sin, (reps, 1)).astype(np.float32))


def rot_lhsT(d):
    half = d // 2
    Pm = np.zeros((128, 128), np.float32)
    for blk in range(128 // d):
        o = blk * d
        for m in range(half):
            Pm[o + m, o + m + half] = -1.0
            Pm[o + m + half, o + m] = 1.0
    return np.ascontiguousarray(Pm.T)


_CONST_CACHE = {}


def make_consts(c):
    if c in _CONST_CACHE:
        return _CONST_CACHE[c]
    cs = {}
    own_pos = np.concatenate([np.arange(128) + (2 * i + c) * 128 for i in range(16)])
    allpos = np.arange(T)
    cs["cosK"], cs["sinK"] = rope_tabs(allpos, 64, 1.0)
    cs["cosQ"], cs["sinQ"] = rope_tabs(own_pos, 64, 0.125)
    cs["cosRK"], cs["sinRK"] = rope_tabs(allpos, 128, 128.0 ** -0.5)
    cs["cosRQ"], cs["sinRQ"] = rope_tabs(own_pos, 128, 1.0)
    cend = np.arange(256) * 16 + 31
    cs["cosC"], cs["sinC"] = rope_tabs(cend, 64, 1.0)
    cs["pt64"] = rot_lhsT(64).astype(NPBF)
    cs["pt128"] = rot_lhsT(128).astype(NPBF)
    cs["identb"] = np.eye(128, dtype=np.float32).astype(NPBF)
    E = np.zeros((128, 32, 128), np.float32)
    for j in range(32):
        for k in range(128):
            E[2 * j + k // 64, j, k] = 1.0
            E[64 + 2 * j + k // 64, j, k] = 1.0
    cs["eall"] = E.reshape(128, -1).astype(NPBF)
    wm = np.zeros((128, 6, 128), np.float32)
    kk = np.arange(128)[:, None]
    tt = np.arange(128)[None, :]
    for r in range(6):
        dj = (r - 4) - c
        tk = dj * 128 + kk
        ok = (tk <= tt) & (tt - tk < 512)
        wm[:, r, :] = np.where(ok, 0.0, NEGM)
    cs["wmask"] = wm.reshape(128, -1).astype(NPBF)
    cm = np.zeros((128, 2, 16, 128), np.float32)
    for a in range(2):
        for i in range(16):
            G = 2 * i + c
            n = a * 128 + kk
            t = G * 128 + tt
            cm[:, a, i, :] = np.where(16 * n + 31 <= t, 0.0, NEGM)
    cs["cmpmask"] = cm.reshape(128, -1).astype(NPBF)
    cstart = np.arange(255) * 16
    sstart = np.arange(64) * 64
    ov = np.clip(np.minimum(cstart[None, :] + 32, sstart[:, None] + 64) - np.maximum(cstart[None, :], sstart[:, None]), 0, None) / 16.0
    ovT = np.zeros((256, 64), np.float32)
    ovT[:255] = ov.T
    cs["ovT"] = np.ascontiguousarray(ovT.reshape(2, 128, 64).transpose(1, 0, 2).reshape(128, -1)).astype(NPBF)
    tkm = np.zeros((128, 16, 64), np.float32)
    tkb = np.zeros((128, 16, 64), np.float32)
    for i in range(16):
        G = 2 * i + c
        for p in range(128):
            bt = (G * 128 + p) // 64
            for s in range(64):
                if s == 0:
                    tkb[p, i, s] = 1e9
                elif s == bt:
                    tkb[p, i, s] = 2e9
                elif s == bt - 1:
                    tkb[p, i, s] = 3e9
                elif s <= bt:
                    tkm[p, i, s] = 1.0
                else:
                    tkb[p, i, s] = -1e9 - 1e6 * s
    cs["tkm"] = tkm.reshape(128, -1)
    cs["tkb"] = tkb.reshape(128, -1)
    gam = 1.0 - 2.0 ** (-5.0 - np.arange(4, dtype=np.float64))
    lg = np.log(gam)
    m = np.arange(256)[:, None]
    cq = np.arange(128)[None, :]
    qq = 128 * c + cq
    Dc = np.zeros((128, 2, 4, 128), np.float32)
    for h in range(4):
        dd = np.where(qq >= m, np.exp(np.maximum(qq - m, 0) * lg[h]), 0.0)
        Dc[:, :, h, :] = dd.reshape(2, 128, 128).transpose(1, 0, 2)
    cs["Dc"] = Dc.reshape(128, -1)
    xi = np.zeros((128, 4, 128), np.float32)
    for h in range(4):
        xi[:, h, :] = np.exp((qq + 1.0) * lg[h])
    cs["xi"] = xi.reshape(128, -1)
    zt = np.zeros((128, 2, 4), np.float32)
    for h in range(4):
        zt[:, :, h] = np.exp((255.0 - np.arange(256)) * lg[h]).reshape(2, 128).T
    cs["zeta"] = zt.reshape(128, -1)
    cs["_decay256"] = [float(np.exp(256.0 * lg[h])) for h in range(4)]
    _CONST_CACHE[c] = cs
    return cs


CONST_SHAPES = None


def build_program(n_stage=6, debug=()):
    nc = bass.Bass("TRN2", target_bir_lowering=False)
    cs0 = make_consts(0)
    dram = {}

    def din(name, shape, dt=F32):
        dram[name] = nc.dram_tensor(name, list(shape), dt, kind="ExternalInput").ap()
        return dram[name]

    xT = din("xT", [1024, T])
    xTo = din("xTo", [1024, TO])
    xo = din("xo", [TO, 1024])
    w1t = din("w1t", [128, 8 * 1304])
    w4t = din("w4t", [128, 8 * 2048])
    wmgt = din("wmgt", [128, 8 * 2048])
    cw1 = {kv: din("cw1" + kv, [128, 32 * 256]) for kv in "kv"}
    cpos = {kv: din("cpos" + kv, [128, 32]) for kv in "kv"}
    cb1 = {kv: din("cb1" + kv, [128, 2]) for kv in "kv"}
    cw2k = din("cw2k", [128, 2 * 128])
    cw2v = din("cw2v", [128, 2 * 64])
    cb2k = din("cb2k", [128, 1])
    cb2v = din("cb2v", [64])
    gng8 = din("gng8", [128, 8])
    gnb8 = din("gnb8", [128, 8])
    wgtt = din("wgtt", [128, 8 * 1024])
    wat = din("wat", [128, 4 * 1024])
    wrt = din("wrt", [128, 8 * 1024])
    wot = din("wot", [128, 8 * 1024])
    ln1g = din("ln1g", [1024])
    ln1b = din("ln1b", [1024])
    ln2g = din("ln2g", [1024])
    ln2b = din("ln2b", [1024])
    wrout = din("wrout", [128, 8 * 36])
    brout = din("brout", [36])
    wexp = din("wexp", [32, 128, 12288])
    cdr = {}
    for k, v in cs0.items():
        if k.startswith("_"):
            continue
        cdr[k] = din("c_" + k, v.shape, BF16 if v.dtype == NPBF else F32)
    out = nc.dram_tensor("out", [TO, 1024], F32, kind="ExternalOutput").ap()
    dbg_out = {}

    decay256 = cs0["_decay256"]

    with ExitStack() as G:
        P = Prog(nc)

        def sb(stack, name, shape, dt):
            return stack.enter_context(nc.sbuf_tensor(name, list(shape), dt))

        def ps(stack, name, shape, dt=F32):
            P.psum_keys.add(name)
            ncol = 512 if dt == F32 else 1024
            full = stack.enter_context(nc.psum_tensor(name, [128, ncol], dt))
            n = 1
            for d_ in shape[1:]:
                n *= d_
            v = full[0:shape[0], 0:n]
            if len(shape) == 3:
                v = v.rearrange("p (a b) -> p a b", a=shape[1])
            return v

        def dump(name, ap, shape, key):
            if name in debug:
                t = nc.dram_tensor("dbg_" + name, list(shape), ap.dtype, kind="ExternalOutput").ap()
                dbg_out[name] = t
                P.dma(t, ap, reads=[key], writes=["dbg_" + name], sem="dbg")

        identb = sb(G, "identb", [128, 128], BF16)
        P.dma(identb[:], cdr["identb"], writes=["identb"])
        wst = sb(G, "wst", [128, 4096], F32)
        cast_rr = [0]

        def load_cast(dst_ap, src_ap, n, dst_key, shape3=None):
            o = 0
            while o < n:
                m = min(4096, n - o)
                P.dma(wst[:, 0:m], src_ap[:, o:o + m], writes=["wst"])
                d = dst_ap[:, o:o + m]
                if cast_rr[0] % 2 == 0:
                    P.act(lambda e, d=d, m=m: e.copy(out=d, in_=wst[:, 0:m]), ["wst"], [dst_key])
                else:
                    P.dve(lambda e, d=d, m=m: e.tensor_copy(out=d, in_=wst[:, 0:m]), ["wst"], [dst_key])
                cast_rr[0] += 1
                o += m

        x1T = sb(G, "x1T", [128, 8, TO], BF16)
        wst3 = wst[:].rearrange("p (k n) -> p k n", k=8)
        A_ = ExitStack()
        oattnT = sb(A_, "oattnT", [128, 4, TO], BF16)

        with ExitStack() as SN:
            QT = sb(SN, "QT", [128, 16, 4, 128], BF16)
            KE = [sb(SN, "KE%d" % i_, [128, T], BF16) for i_ in range(2)]
            P.dma(KE[0][64:128, :], cdr["eall"][64:128, :], writes=["KE0"])
            P.dma(KE[1][0:64, :], cdr["eall"][0:64, :], writes=["KE1"])
            winkT = sb(SN, "winkT", [128, T], BF16)
            slcv1 = sb(SN, "slcv1", [128, 32, 2, 65], BF16)
            winv1 = sb(SN, "winv1", [128, 32, 2, 65], BF16)
            gates = sb(SN, "gates", [128, 16, 24], F32)
            kcmpT = sb(SN, "kcmpT", [128, 256], BF16)
            vcmp1 = sb(SN, "vcmp1", [128, 2, 2, 65], BF16)
            pt64 = sb(SN, "pt64", [128, 128], BF16)
            P.dma(pt64[:], cdr["pt64"], writes=["pt64"])
            P.dve(lambda e: e.memset(slcv1[:].rearrange("p a g d -> p (a g d)"), 1.0), [], ["slcv1"])
            P.dve(lambda e: e.memset(winv1[:].rearrange("p a g d -> p (a g d)"), 1.0), [], ["winv1"])
            P.dve(lambda e: e.memset(kcmpT[:], 0.0), [], ["kcmpT"])
            P.dve(lambda e: e.memset(vcmp1[:].rearrange("p a g d -> p (a g d)"), 0.0), [], ["vcmp1"])
            P.dve(lambda e: e.memset(vcmp1[:, :, :, 64:65], 1.0), [], ["vcmp1"])

            with ExitStack() as S12:
                cmpT = {"k": sb(S12, "cmpkT", [128, T], BF16), "v": sb(S12, "cmpvT", [128, T], BF16)}
                with ExitStack() as S1:
                    Wn = sb(S1, "Wn", [128, 8, 1304], BF16)
                    load_cast(Wn[:].rearrange("p k n -> p (k n)"), w1t, 8 * 1304, "Wn")
                    xb = [sb(S1, "xb%d" % i, [128, 8, 512], BF16) for i in range(2)]
                    tabs = [sb(S1, "tab%d" % i, [128, 2, 512], F32) for i in range(2)]
                    ybf = [sb(S1, "ybf%d" % i, [128, 512], BF16) for i in range(2)]
                    t1 = [sb(S1, "t1_%d" % i, [128, 512], F32) for i in range(2)]
                    t2 = [sb(S1, "t2_%d" % i, [128, 512], F32) for i in range(2)]
                    pj = [ps(S1, "pj%d" % i, [128, 512]) for i in range(3)]
                    prot = [ps(S1, "prot%d" % i, [128, 512]) for i in range(2)]
                    pv = [ps(S1, "pv%d" % i, [128, 256]) for i in range(2)]
                    pjr = Ring(list(range(3)))
                    rr = Ring(list(range(2)))
                    pvr = Ring(list(range(2)))
                    xTv = xT.rearrange("(k p) t -> p k t", p=128)
                    xTov = xTo.rearrange("(k p) t -> p k t", p=128)

                    def load_x(src_view, c0, n, slot):
                        P.dma(wst3[:, :, 0:n], src_view[:, :, c0:c0 + n], writes=["wst"])
                        P.act(lambda e: e.copy(out=xb[slot][:, 0:4, 0:n], in_=wst3[:, 0:4, 0:n]), ["wst"], ["xb%d" % slot])
                        P.dve(lambda e: e.tensor_copy(out=xb[slot][:, 4:8, 0:n], in_=wst3[:, 4:8, 0:n]), ["wst"], ["xb%d" % slot])

                    def proj_fm(col0, slot, n=512):
                        pi = pjr.next()
                        for k in range(8):
                            P.pe(lambda e, k=k, pi=pi: e.matmul(pj[pi][:, 0:n], lhsT=Wn[:, k, col0:col0 + 128], rhs=xb[slot][:, k, 0:n],
                                                                 start=(k == 0), stop=(k == 7)), ["Wn", "xb%d" % slot], ["pj%d" % pi])
                        return pi

                    def rope_fm(pi, tslot, dst_ap, dst_key, ptm, ptkey, n=512, src=None, srckey=None):
                        r = rr.next()
                        srcap = pj[pi][:, 0:n] if src is None else src
                        sk = ("pj%d" % pi) if srckey is None else srckey
                        P.act(lambda e: e.copy(out=ybf[r][:, 0:n], in_=srcap), [sk], ["ybf%d" % r])
                        P.pe(lambda e: e.matmul(prot[r][:, 0:n], lhsT=ptm[:], rhs=ybf[r][:, 0:n], start=True, stop=True),
                             [ptkey, "ybf%d" % r], ["prot%d" % r])
                        P.dve(lambda e: e.tensor_tensor(out=t1[r][:, 0:n], in0=srcap, in1=tabs[tslot][:, 0, 0:n], op=ALU.mult),
                              [sk, "tab%d" % tslot], ["t1_%d" % r])
                        P.dve(lambda e: e.tensor_tensor(out=t2[r][:, 0:n], in0=prot[r][:, 0:n], in1=tabs[tslot][:, 1, 0:n], op=ALU.mult),
                              ["prot%d" % r, "tab%d" % tslot], ["t2_%d" % r])
                        if isinstance(dst_ap, list):
                            for (d_ap, rows, dkey) in dst_ap:
                                P.pool(lambda e: e.tensor_tensor(out=d_ap, in0=t1[r][rows, 0:n], in1=t2[r][rows, 0:n], op=ALU.add),
                                       ["t1_%d" % r, "t2_%d" % r], [dkey])
                        elif dst_key == "QT":
                            P.pool(lambda e: e.tensor_tensor(out=dst_ap, in0=t1[r][:, 0:n].rearrange("p (a t) -> p a t", a=4),
                                                             in1=t2[r][:, 0:n].rearrange("p (a t) -> p a t", a=4), op=ALU.add),
                                   ["t1_%d" % r, "t2_%d" % r], [dst_key])
                        else:
                            P.pool(lambda e: e.tensor_tensor(out=dst_ap, in0=t1[r][:, 0:n], in1=t2[r][:, 0:n], op=ALU.add),
                                   ["t1_%d" % r, "t2_%d" % r], [dst_key])

                    for ch in range(8):
                        slot = ch % 2
                        c0 = ch * 512
                        load_x(xTv, c0, 512, slot)
                        P.dma(tabs[slot][:, 0, :], cdr["cosK"][:, c0:c0 + 512], writes=["tab%d" % slot])
                        P.dma(tabs[slot][:, 1, :], cdr["sinK"][:, c0:c0 + 512], writes=["tab%d" % slot])
                        for col0, kv in ((512, "k"), (640, "v")):
                            pi = proj_fm(col0, slot)
                            P.act(lambda e, pi=pi, kv=kv: e.copy(out=cmpT[kv][:, c0:c0 + 512], in_=pj[pi][:]), ["pj%d" % pi], ["cmp" + kv + "T"])
                        pi = proj_fm(768, slot)
                        rope_fm(pi, slot, [(KE[0][0:64, c0:c0 + 512], slice(0, 64), "KE0"), (KE[1][64:128, c0:c0 + 512], slice(64, 128), "KE1")], None, pt64, "pt64")
                        pi = proj_fm(896, slot)
                        rope_fm(pi, slot, winkT[:, c0:c0 + 512], "winkT", pt64, "pt64")
                        for tt in range(4):
                            vi = pvr.next()
                            for k in range(8):
                                P.pe(lambda e, k=k, vi=vi, tt=tt: e.matmul(pv[vi][:], lhsT=xb[slot][:, k, tt * 128:(tt + 1) * 128], rhs=Wn[:, k, 1024:1280],
                                                                            start=(k == 0), stop=(k == 7)), ["Wn", "xb%d" % slot], ["pv%d" % vi])
                            tg = ch * 4 + tt
                            P.act(lambda e, vi=vi, tg=tg: e.copy(out=slcv1[:, tg, :, 0:64], in_=pv[vi][:, 0:128].rearrange("p (g d) -> p g d", g=2)),
                                  ["pv%d" % vi], ["slcv1"])
                            P.dve(lambda e, vi=vi, tg=tg: e.tensor_copy(out=winv1[:, tg, :, 0:64], in_=pv[vi][:, 128:256].rearrange("p (g d) -> p g d", g=2)),
                                  ["pv%d" % vi], ["winv1"])
                    for oc in range(4):
                        slot = oc % 2
                        c0 = oc * 512
                        load_x(xTov, c0, 512, slot)
                        P.dma(tabs[slot][:, 0, :], cdr["cosQ"][:, c0:c0 + 512], writes=["tab%d" % slot])
                        P.dma(tabs[slot][:, 1, :], cdr["sinQ"][:, c0:c0 + 512], writes=["tab%d" % slot])
                        for hh in range(4):
                            pi = proj_fm(hh * 128, slot)
                            rope_fm(pi, slot, QT[:, oc * 4:(oc + 1) * 4, hh, :], "QT", pt64, "pt64")
                        for tt in range(4):
                            vi = pvr.next()
                            for k in range(8):
                                P.pe(lambda e, k=k, vi=vi, tt=tt: e.matmul(pv[vi][:, 0:24], lhsT=xb[slot][:, k, tt * 128:(tt + 1) * 128], rhs=Wn[:, k, 1280:1304],
                                                                            start=(k == 0), stop=(k == 7)), ["Wn", "xb%d" % slot], ["pv%d" % vi])
                            tg = oc * 4 + tt
                            P.act(lambda e, vi=vi, tg=tg: e.activation(out=gates[:, tg, :], in_=pv[vi][:, 0:24], func=AF.Sigmoid), ["pv%d" % vi], ["gates"])
                    dump("QT", QT[:].rearrange("p i a t -> p (i a t)"), [128, 4 * TO], "QT")
                    dump("cmpkT", cmpT["k"][:], [128, T], "cmpkT")
                    dump("slcv1", slcv1[:].rearrange("p a g d -> p (a g d)"), [128, 32 * 130], "slcv1")
                    dump("gates", gates[:].rearrange("p a g -> p (a g)"), [128, 16 * 24], "gates")
                P.barrier()
                if n_stage >= 2:
                    with ExitStack() as S2:
                        w1b = sb(S2, "w1b", [128, 32, 256], BF16)
                        posT = sb(S2, "posT", [128, 32], F32)
                        posTb = sb(S2, "posTb", [128, 32], BF16)
                        b1 = sb(S2, "b1", [128, 2], F32)
                        bias1 = sb(S2, "bias1", [128, 2], F32)
                        w2kf = sb(S2, "w2kf", [128, 2, 128], F32)
                        w2k = sb(S2, "w2k", [128, 2, 128], BF16)
                        w2vf = sb(S2, "w2vf", [128, 2, 64], F32)
                        w2v = sb(S2, "w2v", [128, 2, 64], BF16)
                        b2k = sb(S2, "b2k", [128, 1], F32)
                        b2v = sb(S2, "b2v", [128, 64], F32)
                        tabC = sb(S2, "tabC", [128, 2, 256], F32)
                        h1 = sb(S2, "h1", [128, 2, 256], BF16)
                        xg = sb(S2, "xg", [128, 256], F32)
                        ug = sb(S2, "ug", [128, 256], F32)
                        sg_ = sb(S2, "sg_", [128, 256], F32)
                        yk = sb(S2, "yk", [128, 256], F32)
                        ykb = sb(S2, "ykb", [128, 256], BF16)
                        tk1 = sb(S2, "tk1", [128, 256], F32)
                        tk2 = sb(S2, "tk2", [128, 256], F32)
                        ph = [ps(S2, "ph%d" % i, [128, 256]) for i in range(2)]
                        pcv = ps(S2, "pcv", [128, 2])
                        pkc = ps(S2, "pkc", [128, 256])
                        prk = ps(S2, "prk", [128, 256])
                        pvc = ps(S2, "pvc", [128, 64])
                        P.dma(w2kf[:].rearrange("p a n -> p (a n)"), cw2k, writes=["w2kf"])
                        P.dve(lambda e: e.tensor_copy(out=w2k[:], in_=w2kf[:]), ["w2kf"], ["w2k"])
                        P.dma(w2vf[:].rearrange("p a n -> p (a n)"), cw2v, writes=["w2vf"])
                        P.dve(lambda e: e.tensor_copy(out=w2v[:], in_=w2vf[:]), ["w2vf"], ["w2v"])
                        P.dma(b2k[:], cb2k, writes=["b2k"])
                        P.dma(b2v[:], cb2v.partition_broadcast(128), writes=["b2v"])
                        P.dma(tabC[:, 0, :], cdr["cosC"], writes=["tabC"])
                        P.dma(tabC[:, 1, :], cdr["sinC"], writes=["tabC"])
                        for kv in "kv":
                            load_cast(w1b[:].rearrange("p l n -> p (l n)"), cw1[kv], 32 * 256, "w1b")
                            P.dma(posT[:], cpos[kv], writes=["posT"])
                            P.dve(lambda e: e.tensor_copy(out=posTb[:], in_=posT[:]), ["posT"], ["posTb"])
                            P.dma(b1[:], cb1[kv], writes=["b1"])
                            for ht in range(2):
                                for l in range(32):
                                    P.pe(lambda e, ht=ht, l=l: e.matmul(pcv[:, ht:ht + 1], lhsT=w1b[0:64, l, ht * 128:(ht + 1) * 128], rhs=posTb[0:64, l:l + 1],
                                                                         start=(l == 0), stop=(l == 31)), ["w1b", "posTb"], ["pcv"])
                            P.dve(lambda e: e.tensor_tensor(out=bias1[:], in0=pcv[:], in1=b1[:], op=ALU.add), ["pcv", "b1"], ["bias1"])
                            for g in range(2):
                                gp = slice(g * 64, (g + 1) * 64)
                                for ht in range(2):
                                    for l in range(32):
                                        P.pe(lambda e, ht=ht, l=l, gp=gp, kv=kv: e.matmul(ph[ht][:, 0:255], lhsT=w1b[gp, l, ht * 128:(ht + 1) * 128],
                                                                                        rhs=cmpT[kv][gp, l:l + 16 * 254 + 1:16],
                                                                                        start=(l == 0), stop=(l == 31)), ["w1b", "cmp" + kv + "T"], ["ph%d" % ht])
                                    P.act(lambda e, ht=ht: e.activation(out=xg[:, 0:255], in_=ph[ht][:, 0:255], func=AF.Identity, bias=bias1[:, ht:ht + 1], scale=1.0),
                                          ["ph%d" % ht, "bias1"], ["xg"])
                                    P.dve(lambda e: e.tensor_tensor(out=ug[:, 0:255], in0=xg[:, 0:255], in1=xg[:, 0:255], op=ALU.mult), ["xg"], ["ug"])
                                    P.dve(lambda e: e.tensor_scalar(out=ug[:, 0:255], in0=ug[:, 0:255], scalar1=0.044715, scalar2=1.0, op0=ALU.mult, op1=ALU.add), ["ug"], ["ug"])
                                    P.dve(lambda e: e.tensor_tensor(out=ug[:, 0:255], in0=ug[:, 0:255], in1=xg[:, 0:255], op=ALU.mult), ["ug", "xg"], ["ug"])
                                    P.act(lambda e: e.activation(out=sg_[:, 0:255], in_=ug[:, 0:255], func=AF.Sigmoid, scale=1.5957691216057308), ["ug"], ["sg_"])
                                    P.dve(lambda e, ht=ht: e.tensor_tensor(out=h1[:, ht, 0:255], in0=xg[:, 0:255], in1=sg_[:, 0:255], op=ALU.mult), ["xg", "sg_"], ["h1"])
                                if kv == "k":
                                    for ht in range(2):
                                        P.pe(lambda e, ht=ht: e.matmul(pkc[:, 0:255], lhsT=w2k[:, ht, :], rhs=h1[:, ht, 0:255], start=(ht == 0), stop=(ht == 1)),
                                             ["w2k", "h1"], ["pkc"])
                                    P.act(lambda e: e.activation(out=yk[:, 0:255], in_=pkc[:, 0:255], func=AF.Identity, bias=b2k[:, 0:1], scale=1.0), ["pkc", "b2k"], ["yk"])
                                    P.act(lambda e: e.copy(out=ykb[:, 0:255], in_=yk[:, 0:255]), ["yk"], ["ykb"])
                                    P.pe(lambda e: e.matmul(prk[:, 0:255], lhsT=pt64[:], rhs=ykb[:, 0:255], start=True, stop=True), ["pt64", "ykb"], ["prk"])
                                    P.dve(lambda e: e.tensor_tensor(out=tk1[:, 0:255], in0=yk[:, 0:255], in1=tabC[:, 0, 0:255], op=ALU.mult), ["yk", "tabC"], ["tk1"])
                                    P.dve(lambda e: e.tensor_tensor(out=tk2[:, 0:255], in0=prk[:, 0:255], in1=tabC[:, 1, 0:255], op=ALU.mult), ["prk", "tabC"], ["tk2"])
                                    P.dve(lambda e, gp=gp: e.tensor_tensor(out=kcmpT[gp, 0:255], in0=tk1[gp, 0:255], in1=tk2[gp, 0:255], op=ALU.add), ["tk1", "tk2"], ["kcmpT"])
                                else:
                                    for a in range(2):
                                        cntn = 128 if a == 0 else 127
                                        for ht in range(2):
                                            P.pe(lambda e, ht=ht, a=a, cntn=cntn: e.matmul(pvc[0:cntn, :], lhsT=h1[:, ht, a * 128:a * 128 + cntn], rhs=w2v[:, ht, :],
                                                                                            start=(ht == 0), stop=(ht == 1)), ["w2v", "h1"], ["pvc"])
                                        P.dve(lambda e, a=a, cntn=cntn, g=g: e.tensor_tensor(out=vcmp1[0:cntn, a, g, 0:64], in0=pvc[0:cntn, :], in1=b2v[0:cntn, :], op=ALU.add),
                                              ["pvc", "b2v"], ["vcmp1"])
                        dump("kcmpT", kcmpT[:], [128, 256], "kcmpT")
                        dump("vcmp1", vcmp1[:].rearrange("p a g d -> p (a g d)"), [128, 260], "vcmp1")
                    P.barrier()
            P.barrier()
            if n_stage >= 3:
                with ExitStack() as S3:
                    def bc4(ap):
                        return ap.unsqueeze(1).broadcast_to([ap.shape[0], 4, ap.shape[1]])

                    wmask = sb(S3, "wmask", [128, 6, 128], BF16)
                    cmpmask = sb(S3, "cmpmask", [128, 2, 16, 128], BF16)
                    ovT = sb(S3, "ovT", [128, 2, 64], BF16)
                    tkm = sb(S3, "tkm", [128, 16, 64], F32)
                    tkb = sb(S3, "tkb", [128, 16, 64], F32)
                    wmask4 = sb(S3, "wmask4", [128, 6, 512], BF16)
                    cm4 = [sb(S3, "cm4_%d" % i_, [128, 2, 512], BF16) for i_ in range(2)]
                    QN = [sb(S3, "QN%d" % i_, [128, 512], BF16) for i_ in range(4)]
                    P.dma(wmask[:].rearrange("p a k -> p (a k)"), cdr["wmask"], writes=["wmask"])
                    P.dma(cmpmask[:].rearrange("p a i k -> p (a i k)"), cdr["cmpmask"], writes=["cmpmask"])
                    P.dma(ovT[:].rearrange("p a k -> p (a k)"), cdr["ovT"], writes=["ovT"])
                    P.dma(tkm[:].rearrange("p a k -> p (a k)"), cdr["tkm"], writes=["tkm"])
                    P.dma(tkb[:].rearrange("p a k -> p (a k)"), cdr["tkb"], writes=["tkb"])
                    for r_ in range(6):
                        P.pool(lambda e: e.tensor_copy(out=wmask4[:, r_, :].rearrange("p (a t) -> p a t", a=4), in_=bc4(wmask[:, r_, :])), ["wmask"], ["wmask4"])
                    eT = [sb(S3, "eT%d" % i, [128, 512], BF16) for i in range(4)]
                    oacc = sb(S3, "oacc", [128, 512], F32)
                    oab = sb(S3, "oab", [128, 512], BF16)
                    rz = sb(S3, "rz", [128, 4], F32)
                    coef = sb(S3, "coef", [128, 4], F32)
                    imp = sb(S3, "imp", [128, 64], F32)
                    score = sb(S3, "score", [128, 64], F32)
                    work = sb(S3, "work", [128, 64], F32)
                    m8 = sb(S3, "m8", [128, 16], F32)
                    nmk = [sb(S3, "nmk%d" % i_, [128, 2, 64], BF16) for i_ in range(2)]
                    pST = [ps(S3, "pST%d" % i, [128, 512]) for i in range(3)]
                    pA = ps(S3, "pA", [128, 4, 65])
                    pB = ps(S3, "pB", [128, 4, 64])
                    pS = ps(S3, "pS", [128, 4, 65])
                    pW = ps(S3, "pW", [128, 4, 65])
                    pTr3w = ps(S3, "pTr", [128, 512], BF16)
                    pTr = pTr3w[:, 0:128]
                    str_ = Ring([0, 1, 2])
                    etr = Ring([0, 1, 2, 3])

                    def scores(kT_ap, kkey, g, i, masks, q_ap=None, qkey="QT"):
                        gp = slice(g * 64, (g + 1) * 64)
                        si = str_.next()
                        ei = etr.next()
                        nm = len(masks)
                        if q_ap is None:
                            q_ap = QT[gp, i, :, :].rearrange("p a t -> p (a t)")
                        P.pe(lambda e: e.matmul(pST[si][:], lhsT=kT_ap, rhs=q_ap, start=True, stop=(nm == 0)), [kkey, qkey], ["pST%d" % si])
                        for mi, (ml, mr, mkeys) in enumerate(masks):
                            P.pe(lambda e: e.matmul(pST[si][:], lhsT=ml, rhs=mr, start=False, stop=(mi == nm - 1)), mkeys, ["pST%d" % si])
                        P.act(lambda e: e.activation(out=eT[ei][:], in_=pST[si][:], func=AF.Exp), ["pST%d" % si], ["eT%d" % ei])
                        return ei

                    def finish_branch(pacc, pkey, i, g, br, first):
                        P.dve(lambda e: e.tensor_scalar(out=rz[:], in0=pacc[:, :, 64], scalar1=1e-30, scalar2=None, op0=ALU.max), [pkey], ["rz"])
                        P.dve(lambda e: e.reciprocal(out=rz[:], in_=rz[:]), ["rz"], ["rz"])
                        P.dve(lambda e: e.tensor_tensor(out=coef[:], in0=rz[:], in1=gates[:, i, g * 12 + br:g * 12 + 12:3], op=ALU.mult), ["rz", "gates"], ["coef"])
                        for hh in range(4):
                            o = oacc[:, g * 256 + hh * 64:g * 256 + (hh + 1) * 64]
                            if first:
                                P.dve(lambda e, hh=hh, o=o: e.tensor_scalar(out=o, in0=pacc[:, hh, 0:64], scalar1=coef[:, hh:hh + 1], scalar2=None, op0=ALU.mult),
                                      [pkey, "coef"], ["oacc"])
                            else:
                                P.dve(lambda e, hh=hh, o=o: e.scalar_tensor_tensor(out=o, in0=pacc[:, hh, 0:64], scalar=coef[:, hh:hh + 1], in1=o, op0=ALU.mult, op1=ALU.add),
                                      [pkey, "coef", "oacc"], ["oacc"])

                    tasks = []

                    def mk_cmp(i, g, a, na):
                        gp = slice(g * 64, (g + 1) * 64)

                        def sc():
                            if g == 0:
                                P.pool(lambda e: e.tensor_copy(out=cm4[i % 2][:, a, :].rearrange("p (h t) -> p h t", h=4), in_=bc4(cmpmask[:, a, i, :])),
                                       ["cmpmask"], ["cm4_%d" % (i % 2)])
                            return scores(kcmpT[gp, a * 128:(a + 1) * 128], "kcmpT", g, i,
                                          [(identb[:], cm4[i % 2][:, a, :], ["identb", "cm4_%d" % (i % 2)])])

                        def pvf(ei):
                            for hh in range(4):
                                P.pe(lambda e: e.matmul(pA[:, hh, :], lhsT=eT[ei][:, hh * 128:(hh + 1) * 128], rhs=vcmp1[:, a, g, :],
                                                        start=(a == 0 and hh == 0), stop=(a == na - 1 and hh == 3)), ["eT%d" % ei, "vcmp1"], ["pA"])
                                P.pe(lambda e: e.matmul(pB[:, hh, :], lhsT=eT[ei][:, hh * 128:(hh + 1) * 128], rhs=ovT[:, a, :],
                                                        start=(a == 0 and hh == 0), stop=(a == na - 1 and hh == 3)), ["eT%d" % ei, "ovT"], ["pB"])

                        def post():
                            finish_branch(pA, "pA", i, g, 0, True)
                            P.dve(lambda e: e.tensor_scalar(out=imp[:], in0=pB[:, 0, :], scalar1=rz[:, 0:1], scalar2=None, op0=ALU.mult), ["pB", "rz"], ["imp"])
                            for hh in range(1, 4):
                                P.dve(lambda e: e.scalar_tensor_tensor(out=imp[:], in0=pB[:, hh, :], scalar=rz[:, hh:hh + 1], in1=imp[:], op0=ALU.mult, op1=ALU.add),
                                      ["pB", "rz", "imp"], ["imp"])
                            P.dve(lambda e: e.tensor_tensor(out=score[:], in0=imp[:], in1=tkm[:, i, :], op=ALU.mult), ["imp", "tkm"], ["score"])
                            P.dve(lambda e: e.tensor_tensor(out=score[:], in0=score[:], in1=tkb[:, i, :], op=ALU.add), ["score", "tkb"], ["score"])
                            P.dve(lambda e: e.max(out=m8[:, 0:8], in_=score[:]), ["score"], ["m8"])
                            P.dve(lambda e: e.match_replace(out=work[:], in_to_replace=m8[:, 0:8], in_values=score[:], imm_value=-3.0e38), ["score", "m8"], ["work"])
                            P.dve(lambda e: e.max(out=m8[:, 8:16], in_=work[:]), ["work"], ["m8"])
                            P.dve(lambda e: e.tensor_scalar(out=nmk[g][:], in0=score[:].unsqueeze(1).broadcast_to([128, 2, 64]), scalar1=m8[:, 15:16], scalar2=NEGM,
                                                            op0=ALU.is_lt, op1=ALU.mult), ["score", "m8"], ["nmk%d" % g])
                            if ("imp%d_%d" % (i, g)) in debug:
                                dump("imp%d_%d" % (i, g), imp[:], [128, 64], "imp")
                                dump("score%d_%d" % (i, g), score[:], [128, 64], "score")
                                dump("m8%d_%d" % (i, g), m8[:], [128, 16], "m8")
                        return [None, sc, pvf, post if a == na - 1 else None]

                    def mk_win(i, g, idx, r, j, nw):
                        gp = slice(g * 64, (g + 1) * 64)

                        def sc():
                            return scores(winkT[gp, j * 128:(j + 1) * 128], "winkT", g, i,
                                          [(identb[:], wmask4[:, r, :], ["identb", "wmask4"])])

                        def pvf(ei):
                            for hh in range(4):
                                P.pe(lambda e: e.matmul(pW[:, hh, :], lhsT=eT[ei][:, hh * 128:(hh + 1) * 128], rhs=winv1[:, j, g, :],
                                                        start=(idx == 0 and hh == 0), stop=(idx == nw - 1 and hh == 3)), ["eT%d" % ei, "winv1"], ["pW"])

                        def post():
                            finish_branch(pW, "pW", i, g, 2, False)
                        return [None, sc, pvf, post if idx == nw - 1 else None]

                    def tile_end_pe(i):
                        for ct in range(4):
                            P.pe(lambda e: e.transpose(out=pTr3w[:, ct * 128:(ct + 1) * 128], in_=oab[:, ct * 128:(ct + 1) * 128], identity=identb[:]), ["oab", "identb"], ["pTr"])
                        P.dve(lambda e: e.tensor_copy(out=oattnT[:, :, i * 128:(i + 1) * 128], in_=pTr3w[:].rearrange("p (a t) -> p a t", a=4)), ["pTr"], ["oattnT"])

                    def mk_slc(i, g, j, nj):
                        gp = slice(g * 64, (g + 1) * 64)

                        qn_i = (2 * i + g) % 4
                        oh = slice((1 - g) * 64, (2 - g) * 64)

                        def pre():
                            P.pool(lambda e: e.tensor_copy(out=QN[qn_i][gp, :], in_=QT[gp, i, :, :].rearrange("p a t -> p (a t)")), ["QT"], ["QN%d" % qn_i])
                            P.pe(lambda e: e.transpose(out=pTr[:], in_=nmk[g][:].rearrange("p a s -> p (a s)"), identity=identb[:]), ["nmk%d" % g, "identb"], ["pTr"])
                            P.dve(lambda e: e.tensor_copy(out=QN[qn_i][oh, :].rearrange("p (a t) -> p a t", a=4), in_=bc4(pTr[oh, :])), ["pTr"], ["QN%d" % qn_i])
                            if g == 0 and i > 0:
                                tile_end_pe(i - 1)

                        def sc():
                            masks = []
                            if j >= 2 * i:
                                masks.append((identb[:], wmask4[:, 4 + (j - 2 * i), :], ["identb", "wmask4"]))
                            return scores(KE[g][:, j * 128:(j + 1) * 128], "KE%d" % g, g, i, masks, q_ap=QN[qn_i][:], qkey="QN%d" % qn_i)

                        def pvf(ei):
                            for hh in range(4):
                                P.pe(lambda e: e.matmul(pS[:, hh, :], lhsT=eT[ei][:, hh * 128:(hh + 1) * 128], rhs=slcv1[:, j, g, :],
                                                        start=(j == 0 and hh == 0), stop=(j == nj - 1 and hh == 3)), ["eT%d" % ei, "slcv1"], ["pS"])

                        def post():
                            finish_branch(pS, "pS", i, g, 1, False)
                            if g == 1:
                                if ("oacc%d" % i) in debug:
                                    dump("oacc%d" % i, oacc[:], [128, 512], "oacc")
                                P.pool(lambda e: e.tensor_copy(out=oab[:], in_=oacc[:]), ["oacc"], ["oab"])
                        return [pre if j == 0 else None, sc, pvf, post if j == nj - 1 else None]

                    for i in range(16):
                        for g in range(2):
                            na = 1 if i < 8 else 2
                            for a in range(na):
                                tasks.append(mk_cmp(i, g, a, na))
                            js = [(r, 2 * i - 4 + r) for r in range(6) if 2 * i - 4 + r >= 0]
                            for idx, (r, j) in enumerate(js):
                                tasks.append(mk_win(i, g, idx, r, j, len(js)))
                            nj = 2 * i + 2
                            for j in range(nj):
                                tasks.append(mk_slc(i, g, j, nj))
                    nt = len(tasks)
                    eis = [None] * nt

                    def emit_score(k):
                        if tasks[k][0] is not None:
                            tasks[k][0]()
                        eis[k] = tasks[k][1]()

                    emit_score(0)
                    emit_score(1)
                    for k in range(nt):
                        if k + 2 < nt:
                            emit_score(k + 2)
                        tasks[k][2](eis[k])
                        if tasks[k][3] is not None:
                            tasks[k][3]()
                    tile_end_pe(15)
                    dump("oattnT", oattnT[:].rearrange("p a t -> p (a t)"), [128, 4 * TO], "oattnT")
                P.barrier()
        P.barrier()

        B_ = ExitStack()
        oretT = sb(B_, "oretT", [128, 8, TO], BF16)
        if n_stage >= 4:
            with ExitStack() as S4:
                W4 = sb(S4, "W4", [128, 8, 2048], BF16)
                load_cast(W4[:].rearrange("p k n -> p (k n)"), w4t, 8 * 2048, "W4")
                pt128 = sb(S4, "pt128", [128, 128], BF16)
                P.dma(pt128[:], cdr["pt128"], writes=["pt128"])
                Dc = sb(S4, "Dc", [128, 2, 4, 128], F32)
                xi = sb(S4, "xi", [128, 4, 128], F32)
                zeta = sb(S4, "zeta", [128, 2, 4], F32)
                P.dma(Dc[:].rearrange("p a h c -> p (a h c)"), cdr["Dc"], writes=["Dc"])
                P.dma(xi[:].rearrange("p h c -> p (h c)"), cdr["xi"], writes=["xi"])
                P.dma(zeta[:].rearrange("p a h -> p (a h)"), cdr["zeta"], writes=["zeta"])
                xst = wst3
                xb = sb(S4, "xb4", [128, 8, 512], BF16)
                xob = sb(S4, "xob4", [128, 8, 256], BF16)
                tabs = sb(S4, "tab4", [128, 2, 512], F32)
                tabq = sb(S4, "tabq4", [128, 2, 256], F32)
                ybf2 = [sb(S4, "ybf4_%d" % i_, [128, 512], BF16) for i_ in range(2)]
                t12 = [sb(S4, "t1_4_%d" % i_, [128, 512], F32) for i_ in range(2)]
                t22 = [sb(S4, "t2_4_%d" % i_, [128, 512], F32) for i_ in range(2)]
                rr4 = Ring([0, 1])
                kT = sb(S4, "kT4", [128, 4, 512], BF16)
                qT = sb(S4, "qT4", [128, 4, 256], BF16)
                qxT = sb(S4, "qxT4", [128, 4, 256], BF16)
                vtok = sb(S4, "vtok", [128, 4, 1024], BF16)
                kz = sb(S4, "kz", [128, 4, 4, 128], BF16)
                R = sb(S4, "R", [128, 4, 256], F32)
                Rb = sb(S4, "Rb", [128, 4, 256], BF16)
                sc = [sb(S4, "sc%d" % i_, [128, 2, 128], BF16) for i_ in range(2)]
                epsT = sb(S4, "epsT", [128, 1], F32)
                P.dve(lambda e: e.memset(epsT[:], LN_EPS), [], ["epsT"])
                pending4 = []
                st6 = sb(S4, "st6", [128, 6], F32)
                mv = sb(S4, "mv", [128, 2], F32)
                rstd = sb(S4, "rstd", [128, 1], F32)
                oretb = [sb(S4, "oretb%d" % i_, [128, 1024], BF16) for i_ in range(2)]
                pj = [ps(S4, "pj4_%d" % i, [128, 512]) for i in range(3)]
                psc = [ps(S4, "psc%d" % i_, [128, 2, 128]) for i_ in range(2)]
                po = [ps(S4, "po%d" % i_, [128, 256]) for i_ in range(2)]
                pTrw = ps(S4, "pTr4", [128, 1024], BF16)
                pTr = pTrw[:, 0:128]
                pjr = Ring([0, 1, 2])
                P.dve(lambda e: e.memset(R[:].rearrange("p h e -> p (h e)"), 0.0), [], ["R%d" % h_ for h_ in range(4)])
                P.dve(lambda e: e.memset(Rb[:].rearrange("p h e -> p (h e)"), 0.0), [], ["Rb%d" % h_ for h_ in range(4)])
                xTv = xT.rearrange("(k p) t -> p k t", p=128)
                xTov = xTo.rearrange("(k p) t -> p k t", p=128)

                def rope4(pi, n, tab, tabkey, dst_ap, dst_key):
                    ri = pjr.next()
                    prot = pj[ri]
                    rb = rr4.next()
                    ybf, t1, t2 = ybf2[rb], t12[rb], t22[rb]
                    P.act(lambda e: e.copy(out=ybf[:, 0:n], in_=pj[pi][:, 0:n]), ["pj4_%d" % pi], ["ybf4_%d" % rb])
                    P.pe(lambda e: e.matmul(prot[:, 0:n], lhsT=pt128[:], rhs=ybf[:, 0:n], start=True, stop=True), ["pt128", "ybf4_%d" % rb], ["pj4_%d" % ri])
                    P.dve(lambda e: e.tensor_tensor(out=t1[:, 0:n], in0=pj[pi][:, 0:n], in1=tab[:, 0, 0:n], op=ALU.mult), ["pj4_%d" % pi, tabkey], ["t1_4_%d" % rb])
                    P.dve(lambda e: e.tensor_tensor(out=t2[:, 0:n], in0=prot[:, 0:n], in1=tab[:, 1, 0:n], op=ALU.mult), ["pj4_%d" % ri, tabkey], ["t2_4_%d" % rb])
                    P.pool(lambda e: e.tensor_tensor(out=dst_ap, in0=t1[:, 0:n], in1=t2[:, 0:n], op=ALU.add), ["t1_4_%d" % rb, "t2_4_%d" % rb], [dst_key])

                for gch in range(8):
                    c0 = gch * 512
                    o0 = gch * 256
                    P.dma(xst[:], xTv[:, :, c0:c0 + 512], writes=["wst"])
                    P.act(lambda e: e.copy(out=xb[:, 0:4, :], in_=xst[:, 0:4, :]), ["wst"], ["xb4"])
                    P.dve(lambda e: e.tensor_copy(out=xb[:, 4:8, :], in_=xst[:, 4:8, :]), ["wst"], ["xb4"])
                    P.dma(xst[:, :, 0:256], xTov[:, :, o0:o0 + 256], writes=["wst"])
                    P.act(lambda e: e.copy(out=xob[:, 0:4, :], in_=xst[:, 0:4, 0:256]), ["wst"], ["xob4"])
                    P.dve(lambda e: e.tensor_copy(out=xob[:, 4:8, :], in_=xst[:, 4:8, 0:256]), ["wst"], ["xob4"])
                    P.dma(tabs[:, 0, :], cdr["cosRK"][:, c0:c0 + 512], writes=["tab4"])
                    P.dma(tabs[:, 1, :], cdr["sinRK"][:, c0:c0 + 512], writes=["tab4"])
                    P.dma(tabq[:, 0, :], cdr["cosRQ"][:, o0:o0 + 256], writes=["tabq4"])
                    P.dma(tabq[:, 1, :], cdr["sinRQ"][:, o0:o0 + 256], writes=["tabq4"])
                    for h in range(4):
                        pi = pjr.next()
                        for k in range(8):
                            P.pe(lambda e, k=k, pi=pi, h=h: e.matmul(pj[pi][:], lhsT=W4[:, k, 512 + h * 128:512 + (h + 1) * 128], rhs=xb[:, k, :],
                                                                      start=(k == 0), stop=(k == 7)), ["W4", "xb4"], ["pj4_%d" % pi])
                        rope4(pi, 512, tabs, "tab4", kT[:, h, :], "kT4")
                    for h in range(4):
                        pi = pjr.next()
                        for k in range(8):
                            P.pe(lambda e, k=k, pi=pi, h=h: e.matmul(pj[pi][:, 0:256], lhsT=W4[:, k, h * 128:(h + 1) * 128], rhs=xob[:, k, :],
                                                                      start=(k == 0), stop=(k == 7)), ["W4", "xob4"], ["pj4_%d" % pi])
                        rope4(pi, 256, tabq, "tabq4", qT[:, h, :], "qT4")
                    for pp in range(2):
                        P.dve(lambda e, pp=pp: e.tensor_tensor(out=qxT[:, :, pp * 128:(pp + 1) * 128], in0=qT[:, :, pp * 128:(pp + 1) * 128], in1=xi[:], op=ALU.mult),
                              ["qT4", "xi"], ["qxT4"])
                    for tt in range(4):
                        for hf in range(2):
                            pi = pjr.next()
                            for k in range(8):
                                P.pe(lambda e, k=k, pi=pi, tt=tt, hf=hf: e.matmul(pj[pi][:], lhsT=xb[:, k, tt * 128:(tt + 1) * 128],
                                                                                   rhs=W4[:, k, 1024 + hf * 512:1024 + (hf + 1) * 512],
                                                                                   start=(k == 0), stop=(k == 7)), ["W4", "xb4"], ["pj4_%d" % pi])
                            P.act(lambda e, pi=pi, tt=tt, hf=hf: e.copy(out=vtok[:, tt, hf * 512:(hf + 1) * 512], in_=pj[pi][:]), ["pj4_%d" % pi], ["vtok"])
                    for tt in range(4):
                        for h in range(4):
                            P.pe(lambda e: e.transpose(out=pTrw[:, h * 128:(h + 1) * 128], in_=kT[:, h, tt * 128:(tt + 1) * 128], identity=identb[:]), ["kT4", "identb"], ["pTr4"])
                        P.dve(lambda e: e.tensor_tensor(out=kz[:, tt, :, :], in0=pTrw[:, 0:512].rearrange("p (h d) -> p h d", h=4),
                                                        in1=zeta[:, tt % 2, :].unsqueeze(2).broadcast_to([128, 4, 128]), op=ALU.mult), ["pTr4", "zeta"], ["kz"])
                    units = [(pp, h) for pp in range(2) for h in range(4)]

                    def phaseA(pp, h, ub):
                        qs = slice(pp * 128, (pp + 1) * 128)
                        for mt in range(2):
                            tt = pp * 2 + mt
                            P.pe(lambda e: e.matmul(psc[ub][:, mt, :], lhsT=kT[:, h, tt * 128:(tt + 1) * 128], rhs=qT[:, h, qs], start=True, stop=True),
                                 ["kT4", "qT4"], ["psc%d" % ub])
                        P.dve(lambda e: e.tensor_tensor(out=sc[ub][:], in0=psc[ub][:], in1=Dc[:, :, h, :], op=ALU.mult), ["psc%d" % ub, "Dc"], ["sc%d" % ub])

                    def phaseBC(pp, h, ub):
                        i = gch * 2 + pp
                        qs = slice(pp * 128, (pp + 1) * 128)
                        hs = slice(h * 256, (h + 1) * 256)
                        ob = oretb[pp]
                        for mt in range(2):
                            tt = pp * 2 + mt
                            P.pe(lambda e: e.matmul(po[ub][:], lhsT=sc[ub][:, mt, :], rhs=vtok[:, tt, hs], start=(mt == 0), stop=False), ["sc%d" % ub, "vtok"], ["po%d" % ub])
                        P.pe(lambda e: e.matmul(po[ub][:], lhsT=qxT[:, h, qs], rhs=Rb[:, h, :], start=False, stop=True), ["qxT4", "Rb%d" % h], ["po%d" % ub])
                        ri = pjr.next()
                        for mt in range(2):
                            tt = pp * 2 + mt
                            P.pe(lambda e: e.matmul(pj[ri][:, 0:256], lhsT=kz[:, tt, h, :], rhs=vtok[:, tt, hs], start=(mt == 0), stop=(mt == 1)),
                                 ["kz", "vtok"], ["pj4_%d" % ri])
                        P.dve(lambda e: e.bn_stats(out=st6[:], in_=po[ub][:]), ["po%d" % ub], ["st6"])
                        P.dve(lambda e: e.bn_aggr(out=mv[:], in_=st6[:]), ["st6"], ["mv"])
                        P.act(lambda e: e.activation(out=rstd[:], in_=mv[:, 1:2], func=AF.Sqrt, bias=epsT[:, 0:1], scale=1.0), ["mv", "epsT"], ["rstd"])
                        P.dve(lambda e: e.reciprocal(out=rstd[:], in_=rstd[:]), ["rstd"], ["rstd"])
                        P.dve(lambda e: e.tensor_scalar(out=ob[:, hs], in0=po[ub][:], scalar1=mv[:, 0:1], scalar2=rstd[:, 0:1], op0=ALU.subtract, op1=ALU.mult),
                              ["po%d" % ub, "mv", "rstd"], ["oretb%d" % pp])
                        P.dve(lambda e: e.scalar_tensor_tensor(out=R[:, h, :], in0=R[:, h, :], scalar=decay256[h], in1=pj[ri][:, 0:256], op0=ALU.mult, op1=ALU.add),
                              ["R%d" % h, "pj4_%d" % ri], ["R%d" % h])
                        P.act(lambda e: e.copy(out=Rb[:, h, :], in_=R[:, h, :]), ["R%d" % h], ["Rb%d" % h])

                    def pair_end(pp, i):
                        ob = oretb[pp]
                        for et in range(8):
                            P.pe(lambda e: e.transpose(out=pTrw[:, et * 128:(et + 1) * 128], in_=ob[:, et * 128:(et + 1) * 128], identity=identb[:]), ["oretb%d" % pp, "identb"], ["pTr4"])
                        P.act(lambda e: e.copy(out=oretT[:, :, i * 128:(i + 1) * 128], in_=pTrw[:].rearrange("p (a t) -> p a t", a=8)), ["pTr4"], ["oretT"])

                    phaseA(units[0][0], units[0][1], 0)
                    for u, (pp, h) in enumerate(units):
                        if u + 1 < len(units):
                            phaseA(units[u + 1][0], units[u + 1][1], (u + 1) % 2)
                        phaseBC(pp, h, u % 2)
                        if pending4:
                            pending4.pop(0)()
                        if h == 3:
                            pending4.append(lambda pp=pp, i=gch * 2 + pp: pair_end(pp, i))
                while pending4:
                    pending4.pop(0)()
                dump("oretT", oretT[:].rearrange("p a t -> p (a t)"), [128, 8 * TO], "oretT")
            P.barrier()

        if n_stage >= 5:
            with ExitStack() as S5a:
                Wmg = sb(S5a, "Wmg", [128, 8, 2048], BF16)
                Wa = sb(S5a, "Wa", [128, 4, 1024], BF16)
                Wr = sb(S5a, "Wr", [128, 8, 1024], BF16)
                Wgt = sb(S5a, "Wgt", [128, 8, 1024], BF16)
                load_cast(Wmg[:].rearrange("p k n -> p (k n)"), wmgt, 8 * 2048, "Wmg")
                load_cast(Wa[:].rearrange("p k n -> p (k n)"), wat, 4 * 1024, "Wa")
                load_cast(Wr[:].rearrange("p k n -> p (k n)"), wrt, 8 * 1024, "Wr")
                load_cast(Wgt[:].rearrange("p k n -> p (k n)"), wgtt, 8 * 1024, "Wgt")
                gg8 = sb(S5a, "gg8", [128, 8], F32)
                gb8 = sb(S5a, "gb8", [128, 8], F32)
                P.dma(gg8[:], gng8, writes=["gg8"])
                P.dma(gb8[:], gnb8, writes=["gb8"])
                xb = sb(S5a, "xb5", [128, 8, 512], BF16)
                og = sb(S5a, "og", [128, 8, 512], BF16)
                sgt = [sb(S5a, "sgt%d" % i, [128, 512], F32) for i in range(2)]
                yn = [sb(S5a, "yn%d" % i, [128, 512], F32) for i in range(2)]
                ga2 = [sb(S5a, "ga%d" % i, [128, 512], F32) for i in range(2)]
                gr2 = [sb(S5a, "gr%d" % i, [128, 512], F32) for i in range(2)]
                ma2 = [sb(S5a, "ma%d" % i, [128, 512], F32) for i in range(2)]
                bk = [ps(S5a, "bk%d" % i, [128, 512]) for i in range(8)]
                pgt = [bk[4], bk[5]]
                xTov = xTo.rearrange("(k p) t -> p k t", p=128)
                for oc in range(4):
                    c0 = oc * 512
                    cs_ = slice(c0, c0 + 512)
                    P.dma(wst3[:], xTov[:, :, cs_], writes=["wst"])
                    P.act(lambda e: e.copy(out=xb[:, 0:4, :], in_=wst3[:, 0:4, :]), ["wst"], ["xb5"])
                    P.dve(lambda e: e.tensor_copy(out=xb[:, 4:8, :], in_=wst3[:, 4:8, :]), ["wst"], ["xb5"])
                    for et in range(8):
                        b_ = et % 2
                        for k in range(8):
                            P.pe(lambda e: e.matmul(pgt[b_][:], lhsT=Wgt[:, k, et * 128:(et + 1) * 128], rhs=xb[:, k, :], start=(k == 0), stop=(k == 7)),
                                 ["Wgt", "xb5"], ["bk%d" % (4 + b_)])
                        P.act(lambda e: e.activation(out=sgt[b_][:], in_=pgt[b_][:], func=AF.Silu), ["bk%d" % (4 + b_)], ["sgt%d" % b_])
                        P.act(lambda e: e.activation(out=yn[b_][:], in_=oretT[:, et, cs_], func=AF.Identity, scale=gg8[:, et:et + 1], bias=gb8[:, et:et + 1]),
                              ["oretT", "gg8", "gb8"], ["yn%d" % b_])
                        P.dve(lambda e: e.tensor_tensor(out=og[:, et, :], in0=yn[b_][:], in1=sgt[b_][:], op=ALU.mult), ["yn%d" % b_, "sgt%d" % b_], ["og"])
                    for ct in range(8):
                        cb = (ct % 2) * 4
                        cp = ct % 2
                        pg0, pg1, pu0, pu1 = bk[cb], bk[cb + 1], bk[cb + 2], bk[cb + 3]
                        kg0, kg1, ku0, ku1 = ["bk%d" % (cb + q_) for q_ in range(4)]
                        ga, gr, ma = ga2[cp], gr2[cp], ma2[cp]
                        for k in range(8):
                            P.pe(lambda e: e.matmul(pg0[:], lhsT=Wmg[:, k, ct * 128:(ct + 1) * 128], rhs=xb[:, k, :], start=(k == 0), stop=(k == 7)),
                                 ["Wmg", "xb5"], [kg0])
                        for k in range(8):
                            P.pe(lambda e: e.matmul(pg1[:], lhsT=Wmg[:, k, 1024 + ct * 128:1024 + (ct + 1) * 128], rhs=xb[:, k, :], start=(k == 0), stop=(k == 7)),
                                 ["Wmg", "xb5"], [kg1])
                        for k in range(4):
                            P.pe(lambda e: e.matmul(pu0[:], lhsT=Wa[:, k, ct * 128:(ct + 1) * 128], rhs=oattnT[:, k, cs_], start=(k == 0), stop=(k == 3)),
                                 ["Wa", "oattnT"], [ku0])
                        for k in range(8):
                            P.pe(lambda e: e.matmul(pu1[:], lhsT=Wr[:, k, ct * 128:(ct + 1) * 128], rhs=og[:, k, :], start=(k == 0), stop=(k == 7)),
                                 ["Wr", "og"], [ku1])
                        P.act(lambda e: e.activation(out=ga[:], in_=pg0[:], func=AF.Sigmoid), [kg0], ["ga%d" % cp])
                        P.act(lambda e: e.activation(out=gr[:], in_=pg1[:], func=AF.Sigmoid), [kg1], ["gr%d" % cp])
                        P.dve(lambda e: e.tensor_tensor(out=ma[:], in0=pu0[:], in1=ga[:], op=ALU.mult), [ku0, "ga%d" % cp], ["ma%d" % cp])
                        P.dve(lambda e: e.tensor_tensor(out=gr[:], in0=pu1[:], in1=gr[:], op=ALU.mult), [ku1, "gr%d" % cp], ["gr%d" % cp])
                        P.pool(lambda e: e.tensor_tensor(out=x1T[:, ct, cs_], in0=ma[:], in1=gr[:], op=ALU.add), ["ma%d" % cp, "gr%d" % cp], ["mx%d" % (oc * 4 + t_) for t_ in range(4)])
                dump("mergedT", x1T[:].rearrange("p a t -> p (a t)"), [128, 8 * TO], "mx0")
            P.barrier()
        B_.close()
        A_.close()
        if n_stage >= 5:
            with ExitStack() as S56:
                acc = sb(S56, "acc", [128, 16, 1024], F32)
                lng = sb(S56, "lng", [128, 1024], F32)
                lnb = sb(S56, "lnb", [128, 1024], F32)
                st12 = sb(S56, "st12", [128, 2, 6], F32)
                mv = sb(S56, "mv5", [128, 2], F32)
                rstd = sb(S56, "rstd5", [128, 1], F32)

                def layer_norm(src_ap, src_key, dst_ap, dst_key, tmp_ap, tmp_key, eps=LN_EPS):
                    for hf in range(2):
                        P.dve(lambda e: e.bn_stats(out=st12[:, hf, :], in_=src_ap[:, hf * 512:(hf + 1) * 512]), [src_key], ["st12"])
                    P.dve(lambda e: e.bn_aggr(out=mv[:], in_=st12[:].rearrange("p a s -> p (a s)")), ["st12"], ["mv5"])
                    P.dve(lambda e: e.tensor_scalar(out=rstd[:], in0=mv[:, 1:2], scalar1=eps, scalar2=None, op0=ALU.add), ["mv5"], ["rstd5"])
                    P.act(lambda e: e.activation(out=rstd[:], in_=rstd[:], func=AF.Sqrt), ["rstd5"], ["rstd5"])
                    P.dve(lambda e: e.reciprocal(out=rstd[:], in_=rstd[:]), ["rstd5"], ["rstd5"])
                    P.dve(lambda e: e.scalar_tensor_tensor(out=tmp_ap, in0=src_ap, scalar=mv[:, 0:1], in1=lng[:], op0=ALU.subtract, op1=ALU.mult),
                          [src_key, "mv5", "lng"], [tmp_key])
                    P.dve(lambda e: e.scalar_tensor_tensor(out=dst_ap, in0=tmp_ap, scalar=rstd[:, 0:1], in1=lnb[:], op0=ALU.mult, op1=ALU.add),
                          [tmp_key, "rstd5", "lnb"], [dst_key])

                with ExitStack() as S5b:
                    Wo = sb(S5b, "Wo", [128, 8, 1024], BF16)
                    load_cast(Wo[:].rearrange("p k n -> p (k n)"), wot, 8 * 1024, "Wo")
                    P.dma(lng[:], ln1g.partition_broadcast(128), writes=["lng"])
                    P.dma(lnb[:], ln1b.partition_broadcast(128), writes=["lnb"])
                    xres2 = [sb(S5b, "xres%d" % i_, [128, 1024], F32) for i_ in range(2)]
                    yt2 = [sb(S5b, "yt%d" % i_, [128, 1024], F32) for i_ in range(2)]
                    x1b2 = [sb(S5b, "x1b%d" % i_, [128, 1024], BF16) for i_ in range(2)]
                    pm2 = [[ps(S5b, "pm%d_%d" % (q_, i_), [128, 512]) for i_ in range(2)] for q_ in range(2)]
                    pTr = ps(S5b, "pTr5", [128, 1024], BF16)
                    pend5 = []
                    for i in range(16):
                        q_ = i % 2
                        xres, yt, x1b, pm = xres2[q_], yt2[q_], x1b2[q_], pm2[q_]
                        ts_ = slice(i * 128, (i + 1) * 128)
                        if len(pend5) >= 2:
                            pend5.pop(0)()
                        P.dma(xres[:], xo[ts_, :], writes=["xres%d" % q_])
                        for hf in range(2):
                            for k in range(8):
                                P.pe(lambda e: e.matmul(pm[hf][:], lhsT=x1T[:, k, ts_], rhs=Wo[:, k, hf * 512:(hf + 1) * 512], start=(k == 0), stop=(k == 7)),
                                     ["mx%d" % i, "Wo"], ["pm%d_%d" % (q_, hf)])
                            P.dve(lambda e: e.scalar_tensor_tensor(out=yt[:, hf * 512:(hf + 1) * 512], in0=xres[:, hf * 512:(hf + 1) * 512], scalar=ALPHA,
                                                                   in1=pm[hf][:], op0=ALU.mult, op1=ALU.add), ["xres%d" % q_, "pm%d_%d" % (q_, hf)], ["yt%d" % q_])
                        layer_norm(yt[:], "yt%d" % q_, acc[:, i, :], "acc%d" % i, yt[:], "yt%d" % q_)
                        if ("x1_%d" % i) in debug:
                            dump("x1_%d" % i, acc[:, i, :], [128, 1024], "acc%d" % i)
                        P.pool(lambda e: e.tensor_copy(out=x1b[:], in_=acc[:, i, :]), ["acc%d" % i], ["x1b%d" % q_])

                        def tr5(i=i, q_=q_, x1b=x1b, ts_=ts_):
                            for dt_ in range(8):
                                P.pe(lambda e: e.transpose(out=pTr[:, dt_ * 128:(dt_ + 1) * 128], in_=x1b[:, dt_ * 128:(dt_ + 1) * 128], identity=identb[:]), ["x1b%d" % q_, "identb"], ["pTr5"])
                            P.act(lambda e: e.copy(out=x1T[:, :, ts_], in_=pTr[:].rearrange("p (a t) -> p a t", a=8)), ["pTr5"], ["mx%d" % i])
                        pend5.append(tr5)
                    while pend5:
                        pend5.pop(0)()
                P.barrier()
                if n_stage >= 6:
                    with ExitStack() as S6:
                        P.dma(lng[:], ln2g.partition_broadcast(128), writes=["lng"])
                        P.dma(lnb[:], ln2b.partition_broadcast(128), writes=["lnb"])
                        comb = sb(S6, "comb", [128, 16, 32], F32)
                        wrf = sb(S6, "wrf", [128, 8, 36], F32)
                        wrb = sb(S6, "wrb", [128, 8, 36], BF16)
                        brb = sb(S6, "brb", [128, 36], F32)
                        P.dma(wrf[:].rearrange("p k n -> p (k n)"), wrout, writes=["wrf"])
                        P.dve(lambda e: e.tensor_copy(out=wrb[:], in_=wrf[:]), ["wrf"], ["wrb"])
                        P.dma(brb[:], brout.partition_broadcast(128), writes=["brb"])
                        lg = sb(S6, "lg", [128, 16, 36], F32)
                        gmx = sb(S6, "gmx", [128, 16], F32)
                        gsh = sb(S6, "gsh", [128, 16, 4], F32)
                        gex = sb(S6, "gex", [128, 16, 4], F32)
                        gsum = sb(S6, "gsum", [128, 16], F32)
                        gprob = sb(S6, "gprob", [128, 16], F32)
                        ohg = sb(S6, "ohg", [128, 16, 4], F32)
                        tmp48 = sb(S6, "tmp48", [128, 16, 4, 8], F32)
                        isel = sb(S6, "isel", [128, 16, 8], F32)
                        isel2 = sb(S6, "isel2", [128, 16, 8], F32)
                        eq0 = sb(S6, "eq0", [128, 16, 8], F32)
                        eq1 = sb(S6, "eq1", [128, 16, 8], F32)
                        m0 = sb(S6, "m0r", [128, 16], F32)
                        m1 = sb(S6, "m1r", [128, 16], F32)
                        dlt = sb(S6, "dlt", [128, 16], F32)
                        w2e = sb(S6, "w2e", [128, 16], F32)
                        wsum = sb(S6, "wsum", [128, 16], F32)
                        wt1 = sb(S6, "wt1", [128, 16], F32)
                        wt2 = sb(S6, "wt2", [128, 16], F32)
                        ce = sb(S6, "ce", [128, 16, 8], F32)
                        ce2 = sb(S6, "ce2", [128, 16, 8], F32)
                        SR = ExitStack()
                        plg = [ps(SR, "plg%d" % i_, [128, 8, 36]) for i_ in range(2)]
                        AXX = mybir.AxisListType.X

                        def b3(ap, n):
                            return ap.unsqueeze(2).broadcast_to([128, 16, n])
                        for i in range(16):
                            ts_ = slice(i * 128, (i + 1) * 128)
                            for k in range(8):
                                P.pe(lambda e: e.matmul(plg[i // 8][:, i % 8, :], lhsT=x1T[:, k, ts_], rhs=wrb[:, k, :], start=(k == 0), stop=(k == 7)),
                                     ["mx%d" % i, "wrb"], ["plg%d" % (i // 8)])
                        for hf in range(2):
                            P.dve(lambda e: e.tensor_tensor(out=lg[:, hf * 8:(hf + 1) * 8, :], in0=plg[hf][:], in1=brb[:].unsqueeze(1).broadcast_to([128, 8, 36]), op=ALU.add),
                                  ["plg%d" % hf, "brb"], ["lg"])
                        P.dve(lambda e: e.tensor_reduce(out=gmx[:], in_=lg[:, :, 0:4], axis=AXX, op=ALU.max), ["lg"], ["gmx"])
                        P.dve(lambda e: e.tensor_tensor(out=gsh[:], in0=lg[:, :, 0:4], in1=b3(gmx[:], 4), op=ALU.subtract), ["lg", "gmx"], ["gsh"])
                        P.act(lambda e: e.activation(out=gex[:].rearrange("p t g -> p (t g)"), in_=gsh[:].rearrange("p t g -> p (t g)"), func=AF.Exp), ["gsh"], ["gex"])
                        P.dve(lambda e: e.tensor_reduce(out=gsum[:], in_=gex[:], axis=AXX, op=ALU.add), ["gex"], ["gsum"])
                        P.dve(lambda e: e.reciprocal(out=gprob[:], in_=gsum[:]), ["gsum"], ["gprob"])
                        P.dve(lambda e: e.tensor_scalar(out=ohg[:].rearrange("p t g -> p (t g)"), in0=gsh[:].rearrange("p t g -> p (t g)"), scalar1=0.0, scalar2=None, op0=ALU.is_ge),
                              ["gsh"], ["ohg"])
                        P.dve(lambda e: e.tensor_tensor(out=tmp48[:], in0=lg[:, :, 4:36].rearrange("p t (g e) -> p t g e", g=4),
                                                        in1=ohg[:].unsqueeze(3).broadcast_to([128, 16, 4, 8]), op=ALU.mult), ["lg", "ohg"], ["tmp48"])
                        P.dve(lambda e: e.tensor_reduce(out=isel[:], in_=tmp48[:].rearrange("p t g e -> p t e g"), axis=AXX, op=ALU.add), ["tmp48"], ["isel"])
                        P.dve(lambda e: e.tensor_reduce(out=m0[:], in_=isel[:], axis=AXX, op=ALU.max), ["isel"], ["m0r"])
                        P.dve(lambda e: e.tensor_tensor(out=eq0[:], in0=isel[:], in1=b3(m0[:], 8), op=ALU.is_equal), ["isel", "m0r"], ["eq0"])
                        P.dve(lambda e: e.scalar_tensor_tensor(out=isel2[:].rearrange("p t e -> p (t e)"), in0=eq0[:].rearrange("p t e -> p (t e)"), scalar=-1.0e30,
                                                               in1=isel[:].rearrange("p t e -> p (t e)"), op0=ALU.mult, op1=ALU.add), ["eq0", "isel"], ["isel2"])
                        P.dve(lambda e: e.tensor_reduce(out=m1[:], in_=isel2[:], axis=AXX, op=ALU.max), ["isel2"], ["m1r"])
                        P.dve(lambda e: e.tensor_tensor(out=eq1[:], in0=isel2[:], in1=b3(m1[:], 8), op=ALU.is_equal), ["isel2", "m1r"], ["eq1"])
                        P.dve(lambda e: e.tensor_tensor(out=dlt[:], in0=m1[:], in1=m0[:], op=ALU.subtract), ["m1r", "m0r"], ["dlt"])
                        P.act(lambda e: e.activation(out=w2e[:], in_=dlt[:], func=AF.Exp), ["dlt"], ["w2e"])
                        P.dve(lambda e: e.tensor_scalar(out=wsum[:], in0=w2e[:], scalar1=1.0, scalar2=None, op0=ALU.add), ["w2e"], ["wsum"])
                        P.dve(lambda e: e.reciprocal(out=wsum[:], in_=wsum[:]), ["wsum"], ["wsum"])
                        P.dve(lambda e: e.tensor_tensor(out=wt1[:], in0=wsum[:], in1=gprob[:], op=ALU.mult), ["wsum", "gprob"], ["wt1"])
                        P.dve(lambda e: e.tensor_tensor(out=wt2[:], in0=wt1[:], in1=w2e[:], op=ALU.mult), ["wt1", "w2e"], ["wt2"])
                        P.dve(lambda e: e.tensor_tensor(out=ce[:], in0=eq0[:], in1=b3(wt1[:], 8), op=ALU.mult), ["eq0", "wt1"], ["ce"])
                        P.dve(lambda e: e.tensor_tensor(out=ce2[:], in0=eq1[:], in1=b3(wt2[:], 8), op=ALU.mult), ["eq1", "wt2"], ["ce2"])
                        P.dve(lambda e: e.tensor_tensor(out=ce[:], in0=ce[:], in1=ce2[:], op=ALU.add), ["ce", "ce2"], ["ce"])
                        P.dve(lambda e: e.tensor_tensor(out=comb[:].rearrange("p t (g e) -> p t g e", g=4), in0=ce[:].unsqueeze(2).broadcast_to([128, 16, 4, 8]),
                                                        in1=ohg[:].unsqueeze(3).broadcast_to([128, 16, 4, 8]), op=ALU.mult), ["ce", "ohg"], ["comb"])
                        P.dve(lambda e: e.tensor_scalar(out=comb[:].rearrange("p a e -> p (a e)"), in0=comb[:].rearrange("p a e -> p (a e)"), scalar1=1.0 / ALPHA, scalar2=None, op0=ALU.mult),
                              ["comb"], ["comb"])
                        dump("comb", comb[:].rearrange("p a e -> p (a e)"), [128, 512], "comb")
                        SR.close()
                        P.barrier()
                        wstE = [wst[:, 0:2048], wst[:, 2048:4096]]
                        wE = [sb(S6, "wE%d" % i, [128, 12288], BF16) for i in range(2)]
                        sgE = [sb(S6, "sgE%d" % i, [128, 512], F32) for i in range(2)]
                        hT = [sb(S6, "hT%d" % i, [128, 4, 512], BF16) for i in range(2)]
                        pG = [ps(S6, "pG%d" % i, [128, 512]) for i in range(2)]
                        pU = [ps(S6, "pU%d" % i, [128, 512]) for i in range(2)]
                        pO = [ps(S6, "pO%d" % i, [128, 512]) for i in range(3)]
                        wsr = Ring([0, 1])
                        gr_ = Ring([0, 1])
                        or_ = Ring([0, 1, 2])
                        crr = [0]
                        n_exp = DEBUG.get("n_exp", 32)

                        def load_expert(ex):
                            ws = ex % 2
                            for pc in range(6):
                                si = wsr.next()
                                P.dma(wstE[si], wexp[ex, :, pc * 2048:(pc + 1) * 2048], writes=["wstE%d" % si])
                                d = wE[ws][:, pc * 2048:(pc + 1) * 2048]
                                P.pool(lambda e: e.tensor_copy(out=d, in_=wstE[si]), ["wstE%d" % si], ["wE%d" % ws])

                        def gu_phase(ex, cth):
                            ws = ex % 2
                            Wg = wE[ws][:, 0:4096].rearrange("p (k n) -> p k n", k=8)
                            Wu = wE[ws][:, 4096:8192].rearrange("p (k n) -> p k n", k=8)
                            wkey = "wE%d" % ws
                            cs_ = slice(cth * 512, (cth + 1) * 512)
                            xkeys = ["mx%d" % (cth * 4 + t_) for t_ in range(4)]
                            hs_ = cth % 2
                            for ft in range(4):
                                gi = gr_.next()
                                for k in range(8):
                                    P.pe(lambda e: e.matmul(pG[gi][:], lhsT=Wg[:, k, ft * 128:(ft + 1) * 128], rhs=x1T[:, k, cs_], start=(k == 0), stop=(k == 7)),
                                         [wkey] + xkeys, ["pG%d" % gi])
                                for k in range(8):
                                    P.pe(lambda e: e.matmul(pU[gi][:], lhsT=Wu[:, k, ft * 128:(ft + 1) * 128], rhs=x1T[:, k, cs_], start=(k == 0), stop=(k == 7)),
                                         [wkey] + xkeys, ["pU%d" % gi])
                                P.act(lambda e: e.activation(out=sgE[gi][:], in_=pG[gi][:], func=AF.Silu), ["pG%d" % gi], ["sgE%d" % gi])
                                P.dve(lambda e: e.tensor_tensor(out=hT[hs_][:, ft, :], in0=pU[gi][:], in1=sgE[gi][:], op=ALU.mult),
                                      ["pU%d" % gi, "sgE%d" % gi], ["hT%d" % hs_])

                        def d_phase(ex, cth):
                            ws = ex % 2
                            Wd = wE[ws][:, 8192:12288].rearrange("p (k n) -> p k n", k=4)
                            wkey = "wE%d" % ws
                            hs_ = cth % 2
                            for tt in range(4):
                                ti = cth * 4 + tt
                                for hf in range(2):
                                    oi = or_.next()
                                    for ft in range(4):
                                        P.pe(lambda e: e.matmul(pO[oi][:], lhsT=hT[hs_][:, ft, tt * 128:(tt + 1) * 128],
                                                                rhs=Wd[:, ft, hf * 512:(hf + 1) * 512], start=(ft == 0), stop=(ft == 3)),
                                             [wkey, "hT%d" % hs_], ["pO%d" % oi])
                                    a_ = acc[:, ti, hf * 512:(hf + 1) * 512]
                                    P.dve(lambda e: e.scalar_tensor_tensor(out=a_, in0=pO[oi][:], scalar=comb[:, ti, ex:ex + 1], in1=a_, op0=ALU.mult, op1=ALU.add),
                                          ["pO%d" % oi, "comb", "acc%d" % ti], ["acc%d" % ti])

                        units = [(ex, cth) for ex in range(n_exp) for cth in range(4)]
                        load_expert(0)
                        if n_exp > 1:
                            load_expert(1)
                        gu_phase(*units[0])
                        for u, (ex, cth) in enumerate(units):
                            if u + 1 < len(units):
                                gu_phase(*units[u + 1])
                            d_phase(ex, cth)
                            if cth == 3 and ex + 2 < n_exp:
                                load_expert(ex + 2)
                        yo = [sb(S6, "yo%d" % i, [128, 1024], F32) for i in range(2)]
                        for i in range(16):
                            yi = i % 2
                            layer_norm(acc[:, i, :], "acc%d" % i, yo[yi][:], "yo%d" % yi, yo[yi][:], "yo%d" % yi, eps=LN_EPS / (ALPHA * ALPHA))
                            P.dma(out[i * 128:(i + 1) * 128, :], yo[yi][:], reads=["yo%d" % yi], writes=["out%d" % yi], sem="outd%d" % yi)
        fin_reads = ["out0", "out1"] + ["dbg_" + n for n in dbg_out]
        P.add("sp", lambda e: e.nop(), reads=fin_reads, sem="fin")
        if n_stage < 6:
            pass
        cnt = P.emit(G)
        nsem = len(cnt)
    return nc, dbg_out, nsem


def prep_shared(inp):
    f = lambda a: np.ascontiguousarray(np.asarray(a, dtype=np.float32))
    w_in = f(inp["w_in"])[0]
    sh = {}
    q = w_in[:, 0:512].reshape(1024, 2, 4, 64).transpose(0, 2, 1, 3).reshape(1024, 512)
    w1 = np.concatenate([q, w_in[:, 512:640], w_in[:, 640:768], w_in[:, 768:896], w_in[:, 1024:1152],
                         w_in[:, 896:1024], w_in[:, 1152:1280], w_in[:, 1280:1304]], axis=1)
    sh["w1t"] = tile_w(w1)
    sh["w4t"] = tile_w(w_in[:, 1304:3352])
    sh["wgtt"] = tile_w(w_in[:, 3352:4376])
    sh["wmgt"] = tile_w(w_in[:, 4376:6424])
    lit = {"k": (inp["cmp_k_w1"], inp["cmp_k_b1"], inp["cmp_pos_k"]), "v": (inp["cmp_v_w1"], inp["cmp_v_b1"], inp["cmp_pos_v"])}
    for kv in "kv":
        cw1 = f(lit[kv][0])[0]
        r = cw1.reshape(32, 64, 256).transpose(1, 0, 2).reshape(64, 32 * 256)
        sh["cw1" + kv] = np.ascontiguousarray(np.concatenate([r, r], axis=0))
        pos = f(lit[kv][2])[0]
        sh["cpos" + kv] = np.ascontiguousarray(np.concatenate([pos.T, pos.T], axis=0))
        sh["cb1" + kv] = np.ascontiguousarray(f(lit[kv][1])[0].reshape(2, 128).T)
    w2k = f(inp["cmp_k_w2"])[0]
    sh["cw2k"] = tile_w(np.concatenate([w2k, w2k], axis=1))
    sh["cw2v"] = tile_w(f(inp["cmp_v_w2"])[0])
    b2k = f(inp["cmp_k_b2"])[0]
    sh["cb2k"] = np.ascontiguousarray(np.concatenate([b2k, b2k])[:, None])
    sh["cb2v"] = f(inp["cmp_v_b2"])[0]
    sh["gng8"] = np.ascontiguousarray(f(inp["ret_gn_g"])[0].reshape(8, 128).T)
    sh["gnb8"] = np.ascontiguousarray(f(inp["ret_gn_b"])[0].reshape(8, 128).T)
    sh["wat"] = tile_w(f(inp["w_up_attn"])[0])
    sh["wrt"] = tile_w(f(inp["w_up_ret"])[0])
    sh["wot"] = tile_w(f(inp["w_out"])[0])
    for n in ("ln1_g", "ln1_b", "ln2_g", "ln2_b"):
        sh[n.replace("_", "")] = f(inp[n])[0]
    rg = f(inp["router_group_w"])[0]
    ri = f(inp["router_inner_w"])[0]
    sh["wrout"] = tile_w(np.concatenate([rg, ri.transpose(1, 0, 2).reshape(1024, 32)], axis=1))
    sh["brout"] = np.ascontiguousarray(np.concatenate([f(inp["router_group_b"])[0], f(inp["router_inner_b"])[0].reshape(32)]))
    wg = f(inp["expert_w_gate"])[0]
    wu = f(inp["expert_w_up"])[0]
    wd = f(inp["expert_w_down"])[0]
    we = np.empty((32, 128, 12288), np.float32)
    we[:, :, 0:4096] = wg.reshape(32, 8, 128, 512).transpose(0, 2, 1, 3).reshape(32, 128, 4096)
    we[:, :, 4096:8192] = wu.reshape(32, 8, 128, 512).transpose(0, 2, 1, 3).reshape(32, 128, 4096)
    we[:, :, 8192:12288] = wd.reshape(32, 4, 128, 1024).transpose(0, 2, 1, 3).reshape(32, 128, 4096)
    sh["wexp"] = we
    return sh


def make_in_maps(inp):
    sh = prep_shared(inp)
    x = np.asarray(inp["x"], dtype=np.float32)
    maps = []
    for core in range(8):
        b, c = core // 2, core % 2
        m = dict(sh)
        xb = x[b]
        own = xb.reshape(16, 2, 128, 1024)[:, c].reshape(TO, 1024)
        m["xT"] = np.ascontiguousarray(xb.T)
        m["xTo"] = np.ascontiguousarray(own.T)
        m["xo"] = np.ascontiguousarray(own)
        for k, v in make_consts(c).items():
            if not k.startswith("_"):
                m["c_" + k] = v
        maps.append(m)
    return maps


_PROG_CACHE = {}


def kernel(**inputs):
    if "prog" not in _PROG_CACHE:
        _PROG_CACHE["prog"] = build_program()
    nc, _, _ = _PROG_CACHE["prog"]
    maps = make_in_maps(inputs)
    res = run_bass_kernel_spmd(nc, maps, core_ids=list(range(8)))
    outp = np.empty((4, 16, 2, 128, 1024), np.float32)
    for core in range(8):
        b, c = core // 2, core % 2
        outp[b, :, c] = res.results[core]["out"].reshape(16, 128, 1024)
    return outp.reshape(4, T, 1024)
```
